# Optimizing a Trainium2 kernel written in Bass

```python
import jax, jax.numpy as jnp
from jax import lax
import numpy as np

D_MODEL = 1024
BATCH = 4
SEQ = 4096
DEPTH = 2

GRID_W = 64
CTX_LEN = 256
GLA_HEADS = 4
GLA_DK = 64
GLA_DV = 128
GLA_RANK = 16
GLA_TAU = 16.0
NA_HEADS = 8
NA_DH = 64
NA_WIN_R = 8
NA_WIN_C = 16
WA_HEADS = 8
WA_KV_HEADS = 2
WA_DH = 64
WA_RADIUS = 128
WA_BLOCK = 128
ROPE_BASE = 10000.0
HG_HEADS = 4
HG_DK = 128
HG_DV = 128
CHUNK = 64
N_BRANCH = 4
BRANCH_W = 512
N_GROUPS = 4
EXPERTS_PER_GROUP = 8
EXPERT_HIDDEN = 256
TOP_K = 2
LN_EPS = 1e-5

IN_COLS = (
    ("a_q", GLA_HEADS * GLA_DK), ("a_k", GLA_HEADS * GLA_DK), ("a_v", GLA_HEADS * GLA_DV),
    ("a_g", GLA_HEADS * GLA_DV), ("a_lr", 2 * GLA_RANK),
    ("b_q", NA_HEADS * NA_DH), ("b_k", NA_HEADS * NA_DH), ("b_v", NA_HEADS * NA_DH),
    ("c_q", WA_HEADS * WA_DH), ("c_k", WA_KV_HEADS * WA_DH), ("c_v", WA_KV_HEADS * WA_DH),
    ("d_q", HG_HEADS * HG_DK), ("d_f", 2 * HG_HEADS * HG_DK), ("d_i", HG_HEADS * HG_DV),
    ("d_g", HG_HEADS * HG_DV), ("gate", N_BRANCH * D_MODEL),
)
D_IN = sum(s for _, s in IN_COLS)

kernel_name = "hybrid_gla_natten_swa_hgrn2_hmoe_dit"

F32 = jnp.float32


def _split_cols(z):
    offs = np.cumsum([s for _, s in IN_COLS])[:-1].tolist()
    return dict(zip([n for n, _ in IN_COLS], jnp.split(z, offs, axis=-1)))


def _heads(t, n):
    b, l, w = t.shape
    return t.reshape(b, l, n, w // n).transpose(0, 2, 1, 3)


def _merge(t):
    b, h, l, d = t.shape
    return t.transpose(0, 2, 1, 3).reshape(b, l, h * d)


def _layer_norm(t, g, b):
    tf = t.astype(F32)
    mu = tf.mean(-1, keepdims=True)
    var = jnp.square(tf - mu).mean(-1, keepdims=True)
    return ((tf - mu) * lax.rsqrt(var + LN_EPS) * g + b).astype(t.dtype)


def _head_rms(o):
    of = o.astype(F32)
    return of * lax.rsqrt(jnp.mean(of * of, -1, keepdims=True) + 1e-6)


def _rope_2d(t):
    l, dh = t.shape[2], t.shape[3]
    half = dh // 2
    nf = half // 2
    pos = jnp.arange(l)
    inv = ROPE_BASE ** (-jnp.arange(nf, dtype=F32) / nf)

    def rot(u, p):
        ang = p.astype(F32)[:, None] * inv
        cos, sin = jnp.cos(ang), jnp.sin(ang)
        u1, u2 = u[..., :nf], u[..., nf:]
        return jnp.concatenate([u1 * cos - u2 * sin, u1 * sin + u2 * cos], -1)

    out = jnp.concatenate([rot(t[..., :half], pos // GRID_W), rot(t[..., half:], pos % GRID_W)], -1)
    return out.astype(t.dtype)


def _chunked_gated_scan(q, k, v, g, s0):
    b, h, l, dk = q.shape
    n = l // CHUNK

    def to_chunks(t):
        return jnp.moveaxis(t.reshape(b, h, n, CHUNK, t.shape[-1]), 2, 0)

    causal = jnp.tril(jnp.ones((CHUNK, CHUNK), bool))

    def step(s, inp):
        qi, ki, vi, gi = inp
        bc = jnp.cumsum(gi.astype(F32), axis=2)
        diff = bc[:, :, :, None, :] - bc[:, :, None, :, :]
        decay = jnp.exp(jnp.where(causal[:, :, None], diff, -jnp.inf))
        a = jnp.einsum("bhid,bhjd,bhijd->bhij", qi, ki, decay)
        o = jnp.einsum("bhij,bhjv->bhiv", a, vi) + jnp.einsum("bhid,bhdv->bhiv", qi * jnp.exp(bc), s)
        b_last = bc[:, :, -1:, :]
        s_new = jnp.exp(b_last[:, :, 0, :])[..., None] * s + jnp.einsum(
            "bhjd,bhjv->bhdv", ki * jnp.exp(b_last - bc), vi)
        return s_new, o

    s_fin, oc = lax.scan(step, s0, (to_chunks(q), to_chunks(k), to_chunks(v), to_chunks(g)))
    return jnp.moveaxis(oc, 0, 2).reshape(b, h, l, v.shape[-1]), s_fin


def _bidir_scan(q, k_fb, v, g_fb, qc, kc_fb, vc, gc_fb):
    b, h, _, dk = q.shape
    s0 = jnp.zeros((b, h, dk, v.shape[-1]), F32)
    flip = lambda t: jnp.flip(t, axis=2)
    oc_f, sc_f = _chunked_gated_scan(qc, kc_fb[0], vc, gc_fb[0], s0)
    oc_b, sc_b = _chunked_gated_scan(flip(qc), flip(kc_fb[1]), flip(vc), flip(gc_fb[1]), s0)
    o_f, _ = _chunked_gated_scan(q, k_fb[0], v, g_fb[0], sc_f)
    o_b, _ = _chunked_gated_scan(flip(q), flip(k_fb[1]), flip(v), flip(g_fb[1]), sc_b)
    return o_f + flip(o_b), oc_f + flip(oc_b)


def _gla_branch(p, pc, lr_w, lr_b, with_ctx):
    def prep(pp):
        q = _heads(pp["a_q"], GLA_HEADS) * (GLA_DK ** -0.5)
        k = _heads(pp["a_k"], GLA_HEADS)
        v = _heads(pp["a_v"], GLA_HEADS)
        lr = pp["a_lr"]
        lr = lr.reshape(lr.shape[:-1] + (2, GLA_RANK))
        logit = jnp.einsum("blzr,zrk->zblk", lr, lr_w) + lr_b[:, None, None, :]
        g = jax.nn.log_sigmoid(logit.astype(F32)) / GLA_TAU
        return q, (k, k), v, (_heads(g[0], GLA_HEADS), _heads(g[1], GLA_HEADS))

    o, oc = _bidir_scan(*prep(p), *prep(pc))

    def out(oo, pp):
        return (_merge(_head_rms(oo)) * jax.nn.silu(pp["a_g"].astype(F32))).astype(pp["a_g"].dtype)

    return out(o, p), (out(oc, pc) if with_ctx else None)


def _hgrn2_branch(p, pc, lb, with_ctx):
    def prep(pp):
        q = jax.nn.silu(_heads(pp["d_q"], HG_HEADS))
        f = pp["d_f"]
        f = f.reshape(f.shape[:-1] + (2, HG_HEADS * HG_DK)).astype(F32)
        log_f = jnp.logaddexp(jnp.log(lb), jnp.log1p(-lb) + jax.nn.log_sigmoid(f))
        k = -jnp.expm1(log_f)
        v = _heads(pp["d_i"], HG_HEADS)
        kk = (_heads(k[:, :, 0], HG_HEADS), _heads(k[:, :, 1], HG_HEADS))
        gg = (_heads(log_f[:, :, 0], HG_HEADS), _heads(log_f[:, :, 1], HG_HEADS))
        return q, kk, v, gg

    o, oc = _bidir_scan(*prep(p), *prep(pc))

    def out(oo, pp):
        return (_merge(_head_rms(oo)) * jax.nn.silu(pp["d_g"].astype(F32))).astype(pp["d_g"].dtype)

    return out(o, p), (out(oc, pc) if with_ctx else None)


def _ctx_attention(q, k, v, sink=None):
    b, hq, lc, dh = q.shape
    g = k.shape[1]
    qg = q.reshape(b, g, hq // g, lc, dh) * (dh ** -0.5)
    s = jnp.einsum("bgrid,bgjd->bgrij", qg, k).astype(F32)
    if sink is not None:
        s_sink = jnp.broadcast_to(sink.reshape(1, g, hq // g, 1, 1).astype(F32), s.shape[:-1] + (1,))
        s = jnp.concatenate([s, s_sink], -1)
    pr = jax.nn.softmax(s, -1)[..., :lc]
    o = jnp.einsum("bgrij,bgjd->bgrid", pr, v)
    return o.reshape(b, hq, lc, dh).astype(q.dtype)


def _neighborhood_attention(q, k, v, kc, vc, rpb):
    b, h, l, dh = q.shape
    rows = l // GRID_W
    wr = min(NA_WIN_R, rows)
    qg = q.reshape(b, h, rows, GRID_W, dh) * (dh ** -0.5)
    kg = k.reshape(b, h, rows, GRID_W, dh)
    vg = v.reshape(b, h, rows, GRID_W, dh)
    cols = np.arange(GRID_W)
    cs = np.clip(cols - NA_WIN_C // 2, 0, GRID_W - NA_WIN_C)
    col_idx = cs[:, None] + np.arange(NA_WIN_C)[None, :]
    col_bias_idx = col_idx - cols[:, None] + NA_WIN_C - 1
    rpb_cols = rpb[:, :, col_bias_idx]
    n_loc = wr * NA_WIN_C

    def one_row(r):
        rs = jnp.clip(r - wr // 2, 0, rows - wr)
        k_win = lax.dynamic_slice_in_dim(kg, rs, wr, axis=2)[:, :, :, col_idx]
        v_win = lax.dynamic_slice_in_dim(vg, rs, wr, axis=2)[:, :, :, col_idx]
        q_r = lax.dynamic_index_in_dim(qg, r, axis=2, keepdims=False)
        row_off = rs + jnp.arange(wr) - r + NA_WIN_R - 1
        bias = jnp.transpose(jnp.take(rpb_cols, row_off, axis=1), (0, 2, 1, 3))
        s_loc = (jnp.einsum("bhcd,bhrcwd->bhcrw", q_r, k_win) + bias[None]).astype(F32)
        s_ctx = jnp.einsum("bhcd,bhnd->bhcn", q_r, kc).astype(F32)
        pr = jax.nn.softmax(jnp.concatenate([s_loc.reshape(b, h, GRID_W, n_loc), s_ctx], -1), -1)
        p_loc = pr[..., :n_loc].reshape(b, h, GRID_W, wr, NA_WIN_C)
        o = jnp.einsum("bhcrw,bhrcwd->bhcd", p_loc, v_win) + jnp.einsum("bhcn,bhnd->bhcd", pr[..., n_loc:], vc)
        return o.astype(q.dtype)

    out = lax.map(one_row, jnp.arange(rows))
    return jnp.transpose(out, (1, 2, 0, 3, 4)).reshape(b, h, l, dh)


def _window_attention(q, k, v, kc, vc, sink):
    b, hq, l, dh = q.shape
    g = k.shape[1]
    rep = hq // g
    nb = l // WA_BLOCK
    lc = kc.shape[2]
    qb = q.reshape(b, g, rep, nb, WA_BLOCK, dh) * (dh ** -0.5)

    def band(t):
        tb = t.reshape(b, g, nb, WA_BLOCK, dh)
        tp = jnp.pad(tb, ((0, 0), (0, 0), (1, 1), (0, 0), (0, 0)))
        return jnp.concatenate([tp[:, :, :-2], tp[:, :, 1:-1], tp[:, :, 2:]], axis=3)

    kb, vb = band(k), band(v)
    qpos = np.arange(l).reshape(nb, WA_BLOCK)
    kpos = (np.arange(nb)[:, None] - 1) * WA_BLOCK + np.arange(3 * WA_BLOCK)[None, :]
    valid = ((np.abs(kpos[:, None, :] - qpos[:, :, None]) <= WA_RADIUS)
             & (kpos[:, None, :] >= 0) & (kpos[:, None, :] < l))
    s_loc = jnp.where(valid, jnp.einsum("bgrnid,bgnjd->bgrnij", qb, kb).astype(F32), -jnp.inf)
    s_ctx = jnp.einsum("bgrnid,bgcd->bgrnic", qb, kc).astype(F32)
    s_sink = jnp.broadcast_to(sink.reshape(1, g, rep, 1, 1, 1).astype(F32), s_loc.shape[:-1] + (1,))
    pr = jax.nn.softmax(jnp.concatenate([s_loc, s_ctx, s_sink], -1), -1)
    n_loc = 3 * WA_BLOCK
    o = (jnp.einsum("bgrnij,bgnjd->bgrnid", pr[..., :n_loc], vb)
         + jnp.einsum("bgrnic,bgcd->bgrnid", pr[..., n_loc:n_loc + lc], vc))
    return o.reshape(b, hq, l, dh).astype(q.dtype)


def _na_branch(p, pc, rpb, with_ctx):
    q, k, v = (_heads(p[n], NA_HEADS) for n in ("b_q", "b_k", "b_v"))
    kc, vc = _heads(pc["b_k"], NA_HEADS), _heads(pc["b_v"], NA_HEADS)
    o = _merge(_neighborhood_attention(q, k, v, kc, vc, rpb))
    oc = _merge(_ctx_attention(_heads(pc["b_q"], NA_HEADS), kc, vc)) if with_ctx else None
    return o, oc


def _wa_branch(p, pc, sink, with_ctx):
    q = _rope_2d(_heads(p["c_q"], WA_HEADS))
    k = _rope_2d(_heads(p["c_k"], WA_KV_HEADS))
    v = _heads(p["c_v"], WA_KV_HEADS)
    kc, vc = _heads(pc["c_k"], WA_KV_HEADS), _heads(pc["c_v"], WA_KV_HEADS)
    o = _merge(_window_attention(q, k, v, kc, vc, sink))
    oc = _merge(_ctx_attention(_heads(pc["c_q"], WA_HEADS), kc, vc, sink)) if with_ctx else None
    return o, oc


def _merge_branches(gate_pre, branches, w_br):
    gates = jax.nn.sigmoid(gate_pre.astype(F32)).astype(gate_pre.dtype)
    out = gates[..., :D_MODEL] * (branches[0] @ w_br[0])
    for i in range(1, N_BRANCH):
        out = out + gates[..., i * D_MODEL:(i + 1) * D_MODEL] * (branches[i] @ w_br[i])
    return out


def _hier_moe(h, w_grp, b_grp, w_er, b_er, w_gate, w_up, w_down):
    g_logits = (h @ w_grp + b_grp).astype(F32)
    g_top, g_idx = lax.top_k(g_logits, 1)
    g_weight = jnp.exp(g_top[..., 0] - jax.nn.logsumexp(g_logits, -1))
    e_logits = (h @ w_er + b_er).astype(F32)
    e_logits = e_logits.reshape(e_logits.shape[:-1] + (N_GROUPS, EXPERTS_PER_GROUP))
    g_onehot = jax.nn.one_hot(g_idx[..., 0], N_GROUPS, dtype=F32)
    e_in = jnp.einsum("...ge,...g->...e", e_logits, g_onehot)
    e_top, e_idx = lax.top_k(e_in, TOP_K)
    e_w = jax.nn.softmax(e_top, -1)
    local = jnp.sum(e_w[..., None] * jax.nn.one_hot(e_idx, EXPERTS_PER_GROUP, dtype=F32), axis=-2)
    out = None
    for gi in range(N_GROUPS):
        wg = (jnp.where(g_idx[..., 0] == gi, g_weight, 0.0)[..., None] * local).astype(h.dtype)
        hid = (jax.nn.silu(jnp.einsum("bld,edf->blef", h, w_gate[gi]))
               * jnp.einsum("bld,edf->blef", h, w_up[gi]))
        y = jnp.einsum("blef,efd->bld", hid * wg[..., None], w_down[gi])
        out = y if out is None else out + y
    return out


def setup_inputs(seed: int = 0) -> dict:
    key = jax.random.key(seed)
    ks = jax.random.split(key, 24)
    d = D_MODEL
    beta = (8.0 * DEPTH) ** -0.25
    nrm = lambda k, shape, scale: jax.random.normal(k, shape, F32) * scale
    n_exp = N_GROUPS * EXPERTS_PER_GROUP
    return {
        "x": nrm(ks[0], (BATCH, SEQ, d), 1.0),
        "c": nrm(ks[1], (BATCH, d), 1.0),
        "ctx": nrm(ks[2], (BATCH, CTX_LEN, d), 1.0),
        "c_ctx": nrm(ks[3], (d,), 1.0),
        "w_mod": nrm(ks[4], (DEPTH, d, 6 * d), 0.5 * d ** -0.5),
        "b_mod": nrm(ks[5], (DEPTH, 6 * d), 0.01),
        "w_in": nrm(ks[6], (DEPTH, d, D_IN), d ** -0.5),
        "gla_lr_w": nrm(ks[7], (DEPTH, 2, GLA_RANK, GLA_HEADS * GLA_DK), GLA_RANK ** -0.5),
        "gla_lr_b": nrm(ks[8], (DEPTH, 2, GLA_HEADS * GLA_DK), 0.1),
        "hg_gamma": nrm(ks[9], (2, DEPTH, HG_HEADS * HG_DK), 0.5),
        "na_rpb": nrm(ks[10], (DEPTH, NA_HEADS, 2 * NA_WIN_R - 1, 2 * NA_WIN_C - 1), 0.02),
        "wa_sink": nrm(ks[11], (DEPTH, WA_HEADS), 0.5),
        "w_branch": nrm(ks[12], (DEPTH, N_BRANCH, BRANCH_W, d), BRANCH_W ** -0.5),
        "w_out": nrm(ks[13], (DEPTH, d, d), beta * d ** -0.5),
        "ln_g": 1.0 + nrm(ks[14], (DEPTH, 2, d), 0.01),
        "ln_b": nrm(ks[15], (DEPTH, 2, d), 0.01),
        "moe_w_group": nrm(ks[16], (DEPTH, d, N_GROUPS), d ** -0.5),
        "moe_b_group": nrm(ks[17], (DEPTH, N_GROUPS), 0.01),
        "moe_w_expert": nrm(ks[18], (DEPTH, d, n_exp), d ** -0.5),
        "moe_b_expert": nrm(ks[19], (DEPTH, n_exp), 0.01),
        "moe_w_gate": nrm(ks[20], (DEPTH, N_GROUPS, EXPERTS_PER_GROUP, d, EXPERT_HIDDEN), d ** -0.5),
        "moe_w_up": nrm(ks[21], (DEPTH, N_GROUPS, EXPERTS_PER_GROUP, d, EXPERT_HIDDEN), d ** -0.5),
        "moe_w_down": nrm(ks[22], (DEPTH, N_GROUPS, EXPERTS_PER_GROUP, EXPERT_HIDDEN, d),
                          beta * EXPERT_HIDDEN ** -0.5),
    }


def reference(x, c, ctx, c_ctx, w_mod, b_mod, w_in, gla_lr_w, gla_lr_b, hg_gamma, na_rpb, wa_sink,
              w_branch, w_out, ln_g, ln_b, moe_w_group, moe_b_group, moe_w_expert, moe_b_expert,
              moe_w_gate, moe_w_up, moe_w_down):
    alpha = (2.0 * DEPTH) ** 0.25
    cum = jnp.cumsum(jax.nn.softmax(hg_gamma.astype(F32), axis=1), axis=1)
    lower_bounds = cum - cum[:, :1]
    sc = jax.nn.silu(c)
    scc = jax.nn.silu(c_ctx)
    cx = ctx
    for l in range(DEPTH):
        with_ctx = l < DEPTH - 1
        mod = jnp.split(sc @ w_mod[l] + b_mod[l], 6, axis=-1)
        modc = jnp.split(scc @ w_mod[l] + b_mod[l], 6, axis=-1)
        hx = x * (1.0 + mod[1][:, None]) + mod[0][:, None]
        hc = cx * (1.0 + modc[1]) + modc[0]
        p = _split_cols(hx @ w_in[l])
        pc = _split_cols(hc @ w_in[l])
        o_a, oc_a = _gla_branch(p, pc, gla_lr_w[l], gla_lr_b[l], with_ctx)
        o_b, oc_b = _na_branch(p, pc, na_rpb[l], with_ctx)
        o_c, oc_c = _wa_branch(p, pc, wa_sink[l], with_ctx)
        o_d, oc_d = _hgrn2_branch(p, pc, lower_bounds[:, l], with_ctx)
        mix = _merge_branches(p["gate"], (o_a, o_b, o_c, o_d), w_branch[l]) @ w_out[l]
        x = _layer_norm(alpha * x + mod[2][:, None] * mix, ln_g[l, 0], ln_b[l, 0])
        if with_ctx:
            mix_c = _merge_branches(pc["gate"], (oc_a, oc_b, oc_c, oc_d), w_branch[l]) @ w_out[l]
            cx = _layer_norm(alpha * cx + modc[2] * mix_c, ln_g[l, 0], ln_b[l, 0])
        moe_args = (moe_w_group[l], moe_b_group[l], moe_w_expert[l], moe_b_expert[l],
                    moe_w_gate[l], moe_w_up[l], moe_w_down[l])
        hx = x * (1.0 + mod[4][:, None]) + mod[3][:, None]
        x = _layer_norm(alpha * x + mod[5][:, None] * _hier_moe(hx, *moe_args), ln_g[l, 1], ln_b[l, 1])
        if with_ctx:
            hc = cx * (1.0 + modc[4]) + modc[3]
            cx = _layer_norm(alpha * cx + modc[5] * _hier_moe(hc, *moe_args), ln_g[l, 1], ln_b[l, 1])
    return x
```

```python
import numpy as np
from contextlib import ExitStack
import concourse.bass as bass
import concourse.mybir as mybir
from concourse.bass_utils import run_bass_kernel_spmd
import os

F32 = mybir.dt.float32
BF16 = mybir.dt.bfloat16
U32 = mybir.dt.uint32
AF = mybir.ActivationFunctionType
ALU = mybir.AluOpType
AX = mybir.AxisListType


class Buf:
    __slots__ = ("t", "w", "r", "name")

    def __init__(self, t, name=""):
        self.t = t
        self.w = []
        self.r = []
        self.name = name

    def __getitem__(self, idx):
        return self.t[idx]


class Dep:
    __slots__ = ("w", "r")

    def __init__(self):
        self.w = []
        self.r = []


class Eng:
    def __init__(self, name, h, sem):
        self.name = name
        self.h = h
        self.sem = sem
        self.count = 0
        self.seen = {}


class KB:
    def __init__(self, nc, stack, n_dma_sems=8):
        self.nc = nc
        self.stack = stack
        self.sems = {}
        self.engs = {}
        for name, h in (("pe", nc.tensor), ("act", nc.scalar), ("dve", nc.vector),
                        ("pool", nc.gpsimd), ("sp", nc.sync)):
            s = stack.enter_context(nc.semaphore("s_" + name))
            self.sems[id(s)] = s
            self.engs[name] = Eng(name, h, s)
        self.dma_pool = {}
        for q in ("sp", "pool", "act"):
            lst = []
            for i in range(n_dma_sems):
                s = stack.enter_context(nc.semaphore("d_%s%d" % (q, i)))
                self.sems[id(s)] = s
                lst.append([s, 0])
            self.dma_pool[q] = [lst, 0]
        self.ninstr = 0

    def scope(self):
        kb = self

        class _Scope:
            def __enter__(self_s):
                self_s.prev = kb.stack
                self_s.st = ExitStack()
                self_s.st.__enter__()
                kb.stack = self_s.st
                kb.nscope = getattr(kb, "nscope", 0) + 1
                return self_s

            def __exit__(self_s, *a):
                if a[0] is None:
                    kb.barrier()
                kb.stack = self_s.prev
                return self_s.st.__exit__(*a)
        return _Scope()

    def sbuf(self, name, shape, dtype):
        name = "%s_s%d" % (name, getattr(self, "nscope", 0))
        t = self.stack.enter_context(self.nc.sbuf_tensor(name, list(shape), dtype))
        return Buf(t, name)

    def psum(self, name, shape, dtype=F32):
        name = "%s_s%d" % (name, getattr(self, "nscope", 0))
        t = self.stack.enter_context(self.nc.psum_tensor(name, list(shape), dtype))
        return Buf(t, name)

    def dram(self, name, shape, dtype, kind="Internal"):
        t = self.nc.dram_tensor(name, list(shape), dtype, kind=kind)
        return Buf(t, name)

    def _wait(self, e, deps):
        need = {}
        for (sid, val) in deps:
            if need.get(sid, 0) < val:
                need[sid] = val
        for sid, val in need.items():
            if e.seen.get(sid, 0) < val:
                e.h.wait_ge(self.sems[sid], val)
                e.seen[sid] = val
                self.ninstr += 1

    def _collect(self, reads, writes, acc_writes=()):
        deps = []
        for b in reads:
            deps += b.w
        for b in writes:
            deps += b.w
            deps += b.r
        for b in acc_writes:
            deps += b.r
        return deps

    @staticmethod
    def _compact(lst):
        mx = {}
        for (sid, v) in lst:
            if mx.get(sid, 0) < v:
                mx[sid] = v
        return list(mx.items())

    def _commit(self, token, reads, writes, acc_writes=()):
        for b in acc_writes:
            b.w.append(token)
            if len(b.w) > 64:
                b.w = self._compact(b.w)
        for b in reads:
            b.r.append(token)
            if len(b.r) > 64:
                mx = {}
                for (sid, v) in b.r:
                    if mx.get(sid, 0) < v:
                        mx[sid] = v
                b.r = list(mx.items())
        for b in writes:
            b.w = [token]
            b.r = []

    def _cutcheck(self):
        import os
        cut = os.environ.get("KB_CUT")
        self.nops = getattr(self, "nops", 0) + 1
        skip = os.environ.get("KB_SKIP")
        if skip and self.nops in [int(x) for x in skip.split(",")]:
            return True
        return cut is not None and self.nops > int(cut)

    def op(self, eng, fn, reads=(), writes=(), same_engine_sync=True, acc_writes=()):
        if self._cutcheck():
            return None
        e = self.engs[eng]
        deps = self._collect(reads, writes, acc_writes)
        if not same_engine_sync:
            deps = [d for d in deps if d[0] != id(e.sem)]
        self._wait(e, deps)
        ins = fn()
        e.count += 1
        ins.then_inc(e.sem, 1)
        self.ninstr += 1
        token = (id(e.sem), e.count)
        self._commit(token, reads, writes, acc_writes)
        return token

    def mm(self, fns, reads=(), writes=()):
        if self._cutcheck():
            return None
        e = self.engs["pe"]
        deps = self._collect(reads, writes)
        deps = [d for d in deps if d[0] != id(e.sem)]
        self._wait(e, deps)
        ins = None
        for fn in fns:
            ins = fn()
            self.ninstr += 1
        e.count += 1
        ins.then_inc(e.sem, 1)
        token = (id(e.sem), e.count)
        self._commit(token, reads, writes)
        return token

    def dma(self, q, out, in_, reads=(), writes=(), acc_writes=(), **kw):
        e = self.engs[q]
        pool = self.dma_pool[q]
        lst, idx = pool
        ent = lst[idx % len(lst)]
        pool[1] = idx + 1
        s, total = ent
        deps = self._collect(reads, writes, acc_writes)
        deps.append((id(s), total))
        import os
        if os.environ.get("DMA_SERIAL") and getattr(self, "_last_dma", {}).get(q):
            deps.append(self._last_dma[q])
        self._wait(e, deps)
        ins = e.h.dma_start(out=out, in_=in_, **kw)
        ins.then_inc(s, 16)
        ent[1] = total + 16
        self.ninstr += 1
        token = (id(s), total + 16)
        if not hasattr(self, "_last_dma"):
            self._last_dma = {}
        self._last_dma[q] = token
        self._commit(token, reads, writes, acc_writes)
        return token

    def init_memory(self, nbytes=196608):
        nc = self.nc
        with self.scope():
            n = nbytes // 4
            big = self.sbuf("initbig", [128, n], F32)
            q = n // 4
            self.op("dve", lambda: nc.vector.memset(big[:, 0:q], 0.0), writes=[])
            self.op("pool", lambda: nc.gpsimd.memset(big[:, q:2 * q], 0.0), writes=[])
            self.op("dve", lambda: nc.vector.memset(big[:, 2 * q:3 * q], 0.0), writes=[])
            self.op("pool", lambda: nc.gpsimd.memset(big[:, 3 * q:n], 0.0), writes=[])
            pss = [self.psum("initps%d" % i, [128, 512]) for i in range(8)]
            for p in pss:
                self.op("dve", lambda p=p: nc.vector.memset(p[:], 0.0), writes=[])

    def barrier(self):
        allt = []
        for e in self.engs.values():
            if e.count:
                allt.append((id(e.sem), e.count))
        for q, (lst, _) in self.dma_pool.items():
            for s, total in lst:
                if total:
                    allt.append((id(s), total))
        for e in self.engs.values():
            self._wait(e, allt)

    def finish(self, tokens_bufs):
        e = self.engs["sp"]
        deps = []
        for b in tokens_bufs:
            deps += b.w
        self._wait(e, deps)

T = 4352
TL = 4096

D = 1024
TL = 4096
TC = 256
T = TL + TC
NT = T // 128
BLKS = [(i * 512, 512) for i in range(8)] + [(4096, 256)]
DIN = 10528
ALPHA = (2.0 * 2) ** 0.25

COLS = dict(a_q=(0, 256), a_k=(256, 256), a_v=(512, 512), a_g=(1024, 512), a_lr=(1536, 32),
            b_q=(1568, 512), b_k=(2080, 512), b_v=(2592, 512),
            c_q=(3104, 512), c_k=(3616, 128), c_v=(3744, 128),
            d_q=(3872, 512), d_f=(4384, 1024), d_i=(5408, 512), d_g=(5920, 512), gate=(6432, 4096))


def blkcol(t0):
    return 1 if t0 >= TL else 0


class Ctx:
    pass


def declare_inputs(cx, nc):
    def din(name, shape, dt=F32):
        return Buf(nc.dram_tensor(name, list(shape), dt, kind="ExternalInput"), name)
    cx.xin = din("xin", [D, T])
    cx.cvec = din("cvec", [128, 8, 2])
    cx.w_mod = din("w_mod", [2, D, 6 * D])
    cx.b_modT = din("b_modT", [2, 128, 48])
    cx.w_in = din("w_in", [2, D, DIN])
    cx.rope_cos = din("rope_cos", [128, T])
    cx.rope_sin = din("rope_sin", [128, T])
    cx.rope_pt = din("rope_pt", [128, 128])
    cx.hg_gamma = din("hg_gamma", [2, 2, 512])
    cx.gla_lr_w = din("gla_lr_w", [2, 2, 16, 256])
    cx.gla_lr_b = din("gla_lr_b", [2, 2, 256])
    for nm in ("mg", "mh"):
        setattr(cx, nm + "_mq", din(nm + "_mq", [128, 2, 130]))
        setattr(cx, nm + "_mk", din(nm + "_mk", [128, 2, 128]))
    cx.m_ma = din("m_ma", [128, 2, 128])
    cx.m_ident = din("m_ident", [128, 128])
    cx.m_mau = din("m_mau", [128, 2, 128], U32)
    cx.m_wa = din("m_wa", [128, 2, 128])
    cx.wa_sink = din("wa_sink", [2, 8])
    cx.m_navalid = din("m_navalid", [128, 21, 128])
    cx.w_branch = din("w_branch", [2, 4, 512, 1024])
    cx.w_out = din("w_out", [2, 1024, 1024])
    cx.ln_gT = din("ln_gT", [2, 2, 128, 8])
    cx.ln_bT = din("ln_bT", [2, 2, 128, 8])
    cx.moe_w_group = din("moe_w_group", [2, 1024, 4])
    cx.moe_b_group = din("moe_b_group", [2, 4])
    cx.moe_w_expert = din("moe_w_expert", [2, 1024, 32])
    cx.moe_b_expert = din("moe_b_expert", [2, 32])
    cx.moe_w_gate = din("moe_w_gate", [2, 4, 8, 1024, 256])
    cx.moe_w_up = din("moe_w_up", [2, 4, 8, 1024, 256])
    cx.moe_w_down = din("moe_w_down", [2, 4, 8, 256, 1024])
    cx.m_sel = din("m_sel", [32, 32, 128])
    cx.m_ident32 = din("m_ident32", [128, 128])
    cx.na_bias = din("na_bias", [2, 8, 128, 21, 128])


def declare_scratch(cx, kb, debug, as_input=(), as_output=()):
    kind = "ExternalOutput" if debug else "Internal"

    def ds(name, shape, dt):
        b = kb.dram(name, shape, dt, kind=("ExternalInput" if name in as_input else ("ExternalOutput" if name in as_output else kind)))
        setattr(cx, name, b)
        return b
    ds("XT", [D, T], F32)
    ds("QA", [256, T], BF16)
    ds("GA", [512, T], BF16)
    ds("LR1", [33, T], F32)
    ds("QB", [512, T], BF16)
    ds("KB_", [512, T], BF16)
    ds("QC", [512, T], BF16)
    ds("KC", [128, T], BF16)
    ds("QD", [512, T], BF16)
    ds("GD", [512, T], BF16)
    ds("GATE", [4096, T], BF16)
    ds("KVA_t", [128, NT, 768], BF16)
    ds("VB_t", [128, NT, 512], BF16)
    ds("VC_t", [128, NT, 128], BF16)
    ds("VD_t", [128, NT, 512], BF16)
    ds("LGD_t", [128, NT, 1024], F32)
    ds("KD_t", [128, NT, 1024], BF16)
    ds("LGA_t", [128, NT, 512], F32)
    ds("MODT", [2, 128, 96], F32)
    ds("HX2", [D, T], BF16)
    ds("WG_t", [128, NT, 32], F32)
    ds("WGT", [32, T], F32)
    ds("BR_A", [512, T], BF16)
    ds("BR_B", [512, T], BF16)
    ds("BR_C", [512, T], BF16)
    ds("BR_D", [512, T], BF16)


def stage_mod(cx, kb, nc, modT):
    with kb.scope():
        cv = kb.sbuf("cv", [128, 8, 2], F32)
        sc = kb.sbuf("sc", [128, 8, 2], F32)
        kb.dma("sp", cv[:], cx.cvec[:], reads=[cx.cvec], writes=[cv])
        kb.op("act", lambda: nc.scalar.activation(out=sc[:], in_=cv[:], func=AF.Silu), reads=[cv], writes=[sc])
        wbufs = [kb.sbuf("wm%d" % i, [128, 8, 1024], F32) for i in range(2)]
        bm = kb.sbuf("bm", [128, 48], F32)
        ps = kb.psum("ps_mod", [128, 96])
        wi = 0
        for l in range(2):
            kb.dma("sp", bm[:], cx.b_modT[l], reads=[cx.b_modT], writes=[bm])
            for g in range(6):
                wb = wbufs[wi % 2]
                wi += 1
                src = cx.w_mod[l, :, g * 1024:(g + 1) * 1024].rearrange("(kc p) c -> p kc c", p=128)
                kb.dma("sp", wb[:], src, reads=[cx.w_mod], writes=[wb])
                for j in range(8):
                    jj = g * 8 + j
                    fns = []
                    for kc in range(8):
                        fns.append(lambda kc=kc, j=j, jj=jj, wb=wb: nc.tensor.matmul(
                            ps[:, 2 * jj:2 * jj + 2], lhsT=wb[:, kc, j * 128:(j + 1) * 128], rhs=sc[:, kc, :],
                            start=(kc == 0), stop=(kc == 7)))
                    kb.mm(fns, reads=[wb, sc], writes=[ps])
            m = modT[l]
            kb.op("dve", lambda m=m: nc.vector.tensor_tensor(
                out=m[:], in0=ps[:].rearrange("p (j c) -> p j c", c=2),
                in1=bm[:].unsqueeze(2).broadcast_to([128, 48, 2]), op=ALU.add),
                reads=[ps, bm], writes=[m])
            kb.dma("sp", cx.MODT[l], m[:].rearrange("p j c -> p (j c)"), reads=[m], writes=[cx.MODT])


def stage_modulate(cx, kb, nc, src_dram, modv, shift_idx, scale_idx, hxT, hx32=None):
    with kb.scope():
        onep = kb.sbuf("onep", [128, 8, 2], F32)
        kb.op("dve", lambda: nc.vector.tensor_scalar(
            out=onep[:], in0=modv[:, scale_idx * 8:(scale_idx + 1) * 8, :], scalar1=1.0, scalar2=None, op0=ALU.add),
            reads=[modv], writes=[onep])
        xb = [kb.sbuf("xb%d" % i, [128, 8, 512], F32) for i in range(2)]
        for bi, (t0, nb) in enumerate(BLKS):
            col = blkcol(t0)
            x = xb[bi % 2]
            kb.dma("sp", x[:, :, :nb], src_dram[:, t0:t0 + nb].rearrange("(kc p) t -> p kc t", p=128),
                   reads=[src_dram], writes=[x])
            for kc in range(8):
                eng = "dve" if kc % 2 == 0 else "pool"
                h = nc.vector if eng == "dve" else nc.gpsimd
                kb.op(eng, lambda h=h, kc=kc, x=x: h.tensor_scalar(
                    out=hxT[:, kc, t0:t0 + nb], in0=x[:, kc, :nb],
                    scalar1=onep[:, kc, col:col + 1], scalar2=modv[:, shift_idx * 8 + kc, col:col + 1],
                    op0=ALU.mult, op1=ALU.add), reads=[x, onep, modv], writes=[hxT])


FM_GROUPS = [
    ("a_q", "QA", "copy"), ("a_g", "GA", "silu"), ("a_lr", "LR1", "copy32"),
    ("b_q", "QB", "copy"), ("b_k", "KB_", "copy"),
    ("c_q", "QC", "rope"), ("c_k", "KC", "rope"),
    ("d_q", "QD", "silu"), ("d_g", "GD", "silu"), ("gate", "GATE", "sigmoid"),
]


def stage_inproj_fm(cx, kb, nc, l, hxT, only=None):
    with kb.scope():
        wsl = [kb.sbuf("wsl%d" % i, [128, 8, 512], BF16) for i in range(2)]
        stg = [kb.sbuf("stg%d" % i, [128, T], BF16) for i in range(2)]
        stg32 = kb.sbuf("stg32", [32, T], F32)
        pss = [kb.psum("psf%d" % i, [128, 512]) for i in range(4)]
        psr = [kb.psum("psr%d" % i, [128, 512]) for i in range(2)]
        cosT = kb.sbuf("cosT", [128, T], F32)
        sinT = kb.sbuf("sinT", [128, T], F32)
        ptm = kb.sbuf("ptm", [128, 128], BF16)
        tmpb = [kb.sbuf("tmpb%d" % i, [128, 512], BF16) for i in range(2)]
        t1 = [kb.sbuf("t1_%d" % i, [128, 512], F32) for i in range(2)]
        t2 = [kb.sbuf("t2_%d" % i, [128, 512], F32) for i in range(2)]
        ones = kb.sbuf("ones1", [1, T], F32)
        kb.dma("sp", cosT[:], cx.rope_cos[:], reads=[cx.rope_cos], writes=[cosT])
        kb.dma("sp", sinT[:], cx.rope_sin[:], reads=[cx.rope_sin], writes=[sinT])
        kb.dma("pool", ptm[:], cx.rope_pt[:], reads=[cx.rope_pt], writes=[ptm])
        kb.op("dve", lambda: nc.vector.memset(ones[:], 1.0), writes=[ones])
        kb.dma("sp", cx.LR1[32:33, :], ones[:], reads=[ones], writes=[], acc_writes=[cx.LR1])
        nsl = 0
        nps = 0
        nst = 0
        nrp = 0
        for (name, dest, kind) in FM_GROUPS:
            if only is not None and name not in only:
                continue
            c0, nc_ = COLS[name]
            dbuf = getattr(cx, dest)
            for s0 in range(0, nc_, 512):
                sw = min(512, nc_ - s0)
                w = wsl[nsl % 2]
                nsl += 1
                kb.dma("pool", w[:, :, :sw],
                       cx.w_in[l, :, c0 + s0:c0 + s0 + sw].rearrange("(kc p) c -> p kc c", p=128),
                       reads=[cx.w_in], writes=[w])
                for m0 in range(0, sw, 128):
                    mw = min(128, sw - m0)
                    if kind == "copy32":
                        st = stg32
                    else:
                        st = stg[nst % 2]
                        nst += 1
                    for bi, (t0, nb) in enumerate(BLKS):
                        ps = pss[nps % 4]
                        nps += 1
                        fns = [(lambda kc=kc, ps=ps, w=w: nc.tensor.matmul(
                            ps[:mw, :nb], lhsT=w[:, kc, m0:m0 + mw], rhs=hxT[:, kc, t0:t0 + nb],
                            start=(kc == 0), stop=(kc == 7))) for kc in range(8)]
                        kb.mm(fns, reads=[w, hxT], writes=[ps])
                        o = st[:mw, t0:t0 + nb]
                        if kind in ("copy", "copy32"):
                            if nps % 2 == 0:
                                kb.op("dve", lambda ps=ps, o=o: nc.vector.tensor_copy(out=o, in_=ps[:mw, :nb]),
                                      reads=[ps], writes=[st])
                            else:
                                kb.op("act", lambda ps=ps, o=o: nc.scalar.copy(out=o, in_=ps[:mw, :nb]),
                                      reads=[ps], writes=[st])
                        elif kind == "silu":
                            kb.op("act", lambda ps=ps, o=o: nc.scalar.activation(out=o, in_=ps[:mw, :nb], func=AF.Silu),
                                  reads=[ps], writes=[st])
                        elif kind == "sigmoid":
                            kb.op("act", lambda ps=ps, o=o: nc.scalar.activation(out=o, in_=ps[:mw, :nb], func=AF.Sigmoid),
                                  reads=[ps], writes=[st])
                        elif kind == "rope":
                            tb = tmpb[nrp % 2]
                            a1 = t1[nrp % 2]
                            a2 = t2[nrp % 2]
                            pr = psr[nrp % 2]
                            nrp += 1
                            kb.op("act", lambda ps=ps, tb=tb: nc.scalar.copy(out=tb[:mw, :nb], in_=ps[:mw, :nb]),
                                  reads=[ps], writes=[tb])
                            kb.mm([lambda pr=pr, tb=tb: nc.tensor.matmul(pr[:mw, :nb], lhsT=ptm[:mw, :mw], rhs=tb[:mw, :nb],
                                                                         start=True, stop=True)],
                                  reads=[ptm, tb], writes=[pr])
                            kb.op("dve", lambda tb=tb, a1=a1: nc.vector.tensor_tensor(
                                out=a1[:mw, :nb], in0=tb[:mw, :nb], in1=cosT[:mw, t0:t0 + nb], op=ALU.mult),
                                reads=[tb, cosT], writes=[a1])
                            kb.op("dve", lambda pr=pr, a2=a2: nc.vector.tensor_tensor(
                                out=a2[:mw, :nb], in0=pr[:mw, :nb], in1=sinT[:mw, t0:t0 + nb], op=ALU.mult),
                                reads=[pr, sinT], writes=[a2])
                            kb.op("pool", lambda a1=a1, a2=a2, o=o: nc.gpsimd.tensor_tensor(
                                out=o, in0=a1[:mw, :nb], in1=a2[:mw, :nb], op=ALU.add),
                                reads=[a1, a2], writes=[st])
                    kb.dma("sp", dbuf[s0 + m0:s0 + m0 + mw, :], st[:mw, :], reads=[st], writes=[], acc_writes=[dbuf])


TM_GROUPS = [
    (256, 512, "KVA_t", 0, "copy"), (768, 256, "KVA_t", 512, "copy"),
    (2592, 512, "VB_t", 0, "copy"), (3744, 128, "VC_t", 0, "copy"),
    (5408, 512, "VD_t", 0, "copy"),
    (4384, 512, None, 0, "hgf"), (4896, 512, None, 512, "hgf"),
]


def stage_inproj_tm(cx, kb, nc, l, hxT, lbt, omlt, only=None):
    with kb.scope():
        wsl = [kb.sbuf("wtl%d" % i, [128, 8, 512], BF16) for i in range(2)]
        pss = [kb.psum("pst%d" % i, [128, 512]) for i in range(4)]
        stb = [kb.sbuf("stb%d" % i, [128, 512], BF16) for i in range(4)]
        e1 = [kb.sbuf("e1_%d" % i, [128, 512], F32) for i in range(2)]
        r1 = [kb.sbuf("r1_%d" % i, [128, 512], F32) for i in range(2)]
        lg = [kb.sbuf("lg_%d" % i, [128, 512], F32) for i in range(2)]
        kk = [kb.sbuf("kk_%d" % i, [128, 512], BF16) for i in range(2)]
        nsl = nps = nst = nh = 0
        for (c0, ncol, dest, doff, kind) in TM_GROUPS:
            if only is not None and kind not in only:
                continue
            w = wsl[nsl % 2]
            nsl += 1
            kb.dma("pool", w[:, :, :ncol], cx.w_in[l, :, c0:c0 + ncol].rearrange("(kc p) c -> p kc c", p=128),
                   reads=[cx.w_in], writes=[w])
            for ti in range(NT):
                ts = slice(ti * 128, (ti + 1) * 128)
                ps = pss[nps % 4]
                nps += 1
                fns = [(lambda kc=kc, ps=ps, w=w: nc.tensor.matmul(
                    ps[:, :ncol], lhsT=hxT[:, kc, ts], rhs=w[:, kc, :ncol],
                    start=(kc == 0), stop=(kc == 7))) for kc in range(8)]
                kb.mm(fns, reads=[w, hxT], writes=[ps])
                if kind == "copy":
                    st = stb[nst % 4]
                    nst += 1
                    if nst % 2 == 0:
                        kb.op("dve", lambda ps=ps, st=st: nc.vector.tensor_copy(out=st[:, :ncol], in_=ps[:, :ncol]),
                              reads=[ps], writes=[st])
                    else:
                        kb.op("act", lambda ps=ps, st=st: nc.scalar.copy(out=st[:, :ncol], in_=ps[:, :ncol]),
                              reads=[ps], writes=[st])
                    db = getattr(cx, dest)
                    kb.dma("sp", db[:, ti, doff:doff + ncol], st[:, :ncol], reads=[st], writes=[], acc_writes=[db])
                else:
                    e = e1[nh % 2]
                    r = r1[nh % 2]
                    g = lg[nh % 2]
                    k = kk[nh % 2]
                    nh += 1
                    cs = slice(doff, doff + 512)
                    kb.op("act", lambda ps=ps, e=e: nc.scalar.activation(out=e[:], in_=ps[:], func=AF.Exp, scale=-1.0),
                          reads=[ps], writes=[e])
                    kb.op("pool", lambda e=e: nc.gpsimd.tensor_scalar(
                        out=e[:], in0=e[:], scalar1=1.0, scalar2=None, op0=ALU.add),
                        reads=[e], writes=[e])
                    kb.op("dve", lambda e=e, r=r: nc.vector.reciprocal(out=r[:], in_=e[:]),
                          reads=[e], writes=[r])
                    kb.op("dve", lambda r=r: nc.vector.tensor_tensor(out=r[:], in0=r[:], in1=omlt[:, cs], op=ALU.mult),
                          reads=[r, omlt], writes=[r])
                    kb.op("pool", lambda r=r: nc.gpsimd.tensor_tensor(out=r[:], in0=r[:], in1=lbt[:, cs], op=ALU.add),
                          reads=[r, lbt], writes=[r])
                    kb.op("act", lambda r=r, g=g: nc.scalar.activation(out=g[:], in_=r[:], func=AF.Ln),
                          reads=[r], writes=[g])
                    kb.op("pool", lambda r=r, k=k: nc.gpsimd.tensor_scalar(
                        out=k[:], in0=r[:], scalar1=-1.0, scalar2=1.0, op0=ALU.mult, op1=ALU.add),
                        reads=[r], writes=[k])
                    kb.dma("sp", cx.LGD_t[:, ti, cs], g[:], reads=[g], writes=[], acc_writes=[cx.LGD_t])
                    kb.dma("sp", cx.KD_t[:, ti, cs], k[:], reads=[k], writes=[], acc_writes=[cx.KD_t])


def stage_lb(cx, kb, nc, lbt, omlt):
    with kb.scope():
        gam = kb.sbuf("gam", [128, 2, 2, 512], F32)
        kb.dma("sp", gam[:].rearrange("p a b c -> p (a b c)"),
               cx.hg_gamma[:].rearrange("a b c -> (a b c)").partition_broadcast(128),
               reads=[cx.hg_gamma], writes=[gam])
        d = kb.sbuf("gamd", [128, 2, 512], F32)
        kb.op("dve", lambda: nc.vector.tensor_tensor(out=d[:], in0=gam[:, :, 0, :], in1=gam[:, :, 1, :], op=ALU.subtract),
              reads=[gam], writes=[d])
        kb.op("act", lambda: nc.scalar.activation(out=d[:], in_=d[:], func=AF.Exp), reads=[d], writes=[d])
        l1 = lbt[1]
        kb.op("dve", lambda: nc.vector.tensor_scalar(
            out=d[:], in0=d[:], scalar1=1.0, scalar2=None, op0=ALU.add), reads=[d], writes=[d])
        kb.op("dve", lambda: nc.vector.reciprocal(out=l1[:], in_=d[:].rearrange("p a c -> p (a c)")),
              reads=[d], writes=[l1])
        l0 = lbt[0]
        kb.op("dve", lambda: nc.vector.memset(l0[:], 0.0), writes=[l0])
        for l in range(2):
            o = omlt[l]
            lb = lbt[l]
            kb.op("dve", lambda o=o, lb=lb: nc.vector.tensor_scalar(
                out=o[:], in0=lb[:], scalar1=-1.0, scalar2=1.0, op0=ALU.mult, op1=ALU.add),
                reads=[lb], writes=[o])


def stage_gla_gate(cx, kb, nc, l):
    with kb.scope():
        wlr = kb.sbuf("wlr", [33, 512], F32)
        lr1 = kb.sbuf("lr1", [33, T], F32)
        kb.op("dve", lambda: nc.vector.memset(wlr[:], 0.0), writes=[wlr])
        kb.dma("sp", wlr[0:16, 0:256], cx.gla_lr_w[l, 0], reads=[cx.gla_lr_w], writes=[wlr])
        kb.dma("sp", wlr[16:32, 256:512], cx.gla_lr_w[l, 1], reads=[cx.gla_lr_w], writes=[wlr])
        kb.dma("sp", wlr[32:33, :], cx.gla_lr_b[l].rearrange("a c -> (a c)").unsqueeze(0),
               reads=[cx.gla_lr_b], writes=[wlr])
        kb.dma("sp", lr1[:], cx.LR1[:], reads=[cx.LR1], writes=[lr1])
        pss = [kb.psum("psg%d" % i, [128, 512]) for i in range(2)]
        ee = [kb.sbuf("ge%d" % i, [128, 512], F32) for i in range(2)]
        gg = [kb.sbuf("gg%d" % i, [128, 512], F32) for i in range(2)]
        for ti in range(NT):
            ts = slice(ti * 128, (ti + 1) * 128)
            ps = pss[ti % 2]
            e = ee[ti % 2]
            g = gg[ti % 2]
            kb.mm([lambda ps=ps: nc.tensor.matmul(ps[:], lhsT=lr1[:, ts], rhs=wlr[:], start=True, stop=True)],
                  reads=[lr1, wlr], writes=[ps])
            kb.op("act", lambda ps=ps, e=e: nc.scalar.activation(out=e[:], in_=ps[:], func=AF.Exp, scale=-1.0),
                  reads=[ps], writes=[e])
            kb.op("act", lambda e=e, g=g: nc.scalar.activation(out=g[:], in_=e[:], func=AF.Ln, bias=1.0),
                  reads=[e], writes=[g])
            kb.dma("sp", cx.LGA_t[:, ti, :], g[:], reads=[g], writes=[], acc_writes=[cx.LGA_t])

SCAN_CFG = {
    "gla": dict(H=4, dk=64, qsrc="QA", qscale=0.125, ksrc="KVA_t", kz=0, kcol0=0, vsrc="KVA_t", vcol0=256,
                gsrc="LGA_t", gz=256, gate="GA", dest="BR_A", mask="mg"),
    "hgrn": dict(H=4, dk=128, qsrc="QD", qscale=1.0, ksrc="KD_t", kz=512, kcol0=0, vsrc="VD_t", vcol0=0,
                 gsrc="LGD_t", gz=512, gate="GD", dest="BR_D", mask="mh"),
}


def view(b):
    return Buf(b.t, b.name + "_v")


def stage_scan(cx, kb, nc, which, heads=None):
    cfg = SCAN_CFG[which]
    H, dk = cfg["H"], cfg["dk"]
    dv = 128
    qsrc, ksrc, vsrc, gsrc = (getattr(cx, cfg[k]) for k in ("qsrc", "ksrc", "vsrc", "gsrc"))
    gate, dest = getattr(cx, cfg["gate"]), getattr(cx, cfg["dest"])
    mq_d, mk_d = getattr(cx, cfg["mask"] + "_mq"), getattr(cx, cfg["mask"] + "_mk")
    with kb.scope():
        mq = kb.sbuf("mq", [128, 2, 130], F32)
        mk = kb.sbuf("mk", [128, 2, 128], F32)
        ma = kb.sbuf("ma", [128, 2, 128], F32)
        ident = kb.sbuf("ident", [128, 128], BF16)
        onesdv = kb.sbuf("onesdv", [128, 128], F32)
        kb.dma("sp", mq[:], mq_d[:], reads=[mq_d], writes=[mq])
        kb.dma("sp", mk[:], mk_d[:], reads=[mk_d], writes=[mk])
        kb.dma("sp", ma[:], cx.m_ma[:], reads=[cx.m_ma], writes=[ma])
        mau = kb.sbuf("mau", [128, 2, 128], U32)
        kb.dma("sp", mau[:], cx.m_mau[:], reads=[cx.m_mau], writes=[mau])
        kb.dma("pool", ident[:], cx.m_ident[:], reads=[cx.m_ident], writes=[ident])
        kb.op("dve", lambda: nc.vector.memset(onesdv[:], 1.0 / dv), writes=[onesdv])
        vt = kb.sbuf("vt", [128, NT, dv], BF16)
        qT = kb.sbuf("qT", [128, T], BF16)
        gsil = kb.sbuf("gsil", [128, T], BF16)
        obr = kb.sbuf("obr", [128, T], BF16)
        gts = [kb.sbuf("gt%d" % i, [128, NT, dk], F32) for i in range(2)]
        kts = [kb.sbuf("kt%d" % i, [128, NT, dk], BF16) for i in range(2)]
        qt = [kb.sbuf("qt%d" % z, [128, T], BF16) for z in range(2)]
        atm = [kb.sbuf("atm%d" % z, [128, NT, 128], BF16) for z in range(2)]
        Sp = [kb.sbuf("Sp%d" % z, [128, 2 * NT, dv], BF16) for z in range(2)]
        for z in range(2):
            kb.op("pool", lambda z=z: nc.gpsimd.memset(atm[z][:], 0.0), writes=[atm[z]])
        Sst = [kb.sbuf("Sst%d" % i, [128, dv], F32) for i in range(2)]
        R = 3
        eq = [kb.sbuf("eq%d" % i, [128, 128], F32) for i in range(R)]
        ek = [kb.sbuf("ek%d" % i, [128, 128], F32) for i in range(R)]
        ekh = [kb.sbuf("ekh%d" % i, [128, 128], F32) for i in range(R)]
        ktl = [kb.sbuf("ktl%d" % i, [128, 128], BF16) for i in range(R)]
        khat = [kb.sbuf("khat%d" % i, [128, 256], BF16) for i in range(R)]
        ktr = [kb.sbuf("ktr%d" % i, [128, 128], F32) for i in range(R)]
        atr = [kb.sbuf("atr%d" % i, [128, 128], F32) for i in range(R)]
        usb = [kb.sbuf("usb%d" % i, [128, 256], F32) for i in range(R)]
        osb = [kb.sbuf("osb%d" % i, [128, 512], F32) for i in range(2)]
        cm = kb.sbuf("cm", [128, 2], F32)
        kb.op("dve", lambda: nc.vector.memset(cm[:], 0.0), writes=[cm])
        kb.op("dve", lambda: nc.vector.memset(cm[0:64, 0:1], 1.0), writes=[cm])
        kb.op("dve", lambda: nc.vector.memset(cm[64:128, 1:2], 1.0), writes=[cm])
        er2 = [kb.sbuf("er%d" % i, [128, 2], F32) for i in range(R)]
        ed2 = [kb.sbuf("ed%d" % i, [128, 2], F32) for i in range(R)]
        psA = [kb.psum("psA%d" % i, [128, 512]) for i in range(2)]
        psB = [kb.psum("psB%d" % i, [128, 512]) for i in range(2)]
        psA_ec = [view(p) for p in psA]
        import os
        if True:
            psK = [kb.psum("psK%d" % i, [128, 512]) for i in range(2)]
        else:
            psK = psA
        psA_kt = [view(p) for p in psK]
        psA_e2 = [view(p) for p in psK]
        psB_u = [view(p) for p in psB]
        psB_at = [view(p) for p in psB]
        psO = [kb.psum("psO%d" % i, [128, 512]) for i in range(1)] * 2
        psM = [kb.psum("psM%d" % i, [128, 512]) for i in range(1)] * 2
        sq = [kb.sbuf("sq%d" % i, [128, 512], F32) for i in range(2)]
        sd = [kb.sbuf("sd%d" % i, [128, 512], F32) for i in range(2)]
        on = [kb.sbuf("on%d" % i, [128, 512], F32) for i in range(2)]
        eps_t = kb.sbuf("eps_t", [128, 1], F32)
        kb.op("dve", lambda: nc.vector.memset(eps_t[:], 1e-6), writes=[eps_t])

        it = 0
        ghz = 0
        import os
        for h in (range(H) if heads is None else heads):
            vc0 = cfg["vcol0"] + h * dv
            kb.dma("sp", vt[:], vsrc[:, :, vc0:vc0 + dv], reads=[vsrc], writes=[vt])
            kb.dma("sp", qT[:dk, :], qsrc[h * dk:(h + 1) * dk, :], reads=[qsrc], writes=[qT])
            kb.dma("sp", gsil[:], gate[h * dv:(h + 1) * dv, :], reads=[gate], writes=[gsil])
            for z in range(2):
                gt = gts[ghz % 2]
                kt = kts[ghz % 2]
                ghz += 1
                gc0 = z * cfg["gz"] + h * dk
                kc0 = cfg["kcol0"] + z * cfg["kz"] + h * dk
                kb.dma("sp", gt[:], gsrc[:, :, gc0:gc0 + dk], reads=[gsrc], writes=[gt])
                if os.environ.get("KTMEMSET"):
                    kb.op("dve", lambda kt=kt: nc.vector.memset(kt[:], 0.0), writes=[kt])
                kb.dma("sp", kt[:], ksrc[:, :, kc0:kc0 + dk], reads=[ksrc], writes=[kt])
                order = [32, 33] + list(range(32)) if z == 0 else [33, 32] + list(range(31, -1, -1))
                import os
                DBG = os.environ.get("SCAN_DBG", "")
                if DBG == "loads":
                    continue
                if DBG.startswith("nt"):
                    order = order[:int(DBG[2:])]
                S = Sst[0]
                kb.op("dve", lambda S=S: nc.vector.memset(S[:], 0.0), writes=[S])
                si = 0
                for ti in order:
                    a = it % 2
                    r = it % R
                    it += 1
                    ts = slice(ti * 128, (ti + 1) * 128)
                    pa, pb = psA[a], psB[a]
                    pk = psK[a]
                    kb.mm([lambda pa=pa, gt=gt: nc.tensor.matmul(pa[:dk, 0:130], lhsT=gt[:, ti, :], rhs=mq[:, z, :],
                                                                 start=True, stop=True)],
                          reads=[gt, mq], writes=[psA_ec[a]])
                    kb.mm([lambda pk=pk, kt=kt: nc.tensor.matmul(pk[:dk, 256:384], lhsT=(ident[:, :dk] if os.environ.get("A2ID") else kt[:, ti, :]), rhs=ident[:],
                                                                 start=True, stop=True)],
                          reads=[kt, ident], writes=[psA_kt[a]])
                    kb.mm([lambda pk=pk, gt=gt: nc.tensor.matmul(pk[:, 384:384 + dk], lhsT=mk[:, z, :], rhs=gt[:, ti, :],
                                                                 start=True, stop=True)],
                          reads=[gt, mk], writes=[psA_e2[a]])
                    kb.op("act", lambda pa=pa, o=eq[r]: nc.scalar.activation(out=o[:dk, :], in_=pa[:dk, 0:128], func=AF.Exp),
                          reads=[psA_ec[a]], writes=[eq[r]])
                    kb.op("act", lambda pa=pa, o=ek[r]: nc.scalar.activation(out=o[:dk, :], in_=pa[:dk, 0:128], func=AF.Exp, scale=-1.0),
                          reads=[psA_ec[a]], writes=[ek[r]])
                    kb.op("act", lambda pa=pa, o=er2[r]: nc.scalar.activation(out=o[:dk, :], in_=pa[:dk, 128:130], func=AF.Exp, scale=0.5),
                          reads=[psA_ec[a]], writes=[er2[r]])
                    kb.op("act", lambda pa=pa, o=ed2[r]: nc.scalar.activation(out=o[:dk, :], in_=pa[:dk, 128:130], func=AF.Exp),
                          reads=[psA_ec[a]], writes=[ed2[r]])
                    kb.op("act", lambda pk=pk, o=ekh[r]: nc.scalar.activation(out=o[:, :dk], in_=pk[:, 384:384 + dk], func=AF.Exp),
                          reads=[psA_e2[a]], writes=[ekh[r]])
                    qz = qt[z]
                    kb.op("dve", lambda qz=qz, e=eq[r]: nc.vector.scalar_tensor_tensor(
                        out=qz[:dk, ts], in0=qT[:dk, ts], scalar=cfg["qscale"], in1=e[:dk, :], op0=ALU.mult, op1=ALU.mult),
                        reads=[qT, eq[r]], writes=[], acc_writes=[qz])
                    kb.op("act", lambda pk=pk, o=ktr[r]: nc.scalar.copy(out=o[:dk, :], in_=pk[:dk, 256:384]),
                          reads=[psA_kt[a]], writes=[ktr[r]])
                    kb.op("dve", lambda e=ek[r], o=ktl[r], kr=ktr[r]: nc.vector.tensor_tensor(
                        out=o[:dk, :], in0=kr[:dk, :], in1=e[:dk, :], op=ALU.mult),
                        reads=[ktr[r], ek[r]], writes=[ktl[r]])
                    for c in range(2):
                        kb.op("dve", lambda kt=kt, e=ekh[r], o=khat[r], c=c: nc.vector.scalar_tensor_tensor(
                            out=o[:, c * 128:c * 128 + dk], in0=kt[:, ti, :], scalar=cm[:, c:c + 1], in1=e[:, :dk],
                            op0=ALU.mult, op1=ALU.mult),
                            reads=[kt, ekh[r], cm], writes=[], acc_writes=[khat[r]])
                    kb.mm([(lambda c=c, pb=pb, kh=khat[r]: nc.tensor.matmul(
                        pb[:dk, c * 128:(c + 1) * 128], lhsT=kh[:, c * 128:c * 128 + dk],
                        rhs=vt[:, ti, :], start=True, stop=True)) for c in range(2)],
                        reads=[khat[r], vt], writes=[psB_u[a]])
                    kb.mm([lambda pb=pb, k_=ktl[r], qz=qz: nc.tensor.matmul(
                        pb[:, 256:384], lhsT=k_[:dk, :], rhs=qz[:dk, ts], start=True, stop=True)],
                        reads=[ktl[r], qz], writes=[psB_at[a]])
                    az = atm[z]
                    kb.op("act", lambda pb=pb, o=atr[r]: nc.scalar.copy(out=o[:], in_=pb[:, 256:384]),
                          reads=[psB_at[a]], writes=[atr[r]])
                    kb.op("dve", lambda az=az, o=atr[r]: nc.vector.copy_predicated(
                        out=az[:, ti, :], mask=mau[:, z, :], data=o[:]),
                        reads=[atr[r], mau], writes=[], acc_writes=[az])
                    kb.op("act", lambda pb=pb, o=usb[r]: nc.scalar.copy(out=o[:dk, :], in_=pb[:dk, 0:256]),
                          reads=[psB_u[a]], writes=[usb[r]])
                    spz = Sp[z]
                    for c in ((0, 1) if z == 0 else (1, 0)):
                        ci = 2 * ti + c
                        kb.op("act", lambda S=S, spz=spz, ci=ci, e=er2[r], c=c: nc.scalar.activation(
                            out=spz[:dk, ci, :], in_=S[:dk, :], func=AF.Copy, scale=e[:dk, c:c + 1]),
                            reads=[S, er2[r]], writes=[], acc_writes=[spz])
                        S2 = Sst[(si + 1) % 2]
                        si += 1
                        kb.op("dve", lambda S=S, S2=S2, e=ed2[r], u=usb[r], c=c: nc.vector.scalar_tensor_tensor(
                            out=S2[:dk, :], in0=S[:dk, :], scalar=e[:dk, c:c + 1], in1=u[:dk, c * 128:(c + 1) * 128],
                            op0=ALU.mult, op1=ALU.add),
                            reads=[S, ed2[r], usb[r]], writes=[S2])
                        S = S2
            groups = [list(range(g * 4, g * 4 + 4)) for g in range(8)] + [[32, 33]]
            if DBG:
                groups = []
            for gi, tiles in enumerate(groups):
                po = psO[gi % 2]
                pm = psM[gi % 2]
                nb = len(tiles) * 128
                t0 = tiles[0] * 128
                fns = []
                for k_, ti in enumerate(tiles):
                    cs = k_ * 128
                    for z in range(2):
                        fns.append(lambda po=po, ti=ti, cs=cs, z=z: nc.tensor.matmul(
                            po[:, cs:cs + 128], lhsT=vt[:, ti, :], rhs=atm[z][:, ti, :], start=(z == 0), stop=False))
                        for c in range(2):
                            fns.append(lambda po=po, ti=ti, cs=cs, z=z, c=c: nc.tensor.matmul(
                                po[:, cs + c * 64:cs + (c + 1) * 64], lhsT=Sp[z][:dk, 2 * ti + c, :],
                                rhs=qt[z][:dk, ti * 128 + c * 64:ti * 128 + (c + 1) * 64],
                                start=False, stop=(z == 1)))
                kb.mm(fns, reads=[vt, atm[0], atm[1], Sp[0], Sp[1], qt[0], qt[1]], writes=[po])
                s_, d_, o_ = sq[gi % 2], sd[gi % 2], on[gi % 2]
                kb.op("act", lambda po=po, s_=s_: nc.scalar.activation(out=s_[:, :nb], in_=po[:, :nb], func=AF.Square),
                      reads=[po], writes=[s_])
                kb.mm([lambda pm=pm, s_=s_: nc.tensor.matmul(pm[:, :nb], lhsT=onesdv[:], rhs=s_[:, :nb], start=True, stop=True)],
                      reads=[onesdv, s_], writes=[pm])
                kb.op("act", lambda pm=pm, d_=d_: nc.scalar.activation(out=d_[:, :nb], in_=pm[:, :nb], func=AF.Sqrt, bias=eps_t[:]),
                      reads=[pm, eps_t], writes=[d_])
                kb.op("dve", lambda d_=d_: nc.vector.reciprocal(out=d_[:, :nb], in_=d_[:, :nb]), reads=[d_], writes=[d_])
                ob_ = osb[gi % 2]
                kb.op("act", lambda po=po, ob_=ob_: nc.scalar.copy(out=ob_[:, :nb], in_=po[:, :nb]), reads=[po], writes=[ob_])
                kb.op("dve", lambda ob_=ob_, d_=d_, o_=o_: nc.vector.tensor_tensor(
                    out=o_[:, :nb], in0=ob_[:, :nb], in1=d_[:, :nb], op=ALU.mult), reads=[ob_, d_], writes=[o_])
                kb.op("pool", lambda o_=o_: nc.gpsimd.tensor_tensor(
                    out=obr[:, t0:t0 + nb], in0=o_[:, :nb], in1=gsil[:, t0:t0 + nb], op=ALU.mult),
                    reads=[o_, gsil], writes=[], acc_writes=[obr])
            kb.dma("sp", dest[h * dv:(h + 1) * dv, :], obr[:], reads=[obr], writes=[], acc_writes=[dest])


def scan_masks():
    j = np.arange(128)[:, None]
    i = np.arange(128)[None, :]
    same = (j // 64 == i // 64).astype(np.float32)
    out = {}
    for name, gs in (("mg", -1.0 / 16.0), ("mh", 1.0)):
        mq = np.zeros((128, 2, 130), np.float32)
        mk = np.zeros((128, 2, 128), np.float32)
        mq[:, 0, :128] = gs * same * ((j <= i).astype(np.float32) - 0.5)
        mq[:, 1, :128] = gs * same * ((j >= i).astype(np.float32) - 0.5)
        for z in range(2):
            for c in range(2):
                mq[:, z, 128 + c] = gs * (np.arange(128) // 64 == c)
        mk[:, 0, :] = gs * same * (j > i)
        mk[:, 1, :] = gs * same * (j < i)
        out[name + "_mq"] = mq
        out[name + "_mk"] = mk
    ma = np.zeros((128, 2, 128), np.float32)
    ma[:, 0, :] = same * (j <= i)
    ma[:, 1, :] = same * (j >= i)
    out["m_ma"] = ma
    out["m_mau"] = ma.astype(np.uint32)
    out["m_ident"] = np.eye(128, dtype=np.float32)
    return out


def stage_wa(cx, kb, nc, l, groups=None):
    with kb.scope():
        wm = kb.sbuf("wm", [128, 2, 128], BF16)
        kb.dma("pool", wm[:], cx.m_wa[:], reads=[cx.m_wa], writes=[wm])
        ones64 = kb.sbuf("ones64", [128, 64], BF16)
        kb.op("dve", lambda: nc.vector.memset(ones64[:], 1.0), writes=[ones64])
        sk = kb.sbuf("sk", [64, 8], F32)
        kb.dma("sp", sk[:], cx.wa_sink[l].partition_broadcast(64), reads=[cx.wa_sink], writes=[sk])
        kb.op("act", lambda: nc.scalar.activation(out=sk[:], in_=sk[:], func=AF.Exp), reads=[sk], writes=[sk])
        q4 = kb.sbuf("q4", [64, 4, T], BF16)
        kT = kb.sbuf("kTc", [64, T], BF16)
        vt = kb.sbuf("vtc", [128, NT, 64], BF16)
        ost = kb.sbuf("ost", [64, 4, T], BF16)
        pS = [kb.psum("pS%d" % i, [128, 512]) for i in range(2)]
        pO = [kb.psum("pO%d" % i, [128, 512]) for i in range(2)]
        pD = [kb.psum("pD%d" % i, [128, 512]) for i in range(2)]
        R = 3
        pt = [kb.sbuf("pt%d" % i, [128, 4, 128], BF16) for i in range(R)]
        ptm = [kb.sbuf("ptm%d" % i, [128, 4, 128], BF16) for i in range(R)]
        osb = [kb.sbuf("osbw%d" % i, [64, 4, 128], F32) for i in range(2)]
        dsb = [kb.sbuf("dsbw%d" % i, [64, 4, 128], F32) for i in range(2)]
        it = 0
        for g in (range(2) if groups is None else groups):
            kb.dma("sp", q4[:], cx.QC[g * 256:(g + 1) * 256, :].rearrange("(hh d) t -> d hh t", d=64),
                   reads=[cx.QC], writes=[q4])
            kb.dma("sp", kT[:], cx.KC[g * 64:(g + 1) * 64, :], reads=[cx.KC], writes=[kT])
            kb.dma("sp", vt[:], cx.VC_t[:, :, g * 64:(g + 1) * 64], reads=[cx.VC_t], writes=[vt])
            for qb in range(NT):
                if qb < 32:
                    keys = []
                    if qb > 0:
                        keys.append((qb - 1, 0))
                    keys.append((qb, None))
                    if qb < 31:
                        keys.append((qb + 1, 1))
                    keys += [(32, None), (33, None)]
                else:
                    keys = [(32, None), (33, None)]
                qs = slice(qb * 128, (qb + 1) * 128)
                a = qb % 2
                po, pd = pO[a], pD[a]
                nk = len(keys)
                for ki, (kt_i, mi) in enumerate(keys):
                    ps = pS[it % 2]
                    p_ = pt[it % R]
                    pm_ = ptm[it % R]
                    it += 1
                    kb.mm([lambda ps=ps, kt_i=kt_i: nc.tensor.matmul(
                        ps[:, :], lhsT=kT[:, kt_i * 128:(kt_i + 1) * 128], rhs=q4[:, :, qs], start=True, stop=True)],
                        reads=[kT, q4], writes=[ps])
                    kb.op("act", lambda ps=ps, p_=p_: nc.scalar.activation(
                        out=p_[:].rearrange("p h q -> p (h q)"), in_=ps[:, :], func=AF.Exp, scale=0.125),
                        reads=[ps], writes=[p_])
                    src = p_
                    if mi is not None:
                        eng = "dve" if it % 2 == 0 else "pool"
                        h_ = nc.vector if eng == "dve" else nc.gpsimd
                        kb.op(eng, lambda h_=h_, p_=p_, pm_=pm_, mi=mi: h_.tensor_tensor(
                            out=pm_[:], in0=p_[:], in1=wm[:, mi, :].unsqueeze(1).broadcast_to([128, 4, 128]), op=ALU.mult),
                            reads=[p_, wm], writes=[pm_])
                        src = pm_
                    kb.mm([lambda po=po, src=src, kt_i=kt_i, ki=ki: nc.tensor.matmul(
                        po[:64, :], lhsT=vt[:, kt_i, :], rhs=src[:].rearrange("p h q -> p (h q)"),
                        start=(ki == 0), stop=(ki == nk - 1)),
                        lambda pd=pd, src=src, ki=ki: nc.tensor.matmul(
                        pd[:64, :], lhsT=ones64[:], rhs=src[:].rearrange("p h q -> p (h q)"),
                        start=(ki == 0), stop=(ki == nk - 1))],
                        reads=[vt, src, ones64], writes=[po, pd])
                o_, d_ = osb[a], dsb[a]
                kb.op("act", lambda po=po, o_=o_: nc.scalar.copy(out=o_[:].rearrange("p h q -> p (h q)"), in_=po[:64, :]),
                      reads=[po], writes=[o_])
                kb.op("act", lambda pd=pd, d_=d_: nc.scalar.copy(out=d_[:].rearrange("p h q -> p (h q)"), in_=pd[:64, :]),
                      reads=[pd], writes=[d_])
                kb.op("dve", lambda d_=d_: nc.vector.tensor_tensor(
                    out=d_[:], in0=d_[:], in1=sk[:, g * 4:(g + 1) * 4].unsqueeze(2).broadcast_to([64, 4, 128]), op=ALU.add),
                    reads=[d_, sk], writes=[d_])
                kb.op("dve", lambda d_=d_: nc.vector.reciprocal(out=d_[:], in_=d_[:]), reads=[d_], writes=[d_])
                kb.op("pool", lambda o_=o_, d_=d_: nc.gpsimd.tensor_tensor(
                    out=ost[:, :, qs], in0=o_[:], in1=d_[:], op=ALU.mult),
                    reads=[o_, d_], writes=[], acc_writes=[ost])
            kb.dma("sp", cx.BR_C[g * 256:(g + 1) * 256, :].rearrange("(hh d) t -> d hh t", d=64), ost[:],
                   reads=[ost], writes=[], acc_writes=[cx.BR_C])


def wa_masks():
    kk = np.arange(128)[:, None]
    qq = np.arange(128)[None, :]
    m = np.zeros((128, 2, 128), np.float32)
    m[:, 0, :] = (kk >= qq)
    m[:, 1, :] = (kk <= qq)
    return {"m_wa": m}


def na_patterns():
    W = 64
    tok = np.arange(128)
    lr, lc = tok // W, tok % W
    pats = []
    keys = []
    for m in range(32):
        qr = 2 * m + lr[None, :]
        qc = lc[None, :]
        rs = np.clip(qr - 4, 0, 56)
        cs = np.clip(qc - 8, 0, 48)
        lst = []
        for kt_i in range(32):
            kr = 2 * kt_i + lr[:, None]
            kc = lc[:, None]
            valid = (kr >= rs) & (kr < rs + 8) & (kc >= cs) & (kc < cs + 16)
            if not valid.any():
                continue
            ri = np.clip(kr - qr + 7, 0, 14)
            ci = np.clip(kc - qc + 15, 0, 30)
            if 2 <= m <= 29:
                pid = kt_i - m + 2
                if m == 2:
                    pats.append((valid, ri, ci))
            else:
                pid = len(pats)
                pats.append((valid, ri, ci))
            lst.append((kt_i, pid))
        keys.append(lst)
    return keys, pats


def na_consts():
    keys, pats = na_patterns()
    n0 = len(keys[0]) + len(keys[1])
    order = list(range(n0, n0 + 5)) + list(range(0, n0)) + list(range(n0 + 5, len(pats)))
    remap = {old: new for new, old in enumerate(order)}
    pats2 = [pats[o] for o in order]
    keys2 = []
    for m, lst in enumerate(keys):
        if 2 <= m <= 29:
            keys2.append(lst)
        else:
            keys2.append([(k, remap[p]) for (k, p) in lst])
    valid = np.stack([p[0] for p in pats2], 1).astype(np.float32)
    ri = np.stack([p[1] for p in pats2], 1)
    ci = np.stack([p[2] for p in pats2], 1)
    return keys2, valid, ri, ci


def stage_na(cx, kb, nc, l, heads=None):
    keys_tab, valid_np, _, _ = na_consts()
    npat = valid_np.shape[1]
    with kb.scope():
        vmask = kb.sbuf("vmask", [128, npat, 128], F32)
        kb.dma("sp", vmask[:], cx.m_navalid[:], reads=[cx.m_navalid], writes=[vmask])
        ones64 = kb.sbuf("ones64n", [128, 64], BF16)
        kb.op("dve", lambda: nc.vector.memset(ones64[:], 1.0), writes=[ones64])
        qT = kb.sbuf("qTn", [64, T], BF16)
        kT = kb.sbuf("kTn", [64, T], BF16)
        vt = kb.sbuf("vtn", [128, NT, 64], BF16)
        ost = kb.sbuf("ostn", [64, T], BF16)
        bias = kb.sbuf("biasn", [128, npat, 128], F32)
        EB = kb.sbuf("EBn", [128, npat, 128], BF16)
        pS = [kb.psum("pSn%d" % i, [128, 512]) for i in range(4)]
        pO = [kb.psum("pOn%d" % i, [128, 512]) for i in range(2)]
        pD = [kb.psum("pDn%d" % i, [128, 512]) for i in range(2)]
        R = 4
        pt = [kb.sbuf("ptn%d" % i, [128, 4, 128], BF16) for i in range(R)]
        osb = [kb.sbuf("osbn%d" % i, [64, 256], F32) for i in range(2)]
        it = 0
        for h in (range(8) if heads is None else heads):
            kb.dma("sp", bias[:], cx.na_bias[l, h], reads=[cx.na_bias], writes=[bias])
            kb.op("act", lambda: nc.scalar.activation(out=bias[:], in_=bias[:], func=AF.Exp), reads=[bias], writes=[bias])
            kb.op("pool", lambda: nc.gpsimd.tensor_tensor(out=EB[:], in0=bias[:], in1=vmask[:], op=ALU.mult),
                  reads=[bias, vmask], writes=[EB])
            kb.dma("sp", qT[:], cx.QB[h * 64:(h + 1) * 64, :], reads=[cx.QB], writes=[qT])
            kb.dma("sp", kT[:], cx.KB_[h * 64:(h + 1) * 64, :], reads=[cx.KB_], writes=[kT])
            kb.dma("sp", vt[:], cx.VB_t[:, :, h * 64:(h + 1) * 64], reads=[cx.VB_t], writes=[vt])
            for qb in range(NT):
                if qb < 32:
                    keys = list(keys_tab[qb]) + [(32, None), (33, None)]
                else:
                    keys = [(32, None), (33, None)]
                qs = slice(qb * 128, (qb + 1) * 128)
                a = qb % 2
                po = pO[a]
                pd = pD[a]
                nk = len(keys)
                grp = [keys[i:i + 4] for i in range(0, nk, 4)]
                done = 0
                for gk in grp:
                    ps = pS[it % 4]
                    p_ = pt[it % R]
                    it += 1
                    ng = len(gk)
                    kb.mm([(lambda ps=ps, j=j, kt_i=kt_i: nc.tensor.matmul(
                        ps[:, j * 128:(j + 1) * 128], lhsT=kT[:, kt_i * 128:(kt_i + 1) * 128], rhs=qT[:, qs],
                        start=True, stop=True)) for j, (kt_i, _) in enumerate(gk)],
                        reads=[kT, qT], writes=[ps])
                    kb.op("act", lambda ps=ps, p_=p_, ng=ng: nc.scalar.activation(
                        out=p_[:, :ng, :].rearrange("p h q -> p (h q)"), in_=ps[:, :ng * 128], func=AF.Exp, scale=0.125),
                        reads=[ps], writes=[p_])
                    nloc = sum(1 for (_, pid) in gk if pid is not None)
                    if nloc:
                        pid0 = gk[0][1]
                        eng = "dve" if it % 2 == 0 else "pool"
                        h_ = nc.vector if eng == "dve" else nc.gpsimd
                        kb.op(eng, lambda h_=h_, p_=p_, pid0=pid0, nloc=nloc: h_.tensor_tensor(
                            out=p_[:, :nloc, :], in0=p_[:, :nloc, :], in1=EB[:, pid0:pid0 + nloc, :], op=ALU.mult),
                            reads=[p_, EB], writes=[p_])
                    fns = []
                    for j, (kt_i, _) in enumerate(gk):
                        first = (done == 0)
                        last = (done == nk - 1)
                        fns.append(lambda po=po, p_=p_, j=j, kt_i=kt_i, first=first, last=last: nc.tensor.matmul(
                            po[:64, 0:128], lhsT=vt[:, kt_i, :], rhs=p_[:, j, :], start=first, stop=last))
                        fns.append(lambda pd=pd, p_=p_, j=j, first=first, last=last: nc.tensor.matmul(
                            pd[:64, 0:128], lhsT=ones64[:], rhs=p_[:, j, :], start=first, stop=last))
                        done += 1
                    kb.mm(fns, reads=[vt, p_, ones64], writes=[po, pd])
                o_ = osb[a]
                kb.op("act", lambda po=po, o_=o_: nc.scalar.copy(out=o_[:, 0:128], in_=po[:64, 0:128]), reads=[po], writes=[o_])
                kb.op("act", lambda pd=pd, o_=o_: nc.scalar.copy(out=o_[:, 128:256], in_=pd[:64, 0:128]), reads=[pd, o_], writes=[o_])
                kb.op("dve", lambda o_=o_: nc.vector.reciprocal(out=o_[:, 128:256], in_=o_[:, 128:256]), reads=[o_], writes=[o_])
                kb.op("pool", lambda o_=o_: nc.gpsimd.tensor_tensor(
                    out=ost[:, qs], in0=o_[:, 0:128], in1=o_[:, 128:256], op=ALU.mult),
                    reads=[o_], writes=[], acc_writes=[ost])
            kb.dma("sp", cx.BR_B[h * 64:(h + 1) * 64, :], ost[:], reads=[ost], writes=[], acc_writes=[cx.BR_B])

LN_EPS = 1e-5
MB = 256
MBLKS = [(i * MB, MB) for i in range(T // MB)]


def ln_setup(cx, kb, nc):
    ones = kb.sbuf("ln_ones", [128, 128], F32)
    kb.op("dve", lambda: nc.vector.memset(ones[:], 1.0 / D), writes=[ones])
    eps = kb.sbuf("ln_eps", [128, 1], F32)
    kb.op("dve", lambda: nc.vector.memset(eps[:], LN_EPS), writes=[eps])
    return ones, eps


def ln_block(kb, nc, r, nb, gam, bet, out, ones, eps, scr):
    rsq, ps1, ps2, mean, var = scr
    kb.op("act", lambda: nc.scalar.activation(out=rsq[:, :, :nb], in_=r[:, :, :nb], func=AF.Square), reads=[r], writes=[rsq])
    kb.mm([(lambda oc=oc: nc.tensor.matmul(ps1[:, :nb], lhsT=ones[:], rhs=r[:, oc, :nb], start=(oc == 0), stop=(oc == 7)))
           for oc in range(8)], reads=[ones, r], writes=[ps1])
    kb.mm([(lambda oc=oc: nc.tensor.matmul(ps2[:, :nb], lhsT=ones[:], rhs=rsq[:, oc, :nb], start=(oc == 0), stop=(oc == 7)))
           for oc in range(8)], reads=[ones, rsq], writes=[ps2])
    kb.op("act", lambda: nc.scalar.copy(out=mean[:, :nb], in_=ps1[:, :nb]), reads=[ps1], writes=[mean])
    kb.op("act", lambda: nc.scalar.copy(out=var[:, :nb], in_=ps2[:, :nb]), reads=[ps2], writes=[var])
    m2 = rsq
    kb.op("dve", lambda: nc.vector.tensor_tensor(out=m2[:, 0, :nb], in0=mean[:, :nb], in1=mean[:, :nb], op=ALU.mult),
          reads=[mean, rsq], writes=[rsq])
    kb.op("dve", lambda: nc.vector.tensor_tensor(out=var[:, :nb], in0=var[:, :nb], in1=m2[:, 0, :nb], op=ALU.subtract),
          reads=[var, rsq], writes=[var])
    kb.op("act", lambda: nc.scalar.activation(out=var[:, :nb], in_=var[:, :nb], func=AF.Sqrt, bias=eps[:]),
          reads=[var, eps], writes=[var])
    kb.op("dve", lambda: nc.vector.reciprocal(out=var[:, :nb], in_=var[:, :nb]), reads=[var], writes=[var])
    kb.op("dve", lambda: nc.vector.tensor_tensor(
        out=r[:, :, :nb], in0=r[:, :, :nb], in1=mean[:, :nb].unsqueeze(1).broadcast_to([128, 8, nb]), op=ALU.subtract),
        reads=[r, mean], writes=[r])
    kb.op("pool", lambda: nc.gpsimd.tensor_tensor(
        out=r[:, :, :nb], in0=r[:, :, :nb], in1=var[:, :nb].unsqueeze(1).broadcast_to([128, 8, nb]), op=ALU.mult),
        reads=[r, var], writes=[r])
    for oc in range(8):
        eng = "dve" if oc % 2 == 0 else "pool"
        h_ = nc.vector if eng == "dve" else nc.gpsimd
        kb.op(eng, lambda h_=h_, oc=oc: h_.tensor_scalar(
            out=out[:, oc, :nb], in0=r[:, oc, :nb], scalar1=gam[:, oc:oc + 1], scalar2=bet[:, oc:oc + 1],
            op0=ALU.mult, op1=ALU.add), reads=[r, gam, bet], writes=[out])


def load_ln_params(cx, kb, nc, l, which):
    gam = kb.sbuf("ln_g", [128, 8], F32)
    bet = kb.sbuf("ln_b", [128, 8], F32)
    kb.dma("sp", gam[:], cx.ln_gT[l, which], reads=[cx.ln_gT], writes=[gam])
    kb.dma("sp", bet[:], cx.ln_bT[l, which], reads=[cx.ln_bT], writes=[bet])
    return gam, bet


def stage_merge(cx, kb, nc, l, xsrc, modv):
    with kb.scope():
        ones, eps = ln_setup(cx, kb, nc)
        gam, bet = load_ln_params(cx, kb, nc, l, 0)
        wbr = kb.sbuf("wbr", [128, 4, 4, 1024], BF16)
        wout = kb.sbuf("wout", [128, 8, 1024], BF16)
        for i in range(4):
            kb.dma("pool", wbr[:, i], cx.w_branch[l, i].rearrange("(kc p) c -> p kc c", p=128), reads=[cx.w_branch], writes=[wbr])
        kb.dma("pool", wout[:], cx.w_out[l].rearrange("(kc p) c -> p kc c", p=128), reads=[cx.w_out], writes=[wout])
        wr = kb.sbuf("wr", [128, 8, 36], F32)
        kb.dma("sp", wr[:, :, 0:4], cx.moe_w_group[l].rearrange("(kc p) c -> p kc c", p=128), reads=[cx.moe_w_group], writes=[wr])
        kb.dma("sp", wr[:, :, 4:36], cx.moe_w_expert[l].rearrange("(kc p) c -> p kc c", p=128), reads=[cx.moe_w_expert], writes=[wr])
        rb = kb.sbuf("rb", [128, 36], F32)
        kb.dma("sp", rb[:, 0:4], cx.moe_b_group[l].partition_broadcast(128), reads=[cx.moe_b_group], writes=[rb])
        kb.dma("sp", rb[:, 4:36], cx.moe_b_expert[l].partition_broadcast(128), reads=[cx.moe_b_expert], writes=[rb])
        onep4 = kb.sbuf("onep4", [128, 8, 2], F32)
        kb.op("dve", lambda: nc.vector.tensor_scalar(out=onep4[:], in0=modv[:, 32:40, :], scalar1=1.0, scalar2=None, op0=ALU.add),
              reads=[modv], writes=[onep4])
        brt = [kb.sbuf("brt%d" % i, [128, 4, 4, MB], BF16) for i in range(1)]
        gtt = kb.sbuf("gtt", [128, 32, MB], BF16)
        xb = kb.sbuf("xbm", [128, 8, MB], F32)
        mrg = kb.sbuf("mrg", [128, 8, MB], BF16)
        r = kb.sbuf("rm", [128, 8, MB], F32)
        hx2b = kb.sbuf("hx2b", [128, 8, MB], BF16)
        rsq = kb.sbuf("rsqm", [128, 8, MB], F32)
        x1 = r
        hx2f = rsq
        mean = kb.sbuf("meanm", [128, MB], F32)
        var = kb.sbuf("varm", [128, MB], F32)
        csb = [kb.sbuf("csb%d" % i, [128, MB], F32) for i in range(4)]
        acc = kb.sbuf("accm", [128, MB], F32)
        mixs = kb.sbuf("mixs", [128, MB], F32)
        pbr = [kb.psum("pbr%d" % i, [128, MB]) for i in range(4)]
        pmx = kb.psum("pmx", [128, MB])
        ps1 = kb.psum("ps1m", [128, MB])
        ps2 = kb.psum("ps2m", [128, MB])
        prt = kb.psum("prt", [128, MB])
        brs = [cx.BR_A, cx.BR_B, cx.BR_C, cx.BR_D]
        lgt = kb.sbuf("lgt", [128, 36], F32)
        gmx = kb.sbuf("gmx", [128, 1], F32)
        ngm = kb.sbuf("ngm", [128, 1], F32)
        ge = kb.sbuf("ge", [128, 4], F32)
        gs = kb.sbuf("gsr", [128, 1], F32)
        oh = kb.sbuf("oh", [128, 4], F32)
        lem = kb.sbuf("lem", [128, 4, 8], F32)
        mx8 = kb.sbuf("mx8", [128, 8], F32)
        dd = kb.sbuf("dd", [128, 1], F32)
        w1 = kb.sbuf("w1", [128, 1], F32)
        w2 = kb.sbuf("w2", [128, 1], F32)
        tt1 = kb.sbuf("tt1", [128, 32], F32)
        tt2 = kb.sbuf("tt2", [128, 32], F32)
        wgt = kb.sbuf("wgt", [128, 32], F32)
        for bi, (t0, nb) in enumerate(MBLKS):
            col = blkcol(t0)
            b_ = brt[0]
            for i in range(4):
                kb.dma("sp", b_[:, i, :, :nb], brs[i][:, t0:t0 + nb].rearrange("(kc p) t -> p kc t", p=128),
                       reads=[brs[i]], writes=[b_])
            kb.dma("sp", gtt[:, :, :nb], cx.GATE[:, t0:t0 + nb].rearrange("(j p) t -> p j t", p=128), reads=[cx.GATE], writes=[gtt])
            kb.dma("sp", xb[:, :, :nb], xsrc[:, t0:t0 + nb].rearrange("(kc p) t -> p kc t", p=128), reads=[xsrc], writes=[xb])
            for oc in range(8):
                for i in range(4):
                    kb.mm([(lambda i=i, kc=kc, oc=oc: nc.tensor.matmul(
                        pbr[i][:, :nb], lhsT=wbr[:, i, kc, oc * 128:(oc + 1) * 128], rhs=b_[:, i, kc, :nb],
                        start=(kc == 0), stop=(kc == 3))) for kc in range(4)], reads=[wbr, b_], writes=[pbr[i]])
                    kb.op("act", lambda i=i: nc.scalar.copy(out=csb[i][:, :nb], in_=pbr[i][:, :nb]), reads=[pbr[i]], writes=[csb[i]])
                    eng = "dve" if i % 2 == 0 else "pool"
                    h_ = nc.vector if eng == "dve" else nc.gpsimd
                    kb.op(eng, lambda h_=h_, i=i, oc=oc: h_.tensor_tensor(
                        out=csb[i][:, :nb], in0=csb[i][:, :nb], in1=gtt[:, i * 8 + oc, :nb], op=ALU.mult),
                        reads=[csb[i], gtt], writes=[csb[i]])
                kb.op("dve", lambda: nc.vector.tensor_tensor(out=acc[:, :nb], in0=csb[0][:, :nb], in1=csb[1][:, :nb], op=ALU.add),
                      reads=[csb[0], csb[1]], writes=[acc])
                kb.op("pool", lambda: nc.gpsimd.tensor_tensor(out=csb[2][:, :nb], in0=csb[2][:, :nb], in1=csb[3][:, :nb], op=ALU.add),
                      reads=[csb[2], csb[3]], writes=[csb[2]])
                kb.op("dve", lambda oc=oc: nc.vector.tensor_tensor(out=mrg[:, oc, :nb], in0=acc[:, :nb], in1=csb[2][:, :nb], op=ALU.add),
                      reads=[acc, csb[2]], writes=[mrg])
            for oc in range(8):
                kb.mm([(lambda kc=kc, oc=oc: nc.tensor.matmul(
                    pmx[:, :nb], lhsT=wout[:, kc, oc * 128:(oc + 1) * 128], rhs=mrg[:, kc, :nb],
                    start=(kc == 0), stop=(kc == 7))) for kc in range(8)], reads=[wout, mrg], writes=[pmx])
                kb.op("act", lambda: nc.scalar.copy(out=mixs[:, :nb], in_=pmx[:, :nb]), reads=[pmx], writes=[mixs])
                kb.op("pool", lambda oc=oc: nc.gpsimd.tensor_scalar(
                    out=mixs[:, :nb], in0=mixs[:, :nb], scalar1=modv[:, 16 + oc, col:col + 1], scalar2=None, op0=ALU.mult),
                    reads=[mixs, modv], writes=[mixs])
                kb.op("dve", lambda oc=oc: nc.vector.scalar_tensor_tensor(
                    out=r[:, oc, :nb], in0=xb[:, oc, :nb], scalar=ALPHA, in1=mixs[:, :nb], op0=ALU.mult, op1=ALU.add),
                    reads=[xb, mixs], writes=[r])
            ln_block(kb, nc, r, nb, gam, bet, x1, ones, eps, (rsq, ps1, ps2, mean, var))
            kb.dma("sp", cx.XT[:, t0:t0 + nb].rearrange("(kc p) t -> p kc t", p=128), x1[:, :, :nb],
                   reads=[x1], writes=[], acc_writes=[cx.XT])
            for oc in range(8):
                eng = "dve" if oc % 2 == 0 else "pool"
                h_ = nc.vector if eng == "dve" else nc.gpsimd
                kb.op(eng, lambda h_=h_, oc=oc: h_.tensor_scalar(
                    out=hx2f[:, oc, :nb], in0=x1[:, oc, :nb], scalar1=onep4[:, oc, col:col + 1],
                    scalar2=modv[:, 24 + oc, col:col + 1], op0=ALU.mult, op1=ALU.add),
                    reads=[x1, onep4, modv], writes=[hx2f])
            kb.op("act", lambda: nc.scalar.copy(out=hx2b[:, :, :nb], in_=hx2f[:, :, :nb]), reads=[hx2f], writes=[hx2b])
            kb.dma("sp", cx.HX2[:, t0:t0 + nb].rearrange("(kc p) t -> p kc t", p=128), hx2b[:, :, :nb],
                   reads=[hx2b], writes=[], acc_writes=[cx.HX2])
            for tj in range(nb // 128):
                ti = t0 // 128 + tj
                tsl = slice(tj * 128, (tj + 1) * 128)
                kb.mm([(lambda kc=kc: nc.tensor.matmul(prt[:, 0:36], lhsT=hx2f[:, kc, tsl], rhs=wr[:, kc, :],
                                                       start=(kc == 0), stop=(kc == 7))) for kc in range(8)],
                      reads=[hx2f, wr], writes=[prt])
                kb.op("act", lambda: nc.scalar.copy(out=lgt[:], in_=prt[:, 0:36]), reads=[prt], writes=[lgt])
                kb.op("dve", lambda: nc.vector.tensor_tensor(out=lgt[:], in0=lgt[:], in1=rb[:], op=ALU.add), reads=[lgt, rb], writes=[lgt])
                kb.op("dve", lambda: nc.vector.tensor_reduce(out=gmx[:], in_=lgt[:, 0:4], axis=AX.X, op=ALU.max), reads=[lgt], writes=[gmx])
                kb.op("dve", lambda: nc.vector.tensor_scalar(out=ngm[:], in0=gmx[:], scalar1=-1.0, scalar2=None, op0=ALU.mult), reads=[gmx], writes=[ngm])
                kb.op("act", lambda: nc.scalar.activation(out=ge[:], in_=lgt[:, 0:4], func=AF.Exp, bias=ngm[:]), reads=[lgt, ngm], writes=[ge])
                kb.op("dve", lambda: nc.vector.tensor_reduce(out=gs[:], in_=ge[:], axis=AX.X, op=ALU.add), reads=[ge], writes=[gs])
                kb.op("dve", lambda: nc.vector.reciprocal(out=gs[:], in_=gs[:]), reads=[gs], writes=[gs])
                kb.op("dve", lambda: nc.vector.tensor_scalar(out=oh[:], in0=lgt[:, 0:4], scalar1=gmx[:], scalar2=None, op0=ALU.is_equal), reads=[lgt, gmx], writes=[oh])
                kb.op("dve", lambda: nc.vector.tensor_scalar(out=oh[:], in0=oh[:], scalar1=-1.0, scalar2=1e30, op0=ALU.add, op1=ALU.mult), reads=[oh], writes=[oh])
                kb.op("dve", lambda: nc.vector.tensor_tensor(
                    out=lem[:], in0=lgt[:, 4:36].rearrange("p (g e) -> p g e", e=8), in1=oh[:].unsqueeze(2).broadcast_to([128, 4, 8]), op=ALU.add),
                    reads=[lgt, oh], writes=[lem])
                lemf = lem[:].rearrange("p g e -> p (g e)")
                kb.op("dve", lambda: nc.vector.max(out=mx8[:], in_=lem[:].rearrange("p g e -> p (g e)")), reads=[lem], writes=[mx8])
                kb.op("dve", lambda: nc.vector.tensor_tensor(out=dd[:], in0=mx8[:, 1:2], in1=mx8[:, 0:1], op=ALU.subtract), reads=[mx8], writes=[dd])
                kb.op("act", lambda: nc.scalar.activation(out=dd[:], in_=dd[:], func=AF.Exp), reads=[dd], writes=[dd])
                kb.op("dve", lambda: nc.vector.tensor_scalar(out=w1[:], in0=dd[:], scalar1=1.0, scalar2=None, op0=ALU.add), reads=[dd], writes=[w1])
                kb.op("dve", lambda: nc.vector.reciprocal(out=w1[:], in_=w1[:]), reads=[w1], writes=[w1])
                kb.op("dve", lambda: nc.vector.tensor_tensor(out=w2[:], in0=dd[:], in1=w1[:], op=ALU.mult), reads=[dd, w1], writes=[w2])
                kb.op("dve", lambda: nc.vector.tensor_scalar(
                    out=tt1[:], in0=lem[:].rearrange("p g e -> p (g e)"), scalar1=mx8[:, 0:1], scalar2=w1[:], op0=ALU.is_equal, op1=ALU.mult),
                    reads=[lem, mx8, w1], writes=[tt1])
                kb.op("dve", lambda: nc.vector.tensor_scalar(
                    out=tt2[:], in0=lem[:].rearrange("p g e -> p (g e)"), scalar1=mx8[:, 1:2], scalar2=w2[:], op0=ALU.is_equal, op1=ALU.mult),
                    reads=[lem, mx8, w2], writes=[tt2])
                kb.op("dve", lambda: nc.vector.tensor_tensor(out=tt1[:], in0=tt1[:], in1=tt2[:], op=ALU.add), reads=[tt1, tt2], writes=[tt1])
                kb.op("dve", lambda: nc.vector.tensor_scalar(out=wgt[:], in0=tt1[:], scalar1=gs[:], scalar2=None, op0=ALU.mult), reads=[tt1, gs], writes=[wgt])
                kb.dma("sp", cx.WG_t[:, ti, :], wgt[:], reads=[wgt], writes=[], acc_writes=[cx.WG_t])


def stage_moe(cx, kb, nc, l, modv, yout=None):
    with kb.scope():
        ones, eps = ln_setup(cx, kb, nc)
        gam, bet = load_ln_params(cx, kb, nc, l, 1)
        wgT = kb.sbuf("wgT", [32, T], F32)
        wgl = kb.sbuf("wgl", [128, NT, 32], F32)
        id32 = kb.sbuf("id32", [128, 128], F32)
        sel = kb.sbuf("sel", [32, 32, 128], F32)
        kb.dma("sp", wgl[:], cx.WG_t[:], reads=[cx.WG_t], writes=[wgl])
        kb.dma("sp", id32[:], cx.m_ident32[:], reads=[cx.m_ident32], writes=[id32])
        kb.dma("sp", sel[:], cx.m_sel[:], reads=[cx.m_sel], writes=[sel])
        ptr = [kb.psum("ptr%d" % i, [128, 512]) for i in range(2)]
        for ti in range(NT):
            p_ = ptr[ti % 2]
            kb.mm([lambda p_=p_, ti=ti: nc.tensor.matmul(p_[:32, 0:128], lhsT=wgl[:, ti, :], rhs=id32[:], start=True, stop=True)],
                  reads=[wgl, id32], writes=[p_])
            kb.op("act", lambda p_=p_, ti=ti: nc.scalar.copy(out=wgT[:, ti * 128:(ti + 1) * 128], in_=p_[:32, 0:128]),
                  reads=[p_], writes=[], acc_writes=[wgT])
        hx = kb.sbuf("hxm", [128, 8, 512], BF16)
        yacc = kb.sbuf("yacc", [128, 8, 512], F32)
        hid = kb.sbuf("hid", [128, 16, 512], BF16)
        wdg = kb.sbuf("wdg", [128, 8, 2, 1024], BF16)
        wgs = [kb.sbuf("wgs%d" % i, [128, 8, 256], BF16) for i in range(2)]
        wus = [kb.sbuf("wus%d" % i, [128, 8, 256], BF16) for i in range(2)]
        xb = kb.sbuf("xbe", [128, 8, 512], F32)
        rsq = kb.sbuf("rsqe", [128, 8, 512], F32)
        mean = kb.sbuf("meane", [128, 512], F32)
        var = kb.sbuf("vare", [128, 512], F32)
        reps = [kb.sbuf("reps%d" % i, [128, 512], F32) for i in range(2)]
        ssb = [kb.sbuf("ssb%d" % i, [128, 512], F32) for i in range(2)]
        tsb = [kb.sbuf("tsb%d" % i, [128, 512], F32) for i in range(2)]
        ytmp = [kb.sbuf("ytmp%d" % i, [128, 512], F32) for i in range(2)]
        pg = [kb.psum("pg%d" % i, [128, 512]) for i in range(2)]
        pu = [kb.psum("pu%d" % i, [128, 512]) for i in range(2)]
        py = ptr
        ps1 = kb.psum("ps1e", [128, 512])
        ps2 = kb.psum("ps2e", [128, 512])
        prep = ps1
        nw = 0
        it = 0
        for bi, (t0, nb) in enumerate(BLKS):
            col = blkcol(t0)
            kb.dma("sp", hx[:, :, :nb], cx.HX2[:, t0:t0 + nb].rearrange("(kc p) t -> p kc t", p=128), reads=[cx.HX2], writes=[hx])
            kb.dma("sp", xb[:, :, :nb], cx.XT[:, t0:t0 + nb].rearrange("(kc p) t -> p kc t", p=128), reads=[cx.XT], writes=[xb])
            for g in range(4):
                kb.dma("pool", wdg[:].rearrange("p e hc d -> p (e hc) d"),
                       cx.moe_w_down[l, g].rearrange("e (hc p) d -> p (e hc) d", p=128), reads=[cx.moe_w_down], writes=[wdg])
                for e_ in range(8):
                    e = g * 8 + e_
                    wg_, wu_ = wgs[nw % 2], wus[nw % 2]
                    nw += 1
                    kb.dma("pool", wg_[:], cx.moe_w_gate[l, g, e_].rearrange("(kc p) f -> p kc f", p=128), reads=[cx.moe_w_gate], writes=[wg_])
                    kb.dma("pool", wu_[:], cx.moe_w_up[l, g, e_].rearrange("(kc p) f -> p kc f", p=128), reads=[cx.moe_w_up], writes=[wu_])
                    rp = reps[e % 2]
                    kb.mm([lambda e=e: nc.tensor.matmul(prep[:, :nb], lhsT=sel[:, e, :], rhs=wgT[:, t0:t0 + nb], start=True, stop=True)],
                          reads=[sel, wgT], writes=[prep])
                    kb.op("act", lambda rp=rp: nc.scalar.copy(out=rp[:, :nb], in_=prep[:, :nb]), reads=[prep], writes=[rp])
                    for hc in range(2):
                        a = it % 2
                        it += 1
                        kb.mm([(lambda kc=kc, a=a, hc=hc, wg_=wg_: nc.tensor.matmul(
                            pg[a][:, :nb], lhsT=wg_[:, kc, hc * 128:(hc + 1) * 128], rhs=hx[:, kc, :nb],
                            start=(kc == 0), stop=(kc == 7))) for kc in range(8)], reads=[wg_, hx], writes=[pg[a]])
                        kb.mm([(lambda kc=kc, a=a, hc=hc, wu_=wu_: nc.tensor.matmul(
                            pu[a][:, :nb], lhsT=wu_[:, kc, hc * 128:(hc + 1) * 128], rhs=hx[:, kc, :nb],
                            start=(kc == 0), stop=(kc == 7))) for kc in range(8)], reads=[wu_, hx], writes=[pu[a]])
                        kb.op("act", lambda a=a: nc.scalar.activation(out=ssb[a][:, :nb], in_=pg[a][:, :nb], func=AF.Silu),
                              reads=[pg[a]], writes=[ssb[a]])
                        kb.op("act", lambda a=a: nc.scalar.copy(out=tsb[a][:, :nb], in_=pu[a][:, :nb]), reads=[pu[a]], writes=[tsb[a]])
                        kb.op("dve", lambda a=a: nc.vector.tensor_tensor(out=tsb[a][:, :nb], in0=tsb[a][:, :nb], in1=ssb[a][:, :nb], op=ALU.mult),
                              reads=[tsb[a], ssb[a]], writes=[tsb[a]])
                        kb.op("pool", lambda a=a, rp=rp, e_=e_, hc=hc: nc.gpsimd.tensor_tensor(
                            out=hid[:, e_ * 2 + hc, :nb], in0=tsb[a][:, :nb], in1=rp[:, :nb], op=ALU.mult),
                            reads=[tsb[a], rp], writes=[], acc_writes=[hid])
                for oc in range(8):
                    p_ = py[oc % 2]
                    fns = []
                    for j in range(16):
                        fns.append(lambda j=j, oc=oc, p_=p_: nc.tensor.matmul(
                            p_[:, :nb], lhsT=wdg[:, j // 2, j % 2, oc * 128:(oc + 1) * 128], rhs=hid[:, j, :nb],
                            start=(j == 0), stop=(j == 15)))
                    kb.mm(fns, reads=[wdg, hid], writes=[p_])
                    if g == 0:
                        kb.op("act", lambda oc=oc, p_=p_: nc.scalar.copy(out=yacc[:, oc, :nb], in_=p_[:, :nb]), reads=[p_], writes=[], acc_writes=[yacc])
                    else:
                        yt = ytmp[oc % 2]
                        kb.op("act", lambda yt=yt, p_=p_: nc.scalar.copy(out=yt[:, :nb], in_=p_[:, :nb]), reads=[p_], writes=[yt])
                        eng = "dve" if oc % 2 == 0 else "pool"
                        h_ = nc.vector if eng == "dve" else nc.gpsimd
                        kb.op(eng, lambda h_=h_, oc=oc, yt=yt: h_.tensor_tensor(
                            out=yacc[:, oc, :nb], in0=yacc[:, oc, :nb], in1=yt[:, :nb], op=ALU.add),
                            reads=[yt, yacc], writes=[], acc_writes=[yacc])
            for oc in range(8):
                kb.op("pool", lambda oc=oc: nc.gpsimd.tensor_scalar(
                    out=yacc[:, oc, :nb], in0=yacc[:, oc, :nb], scalar1=modv[:, 40 + oc, col:col + 1], scalar2=None, op0=ALU.mult),
                    reads=[yacc, modv], writes=[yacc])
                kb.op("dve", lambda oc=oc: nc.vector.scalar_tensor_tensor(
                    out=yacc[:, oc, :nb], in0=xb[:, oc, :nb], scalar=ALPHA, in1=yacc[:, oc, :nb], op0=ALU.mult, op1=ALU.add),
                    reads=[xb, yacc], writes=[yacc])
            ln_block(kb, nc, yacc, nb, gam, bet, yacc, ones, eps, (rsq, ps1, ps2, mean, var))
            kb.dma("sp", cx.XT[:, t0:t0 + nb].rearrange("(kc p) t -> p kc t", p=128), yacc[:, :, :nb],
                   reads=[yacc], writes=[], acc_writes=[cx.XT])
            if yout is not None and t0 < TL:
                kb.dma("sp", yout[:, t0:t0 + nb].rearrange("(kc p) t -> p kc t", p=128), yacc[:, :, :nb],
                       reads=[yacc], writes=[], acc_writes=[yout])


def rope_tables():
    t = np.arange(TL)
    row = (t // 64).astype(np.float32)
    colp = (t % 64).astype(np.float32)
    nf = 16
    inv = (np.float32(10000.0) ** (-np.arange(nf, dtype=np.float32) / np.float32(nf))).astype(np.float32)
    cos = np.ones((128, T), np.float32)
    sin = np.zeros((128, T), np.float32)
    for q in range(128):
        d = q % 64
        pos = row if d < 32 else colp
        ang = (pos * inv[d % 16]).astype(np.float32)
        cos[q, :TL] = np.cos(ang).astype(np.float32)
        sin[q, :TL] = np.sin(ang).astype(np.float32)
    pt = np.zeros((128, 128), np.float32)
    for m in range(128):
        if (m % 32) < 16:
            pt[m + 16, m] = -1.0
        else:
            pt[m - 16, m] = 1.0
    return cos, sin, pt


_CONST = {}
_NAIDX = {}


def consts():
    if not _CONST:
        cos, sin, pt = rope_tables()
        _CONST.update(rope_cos=cos, rope_sin=sin, rope_pt=pt)
        _CONST.update(scan_masks())
        _CONST.update(wa_masks())
        sel = np.zeros((32, 32, 128), np.float32)
        for e in range(32):
            sel[e, e, :] = 1.0
        _CONST["m_sel"] = sel
        _CONST["m_ident32"] = np.eye(128, dtype=np.float32)
        _k, _valid, _ri, _ci = na_consts()
        _CONST["m_navalid"] = _valid
        _NAIDX["ri"] = _ri
        _NAIDX["ci"] = _ci
    return _CONST


def core_inputs(inp, b):
    f = lambda a: np.ascontiguousarray(a, dtype=np.float32)
    m = {}
    m["xin"] = f(np.concatenate([inp["x"][b].T, inp["ctx"][b].T], axis=1))
    cv = np.stack([inp["c"][b].reshape(8, 128).T, inp["c_ctx"].reshape(8, 128).T], axis=2)
    m["cvec"] = f(cv)
    m["w_mod"] = f(inp["w_mod"])
    m["b_modT"] = f(inp["b_mod"].reshape(2, 48, 128).transpose(0, 2, 1))
    m["w_in"] = f(inp["w_in"])
    m["hg_gamma"] = f(inp["hg_gamma"])
    m["gla_lr_w"] = f(inp["gla_lr_w"])
    m["gla_lr_b"] = f(inp["gla_lr_b"])
    m["wa_sink"] = f(inp["wa_sink"])
    for k in ("w_branch", "w_out", "moe_w_group", "moe_b_group", "moe_w_expert", "moe_b_expert",
              "moe_w_gate", "moe_w_up", "moe_w_down"):
        m[k] = f(inp[k])
    m["ln_gT"] = f(inp["ln_g"].reshape(2, 2, 8, 128).transpose(0, 1, 3, 2))
    m["ln_bT"] = f(inp["ln_b"].reshape(2, 2, 8, 128).transpose(0, 1, 3, 2))
    consts()
    m["na_bias"] = f(inp["na_rpb"][:, :, _NAIDX["ri"], _NAIDX["ci"]])
    m.update(consts())
    return m


FUSED = 0


def build_program(layers, final):
    nc = bass.Bass("TRN2", target_bir_lowering=False)
    cx = Ctx()
    declare_inputs(cx, nc)
    if final:
        yout = Buf(nc.dram_tensor("yout", [D, TL], F32, kind="ExternalOutput"), "yout")
    else:
        yout = None
    with ExitStack() as st:
        kb = KB(nc, st)
        declare_scratch(cx, kb, debug=False, as_output=(() if final else ("XT",)))
        modT = [kb.sbuf("modT%d" % l, [128, 48, 2], F32) for l in range(2)]
        stage_mod(cx, kb, nc, modT)
        first = True
        for l in layers:
            xsrc = cx.xin if first else cx.XT
            first = False
            with kb.scope():
                lbt = [kb.sbuf("lbt%d" % i, [128, 1024], F32) for i in range(2)]
                omlt = [kb.sbuf("omlt%d" % i, [128, 1024], F32) for i in range(2)]
                stage_lb(cx, kb, nc, lbt, omlt)
                hxT = kb.sbuf("hxT", [128, 8, T], BF16)
                stage_modulate(cx, kb, nc, xsrc, modT[l], 0, 1, hxT)
                stage_inproj_fm(cx, kb, nc, l, hxT)
                stage_inproj_tm(cx, kb, nc, l, hxT, lbt[l], omlt[l])
            stage_gla_gate(cx, kb, nc, l)
            stage_scan(cx, kb, nc, "gla")
            stage_scan(cx, kb, nc, "hgrn")
            stage_wa(cx, kb, nc, l)
            stage_na(cx, kb, nc, l)
            stage_merge(cx, kb, nc, l, xsrc, modT[l])
            stage_moe(cx, kb, nc, l, modT[l], yout if (final and l == layers[-1]) else None)
        if final:
            kb.finish([yout])
        else:
            kb.finish([cx.XT])
    return nc


def kernel(**inputs):
    inp = {k: np.asarray(v) for k, v in inputs.items()}
    in_maps = [core_inputs(inp, b) for b in range(4)]
    if FUSED:
        nc = build_program([0, 1], True)
        res = run_bass_kernel_spmd(nc, in_maps, core_ids=list(range(4)))
    else:
        nc0 = build_program([0], False)
        res0 = run_bass_kernel_spmd(nc0, in_maps, core_ids=list(range(4)))
        for b in range(4):
            in_maps[b]["xin"] = np.ascontiguousarray(res0.results[b]["XT"])
        nc1 = build_program([1], True)
        res = run_bass_kernel_spmd(nc1, in_maps, core_ids=list(range(4)))
    out = np.stack([np.ascontiguousarray(res.results[b]["yout"].T) for b in range(4)], axis=0)
    return out.astype(np.float32)
```

```python
import numpy as np
from contextlib import ExitStack
import concourse.bass as bass
import concourse.mybir as mybir
from concourse.bass_utils import run_bass_kernel_spmd
import os

F32 = mybir.dt.float32
BF16 = mybir.dt.bfloat16
U32 = mybir.dt.uint32
AF = mybir.ActivationFunctionType
ALU = mybir.AluOpType
AX = mybir.AxisListType


class Buf:
    __slots__ = ("t", "w", "r", "name")

    def __init__(self, t, name=""):
        self.t = t
        self.w = []
        self.r = []
        self.name = name

    def __getitem__(self, idx):
        return self.t[idx]


class Dep:
    __slots__ = ("w", "r")

    def __init__(self):
        self.w = []
        self.r = []


class Eng:
    def __init__(self, name, h, sem):
        self.name = name
        self.h = h
        self.sem = sem
        self.count = 0
        self.seen = {}


class KB:
    def __init__(self, nc, stack, n_dma_sems=8):
        self.nc = nc
        self.stack = stack
        self.sems = {}
        self.engs = {}
        for name, h in (("pe", nc.tensor), ("act", nc.scalar), ("dve", nc.vector),
                        ("pool", nc.gpsimd), ("sp", nc.sync)):
            s = stack.enter_context(nc.semaphore("s_" + name))
            self.sems[id(s)] = s
            self.engs[name] = Eng(name, h, s)
        self.dma_pool = {}
        for q in ("sp", "pool", "act"):
            lst = []
            for i in range(n_dma_sems):
                s = stack.enter_context(nc.semaphore("d_%s%d" % (q, i)))
                self.sems[id(s)] = s
                lst.append([s, 0])
            self.dma_pool[q] = [lst, 0]
        self.ninstr = 0

    def scope(self):
        kb = self

        class _Scope:
            def __enter__(self_s):
                self_s.prev = kb.stack
                self_s.st = ExitStack()
                self_s.st.__enter__()
                kb.stack = self_s.st
                kb.nscope = getattr(kb, "nscope", 0) + 1
                return self_s

            def __exit__(self_s, *a):
                if a[0] is None:
                    kb.barrier()
                kb.stack = self_s.prev
                return self_s.st.__exit__(*a)
        return _Scope()

    def sbuf(self, name, shape, dtype):
        name = "%s_s%d" % (name, getattr(self, "nscope", 0))
        t = self.stack.enter_context(self.nc.sbuf_tensor(name, list(shape), dtype))
        return Buf(t, name)

    def psum(self, name, shape, dtype=F32):
        name = "%s_s%d" % (name, getattr(self, "nscope", 0))
        t = self.stack.enter_context(self.nc.psum_tensor(name, list(shape), dtype))
        return Buf(t, name)

    def dram(self, name, shape, dtype, kind="Internal"):
        t = self.nc.dram_tensor(name, list(shape), dtype, kind=kind)
        return Buf(t, name)

    def _wait(self, e, deps):
        need = {}
        for (sid, val) in deps:
            if need.get(sid, 0) < val:
                need[sid] = val
        for sid, val in need.items():
            if e.seen.get(sid, 0) < val:
                e.h.wait_ge(self.sems[sid], val)
                e.seen[sid] = val
                self.ninstr += 1

    def _collect(self, reads, writes, acc_writes=()):
        deps = []
        for b in reads:
            deps += b.w
        for b in writes:
            deps += b.w
            deps += b.r
        for b in acc_writes:
            deps += b.r
        return deps

    @staticmethod
    def _compact(lst):
        mx = {}
        for (sid, v) in lst:
            if mx.get(sid, 0) < v:
                mx[sid] = v
        return list(mx.items())

    def _commit(self, token, reads, writes, acc_writes=()):
        for b in acc_writes:
            b.w.append(token)
            if len(b.w) > 64:
                b.w = self._compact(b.w)
        for b in reads:
            b.r.append(token)
            if len(b.r) > 64:
                mx = {}
                for (sid, v) in b.r:
                    if mx.get(sid, 0) < v:
                        mx[sid] = v
                b.r = list(mx.items())
        for b in writes:
            b.w = [token]
            b.r = []

    def _cutcheck(self):
        import os
        cut = os.environ.get("KB_CUT")
        self.nops = getattr(self, "nops", 0) + 1
        skip = os.environ.get("KB_SKIP")
        if skip and self.nops in [int(x) for x in skip.split(",")]:
            return True
        return cut is not None and self.nops > int(cut)

    def op(self, eng, fn, reads=(), writes=(), same_engine_sync=True, acc_writes=()):
        if self._cutcheck():
            return None
        e = self.engs[eng]
        deps = self._collect(reads, writes, acc_writes)
        if not same_engine_sync:
            deps = [d for d in deps if d[0] != id(e.sem)]
        self._wait(e, deps)
        ins = fn()
        e.count += 1
        ins.then_inc(e.sem, 1)
        self.ninstr += 1
        token = (id(e.sem), e.count)
        self._commit(token, reads, writes, acc_writes)
        return token

    def mm(self, fns, reads=(), writes=()):
        if self._cutcheck():
            return None
        e = self.engs["pe"]
        deps = self._collect(reads, writes)
        deps = [d for d in deps if d[0] != id(e.sem)]
        self._wait(e, deps)
        ins = None
        for fn in fns:
            ins = fn()
            self.ninstr += 1
        e.count += 1
        ins.then_inc(e.sem, 1)
        token = (id(e.sem), e.count)
        self._commit(token, reads, writes)
        return token

    def dma(self, q, out, in_, reads=(), writes=(), acc_writes=(), **kw):
        e = self.engs[q]
        pool = self.dma_pool[q]
        lst, idx = pool
        ent = lst[idx % len(lst)]
        pool[1] = idx + 1
        s, total = ent
        deps = self._collect(reads, writes, acc_writes)
        deps.append((id(s), total))
        import os
        if os.environ.get("DMA_SERIAL") and getattr(self, "_last_dma", {}).get(q):
            deps.append(self._last_dma[q])
        self._wait(e, deps)
        ins = e.h.dma_start(out=out, in_=in_, **kw)
        ins.then_inc(s, 16)
        ent[1] = total + 16
        self.ninstr += 1
        token = (id(s), total + 16)
        if not hasattr(self, "_last_dma"):
            self._last_dma = {}
        self._last_dma[q] = token
        self._commit(token, reads, writes, acc_writes)
        return token

    def init_memory(self, nbytes=196608):
        nc = self.nc
        with self.scope():
            n = nbytes // 4
            big = self.sbuf("initbig", [128, n], F32)
            q = n // 4
            self.op("dve", lambda: nc.vector.memset(big[:, 0:q], 0.0), writes=[])
            self.op("pool", lambda: nc.gpsimd.memset(big[:, q:2 * q], 0.0), writes=[])
            self.op("dve", lambda: nc.vector.memset(big[:, 2 * q:3 * q], 0.0), writes=[])
            self.op("pool", lambda: nc.gpsimd.memset(big[:, 3 * q:n], 0.0), writes=[])
            pss = [self.psum("initps%d" % i, [128, 512]) for i in range(8)]
            for p in pss:
                self.op("dve", lambda p=p: nc.vector.memset(p[:], 0.0), writes=[])

    def barrier(self):
        allt = []
        for e in self.engs.values():
            if e.count:
                allt.append((id(e.sem), e.count))
        for q, (lst, _) in self.dma_pool.items():
            for s, total in lst:
                if total:
                    allt.append((id(s), total))
        for e in self.engs.values():
            self._wait(e, allt)

    def finish(self, tokens_bufs):
        e = self.engs["sp"]
        deps = []
        for b in tokens_bufs:
            deps += b.w
        self._wait(e, deps)

T = 4352
TL = 4096

D = 1024
TL = 4096
TC = 256
T = TL + TC
NT = T // 128
BLKS = [(i * 512, 512) for i in range(8)] + [(4096, 256)]
DIN = 10528
ALPHA = (2.0 * 2) ** 0.25

COLS = dict(a_q=(0, 256), a_k=(256, 256), a_v=(512, 512), a_g=(1024, 512), a_lr=(1536, 32),
            b_q=(1568, 512), b_k=(2080, 512), b_v=(2592, 512),
            c_q=(3104, 512), c_k=(3616, 128), c_v=(3744, 128),
            d_q=(3872, 512), d_f=(4384, 1024), d_i=(5408, 512), d_g=(5920, 512), gate=(6432, 4096))


def blkcol(t0):
    return 1 if t0 >= TL else 0


class Ctx:
    pass


def declare_inputs(cx, nc):
    def din(name, shape, dt=F32):
        return Buf(nc.dram_tensor(name, list(shape), dt, kind="ExternalInput"), name)
    cx.xin = din("xin", [D, T])
    cx.cvec = din("cvec", [128, 8, 2])
    cx.w_mod = din("w_mod", [2, D, 6 * D])
    cx.b_modT = din("b_modT", [2, 128, 48])
    cx.w_in = din("w_in", [2, D, DIN])
    cx.rope_cos = din("rope_cos", [128, T])
    cx.rope_sin = din("rope_sin", [128, T])
    cx.rope_pt = din("rope_pt", [128, 128])
    cx.hg_gamma = din("hg_gamma", [2, 2, 512])
    cx.gla_lr_w = din("gla_lr_w", [2, 2, 16, 256])
    cx.gla_lr_b = din("gla_lr_b", [2, 2, 256])
    for nm in ("mg", "mh"):
        setattr(cx, nm + "_mq", din(nm + "_mq", [128, 2, 130]))
        setattr(cx, nm + "_mk", din(nm + "_mk", [128, 2, 128]))
    cx.m_ma = din("m_ma", [128, 2, 128])
    cx.m_ident = din("m_ident", [128, 128])
    cx.m_mau = din("m_mau", [128, 2, 128], U32)
    cx.m_wa = din("m_wa", [128, 2, 128])
    cx.wa_sink = din("wa_sink", [2, 8])
    cx.m_navalid = din("m_navalid", [128, 21, 128])
    cx.w_branch = din("w_branch", [2, 4, 512, 1024])
    cx.w_out = din("w_out", [2, 1024, 1024])
    cx.ln_gT = din("ln_gT", [2, 2, 128, 8])
    cx.ln_bT = din("ln_bT", [2, 2, 128, 8])
    cx.moe_w_group = din("moe_w_group", [2, 1024, 4])
    cx.moe_b_group = din("moe_b_group", [2, 4])
    cx.moe_w_expert = din("moe_w_expert", [2, 1024, 32])
    cx.moe_b_expert = din("moe_b_expert", [2, 32])
    cx.moe_w_gate = din("moe_w_gate", [2, 4, 8, 1024, 256])
    cx.moe_w_up = din("moe_w_up", [2, 4, 8, 1024, 256])
    cx.moe_w_down = din("moe_w_down", [2, 4, 8, 256, 1024])
    cx.m_sel = din("m_sel", [32, 32, 128])
    cx.m_ident32 = din("m_ident32", [128, 128])
    cx.na_bias = din("na_bias", [2, 8, 128, 21, 128])


def declare_scratch(cx, kb, debug, as_input=(), as_output=()):
    kind = "ExternalOutput" if debug else "Internal"

    def ds(name, shape, dt):
        b = kb.dram(name, shape, dt, kind=("ExternalInput" if name in as_input else ("ExternalOutput" if name in as_output else kind)))
        setattr(cx, name, b)
        return b
    ds("XT", [D, T], F32)
    ds("QA", [256, T], BF16)
    ds("GA", [512, T], BF16)
    ds("LR1", [33, T], F32)
    ds("QB", [512, T], BF16)
    ds("KB_", [512, T], BF16)
    ds("QC", [512, T], BF16)
    ds("KC", [128, T], BF16)
    ds("QD", [512, T], BF16)
    ds("GD", [512, T], BF16)
    ds("GATE", [4096, T], BF16)
    ds("KVA_t", [128, NT, 768], BF16)
    ds("VB_t", [128, NT, 512], BF16)
    ds("VC_t", [128, NT, 128], BF16)
    ds("VD_t", [128, NT, 512], BF16)
    ds("LGD_t", [128, NT, 1024], F32)
    ds("KD_t", [128, NT, 1024], BF16)
    ds("LGA_t", [128, NT, 512], F32)
    ds("MODT", [2, 128, 96], F32)
    ds("HX2", [D, T], BF16)
    ds("WG_t", [128, NT, 32], F32)
    ds("WGT", [32, T], F32)
    ds("BR_A", [512, T], BF16)
    ds("BR_B", [512, T], BF16)
    ds("BR_C", [512, T], BF16)
    ds("BR_D", [512, T], BF16)


def stage_mod(cx, kb, nc, modT):
    with kb.scope():
        cv = kb.sbuf("cv", [128, 8, 2], F32)
        sc = kb.sbuf("sc", [128, 8, 2], F32)
        kb.dma("sp", cv[:], cx.cvec[:], reads=[cx.cvec], writes=[cv])
        kb.op("act", lambda: nc.scalar.activation(out=sc[:], in_=cv[:], func=AF.Silu), reads=[cv], writes=[sc])
        wbufs = [kb.sbuf("wm%d" % i, [128, 8, 1024], F32) for i in range(2)]
        bm = kb.sbuf("bm", [128, 48], F32)
        ps = kb.psum("ps_mod", [128, 96])
        wi = 0
        for l in range(2):
            kb.dma("sp", bm[:], cx.b_modT[l], reads=[cx.b_modT], writes=[bm])
            for g in range(6):
                wb = wbufs[wi % 2]
                wi += 1
                src = cx.w_mod[l, :, g * 1024:(g + 1) * 1024].rearrange("(kc p) c -> p kc c", p=128)
                kb.dma("sp", wb[:], src, reads=[cx.w_mod], writes=[wb])
                for j in range(8):
                    jj = g * 8 + j
                    fns = []
                    for kc in range(8):
                        fns.append(lambda kc=kc, j=j, jj=jj, wb=wb: nc.tensor.matmul(
                            ps[:, 2 * jj:2 * jj + 2], lhsT=wb[:, kc, j * 128:(j + 1) * 128], rhs=sc[:, kc, :],
                            start=(kc == 0), stop=(kc == 7)))
                    kb.mm(fns, reads=[wb, sc], writes=[ps])
            m = modT[l]
            kb.op("dve", lambda m=m: nc.vector.tensor_tensor(
                out=m[:], in0=ps[:].rearrange("p (j c) -> p j c", c=2),
                in1=bm[:].unsqueeze(2).broadcast_to([128, 48, 2]), op=ALU.add),
                reads=[ps, bm], writes=[m])
            kb.dma("sp", cx.MODT[l], m[:].rearrange("p j c -> p (j c)"), reads=[m], writes=[cx.MODT])


def stage_modulate(cx, kb, nc, src_dram, modv, shift_idx, scale_idx, hxT, hx32=None):
    with kb.scope():
        onep = kb.sbuf("onep", [128, 8, 2], F32)
        kb.op("dve", lambda: nc.vector.tensor_scalar(
            out=onep[:], in0=modv[:, scale_idx * 8:(scale_idx + 1) * 8, :], scalar1=1.0, scalar2=None, op0=ALU.add),
            reads=[modv], writes=[onep])
        xb = [kb.sbuf("xb%d" % i, [128, 8, 512], F32) for i in range(2)]
        for bi, (t0, nb) in enumerate(BLKS):
            col = blkcol(t0)
            x = xb[bi % 2]
            kb.dma("sp", x[:, :, :nb], src_dram[:, t0:t0 + nb].rearrange("(kc p) t -> p kc t", p=128),
                   reads=[src_dram], writes=[x])
            for kc in range(8):
                eng = "dve" if kc % 2 == 0 else "pool"
                h = nc.vector if eng == "dve" else nc.gpsimd
                kb.op(eng, lambda h=h, kc=kc, x=x: h.tensor_scalar(
                    out=hxT[:, kc, t0:t0 + nb], in0=x[:, kc, :nb],
                    scalar1=onep[:, kc, col:col + 1], scalar2=modv[:, shift_idx * 8 + kc, col:col + 1],
                    op0=ALU.mult, op1=ALU.add), reads=[x, onep, modv], writes=[hxT])


FM_GROUPS = [
    ("a_q", "QA", "copy"), ("a_g", "GA", "silu"), ("a_lr", "LR1", "copy32"),
    ("b_q", "QB", "copy"), ("b_k", "KB_", "copy"),
    ("c_q", "QC", "rope"), ("c_k", "KC", "rope"),
    ("d_q", "QD", "silu"), ("d_g", "GD", "silu"), ("gate", "GATE", "sigmoid"),
]


def stage_inproj_fm(cx, kb, nc, l, hxT, only=None):
    with kb.scope():
        wsl = [kb.sbuf("wsl%d" % i, [128, 8, 512], BF16) for i in range(2)]
        stg = [kb.sbuf("stg%d" % i, [128, T], BF16) for i in range(2)]
        stg32 = kb.sbuf("stg32", [32, T], F32)
        pss = [kb.psum("psf%d" % i, [128, 512]) for i in range(4)]
        psr = [kb.psum("psr%d" % i, [128, 512]) for i in range(2)]
        cosT = kb.sbuf("cosT", [128, T], F32)
        sinT = kb.sbuf("sinT", [128, T], F32)
        ptm = kb.sbuf("ptm", [128, 128], BF16)
        tmpb = [kb.sbuf("tmpb%d" % i, [128, 512], BF16) for i in range(2)]
        t1 = [kb.sbuf("t1_%d" % i, [128, 512], F32) for i in range(2)]
        t2 = [kb.sbuf("t2_%d" % i, [128, 512], F32) for i in range(2)]
        ones = kb.sbuf("ones1", [1, T], F32)
        kb.dma("sp", cosT[:], cx.rope_cos[:], reads=[cx.rope_cos], writes=[cosT])
        kb.dma("sp", sinT[:], cx.rope_sin[:], reads=[cx.rope_sin], writes=[sinT])
        kb.dma("pool", ptm[:], cx.rope_pt[:], reads=[cx.rope_pt], writes=[ptm])
        kb.op("dve", lambda: nc.vector.memset(ones[:], 1.0), writes=[ones])
        kb.dma("sp", cx.LR1[32:33, :], ones[:], reads=[ones], writes=[], acc_writes=[cx.LR1])
        nsl = 0
        nps = 0
        nst = 0
        nrp = 0
        for (name, dest, kind) in FM_GROUPS:
            if only is not None and name not in only:
                continue
            c0, nc_ = COLS[name]
            dbuf = getattr(cx, dest)
            for s0 in range(0, nc_, 512):
                sw = min(512, nc_ - s0)
                w = wsl[nsl % 2]
                nsl += 1
                kb.dma("pool", w[:, :, :sw],
                       cx.w_in[l, :, c0 + s0:c0 + s0 + sw].rearrange("(kc p) c -> p kc c", p=128),
                       reads=[cx.w_in], writes=[w])
                for m0 in range(0, sw, 128):
                    mw = min(128, sw - m0)
                    if kind == "copy32":
                        st = stg32
                    else:
                        st = stg[nst % 2]
                        nst += 1
                    for bi, (t0, nb) in enumerate(BLKS):
                        ps = pss[nps % 4]
                        nps += 1
                        fns = [(lambda kc=kc, ps=ps, w=w: nc.tensor.matmul(
                            ps[:mw, :nb], lhsT=w[:, kc, m0:m0 + mw], rhs=hxT[:, kc, t0:t0 + nb],
                            start=(kc == 0), stop=(kc == 7))) for kc in range(8)]
                        kb.mm(fns, reads=[w, hxT], writes=[ps])
                        o = st[:mw, t0:t0 + nb]
                        if kind in ("copy", "copy32"):
                            if nps % 2 == 0:
                                kb.op("dve", lambda ps=ps, o=o: nc.vector.tensor_copy(out=o, in_=ps[:mw, :nb]),
                                      reads=[ps], writes=[st])
                            else:
                                kb.op("act", lambda ps=ps, o=o: nc.scalar.copy(out=o, in_=ps[:mw, :nb]),
                                      reads=[ps], writes=[st])
                        elif kind == "silu":
                            kb.op("act", lambda ps=ps, o=o: nc.scalar.activation(out=o, in_=ps[:mw, :nb], func=AF.Silu),
                                  reads=[ps], writes=[st])
                        elif kind == "sigmoid":
                            kb.op("act", lambda ps=ps, o=o: nc.scalar.activation(out=o, in_=ps[:mw, :nb], func=AF.Sigmoid),
                                  reads=[ps], writes=[st])
                        elif kind == "rope":
                            tb = tmpb[nrp % 2]
                            a1 = t1[nrp % 2]
                            a2 = t2[nrp % 2]
                            pr = psr[nrp % 2]
                            nrp += 1
                            kb.op("act", lambda ps=ps, tb=tb: nc.scalar.copy(out=tb[:mw, :nb], in_=ps[:mw, :nb]),
                                  reads=[ps], writes=[tb])
                            kb.mm([lambda pr=pr, tb=tb: nc.tensor.matmul(pr[:mw, :nb], lhsT=ptm[:mw, :mw], rhs=tb[:mw, :nb],
                                                                         start=True, stop=True)],
                                  reads=[ptm, tb], writes=[pr])
                            kb.op("dve", lambda tb=tb, a1=a1: nc.vector.tensor_tensor(
                                out=a1[:mw, :nb], in0=tb[:mw, :nb], in1=cosT[:mw, t0:t0 + nb], op=ALU.mult),
                                reads=[tb, cosT], writes=[a1])
                            kb.op("dve", lambda pr=pr, a2=a2: nc.vector.tensor_tensor(
                                out=a2[:mw, :nb], in0=pr[:mw, :nb], in1=sinT[:mw, t0:t0 + nb], op=ALU.mult),
                                reads=[pr, sinT], writes=[a2])
                            kb.op("pool", lambda a1=a1, a2=a2, o=o: nc.gpsimd.tensor_tensor(
                                out=o, in0=a1[:mw, :nb], in1=a2[:mw, :nb], op=ALU.add),
                                reads=[a1, a2], writes=[st])
                    kb.dma("sp", dbuf[s0 + m0:s0 + m0 + mw, :], st[:mw, :], reads=[st], writes=[], acc_writes=[dbuf])


TM_GROUPS = [
    (256, 512, "KVA_t", 0, "copy"), (768, 256, "KVA_t", 512, "copy"),
    (2592, 512, "VB_t", 0, "copy"), (3744, 128, "VC_t", 0, "copy"),
    (5408, 512, "VD_t", 0, "copy"),
    (4384, 512, None, 0, "hgf"), (4896, 512, None, 512, "hgf"),
]


def stage_inproj_tm(cx, kb, nc, l, hxT, lbt, omlt, only=None):
    with kb.scope():
        wsl = [kb.sbuf("wtl%d" % i, [128, 8, 512], BF16) for i in range(2)]
        pss = [kb.psum("pst%d" % i, [128, 512]) for i in range(4)]
        stb = [kb.sbuf("stb%d" % i, [128, 512], BF16) for i in range(4)]
        e1 = [kb.sbuf("e1_%d" % i, [128, 512], F32) for i in range(2)]
        r1 = [kb.sbuf("r1_%d" % i, [128, 512], F32) for i in range(2)]
        lg = [kb.sbuf("lg_%d" % i, [128, 512], F32) for i in range(2)]
        kk = [kb.sbuf("kk_%d" % i, [128, 512], BF16) for i in range(2)]
        nsl = nps = nst = nh = 0
        for (c0, ncol, dest, doff, kind) in TM_GROUPS:
            if only is not None and kind not in only:
                continue
            w = wsl[nsl % 2]
            nsl += 1
            kb.dma("pool", w[:, :, :ncol], cx.w_in[l, :, c0:c0 + ncol].rearrange("(kc p) c -> p kc c", p=128),
                   reads=[cx.w_in], writes=[w])
            for ti in range(NT):
                ts = slice(ti * 128, (ti + 1) * 128)
                ps = pss[nps % 4]
                nps += 1
                fns = [(lambda kc=kc, ps=ps, w=w: nc.tensor.matmul(
                    ps[:, :ncol], lhsT=hxT[:, kc, ts], rhs=w[:, kc, :ncol],
                    start=(kc == 0), stop=(kc == 7))) for kc in range(8)]
                kb.mm(fns, reads=[w, hxT], writes=[ps])
                if kind == "copy":
                    st = stb[nst % 4]
                    nst += 1
                    if nst % 2 == 0:
                        kb.op("dve", lambda ps=ps, st=st: nc.vector.tensor_copy(out=st[:, :ncol], in_=ps[:, :ncol]),
                              reads=[ps], writes=[st])
                    else:
                        kb.op("act", lambda ps=ps, st=st: nc.scalar.copy(out=st[:, :ncol], in_=ps[:, :ncol]),
                              reads=[ps], writes=[st])
                    db = getattr(cx, dest)
                    kb.dma("sp", db[:, ti, doff:doff + ncol], st[:, :ncol], reads=[st], writes=[], acc_writes=[db])
                else:
                    e = e1[nh % 2]
                    r = r1[nh % 2]
                    g = lg[nh % 2]
                    k = kk[nh % 2]
                    nh += 1
                    cs = slice(doff, doff + 512)
                    kb.op("act", lambda ps=ps, e=e: nc.scalar.activation(out=e[:], in_=ps[:], func=AF.Exp, scale=-1.0),
                          reads=[ps], writes=[e])
                    kb.op("pool", lambda e=e: nc.gpsimd.tensor_scalar(
                        out=e[:], in0=e[:], scalar1=1.0, scalar2=None, op0=ALU.add),
                        reads=[e], writes=[e])
                    kb.op("dve", lambda e=e, r=r: nc.vector.reciprocal(out=r[:], in_=e[:]),
                          reads=[e], writes=[r])
                    kb.op("dve", lambda r=r: nc.vector.tensor_tensor(out=r[:], in0=r[:], in1=omlt[:, cs], op=ALU.mult),
                          reads=[r, omlt], writes=[r])
                    kb.op("pool", lambda r=r: nc.gpsimd.tensor_tensor(out=r[:], in0=r[:], in1=lbt[:, cs], op=ALU.add),
                          reads=[r, lbt], writes=[r])
                    kb.op("act", lambda r=r, g=g: nc.scalar.activation(out=g[:], in_=r[:], func=AF.Ln),
                          reads=[r], writes=[g])
                    kb.op("pool", lambda r=r, k=k: nc.gpsimd.tensor_scalar(
                        out=k[:], in0=r[:], scalar1=-1.0, scalar2=1.0, op0=ALU.mult, op1=ALU.add),
                        reads=[r], writes=[k])
                    kb.dma("sp", cx.LGD_t[:, ti, cs], g[:], reads=[g], writes=[], acc_writes=[cx.LGD_t])
                    kb.dma("sp", cx.KD_t[:, ti, cs], k[:], reads=[k], writes=[], acc_writes=[cx.KD_t])


def stage_lb(cx, kb, nc, lbt, omlt):
    with kb.scope():
        gam = kb.sbuf("gam", [128, 2, 2, 512], F32)
        kb.dma("sp", gam[:].rearrange("p a b c -> p (a b c)"),
               cx.hg_gamma[:].rearrange("a b c -> (a b c)").partition_broadcast(128),
               reads=[cx.hg_gamma], writes=[gam])
        d = kb.sbuf("gamd", [128, 2, 512], F32)
        kb.op("dve", lambda: nc.vector.tensor_tensor(out=d[:], in0=gam[:, :, 0, :], in1=gam[:, :, 1, :], op=ALU.subtract),
              reads=[gam], writes=[d])
        kb.op("act", lambda: nc.scalar.activation(out=d[:], in_=d[:], func=AF.Exp), reads=[d], writes=[d])
        l1 = lbt[1]
        kb.op("dve", lambda: nc.vector.tensor_scalar(
            out=d[:], in0=d[:], scalar1=1.0, scalar2=None, op0=ALU.add), reads=[d], writes=[d])
        kb.op("dve", lambda: nc.vector.reciprocal(out=l1[:], in_=d[:].rearrange("p a c -> p (a c)")),
              reads=[d], writes=[l1])
        l0 = lbt[0]
        kb.op("dve", lambda: nc.vector.memset(l0[:], 0.0), writes=[l0])
        for l in range(2):
            o = omlt[l]
            lb = lbt[l]
            kb.op("dve", lambda o=o, lb=lb: nc.vector.tensor_scalar(
                out=o[:], in0=lb[:], scalar1=-1.0, scalar2=1.0, op0=ALU.mult, op1=ALU.add),
                reads=[lb], writes=[o])


def stage_gla_gate(cx, kb, nc, l):
    with kb.scope():
        wlr = kb.sbuf("wlr", [33, 512], F32)
        lr1 = kb.sbuf("lr1", [33, T], F32)
        kb.op("dve", lambda: nc.vector.memset(wlr[:], 0.0), writes=[wlr])
        kb.dma("sp", wlr[0:16, 0:256], cx.gla_lr_w[l, 0], reads=[cx.gla_lr_w], writes=[wlr])
        kb.dma("sp", wlr[16:32, 256:512], cx.gla_lr_w[l, 1], reads=[cx.gla_lr_w], writes=[wlr])
        kb.dma("sp", wlr[32:33, :], cx.gla_lr_b[l].rearrange("a c -> (a c)").unsqueeze(0),
               reads=[cx.gla_lr_b], writes=[wlr])
        kb.dma("sp", lr1[:], cx.LR1[:], reads=[cx.LR1], writes=[lr1])
        pss = [kb.psum("psg%d" % i, [128, 512]) for i in range(2)]
        ee = [kb.sbuf("ge%d" % i, [128, 512], F32) for i in range(2)]
        gg = [kb.sbuf("gg%d" % i, [128, 512], F32) for i in range(2)]
        for ti in range(NT):
            ts = slice(ti * 128, (ti + 1) * 128)
            ps = pss[ti % 2]
            e = ee[ti % 2]
            g = gg[ti % 2]
            kb.mm([lambda ps=ps: nc.tensor.matmul(ps[:], lhsT=lr1[:, ts], rhs=wlr[:], start=True, stop=True)],
                  reads=[lr1, wlr], writes=[ps])
            kb.op("act", lambda ps=ps, e=e: nc.scalar.activation(out=e[:], in_=ps[:], func=AF.Exp, scale=-1.0),
                  reads=[ps], writes=[e])
            kb.op("act", lambda e=e, g=g: nc.scalar.activation(out=g[:], in_=e[:], func=AF.Ln, bias=1.0),
                  reads=[e], writes=[g])
            kb.dma("sp", cx.LGA_t[:, ti, :], g[:], reads=[g], writes=[], acc_writes=[cx.LGA_t])

SCAN_CFG = {
    "gla": dict(H=4, dk=64, qsrc="QA", qscale=0.125, ksrc="KVA_t", kz=0, kcol0=0, vsrc="KVA_t", vcol0=256,
                gsrc="LGA_t", gz=256, gate="GA", dest="BR_A", mask="mg"),
    "hgrn": dict(H=4, dk=128, qsrc="QD", qscale=1.0, ksrc="KD_t", kz=512, kcol0=0, vsrc="VD_t", vcol0=0,
                 gsrc="LGD_t", gz=512, gate="GD", dest="BR_D", mask="mh"),
}


def view(b):
    return Buf(b.t, b.name + "_v")


def stage_scan(cx, kb, nc, which, heads=None):
    cfg = SCAN_CFG[which]
    H, dk = cfg["H"], cfg["dk"]
    dv = 128
    qsrc, ksrc, vsrc, gsrc = (getattr(cx, cfg[k]) for k in ("qsrc", "ksrc", "vsrc", "gsrc"))
    gate, dest = getattr(cx, cfg["gate"]), getattr(cx, cfg["dest"])
    mq_d, mk_d = getattr(cx, cfg["mask"] + "_mq"), getattr(cx, cfg["mask"] + "_mk")
    with kb.scope():
        mq = kb.sbuf("mq", [128, 2, 130], F32)
        mk = kb.sbuf("mk", [128, 2, 128], F32)
        ma = kb.sbuf("ma", [128, 2, 128], F32)
        ident = kb.sbuf("ident", [128, 128], BF16)
        onesdv = kb.sbuf("onesdv", [128, 128], F32)
        kb.dma("sp", mq[:], mq_d[:], reads=[mq_d], writes=[mq])
        kb.dma("sp", mk[:], mk_d[:], reads=[mk_d], writes=[mk])
        kb.dma("sp", ma[:], cx.m_ma[:], reads=[cx.m_ma], writes=[ma])
        mau = kb.sbuf("mau", [128, 2, 128], U32)
        kb.dma("sp", mau[:], cx.m_mau[:], reads=[cx.m_mau], writes=[mau])
        kb.dma("pool", ident[:], cx.m_ident[:], reads=[cx.m_ident], writes=[ident])
        kb.op("dve", lambda: nc.vector.memset(onesdv[:], 1.0 / dv), writes=[onesdv])
        vt = kb.sbuf("vt", [128, NT, dv], BF16)
        qT = kb.sbuf("qT", [128, T], BF16)
        gsil = kb.sbuf("gsil", [128, T], BF16)
        obr = kb.sbuf("obr", [128, T], BF16)
        gts = [kb.sbuf("gt%d" % i, [128, NT, dk], F32) for i in range(2)]
        kts = [kb.sbuf("kt%d" % i, [128, NT, dk], BF16) for i in range(2)]
        qt = [kb.sbuf("qt%d" % z, [128, T], BF16) for z in range(2)]
        atm = [kb.sbuf("atm%d" % z, [128, NT, 128], BF16) for z in range(2)]
        Sp = [kb.sbuf("Sp%d" % z, [128, 2 * NT, dv], BF16) for z in range(2)]
        for z in range(2):
            kb.op("pool", lambda z=z: nc.gpsimd.memset(atm[z][:], 0.0), writes=[atm[z]])
        Sst = [kb.sbuf("Sst%d" % i, [128, dv], F32) for i in range(2)]
        R = 3
        eq = [kb.sbuf("eq%d" % i, [128, 128], F32) for i in range(R)]
        ek = [kb.sbuf("ek%d" % i, [128, 128], F32) for i in range(R)]
        ekh = [kb.sbuf("ekh%d" % i, [128, 128], F32) for i in range(R)]
        ktl = [kb.sbuf("ktl%d" % i, [128, 128], BF16) for i in range(R)]
        khat = [kb.sbuf("khat%d" % i, [128, 256], BF16) for i in range(R)]
        ktr = [kb.sbuf("ktr%d" % i, [128, 128], F32) for i in range(R)]
        atr = [kb.sbuf("atr%d" % i, [128, 128], F32) for i in range(R)]
        usb = [kb.sbuf("usb%d" % i, [128, 256], F32) for i in range(R)]
        osb = [kb.sbuf("osb%d" % i, [128, 512], F32) for i in range(2)]
        cm = kb.sbuf("cm", [128, 2], F32)
        kb.op("dve", lambda: nc.vector.memset(cm[:], 0.0), writes=[cm])
        kb.op("dve", lambda: nc.vector.memset(cm[0:64, 0:1], 1.0), writes=[cm])
        kb.op("dve", lambda: nc.vector.memset(cm[64:128, 1:2], 1.0), writes=[cm])
        er2 = [kb.sbuf("er%d" % i, [128, 2], F32) for i in range(R)]
        ed2 = [kb.sbuf("ed%d" % i, [128, 2], F32) for i in range(R)]
        psA = [kb.psum("psA%d" % i, [128, 512]) for i in range(2)]
        psB = [kb.psum("psB%d" % i, [128, 512]) for i in range(2)]
        psA_ec = [view(p) for p in psA]
        import os
        if True:
            psK = [kb.psum("psK%d" % i, [128, 512]) for i in range(2)]
        else:
            psK = psA
        psA_kt = [view(p) for p in psK]
        psA_e2 = [view(p) for p in psK]
        psB_u = [view(p) for p in psB]
        psB_at = [view(p) for p in psB]
        psO = [kb.psum("psO%d" % i, [128, 512]) for i in range(1)] * 2
        psM = [kb.psum("psM%d" % i, [128, 512]) for i in range(1)] * 2
        sq = [kb.sbuf("sq%d" % i, [128, 512], F32) for i in range(2)]
        sd = [kb.sbuf("sd%d" % i, [128, 512], F32) for i in range(2)]
        on = [kb.sbuf("on%d" % i, [128, 512], F32) for i in range(2)]
        eps_t = kb.sbuf("eps_t", [128, 1], F32)
        kb.op("dve", lambda: nc.vector.memset(eps_t[:], 1e-6), writes=[eps_t])

        it = 0
        ghz = 0
        import os
        for h in (range(H) if heads is None else heads):
            vc0 = cfg["vcol0"] + h * dv
            kb.dma("sp", vt[:], vsrc[:, :, vc0:vc0 + dv], reads=[vsrc], writes=[vt])
            kb.dma("sp", qT[:dk, :], qsrc[h * dk:(h + 1) * dk, :], reads=[qsrc], writes=[qT])
            kb.dma("sp", gsil[:], gate[h * dv:(h + 1) * dv, :], reads=[gate], writes=[gsil])
            for z in range(2):
                gt = gts[ghz % 2]
                kt = kts[ghz % 2]
                ghz += 1
                gc0 = z * cfg["gz"] + h * dk
                kc0 = cfg["kcol0"] + z * cfg["kz"] + h * dk
                kb.dma("sp", gt[:], gsrc[:, :, gc0:gc0 + dk], reads=[gsrc], writes=[gt])
                if os.environ.get("KTMEMSET"):
                    kb.op("dve", lambda kt=kt: nc.vector.memset(kt[:], 0.0), writes=[kt])
                kb.dma("sp", kt[:], ksrc[:, :, kc0:kc0 + dk], reads=[ksrc], writes=[kt])
                order = [32, 33] + list(range(32)) if z == 0 else [33, 32] + list(range(31, -1, -1))
                import os
                DBG = os.environ.get("SCAN_DBG", "")
                if DBG == "loads":
                    continue
                if DBG.startswith("nt"):
                    order = order[:int(DBG[2:])]
                S = Sst[0]
                kb.op("dve", lambda S=S: nc.vector.memset(S[:], 0.0), writes=[S])
                si = 0
                for ti in order:
                    a = it % 2
                    r = it % R
                    it += 1
                    ts = slice(ti * 128, (ti + 1) * 128)
                    pa, pb = psA[a], psB[a]
                    pk = psK[a]
                    kb.mm([lambda pa=pa, gt=gt: nc.tensor.matmul(pa[:dk, 0:130], lhsT=gt[:, ti, :], rhs=mq[:, z, :],
                                                                 start=True, stop=True)],
                          reads=[gt, mq], writes=[psA_ec[a]])
                    kb.mm([lambda pk=pk, kt=kt: nc.tensor.matmul(pk[:dk, 256:384], lhsT=(ident[:, :dk] if os.environ.get("A2ID") else kt[:, ti, :]), rhs=ident[:],
                                                                 start=True, stop=True)],
                          reads=[kt, ident], writes=[psA_kt[a]])
                    kb.mm([lambda pk=pk, gt=gt: nc.tensor.matmul(pk[:, 384:384 + dk], lhsT=mk[:, z, :], rhs=gt[:, ti, :],
                                                                 start=True, stop=True)],
                          reads=[gt, mk], writes=[psA_e2[a]])
                    kb.op("act", lambda pa=pa, o=eq[r]: nc.scalar.activation(out=o[:dk, :], in_=pa[:dk, 0:128], func=AF.Exp),
                          reads=[psA_ec[a]], writes=[eq[r]])
                    kb.op("act", lambda pa=pa, o=ek[r]: nc.scalar.activation(out=o[:dk, :], in_=pa[:dk, 0:128], func=AF.Exp, scale=-1.0),
                          reads=[psA_ec[a]], writes=[ek[r]])
                    kb.op("act", lambda pa=pa, o=er2[r]: nc.scalar.activation(out=o[:dk, :], in_=pa[:dk, 128:130], func=AF.Exp, scale=0.5),
                          reads=[psA_ec[a]], writes=[er2[r]])
                    kb.op("act", lambda pa=pa, o=ed2[r]: nc.scalar.activation(out=o[:dk, :], in_=pa[:dk, 128:130], func=AF.Exp),
                          reads=[psA_ec[a]], writes=[ed2[r]])
                    kb.op("act", lambda pk=pk, o=ekh[r]: nc.scalar.activation(out=o[:, :dk], in_=pk[:, 384:384 + dk], func=AF.Exp),
                          reads=[psA_e2[a]], writes=[ekh[r]])
                    qz = qt[z]
                    kb.op("dve", lambda qz=qz, e=eq[r]: nc.vector.scalar_tensor_tensor(
                        out=qz[:dk, ts], in0=qT[:dk, ts], scalar=cfg["qscale"], in1=e[:dk, :], op0=ALU.mult, op1=ALU.mult),
                        reads=[qT, eq[r]], writes=[], acc_writes=[qz])
                    kb.op("act", lambda pk=pk, o=ktr[r]: nc.scalar.copy(out=o[:dk, :], in_=pk[:dk, 256:384]),
                          reads=[psA_kt[a]], writes=[ktr[r]])
                    kb.op("dve", lambda e=ek[r], o=ktl[r], kr=ktr[r]: nc.vector.tensor_tensor(
                        out=o[:dk, :], in0=kr[:dk, :], in1=e[:dk, :], op=ALU.mult),
                        reads=[ktr[r], ek[r]], writes=[ktl[r]])
                    for c in range(2):
                        kb.op("dve", lambda kt=kt, e=ekh[r], o=khat[r], c=c: nc.vector.scalar_tensor_tensor(
                            out=o[:, c * 128:c * 128 + dk], in0=kt[:, ti, :], scalar=cm[:, c:c + 1], in1=e[:, :dk],
                            op0=ALU.mult, op1=ALU.mult),
                            reads=[kt, ekh[r], cm], writes=[], acc_writes=[khat[r]])
                    kb.mm([(lambda c=c, pb=pb, kh=khat[r]: nc.tensor.matmul(
                        pb[:dk, c * 128:(c + 1) * 128], lhsT=kh[:, c * 128:c * 128 + dk],
                        rhs=vt[:, ti, :], start=True, stop=True)) for c in range(2)],
                        reads=[khat[r], vt], writes=[psB_u[a]])
                    kb.mm([lambda pb=pb, k_=ktl[r], qz=qz: nc.tensor.matmul(
                        pb[:, 256:384], lhsT=k_[:dk, :], rhs=qz[:dk, ts], start=True, stop=True)],
                        reads=[ktl[r], qz], writes=[psB_at[a]])
                    az = atm[z]
                    kb.op("act", lambda pb=pb, o=atr[r]: nc.scalar.copy(out=o[:], in_=pb[:, 256:384]),
                          reads=[psB_at[a]], writes=[atr[r]])
                    kb.op("dve", lambda az=az, o=atr[r]: nc.vector.copy_predicated(
                        out=az[:, ti, :], mask=mau[:, z, :], data=o[:]),
                        reads=[atr[r], mau], writes=[], acc_writes=[az])
                    kb.op("act", lambda pb=pb, o=usb[r]: nc.scalar.copy(out=o[:dk, :], in_=pb[:dk, 0:256]),
                          reads=[psB_u[a]], writes=[usb[r]])
                    spz = Sp[z]
                    for c in ((0, 1) if z == 0 else (1, 0)):
                        ci = 2 * ti + c
                        kb.op("act", lambda S=S, spz=spz, ci=ci, e=er2[r], c=c: nc.scalar.activation(
                            out=spz[:dk, ci, :], in_=S[:dk, :], func=AF.Copy, scale=e[:dk, c:c + 1]),
                            reads=[S, er2[r]], writes=[], acc_writes=[spz])
                        S2 = Sst[(si + 1) % 2]
                        si += 1
                        kb.op("dve", lambda S=S, S2=S2, e=ed2[r], u=usb[r], c=c: nc.vector.scalar_tensor_tensor(
                            out=S2[:dk, :], in0=S[:dk, :], scalar=e[:dk, c:c + 1], in1=u[:dk, c * 128:(c + 1) * 128],
                            op0=ALU.mult, op1=ALU.add),
                            reads=[S, ed2[r], usb[r]], writes=[S2])
                        S = S2
            groups = [list(range(g * 4, g * 4 + 4)) for g in range(8)] + [[32, 33]]
            if DBG:
                groups = []
            for gi, tiles in enumerate(groups):
                po = psO[gi % 2]
                pm = psM[gi % 2]
                nb = len(tiles) * 128
                t0 = tiles[0] * 128
                fns = []
                for k_, ti in enumerate(tiles):
                    cs = k_ * 128
                    for z in range(2):
                        fns.append(lambda po=po, ti=ti, cs=cs, z=z: nc.tensor.matmul(
                            po[:, cs:cs + 128], lhsT=vt[:, ti, :], rhs=atm[z][:, ti, :], start=(z == 0), stop=False))
                        for c in range(2):
                            fns.append(lambda po=po, ti=ti, cs=cs, z=z, c=c: nc.tensor.matmul(
                                po[:, cs + c * 64:cs + (c + 1) * 64], lhsT=Sp[z][:dk, 2 * ti + c, :],
                                rhs=qt[z][:dk, ti * 128 + c * 64:ti * 128 + (c + 1) * 64],
                                start=False, stop=(z == 1)))
                kb.mm(fns, reads=[vt, atm[0], atm[1], Sp[0], Sp[1], qt[0], qt[1]], writes=[po])
                s_, d_, o_ = sq[gi % 2], sd[gi % 2], on[gi % 2]
                kb.op("act", lambda po=po, s_=s_: nc.scalar.activation(out=s_[:, :nb], in_=po[:, :nb], func=AF.Square),
                      reads=[po], writes=[s_])
                kb.mm([lambda pm=pm, s_=s_: nc.tensor.matmul(pm[:, :nb], lhsT=onesdv[:], rhs=s_[:, :nb], start=True, stop=True)],
                      reads=[onesdv, s_], writes=[pm])
                kb.op("act", lambda pm=pm, d_=d_: nc.scalar.activation(out=d_[:, :nb], in_=pm[:, :nb], func=AF.Sqrt, bias=eps_t[:]),
                      reads=[pm, eps_t], writes=[d_])
                kb.op("dve", lambda d_=d_: nc.vector.reciprocal(out=d_[:, :nb], in_=d_[:, :nb]), reads=[d_], writes=[d_])
                ob_ = osb[gi % 2]
                kb.op("act", lambda po=po, ob_=ob_: nc.scalar.copy(out=ob_[:, :nb], in_=po[:, :nb]), reads=[po], writes=[ob_])
                kb.op("dve", lambda ob_=ob_, d_=d_, o_=o_: nc.vector.tensor_tensor(
                    out=o_[:, :nb], in0=ob_[:, :nb], in1=d_[:, :nb], op=ALU.mult), reads=[ob_, d_], writes=[o_])
                kb.op("pool", lambda o_=o_: nc.gpsimd.tensor_tensor(
                    out=obr[:, t0:t0 + nb], in0=o_[:, :nb], in1=gsil[:, t0:t0 + nb], op=ALU.mult),
                    reads=[o_, gsil], writes=[], acc_writes=[obr])
            kb.dma("sp", dest[h * dv:(h + 1) * dv, :], obr[:], reads=[obr], writes=[], acc_writes=[dest])


def scan_masks():
    j = np.arange(128)[:, None]
    i = np.arange(128)[None, :]
    same = (j // 64 == i // 64).astype(np.float32)
    out = {}
    for name, gs in (("mg", -1.0 / 16.0), ("mh", 1.0)):
        mq = np.zeros((128, 2, 130), np.float32)
        mk = np.zeros((128, 2, 128), np.float32)
        mq[:, 0, :128] = gs * same * ((j <= i).astype(np.float32) - 0.5)
        mq[:, 1, :128] = gs * same * ((j >= i).astype(np.float32) - 0.5)
        for z in range(2):
            for c in range(2):
                mq[:, z, 128 + c] = gs * (np.arange(128) // 64 == c)
        mk[:, 0, :] = gs * same * (j > i)
        mk[:, 1, :] = gs * same * (j < i)
        out[name + "_mq"] = mq
        out[name + "_mk"] = mk
    ma = np.zeros((128, 2, 128), np.float32)
    ma[:, 0, :] = same * (j <= i)
    ma[:, 1, :] = same * (j >= i)
    out["m_ma"] = ma
    out["m_mau"] = ma.astype(np.uint32)
    out["m_ident"] = np.eye(128, dtype=np.float32)
    return out


def stage_wa(cx, kb, nc, l, groups=None):
    with kb.scope():
        wm = kb.sbuf("wm", [128, 2, 128], BF16)
        kb.dma("pool", wm[:], cx.m_wa[:], reads=[cx.m_wa], writes=[wm])
        ones64 = kb.sbuf("ones64", [128, 64], BF16)
        kb.op("dve", lambda: nc.vector.memset(ones64[:], 1.0), writes=[ones64])
        sk = kb.sbuf("sk", [64, 8], F32)
        kb.dma("sp", sk[:], cx.wa_sink[l].partition_broadcast(64), reads=[cx.wa_sink], writes=[sk])
        kb.op("act", lambda: nc.scalar.activation(out=sk[:], in_=sk[:], func=AF.Exp), reads=[sk], writes=[sk])
        q4 = kb.sbuf("q4", [64, 4, T], BF16)
        kT = kb.sbuf("kTc", [64, T], BF16)
        vt = kb.sbuf("vtc", [128, NT, 64], BF16)
        ost = kb.sbuf("ost", [64, 4, T], BF16)
        pS = [kb.psum("pS%d" % i, [128, 512]) for i in range(2)]
        pO = [kb.psum("pO%d" % i, [128, 512]) for i in range(2)]
        pD = [kb.psum("pD%d" % i, [128, 512]) for i in range(2)]
        R = 3
        pt = [kb.sbuf("pt%d" % i, [128, 4, 128], BF16) for i in range(R)]
        ptm = [kb.sbuf("ptm%d" % i, [128, 4, 128], BF16) for i in range(R)]
        osb = [kb.sbuf("osbw%d" % i, [64, 4, 128], F32) for i in range(2)]
        dsb = [kb.sbuf("dsbw%d" % i, [64, 4, 128], F32) for i in range(2)]
        it = 0
        for g in (range(2) if groups is None else groups):
            kb.dma("sp", q4[:], cx.QC[g * 256:(g + 1) * 256, :].rearrange("(hh d) t -> d hh t", d=64),
                   reads=[cx.QC], writes=[q4])
            kb.dma("sp", kT[:], cx.KC[g * 64:(g + 1) * 64, :], reads=[cx.KC], writes=[kT])
            kb.dma("sp", vt[:], cx.VC_t[:, :, g * 64:(g + 1) * 64], reads=[cx.VC_t], writes=[vt])
            for qb in range(NT):
                if qb < 32:
                    keys = []
                    if qb > 0:
                        keys.append((qb - 1, 0))
                    keys.append((qb, None))
                    if qb < 31:
                        keys.append((qb + 1, 1))
                    keys += [(32, None), (33, None)]
                else:
                    keys = [(32, None), (33, None)]
                qs = slice(qb * 128, (qb + 1) * 128)
                a = qb % 2
                po, pd = pO[a], pD[a]
                nk = len(keys)
                for ki, (kt_i, mi) in enumerate(keys):
                    ps = pS[it % 2]
                    p_ = pt[it % R]
                    pm_ = ptm[it % R]
                    it += 1
                    kb.mm([lambda ps=ps, kt_i=kt_i: nc.tensor.matmul(
                        ps[:, :], lhsT=kT[:, kt_i * 128:(kt_i + 1) * 128], rhs=q4[:, :, qs], start=True, stop=True)],
                        reads=[kT, q4], writes=[ps])
                    kb.op("act", lambda ps=ps, p_=p_: nc.scalar.activation(
                        out=p_[:].rearrange("p h q -> p (h q)"), in_=ps[:, :], func=AF.Exp, scale=0.125),
                        reads=[ps], writes=[p_])
                    src = p_
                    if mi is not None:
                        eng = "dve" if it % 2 == 0 else "pool"
                        h_ = nc.vector if eng == "dve" else nc.gpsimd
                        kb.op(eng, lambda h_=h_, p_=p_, pm_=pm_, mi=mi: h_.tensor_tensor(
                            out=pm_[:], in0=p_[:], in1=wm[:, mi, :].unsqueeze(1).broadcast_to([128, 4, 128]), op=ALU.mult),
                            reads=[p_, wm], writes=[pm_])
                        src = pm_
                    kb.mm([lambda po=po, src=src, kt_i=kt_i, ki=ki: nc.tensor.matmul(
                        po[:64, :], lhsT=vt[:, kt_i, :], rhs=src[:].rearrange("p h q -> p (h q)"),
                        start=(ki == 0), stop=(ki == nk - 1)),
                        lambda pd=pd, src=src, ki=ki: nc.tensor.matmul(
                        pd[:64, :], lhsT=ones64[:], rhs=src[:].rearrange("p h q -> p (h q)"),
                        start=(ki == 0), stop=(ki == nk - 1))],
                        reads=[vt, src, ones64], writes=[po, pd])
                o_, d_ = osb[a], dsb[a]
                kb.op("act", lambda po=po, o_=o_: nc.scalar.copy(out=o_[:].rearrange("p h q -> p (h q)"), in_=po[:64, :]),
                      reads=[po], writes=[o_])
                kb.op("act", lambda pd=pd, d_=d_: nc.scalar.copy(out=d_[:].rearrange("p h q -> p (h q)"), in_=pd[:64, :]),
                      reads=[pd], writes=[d_])
                kb.op("dve", lambda d_=d_: nc.vector.tensor_tensor(
                    out=d_[:], in0=d_[:], in1=sk[:, g * 4:(g + 1) * 4].unsqueeze(2).broadcast_to([64, 4, 128]), op=ALU.add),
                    reads=[d_, sk], writes=[d_])
                kb.op("dve", lambda d_=d_: nc.vector.reciprocal(out=d_[:], in_=d_[:]), reads=[d_], writes=[d_])
                kb.op("pool", lambda o_=o_, d_=d_: nc.gpsimd.tensor_tensor(
                    out=ost[:, :, qs], in0=o_[:], in1=d_[:], op=ALU.mult),
                    reads=[o_, d_], writes=[], acc_writes=[ost])
            kb.dma("sp", cx.BR_C[g * 256:(g + 1) * 256, :].rearrange("(hh d) t -> d hh t", d=64), ost[:],
                   reads=[ost], writes=[], acc_writes=[cx.BR_C])


def wa_masks():
    kk = np.arange(128)[:, None]
    qq = np.arange(128)[None, :]
    m = np.zeros((128, 2, 128), np.float32)
    m[:, 0, :] = (kk >= qq)
    m[:, 1, :] = (kk <= qq)
    return {"m_wa": m}


def na_patterns():
    W = 64
    tok = np.arange(128)
    lr, lc = tok // W, tok % W
    pats = []
    keys = []
    for m in range(32):
        qr = 2 * m + lr[None, :]
        qc = lc[None, :]
        rs = np.clip(qr - 4, 0, 56)
        cs = np.clip(qc - 8, 0, 48)
        lst = []
        for kt_i in range(32):
            kr = 2 * kt_i + lr[:, None]
            kc = lc[:, None]
            valid = (kr >= rs) & (kr < rs + 8) & (kc >= cs) & (kc < cs + 16)
            if not valid.any():
                continue
            ri = np.clip(kr - qr + 7, 0, 14)
            ci = np.clip(kc - qc + 15, 0, 30)
            if 2 <= m <= 29:
                pid = kt_i - m + 2
                if m == 2:
                    pats.append((valid, ri, ci))
            else:
                pid = len(pats)
                pats.append((valid, ri, ci))
            lst.append((kt_i, pid))
        keys.append(lst)
    return keys, pats


def na_consts():
    keys, pats = na_patterns()
    n0 = len(keys[0]) + len(keys[1])
    order = list(range(n0, n0 + 5)) + list(range(0, n0)) + list(range(n0 + 5, len(pats)))
    remap = {old: new for new, old in enumerate(order)}
    pats2 = [pats[o] for o in order]
    keys2 = []
    for m, lst in enumerate(keys):
        if 2 <= m <= 29:
            keys2.append(lst)
        else:
            keys2.append([(k, remap[p]) for (k, p) in lst])
    valid = np.stack([p[0] for p in pats2], 1).astype(np.float32)
    ri = np.stack([p[1] for p in pats2], 1)
    ci = np.stack([p[2] for p in pats2], 1)
    return keys2, valid, ri, ci


def stage_na(cx, kb, nc, l, heads=None):
    keys_tab, valid_np, _, _ = na_consts()
    npat = valid_np.shape[1]
    with kb.scope():
        vmask = kb.sbuf("vmask", [128, npat, 128], F32)
        kb.dma("sp", vmask[:], cx.m_navalid[:], reads=[cx.m_navalid], writes=[vmask])
        ones64 = kb.sbuf("ones64n", [128, 64], BF16)
        kb.op("dve", lambda: nc.vector.memset(ones64[:], 1.0), writes=[ones64])
        qT = kb.sbuf("qTn", [64, T], BF16)
        kT = kb.sbuf("kTn", [64, T], BF16)
        vt = kb.sbuf("vtn", [128, NT, 64], BF16)
        ost = kb.sbuf("ostn", [64, T], BF16)
        bias = kb.sbuf("biasn", [128, npat, 128], F32)
        EB = kb.sbuf("EBn", [128, npat, 128], BF16)
        pS = [kb.psum("pSn%d" % i, [128, 512]) for i in range(4)]
        pO = [kb.psum("pOn%d" % i, [128, 512]) for i in range(2)]
        pD = [kb.psum("pDn%d" % i, [128, 512]) for i in range(2)]
        R = 4
        pt = [kb.sbuf("ptn%d" % i, [128, 4, 128], BF16) for i in range(R)]
        osb = [kb.sbuf("osbn%d" % i, [64, 256], F32) for i in range(2)]
        it = 0
        for h in (range(8) if heads is None else heads):
            kb.dma("sp", bias[:], cx.na_bias[l, h], reads=[cx.na_bias], writes=[bias])
            kb.op("act", lambda: nc.scalar.activation(out=bias[:], in_=bias[:], func=AF.Exp), reads=[bias], writes=[bias])
            kb.op("pool", lambda: nc.gpsimd.tensor_tensor(out=EB[:], in0=bias[:], in1=vmask[:], op=ALU.mult),
                  reads=[bias, vmask], writes=[EB])
            kb.dma("sp", qT[:], cx.QB[h * 64:(h + 1) * 64, :], reads=[cx.QB], writes=[qT])
            kb.dma("sp", kT[:], cx.KB_[h * 64:(h + 1) * 64, :], reads=[cx.KB_], writes=[kT])
            kb.dma("sp", vt[:], cx.VB_t[:, :, h * 64:(h + 1) * 64], reads=[cx.VB_t], writes=[vt])
            for qb in range(NT):
                if qb < 32:
                    keys = list(keys_tab[qb]) + [(32, None), (33, None)]
                else:
                    keys = [(32, None), (33, None)]
                qs = slice(qb * 128, (qb + 1) * 128)
                a = qb % 2
                po = pO[a]
                pd = pD[a]
                nk = len(keys)
                grp = [keys[i:i + 4] for i in range(0, nk, 4)]
                done = 0
                for gk in grp:
                    ps = pS[it % 4]
                    p_ = pt[it % R]
                    it += 1
                    ng = len(gk)
                    kb.mm([(lambda ps=ps, j=j, kt_i=kt_i: nc.tensor.matmul(
                        ps[:, j * 128:(j + 1) * 128], lhsT=kT[:, kt_i * 128:(kt_i + 1) * 128], rhs=qT[:, qs],
                        start=True, stop=True)) for j, (kt_i, _) in enumerate(gk)],
                        reads=[kT, qT], writes=[ps])
                    kb.op("act", lambda ps=ps, p_=p_, ng=ng: nc.scalar.activation(
                        out=p_[:, :ng, :].rearrange("p h q -> p (h q)"), in_=ps[:, :ng * 128], func=AF.Exp, scale=0.125),
                        reads=[ps], writes=[p_])
                    nloc = sum(1 for (_, pid) in gk if pid is not None)
                    if nloc:
                        pid0 = gk[0][1]
                        eng = "dve" if it % 2 == 0 else "pool"
                        h_ = nc.vector if eng == "dve" else nc.gpsimd
                        kb.op(eng, lambda h_=h_, p_=p_, pid0=pid0, nloc=nloc: h_.tensor_tensor(
                            out=p_[:, :nloc, :], in0=p_[:, :nloc, :], in1=EB[:, pid0:pid0 + nloc, :], op=ALU.mult),
                            reads=[p_, EB], writes=[p_])
                    fns = []
                    for j, (kt_i, _) in enumerate(gk):
                        first = (done == 0)
                        last = (done == nk - 1)
                        fns.append(lambda po=po, p_=p_, j=j, kt_i=kt_i, first=first, last=last: nc.tensor.matmul(
                            po[:64, 0:128], lhsT=vt[:, kt_i, :], rhs=p_[:, j, :], start=first, stop=last))
                        fns.append(lambda pd=pd, p_=p_, j=j, first=first, last=last: nc.tensor.matmul(
                            pd[:64, 0:128], lhsT=ones64[:], rhs=p_[:, j, :], start=first, stop=last))
                        done += 1
                    kb.mm(fns, reads=[vt, p_, ones64], writes=[po, pd])
                o_ = osb[a]
                kb.op("act", lambda po=po, o_=o_: nc.scalar.copy(out=o_[:, 0:128], in_=po[:64, 0:128]), reads=[po], writes=[o_])
                kb.op("act", lambda pd=pd, o_=o_: nc.scalar.copy(out=o_[:, 128:256], in_=pd[:64, 0:128]), reads=[pd, o_], writes=[o_])
                kb.op("dve", lambda o_=o_: nc.vector.reciprocal(out=o_[:, 128:256], in_=o_[:, 128:256]), reads=[o_], writes=[o_])
                kb.op("pool", lambda o_=o_: nc.gpsimd.tensor_tensor(
                    out=ost[:, qs], in0=o_[:, 0:128], in1=o_[:, 128:256], op=ALU.mult),
                    reads=[o_], writes=[], acc_writes=[ost])
            kb.dma("sp", cx.BR_B[h * 64:(h + 1) * 64, :], ost[:], reads=[ost], writes=[], acc_writes=[cx.BR_B])

LN_EPS = 1e-5
MB = 256
MBLKS = [(i * MB, MB) for i in range(T // MB)]


def ln_setup(cx, kb, nc):
    ones = kb.sbuf("ln_ones", [128, 128], F32)
    kb.op("dve", lambda: nc.vector.memset(ones[:], 1.0 / D), writes=[ones])
    eps = kb.sbuf("ln_eps", [128, 1], F32)
    kb.op("dve", lambda: nc.vector.memset(eps[:], LN_EPS), writes=[eps])
    return ones, eps


def ln_block(kb, nc, r, nb, gam, bet, out, ones, eps, scr):
    rsq, ps1, ps2, mean, var = scr
    kb.op("act", lambda: nc.scalar.activation(out=rsq[:, :, :nb], in_=r[:, :, :nb], func=AF.Square), reads=[r], writes=[rsq])
    kb.mm([(lambda oc=oc: nc.tensor.matmul(ps1[:, :nb], lhsT=ones[:], rhs=r[:, oc, :nb], start=(oc == 0), stop=(oc == 7)))
           for oc in range(8)], reads=[ones, r], writes=[ps1])
    kb.mm([(lambda oc=oc: nc.tensor.matmul(ps2[:, :nb], lhsT=ones[:], rhs=rsq[:, oc, :nb], start=(oc == 0), stop=(oc == 7)))
           for oc in range(8)], reads=[ones, rsq], writes=[ps2])
    kb.op("act", lambda: nc.scalar.copy(out=mean[:, :nb], in_=ps1[:, :nb]), reads=[ps1], writes=[mean])
    kb.op("act", lambda: nc.scalar.copy(out=var[:, :nb], in_=ps2[:, :nb]), reads=[ps2], writes=[var])
    m2 = rsq
    kb.op("dve", lambda: nc.vector.tensor_tensor(out=m2[:, 0, :nb], in0=mean[:, :nb], in1=mean[:, :nb], op=ALU.mult),
          reads=[mean, rsq], writes=[rsq])
    kb.op("dve", lambda: nc.vector.tensor_tensor(out=var[:, :nb], in0=var[:, :nb], in1=m2[:, 0, :nb], op=ALU.subtract),
          reads=[var, rsq], writes=[var])
    kb.op("act", lambda: nc.scalar.activation(out=var[:, :nb], in_=var[:, :nb], func=AF.Sqrt, bias=eps[:]),
          reads=[var, eps], writes=[var])
    kb.op("dve", lambda: nc.vector.reciprocal(out=var[:, :nb], in_=var[:, :nb]), reads=[var], writes=[var])
    kb.op("dve", lambda: nc.vector.tensor_tensor(
        out=r[:, :, :nb], in0=r[:, :, :nb], in1=mean[:, :nb].unsqueeze(1).broadcast_to([128, 8, nb]), op=ALU.subtract),
        reads=[r, mean], writes=[r])
    kb.op("pool", lambda: nc.gpsimd.tensor_tensor(
        out=r[:, :, :nb], in0=r[:, :, :nb], in1=var[:, :nb].unsqueeze(1).broadcast_to([128, 8, nb]), op=ALU.mult),
        reads=[r, var], writes=[r])
    for oc in range(8):
        eng = "dve" if oc % 2 == 0 else "pool"
        h_ = nc.vector if eng == "dve" else nc.gpsimd
        kb.op(eng, lambda h_=h_, oc=oc: h_.tensor_scalar(
            out=out[:, oc, :nb], in0=r[:, oc, :nb], scalar1=gam[:, oc:oc + 1], scalar2=bet[:, oc:oc + 1],
            op0=ALU.mult, op1=ALU.add), reads=[r, gam, bet], writes=[out])


def load_ln_params(cx, kb, nc, l, which):
    gam = kb.sbuf("ln_g", [128, 8], F32)
    bet = kb.sbuf("ln_b", [128, 8], F32)
    kb.dma("sp", gam[:], cx.ln_gT[l, which], reads=[cx.ln_gT], writes=[gam])
    kb.dma("sp", bet[:], cx.ln_bT[l, which], reads=[cx.ln_bT], writes=[bet])
    return gam, bet


def stage_merge(cx, kb, nc, l, xsrc, modv):
    with kb.scope():
        ones, eps = ln_setup(cx, kb, nc)
        gam, bet = load_ln_params(cx, kb, nc, l, 0)
        wbr = kb.sbuf("wbr", [128, 4, 4, 1024], BF16)
        wout = kb.sbuf("wout", [128, 8, 1024], BF16)
        for i in range(4):
            kb.dma("pool", wbr[:, i], cx.w_branch[l, i].rearrange("(kc p) c -> p kc c", p=128), reads=[cx.w_branch], writes=[wbr])
        kb.dma("pool", wout[:], cx.w_out[l].rearrange("(kc p) c -> p kc c", p=128), reads=[cx.w_out], writes=[wout])
        wr = kb.sbuf("wr", [128, 8, 36], F32)
        kb.dma("sp", wr[:, :, 0:4], cx.moe_w_group[l].rearrange("(kc p) c -> p kc c", p=128), reads=[cx.moe_w_group], writes=[wr])
        kb.dma("sp", wr[:, :, 4:36], cx.moe_w_expert[l].rearrange("(kc p) c -> p kc c", p=128), reads=[cx.moe_w_expert], writes=[wr])
        rb = kb.sbuf("rb", [128, 36], F32)
        kb.dma("sp", rb[:, 0:4], cx.moe_b_group[l].partition_broadcast(128), reads=[cx.moe_b_group], writes=[rb])
        kb.dma("sp", rb[:, 4:36], cx.moe_b_expert[l].partition_broadcast(128), reads=[cx.moe_b_expert], writes=[rb])
        onep4 = kb.sbuf("onep4", [128, 8, 2], F32)
        kb.op("dve", lambda: nc.vector.tensor_scalar(out=onep4[:], in0=modv[:, 32:40, :], scalar1=1.0, scalar2=None, op0=ALU.add),
              reads=[modv], writes=[onep4])
        brt = [kb.sbuf("brt%d" % i, [128, 4, 4, MB], BF16) for i in range(1)]
        gtt = kb.sbuf("gtt", [128, 32, MB], BF16)
        xb = kb.sbuf("xbm", [128, 8, MB], F32)
        mrg = kb.sbuf("mrg", [128, 8, MB], BF16)
        r = kb.sbuf("rm", [128, 8, MB], F32)
        hx2b = kb.sbuf("hx2b", [128, 8, MB], BF16)
        rsq = kb.sbuf("rsqm", [128, 8, MB], F32)
        x1 = r
        hx2f = rsq
        mean = kb.sbuf("meanm", [128, MB], F32)
        var = kb.sbuf("varm", [128, MB], F32)
        csb = [kb.sbuf("csb%d" % i, [128, MB], F32) for i in range(4)]
        acc = kb.sbuf("accm", [128, MB], F32)
        mixs = kb.sbuf("mixs", [128, MB], F32)
        pbr = [kb.psum("pbr%d" % i, [128, MB]) for i in range(4)]
        pmx = kb.psum("pmx", [128, MB])
        ps1 = kb.psum("ps1m", [128, MB])
        ps2 = kb.psum("ps2m", [128, MB])
        prt = kb.psum("prt", [128, MB])
        brs = [cx.BR_A, cx.BR_B, cx.BR_C, cx.BR_D]
        lgt = kb.sbuf("lgt", [128, 36], F32)
        gmx = kb.sbuf("gmx", [128, 1], F32)
        ngm = kb.sbuf("ngm", [128, 1], F32)
        ge = kb.sbuf("ge", [128, 4], F32)
        gs = kb.sbuf("gsr", [128, 1], F32)
        oh = kb.sbuf("oh", [128, 4], F32)
        lem = kb.sbuf("lem", [128, 4, 8], F32)
        mx8 = kb.sbuf("mx8", [128, 8], F32)
        dd = kb.sbuf("dd", [128, 1], F32)
        w1 = kb.sbuf("w1", [128, 1], F32)
        w2 = kb.sbuf("w2", [128, 1], F32)
        tt1 = kb.sbuf("tt1", [128, 32], F32)
        tt2 = kb.sbuf("tt2", [128, 32], F32)
        wgt = kb.sbuf("wgt", [128, 32], F32)
        for bi, (t0, nb) in enumerate(MBLKS):
            col = blkcol(t0)
            b_ = brt[0]
            for i in range(4):
                kb.dma("sp", b_[:, i, :, :nb], brs[i][:, t0:t0 + nb].rearrange("(kc p) t -> p kc t", p=128),
                       reads=[brs[i]], writes=[b_])
            kb.dma("sp", gtt[:, :, :nb], cx.GATE[:, t0:t0 + nb].rearrange("(j p) t -> p j t", p=128), reads=[cx.GATE], writes=[gtt])
            kb.dma("sp", xb[:, :, :nb], xsrc[:, t0:t0 + nb].rearrange("(kc p) t -> p kc t", p=128), reads=[xsrc], writes=[xb])
            for oc in range(8):
                for i in range(4):
                    kb.mm([(lambda i=i, kc=kc, oc=oc: nc.tensor.matmul(
                        pbr[i][:, :nb], lhsT=wbr[:, i, kc, oc * 128:(oc + 1) * 128], rhs=b_[:, i, kc, :nb],
                        start=(kc == 0), stop=(kc == 3))) for kc in range(4)], reads=[wbr, b_], writes=[pbr[i]])
                    kb.op("act", lambda i=i: nc.scalar.copy(out=csb[i][:, :nb], in_=pbr[i][:, :nb]), reads=[pbr[i]], writes=[csb[i]])
                    eng = "dve" if i % 2 == 0 else "pool"
                    h_ = nc.vector if eng == "dve" else nc.gpsimd
                    kb.op(eng, lambda h_=h_, i=i, oc=oc: h_.tensor_tensor(
                        out=csb[i][:, :nb], in0=csb[i][:, :nb], in1=gtt[:, i * 8 + oc, :nb], op=ALU.mult),
                        reads=[csb[i], gtt], writes=[csb[i]])
                kb.op("dve", lambda: nc.vector.tensor_tensor(out=acc[:, :nb], in0=csb[0][:, :nb], in1=csb[1][:, :nb], op=ALU.add),
                      reads=[csb[0], csb[1]], writes=[acc])
                kb.op("pool", lambda: nc.gpsimd.tensor_tensor(out=csb[2][:, :nb], in0=csb[2][:, :nb], in1=csb[3][:, :nb], op=ALU.add),
                      reads=[csb[2], csb[3]], writes=[csb[2]])
                kb.op("dve", lambda oc=oc: nc.vector.tensor_tensor(out=mrg[:, oc, :nb], in0=acc[:, :nb], in1=csb[2][:, :nb], op=ALU.add),
                      reads=[acc, csb[2]], writes=[mrg])
            for oc in range(8):
                kb.mm([(lambda kc=kc, oc=oc: nc.tensor.matmul(
                    pmx[:, :nb], lhsT=wout[:, kc, oc * 128:(oc + 1) * 128], rhs=mrg[:, kc, :nb],
                    start=(kc == 0), stop=(kc == 7))) for kc in range(8)], reads=[wout, mrg], writes=[pmx])
                kb.op("act", lambda: nc.scalar.copy(out=mixs[:, :nb], in_=pmx[:, :nb]), reads=[pmx], writes=[mixs])
                kb.op("pool", lambda oc=oc: nc.gpsimd.tensor_scalar(
                    out=mixs[:, :nb], in0=mixs[:, :nb], scalar1=modv[:, 16 + oc, col:col + 1], scalar2=None, op0=ALU.mult),
                    reads=[mixs, modv], writes=[mixs])
                kb.op("dve", lambda oc=oc: nc.vector.scalar_tensor_tensor(
                    out=r[:, oc, :nb], in0=xb[:, oc, :nb], scalar=ALPHA, in1=mixs[:, :nb], op0=ALU.mult, op1=ALU.add),
                    reads=[xb, mixs], writes=[r])
            ln_block(kb, nc, r, nb, gam, bet, x1, ones, eps, (rsq, ps1, ps2, mean, var))
            kb.dma("sp", cx.XT[:, t0:t0 + nb].rearrange("(kc p) t -> p kc t", p=128), x1[:, :, :nb],
                   reads=[x1], writes=[], acc_writes=[cx.XT])
            for oc in range(8):
                eng = "dve" if oc % 2 == 0 else "pool"
                h_ = nc.vector if eng == "dve" else nc.gpsimd
                kb.op(eng, lambda h_=h_, oc=oc: h_.tensor_scalar(
                    out=hx2f[:, oc, :nb], in0=x1[:, oc, :nb], scalar1=onep4[:, oc, col:col + 1],
                    scalar2=modv[:, 24 + oc, col:col + 1], op0=ALU.mult, op1=ALU.add),
                    reads=[x1, onep4, modv], writes=[hx2f])
            kb.op("act", lambda: nc.scalar.copy(out=hx2b[:, :, :nb], in_=hx2f[:, :, :nb]), reads=[hx2f], writes=[hx2b])
            kb.dma("sp", cx.HX2[:, t0:t0 + nb].rearrange("(kc p) t -> p kc t", p=128), hx2b[:, :, :nb],
                   reads=[hx2b], writes=[], acc_writes=[cx.HX2])
            for tj in range(nb // 128):
                ti = t0 // 128 + tj
                tsl = slice(tj * 128, (tj + 1) * 128)
                kb.mm([(lambda kc=kc: nc.tensor.matmul(prt[:, 0:36], lhsT=hx2f[:, kc, tsl], rhs=wr[:, kc, :],
                                                       start=(kc == 0), stop=(kc == 7))) for kc in range(8)],
                      reads=[hx2f, wr], writes=[prt])
                kb.op("act", lambda: nc.scalar.copy(out=lgt[:], in_=prt[:, 0:36]), reads=[prt], writes=[lgt])
                kb.op("dve", lambda: nc.vector.tensor_tensor(out=lgt[:], in0=lgt[:], in1=rb[:], op=ALU.add), reads=[lgt, rb], writes=[lgt])
                kb.op("dve", lambda: nc.vector.tensor_reduce(out=gmx[:], in_=lgt[:, 0:4], axis=AX.X, op=ALU.max), reads=[lgt], writes=[gmx])
                kb.op("dve", lambda: nc.vector.tensor_scalar(out=ngm[:], in0=gmx[:], scalar1=-1.0, scalar2=None, op0=ALU.mult), reads=[gmx], writes=[ngm])
                kb.op("act", lambda: nc.scalar.activation(out=ge[:], in_=lgt[:, 0:4], func=AF.Exp, bias=ngm[:]), reads=[lgt, ngm], writes=[ge])
                kb.op("dve", lambda: nc.vector.tensor_reduce(out=gs[:], in_=ge[:], axis=AX.X, op=ALU.add), reads=[ge], writes=[gs])
                kb.op("dve", lambda: nc.vector.reciprocal(out=gs[:], in_=gs[:]), reads=[gs], writes=[gs])
                kb.op("dve", lambda: nc.vector.tensor_scalar(out=oh[:], in0=lgt[:, 0:4], scalar1=gmx[:], scalar2=None, op0=ALU.is_equal), reads=[lgt, gmx], writes=[oh])
                kb.op("dve", lambda: nc.vector.tensor_scalar(out=oh[:], in0=oh[:], scalar1=-1.0, scalar2=1e30, op0=ALU.add, op1=ALU.mult), reads=[oh], writes=[oh])
                kb.op("dve", lambda: nc.vector.tensor_tensor(
                    out=lem[:], in0=lgt[:, 4:36].rearrange("p (g e) -> p g e", e=8), in1=oh[:].unsqueeze(2).broadcast_to([128, 4, 8]), op=ALU.add),
                    reads=[lgt, oh], writes=[lem])
                lemf = lem[:].rearrange("p g e -> p (g e)")
                kb.op("dve", lambda: nc.vector.max(out=mx8[:], in_=lem[:].rearrange("p g e -> p (g e)")), reads=[lem], writes=[mx8])
                kb.op("dve", lambda: nc.vector.tensor_tensor(out=dd[:], in0=mx8[:, 1:2], in1=mx8[:, 0:1], op=ALU.subtract), reads=[mx8], writes=[dd])
                kb.op("act", lambda: nc.scalar.activation(out=dd[:], in_=dd[:], func=AF.Exp), reads=[dd], writes=[dd])
                kb.op("dve", lambda: nc.vector.tensor_scalar(out=w1[:], in0=dd[:], scalar1=1.0, scalar2=None, op0=ALU.add), reads=[dd], writes=[w1])
                kb.op("dve", lambda: nc.vector.reciprocal(out=w1[:], in_=w1[:]), reads=[w1], writes=[w1])
                kb.op("dve", lambda: nc.vector.tensor_tensor(out=w2[:], in0=dd[:], in1=w1[:], op=ALU.mult), reads=[dd, w1], writes=[w2])
                kb.op("dve", lambda: nc.vector.tensor_scalar(
                    out=tt1[:], in0=lem[:].rearrange("p g e -> p (g e)"), scalar1=mx8[:, 0:1], scalar2=w1[:], op0=ALU.is_equal, op1=ALU.mult),
                    reads=[lem, mx8, w1], writes=[tt1])
                kb.op("dve", lambda: nc.vector.tensor_scalar(
                    out=tt2[:], in0=lem[:].rearrange("p g e -> p (g e)"), scalar1=mx8[:, 1:2], scalar2=w2[:], op0=ALU.is_equal, op1=ALU.mult),
                    reads=[lem, mx8, w2], writes=[tt2])
                kb.op("dve", lambda: nc.vector.tensor_tensor(out=tt1[:], in0=tt1[:], in1=tt2[:], op=ALU.add), reads=[tt1, tt2], writes=[tt1])
                kb.op("dve", lambda: nc.vector.tensor_scalar(out=wgt[:], in0=tt1[:], scalar1=gs[:], scalar2=None, op0=ALU.mult), reads=[tt1, gs], writes=[wgt])
                kb.dma("sp", cx.WG_t[:, ti, :], wgt[:], reads=[wgt], writes=[], acc_writes=[cx.WG_t])


def stage_moe(cx, kb, nc, l, modv, yout=None):
    with kb.scope():
        ones, eps = ln_setup(cx, kb, nc)
        gam, bet = load_ln_params(cx, kb, nc, l, 1)
        wgT = kb.sbuf("wgT", [32, T], F32)
        wgl = kb.sbuf("wgl", [128, NT, 32], F32)
        id32 = kb.sbuf("id32", [128, 128], F32)
        sel = kb.sbuf("sel", [32, 32, 128], F32)
        kb.dma("sp", wgl[:], cx.WG_t[:], reads=[cx.WG_t], writes=[wgl])
        kb.dma("sp", id32[:], cx.m_ident32[:], reads=[cx.m_ident32], writes=[id32])
        kb.dma("sp", sel[:], cx.m_sel[:], reads=[cx.m_sel], writes=[sel])
        ptr = [kb.psum("ptr%d" % i, [128, 512]) for i in range(2)]
        for ti in range(NT):
            p_ = ptr[ti % 2]
            kb.mm([lambda p_=p_, ti=ti: nc.tensor.matmul(p_[:32, 0:128], lhsT=wgl[:, ti, :], rhs=id32[:], start=True, stop=True)],
                  reads=[wgl, id32], writes=[p_])
            kb.op("act", lambda p_=p_, ti=ti: nc.scalar.copy(out=wgT[:, ti * 128:(ti + 1) * 128], in_=p_[:32, 0:128]),
                  reads=[p_], writes=[], acc_writes=[wgT])
        hx = kb.sbuf("hxm", [128, 8, 512], BF16)
        yacc = kb.sbuf("yacc", [128, 8, 512], F32)
        hid = kb.sbuf("hid", [128, 16, 512], BF16)
        wdg = kb.sbuf("wdg", [128, 8, 2, 1024], BF16)
        wgs = [kb.sbuf("wgs%d" % i, [128, 8, 256], BF16) for i in range(2)]
        wus = [kb.sbuf("wus%d" % i, [128, 8, 256], BF16) for i in range(2)]
        xb = kb.sbuf("xbe", [128, 8, 512], F32)
        rsq = kb.sbuf("rsqe", [128, 8, 512], F32)
        mean = kb.sbuf("meane", [128, 512], F32)
        var = kb.sbuf("vare", [128, 512], F32)
        reps = [kb.sbuf("reps%d" % i, [128, 512], F32) for i in range(2)]
        ssb = [kb.sbuf("ssb%d" % i, [128, 512], F32) for i in range(2)]
        tsb = [kb.sbuf("tsb%d" % i, [128, 512], F32) for i in range(2)]
        ytmp = [kb.sbuf("ytmp%d" % i, [128, 512], F32) for i in range(2)]
        pg = [kb.psum("pg%d" % i, [128, 512]) for i in range(2)]
        pu = [kb.psum("pu%d" % i, [128, 512]) for i in range(2)]
        py = ptr
        ps1 = kb.psum("ps1e", [128, 512])
        ps2 = kb.psum("ps2e", [128, 512])
        prep = ps1
        nw = 0
        it = 0
        for bi, (t0, nb) in enumerate(BLKS):
            col = blkcol(t0)
            kb.dma("sp", hx[:, :, :nb], cx.HX2[:, t0:t0 + nb].rearrange("(kc p) t -> p kc t", p=128), reads=[cx.HX2], writes=[hx])
            kb.dma("sp", xb[:, :, :nb], cx.XT[:, t0:t0 + nb].rearrange("(kc p) t -> p kc t", p=128), reads=[cx.XT], writes=[xb])
            for g in range(4):
                kb.dma("pool", wdg[:].rearrange("p e hc d -> p (e hc) d"),
                       cx.moe_w_down[l, g].rearrange("e (hc p) d -> p (e hc) d", p=128), reads=[cx.moe_w_down], writes=[wdg])
                for e_ in range(8):
                    e = g * 8 + e_
                    wg_, wu_ = wgs[nw % 2], wus[nw % 2]
                    nw += 1
                    kb.dma("pool", wg_[:], cx.moe_w_gate[l, g, e_].rearrange("(kc p) f -> p kc f", p=128), reads=[cx.moe_w_gate], writes=[wg_])
                    kb.dma("pool", wu_[:], cx.moe_w_up[l, g, e_].rearrange("(kc p) f -> p kc f", p=128), reads=[cx.moe_w_up], writes=[wu_])
                    rp = reps[e % 2]
                    kb.mm([lambda e=e: nc.tensor.matmul(prep[:, :nb], lhsT=sel[:, e, :], rhs=wgT[:, t0:t0 + nb], start=True, stop=True)],
                          reads=[sel, wgT], writes=[prep])
                    kb.op("act", lambda rp=rp: nc.scalar.copy(out=rp[:, :nb], in_=prep[:, :nb]), reads=[prep], writes=[rp])
                    for hc in range(2):
                        a = it % 2
                        it += 1
                        kb.mm([(lambda kc=kc, a=a, hc=hc, wg_=wg_: nc.tensor.matmul(
                            pg[a][:, :nb], lhsT=wg_[:, kc, hc * 128:(hc + 1) * 128], rhs=hx[:, kc, :nb],
                            start=(kc == 0), stop=(kc == 7))) for kc in range(8)], reads=[wg_, hx], writes=[pg[a]])
                        kb.mm([(lambda kc=kc, a=a, hc=hc, wu_=wu_: nc.tensor.matmul(
                            pu[a][:, :nb], lhsT=wu_[:, kc, hc * 128:(hc + 1) * 128], rhs=hx[:, kc, :nb],
                            start=(kc == 0), stop=(kc == 7))) for kc in range(8)], reads=[wu_, hx], writes=[pu[a]])
                        kb.op("act", lambda a=a: nc.scalar.activation(out=ssb[a][:, :nb], in_=pg[a][:, :nb], func=AF.Silu),
                              reads=[pg[a]], writes=[ssb[a]])
                        kb.op("act", lambda a=a: nc.scalar.copy(out=tsb[a][:, :nb], in_=pu[a][:, :nb]), reads=[pu[a]], writes=[tsb[a]])
                        kb.op("dve", lambda a=a: nc.vector.tensor_tensor(out=tsb[a][:, :nb], in0=tsb[a][:, :nb], in1=ssb[a][:, :nb], op=ALU.mult),
                              reads=[tsb[a], ssb[a]], writes=[tsb[a]])
                        kb.op("pool", lambda a=a, rp=rp, e_=e_, hc=hc: nc.gpsimd.tensor_tensor(
                            out=hid[:, e_ * 2 + hc, :nb], in0=tsb[a][:, :nb], in1=rp[:, :nb], op=ALU.mult),
                            reads=[tsb[a], rp], writes=[], acc_writes=[hid])
                for oc in range(8):
                    p_ = py[oc % 2]
                    fns = []
                    for j in range(16):
                        fns.append(lambda j=j, oc=oc, p_=p_: nc.tensor.matmul(
                            p_[:, :nb], lhsT=wdg[:, j // 2, j % 2, oc * 128:(oc + 1) * 128], rhs=hid[:, j, :nb],
                            start=(j == 0), stop=(j == 15)))
                    kb.mm(fns, reads=[wdg, hid], writes=[p_])
                    if g == 0:
                        kb.op("act", lambda oc=oc, p_=p_: nc.scalar.copy(out=yacc[:, oc, :nb], in_=p_[:, :nb]), reads=[p_], writes=[], acc_writes=[yacc])
                    else:
                        yt = ytmp[oc % 2]
                        kb.op("act", lambda yt=yt, p_=p_: nc.scalar.copy(out=yt[:, :nb], in_=p_[:, :nb]), reads=[p_], writes=[yt])
                        eng = "dve" if oc % 2 == 0 else "pool"
                        h_ = nc.vector if eng == "dve" else nc.gpsimd
                        kb.op(eng, lambda h_=h_, oc=oc, yt=yt: h_.tensor_tensor(
                            out=yacc[:, oc, :nb], in0=yacc[:, oc, :nb], in1=yt[:, :nb], op=ALU.add),
                            reads=[yt, yacc], writes=[], acc_writes=[yacc])
            for oc in range(8):
                kb.op("pool", lambda oc=oc: nc.gpsimd.tensor_scalar(
                    out=yacc[:, oc, :nb], in0=yacc[:, oc, :nb], scalar1=modv[:, 40 + oc, col:col + 1], scalar2=None, op0=ALU.mult),
                    reads=[yacc, modv], writes=[yacc])
                kb.op("dve", lambda oc=oc: nc.vector.scalar_tensor_tensor(
                    out=yacc[:, oc, :nb], in0=xb[:, oc, :nb], scalar=ALPHA, in1=yacc[:, oc, :nb], op0=ALU.mult, op1=ALU.add),
                    reads=[xb, yacc], writes=[yacc])
            ln_block(kb, nc, yacc, nb, gam, bet, yacc, ones, eps, (rsq, ps1, ps2, mean, var))
            kb.dma("sp", cx.XT[:, t0:t0 + nb].rearrange("(kc p) t -> p kc t", p=128), yacc[:, :, :nb],
                   reads=[yacc], writes=[], acc_writes=[cx.XT])
            if yout is not None and t0 < TL:
                kb.dma("sp", yout[:, t0:t0 + nb].rearrange("(kc p) t -> p kc t", p=128), yacc[:, :, :nb],
                       reads=[yacc], writes=[], acc_writes=[yout])


def rope_tables():
    t = np.arange(TL)
    row = (t // 64).astype(np.float32)
    colp = (t % 64).astype(np.float32)
    nf = 16
    inv = (np.float32(10000.0) ** (-np.arange(nf, dtype=np.float32) / np.float32(nf))).astype(np.float32)
    cos = np.ones((128, T), np.float32)
    sin = np.zeros((128, T), np.float32)
    for q in range(128):
        d = q % 64
        pos = row if d < 32 else colp
        ang = (pos * inv[d % 16]).astype(np.float32)
        cos[q, :TL] = np.cos(ang).astype(np.float32)
        sin[q, :TL] = np.sin(ang).astype(np.float32)
    pt = np.zeros((128, 128), np.float32)
    for m in range(128):
        if (m % 32) < 16:
            pt[m + 16, m] = -1.0
        else:
            pt[m - 16, m] = 1.0
    return cos, sin, pt


_CONST = {}
_NAIDX = {}


def consts():
    if not _CONST:
        cos, sin, pt = rope_tables()
        _CONST.update(rope_cos=cos, rope_sin=sin, rope_pt=pt)
        _CONST.update(scan_masks())
        _CONST.update(wa_masks())
        sel = np.zeros((32, 32, 128), np.float32)
        for e in range(32):
            sel[e, e, :] = 1.0
        _CONST["m_sel"] = sel
        _CONST["m_ident32"] = np.eye(128, dtype=np.float32)
        _k, _valid, _ri, _ci = na_consts()
        _CONST["m_navalid"] = _valid
        _NAIDX["ri"] = _ri
        _NAIDX["ci"] = _ci
    return _CONST


def core_inputs(inp, b):
    f = lambda a: np.ascontiguousarray(a, dtype=np.float32)
    m = {}
    m["xin"] = f(np.concatenate([inp["x"][b].T, inp["ctx"][b].T], axis=1))
    cv = np.stack([inp["c"][b].reshape(8, 128).T, inp["c_ctx"].reshape(8, 128).T], axis=2)
    m["cvec"] = f(cv)
    m["w_mod"] = f(inp["w_mod"])
    m["b_modT"] = f(inp["b_mod"].reshape(2, 48, 128).transpose(0, 2, 1))
    m["w_in"] = f(inp["w_in"])
    m["hg_gamma"] = f(inp["hg_gamma"])
    m["gla_lr_w"] = f(inp["gla_lr_w"])
    m["gla_lr_b"] = f(inp["gla_lr_b"])
    m["wa_sink"] = f(inp["wa_sink"])
    for k in ("w_branch", "w_out", "moe_w_group", "moe_b_group", "moe_w_expert", "moe_b_expert",
              "moe_w_gate", "moe_w_up", "moe_w_down"):
        m[k] = f(inp[k])
    m["ln_gT"] = f(inp["ln_g"].reshape(2, 2, 8, 128).transpose(0, 1, 3, 2))
    m["ln_bT"] = f(inp["ln_b"].reshape(2, 2, 8, 128).transpose(0, 1, 3, 2))
    consts()
    m["na_bias"] = f(inp["na_rpb"][:, :, _NAIDX["ri"], _NAIDX["ci"]])
    m.update(consts())
    return m


FUSED = 1


def build_program(layers, final):
    nc = bass.Bass("TRN2", target_bir_lowering=False)
    cx = Ctx()
    declare_inputs(cx, nc)
    if final:
        yout = Buf(nc.dram_tensor("yout", [D, TL], F32, kind="ExternalOutput"), "yout")
    else:
        yout = None
    with ExitStack() as st:
        kb = KB(nc, st)
        declare_scratch(cx, kb, debug=False, as_output=(() if final else ("XT",)))
        modT = [kb.sbuf("modT%d" % l, [128, 48, 2], F32) for l in range(2)]
        stage_mod(cx, kb, nc, modT)
        first = True
        for l in layers:
            xsrc = cx.xin if first else cx.XT
            first = False
            with kb.scope():
                lbt = [kb.sbuf("lbt%d" % i, [128, 1024], F32) for i in range(2)]
                omlt = [kb.sbuf("omlt%d" % i, [128, 1024], F32) for i in range(2)]
                stage_lb(cx, kb, nc, lbt, omlt)
                hxT = kb.sbuf("hxT", [128, 8, T], BF16)
                stage_modulate(cx, kb, nc, xsrc, modT[l], 0, 1, hxT)
                stage_inproj_fm(cx, kb, nc, l, hxT)
                stage_inproj_tm(cx, kb, nc, l, hxT, lbt[l], omlt[l])
            stage_gla_gate(cx, kb, nc, l)
            stage_scan(cx, kb, nc, "gla")
            stage_scan(cx, kb, nc, "hgrn")
            stage_wa(cx, kb, nc, l)
            stage_na(cx, kb, nc, l)
            stage_merge(cx, kb, nc, l, xsrc, modT[l])
            stage_moe(cx, kb, nc, l, modT[l], yout if (final and l == layers[-1]) else None)
        if final:
            kb.finish([yout])
        else:
            kb.finish([cx.XT])
    return nc


def kernel(**inputs):
    inp = {k: np.asarray(v) for k, v in inputs.items()}
    in_maps = [core_inputs(inp, b) for b in range(4)]
    if FUSED:
        nc = build_program([0, 1], True)
        res = run_bass_kernel_spmd(nc, in_maps, core_ids=list(range(4)))
    else:
        nc0 = build_program([0], False)
        res0 = run_bass_kernel_spmd(nc0, in_maps, core_ids=list(range(4)))
        for b in range(4):
            in_maps[b]["xin"] = np.ascontiguousarray(res0.results[b]["XT"])
        nc1 = build_program([1], True)
        res = run_bass_kernel_spmd(nc1, in_maps, core_ids=list(range(4)))
    out = np.stack([np.ascontiguousarray(res.results[b]["yout"].T) for b in range(4)], axis=0)
    return out.astype(np.float32)
```

```python
import numpy as np
from contextlib import ExitStack
import concourse.bass as bass
import concourse.mybir as mybir
from concourse.bass_utils import run_bass_kernel_spmd
import os

F32 = mybir.dt.float32
BF16 = mybir.dt.bfloat16
U32 = mybir.dt.uint32
AF = mybir.ActivationFunctionType
ALU = mybir.AluOpType
AX = mybir.AxisListType


class Buf:
    __slots__ = ("t", "w", "r", "name")

    def __init__(self, t, name=""):
        self.t = t
        self.w = []
        self.r = []
        self.name = name

    def __getitem__(self, idx):
        return self.t[idx]


class Dep:
    __slots__ = ("w", "r")

    def __init__(self):
        self.w = []
        self.r = []


class Eng:
    def __init__(self, name, h, sem):
        self.name = name
        self.h = h
        self.sem = sem
        self.count = 0
        self.seen = {}


class KB:
    def __init__(self, nc, stack, n_dma_sems=8):
        self.nc = nc
        self.stack = stack
        self.sems = {}
        self.engs = {}
        for name, h in (("pe", nc.tensor), ("act", nc.scalar), ("dve", nc.vector),
                        ("pool", nc.gpsimd), ("sp", nc.sync)):
            s = stack.enter_context(nc.semaphore("s_" + name))
            self.sems[id(s)] = s
            self.engs[name] = Eng(name, h, s)
        self.dma_pool = {}
        for q in ("sp", "pool", "act"):
            lst = []
            for i in range(n_dma_sems):
                s = stack.enter_context(nc.semaphore("d_%s%d" % (q, i)))
                self.sems[id(s)] = s
                lst.append([s, 0])
            self.dma_pool[q] = [lst, 0]
        self.ninstr = 0

    def scope(self):
        kb = self

        class _Scope:
            def __enter__(self_s):
                self_s.prev = kb.stack
                self_s.st = ExitStack()
                self_s.st.__enter__()
                kb.stack = self_s.st
                kb.nscope = getattr(kb, "nscope", 0) + 1
                return self_s

            def __exit__(self_s, *a):
                if a[0] is None:
                    kb.barrier()
                kb.stack = self_s.prev
                return self_s.st.__exit__(*a)
        return _Scope()

    def sbuf(self, name, shape, dtype):
        name = "%s_s%d" % (name, getattr(self, "nscope", 0))
        t = self.stack.enter_context(self.nc.sbuf_tensor(name, list(shape), dtype))
        return Buf(t, name)

    def psum(self, name, shape, dtype=F32):
        name = "%s_s%d" % (name, getattr(self, "nscope", 0))
        t = self.stack.enter_context(self.nc.psum_tensor(name, list(shape), dtype))
        return Buf(t, name)

    def dram(self, name, shape, dtype, kind="Internal"):
        t = self.nc.dram_tensor(name, list(shape), dtype, kind=kind)
        return Buf(t, name)

    def _wait(self, e, deps):
        need = {}
        for (sid, val) in deps:
            if need.get(sid, 0) < val:
                need[sid] = val
        for sid, val in need.items():
            if e.seen.get(sid, 0) < val:
                e.h.wait_ge(self.sems[sid], val)
                e.seen[sid] = val
                self.ninstr += 1

    def _collect(self, reads, writes, acc_writes=()):
        deps = []
        for b in reads:
            deps += b.w
        for b in writes:
            deps += b.w
            deps += b.r
        for b in acc_writes:
            deps += b.r
        return deps

    @staticmethod
    def _compact(lst):
        mx = {}
        for (sid, v) in lst:
            if mx.get(sid, 0) < v:
                mx[sid] = v
        return list(mx.items())

    def _commit(self, token, reads, writes, acc_writes=()):
        for b in acc_writes:
            b.w.append(token)
            if len(b.w) > 64:
                b.w = self._compact(b.w)
        for b in reads:
            b.r.append(token)
            if len(b.r) > 64:
                mx = {}
                for (sid, v) in b.r:
                    if mx.get(sid, 0) < v:
                        mx[sid] = v
                b.r = list(mx.items())
        for b in writes:
            b.w = [token]
            b.r = []

    def _cutcheck(self):
        import os
        cut = os.environ.get("KB_CUT")
        self.nops = getattr(self, "nops", 0) + 1
        skip = os.environ.get("KB_SKIP")
        if skip and self.nops in [int(x) for x in skip.split(",")]:
            return True
        return cut is not None and self.nops > int(cut)

    def op(self, eng, fn, reads=(), writes=(), same_engine_sync=True, acc_writes=()):
        if self._cutcheck():
            return None
        e = self.engs[eng]
        deps = self._collect(reads, writes, acc_writes)
        if not same_engine_sync:
            deps = [d for d in deps if d[0] != id(e.sem)]
        self._wait(e, deps)
        ins = fn()
        e.count += 1
        ins.then_inc(e.sem, 1)
        self.ninstr += 1
        token = (id(e.sem), e.count)
        self._commit(token, reads, writes, acc_writes)
        return token

    def mm(self, fns, reads=(), writes=()):
        if self._cutcheck():
            return None
        e = self.engs["pe"]
        deps = self._collect(reads, writes)
        deps = [d for d in deps if d[0] != id(e.sem)]
        self._wait(e, deps)
        ins = None
        for fn in fns:
            ins = fn()
            self.ninstr += 1
        e.count += 1
        ins.then_inc(e.sem, 1)
        token = (id(e.sem), e.count)
        self._commit(token, reads, writes)
        return token

    def dma(self, q, out, in_, reads=(), writes=(), acc_writes=(), **kw):
        e = self.engs[q]
        pool = self.dma_pool[q]
        lst, idx = pool
        ent = lst[idx % len(lst)]
        pool[1] = idx + 1
        s, total = ent
        deps = self._collect(reads, writes, acc_writes)
        deps.append((id(s), total))
        import os
        if os.environ.get("DMA_SERIAL") and getattr(self, "_last_dma", {}).get(q):
            deps.append(self._last_dma[q])
        self._wait(e, deps)
        ins = e.h.dma_start(out=out, in_=in_, **kw)
        ins.then_inc(s, 16)
        ent[1] = total + 16
        self.ninstr += 1
        token = (id(s), total + 16)
        if not hasattr(self, "_last_dma"):
            self._last_dma = {}
        self._last_dma[q] = token
        self._commit(token, reads, writes, acc_writes)
        return token

    def init_memory(self, nbytes=196608):
        nc = self.nc
        with self.scope():
            n = nbytes // 4
            big = self.sbuf("initbig", [128, n], F32)
            q = n // 4
            self.op("dve", lambda: nc.vector.memset(big[:, 0:q], 0.0), writes=[])
            self.op("pool", lambda: nc.gpsimd.memset(big[:, q:2 * q], 0.0), writes=[])
            self.op("dve", lambda: nc.vector.memset(big[:, 2 * q:3 * q], 0.0), writes=[])
            self.op("pool", lambda: nc.gpsimd.memset(big[:, 3 * q:n], 0.0), writes=[])
            pss = [self.psum("initps%d" % i, [128, 512]) for i in range(8)]
            for p in pss:
                self.op("dve", lambda p=p: nc.vector.memset(p[:], 0.0), writes=[])

    def barrier(self):
        allt = []
        for e in self.engs.values():
            if e.count:
                allt.append((id(e.sem), e.count))
        for q, (lst, _) in self.dma_pool.items():
            for s, total in lst:
                if total:
                    allt.append((id(s), total))
        for e in self.engs.values():
            self._wait(e, allt)

    def finish(self, tokens_bufs):
        e = self.engs["sp"]
        deps = []
        for b in tokens_bufs:
            deps += b.w
        self._wait(e, deps)

T = 4352
TL = 4096

D = 1024
TL = 4096
TC = 256
T = TL + TC
NT = T // 128
BLKS = [(i * 512, 512) for i in range(8)] + [(4096, 256)]
DIN = 10528
ALPHA = (2.0 * 2) ** 0.25

COLS = dict(a_q=(0, 256), a_k=(256, 256), a_v=(512, 512), a_g=(1024, 512), a_lr=(1536, 32),
            b_q=(1568, 512), b_k=(2080, 512), b_v=(2592, 512),
            c_q=(3104, 512), c_k=(3616, 128), c_v=(3744, 128),
            d_q=(3872, 512), d_f=(4384, 1024), d_i=(5408, 512), d_g=(5920, 512), gate=(6432, 4096))


def blkcol(t0):
    return 1 if t0 >= TL else 0


class Ctx:
    pass


def declare_inputs(cx, nc):
    def din(name, shape, dt=F32):
        return Buf(nc.dram_tensor(name, list(shape), dt, kind="ExternalInput"), name)
    cx.xin = din("xin", [D, T])
    cx.cvec = din("cvec", [128, 8, 2])
    cx.w_mod = din("w_mod", [2, D, 6 * D])
    cx.b_modT = din("b_modT", [2, 128, 48])
    cx.w_in = din("w_in", [2, D, DIN])
    cx.rope_cos = din("rope_cos", [128, T])
    cx.rope_sin = din("rope_sin", [128, T])
    cx.rope_pt = din("rope_pt", [128, 128])
    cx.hg_gamma = din("hg_gamma", [2, 2, 512])
    cx.gla_lr_w = din("gla_lr_w", [2, 2, 16, 256])
    cx.gla_lr_b = din("gla_lr_b", [2, 2, 256])
    for nm in ("mg", "mh"):
        setattr(cx, nm + "_mq", din(nm + "_mq", [128, 2, 130]))
        setattr(cx, nm + "_mk", din(nm + "_mk", [128, 2, 128]))
    cx.m_ma = din("m_ma", [128, 2, 128])
    cx.m_ident = din("m_ident", [128, 128])
    cx.m_mau = din("m_mau", [128, 2, 128], U32)
    cx.m_wa = din("m_wa", [128, 2, 128])
    cx.wa_sink = din("wa_sink", [2, 8])
    cx.m_navalid = din("m_navalid", [128, 21, 128])
    cx.w_branch = din("w_branch", [2, 4, 512, 1024])
    cx.w_out = din("w_out", [2, 1024, 1024])
    cx.ln_gT = din("ln_gT", [2, 2, 128, 8])
    cx.ln_bT = din("ln_bT", [2, 2, 128, 8])
    cx.moe_w_group = din("moe_w_group", [2, 1024, 4])
    cx.moe_b_group = din("moe_b_group", [2, 4])
    cx.moe_w_expert = din("moe_w_expert", [2, 1024, 32])
    cx.moe_b_expert = din("moe_b_expert", [2, 32])
    cx.moe_w_gate = din("moe_w_gate", [2, 4, 8, 1024, 256])
    cx.moe_w_up = din("moe_w_up", [2, 4, 8, 1024, 256])
    cx.moe_w_down = din("moe_w_down", [2, 4, 8, 256, 1024])
    cx.m_sel = din("m_sel", [32, 32, 128])
    cx.m_ident32 = din("m_ident32", [128, 128])
    cx.na_bias = din("na_bias", [2, 8, 128, 21, 128])


def declare_scratch(cx, kb, debug, as_input=(), as_output=()):
    kind = "ExternalOutput" if debug else "Internal"

    def ds(name, shape, dt):
        b = kb.dram(name, shape, dt, kind=("ExternalInput" if name in as_input else ("ExternalOutput" if name in as_output else kind)))
        setattr(cx, name, b)
        return b
    ds("XT", [D, T], F32)
    ds("QA", [256, T], BF16)
    ds("GA", [512, T], BF16)
    ds("LR1", [33, T], F32)
    ds("QB", [512, T], BF16)
    ds("KB_", [512, T], BF16)
    ds("QC", [512, T], BF16)
    ds("KC", [128, T], BF16)
    ds("QD", [512, T], BF16)
    ds("GD", [512, T], BF16)
    ds("GATE", [4096, T], BF16)
    ds("KVA_t", [128, NT, 768], BF16)
    ds("VB_t", [128, NT, 512], BF16)
    ds("VC_t", [128, NT, 128], BF16)
    ds("VD_t", [128, NT, 512], BF16)
    ds("LGD_t", [128, NT, 1024], F32)
    ds("KD_t", [128, NT, 1024], BF16)
    ds("LGA_t", [128, NT, 512], F32)
    ds("MODT", [2, 128, 96], F32)
    ds("HX2", [D, T], BF16)
    ds("WGB", [32, 128, 2048], BF16)
    ds("WUB", [32, 128, 2048], BF16)
    ds("WDB", [4, 128, 16384], BF16)
    ds("WG_t", [128, NT, 32], F32)
    ds("WGT", [32, T], F32)
    ds("BR_A", [512, T], BF16)
    ds("BR_B", [512, T], BF16)
    ds("BR_C", [512, T], BF16)
    ds("BR_D", [512, T], BF16)


def stage_mod(cx, kb, nc, modT):
    with kb.scope():
        cv = kb.sbuf("cv", [128, 8, 2], F32)
        sc = kb.sbuf("sc", [128, 8, 2], F32)
        kb.dma("sp", cv[:], cx.cvec[:], reads=[cx.cvec], writes=[cv])
        kb.op("act", lambda: nc.scalar.activation(out=sc[:], in_=cv[:], func=AF.Silu), reads=[cv], writes=[sc])
        wbufs = [kb.sbuf("wm%d" % i, [128, 8, 1024], F32) for i in range(2)]
        bm = kb.sbuf("bm", [128, 48], F32)
        ps = kb.psum("ps_mod", [128, 96])
        wi = 0
        for l in range(2):
            kb.dma("sp", bm[:], cx.b_modT[l], reads=[cx.b_modT], writes=[bm])
            for g in range(6):
                wb = wbufs[wi % 2]
                wi += 1
                src = cx.w_mod[l, :, g * 1024:(g + 1) * 1024].rearrange("(kc p) c -> p kc c", p=128)
                kb.dma("sp", wb[:], src, reads=[cx.w_mod], writes=[wb])
                for j in range(8):
                    jj = g * 8 + j
                    fns = []
                    for kc in range(8):
                        fns.append(lambda kc=kc, j=j, jj=jj, wb=wb: nc.tensor.matmul(
                            ps[:, 2 * jj:2 * jj + 2], lhsT=wb[:, kc, j * 128:(j + 1) * 128], rhs=sc[:, kc, :],
                            start=(kc == 0), stop=(kc == 7)))
                    kb.mm(fns, reads=[wb, sc], writes=[ps])
            m = modT[l]
            kb.op("dve", lambda m=m: nc.vector.tensor_tensor(
                out=m[:], in0=ps[:].rearrange("p (j c) -> p j c", c=2),
                in1=bm[:].unsqueeze(2).broadcast_to([128, 48, 2]), op=ALU.add),
                reads=[ps, bm], writes=[m])
            kb.dma("sp", cx.MODT[l], m[:].rearrange("p j c -> p (j c)"), reads=[m], writes=[cx.MODT])


def stage_modulate(cx, kb, nc, src_dram, modv, shift_idx, scale_idx, hxT, hx32=None):
    with kb.scope():
        onep = kb.sbuf("onep", [128, 8, 2], F32)
        kb.op("dve", lambda: nc.vector.tensor_scalar(
            out=onep[:], in0=modv[:, scale_idx * 8:(scale_idx + 1) * 8, :], scalar1=1.0, scalar2=None, op0=ALU.add),
            reads=[modv], writes=[onep])
        xb = [kb.sbuf("xb%d" % i, [128, 8, 512], F32) for i in range(2)]
        for bi, (t0, nb) in enumerate(BLKS):
            col = blkcol(t0)
            x = xb[bi % 2]
            kb.dma("sp", x[:, :, :nb], src_dram[:, t0:t0 + nb].rearrange("(kc p) t -> p kc t", p=128),
                   reads=[src_dram], writes=[x])
            for kc in range(8):
                eng = "dve" if kc % 2 == 0 else "pool"
                h = nc.vector if eng == "dve" else nc.gpsimd
                kb.op(eng, lambda h=h, kc=kc, x=x: h.tensor_scalar(
                    out=hxT[:, kc, t0:t0 + nb], in0=x[:, kc, :nb],
                    scalar1=onep[:, kc, col:col + 1], scalar2=modv[:, shift_idx * 8 + kc, col:col + 1],
                    op0=ALU.mult, op1=ALU.add), reads=[x, onep, modv], writes=[hxT])


FM_GROUPS = [
    ("a_q", "QA", "copy"), ("a_g", "GA", "silu"), ("a_lr", "LR1", "copy32"),
    ("b_q", "QB", "copy"), ("b_k", "KB_", "copy"),
    ("c_q", "QC", "rope"), ("c_k", "KC", "rope"),
    ("d_q", "QD", "silu"), ("d_g", "GD", "silu"), ("gate", "GATE", "sigmoid"),
]


def stage_inproj_fm(cx, kb, nc, l, hxT, only=None):
    with kb.scope():
        wsl = [kb.sbuf("wsl%d" % i, [128, 8, 512], BF16) for i in range(2)]
        stg = [kb.sbuf("stg%d" % i, [128, T], BF16) for i in range(2)]
        stg32 = kb.sbuf("stg32", [32, T], F32)
        pss = [kb.psum("psf%d" % i, [128, 512]) for i in range(4)]
        psr = [kb.psum("psr%d" % i, [128, 512]) for i in range(2)]
        cosT = kb.sbuf("cosT", [128, T], F32)
        sinT = kb.sbuf("sinT", [128, T], F32)
        ptm = kb.sbuf("ptm", [128, 128], BF16)
        tmpb = [kb.sbuf("tmpb%d" % i, [128, 512], BF16) for i in range(2)]
        t1 = [kb.sbuf("t1_%d" % i, [128, 512], F32) for i in range(2)]
        t2 = [kb.sbuf("t2_%d" % i, [128, 512], F32) for i in range(2)]
        ones = kb.sbuf("ones1", [1, T], F32)
        kb.dma("sp", cosT[:], cx.rope_cos[:], reads=[cx.rope_cos], writes=[cosT])
        kb.dma("sp", sinT[:], cx.rope_sin[:], reads=[cx.rope_sin], writes=[sinT])
        kb.dma("pool", ptm[:], cx.rope_pt[:], reads=[cx.rope_pt], writes=[ptm])
        kb.op("dve", lambda: nc.vector.memset(ones[:], 1.0), writes=[ones])
        kb.dma("sp", cx.LR1[32:33, :], ones[:], reads=[ones], writes=[], acc_writes=[cx.LR1])
        nsl = 0
        nps = 0
        nst = 0
        nrp = 0
        for (name, dest, kind) in FM_GROUPS:
            if only is not None and name not in only:
                continue
            c0, nc_ = COLS[name]
            dbuf = getattr(cx, dest)
            for s0 in range(0, nc_, 512):
                sw = min(512, nc_ - s0)
                w = wsl[nsl % 2]
                nsl += 1
                kb.dma("pool", w[:, :, :sw],
                       cx.w_in[l, :, c0 + s0:c0 + s0 + sw].rearrange("(kc p) c -> p kc c", p=128),
                       reads=[cx.w_in], writes=[w])
                for m0 in range(0, sw, 128):
                    mw = min(128, sw - m0)
                    if kind == "copy32":
                        st = stg32
                    else:
                        st = stg[nst % 2]
                        nst += 1
                    for bi, (t0, nb) in enumerate(BLKS):
                        ps = pss[nps % 4]
                        nps += 1
                        fns = [(lambda kc=kc, ps=ps, w=w: nc.tensor.matmul(
                            ps[:mw, :nb], lhsT=w[:, kc, m0:m0 + mw], rhs=hxT[:, kc, t0:t0 + nb],
                            start=(kc == 0), stop=(kc == 7))) for kc in range(8)]
                        kb.mm(fns, reads=[w, hxT], writes=[ps])
                        o = st[:mw, t0:t0 + nb]
                        if kind in ("copy", "copy32"):
                            if nps % 2 == 0:
                                kb.op("dve", lambda ps=ps, o=o: nc.vector.tensor_copy(out=o, in_=ps[:mw, :nb]),
                                      reads=[ps], writes=[st])
                            else:
                                kb.op("act", lambda ps=ps, o=o: nc.scalar.copy(out=o, in_=ps[:mw, :nb]),
                                      reads=[ps], writes=[st])
                        elif kind == "silu":
                            kb.op("act", lambda ps=ps, o=o: nc.scalar.activation(out=o, in_=ps[:mw, :nb], func=AF.Silu),
                                  reads=[ps], writes=[st])
                        elif kind == "sigmoid":
                            kb.op("act", lambda ps=ps, o=o: nc.scalar.activation(out=o, in_=ps[:mw, :nb], func=AF.Sigmoid),
                                  reads=[ps], writes=[st])
                        elif kind == "rope":
                            tb = tmpb[nrp % 2]
                            a1 = t1[nrp % 2]
                            a2 = t2[nrp % 2]
                            pr = psr[nrp % 2]
                            nrp += 1
                            kb.op("act", lambda ps=ps, tb=tb: nc.scalar.copy(out=tb[:mw, :nb], in_=ps[:mw, :nb]),
                                  reads=[ps], writes=[tb])
                            kb.mm([lambda pr=pr, tb=tb: nc.tensor.matmul(pr[:mw, :nb], lhsT=ptm[:mw, :mw], rhs=tb[:mw, :nb],
                                                                         start=True, stop=True)],
                                  reads=[ptm, tb], writes=[pr])
                            kb.op("dve", lambda tb=tb, a1=a1: nc.vector.tensor_tensor(
                                out=a1[:mw, :nb], in0=tb[:mw, :nb], in1=cosT[:mw, t0:t0 + nb], op=ALU.mult),
                                reads=[tb, cosT], writes=[a1])
                            kb.op("dve", lambda pr=pr, a2=a2: nc.vector.tensor_tensor(
                                out=a2[:mw, :nb], in0=pr[:mw, :nb], in1=sinT[:mw, t0:t0 + nb], op=ALU.mult),
                                reads=[pr, sinT], writes=[a2])
                            kb.op("pool", lambda a1=a1, a2=a2, o=o: nc.gpsimd.tensor_tensor(
                                out=o, in0=a1[:mw, :nb], in1=a2[:mw, :nb], op=ALU.add),
                                reads=[a1, a2], writes=[st])
                    kb.dma("sp", dbuf[s0 + m0:s0 + m0 + mw, :], st[:mw, :], reads=[st], writes=[], acc_writes=[dbuf])


TM_GROUPS = [
    (256, 512, "KVA_t", 0, "copy"), (768, 256, "KVA_t", 512, "copy"),
    (2592, 512, "VB_t", 0, "copy"), (3744, 128, "VC_t", 0, "copy"),
    (5408, 512, "VD_t", 0, "copy"),
    (4384, 512, None, 0, "hgf"), (4896, 512, None, 512, "hgf"),
]


def stage_inproj_tm(cx, kb, nc, l, hxT, lbt, omlt, only=None):
    with kb.scope():
        wsl = [kb.sbuf("wtl%d" % i, [128, 8, 512], BF16) for i in range(2)]
        pss = [kb.psum("pst%d" % i, [128, 512]) for i in range(4)]
        stb = [kb.sbuf("stb%d" % i, [128, 512], BF16) for i in range(4)]
        e1 = [kb.sbuf("e1_%d" % i, [128, 512], F32) for i in range(2)]
        r1 = [kb.sbuf("r1_%d" % i, [128, 512], F32) for i in range(2)]
        lg = [kb.sbuf("lg_%d" % i, [128, 512], F32) for i in range(2)]
        kk = [kb.sbuf("kk_%d" % i, [128, 512], BF16) for i in range(2)]
        nsl = nps = nst = nh = 0
        for (c0, ncol, dest, doff, kind) in TM_GROUPS:
            if only is not None and kind not in only:
                continue
            w = wsl[nsl % 2]
            nsl += 1
            kb.dma("pool", w[:, :, :ncol], cx.w_in[l, :, c0:c0 + ncol].rearrange("(kc p) c -> p kc c", p=128),
                   reads=[cx.w_in], writes=[w])
            for ti in range(NT):
                ts = slice(ti * 128, (ti + 1) * 128)
                ps = pss[nps % 4]
                nps += 1
                fns = [(lambda kc=kc, ps=ps, w=w: nc.tensor.matmul(
                    ps[:, :ncol], lhsT=hxT[:, kc, ts], rhs=w[:, kc, :ncol],
                    start=(kc == 0), stop=(kc == 7))) for kc in range(8)]
                kb.mm(fns, reads=[w, hxT], writes=[ps])
                if kind == "copy":
                    st = stb[nst % 4]
                    nst += 1
                    if nst % 2 == 0:
                        kb.op("dve", lambda ps=ps, st=st: nc.vector.tensor_copy(out=st[:, :ncol], in_=ps[:, :ncol]),
                              reads=[ps], writes=[st])
                    else:
                        kb.op("act", lambda ps=ps, st=st: nc.scalar.copy(out=st[:, :ncol], in_=ps[:, :ncol]),
                              reads=[ps], writes=[st])
                    db = getattr(cx, dest)
                    kb.dma("sp", db[:, ti, doff:doff + ncol], st[:, :ncol], reads=[st], writes=[], acc_writes=[db])
                else:
                    e = e1[nh % 2]
                    r = r1[nh % 2]
                    g = lg[nh % 2]
                    k = kk[nh % 2]
                    nh += 1
                    cs = slice(doff, doff + 512)
                    kb.op("act", lambda ps=ps, e=e: nc.scalar.activation(out=e[:], in_=ps[:], func=AF.Exp, scale=-1.0),
                          reads=[ps], writes=[e])
                    kb.op("pool", lambda e=e: nc.gpsimd.tensor_scalar(
                        out=e[:], in0=e[:], scalar1=1.0, scalar2=None, op0=ALU.add),
                        reads=[e], writes=[e])
                    kb.op("dve", lambda e=e, r=r: nc.vector.reciprocal(out=r[:], in_=e[:]),
                          reads=[e], writes=[r])
                    kb.op("dve", lambda r=r: nc.vector.tensor_tensor(out=r[:], in0=r[:], in1=omlt[:, cs], op=ALU.mult),
                          reads=[r, omlt], writes=[r])
                    kb.op("pool", lambda r=r: nc.gpsimd.tensor_tensor(out=r[:], in0=r[:], in1=lbt[:, cs], op=ALU.add),
                          reads=[r, lbt], writes=[r])
                    kb.op("act", lambda r=r, g=g: nc.scalar.activation(out=g[:], in_=r[:], func=AF.Ln),
                          reads=[r], writes=[g])
                    kb.op("pool", lambda r=r, k=k: nc.gpsimd.tensor_scalar(
                        out=k[:], in0=r[:], scalar1=-1.0, scalar2=1.0, op0=ALU.mult, op1=ALU.add),
                        reads=[r], writes=[k])
                    kb.dma("sp", cx.LGD_t[:, ti, cs], g[:], reads=[g], writes=[], acc_writes=[cx.LGD_t])
                    kb.dma("sp", cx.KD_t[:, ti, cs], k[:], reads=[k], writes=[], acc_writes=[cx.KD_t])


def stage_lb(cx, kb, nc, lbt, omlt):
    with kb.scope():
        gam = kb.sbuf("gam", [128, 2, 2, 512], F32)
        kb.dma("sp", gam[:].rearrange("p a b c -> p (a b c)"),
               cx.hg_gamma[:].rearrange("a b c -> (a b c)").partition_broadcast(128),
               reads=[cx.hg_gamma], writes=[gam])
        d = kb.sbuf("gamd", [128, 2, 512], F32)
        kb.op("dve", lambda: nc.vector.tensor_tensor(out=d[:], in0=gam[:, :, 0, :], in1=gam[:, :, 1, :], op=ALU.subtract),
              reads=[gam], writes=[d])
        kb.op("act", lambda: nc.scalar.activation(out=d[:], in_=d[:], func=AF.Exp), reads=[d], writes=[d])
        l1 = lbt[1]
        kb.op("dve", lambda: nc.vector.tensor_scalar(
            out=d[:], in0=d[:], scalar1=1.0, scalar2=None, op0=ALU.add), reads=[d], writes=[d])
        kb.op("dve", lambda: nc.vector.reciprocal(out=l1[:], in_=d[:].rearrange("p a c -> p (a c)")),
              reads=[d], writes=[l1])
        l0 = lbt[0]
        kb.op("dve", lambda: nc.vector.memset(l0[:], 0.0), writes=[l0])
        for l in range(2):
            o = omlt[l]
            lb = lbt[l]
            kb.op("dve", lambda o=o, lb=lb: nc.vector.tensor_scalar(
                out=o[:], in0=lb[:], scalar1=-1.0, scalar2=1.0, op0=ALU.mult, op1=ALU.add),
                reads=[lb], writes=[o])


def stage_gla_gate(cx, kb, nc, l):
    with kb.scope():
        wlr = kb.sbuf("wlr", [33, 512], F32)
        lr1 = kb.sbuf("lr1", [33, T], F32)
        kb.op("dve", lambda: nc.vector.memset(wlr[:], 0.0), writes=[wlr])
        kb.dma("sp", wlr[0:16, 0:256], cx.gla_lr_w[l, 0], reads=[cx.gla_lr_w], writes=[wlr])
        kb.dma("sp", wlr[16:32, 256:512], cx.gla_lr_w[l, 1], reads=[cx.gla_lr_w], writes=[wlr])
        kb.dma("sp", wlr[32:33, :], cx.gla_lr_b[l].rearrange("a c -> (a c)").unsqueeze(0),
               reads=[cx.gla_lr_b], writes=[wlr])
        kb.dma("sp", lr1[:], cx.LR1[:], reads=[cx.LR1], writes=[lr1])
        pss = [kb.psum("psg%d" % i, [128, 512]) for i in range(2)]
        ee = [kb.sbuf("ge%d" % i, [128, 512], F32) for i in range(2)]
        gg = [kb.sbuf("gg%d" % i, [128, 512], F32) for i in range(2)]
        for ti in range(NT):
            ts = slice(ti * 128, (ti + 1) * 128)
            ps = pss[ti % 2]
            e = ee[ti % 2]
            g = gg[ti % 2]
            kb.mm([lambda ps=ps: nc.tensor.matmul(ps[:], lhsT=lr1[:, ts], rhs=wlr[:], start=True, stop=True)],
                  reads=[lr1, wlr], writes=[ps])
            kb.op("act", lambda ps=ps, e=e: nc.scalar.activation(out=e[:], in_=ps[:], func=AF.Exp, scale=-1.0),
                  reads=[ps], writes=[e])
            kb.op("act", lambda e=e, g=g: nc.scalar.activation(out=g[:], in_=e[:], func=AF.Ln, bias=1.0),
                  reads=[e], writes=[g])
            kb.dma("sp", cx.LGA_t[:, ti, :], g[:], reads=[g], writes=[], acc_writes=[cx.LGA_t])

SCAN_CFG = {
    "gla": dict(H=4, dk=64, qsrc="QA", qscale=0.125, ksrc="KVA_t", kz=0, kcol0=0, vsrc="KVA_t", vcol0=256,
                gsrc="LGA_t", gz=256, gate="GA", dest="BR_A", mask="mg"),
    "hgrn": dict(H=4, dk=128, qsrc="QD", qscale=1.0, ksrc="KD_t", kz=512, kcol0=0, vsrc="VD_t", vcol0=0,
                 gsrc="LGD_t", gz=512, gate="GD", dest="BR_D", mask="mh"),
}


def view(b):
    return Buf(b.t, b.name + "_v")


def stage_scan(cx, kb, nc, which, heads=None):
    cfg = SCAN_CFG[which]
    H, dk = cfg["H"], cfg["dk"]
    dv = 128
    qsrc, ksrc, vsrc, gsrc = (getattr(cx, cfg[k]) for k in ("qsrc", "ksrc", "vsrc", "gsrc"))
    gate, dest = getattr(cx, cfg["gate"]), getattr(cx, cfg["dest"])
    mq_d, mk_d = getattr(cx, cfg["mask"] + "_mq"), getattr(cx, cfg["mask"] + "_mk")
    with kb.scope():
        mq = kb.sbuf("mq", [128, 2, 130], F32)
        mk = kb.sbuf("mk", [128, 2, 128], F32)
        ma = kb.sbuf("ma", [128, 2, 128], F32)
        ident = kb.sbuf("ident", [128, 128], BF16)
        onesdv = kb.sbuf("onesdv", [128, 128], F32)
        kb.dma("sp", mq[:], mq_d[:], reads=[mq_d], writes=[mq])
        kb.dma("sp", mk[:], mk_d[:], reads=[mk_d], writes=[mk])
        kb.dma("sp", ma[:], cx.m_ma[:], reads=[cx.m_ma], writes=[ma])
        mau = kb.sbuf("mau", [128, 2, 128], U32)
        kb.dma("sp", mau[:], cx.m_mau[:], reads=[cx.m_mau], writes=[mau])
        kb.dma("pool", ident[:], cx.m_ident[:], reads=[cx.m_ident], writes=[ident])
        kb.op("dve", lambda: nc.vector.memset(onesdv[:], 1.0 / dv), writes=[onesdv])
        vt = kb.sbuf("vt", [128, NT, dv], BF16)
        qT = kb.sbuf("qT", [128, T], BF16)
        gsil = kb.sbuf("gsil", [128, T], BF16)
        obr = kb.sbuf("obr", [128, T], BF16)
        gts = [kb.sbuf("gt%d" % i, [128, NT, dk], F32) for i in range(2)]
        kts = [kb.sbuf("kt%d" % i, [128, NT, dk], BF16) for i in range(2)]
        qt = [kb.sbuf("qt%d" % z, [128, T], BF16) for z in range(2)]
        atm = [kb.sbuf("atm%d" % z, [128, NT, 128], BF16) for z in range(2)]
        Sp = [kb.sbuf("Sp%d" % z, [128, 2 * NT, dv], BF16) for z in range(2)]
        for z in range(2):
            kb.op("pool", lambda z=z: nc.gpsimd.memset(atm[z][:], 0.0), writes=[atm[z]])
        Sst = [kb.sbuf("Sst%d" % i, [128, dv], F32) for i in range(2)]
        R = 3
        eq = [kb.sbuf("eq%d" % i, [128, 128], F32) for i in range(R)]
        ek = [kb.sbuf("ek%d" % i, [128, 128], F32) for i in range(R)]
        ekh = [kb.sbuf("ekh%d" % i, [128, 128], F32) for i in range(R)]
        ktl = [kb.sbuf("ktl%d" % i, [128, 128], BF16) for i in range(R)]
        khat = [kb.sbuf("khat%d" % i, [128, 256], BF16) for i in range(R)]
        ktr = [kb.sbuf("ktr%d" % i, [128, 128], F32) for i in range(R)]
        atr = [kb.sbuf("atr%d" % i, [128, 128], F32) for i in range(R)]
        usb = [kb.sbuf("usb%d" % i, [128, 256], F32) for i in range(R)]
        osb = [kb.sbuf("osb%d" % i, [128, 512], F32) for i in range(2)]
        cm = kb.sbuf("cm", [128, 2], F32)
        kb.op("dve", lambda: nc.vector.memset(cm[:], 0.0), writes=[cm])
        kb.op("dve", lambda: nc.vector.memset(cm[0:64, 0:1], 1.0), writes=[cm])
        kb.op("dve", lambda: nc.vector.memset(cm[64:128, 1:2], 1.0), writes=[cm])
        er2 = [kb.sbuf("er%d" % i, [128, 2], F32) for i in range(R)]
        ed2 = [kb.sbuf("ed%d" % i, [128, 2], F32) for i in range(R)]
        psA = [kb.psum("psA%d" % i, [128, 512]) for i in range(2)]
        psB = [kb.psum("psB%d" % i, [128, 512]) for i in range(2)]
        psA_ec = [view(p) for p in psA]
        import os
        if True:
            psK = [kb.psum("psK%d" % i, [128, 512]) for i in range(2)]
        else:
            psK = psA
        psA_kt = [view(p) for p in psK]
        psA_e2 = [view(p) for p in psK]
        psB_u = [view(p) for p in psB]
        psB_at = [view(p) for p in psB]
        psO = [kb.psum("psO%d" % i, [128, 512]) for i in range(1)] * 2
        psM = [kb.psum("psM%d" % i, [128, 512]) for i in range(1)] * 2
        sq = [kb.sbuf("sq%d" % i, [128, 512], F32) for i in range(2)]
        sd = [kb.sbuf("sd%d" % i, [128, 512], F32) for i in range(2)]
        on = [kb.sbuf("on%d" % i, [128, 512], F32) for i in range(2)]
        eps_t = kb.sbuf("eps_t", [128, 1], F32)
        kb.op("dve", lambda: nc.vector.memset(eps_t[:], 1e-6), writes=[eps_t])

        it = 0
        ghz = 0
        import os
        for h in (range(H) if heads is None else heads):
            vc0 = cfg["vcol0"] + h * dv
            kb.dma("sp", vt[:], vsrc[:, :, vc0:vc0 + dv], reads=[vsrc], writes=[vt])
            kb.dma("sp", qT[:dk, :], qsrc[h * dk:(h + 1) * dk, :], reads=[qsrc], writes=[qT])
            kb.dma("sp", gsil[:], gate[h * dv:(h + 1) * dv, :], reads=[gate], writes=[gsil])
            for z in range(2):
                gt = gts[ghz % 2]
                kt = kts[ghz % 2]
                ghz += 1
                gc0 = z * cfg["gz"] + h * dk
                kc0 = cfg["kcol0"] + z * cfg["kz"] + h * dk
                kb.dma("sp", gt[:], gsrc[:, :, gc0:gc0 + dk], reads=[gsrc], writes=[gt])
                if os.environ.get("KTMEMSET"):
                    kb.op("dve", lambda kt=kt: nc.vector.memset(kt[:], 0.0), writes=[kt])
                kb.dma("sp", kt[:], ksrc[:, :, kc0:kc0 + dk], reads=[ksrc], writes=[kt])
                order = [32, 33] + list(range(32)) if z == 0 else [33, 32] + list(range(31, -1, -1))
                import os
                DBG = os.environ.get("SCAN_DBG", "")
                if DBG == "loads":
                    continue
                if DBG.startswith("nt"):
                    order = order[:int(DBG[2:])]
                S = Sst[0]
                kb.op("dve", lambda S=S: nc.vector.memset(S[:], 0.0), writes=[S])
                si = 0
                for ti in order:
                    a = it % 2
                    r = it % R
                    it += 1
                    ts = slice(ti * 128, (ti + 1) * 128)
                    pa, pb = psA[a], psB[a]
                    pk = psK[a]
                    kb.mm([lambda pa=pa, gt=gt: nc.tensor.matmul(pa[:dk, 0:130], lhsT=gt[:, ti, :], rhs=mq[:, z, :],
                                                                 start=True, stop=True)],
                          reads=[gt, mq], writes=[psA_ec[a]])
                    kb.mm([lambda pk=pk, kt=kt: nc.tensor.matmul(pk[:dk, 256:384], lhsT=(ident[:, :dk] if os.environ.get("A2ID") else kt[:, ti, :]), rhs=ident[:],
                                                                 start=True, stop=True)],
                          reads=[kt, ident], writes=[psA_kt[a]])
                    kb.mm([lambda pk=pk, gt=gt: nc.tensor.matmul(pk[:, 384:384 + dk], lhsT=mk[:, z, :], rhs=gt[:, ti, :],
                                                                 start=True, stop=True)],
                          reads=[gt, mk], writes=[psA_e2[a]])
                    kb.op("act", lambda pa=pa, o=eq[r]: nc.scalar.activation(out=o[:dk, :], in_=pa[:dk, 0:128], func=AF.Exp),
                          reads=[psA_ec[a]], writes=[eq[r]])
                    kb.op("act", lambda pa=pa, o=ek[r]: nc.scalar.activation(out=o[:dk, :], in_=pa[:dk, 0:128], func=AF.Exp, scale=-1.0),
                          reads=[psA_ec[a]], writes=[ek[r]])
                    kb.op("act", lambda pa=pa, o=er2[r]: nc.scalar.activation(out=o[:dk, :], in_=pa[:dk, 128:130], func=AF.Exp, scale=0.5),
                          reads=[psA_ec[a]], writes=[er2[r]])
                    kb.op("act", lambda pa=pa, o=ed2[r]: nc.scalar.activation(out=o[:dk, :], in_=pa[:dk, 128:130], func=AF.Exp),
                          reads=[psA_ec[a]], writes=[ed2[r]])
                    kb.op("act", lambda pk=pk, o=ekh[r]: nc.scalar.activation(out=o[:, :dk], in_=pk[:, 384:384 + dk], func=AF.Exp),
                          reads=[psA_e2[a]], writes=[ekh[r]])
                    qz = qt[z]
                    kb.op("dve", lambda qz=qz, e=eq[r]: nc.vector.scalar_tensor_tensor(
                        out=qz[:dk, ts], in0=qT[:dk, ts], scalar=cfg["qscale"], in1=e[:dk, :], op0=ALU.mult, op1=ALU.mult),
                        reads=[qT, eq[r]], writes=[], acc_writes=[qz])
                    kb.op("act", lambda pk=pk, o=ktr[r]: nc.scalar.copy(out=o[:dk, :], in_=pk[:dk, 256:384]),
                          reads=[psA_kt[a]], writes=[ktr[r]])
                    kb.op("dve", lambda e=ek[r], o=ktl[r], kr=ktr[r]: nc.vector.tensor_tensor(
                        out=o[:dk, :], in0=kr[:dk, :], in1=e[:dk, :], op=ALU.mult),
                        reads=[ktr[r], ek[r]], writes=[ktl[r]])
                    for c in range(2):
                        kb.op("dve", lambda kt=kt, e=ekh[r], o=khat[r], c=c: nc.vector.scalar_tensor_tensor(
                            out=o[:, c * 128:c * 128 + dk], in0=kt[:, ti, :], scalar=cm[:, c:c + 1], in1=e[:, :dk],
                            op0=ALU.mult, op1=ALU.mult),
                            reads=[kt, ekh[r], cm], writes=[], acc_writes=[khat[r]])
                    kb.mm([(lambda c=c, pb=pb, kh=khat[r]: nc.tensor.matmul(
                        pb[:dk, c * 128:(c + 1) * 128], lhsT=kh[:, c * 128:c * 128 + dk],
                        rhs=vt[:, ti, :], start=True, stop=True)) for c in range(2)],
                        reads=[khat[r], vt], writes=[psB_u[a]])
                    kb.mm([lambda pb=pb, k_=ktl[r], qz=qz: nc.tensor.matmul(
                        pb[:, 256:384], lhsT=k_[:dk, :], rhs=qz[:dk, ts], start=True, stop=True)],
                        reads=[ktl[r], qz], writes=[psB_at[a]])
                    az = atm[z]
                    kb.op("act", lambda pb=pb, o=atr[r]: nc.scalar.copy(out=o[:], in_=pb[:, 256:384]),
                          reads=[psB_at[a]], writes=[atr[r]])
                    kb.op("dve", lambda az=az, o=atr[r]: nc.vector.copy_predicated(
                        out=az[:, ti, :], mask=mau[:, z, :], data=o[:]),
                        reads=[atr[r], mau], writes=[], acc_writes=[az])
                    kb.op("act", lambda pb=pb, o=usb[r]: nc.scalar.copy(out=o[:dk, :], in_=pb[:dk, 0:256]),
                          reads=[psB_u[a]], writes=[usb[r]])
                    spz = Sp[z]
                    for c in ((0, 1) if z == 0 else (1, 0)):
                        ci = 2 * ti + c
                        kb.op("act", lambda S=S, spz=spz, ci=ci, e=er2[r], c=c: nc.scalar.activation(
                            out=spz[:dk, ci, :], in_=S[:dk, :], func=AF.Copy, scale=e[:dk, c:c + 1]),
                            reads=[S, er2[r]], writes=[], acc_writes=[spz])
                        S2 = Sst[(si + 1) % 2]
                        si += 1
                        kb.op("dve", lambda S=S, S2=S2, e=ed2[r], u=usb[r], c=c: nc.vector.scalar_tensor_tensor(
                            out=S2[:dk, :], in0=S[:dk, :], scalar=e[:dk, c:c + 1], in1=u[:dk, c * 128:(c + 1) * 128],
                            op0=ALU.mult, op1=ALU.add),
                            reads=[S, ed2[r], usb[r]], writes=[S2])
                        S = S2
            groups = [list(range(g * 4, g * 4 + 4)) for g in range(8)] + [[32, 33]]
            if DBG:
                groups = []
            for gi, tiles in enumerate(groups):
                po = psO[gi % 2]
                pm = psM[gi % 2]
                nb = len(tiles) * 128
                t0 = tiles[0] * 128
                fns = []
                for k_, ti in enumerate(tiles):
                    cs = k_ * 128
                    for z in range(2):
                        fns.append(lambda po=po, ti=ti, cs=cs, z=z: nc.tensor.matmul(
                            po[:, cs:cs + 128], lhsT=vt[:, ti, :], rhs=atm[z][:, ti, :], start=(z == 0), stop=False))
                        for c in range(2):
                            fns.append(lambda po=po, ti=ti, cs=cs, z=z, c=c: nc.tensor.matmul(
                                po[:, cs + c * 64:cs + (c + 1) * 64], lhsT=Sp[z][:dk, 2 * ti + c, :],
                                rhs=qt[z][:dk, ti * 128 + c * 64:ti * 128 + (c + 1) * 64],
                                start=False, stop=(z == 1)))
                kb.mm(fns, reads=[vt, atm[0], atm[1], Sp[0], Sp[1], qt[0], qt[1]], writes=[po])
                s_, d_, o_ = sq[gi % 2], sd[gi % 2], on[gi % 2]
                kb.op("act", lambda po=po, s_=s_: nc.scalar.activation(out=s_[:, :nb], in_=po[:, :nb], func=AF.Square),
                      reads=[po], writes=[s_])
                kb.mm([lambda pm=pm, s_=s_: nc.tensor.matmul(pm[:, :nb], lhsT=onesdv[:], rhs=s_[:, :nb], start=True, stop=True)],
                      reads=[onesdv, s_], writes=[pm])
                kb.op("act", lambda pm=pm, d_=d_: nc.scalar.activation(out=d_[:, :nb], in_=pm[:, :nb], func=AF.Sqrt, bias=eps_t[:]),
                      reads=[pm, eps_t], writes=[d_])
                kb.op("dve", lambda d_=d_: nc.vector.reciprocal(out=d_[:, :nb], in_=d_[:, :nb]), reads=[d_], writes=[d_])
                ob_ = osb[gi % 2]
                kb.op("act", lambda po=po, ob_=ob_: nc.scalar.copy(out=ob_[:, :nb], in_=po[:, :nb]), reads=[po], writes=[ob_])
                kb.op("dve", lambda ob_=ob_, d_=d_, o_=o_: nc.vector.tensor_tensor(
                    out=o_[:, :nb], in0=ob_[:, :nb], in1=d_[:, :nb], op=ALU.mult), reads=[ob_, d_], writes=[o_])
                kb.op("pool", lambda o_=o_: nc.gpsimd.tensor_tensor(
                    out=obr[:, t0:t0 + nb], in0=o_[:, :nb], in1=gsil[:, t0:t0 + nb], op=ALU.mult),
                    reads=[o_, gsil], writes=[], acc_writes=[obr])
            kb.dma("sp", dest[h * dv:(h + 1) * dv, :], obr[:], reads=[obr], writes=[], acc_writes=[dest])


def scan_masks():
    j = np.arange(128)[:, None]
    i = np.arange(128)[None, :]
    same = (j // 64 == i // 64).astype(np.float32)
    out = {}
    for name, gs in (("mg", -1.0 / 16.0), ("mh", 1.0)):
        mq = np.zeros((128, 2, 130), np.float32)
        mk = np.zeros((128, 2, 128), np.float32)
        mq[:, 0, :128] = gs * same * ((j <= i).astype(np.float32) - 0.5)
        mq[:, 1, :128] = gs * same * ((j >= i).astype(np.float32) - 0.5)
        for z in range(2):
            for c in range(2):
                mq[:, z, 128 + c] = gs * (np.arange(128) // 64 == c)
        mk[:, 0, :] = gs * same * (j > i)
        mk[:, 1, :] = gs * same * (j < i)
        out[name + "_mq"] = mq
        out[name + "_mk"] = mk
    ma = np.zeros((128, 2, 128), np.float32)
    ma[:, 0, :] = same * (j <= i)
    ma[:, 1, :] = same * (j >= i)
    out["m_ma"] = ma
    out["m_mau"] = ma.astype(np.uint32)
    out["m_ident"] = np.eye(128, dtype=np.float32)
    return out


def stage_wa(cx, kb, nc, l, groups=None):
    with kb.scope():
        wm = kb.sbuf("wm", [128, 2, 128], BF16)
        kb.dma("pool", wm[:], cx.m_wa[:], reads=[cx.m_wa], writes=[wm])
        ones64 = kb.sbuf("ones64", [128, 64], BF16)
        kb.op("dve", lambda: nc.vector.memset(ones64[:], 1.0), writes=[ones64])
        sk = kb.sbuf("sk", [64, 8], F32)
        kb.dma("sp", sk[:], cx.wa_sink[l].partition_broadcast(64), reads=[cx.wa_sink], writes=[sk])
        kb.op("act", lambda: nc.scalar.activation(out=sk[:], in_=sk[:], func=AF.Exp), reads=[sk], writes=[sk])
        q4 = kb.sbuf("q4", [64, 4, T], BF16)
        kT = kb.sbuf("kTc", [64, T], BF16)
        vt = kb.sbuf("vtc", [128, NT, 64], BF16)
        ost = kb.sbuf("ost", [64, 4, T], BF16)
        pS = [kb.psum("pS%d" % i, [128, 512]) for i in range(2)]
        pO = [kb.psum("pO%d" % i, [128, 512]) for i in range(2)]
        pD = [kb.psum("pD%d" % i, [128, 512]) for i in range(2)]
        R = 3
        pt = [kb.sbuf("pt%d" % i, [128, 4, 128], BF16) for i in range(R)]
        ptm = [kb.sbuf("ptm%d" % i, [128, 4, 128], BF16) for i in range(R)]
        osb = [kb.sbuf("osbw%d" % i, [64, 4, 128], F32) for i in range(2)]
        dsb = [kb.sbuf("dsbw%d" % i, [64, 4, 128], F32) for i in range(2)]
        it = 0
        for g in (range(2) if groups is None else groups):
            kb.dma("sp", q4[:], cx.QC[g * 256:(g + 1) * 256, :].rearrange("(hh d) t -> d hh t", d=64),
                   reads=[cx.QC], writes=[q4])
            kb.dma("sp", kT[:], cx.KC[g * 64:(g + 1) * 64, :], reads=[cx.KC], writes=[kT])
            kb.dma("sp", vt[:], cx.VC_t[:, :, g * 64:(g + 1) * 64], reads=[cx.VC_t], writes=[vt])
            for qb in range(NT):
                if qb < 32:
                    keys = []
                    if qb > 0:
                        keys.append((qb - 1, 0))
                    keys.append((qb, None))
                    if qb < 31:
                        keys.append((qb + 1, 1))
                    keys += [(32, None), (33, None)]
                else:
                    keys = [(32, None), (33, None)]
                qs = slice(qb * 128, (qb + 1) * 128)
                a = qb % 2
                po, pd = pO[a], pD[a]
                nk = len(keys)
                for ki, (kt_i, mi) in enumerate(keys):
                    ps = pS[it % 2]
                    p_ = pt[it % R]
                    pm_ = ptm[it % R]
                    it += 1
                    kb.mm([lambda ps=ps, kt_i=kt_i: nc.tensor.matmul(
                        ps[:, :], lhsT=kT[:, kt_i * 128:(kt_i + 1) * 128], rhs=q4[:, :, qs], start=True, stop=True)],
                        reads=[kT, q4], writes=[ps])
                    kb.op("act", lambda ps=ps, p_=p_: nc.scalar.activation(
                        out=p_[:].rearrange("p h q -> p (h q)"), in_=ps[:, :], func=AF.Exp, scale=0.125),
                        reads=[ps], writes=[p_])
                    src = p_
                    if mi is not None:
                        eng = "dve" if it % 2 == 0 else "pool"
                        h_ = nc.vector if eng == "dve" else nc.gpsimd
                        kb.op(eng, lambda h_=h_, p_=p_, pm_=pm_, mi=mi: h_.tensor_tensor(
                            out=pm_[:], in0=p_[:], in1=wm[:, mi, :].unsqueeze(1).broadcast_to([128, 4, 128]), op=ALU.mult),
                            reads=[p_, wm], writes=[pm_])
                        src = pm_
                    kb.mm([lambda po=po, src=src, kt_i=kt_i, ki=ki: nc.tensor.matmul(
                        po[:64, :], lhsT=vt[:, kt_i, :], rhs=src[:].rearrange("p h q -> p (h q)"),
                        start=(ki == 0), stop=(ki == nk - 1)),
                        lambda pd=pd, src=src, ki=ki: nc.tensor.matmul(
                        pd[:64, :], lhsT=ones64[:], rhs=src[:].rearrange("p h q -> p (h q)"),
                        start=(ki == 0), stop=(ki == nk - 1))],
                        reads=[vt, src, ones64], writes=[po, pd])
                o_, d_ = osb[a], dsb[a]
                kb.op("act", lambda po=po, o_=o_: nc.scalar.copy(out=o_[:].rearrange("p h q -> p (h q)"), in_=po[:64, :]),
                      reads=[po], writes=[o_])
                kb.op("act", lambda pd=pd, d_=d_: nc.scalar.copy(out=d_[:].rearrange("p h q -> p (h q)"), in_=pd[:64, :]),
                      reads=[pd], writes=[d_])
                kb.op("dve", lambda d_=d_: nc.vector.tensor_tensor(
                    out=d_[:], in0=d_[:], in1=sk[:, g * 4:(g + 1) * 4].unsqueeze(2).broadcast_to([64, 4, 128]), op=ALU.add),
                    reads=[d_, sk], writes=[d_])
                kb.op("dve", lambda d_=d_: nc.vector.reciprocal(out=d_[:], in_=d_[:]), reads=[d_], writes=[d_])
                kb.op("pool", lambda o_=o_, d_=d_: nc.gpsimd.tensor_tensor(
                    out=ost[:, :, qs], in0=o_[:], in1=d_[:], op=ALU.mult),
                    reads=[o_, d_], writes=[], acc_writes=[ost])
            kb.dma("sp", cx.BR_C[g * 256:(g + 1) * 256, :].rearrange("(hh d) t -> d hh t", d=64), ost[:],
                   reads=[ost], writes=[], acc_writes=[cx.BR_C])


def wa_masks():
    kk = np.arange(128)[:, None]
    qq = np.arange(128)[None, :]
    m = np.zeros((128, 2, 128), np.float32)
    m[:, 0, :] = (kk >= qq)
    m[:, 1, :] = (kk <= qq)
    return {"m_wa": m}


def na_patterns():
    W = 64
    tok = np.arange(128)
    lr, lc = tok // W, tok % W
    pats = []
    keys = []
    for m in range(32):
        qr = 2 * m + lr[None, :]
        qc = lc[None, :]
        rs = np.clip(qr - 4, 0, 56)
        cs = np.clip(qc - 8, 0, 48)
        lst = []
        for kt_i in range(32):
            kr = 2 * kt_i + lr[:, None]
            kc = lc[:, None]
            valid = (kr >= rs) & (kr < rs + 8) & (kc >= cs) & (kc < cs + 16)
            if not valid.any():
                continue
            ri = np.clip(kr - qr + 7, 0, 14)
            ci = np.clip(kc - qc + 15, 0, 30)
            if 2 <= m <= 29:
                pid = kt_i - m + 2
                if m == 2:
                    pats.append((valid, ri, ci))
            else:
                pid = len(pats)
                pats.append((valid, ri, ci))
            lst.append((kt_i, pid))
        keys.append(lst)
    return keys, pats


def na_consts():
    keys, pats = na_patterns()
    n0 = len(keys[0]) + len(keys[1])
    order = list(range(n0, n0 + 5)) + list(range(0, n0)) + list(range(n0 + 5, len(pats)))
    remap = {old: new for new, old in enumerate(order)}
    pats2 = [pats[o] for o in order]
    keys2 = []
    for m, lst in enumerate(keys):
        if 2 <= m <= 29:
            keys2.append(lst)
        else:
            keys2.append([(k, remap[p]) for (k, p) in lst])
    valid = np.stack([p[0] for p in pats2], 1).astype(np.float32)
    ri = np.stack([p[1] for p in pats2], 1)
    ci = np.stack([p[2] for p in pats2], 1)
    return keys2, valid, ri, ci


def stage_na(cx, kb, nc, l, heads=None):
    keys_tab, valid_np, _, _ = na_consts()
    npat = valid_np.shape[1]
    with kb.scope():
        vmask = kb.sbuf("vmask", [128, npat, 128], F32)
        kb.dma("sp", vmask[:], cx.m_navalid[:], reads=[cx.m_navalid], writes=[vmask])
        ones64 = kb.sbuf("ones64n", [128, 64], BF16)
        kb.op("dve", lambda: nc.vector.memset(ones64[:], 1.0), writes=[ones64])
        qT = kb.sbuf("qTn", [64, T], BF16)
        kT = kb.sbuf("kTn", [64, T], BF16)
        vt = kb.sbuf("vtn", [128, NT, 64], BF16)
        ost = kb.sbuf("ostn", [64, T], BF16)
        bias = kb.sbuf("biasn", [128, npat, 128], F32)
        EB = kb.sbuf("EBn", [128, npat, 128], BF16)
        pS = [kb.psum("pSn%d" % i, [128, 512]) for i in range(4)]
        pO = [kb.psum("pOn%d" % i, [128, 512]) for i in range(2)]
        pD = [kb.psum("pDn%d" % i, [128, 512]) for i in range(2)]
        R = 4
        pt = [kb.sbuf("ptn%d" % i, [128, 4, 128], BF16) for i in range(R)]
        osb = [kb.sbuf("osbn%d" % i, [64, 256], F32) for i in range(2)]
        it = 0
        for h in (range(8) if heads is None else heads):
            kb.dma("sp", bias[:], cx.na_bias[l, h], reads=[cx.na_bias], writes=[bias])
            kb.op("act", lambda: nc.scalar.activation(out=bias[:], in_=bias[:], func=AF.Exp), reads=[bias], writes=[bias])
            kb.op("pool", lambda: nc.gpsimd.tensor_tensor(out=EB[:], in0=bias[:], in1=vmask[:], op=ALU.mult),
                  reads=[bias, vmask], writes=[EB])
            kb.dma("sp", qT[:], cx.QB[h * 64:(h + 1) * 64, :], reads=[cx.QB], writes=[qT])
            kb.dma("sp", kT[:], cx.KB_[h * 64:(h + 1) * 64, :], reads=[cx.KB_], writes=[kT])
            kb.dma("sp", vt[:], cx.VB_t[:, :, h * 64:(h + 1) * 64], reads=[cx.VB_t], writes=[vt])
            for qb in range(NT):
                if qb < 32:
                    keys = list(keys_tab[qb]) + [(32, None), (33, None)]
                else:
                    keys = [(32, None), (33, None)]
                qs = slice(qb * 128, (qb + 1) * 128)
                a = qb % 2
                po = pO[a]
                pd = pD[a]
                nk = len(keys)
                grp = [keys[i:i + 4] for i in range(0, nk, 4)]
                done = 0
                for gk in grp:
                    ps = pS[it % 4]
                    p_ = pt[it % R]
                    it += 1
                    ng = len(gk)
                    kb.mm([(lambda ps=ps, j=j, kt_i=kt_i: nc.tensor.matmul(
                        ps[:, j * 128:(j + 1) * 128], lhsT=kT[:, kt_i * 128:(kt_i + 1) * 128], rhs=qT[:, qs],
                        start=True, stop=True)) for j, (kt_i, _) in enumerate(gk)],
                        reads=[kT, qT], writes=[ps])
                    kb.op("act", lambda ps=ps, p_=p_, ng=ng: nc.scalar.activation(
                        out=p_[:, :ng, :].rearrange("p h q -> p (h q)"), in_=ps[:, :ng * 128], func=AF.Exp, scale=0.125),
                        reads=[ps], writes=[p_])
                    nloc = sum(1 for (_, pid) in gk if pid is not None)
                    if nloc:
                        pid0 = gk[0][1]
                        eng = "dve" if it % 2 == 0 else "pool"
                        h_ = nc.vector if eng == "dve" else nc.gpsimd
                        kb.op(eng, lambda h_=h_, p_=p_, pid0=pid0, nloc=nloc: h_.tensor_tensor(
                            out=p_[:, :nloc, :], in0=p_[:, :nloc, :], in1=EB[:, pid0:pid0 + nloc, :], op=ALU.mult),
                            reads=[p_, EB], writes=[p_])
                    fns = []
                    for j, (kt_i, _) in enumerate(gk):
                        first = (done == 0)
                        last = (done == nk - 1)
                        fns.append(lambda po=po, p_=p_, j=j, kt_i=kt_i, first=first, last=last: nc.tensor.matmul(
                            po[:64, 0:128], lhsT=vt[:, kt_i, :], rhs=p_[:, j, :], start=first, stop=last))
                        fns.append(lambda pd=pd, p_=p_, j=j, first=first, last=last: nc.tensor.matmul(
                            pd[:64, 0:128], lhsT=ones64[:], rhs=p_[:, j, :], start=first, stop=last))
                        done += 1
                    kb.mm(fns, reads=[vt, p_, ones64], writes=[po, pd])
                o_ = osb[a]
                kb.op("act", lambda po=po, o_=o_: nc.scalar.copy(out=o_[:, 0:128], in_=po[:64, 0:128]), reads=[po], writes=[o_])
                kb.op("act", lambda pd=pd, o_=o_: nc.scalar.copy(out=o_[:, 128:256], in_=pd[:64, 0:128]), reads=[pd, o_], writes=[o_])
                kb.op("dve", lambda o_=o_: nc.vector.reciprocal(out=o_[:, 128:256], in_=o_[:, 128:256]), reads=[o_], writes=[o_])
                kb.op("pool", lambda o_=o_: nc.gpsimd.tensor_tensor(
                    out=ost[:, qs], in0=o_[:, 0:128], in1=o_[:, 128:256], op=ALU.mult),
                    reads=[o_], writes=[], acc_writes=[ost])
            kb.dma("sp", cx.BR_B[h * 64:(h + 1) * 64, :], ost[:], reads=[ost], writes=[], acc_writes=[cx.BR_B])

LN_EPS = 1e-5
MB = 256
MBLKS = [(i * MB, MB) for i in range(T // MB)]


def ln_setup(cx, kb, nc):
    ones = kb.sbuf("ln_ones", [128, 128], F32)
    kb.op("dve", lambda: nc.vector.memset(ones[:], 1.0 / D), writes=[ones])
    eps = kb.sbuf("ln_eps", [128, 1], F32)
    kb.op("dve", lambda: nc.vector.memset(eps[:], LN_EPS), writes=[eps])
    return ones, eps


def ln_block(kb, nc, r, nb, gam, bet, out, ones, eps, scr):
    rsq, ps1, ps2, mean, var = scr
    kb.op("act", lambda: nc.scalar.activation(out=rsq[:, :, :nb], in_=r[:, :, :nb], func=AF.Square), reads=[r], writes=[rsq])
    kb.mm([(lambda oc=oc: nc.tensor.matmul(ps1[:, :nb], lhsT=ones[:], rhs=r[:, oc, :nb], start=(oc == 0), stop=(oc == 7)))
           for oc in range(8)], reads=[ones, r], writes=[ps1])
    kb.mm([(lambda oc=oc: nc.tensor.matmul(ps2[:, :nb], lhsT=ones[:], rhs=rsq[:, oc, :nb], start=(oc == 0), stop=(oc == 7)))
           for oc in range(8)], reads=[ones, rsq], writes=[ps2])
    kb.op("act", lambda: nc.scalar.copy(out=mean[:, :nb], in_=ps1[:, :nb]), reads=[ps1], writes=[mean])
    kb.op("act", lambda: nc.scalar.copy(out=var[:, :nb], in_=ps2[:, :nb]), reads=[ps2], writes=[var])
    m2 = rsq
    kb.op("dve", lambda: nc.vector.tensor_tensor(out=m2[:, 0, :nb], in0=mean[:, :nb], in1=mean[:, :nb], op=ALU.mult),
          reads=[mean, rsq], writes=[rsq])
    kb.op("dve", lambda: nc.vector.tensor_tensor(out=var[:, :nb], in0=var[:, :nb], in1=m2[:, 0, :nb], op=ALU.subtract),
          reads=[var, rsq], writes=[var])
    kb.op("act", lambda: nc.scalar.activation(out=var[:, :nb], in_=var[:, :nb], func=AF.Sqrt, bias=eps[:]),
          reads=[var, eps], writes=[var])
    kb.op("dve", lambda: nc.vector.reciprocal(out=var[:, :nb], in_=var[:, :nb]), reads=[var], writes=[var])
    kb.op("dve", lambda: nc.vector.tensor_tensor(
        out=r[:, :, :nb], in0=r[:, :, :nb], in1=mean[:, :nb].unsqueeze(1).broadcast_to([128, 8, nb]), op=ALU.subtract),
        reads=[r, mean], writes=[r])
    kb.op("pool", lambda: nc.gpsimd.tensor_tensor(
        out=r[:, :, :nb], in0=r[:, :, :nb], in1=var[:, :nb].unsqueeze(1).broadcast_to([128, 8, nb]), op=ALU.mult),
        reads=[r, var], writes=[r])
    for oc in range(8):
        eng = "dve" if oc % 2 == 0 else "pool"
        h_ = nc.vector if eng == "dve" else nc.gpsimd
        kb.op(eng, lambda h_=h_, oc=oc: h_.tensor_scalar(
            out=out[:, oc, :nb], in0=r[:, oc, :nb], scalar1=gam[:, oc:oc + 1], scalar2=bet[:, oc:oc + 1],
            op0=ALU.mult, op1=ALU.add), reads=[r, gam, bet], writes=[out])


def load_ln_params(cx, kb, nc, l, which):
    gam = kb.sbuf("ln_g", [128, 8], F32)
    bet = kb.sbuf("ln_b", [128, 8], F32)
    kb.dma("sp", gam[:], cx.ln_gT[l, which], reads=[cx.ln_gT], writes=[gam])
    kb.dma("sp", bet[:], cx.ln_bT[l, which], reads=[cx.ln_bT], writes=[bet])
    return gam, bet


def stage_merge(cx, kb, nc, l, xsrc, modv):
    with kb.scope():
        ones, eps = ln_setup(cx, kb, nc)
        gam, bet = load_ln_params(cx, kb, nc, l, 0)
        wbr = kb.sbuf("wbr", [128, 4, 4, 1024], BF16)
        wout = kb.sbuf("wout", [128, 8, 1024], BF16)
        for i in range(4):
            kb.dma("pool", wbr[:, i], cx.w_branch[l, i].rearrange("(kc p) c -> p kc c", p=128), reads=[cx.w_branch], writes=[wbr])
        kb.dma("pool", wout[:], cx.w_out[l].rearrange("(kc p) c -> p kc c", p=128), reads=[cx.w_out], writes=[wout])
        wr = kb.sbuf("wr", [128, 8, 36], F32)
        kb.dma("sp", wr[:, :, 0:4], cx.moe_w_group[l].rearrange("(kc p) c -> p kc c", p=128), reads=[cx.moe_w_group], writes=[wr])
        kb.dma("sp", wr[:, :, 4:36], cx.moe_w_expert[l].rearrange("(kc p) c -> p kc c", p=128), reads=[cx.moe_w_expert], writes=[wr])
        rb = kb.sbuf("rb", [128, 36], F32)
        kb.dma("sp", rb[:, 0:4], cx.moe_b_group[l].partition_broadcast(128), reads=[cx.moe_b_group], writes=[rb])
        kb.dma("sp", rb[:, 4:36], cx.moe_b_expert[l].partition_broadcast(128), reads=[cx.moe_b_expert], writes=[rb])
        onep4 = kb.sbuf("onep4", [128, 8, 2], F32)
        kb.op("dve", lambda: nc.vector.tensor_scalar(out=onep4[:], in0=modv[:, 32:40, :], scalar1=1.0, scalar2=None, op0=ALU.add),
              reads=[modv], writes=[onep4])
        brt = [kb.sbuf("brt%d" % i, [128, 4, 4, MB], BF16) for i in range(1)]
        gtt = kb.sbuf("gtt", [128, 32, MB], BF16)
        xb = kb.sbuf("xbm", [128, 8, MB], F32)
        mrg = kb.sbuf("mrg", [128, 8, MB], BF16)
        r = kb.sbuf("rm", [128, 8, MB], F32)
        hx2b = kb.sbuf("hx2b", [128, 8, MB], BF16)
        rsq = kb.sbuf("rsqm", [128, 8, MB], F32)
        x1 = r
        hx2f = rsq
        mean = kb.sbuf("meanm", [128, MB], F32)
        var = kb.sbuf("varm", [128, MB], F32)
        csb = [kb.sbuf("csb%d" % i, [128, MB], F32) for i in range(4)]
        acc = kb.sbuf("accm", [128, MB], F32)
        mixs = kb.sbuf("mixs", [128, MB], F32)
        pbr = [kb.psum("pbr%d" % i, [128, MB]) for i in range(4)]
        pmx = kb.psum("pmx", [128, MB])
        ps1 = kb.psum("ps1m", [128, MB])
        ps2 = kb.psum("ps2m", [128, MB])
        prt = kb.psum("prt", [128, MB])
        brs = [cx.BR_A, cx.BR_B, cx.BR_C, cx.BR_D]
        lgt = kb.sbuf("lgt", [128, 36], F32)
        gmx = kb.sbuf("gmx", [128, 1], F32)
        ngm = kb.sbuf("ngm", [128, 1], F32)
        ge = kb.sbuf("ge", [128, 4], F32)
        gs = kb.sbuf("gsr", [128, 1], F32)
        oh = kb.sbuf("oh", [128, 4], F32)
        lem = kb.sbuf("lem", [128, 4, 8], F32)
        mx8 = kb.sbuf("mx8", [128, 8], F32)
        dd = kb.sbuf("dd", [128, 1], F32)
        w1 = kb.sbuf("w1", [128, 1], F32)
        w2 = kb.sbuf("w2", [128, 1], F32)
        tt1 = kb.sbuf("tt1", [128, 32], F32)
        tt2 = kb.sbuf("tt2", [128, 32], F32)
        wgt = kb.sbuf("wgt", [128, 32], F32)
        for bi, (t0, nb) in enumerate(MBLKS):
            col = blkcol(t0)
            b_ = brt[0]
            for i in range(4):
                kb.dma("sp", b_[:, i, :, :nb], brs[i][:, t0:t0 + nb].rearrange("(kc p) t -> p kc t", p=128),
                       reads=[brs[i]], writes=[b_])
            kb.dma("sp", gtt[:, :, :nb], cx.GATE[:, t0:t0 + nb].rearrange("(j p) t -> p j t", p=128), reads=[cx.GATE], writes=[gtt])
            kb.dma("sp", xb[:, :, :nb], xsrc[:, t0:t0 + nb].rearrange("(kc p) t -> p kc t", p=128), reads=[xsrc], writes=[xb])
            for oc in range(8):
                for i in range(4):
                    kb.mm([(lambda i=i, kc=kc, oc=oc: nc.tensor.matmul(
                        pbr[i][:, :nb], lhsT=wbr[:, i, kc, oc * 128:(oc + 1) * 128], rhs=b_[:, i, kc, :nb],
                        start=(kc == 0), stop=(kc == 3))) for kc in range(4)], reads=[wbr, b_], writes=[pbr[i]])
                    kb.op("act", lambda i=i: nc.scalar.copy(out=csb[i][:, :nb], in_=pbr[i][:, :nb]), reads=[pbr[i]], writes=[csb[i]])
                    eng = "dve" if i % 2 == 0 else "pool"
                    h_ = nc.vector if eng == "dve" else nc.gpsimd
                    kb.op(eng, lambda h_=h_, i=i, oc=oc: h_.tensor_tensor(
                        out=csb[i][:, :nb], in0=csb[i][:, :nb], in1=gtt[:, i * 8 + oc, :nb], op=ALU.mult),
                        reads=[csb[i], gtt], writes=[csb[i]])
                kb.op("dve", lambda: nc.vector.tensor_tensor(out=acc[:, :nb], in0=csb[0][:, :nb], in1=csb[1][:, :nb], op=ALU.add),
                      reads=[csb[0], csb[1]], writes=[acc])
                kb.op("pool", lambda: nc.gpsimd.tensor_tensor(out=csb[2][:, :nb], in0=csb[2][:, :nb], in1=csb[3][:, :nb], op=ALU.add),
                      reads=[csb[2], csb[3]], writes=[csb[2]])
                kb.op("dve", lambda oc=oc: nc.vector.tensor_tensor(out=mrg[:, oc, :nb], in0=acc[:, :nb], in1=csb[2][:, :nb], op=ALU.add),
                      reads=[acc, csb[2]], writes=[mrg])
            for oc in range(8):
                kb.mm([(lambda kc=kc, oc=oc: nc.tensor.matmul(
                    pmx[:, :nb], lhsT=wout[:, kc, oc * 128:(oc + 1) * 128], rhs=mrg[:, kc, :nb],
                    start=(kc == 0), stop=(kc == 7))) for kc in range(8)], reads=[wout, mrg], writes=[pmx])
                kb.op("act", lambda: nc.scalar.copy(out=mixs[:, :nb], in_=pmx[:, :nb]), reads=[pmx], writes=[mixs])
                kb.op("pool", lambda oc=oc: nc.gpsimd.tensor_scalar(
                    out=mixs[:, :nb], in0=mixs[:, :nb], scalar1=modv[:, 16 + oc, col:col + 1], scalar2=None, op0=ALU.mult),
                    reads=[mixs, modv], writes=[mixs])
                kb.op("dve", lambda oc=oc: nc.vector.scalar_tensor_tensor(
                    out=r[:, oc, :nb], in0=xb[:, oc, :nb], scalar=ALPHA, in1=mixs[:, :nb], op0=ALU.mult, op1=ALU.add),
                    reads=[xb, mixs], writes=[r])
            ln_block(kb, nc, r, nb, gam, bet, x1, ones, eps, (rsq, ps1, ps2, mean, var))
            kb.dma("sp", cx.XT[:, t0:t0 + nb].rearrange("(kc p) t -> p kc t", p=128), x1[:, :, :nb],
                   reads=[x1], writes=[], acc_writes=[cx.XT])
            for oc in range(8):
                eng = "dve" if oc % 2 == 0 else "pool"
                h_ = nc.vector if eng == "dve" else nc.gpsimd
                kb.op(eng, lambda h_=h_, oc=oc: h_.tensor_scalar(
                    out=hx2f[:, oc, :nb], in0=x1[:, oc, :nb], scalar1=onep4[:, oc, col:col + 1],
                    scalar2=modv[:, 24 + oc, col:col + 1], op0=ALU.mult, op1=ALU.add),
                    reads=[x1, onep4, modv], writes=[hx2f])
            kb.op("act", lambda: nc.scalar.copy(out=hx2b[:, :, :nb], in_=hx2f[:, :, :nb]), reads=[hx2f], writes=[hx2b])
            kb.dma("sp", cx.HX2[:, t0:t0 + nb].rearrange("(kc p) t -> p kc t", p=128), hx2b[:, :, :nb],
                   reads=[hx2b], writes=[], acc_writes=[cx.HX2])
            for tj in range(nb // 128):
                ti = t0 // 128 + tj
                tsl = slice(tj * 128, (tj + 1) * 128)
                kb.mm([(lambda kc=kc: nc.tensor.matmul(prt[:, 0:36], lhsT=hx2f[:, kc, tsl], rhs=wr[:, kc, :],
                                                       start=(kc == 0), stop=(kc == 7))) for kc in range(8)],
                      reads=[hx2f, wr], writes=[prt])
                kb.op("act", lambda: nc.scalar.copy(out=lgt[:], in_=prt[:, 0:36]), reads=[prt], writes=[lgt])
                kb.op("dve", lambda: nc.vector.tensor_tensor(out=lgt[:], in0=lgt[:], in1=rb[:], op=ALU.add), reads=[lgt, rb], writes=[lgt])
                kb.op("dve", lambda: nc.vector.tensor_reduce(out=gmx[:], in_=lgt[:, 0:4], axis=AX.X, op=ALU.max), reads=[lgt], writes=[gmx])
                kb.op("dve", lambda: nc.vector.tensor_scalar(out=ngm[:], in0=gmx[:], scalar1=-1.0, scalar2=None, op0=ALU.mult), reads=[gmx], writes=[ngm])
                kb.op("act", lambda: nc.scalar.activation(out=ge[:], in_=lgt[:, 0:4], func=AF.Exp, bias=ngm[:]), reads=[lgt, ngm], writes=[ge])
                kb.op("dve", lambda: nc.vector.tensor_reduce(out=gs[:], in_=ge[:], axis=AX.X, op=ALU.add), reads=[ge], writes=[gs])
                kb.op("dve", lambda: nc.vector.reciprocal(out=gs[:], in_=gs[:]), reads=[gs], writes=[gs])
                kb.op("dve", lambda: nc.vector.tensor_scalar(out=oh[:], in0=lgt[:, 0:4], scalar1=gmx[:], scalar2=None, op0=ALU.is_equal), reads=[lgt, gmx], writes=[oh])
                kb.op("dve", lambda: nc.vector.tensor_scalar(out=oh[:], in0=oh[:], scalar1=-1.0, scalar2=1e30, op0=ALU.add, op1=ALU.mult), reads=[oh], writes=[oh])
                kb.op("dve", lambda: nc.vector.tensor_tensor(
                    out=lem[:], in0=lgt[:, 4:36].rearrange("p (g e) -> p g e", e=8), in1=oh[:].unsqueeze(2).broadcast_to([128, 4, 8]), op=ALU.add),
                    reads=[lgt, oh], writes=[lem])
                lemf = lem[:].rearrange("p g e -> p (g e)")
                kb.op("dve", lambda: nc.vector.max(out=mx8[:], in_=lem[:].rearrange("p g e -> p (g e)")), reads=[lem], writes=[mx8])
                kb.op("dve", lambda: nc.vector.tensor_tensor(out=dd[:], in0=mx8[:, 1:2], in1=mx8[:, 0:1], op=ALU.subtract), reads=[mx8], writes=[dd])
                kb.op("act", lambda: nc.scalar.activation(out=dd[:], in_=dd[:], func=AF.Exp), reads=[dd], writes=[dd])
                kb.op("dve", lambda: nc.vector.tensor_scalar(out=w1[:], in0=dd[:], scalar1=1.0, scalar2=None, op0=ALU.add), reads=[dd], writes=[w1])
                kb.op("dve", lambda: nc.vector.reciprocal(out=w1[:], in_=w1[:]), reads=[w1], writes=[w1])
                kb.op("dve", lambda: nc.vector.tensor_tensor(out=w2[:], in0=dd[:], in1=w1[:], op=ALU.mult), reads=[dd, w1], writes=[w2])
                kb.op("dve", lambda: nc.vector.tensor_scalar(
                    out=tt1[:], in0=lem[:].rearrange("p g e -> p (g e)"), scalar1=mx8[:, 0:1], scalar2=w1[:], op0=ALU.is_equal, op1=ALU.mult),
                    reads=[lem, mx8, w1], writes=[tt1])
                kb.op("dve", lambda: nc.vector.tensor_scalar(
                    out=tt2[:], in0=lem[:].rearrange("p g e -> p (g e)"), scalar1=mx8[:, 1:2], scalar2=w2[:], op0=ALU.is_equal, op1=ALU.mult),
                    reads=[lem, mx8, w2], writes=[tt2])
                kb.op("dve", lambda: nc.vector.tensor_tensor(out=tt1[:], in0=tt1[:], in1=tt2[:], op=ALU.add), reads=[tt1, tt2], writes=[tt1])
                kb.op("dve", lambda: nc.vector.tensor_scalar(out=wgt[:], in0=tt1[:], scalar1=gs[:], scalar2=None, op0=ALU.mult), reads=[tt1, gs], writes=[wgt])
                kb.dma("sp", cx.WG_t[:, ti, :], wgt[:], reads=[wgt], writes=[], acc_writes=[cx.WG_t])


def stage_moe(cx, kb, nc, l, modv, yout=None):
    with kb.scope():
        ones, eps = ln_setup(cx, kb, nc)
        gam, bet = load_ln_params(cx, kb, nc, l, 1)
        wgT = kb.sbuf("wgT", [32, T], F32)
        wgl = kb.sbuf("wgl", [128, NT, 32], F32)
        id32 = kb.sbuf("id32", [128, 128], F32)
        sel = kb.sbuf("sel", [32, 32, 128], F32)
        kb.dma("sp", wgl[:], cx.WG_t[:], reads=[cx.WG_t], writes=[wgl])
        kb.dma("sp", id32[:], cx.m_ident32[:], reads=[cx.m_ident32], writes=[id32])
        kb.dma("sp", sel[:], cx.m_sel[:], reads=[cx.m_sel], writes=[sel])
        ptr = [kb.psum("ptr%d" % i, [128, 512]) for i in range(2)]
        for ti in range(NT):
            p_ = ptr[ti % 2]
            kb.mm([lambda p_=p_, ti=ti: nc.tensor.matmul(p_[:32, 0:128], lhsT=wgl[:, ti, :], rhs=id32[:], start=True, stop=True)],
                  reads=[wgl, id32], writes=[p_])
            kb.op("act", lambda p_=p_, ti=ti: nc.scalar.copy(out=wgT[:, ti * 128:(ti + 1) * 128], in_=p_[:32, 0:128]),
                  reads=[p_], writes=[], acc_writes=[wgT])
        precast_expert_weights(cx, kb, nc, l)
        hx = kb.sbuf("hxm", [128, 8, 512], BF16)
        yacc = kb.sbuf("yacc", [128, 8, 512], F32)
        hid = kb.sbuf("hid", [128, 16, 512], BF16)
        wdg = kb.sbuf("wdg", [128, 8, 2, 1024], BF16)
        wgs = [kb.sbuf("wgs%d" % i, [128, 8, 256], BF16) for i in range(2)]
        wus = [kb.sbuf("wus%d" % i, [128, 8, 256], BF16) for i in range(2)]
        xb = kb.sbuf("xbe", [128, 8, 512], F32)
        rsq = kb.sbuf("rsqe", [128, 8, 512], F32)
        mean = kb.sbuf("meane", [128, 512], F32)
        var = kb.sbuf("vare", [128, 512], F32)
        reps = [kb.sbuf("reps%d" % i, [128, 512], F32) for i in range(2)]
        ssb = [kb.sbuf("ssb%d" % i, [128, 512], F32) for i in range(2)]
        tsb = [kb.sbuf("tsb%d" % i, [128, 512], F32) for i in range(2)]
        ytmp = [kb.sbuf("ytmp%d" % i, [128, 512], F32) for i in range(2)]
        pg = [kb.psum("pg%d" % i, [128, 512]) for i in range(2)]
        pu = [kb.psum("pu%d" % i, [128, 512]) for i in range(2)]
        py = ptr
        ps1 = kb.psum("ps1e", [128, 512])
        ps2 = kb.psum("ps2e", [128, 512])
        prep = ps1
        nw = 0
        it = 0
        for bi, (t0, nb) in enumerate(BLKS):
            col = blkcol(t0)
            kb.dma("sp", hx[:, :, :nb], cx.HX2[:, t0:t0 + nb].rearrange("(kc p) t -> p kc t", p=128), reads=[cx.HX2], writes=[hx])
            kb.dma("sp", xb[:, :, :nb], cx.XT[:, t0:t0 + nb].rearrange("(kc p) t -> p kc t", p=128), reads=[cx.XT], writes=[xb])
            for g in range(4):
                kb.dma("sp", wdg[:].rearrange("p e hc d -> p (e hc d)"), cx.WDB[g], reads=[cx.WDB], writes=[wdg])
                for e_ in range(8):
                    e = g * 8 + e_
                    wg_, wu_ = wgs[nw % 2], wus[nw % 2]
                    nw += 1
                    kb.dma("sp", wg_[:].rearrange("p kc f -> p (kc f)"), cx.WGB[e], reads=[cx.WGB], writes=[wg_])
                    kb.dma("act", wu_[:].rearrange("p kc f -> p (kc f)"), cx.WUB[e], reads=[cx.WUB], writes=[wu_])
                    rp = reps[e % 2]
                    kb.mm([lambda e=e: nc.tensor.matmul(prep[:, :nb], lhsT=sel[:, e, :], rhs=wgT[:, t0:t0 + nb], start=True, stop=True)],
                          reads=[sel, wgT], writes=[prep])
                    kb.op("act", lambda rp=rp: nc.scalar.copy(out=rp[:, :nb], in_=prep[:, :nb]), reads=[prep], writes=[rp])
                    for hc in range(2):
                        a = it % 2
                        it += 1
                        kb.mm([(lambda kc=kc, a=a, hc=hc, wg_=wg_: nc.tensor.matmul(
                            pg[a][:, :nb], lhsT=wg_[:, kc, hc * 128:(hc + 1) * 128], rhs=hx[:, kc, :nb],
                            start=(kc == 0), stop=(kc == 7))) for kc in range(8)], reads=[wg_, hx], writes=[pg[a]])
                        kb.mm([(lambda kc=kc, a=a, hc=hc, wu_=wu_: nc.tensor.matmul(
                            pu[a][:, :nb], lhsT=wu_[:, kc, hc * 128:(hc + 1) * 128], rhs=hx[:, kc, :nb],
                            start=(kc == 0), stop=(kc == 7))) for kc in range(8)], reads=[wu_, hx], writes=[pu[a]])
                        kb.op("act", lambda a=a: nc.scalar.activation(out=ssb[a][:, :nb], in_=pg[a][:, :nb], func=AF.Silu),
                              reads=[pg[a]], writes=[ssb[a]])
                        kb.op("act", lambda a=a: nc.scalar.copy(out=tsb[a][:, :nb], in_=pu[a][:, :nb]), reads=[pu[a]], writes=[tsb[a]])
                        kb.op("dve", lambda a=a: nc.vector.tensor_tensor(out=tsb[a][:, :nb], in0=tsb[a][:, :nb], in1=ssb[a][:, :nb], op=ALU.mult),
                              reads=[tsb[a], ssb[a]], writes=[tsb[a]])
                        kb.op("pool", lambda a=a, rp=rp, e_=e_, hc=hc: nc.gpsimd.tensor_tensor(
                            out=hid[:, e_ * 2 + hc, :nb], in0=tsb[a][:, :nb], in1=rp[:, :nb], op=ALU.mult),
                            reads=[tsb[a], rp], writes=[], acc_writes=[hid])
                for oc in range(8):
                    p_ = py[oc % 2]
                    fns = []
                    for j in range(16):
                        fns.append(lambda j=j, oc=oc, p_=p_: nc.tensor.matmul(
                            p_[:, :nb], lhsT=wdg[:, j // 2, j % 2, oc * 128:(oc + 1) * 128], rhs=hid[:, j, :nb],
                            start=(j == 0), stop=(j == 15)))
                    kb.mm(fns, reads=[wdg, hid], writes=[p_])
                    if g == 0:
                        kb.op("act", lambda oc=oc, p_=p_: nc.scalar.copy(out=yacc[:, oc, :nb], in_=p_[:, :nb]), reads=[p_], writes=[], acc_writes=[yacc])
                    else:
                        yt = ytmp[oc % 2]
                        kb.op("act", lambda yt=yt, p_=p_: nc.scalar.copy(out=yt[:, :nb], in_=p_[:, :nb]), reads=[p_], writes=[yt])
                        eng = "dve" if oc % 2 == 0 else "pool"
                        h_ = nc.vector if eng == "dve" else nc.gpsimd
                        kb.op(eng, lambda h_=h_, oc=oc, yt=yt: h_.tensor_tensor(
                            out=yacc[:, oc, :nb], in0=yacc[:, oc, :nb], in1=yt[:, :nb], op=ALU.add),
                            reads=[yt, yacc], writes=[], acc_writes=[yacc])
            for oc in range(8):
                kb.op("pool", lambda oc=oc: nc.gpsimd.tensor_scalar(
                    out=yacc[:, oc, :nb], in0=yacc[:, oc, :nb], scalar1=modv[:, 40 + oc, col:col + 1], scalar2=None, op0=ALU.mult),
                    reads=[yacc, modv], writes=[yacc])
                kb.op("dve", lambda oc=oc: nc.vector.scalar_tensor_tensor(
                    out=yacc[:, oc, :nb], in0=xb[:, oc, :nb], scalar=ALPHA, in1=yacc[:, oc, :nb], op0=ALU.mult, op1=ALU.add),
                    reads=[xb, yacc], writes=[yacc])
            ln_block(kb, nc, yacc, nb, gam, bet, yacc, ones, eps, (rsq, ps1, ps2, mean, var))
            kb.dma("sp", cx.XT[:, t0:t0 + nb].rearrange("(kc p) t -> p kc t", p=128), yacc[:, :, :nb],
                   reads=[yacc], writes=[], acc_writes=[cx.XT])
            if yout is not None and t0 < TL:
                kb.dma("sp", yout[:, t0:t0 + nb].rearrange("(kc p) t -> p kc t", p=128), yacc[:, :, :nb],
                       reads=[yacc], writes=[], acc_writes=[yout])


def precast_expert_weights(cx, kb, nc, l):
    with kb.scope():
        stw = [kb.sbuf("pcw%d" % i, [128, 8, 256], BF16) for i in range(4)]
        std = [kb.sbuf("pcd%d" % i, [128, 16, 1024], BF16) for i in range(2)]
        n = 0
        for g in range(4):
            d_ = std[g % 2]
            kb.dma("pool", d_[:], cx.moe_w_down[l, g].rearrange("e (hc p) d -> p (e hc) d", p=128), reads=[cx.moe_w_down], writes=[d_])
            kb.dma("sp", cx.WDB[g], d_[:].rearrange("p j d -> p (j d)"), reads=[d_], writes=[], acc_writes=[cx.WDB])
            for e_ in range(8):
                e = g * 8 + e_
                for src, dst in ((cx.moe_w_gate, cx.WGB), (cx.moe_w_up, cx.WUB)):
                    w_ = stw[n % 4]
                    n += 1
                    kb.dma("pool", w_[:], src[l, g, e_].rearrange("(kc p) f -> p kc f", p=128), reads=[src], writes=[w_])
                    kb.dma("sp", dst[e], w_[:].rearrange("p kc f -> p (kc f)"), reads=[w_], writes=[], acc_writes=[dst])


def rope_tables():
    t = np.arange(TL)
    row = (t // 64).astype(np.float32)
    colp = (t % 64).astype(np.float32)
    nf = 16
    inv = (np.float32(10000.0) ** (-np.arange(nf, dtype=np.float32) / np.float32(nf))).astype(np.float32)
    cos = np.ones((128, T), np.float32)
    sin = np.zeros((128, T), np.float32)
    for q in range(128):
        d = q % 64
        pos = row if d < 32 else colp
        ang = (pos * inv[d % 16]).astype(np.float32)
        cos[q, :TL] = np.cos(ang).astype(np.float32)
        sin[q, :TL] = np.sin(ang).astype(np.float32)
    pt = np.zeros((128, 128), np.float32)
    for m in range(128):
        if (m % 32) < 16:
            pt[m + 16, m] = -1.0
        else:
            pt[m - 16, m] = 1.0
    return cos, sin, pt


_CONST = {}
_NAIDX = {}


def consts():
    if not _CONST:
        cos, sin, pt = rope_tables()
        _CONST.update(rope_cos=cos, rope_sin=sin, rope_pt=pt)
        _CONST.update(scan_masks())
        _CONST.update(wa_masks())
        sel = np.zeros((32, 32, 128), np.float32)
        for e in range(32):
            sel[e, e, :] = 1.0
        _CONST["m_sel"] = sel
        _CONST["m_ident32"] = np.eye(128, dtype=np.float32)
        _k, _valid, _ri, _ci = na_consts()
        _CONST["m_navalid"] = _valid
        _NAIDX["ri"] = _ri
        _NAIDX["ci"] = _ci
    return _CONST


def core_inputs(inp, b):
    f = lambda a: np.ascontiguousarray(a, dtype=np.float32)
    m = {}
    m["xin"] = f(np.concatenate([inp["x"][b].T, inp["ctx"][b].T], axis=1))
    cv = np.stack([inp["c"][b].reshape(8, 128).T, inp["c_ctx"].reshape(8, 128).T], axis=2)
    m["cvec"] = f(cv)
    m["w_mod"] = f(inp["w_mod"])
    m["b_modT"] = f(inp["b_mod"].reshape(2, 48, 128).transpose(0, 2, 1))
    m["w_in"] = f(inp["w_in"])
    m["hg_gamma"] = f(inp["hg_gamma"])
    m["gla_lr_w"] = f(inp["gla_lr_w"])
    m["gla_lr_b"] = f(inp["gla_lr_b"])
    m["wa_sink"] = f(inp["wa_sink"])
    for k in ("w_branch", "w_out", "moe_w_group", "moe_b_group", "moe_w_expert", "moe_b_expert",
              "moe_w_gate", "moe_w_up", "moe_w_down"):
        m[k] = f(inp[k])
    m["ln_gT"] = f(inp["ln_g"].reshape(2, 2, 8, 128).transpose(0, 1, 3, 2))
    m["ln_bT"] = f(inp["ln_b"].reshape(2, 2, 8, 128).transpose(0, 1, 3, 2))
    consts()
    m["na_bias"] = f(inp["na_rpb"][:, :, _NAIDX["ri"], _NAIDX["ci"]])
    m.update(consts())
    return m


FUSED = 1


def build_program(layers, final):
    nc = bass.Bass("TRN2", target_bir_lowering=False)
    cx = Ctx()
    declare_inputs(cx, nc)
    if final:
        yout = Buf(nc.dram_tensor("yout", [D, TL], F32, kind="ExternalOutput"), "yout")
    else:
        yout = None
    with ExitStack() as st:
        kb = KB(nc, st)
        declare_scratch(cx, kb, debug=False, as_output=(() if final else ("XT",)))
        modT = [kb.sbuf("modT%d" % l, [128, 48, 2], F32) for l in range(2)]
        stage_mod(cx, kb, nc, modT)
        first = True
        for l in layers:
            xsrc = cx.xin if first else cx.XT
            first = False
            with kb.scope():
                lbt = [kb.sbuf("lbt%d" % i, [128, 1024], F32) for i in range(2)]
                omlt = [kb.sbuf("omlt%d" % i, [128, 1024], F32) for i in range(2)]
                stage_lb(cx, kb, nc, lbt, omlt)
                hxT = kb.sbuf("hxT", [128, 8, T], BF16)
                stage_modulate(cx, kb, nc, xsrc, modT[l], 0, 1, hxT)
                stage_inproj_fm(cx, kb, nc, l, hxT)
                stage_inproj_tm(cx, kb, nc, l, hxT, lbt[l], omlt[l])
            stage_gla_gate(cx, kb, nc, l)
            stage_scan(cx, kb, nc, "gla")
            stage_scan(cx, kb, nc, "hgrn")
            stage_wa(cx, kb, nc, l)
            stage_na(cx, kb, nc, l)
            stage_merge(cx, kb, nc, l, xsrc, modT[l])
            stage_moe(cx, kb, nc, l, modT[l], yout if (final and l == layers[-1]) else None)
        if final:
            kb.finish([yout])
        else:
            kb.finish([cx.XT])
    return nc


def kernel(**inputs):
    inp = {k: np.asarray(v) for k, v in inputs.items()}
    in_maps = [core_inputs(inp, b) for b in range(4)]
    if FUSED:
        nc = build_program([0, 1], True)
        res = run_bass_kernel_spmd(nc, in_maps, core_ids=list(range(4)))
    else:
        nc0 = build_program([0], False)
        res0 = run_bass_kernel_spmd(nc0, in_maps, core_ids=list(range(4)))
        for b in range(4):
            in_maps[b]["xin"] = np.ascontiguousarray(res0.results[b]["XT"])
        nc1 = build_program([1], True)
        res = run_bass_kernel_spmd(nc1, in_maps, core_ids=list(range(4)))
    out = np.stack([np.ascontiguousarray(res.results[b]["yout"].T) for b in range(4)], axis=0)
    return out.astype(np.float32)
```

```python
import numpy as np
from contextlib import ExitStack
import concourse.bass as bass
import concourse.mybir as mybir
from concourse.bass_utils import run_bass_kernel_spmd
import os

F32 = mybir.dt.float32
BF16 = mybir.dt.bfloat16
U32 = mybir.dt.uint32
AF = mybir.ActivationFunctionType
ALU = mybir.AluOpType
AX = mybir.AxisListType


class Buf:
    __slots__ = ("t", "w", "r", "name")

    def __init__(self, t, name=""):
        self.t = t
        self.w = []
        self.r = []
        self.name = name

    def __getitem__(self, idx):
        return self.t[idx]


class Dep:
    __slots__ = ("w", "r")

    def __init__(self):
        self.w = []
        self.r = []


class Eng:
    def __init__(self, name, h, sem):
        self.name = name
        self.h = h
        self.sem = sem
        self.count = 0
        self.seen = {}


class KB:
    def __init__(self, nc, stack, n_dma_sems=8):
        self.nc = nc
        self.stack = stack
        self.sems = {}
        self.engs = {}
        for name, h in (("pe", nc.tensor), ("act", nc.scalar), ("dve", nc.vector),
                        ("pool", nc.gpsimd), ("sp", nc.sync)):
            s = stack.enter_context(nc.semaphore("s_" + name))
            self.sems[id(s)] = s
            self.engs[name] = Eng(name, h, s)
        self.dma_pool = {}
        for q in ("sp", "pool", "act"):
            lst = []
            for i in range(n_dma_sems):
                s = stack.enter_context(nc.semaphore("d_%s%d" % (q, i)))
                self.sems[id(s)] = s
                lst.append([s, 0])
            self.dma_pool[q] = [lst, 0]
        self.ninstr = 0

    def scope(self):
        kb = self

        class _Scope:
            def __enter__(self_s):
                self_s.prev = kb.stack
                self_s.st = ExitStack()
                self_s.st.__enter__()
                kb.stack = self_s.st
                kb.nscope = getattr(kb, "nscope", 0) + 1
                return self_s

            def __exit__(self_s, *a):
                if a[0] is None:
                    kb.barrier()
                kb.stack = self_s.prev
                return self_s.st.__exit__(*a)
        return _Scope()

    def sbuf(self, name, shape, dtype):
        name = "%s_s%d" % (name, getattr(self, "nscope", 0))
        t = self.stack.enter_context(self.nc.sbuf_tensor(name, list(shape), dtype))
        return Buf(t, name)

    def psum(self, name, shape, dtype=F32):
        name = "%s_s%d" % (name, getattr(self, "nscope", 0))
        t = self.stack.enter_context(self.nc.psum_tensor(name, list(shape), dtype))
        return Buf(t, name)

    def dram(self, name, shape, dtype, kind="Internal"):
        t = self.nc.dram_tensor(name, list(shape), dtype, kind=kind)
        return Buf(t, name)

    def _wait(self, e, deps):
        need = {}
        for (sid, val) in deps:
            if need.get(sid, 0) < val:
                need[sid] = val
        for sid, val in need.items():
            if e.seen.get(sid, 0) < val:
                e.h.wait_ge(self.sems[sid], val)
                e.seen[sid] = val
                self.ninstr += 1

    def _collect(self, reads, writes, acc_writes=()):
        deps = []
        for b in reads:
            deps += b.w
        for b in writes:
            deps += b.w
            deps += b.r
        for b in acc_writes:
            deps += b.r
        return deps

    @staticmethod
    def _compact(lst):
        mx = {}
        for (sid, v) in lst:
            if mx.get(sid, 0) < v:
                mx[sid] = v
        return list(mx.items())

    def _commit(self, token, reads, writes, acc_writes=()):
        for b in acc_writes:
            b.w.append(token)
            if len(b.w) > 64:
                b.w = self._compact(b.w)
        for b in reads:
            b.r.append(token)
            if len(b.r) > 64:
                mx = {}
                for (sid, v) in b.r:
                    if mx.get(sid, 0) < v:
                        mx[sid] = v
                b.r = list(mx.items())
        for b in writes:
            b.w = [token]
            b.r = []

    def _cutcheck(self):
        import os
        cut = os.environ.get("KB_CUT")
        self.nops = getattr(self, "nops", 0) + 1
        skip = os.environ.get("KB_SKIP")
        if skip and self.nops in [int(x) for x in skip.split(",")]:
            return True
        return cut is not None and self.nops > int(cut)

    def op(self, eng, fn, reads=(), writes=(), same_engine_sync=True, acc_writes=()):
        if self._cutcheck():
            return None
        e = self.engs[eng]
        deps = self._collect(reads, writes, acc_writes)
        if not same_engine_sync:
            deps = [d for d in deps if d[0] != id(e.sem)]
        self._wait(e, deps)
        ins = fn()
        e.count += 1
        ins.then_inc(e.sem, 1)
        self.ninstr += 1
        token = (id(e.sem), e.count)
        self._commit(token, reads, writes, acc_writes)
        return token

    def mm(self, fns, reads=(), writes=()):
        if self._cutcheck():
            return None
        e = self.engs["pe"]
        deps = self._collect(reads, writes)
        deps = [d for d in deps if d[0] != id(e.sem)]
        self._wait(e, deps)
        ins = None
        for fn in fns:
            ins = fn()
            self.ninstr += 1
        e.count += 1
        ins.then_inc(e.sem, 1)
        token = (id(e.sem), e.count)
        self._commit(token, reads, writes)
        return token

    def dma(self, q, out, in_, reads=(), writes=(), acc_writes=(), **kw):
        e = self.engs[q]
        pool = self.dma_pool[q]
        lst, idx = pool
        ent = lst[idx % len(lst)]
        pool[1] = idx + 1
        s, total = ent
        deps = self._collect(reads, writes, acc_writes)
        deps.append((id(s), total))
        import os
        if os.environ.get("DMA_SERIAL") and getattr(self, "_last_dma", {}).get(q):
            deps.append(self._last_dma[q])
        self._wait(e, deps)
        ins = e.h.dma_start(out=out, in_=in_, **kw)
        ins.then_inc(s, 16)
        ent[1] = total + 16
        self.ninstr += 1
        token = (id(s), total + 16)
        if not hasattr(self, "_last_dma"):
            self._last_dma = {}
        self._last_dma[q] = token
        self._commit(token, reads, writes, acc_writes)
        return token

    def init_memory(self, nbytes=196608):
        nc = self.nc
        with self.scope():
            n = nbytes // 4
            big = self.sbuf("initbig", [128, n], F32)
            q = n // 4
            self.op("dve", lambda: nc.vector.memset(big[:, 0:q], 0.0), writes=[])
            self.op("pool", lambda: nc.gpsimd.memset(big[:, q:2 * q], 0.0), writes=[])
            self.op("dve", lambda: nc.vector.memset(big[:, 2 * q:3 * q], 0.0), writes=[])
            self.op("pool", lambda: nc.gpsimd.memset(big[:, 3 * q:n], 0.0), writes=[])
            pss = [self.psum("initps%d" % i, [128, 512]) for i in range(8)]
            for p in pss:
                self.op("dve", lambda p=p: nc.vector.memset(p[:], 0.0), writes=[])

    def barrier(self):
        allt = []
        for e in self.engs.values():
            if e.count:
                allt.append((id(e.sem), e.count))
        for q, (lst, _) in self.dma_pool.items():
            for s, total in lst:
                if total:
                    allt.append((id(s), total))
        for e in self.engs.values():
            self._wait(e, allt)

    def finish(self, tokens_bufs):
        e = self.engs["sp"]
        deps = []
        for b in tokens_bufs:
            deps += b.w
        self._wait(e, deps)

T = 4352
TL = 4096

D = 1024
TL = 4096
TC = 256
T = TL + TC
NT = T // 128
BLKS = [(i * 512, 512) for i in range(8)] + [(4096, 256)]
DIN = 10528
ALPHA = (2.0 * 2) ** 0.25

COLS = dict(a_q=(0, 256), a_k=(256, 256), a_v=(512, 512), a_g=(1024, 512), a_lr=(1536, 32),
            b_q=(1568, 512), b_k=(2080, 512), b_v=(2592, 512),
            c_q=(3104, 512), c_k=(3616, 128), c_v=(3744, 128),
            d_q=(3872, 512), d_f=(4384, 1024), d_i=(5408, 512), d_g=(5920, 512), gate=(6432, 4096))


def blkcol(t0):
    return 1 if t0 >= TL else 0


class Ctx:
    pass


def declare_inputs(cx, nc):
    def din(name, shape, dt=F32):
        return Buf(nc.dram_tensor(name, list(shape), dt, kind="ExternalInput"), name)
    cx.xin = din("xin", [D, T])
    cx.cvec = din("cvec", [128, 8, 2])
    cx.w_mod = din("w_mod", [2, D, 6 * D])
    cx.b_modT = din("b_modT", [2, 128, 48])
    cx.w_in = din("w_in", [2, D, DIN])
    cx.rope_cos = din("rope_cos", [128, T])
    cx.rope_sin = din("rope_sin", [128, T])
    cx.rope_pt = din("rope_pt", [128, 128])
    cx.hg_gamma = din("hg_gamma", [2, 2, 512])
    cx.gla_lr_w = din("gla_lr_w", [2, 2, 16, 256])
    cx.gla_lr_b = din("gla_lr_b", [2, 2, 256])
    for nm in ("mg", "mh"):
        setattr(cx, nm + "_mq", din(nm + "_mq", [128, 2, 130]))
        setattr(cx, nm + "_mk", din(nm + "_mk", [128, 2, 128]))
    cx.m_ma = din("m_ma", [128, 2, 128])
    cx.m_ident = din("m_ident", [128, 128])
    cx.m_mau = din("m_mau", [128, 2, 128], U32)
    cx.m_wa = din("m_wa", [128, 2, 128])
    cx.wa_sink = din("wa_sink", [2, 8])
    cx.m_navalid = din("m_navalid", [128, 21, 128])
    cx.w_branch = din("w_branch", [2, 4, 512, 1024])
    cx.w_out = din("w_out", [2, 1024, 1024])
    cx.ln_gT = din("ln_gT", [2, 2, 128, 8])
    cx.ln_bT = din("ln_bT", [2, 2, 128, 8])
    cx.moe_w_group = din("moe_w_group", [2, 1024, 4])
    cx.moe_b_group = din("moe_b_group", [2, 4])
    cx.moe_w_expert = din("moe_w_expert", [2, 1024, 32])
    cx.moe_b_expert = din("moe_b_expert", [2, 32])
    cx.moe_w_gate = din("moe_w_gate", [2, 4, 8, 1024, 256])
    cx.moe_w_up = din("moe_w_up", [2, 4, 8, 1024, 256])
    cx.moe_w_down = din("moe_w_down", [2, 4, 8, 256, 1024])
    cx.m_sel = din("m_sel", [32, 32, 128])
    cx.m_ident32 = din("m_ident32", [128, 128])
    cx.na_bias = din("na_bias", [2, 8, 128, 21, 128])


def declare_scratch(cx, kb, debug, as_input=(), as_output=()):
    kind = "ExternalOutput" if debug else "Internal"

    def ds(name, shape, dt):
        b = kb.dram(name, shape, dt, kind=("ExternalInput" if name in as_input else ("ExternalOutput" if name in as_output else kind)))
        setattr(cx, name, b)
        return b
    ds("XT", [D, T], F32)
    ds("QA", [256, T], BF16)
    ds("GA", [512, T], BF16)
    ds("LR1", [33, T], F32)
    ds("QB", [512, T], BF16)
    ds("KB_", [512, T], BF16)
    ds("QC", [512, T], BF16)
    ds("KC", [128, T], BF16)
    ds("QD", [512, T], BF16)
    ds("GD", [512, T], BF16)
    ds("GATE", [4096, T], BF16)
    ds("KVA_t", [128, NT, 768], BF16)
    ds("VB_t", [128, NT, 512], BF16)
    ds("VC_t", [128, NT, 128], BF16)
    ds("VD_t", [128, NT, 512], BF16)
    ds("LGD_t", [128, NT, 1024], F32)
    ds("KD_t", [128, NT, 1024], BF16)
    ds("LGA_t", [128, NT, 512], F32)
    ds("MODT", [2, 128, 96], F32)
    ds("HX2", [D, T], BF16)
    ds("WGB", [32, 128, 2048], BF16)
    ds("WUB", [32, 128, 2048], BF16)
    ds("WDB", [4, 128, 16384], BF16)
    ds("WG_t", [128, NT, 32], F32)
    ds("WGT", [32, T], F32)
    ds("BR_A", [512, T], BF16)
    ds("BR_B", [512, T], BF16)
    ds("BR_C", [512, T], BF16)
    ds("BR_D", [512, T], BF16)


def stage_mod(cx, kb, nc, modT):
    with kb.scope():
        cv = kb.sbuf("cv", [128, 8, 2], F32)
        sc = kb.sbuf("sc", [128, 8, 2], F32)
        kb.dma("sp", cv[:], cx.cvec[:], reads=[cx.cvec], writes=[cv])
        kb.op("act", lambda: nc.scalar.activation(out=sc[:], in_=cv[:], func=AF.Silu), reads=[cv], writes=[sc])
        wbufs = [kb.sbuf("wm%d" % i, [128, 8, 1024], F32) for i in range(2)]
        bm = kb.sbuf("bm", [128, 48], F32)
        ps = kb.psum("ps_mod", [128, 96])
        wi = 0
        for l in range(2):
            kb.dma("sp", bm[:], cx.b_modT[l], reads=[cx.b_modT], writes=[bm])
            for g in range(6):
                wb = wbufs[wi % 2]
                wi += 1
                src = cx.w_mod[l, :, g * 1024:(g + 1) * 1024].rearrange("(kc p) c -> p kc c", p=128)
                kb.dma("sp", wb[:], src, reads=[cx.w_mod], writes=[wb])
                for j in range(8):
                    jj = g * 8 + j
                    fns = []
                    for kc in range(8):
                        fns.append(lambda kc=kc, j=j, jj=jj, wb=wb: nc.tensor.matmul(
                            ps[:, 2 * jj:2 * jj + 2], lhsT=wb[:, kc, j * 128:(j + 1) * 128], rhs=sc[:, kc, :],
                            start=(kc == 0), stop=(kc == 7)))
                    kb.mm(fns, reads=[wb, sc], writes=[ps])
            m = modT[l]
            kb.op("dve", lambda m=m: nc.vector.tensor_tensor(
                out=m[:], in0=ps[:].rearrange("p (j c) -> p j c", c=2),
                in1=bm[:].unsqueeze(2).broadcast_to([128, 48, 2]), op=ALU.add),
                reads=[ps, bm], writes=[m])
            kb.dma("sp", cx.MODT[l], m[:].rearrange("p j c -> p (j c)"), reads=[m], writes=[cx.MODT])


def stage_modulate(cx, kb, nc, src_dram, modv, shift_idx, scale_idx, hxT, hx32=None):
    with kb.scope():
        onep = kb.sbuf("onep", [128, 8, 2], F32)
        kb.op("dve", lambda: nc.vector.tensor_scalar(
            out=onep[:], in0=modv[:, scale_idx * 8:(scale_idx + 1) * 8, :], scalar1=1.0, scalar2=None, op0=ALU.add),
            reads=[modv], writes=[onep])
        xb = [kb.sbuf("xb%d" % i, [128, 8, 512], F32) for i in range(2)]
        for bi, (t0, nb) in enumerate(BLKS):
            col = blkcol(t0)
            x = xb[bi % 2]
            kb.dma("sp", x[:, :, :nb], src_dram[:, t0:t0 + nb].rearrange("(kc p) t -> p kc t", p=128),
                   reads=[src_dram], writes=[x])
            for kc in range(8):
                eng = "dve" if kc % 2 == 0 else "pool"
                h = nc.vector if eng == "dve" else nc.gpsimd
                kb.op(eng, lambda h=h, kc=kc, x=x: h.tensor_scalar(
                    out=hxT[:, kc, t0:t0 + nb], in0=x[:, kc, :nb],
                    scalar1=onep[:, kc, col:col + 1], scalar2=modv[:, shift_idx * 8 + kc, col:col + 1],
                    op0=ALU.mult, op1=ALU.add), reads=[x, onep, modv], writes=[hxT])


FM_GROUPS = [
    ("a_q", "QA", "copy"), ("a_g", "GA", "silu"), ("a_lr", "LR1", "copy32"),
    ("b_q", "QB", "copy"), ("b_k", "KB_", "copy"),
    ("c_q", "QC", "rope"), ("c_k", "KC", "rope"),
    ("d_q", "QD", "silu"), ("d_g", "GD", "silu"), ("gate", "GATE", "sigmoid"),
]


def stage_inproj_fm(cx, kb, nc, l, hxT, only=None):
    with kb.scope():
        wsl = [kb.sbuf("wsl%d" % i, [128, 8, 512], BF16) for i in range(2)]
        stg = [kb.sbuf("stg%d" % i, [128, T], BF16) for i in range(2)]
        stg32 = kb.sbuf("stg32", [32, T], F32)
        pss = [kb.psum("psf%d" % i, [128, 512]) for i in range(4)]
        psr = [kb.psum("psr%d" % i, [128, 512]) for i in range(2)]
        cosT = kb.sbuf("cosT", [128, T], F32)
        sinT = kb.sbuf("sinT", [128, T], F32)
        ptm = kb.sbuf("ptm", [128, 128], BF16)
        tmpb = [kb.sbuf("tmpb%d" % i, [128, 512], BF16) for i in range(2)]
        t1 = [kb.sbuf("t1_%d" % i, [128, 512], F32) for i in range(2)]
        t2 = [kb.sbuf("t2_%d" % i, [128, 512], F32) for i in range(2)]
        ones = kb.sbuf("ones1", [1, T], F32)
        kb.dma("sp", cosT[:], cx.rope_cos[:], reads=[cx.rope_cos], writes=[cosT])
        kb.dma("sp", sinT[:], cx.rope_sin[:], reads=[cx.rope_sin], writes=[sinT])
        kb.dma("pool", ptm[:], cx.rope_pt[:], reads=[cx.rope_pt], writes=[ptm])
        kb.op("dve", lambda: nc.vector.memset(ones[:], 1.0), writes=[ones])
        kb.dma("sp", cx.LR1[32:33, :], ones[:], reads=[ones], writes=[], acc_writes=[cx.LR1])
        nsl = 0
        nps = 0
        nst = 0
        nrp = 0
        for (name, dest, kind) in FM_GROUPS:
            if only is not None and name not in only:
                continue
            c0, nc_ = COLS[name]
            dbuf = getattr(cx, dest)
            for s0 in range(0, nc_, 512):
                sw = min(512, nc_ - s0)
                w = wsl[nsl % 2]
                nsl += 1
                kb.dma("pool", w[:, :, :sw],
                       cx.w_in[l, :, c0 + s0:c0 + s0 + sw].rearrange("(kc p) c -> p kc c", p=128),
                       reads=[cx.w_in], writes=[w])
                for m0 in range(0, sw, 128):
                    mw = min(128, sw - m0)
                    if kind == "copy32":
                        st = stg32
                    else:
                        st = stg[nst % 2]
                        nst += 1
                    for bi, (t0, nb) in enumerate(BLKS):
                        ps = pss[nps % 4]
                        nps += 1
                        fns = [(lambda kc=kc, ps=ps, w=w: nc.tensor.matmul(
                            ps[:mw, :nb], lhsT=w[:, kc, m0:m0 + mw], rhs=hxT[:, kc, t0:t0 + nb],
                            start=(kc == 0), stop=(kc == 7))) for kc in range(8)]
                        kb.mm(fns, reads=[w, hxT], writes=[ps])
                        o = st[:mw, t0:t0 + nb]
                        if kind in ("copy", "copy32"):
                            if nps % 2 == 0:
                                kb.op("dve", lambda ps=ps, o=o: nc.vector.tensor_copy(out=o, in_=ps[:mw, :nb]),
                                      reads=[ps], writes=[st])
                            else:
                                kb.op("act", lambda ps=ps, o=o: nc.scalar.copy(out=o, in_=ps[:mw, :nb]),
                                      reads=[ps], writes=[st])
                        elif kind == "silu":
                            kb.op("act", lambda ps=ps, o=o: nc.scalar.activation(out=o, in_=ps[:mw, :nb], func=AF.Silu),
                                  reads=[ps], writes=[st])
                        elif kind == "sigmoid":
                            kb.op("act", lambda ps=ps, o=o: nc.scalar.activation(out=o, in_=ps[:mw, :nb], func=AF.Sigmoid),
                                  reads=[ps], writes=[st])
                        elif kind == "rope":
                            tb = tmpb[nrp % 2]
                            a1 = t1[nrp % 2]
                            a2 = t2[nrp % 2]
                            pr = psr[nrp % 2]
                            nrp += 1
                            kb.op("act", lambda ps=ps, tb=tb: nc.scalar.copy(out=tb[:mw, :nb], in_=ps[:mw, :nb]),
                                  reads=[ps], writes=[tb])
                            kb.mm([lambda pr=pr, tb=tb: nc.tensor.matmul(pr[:mw, :nb], lhsT=ptm[:mw, :mw], rhs=tb[:mw, :nb],
                                                                         start=True, stop=True)],
                                  reads=[ptm, tb], writes=[pr])
                            kb.op("dve", lambda tb=tb, a1=a1: nc.vector.tensor_tensor(
                                out=a1[:mw, :nb], in0=tb[:mw, :nb], in1=cosT[:mw, t0:t0 + nb], op=ALU.mult),
                                reads=[tb, cosT], writes=[a1])
                            kb.op("dve", lambda pr=pr, a2=a2: nc.vector.tensor_tensor(
                                out=a2[:mw, :nb], in0=pr[:mw, :nb], in1=sinT[:mw, t0:t0 + nb], op=ALU.mult),
                                reads=[pr, sinT], writes=[a2])
                            kb.op("pool", lambda a1=a1, a2=a2, o=o: nc.gpsimd.tensor_tensor(
                                out=o, in0=a1[:mw, :nb], in1=a2[:mw, :nb], op=ALU.add),
                                reads=[a1, a2], writes=[st])
                    kb.dma("sp", dbuf[s0 + m0:s0 + m0 + mw, :], st[:mw, :], reads=[st], writes=[], acc_writes=[dbuf])


TM_GROUPS = [
    (256, 512, "KVA_t", 0, "copy"), (768, 256, "KVA_t", 512, "copy"),
    (2592, 512, "VB_t", 0, "copy"), (3744, 128, "VC_t", 0, "copy"),
    (5408, 512, "VD_t", 0, "copy"),
    (4384, 512, None, 0, "hgf"), (4896, 512, None, 512, "hgf"),
]


def stage_inproj_tm(cx, kb, nc, l, hxT, lbt, omlt, only=None):
    with kb.scope():
        wsl = [kb.sbuf("wtl%d" % i, [128, 8, 512], BF16) for i in range(2)]
        pss = [kb.psum("pst%d" % i, [128, 512]) for i in range(4)]
        stb = [kb.sbuf("stb%d" % i, [128, 512], BF16) for i in range(4)]
        e1 = [kb.sbuf("e1_%d" % i, [128, 512], F32) for i in range(2)]
        r1 = [kb.sbuf("r1_%d" % i, [128, 512], F32) for i in range(2)]
        lg = [kb.sbuf("lg_%d" % i, [128, 512], F32) for i in range(2)]
        kk = [kb.sbuf("kk_%d" % i, [128, 512], BF16) for i in range(2)]
        nsl = nps = nst = nh = 0
        for (c0, ncol, dest, doff, kind) in TM_GROUPS:
            if only is not None and kind not in only:
                continue
            w = wsl[nsl % 2]
            nsl += 1
            kb.dma("pool", w[:, :, :ncol], cx.w_in[l, :, c0:c0 + ncol].rearrange("(kc p) c -> p kc c", p=128),
                   reads=[cx.w_in], writes=[w])
            for ti in range(NT):
                ts = slice(ti * 128, (ti + 1) * 128)
                ps = pss[nps % 4]
                nps += 1
                fns = [(lambda kc=kc, ps=ps, w=w: nc.tensor.matmul(
                    ps[:, :ncol], lhsT=hxT[:, kc, ts], rhs=w[:, kc, :ncol],
                    start=(kc == 0), stop=(kc == 7))) for kc in range(8)]
                kb.mm(fns, reads=[w, hxT], writes=[ps])
                if kind == "copy":
                    st = stb[nst % 4]
                    nst += 1
                    if nst % 2 == 0:
                        kb.op("dve", lambda ps=ps, st=st: nc.vector.tensor_copy(out=st[:, :ncol], in_=ps[:, :ncol]),
                              reads=[ps], writes=[st])
                    else:
                        kb.op("act", lambda ps=ps, st=st: nc.scalar.copy(out=st[:, :ncol], in_=ps[:, :ncol]),
                              reads=[ps], writes=[st])
                    db = getattr(cx, dest)
                    kb.dma("sp", db[:, ti, doff:doff + ncol], st[:, :ncol], reads=[st], writes=[], acc_writes=[db])
                else:
                    e = e1[nh % 2]
                    r = r1[nh % 2]
                    g = lg[nh % 2]
                    k = kk[nh % 2]
                    nh += 1
                    cs = slice(doff, doff + 512)
                    kb.op("act", lambda ps=ps, e=e: nc.scalar.activation(out=e[:], in_=ps[:], func=AF.Exp, scale=-1.0),
                          reads=[ps], writes=[e])
                    kb.op("pool", lambda e=e: nc.gpsimd.tensor_scalar(
                        out=e[:], in0=e[:], scalar1=1.0, scalar2=None, op0=ALU.add),
                        reads=[e], writes=[e])
                    kb.op("dve", lambda e=e, r=r: nc.vector.reciprocal(out=r[:], in_=e[:]),
                          reads=[e], writes=[r])
                    kb.op("dve", lambda r=r: nc.vector.tensor_tensor(out=r[:], in0=r[:], in1=omlt[:, cs], op=ALU.mult),
                          reads=[r, omlt], writes=[r])
                    kb.op("pool", lambda r=r: nc.gpsimd.tensor_tensor(out=r[:], in0=r[:], in1=lbt[:, cs], op=ALU.add),
                          reads=[r, lbt], writes=[r])
                    kb.op("act", lambda r=r, g=g: nc.scalar.activation(out=g[:], in_=r[:], func=AF.Ln),
                          reads=[r], writes=[g])
                    kb.op("pool", lambda r=r, k=k: nc.gpsimd.tensor_scalar(
                        out=k[:], in0=r[:], scalar1=-1.0, scalar2=1.0, op0=ALU.mult, op1=ALU.add),
                        reads=[r], writes=[k])
                    kb.dma("sp", cx.LGD_t[:, ti, cs], g[:], reads=[g], writes=[], acc_writes=[cx.LGD_t])
                    kb.dma("sp", cx.KD_t[:, ti, cs], k[:], reads=[k], writes=[], acc_writes=[cx.KD_t])


def stage_lb(cx, kb, nc, lbt, omlt):
    with kb.scope():
        gam = kb.sbuf("gam", [128, 2, 2, 512], F32)
        kb.dma("sp", gam[:].rearrange("p a b c -> p (a b c)"),
               cx.hg_gamma[:].rearrange("a b c -> (a b c)").partition_broadcast(128),
               reads=[cx.hg_gamma], writes=[gam])
        d = kb.sbuf("gamd", [128, 2, 512], F32)
        kb.op("dve", lambda: nc.vector.tensor_tensor(out=d[:], in0=gam[:, :, 0, :], in1=gam[:, :, 1, :], op=ALU.subtract),
              reads=[gam], writes=[d])
        kb.op("act", lambda: nc.scalar.activation(out=d[:], in_=d[:], func=AF.Exp), reads=[d], writes=[d])
        l1 = lbt[1]
        kb.op("dve", lambda: nc.vector.tensor_scalar(
            out=d[:], in0=d[:], scalar1=1.0, scalar2=None, op0=ALU.add), reads=[d], writes=[d])
        kb.op("dve", lambda: nc.vector.reciprocal(out=l1[:], in_=d[:].rearrange("p a c -> p (a c)")),
              reads=[d], writes=[l1])
        l0 = lbt[0]
        kb.op("dve", lambda: nc.vector.memset(l0[:], 0.0), writes=[l0])
        for l in range(2):
            o = omlt[l]
            lb = lbt[l]
            kb.op("dve", lambda o=o, lb=lb: nc.vector.tensor_scalar(
                out=o[:], in0=lb[:], scalar1=-1.0, scalar2=1.0, op0=ALU.mult, op1=ALU.add),
                reads=[lb], writes=[o])


def stage_gla_gate(cx, kb, nc, l):
    with kb.scope():
        wlr = kb.sbuf("wlr", [33, 512], F32)
        lr1 = kb.sbuf("lr1", [33, T], F32)
        kb.op("dve", lambda: nc.vector.memset(wlr[:], 0.0), writes=[wlr])
        kb.dma("sp", wlr[0:16, 0:256], cx.gla_lr_w[l, 0], reads=[cx.gla_lr_w], writes=[wlr])
        kb.dma("sp", wlr[16:32, 256:512], cx.gla_lr_w[l, 1], reads=[cx.gla_lr_w], writes=[wlr])
        kb.dma("sp", wlr[32:33, :], cx.gla_lr_b[l].rearrange("a c -> (a c)").unsqueeze(0),
               reads=[cx.gla_lr_b], writes=[wlr])
        kb.dma("sp", lr1[:], cx.LR1[:], reads=[cx.LR1], writes=[lr1])
        pss = [kb.psum("psg%d" % i, [128, 512]) for i in range(2)]
        ee = [kb.sbuf("ge%d" % i, [128, 512], F32) for i in range(2)]
        gg = [kb.sbuf("gg%d" % i, [128, 512], F32) for i in range(2)]
        for ti in range(NT):
            ts = slice(ti * 128, (ti + 1) * 128)
            ps = pss[ti % 2]
            e = ee[ti % 2]
            g = gg[ti % 2]
            kb.mm([lambda ps=ps: nc.tensor.matmul(ps[:], lhsT=lr1[:, ts], rhs=wlr[:], start=True, stop=True)],
                  reads=[lr1, wlr], writes=[ps])
            kb.op("act", lambda ps=ps, e=e: nc.scalar.activation(out=e[:], in_=ps[:], func=AF.Exp, scale=-1.0),
                  reads=[ps], writes=[e])
            kb.op("act", lambda e=e, g=g: nc.scalar.activation(out=g[:], in_=e[:], func=AF.Ln, bias=1.0),
                  reads=[e], writes=[g])
            kb.dma("sp", cx.LGA_t[:, ti, :], g[:], reads=[g], writes=[], acc_writes=[cx.LGA_t])

SCAN_CFG = {
    "gla": dict(H=4, dk=64, qsrc="QA", qscale=0.125, ksrc="KVA_t", kz=0, kcol0=0, vsrc="KVA_t", vcol0=256,
                gsrc="LGA_t", gz=256, gate="GA", dest="BR_A", mask="mg"),
    "hgrn": dict(H=4, dk=128, qsrc="QD", qscale=1.0, ksrc="KD_t", kz=512, kcol0=0, vsrc="VD_t", vcol0=0,
                 gsrc="LGD_t", gz=512, gate="GD", dest="BR_D", mask="mh"),
}


def view(b):
    return Buf(b.t, b.name + "_v")


def stage_scan(cx, kb, nc, which, heads=None):
    cfg = SCAN_CFG[which]
    H, dk = cfg["H"], cfg["dk"]
    dv = 128
    qsrc, ksrc, vsrc, gsrc = (getattr(cx, cfg[k]) for k in ("qsrc", "ksrc", "vsrc", "gsrc"))
    gate, dest = getattr(cx, cfg["gate"]), getattr(cx, cfg["dest"])
    mq_d, mk_d = getattr(cx, cfg["mask"] + "_mq"), getattr(cx, cfg["mask"] + "_mk")
    with kb.scope():
        mq = kb.sbuf("mq", [128, 2, 130], F32)
        mk = kb.sbuf("mk", [128, 2, 128], F32)
        ma = kb.sbuf("ma", [128, 2, 128], F32)
        ident = kb.sbuf("ident", [128, 128], BF16)
        onesdv = kb.sbuf("onesdv", [128, 128], F32)
        kb.dma("sp", mq[:], mq_d[:], reads=[mq_d], writes=[mq])
        kb.dma("sp", mk[:], mk_d[:], reads=[mk_d], writes=[mk])
        kb.dma("sp", ma[:], cx.m_ma[:], reads=[cx.m_ma], writes=[ma])
        mau = kb.sbuf("mau", [128, 2, 128], U32)
        kb.dma("sp", mau[:], cx.m_mau[:], reads=[cx.m_mau], writes=[mau])
        kb.dma("pool", ident[:], cx.m_ident[:], reads=[cx.m_ident], writes=[ident])
        kb.op("dve", lambda: nc.vector.memset(onesdv[:], 1.0 / dv), writes=[onesdv])
        vt = kb.sbuf("vt", [128, NT, dv], BF16)
        qT = kb.sbuf("qT", [128, T], BF16)
        gsil = kb.sbuf("gsil", [128, T], BF16)
        obr = kb.sbuf("obr", [128, T], BF16)
        gts = [kb.sbuf("gt%d" % i, [128, NT, dk], F32) for i in range(2)]
        kts = [kb.sbuf("kt%d" % i, [128, NT, dk], BF16) for i in range(2)]
        qt = [kb.sbuf("qt%d" % z, [128, T], BF16) for z in range(2)]
        atm = [kb.sbuf("atm%d" % z, [128, NT, 128], BF16) for z in range(2)]
        Sp = [kb.sbuf("Sp%d" % z, [128, 2 * NT, dv], BF16) for z in range(2)]
        for z in range(2):
            kb.op("pool", lambda z=z: nc.gpsimd.memset(atm[z][:], 0.0), writes=[atm[z]])
        Sst = [kb.sbuf("Sst%d" % i, [128, dv], F32) for i in range(2)]
        R = 3
        eq = [kb.sbuf("eq%d" % i, [128, 128], F32) for i in range(R)]
        ek = [kb.sbuf("ek%d" % i, [128, 128], F32) for i in range(R)]
        ekh = [kb.sbuf("ekh%d" % i, [128, 128], F32) for i in range(R)]
        ktl = [kb.sbuf("ktl%d" % i, [128, 128], BF16) for i in range(R)]
        khat = [kb.sbuf("khat%d" % i, [128, 256], BF16) for i in range(R)]
        ktr = [kb.sbuf("ktr%d" % i, [128, 128], F32) for i in range(R)]
        atr = [kb.sbuf("atr%d" % i, [128, 128], F32) for i in range(R)]
        usb = [kb.sbuf("usb%d" % i, [128, 256], F32) for i in range(R)]
        osb = [kb.sbuf("osb%d" % i, [128, 512], F32) for i in range(2)]
        cm = kb.sbuf("cm", [128, 2], F32)
        kb.op("dve", lambda: nc.vector.memset(cm[:], 0.0), writes=[cm])
        kb.op("dve", lambda: nc.vector.memset(cm[0:64, 0:1], 1.0), writes=[cm])
        kb.op("dve", lambda: nc.vector.memset(cm[64:128, 1:2], 1.0), writes=[cm])
        er2 = [kb.sbuf("er%d" % i, [128, 2], F32) for i in range(R)]
        ed2 = [kb.sbuf("ed%d" % i, [128, 2], F32) for i in range(R)]
        psA = [kb.psum("psA%d" % i, [128, 512]) for i in range(2)]
        psB = [kb.psum("psB%d" % i, [128, 512]) for i in range(2)]
        psA_ec = [view(p) for p in psA]
        import os
        if True:
            psK = [kb.psum("psK%d" % i, [128, 512]) for i in range(2)]
        else:
            psK = psA
        psA_kt = [view(p) for p in psK]
        psA_e2 = [view(p) for p in psK]
        psB_u = [view(p) for p in psB]
        psB_at = [view(p) for p in psB]
        psO = [kb.psum("psO%d" % i, [128, 512]) for i in range(1)] * 2
        psM = [kb.psum("psM%d" % i, [128, 512]) for i in range(1)] * 2
        sq = [kb.sbuf("sq%d" % i, [128, 512], F32) for i in range(2)]
        sd = [kb.sbuf("sd%d" % i, [128, 512], F32) for i in range(2)]
        on = [kb.sbuf("on%d" % i, [128, 512], F32) for i in range(2)]
        eps_t = kb.sbuf("eps_t", [128, 1], F32)
        kb.op("dve", lambda: nc.vector.memset(eps_t[:], 1e-6), writes=[eps_t])

        it = 0
        ghz = 0
        import os
        for h in (range(H) if heads is None else heads):
            vc0 = cfg["vcol0"] + h * dv
            kb.dma("sp", vt[:], vsrc[:, :, vc0:vc0 + dv], reads=[vsrc], writes=[vt])
            kb.dma("sp", qT[:dk, :], qsrc[h * dk:(h + 1) * dk, :], reads=[qsrc], writes=[qT])
            kb.dma("sp", gsil[:], gate[h * dv:(h + 1) * dv, :], reads=[gate], writes=[gsil])
            for z in range(2):
                gt = gts[ghz % 2]
                kt = kts[ghz % 2]
                ghz += 1
                gc0 = z * cfg["gz"] + h * dk
                kc0 = cfg["kcol0"] + z * cfg["kz"] + h * dk
                kb.dma("sp", gt[:], gsrc[:, :, gc0:gc0 + dk], reads=[gsrc], writes=[gt])
                if os.environ.get("KTMEMSET"):
                    kb.op("dve", lambda kt=kt: nc.vector.memset(kt[:], 0.0), writes=[kt])
                kb.dma("sp", kt[:], ksrc[:, :, kc0:kc0 + dk], reads=[ksrc], writes=[kt])
                order = [32, 33] + list(range(32)) if z == 0 else [33, 32] + list(range(31, -1, -1))
                import os
                DBG = os.environ.get("SCAN_DBG", "")
                if DBG == "loads":
                    continue
                if DBG.startswith("nt"):
                    order = order[:int(DBG[2:])]
                S = Sst[0]
                kb.op("dve", lambda S=S: nc.vector.memset(S[:], 0.0), writes=[S])
                si = 0
                def p1(ti, a, r):
                    ts = slice(ti * 128, (ti + 1) * 128)
                    pa, pb = psA[a], psB[a]
                    pk = psK[a]
                    kb.mm([lambda pa=pa, gt=gt: nc.tensor.matmul(pa[:dk, 0:130], lhsT=gt[:, ti, :], rhs=mq[:, z, :],
                                                                 start=True, stop=True)],
                          reads=[gt, mq], writes=[psA_ec[a]])
                    kb.mm([lambda pk=pk, kt=kt: nc.tensor.matmul(pk[:dk, 256:384], lhsT=(ident[:, :dk] if os.environ.get("A2ID") else kt[:, ti, :]), rhs=ident[:],
                                                                 start=True, stop=True)],
                          reads=[kt, ident], writes=[psA_kt[a]])
                    kb.mm([lambda pk=pk, gt=gt: nc.tensor.matmul(pk[:, 384:384 + dk], lhsT=mk[:, z, :], rhs=gt[:, ti, :],
                                                                 start=True, stop=True)],
                          reads=[gt, mk], writes=[psA_e2[a]])
                    kb.op("act", lambda pa=pa, o=eq[r]: nc.scalar.activation(out=o[:dk, :], in_=pa[:dk, 0:128], func=AF.Exp),
                          reads=[psA_ec[a]], writes=[eq[r]])
                    kb.op("act", lambda pa=pa, o=ek[r]: nc.scalar.activation(out=o[:dk, :], in_=pa[:dk, 0:128], func=AF.Exp, scale=-1.0),
                          reads=[psA_ec[a]], writes=[ek[r]])
                    kb.op("act", lambda pa=pa, o=er2[r]: nc.scalar.activation(out=o[:dk, :], in_=pa[:dk, 128:130], func=AF.Exp, scale=0.5),
                          reads=[psA_ec[a]], writes=[er2[r]])
                    kb.op("act", lambda pa=pa, o=ed2[r]: nc.scalar.activation(out=o[:dk, :], in_=pa[:dk, 128:130], func=AF.Exp),
                          reads=[psA_ec[a]], writes=[ed2[r]])
                    kb.op("act", lambda pk=pk, o=ekh[r]: nc.scalar.activation(out=o[:, :dk], in_=pk[:, 384:384 + dk], func=AF.Exp),
                          reads=[psA_e2[a]], writes=[ekh[r]])
                    qz = qt[z]
                    kb.op("dve", lambda qz=qz, e=eq[r]: nc.vector.scalar_tensor_tensor(
                        out=qz[:dk, ts], in0=qT[:dk, ts], scalar=cfg["qscale"], in1=e[:dk, :], op0=ALU.mult, op1=ALU.mult),
                        reads=[qT, eq[r]], writes=[], acc_writes=[qz])
                    kb.op("act", lambda pk=pk, o=ktr[r]: nc.scalar.copy(out=o[:dk, :], in_=pk[:dk, 256:384]),
                          reads=[psA_kt[a]], writes=[ktr[r]])
                    kb.op("dve", lambda e=ek[r], o=ktl[r], kr=ktr[r]: nc.vector.tensor_tensor(
                        out=o[:dk, :], in0=kr[:dk, :], in1=e[:dk, :], op=ALU.mult),
                        reads=[ktr[r], ek[r]], writes=[ktl[r]])
                    for c in range(2):
                        kb.op("dve", lambda kt=kt, e=ekh[r], o=khat[r], c=c: nc.vector.scalar_tensor_tensor(
                            out=o[:, c * 128:c * 128 + dk], in0=kt[:, ti, :], scalar=cm[:, c:c + 1], in1=e[:, :dk],
                            op0=ALU.mult, op1=ALU.mult),
                            reads=[kt, ekh[r], cm], writes=[], acc_writes=[khat[r]])
                    return None

                def p2(ti, a, r, S, si):
                    ts = slice(ti * 128, (ti + 1) * 128)
                    pa, pb = psA[a], psB[a]
                    qz = qt[z]
                    kb.mm([(lambda c=c, pb=pb, kh=khat[r]: nc.tensor.matmul(
                        pb[:dk, c * 128:(c + 1) * 128], lhsT=kh[:, c * 128:c * 128 + dk],
                        rhs=vt[:, ti, :], start=True, stop=True)) for c in range(2)],
                        reads=[khat[r], vt], writes=[psB_u[a]])
                    kb.mm([lambda pb=pb, k_=ktl[r], qz=qz: nc.tensor.matmul(
                        pb[:, 256:384], lhsT=k_[:dk, :], rhs=qz[:dk, ts], start=True, stop=True)],
                        reads=[ktl[r], qz], writes=[psB_at[a]])
                    az = atm[z]
                    kb.op("act", lambda pb=pb, o=atr[r]: nc.scalar.copy(out=o[:], in_=pb[:, 256:384]),
                          reads=[psB_at[a]], writes=[atr[r]])
                    kb.op("dve", lambda az=az, o=atr[r]: nc.vector.copy_predicated(
                        out=az[:, ti, :], mask=mau[:, z, :], data=o[:]),
                        reads=[atr[r], mau], writes=[], acc_writes=[az])
                    kb.op("act", lambda pb=pb, o=usb[r]: nc.scalar.copy(out=o[:dk, :], in_=pb[:dk, 0:256]),
                          reads=[psB_u[a]], writes=[usb[r]])
                    spz = Sp[z]
                    for c in ((0, 1) if z == 0 else (1, 0)):
                        ci = 2 * ti + c
                        kb.op("act", lambda S=S, spz=spz, ci=ci, e=er2[r], c=c: nc.scalar.activation(
                            out=spz[:dk, ci, :], in_=S[:dk, :], func=AF.Copy, scale=e[:dk, c:c + 1]),
                            reads=[S, er2[r]], writes=[], acc_writes=[spz])
                        S2 = Sst[(si + 1) % 2]
                        si += 1
                        kb.op("dve", lambda S=S, S2=S2, e=ed2[r], u=usb[r], c=c: nc.vector.scalar_tensor_tensor(
                            out=S2[:dk, :], in0=S[:dk, :], scalar=e[:dk, c:c + 1], in1=u[:dk, c * 128:(c + 1) * 128],
                            op0=ALU.mult, op1=ALU.add),
                            reads=[S, ed2[r], usb[r]], writes=[S2])
                        S = S2
                    return S, si

                sched = []
                for ti in order:
                    sched.append((ti, it % 2, it % R))
                    it += 1
                for idx in range(len(sched) + 1):
                    if idx < len(sched):
                        p1(*sched[idx])
                    if idx >= 1:
                        S, si = p2(*sched[idx - 1], S, si)
            groups = [list(range(g * 4, g * 4 + 4)) for g in range(8)] + [[32, 33]]
            if DBG:
                groups = []
            for gi, tiles in enumerate(groups):
                po = psO[gi % 2]
                pm = psM[gi % 2]
                nb = len(tiles) * 128
                t0 = tiles[0] * 128
                fns = []
                for k_, ti in enumerate(tiles):
                    cs = k_ * 128
                    for z in range(2):
                        fns.append(lambda po=po, ti=ti, cs=cs, z=z: nc.tensor.matmul(
                            po[:, cs:cs + 128], lhsT=vt[:, ti, :], rhs=atm[z][:, ti, :], start=(z == 0), stop=False))
                        for c in range(2):
                            fns.append(lambda po=po, ti=ti, cs=cs, z=z, c=c: nc.tensor.matmul(
                                po[:, cs + c * 64:cs + (c + 1) * 64], lhsT=Sp[z][:dk, 2 * ti + c, :],
                                rhs=qt[z][:dk, ti * 128 + c * 64:ti * 128 + (c + 1) * 64],
                                start=False, stop=(z == 1)))
                kb.mm(fns, reads=[vt, atm[0], atm[1], Sp[0], Sp[1], qt[0], qt[1]], writes=[po])
                s_, d_, o_ = sq[gi % 2], sd[gi % 2], on[gi % 2]
                kb.op("act", lambda po=po, s_=s_: nc.scalar.activation(out=s_[:, :nb], in_=po[:, :nb], func=AF.Square),
                      reads=[po], writes=[s_])
                kb.mm([lambda pm=pm, s_=s_: nc.tensor.matmul(pm[:, :nb], lhsT=onesdv[:], rhs=s_[:, :nb], start=True, stop=True)],
                      reads=[onesdv, s_], writes=[pm])
                kb.op("act", lambda pm=pm, d_=d_: nc.scalar.activation(out=d_[:, :nb], in_=pm[:, :nb], func=AF.Sqrt, bias=eps_t[:]),
                      reads=[pm, eps_t], writes=[d_])
                kb.op("dve", lambda d_=d_: nc.vector.reciprocal(out=d_[:, :nb], in_=d_[:, :nb]), reads=[d_], writes=[d_])
                ob_ = osb[gi % 2]
                kb.op("act", lambda po=po, ob_=ob_: nc.scalar.copy(out=ob_[:, :nb], in_=po[:, :nb]), reads=[po], writes=[ob_])
                kb.op("dve", lambda ob_=ob_, d_=d_, o_=o_: nc.vector.tensor_tensor(
                    out=o_[:, :nb], in0=ob_[:, :nb], in1=d_[:, :nb], op=ALU.mult), reads=[ob_, d_], writes=[o_])
                kb.op("pool", lambda o_=o_: nc.gpsimd.tensor_tensor(
                    out=obr[:, t0:t0 + nb], in0=o_[:, :nb], in1=gsil[:, t0:t0 + nb], op=ALU.mult),
                    reads=[o_, gsil], writes=[], acc_writes=[obr])
            kb.dma("sp", dest[h * dv:(h + 1) * dv, :], obr[:], reads=[obr], writes=[], acc_writes=[dest])


def scan_masks():
    j = np.arange(128)[:, None]
    i = np.arange(128)[None, :]
    same = (j // 64 == i // 64).astype(np.float32)
    out = {}
    for name, gs in (("mg", -1.0 / 16.0), ("mh", 1.0)):
        mq = np.zeros((128, 2, 130), np.float32)
        mk = np.zeros((128, 2, 128), np.float32)
        mq[:, 0, :128] = gs * same * ((j <= i).astype(np.float32) - 0.5)
        mq[:, 1, :128] = gs * same * ((j >= i).astype(np.float32) - 0.5)
        for z in range(2):
            for c in range(2):
                mq[:, z, 128 + c] = gs * (np.arange(128) // 64 == c)
        mk[:, 0, :] = gs * same * (j > i)
        mk[:, 1, :] = gs * same * (j < i)
        out[name + "_mq"] = mq
        out[name + "_mk"] = mk
    ma = np.zeros((128, 2, 128), np.float32)
    ma[:, 0, :] = same * (j <= i)
    ma[:, 1, :] = same * (j >= i)
    out["m_ma"] = ma
    out["m_mau"] = ma.astype(np.uint32)
    out["m_ident"] = np.eye(128, dtype=np.float32)
    return out


def stage_wa(cx, kb, nc, l, groups=None):
    with kb.scope():
        wm = kb.sbuf("wm", [128, 2, 128], BF16)
        kb.dma("pool", wm[:], cx.m_wa[:], reads=[cx.m_wa], writes=[wm])
        ones64 = kb.sbuf("ones64", [128, 64], BF16)
        kb.op("dve", lambda: nc.vector.memset(ones64[:], 1.0), writes=[ones64])
        sk = kb.sbuf("sk", [64, 8], F32)
        kb.dma("sp", sk[:], cx.wa_sink[l].partition_broadcast(64), reads=[cx.wa_sink], writes=[sk])
        kb.op("act", lambda: nc.scalar.activation(out=sk[:], in_=sk[:], func=AF.Exp), reads=[sk], writes=[sk])
        q4 = kb.sbuf("q4", [64, 4, T], BF16)
        kT = kb.sbuf("kTc", [64, T], BF16)
        vt = kb.sbuf("vtc", [128, NT, 64], BF16)
        ost = kb.sbuf("ost", [64, 4, T], BF16)
        pS = [kb.psum("pS%d" % i, [128, 512]) for i in range(2)]
        pO = [kb.psum("pO%d" % i, [128, 512]) for i in range(2)]
        pD = [kb.psum("pD%d" % i, [128, 512]) for i in range(2)]
        R = 3
        pt = [kb.sbuf("pt%d" % i, [128, 4, 128], BF16) for i in range(R)]
        ptm = [kb.sbuf("ptm%d" % i, [128, 4, 128], BF16) for i in range(R)]
        osb = [kb.sbuf("osbw%d" % i, [64, 4, 128], F32) for i in range(2)]
        dsb = [kb.sbuf("dsbw%d" % i, [64, 4, 128], F32) for i in range(2)]
        it = 0
        for g in (range(2) if groups is None else groups):
            kb.dma("sp", q4[:], cx.QC[g * 256:(g + 1) * 256, :].rearrange("(hh d) t -> d hh t", d=64),
                   reads=[cx.QC], writes=[q4])
            kb.dma("sp", kT[:], cx.KC[g * 64:(g + 1) * 64, :], reads=[cx.KC], writes=[kT])
            kb.dma("sp", vt[:], cx.VC_t[:, :, g * 64:(g + 1) * 64], reads=[cx.VC_t], writes=[vt])
            for qb in range(NT):
                if qb < 32:
                    keys = []
                    if qb > 0:
                        keys.append((qb - 1, 0))
                    keys.append((qb, None))
                    if qb < 31:
                        keys.append((qb + 1, 1))
                    keys += [(32, None), (33, None)]
                else:
                    keys = [(32, None), (33, None)]
                qs = slice(qb * 128, (qb + 1) * 128)
                a = qb % 2
                po, pd = pO[a], pD[a]
                nk = len(keys)
                for ki, (kt_i, mi) in enumerate(keys):
                    ps = pS[it % 2]
                    p_ = pt[it % R]
                    pm_ = ptm[it % R]
                    it += 1
                    kb.mm([lambda ps=ps, kt_i=kt_i: nc.tensor.matmul(
                        ps[:, :], lhsT=kT[:, kt_i * 128:(kt_i + 1) * 128], rhs=q4[:, :, qs], start=True, stop=True)],
                        reads=[kT, q4], writes=[ps])
                    kb.op("act", lambda ps=ps, p_=p_: nc.scalar.activation(
                        out=p_[:].rearrange("p h q -> p (h q)"), in_=ps[:, :], func=AF.Exp, scale=0.125),
                        reads=[ps], writes=[p_])
                    src = p_
                    if mi is not None:
                        eng = "dve" if it % 2 == 0 else "pool"
                        h_ = nc.vector if eng == "dve" else nc.gpsimd
                        kb.op(eng, lambda h_=h_, p_=p_, pm_=pm_, mi=mi: h_.tensor_tensor(
                            out=pm_[:], in0=p_[:], in1=wm[:, mi, :].unsqueeze(1).broadcast_to([128, 4, 128]), op=ALU.mult),
                            reads=[p_, wm], writes=[pm_])
                        src = pm_
                    kb.mm([lambda po=po, src=src, kt_i=kt_i, ki=ki: nc.tensor.matmul(
                        po[:64, :], lhsT=vt[:, kt_i, :], rhs=src[:].rearrange("p h q -> p (h q)"),
                        start=(ki == 0), stop=(ki == nk - 1)),
                        lambda pd=pd, src=src, ki=ki: nc.tensor.matmul(
                        pd[:64, :], lhsT=ones64[:], rhs=src[:].rearrange("p h q -> p (h q)"),
                        start=(ki == 0), stop=(ki == nk - 1))],
                        reads=[vt, src, ones64], writes=[po, pd])
                o_, d_ = osb[a], dsb[a]
                kb.op("act", lambda po=po, o_=o_: nc.scalar.copy(out=o_[:].rearrange("p h q -> p (h q)"), in_=po[:64, :]),
                      reads=[po], writes=[o_])
                kb.op("act", lambda pd=pd, d_=d_: nc.scalar.copy(out=d_[:].rearrange("p h q -> p (h q)"), in_=pd[:64, :]),
                      reads=[pd], writes=[d_])
                kb.op("dve", lambda d_=d_: nc.vector.tensor_tensor(
                    out=d_[:], in0=d_[:], in1=sk[:, g * 4:(g + 1) * 4].unsqueeze(2).broadcast_to([64, 4, 128]), op=ALU.add),
                    reads=[d_, sk], writes=[d_])
                kb.op("dve", lambda d_=d_: nc.vector.reciprocal(out=d_[:], in_=d_[:]), reads=[d_], writes=[d_])
                kb.op("pool", lambda o_=o_, d_=d_: nc.gpsimd.tensor_tensor(
                    out=ost[:, :, qs], in0=o_[:], in1=d_[:], op=ALU.mult),
                    reads=[o_, d_], writes=[], acc_writes=[ost])
            kb.dma("sp", cx.BR_C[g * 256:(g + 1) * 256, :].rearrange("(hh d) t -> d hh t", d=64), ost[:],
                   reads=[ost], writes=[], acc_writes=[cx.BR_C])


def wa_masks():
    kk = np.arange(128)[:, None]
    qq = np.arange(128)[None, :]
    m = np.zeros((128, 2, 128), np.float32)
    m[:, 0, :] = (kk >= qq)
    m[:, 1, :] = (kk <= qq)
    return {"m_wa": m}


def na_patterns():
    W = 64
    tok = np.arange(128)
    lr, lc = tok // W, tok % W
    pats = []
    keys = []
    for m in range(32):
        qr = 2 * m + lr[None, :]
        qc = lc[None, :]
        rs = np.clip(qr - 4, 0, 56)
        cs = np.clip(qc - 8, 0, 48)
        lst = []
        for kt_i in range(32):
            kr = 2 * kt_i + lr[:, None]
            kc = lc[:, None]
            valid = (kr >= rs) & (kr < rs + 8) & (kc >= cs) & (kc < cs + 16)
            if not valid.any():
                continue
            ri = np.clip(kr - qr + 7, 0, 14)
            ci = np.clip(kc - qc + 15, 0, 30)
            if 2 <= m <= 29:
                pid = kt_i - m + 2
                if m == 2:
                    pats.append((valid, ri, ci))
            else:
                pid = len(pats)
                pats.append((valid, ri, ci))
            lst.append((kt_i, pid))
        keys.append(lst)
    return keys, pats


def na_consts():
    keys, pats = na_patterns()
    n0 = len(keys[0]) + len(keys[1])
    order = list(range(n0, n0 + 5)) + list(range(0, n0)) + list(range(n0 + 5, len(pats)))
    remap = {old: new for new, old in enumerate(order)}
    pats2 = [pats[o] for o in order]
    keys2 = []
    for m, lst in enumerate(keys):
        if 2 <= m <= 29:
            keys2.append(lst)
        else:
            keys2.append([(k, remap[p]) for (k, p) in lst])
    valid = np.stack([p[0] for p in pats2], 1).astype(np.float32)
    ri = np.stack([p[1] for p in pats2], 1)
    ci = np.stack([p[2] for p in pats2], 1)
    return keys2, valid, ri, ci


def stage_na(cx, kb, nc, l, heads=None):
    keys_tab, valid_np, _, _ = na_consts()
    npat = valid_np.shape[1]
    with kb.scope():
        vmask = kb.sbuf("vmask", [128, npat, 128], F32)
        kb.dma("sp", vmask[:], cx.m_navalid[:], reads=[cx.m_navalid], writes=[vmask])
        ones64 = kb.sbuf("ones64n", [128, 64], BF16)
        kb.op("dve", lambda: nc.vector.memset(ones64[:], 1.0), writes=[ones64])
        qT = kb.sbuf("qTn", [64, T], BF16)
        kT = kb.sbuf("kTn", [64, T], BF16)
        vt = kb.sbuf("vtn", [128, NT, 64], BF16)
        ost = kb.sbuf("ostn", [64, T], BF16)
        bias = kb.sbuf("biasn", [128, npat, 128], F32)
        EB = kb.sbuf("EBn", [128, npat, 128], BF16)
        pS = [kb.psum("pSn%d" % i, [128, 512]) for i in range(4)]
        pO = [kb.psum("pOn%d" % i, [128, 512]) for i in range(2)]
        pD = [kb.psum("pDn%d" % i, [128, 512]) for i in range(2)]
        R = 4
        pt = [kb.sbuf("ptn%d" % i, [128, 4, 128], BF16) for i in range(R)]
        osb = [kb.sbuf("osbn%d" % i, [64, 256], F32) for i in range(2)]
        it = 0
        for h in (range(8) if heads is None else heads):
            kb.dma("sp", bias[:], cx.na_bias[l, h], reads=[cx.na_bias], writes=[bias])
            kb.op("act", lambda: nc.scalar.activation(out=bias[:], in_=bias[:], func=AF.Exp), reads=[bias], writes=[bias])
            kb.op("pool", lambda: nc.gpsimd.tensor_tensor(out=EB[:], in0=bias[:], in1=vmask[:], op=ALU.mult),
                  reads=[bias, vmask], writes=[EB])
            kb.dma("sp", qT[:], cx.QB[h * 64:(h + 1) * 64, :], reads=[cx.QB], writes=[qT])
            kb.dma("sp", kT[:], cx.KB_[h * 64:(h + 1) * 64, :], reads=[cx.KB_], writes=[kT])
            kb.dma("sp", vt[:], cx.VB_t[:, :, h * 64:(h + 1) * 64], reads=[cx.VB_t], writes=[vt])
            for qb in range(NT):
                if qb < 32:
                    keys = list(keys_tab[qb]) + [(32, None), (33, None)]
                else:
                    keys = [(32, None), (33, None)]
                qs = slice(qb * 128, (qb + 1) * 128)
                a = qb % 2
                po = pO[a]
                pd = pD[a]
                nk = len(keys)
                grp = [keys[i:i + 4] for i in range(0, nk, 4)]
                done = 0
                for gk in grp:
                    ps = pS[it % 4]
                    p_ = pt[it % R]
                    it += 1
                    ng = len(gk)
                    kb.mm([(lambda ps=ps, j=j, kt_i=kt_i: nc.tensor.matmul(
                        ps[:, j * 128:(j + 1) * 128], lhsT=kT[:, kt_i * 128:(kt_i + 1) * 128], rhs=qT[:, qs],
                        start=True, stop=True)) for j, (kt_i, _) in enumerate(gk)],
                        reads=[kT, qT], writes=[ps])
                    kb.op("act", lambda ps=ps, p_=p_, ng=ng: nc.scalar.activation(
                        out=p_[:, :ng, :].rearrange("p h q -> p (h q)"), in_=ps[:, :ng * 128], func=AF.Exp, scale=0.125),
                        reads=[ps], writes=[p_])
                    nloc = sum(1 for (_, pid) in gk if pid is not None)
                    if nloc:
                        pid0 = gk[0][1]
                        eng = "dve" if it % 2 == 0 else "pool"
                        h_ = nc.vector if eng == "dve" else nc.gpsimd
                        kb.op(eng, lambda h_=h_, p_=p_, pid0=pid0, nloc=nloc: h_.tensor_tensor(
                            out=p_[:, :nloc, :], in0=p_[:, :nloc, :], in1=EB[:, pid0:pid0 + nloc, :], op=ALU.mult),
                            reads=[p_, EB], writes=[p_])
                    fns = []
                    for j, (kt_i, _) in enumerate(gk):
                        first = (done == 0)
                        last = (done == nk - 1)
                        fns.append(lambda po=po, p_=p_, j=j, kt_i=kt_i, first=first, last=last: nc.tensor.matmul(
                            po[:64, 0:128], lhsT=vt[:, kt_i, :], rhs=p_[:, j, :], start=first, stop=last))
                        fns.append(lambda pd=pd, p_=p_, j=j, first=first, last=last: nc.tensor.matmul(
                            pd[:64, 0:128], lhsT=ones64[:], rhs=p_[:, j, :], start=first, stop=last))
                        done += 1
                    kb.mm(fns, reads=[vt, p_, ones64], writes=[po, pd])
                o_ = osb[a]
                kb.op("act", lambda po=po, o_=o_: nc.scalar.copy(out=o_[:, 0:128], in_=po[:64, 0:128]), reads=[po], writes=[o_])
                kb.op("act", lambda pd=pd, o_=o_: nc.scalar.copy(out=o_[:, 128:256], in_=pd[:64, 0:128]), reads=[pd, o_], writes=[o_])
                kb.op("dve", lambda o_=o_: nc.vector.reciprocal(out=o_[:, 128:256], in_=o_[:, 128:256]), reads=[o_], writes=[o_])
                kb.op("pool", lambda o_=o_: nc.gpsimd.tensor_tensor(
                    out=ost[:, qs], in0=o_[:, 0:128], in1=o_[:, 128:256], op=ALU.mult),
                    reads=[o_], writes=[], acc_writes=[ost])
            kb.dma("sp", cx.BR_B[h * 64:(h + 1) * 64, :], ost[:], reads=[ost], writes=[], acc_writes=[cx.BR_B])

LN_EPS = 1e-5
MB = 256
MBLKS = [(i * MB, MB) for i in range(T // MB)]


def ln_setup(cx, kb, nc):
    ones = kb.sbuf("ln_ones", [128, 128], F32)
    kb.op("dve", lambda: nc.vector.memset(ones[:], 1.0 / D), writes=[ones])
    eps = kb.sbuf("ln_eps", [128, 1], F32)
    kb.op("dve", lambda: nc.vector.memset(eps[:], LN_EPS), writes=[eps])
    return ones, eps


def ln_block(kb, nc, r, nb, gam, bet, out, ones, eps, scr):
    rsq, ps1, ps2, mean, var = scr
    kb.op("act", lambda: nc.scalar.activation(out=rsq[:, :, :nb], in_=r[:, :, :nb], func=AF.Square), reads=[r], writes=[rsq])
    kb.mm([(lambda oc=oc: nc.tensor.matmul(ps1[:, :nb], lhsT=ones[:], rhs=r[:, oc, :nb], start=(oc == 0), stop=(oc == 7)))
           for oc in range(8)], reads=[ones, r], writes=[ps1])
    kb.mm([(lambda oc=oc: nc.tensor.matmul(ps2[:, :nb], lhsT=ones[:], rhs=rsq[:, oc, :nb], start=(oc == 0), stop=(oc == 7)))
           for oc in range(8)], reads=[ones, rsq], writes=[ps2])
    kb.op("act", lambda: nc.scalar.copy(out=mean[:, :nb], in_=ps1[:, :nb]), reads=[ps1], writes=[mean])
    kb.op("act", lambda: nc.scalar.copy(out=var[:, :nb], in_=ps2[:, :nb]), reads=[ps2], writes=[var])
    m2 = rsq
    kb.op("dve", lambda: nc.vector.tensor_tensor(out=m2[:, 0, :nb], in0=mean[:, :nb], in1=mean[:, :nb], op=ALU.mult),
          reads=[mean, rsq], writes=[rsq])
    kb.op("dve", lambda: nc.vector.tensor_tensor(out=var[:, :nb], in0=var[:, :nb], in1=m2[:, 0, :nb], op=ALU.subtract),
          reads=[var, rsq], writes=[var])
    kb.op("act", lambda: nc.scalar.activation(out=var[:, :nb], in_=var[:, :nb], func=AF.Sqrt, bias=eps[:]),
          reads=[var, eps], writes=[var])
    kb.op("dve", lambda: nc.vector.reciprocal(out=var[:, :nb], in_=var[:, :nb]), reads=[var], writes=[var])
    kb.op("dve", lambda: nc.vector.tensor_tensor(
        out=r[:, :, :nb], in0=r[:, :, :nb], in1=mean[:, :nb].unsqueeze(1).broadcast_to([128, 8, nb]), op=ALU.subtract),
        reads=[r, mean], writes=[r])
    kb.op("pool", lambda: nc.gpsimd.tensor_tensor(
        out=r[:, :, :nb], in0=r[:, :, :nb], in1=var[:, :nb].unsqueeze(1).broadcast_to([128, 8, nb]), op=ALU.mult),
        reads=[r, var], writes=[r])
    for oc in range(8):
        eng = "dve" if oc % 2 == 0 else "pool"
        h_ = nc.vector if eng == "dve" else nc.gpsimd
        kb.op(eng, lambda h_=h_, oc=oc: h_.tensor_scalar(
            out=out[:, oc, :nb], in0=r[:, oc, :nb], scalar1=gam[:, oc:oc + 1], scalar2=bet[:, oc:oc + 1],
            op0=ALU.mult, op1=ALU.add), reads=[r, gam, bet], writes=[out])


def load_ln_params(cx, kb, nc, l, which):
    gam = kb.sbuf("ln_g", [128, 8], F32)
    bet = kb.sbuf("ln_b", [128, 8], F32)
    kb.dma("sp", gam[:], cx.ln_gT[l, which], reads=[cx.ln_gT], writes=[gam])
    kb.dma("sp", bet[:], cx.ln_bT[l, which], reads=[cx.ln_bT], writes=[bet])
    return gam, bet


def stage_merge(cx, kb, nc, l, xsrc, modv):
    with kb.scope():
        ones, eps = ln_setup(cx, kb, nc)
        gam, bet = load_ln_params(cx, kb, nc, l, 0)
        wbr = kb.sbuf("wbr", [128, 4, 4, 1024], BF16)
        wout = kb.sbuf("wout", [128, 8, 1024], BF16)
        for i in range(4):
            kb.dma("pool", wbr[:, i], cx.w_branch[l, i].rearrange("(kc p) c -> p kc c", p=128), reads=[cx.w_branch], writes=[wbr])
        kb.dma("pool", wout[:], cx.w_out[l].rearrange("(kc p) c -> p kc c", p=128), reads=[cx.w_out], writes=[wout])
        wr = kb.sbuf("wr", [128, 8, 36], F32)
        kb.dma("sp", wr[:, :, 0:4], cx.moe_w_group[l].rearrange("(kc p) c -> p kc c", p=128), reads=[cx.moe_w_group], writes=[wr])
        kb.dma("sp", wr[:, :, 4:36], cx.moe_w_expert[l].rearrange("(kc p) c -> p kc c", p=128), reads=[cx.moe_w_expert], writes=[wr])
        rb = kb.sbuf("rb", [128, 36], F32)
        kb.dma("sp", rb[:, 0:4], cx.moe_b_group[l].partition_broadcast(128), reads=[cx.moe_b_group], writes=[rb])
        kb.dma("sp", rb[:, 4:36], cx.moe_b_expert[l].partition_broadcast(128), reads=[cx.moe_b_expert], writes=[rb])
        onep4 = kb.sbuf("onep4", [128, 8, 2], F32)
        kb.op("dve", lambda: nc.vector.tensor_scalar(out=onep4[:], in0=modv[:, 32:40, :], scalar1=1.0, scalar2=None, op0=ALU.add),
              reads=[modv], writes=[onep4])
        brt = [kb.sbuf("brt%d" % i, [128, 4, 4, MB], BF16) for i in range(1)]
        gtt = kb.sbuf("gtt", [128, 32, MB], BF16)
        xb = kb.sbuf("xbm", [128, 8, MB], F32)
        mrg = kb.sbuf("mrg", [128, 8, MB], BF16)
        r = kb.sbuf("rm", [128, 8, MB], F32)
        hx2b = kb.sbuf("hx2b", [128, 8, MB], BF16)
        rsq = kb.sbuf("rsqm", [128, 8, MB], F32)
        x1 = r
        hx2f = rsq
        mean = kb.sbuf("meanm", [128, MB], F32)
        var = kb.sbuf("varm", [128, MB], F32)
        csb = [kb.sbuf("csb%d" % i, [128, MB], F32) for i in range(4)]
        acc = kb.sbuf("accm", [128, MB], F32)
        mixs = kb.sbuf("mixs", [128, MB], F32)
        pbr = [kb.psum("pbr%d" % i, [128, MB]) for i in range(4)]
        pmx = kb.psum("pmx", [128, MB])
        ps1 = kb.psum("ps1m", [128, MB])
        ps2 = kb.psum("ps2m", [128, MB])
        prt = kb.psum("prt", [128, MB])
        brs = [cx.BR_A, cx.BR_B, cx.BR_C, cx.BR_D]
        lgt = kb.sbuf("lgt", [128, 36], F32)
        gmx = kb.sbuf("gmx", [128, 1], F32)
        ngm = kb.sbuf("ngm", [128, 1], F32)
        ge = kb.sbuf("ge", [128, 4], F32)
        gs = kb.sbuf("gsr", [128, 1], F32)
        oh = kb.sbuf("oh", [128, 4], F32)
        lem = kb.sbuf("lem", [128, 4, 8], F32)
        mx8 = kb.sbuf("mx8", [128, 8], F32)
        dd = kb.sbuf("dd", [128, 1], F32)
        w1 = kb.sbuf("w1", [128, 1], F32)
        w2 = kb.sbuf("w2", [128, 1], F32)
        tt1 = kb.sbuf("tt1", [128, 32], F32)
        tt2 = kb.sbuf("tt2", [128, 32], F32)
        wgt = kb.sbuf("wgt", [128, 32], F32)
        for bi, (t0, nb) in enumerate(MBLKS):
            col = blkcol(t0)
            b_ = brt[0]
            for i in range(4):
                kb.dma("sp", b_[:, i, :, :nb], brs[i][:, t0:t0 + nb].rearrange("(kc p) t -> p kc t", p=128),
                       reads=[brs[i]], writes=[b_])
            kb.dma("sp", gtt[:, :, :nb], cx.GATE[:, t0:t0 + nb].rearrange("(j p) t -> p j t", p=128), reads=[cx.GATE], writes=[gtt])
            kb.dma("sp", xb[:, :, :nb], xsrc[:, t0:t0 + nb].rearrange("(kc p) t -> p kc t", p=128), reads=[xsrc], writes=[xb])
            for oc in range(8):
                for i in range(4):
                    kb.mm([(lambda i=i, kc=kc, oc=oc: nc.tensor.matmul(
                        pbr[i][:, :nb], lhsT=wbr[:, i, kc, oc * 128:(oc + 1) * 128], rhs=b_[:, i, kc, :nb],
                        start=(kc == 0), stop=(kc == 3))) for kc in range(4)], reads=[wbr, b_], writes=[pbr[i]])
                    kb.op("act", lambda i=i: nc.scalar.copy(out=csb[i][:, :nb], in_=pbr[i][:, :nb]), reads=[pbr[i]], writes=[csb[i]])
                    eng = "dve" if i % 2 == 0 else "pool"
                    h_ = nc.vector if eng == "dve" else nc.gpsimd
                    kb.op(eng, lambda h_=h_, i=i, oc=oc: h_.tensor_tensor(
                        out=csb[i][:, :nb], in0=csb[i][:, :nb], in1=gtt[:, i * 8 + oc, :nb], op=ALU.mult),
                        reads=[csb[i], gtt], writes=[csb[i]])
                kb.op("dve", lambda: nc.vector.tensor_tensor(out=acc[:, :nb], in0=csb[0][:, :nb], in1=csb[1][:, :nb], op=ALU.add),
                      reads=[csb[0], csb[1]], writes=[acc])
                kb.op("pool", lambda: nc.gpsimd.tensor_tensor(out=csb[2][:, :nb], in0=csb[2][:, :nb], in1=csb[3][:, :nb], op=ALU.add),
                      reads=[csb[2], csb[3]], writes=[csb[2]])
                kb.op("dve", lambda oc=oc: nc.vector.tensor_tensor(out=mrg[:, oc, :nb], in0=acc[:, :nb], in1=csb[2][:, :nb], op=ALU.add),
                      reads=[acc, csb[2]], writes=[mrg])
            for oc in range(8):
                kb.mm([(lambda kc=kc, oc=oc: nc.tensor.matmul(
                    pmx[:, :nb], lhsT=wout[:, kc, oc * 128:(oc + 1) * 128], rhs=mrg[:, kc, :nb],
                    start=(kc == 0), stop=(kc == 7))) for kc in range(8)], reads=[wout, mrg], writes=[pmx])
                kb.op("act", lambda: nc.scalar.copy(out=mixs[:, :nb], in_=pmx[:, :nb]), reads=[pmx], writes=[mixs])
                kb.op("pool", lambda oc=oc: nc.gpsimd.tensor_scalar(
                    out=mixs[:, :nb], in0=mixs[:, :nb], scalar1=modv[:, 16 + oc, col:col + 1], scalar2=None, op0=ALU.mult),
                    reads=[mixs, modv], writes=[mixs])
                kb.op("dve", lambda oc=oc: nc.vector.scalar_tensor_tensor(
                    out=r[:, oc, :nb], in0=xb[:, oc, :nb], scalar=ALPHA, in1=mixs[:, :nb], op0=ALU.mult, op1=ALU.add),
                    reads=[xb, mixs], writes=[r])
            ln_block(kb, nc, r, nb, gam, bet, x1, ones, eps, (rsq, ps1, ps2, mean, var))
            kb.dma("sp", cx.XT[:, t0:t0 + nb].rearrange("(kc p) t -> p kc t", p=128), x1[:, :, :nb],
                   reads=[x1], writes=[], acc_writes=[cx.XT])
            for oc in range(8):
                eng = "dve" if oc % 2 == 0 else "pool"
                h_ = nc.vector if eng == "dve" else nc.gpsimd
                kb.op(eng, lambda h_=h_, oc=oc: h_.tensor_scalar(
                    out=hx2f[:, oc, :nb], in0=x1[:, oc, :nb], scalar1=onep4[:, oc, col:col + 1],
                    scalar2=modv[:, 24 + oc, col:col + 1], op0=ALU.mult, op1=ALU.add),
                    reads=[x1, onep4, modv], writes=[hx2f])
            kb.op("act", lambda: nc.scalar.copy(out=hx2b[:, :, :nb], in_=hx2f[:, :, :nb]), reads=[hx2f], writes=[hx2b])
            kb.dma("sp", cx.HX2[:, t0:t0 + nb].rearrange("(kc p) t -> p kc t", p=128), hx2b[:, :, :nb],
                   reads=[hx2b], writes=[], acc_writes=[cx.HX2])
            for tj in range(nb // 128):
                ti = t0 // 128 + tj
                tsl = slice(tj * 128, (tj + 1) * 128)
                kb.mm([(lambda kc=kc: nc.tensor.matmul(prt[:, 0:36], lhsT=hx2f[:, kc, tsl], rhs=wr[:, kc, :],
                                                       start=(kc == 0), stop=(kc == 7))) for kc in range(8)],
                      reads=[hx2f, wr], writes=[prt])
                kb.op("act", lambda: nc.scalar.copy(out=lgt[:], in_=prt[:, 0:36]), reads=[prt], writes=[lgt])
                kb.op("dve", lambda: nc.vector.tensor_tensor(out=lgt[:], in0=lgt[:], in1=rb[:], op=ALU.add), reads=[lgt, rb], writes=[lgt])
                kb.op("dve", lambda: nc.vector.tensor_reduce(out=gmx[:], in_=lgt[:, 0:4], axis=AX.X, op=ALU.max), reads=[lgt], writes=[gmx])
                kb.op("dve", lambda: nc.vector.tensor_scalar(out=ngm[:], in0=gmx[:], scalar1=-1.0, scalar2=None, op0=ALU.mult), reads=[gmx], writes=[ngm])
                kb.op("act", lambda: nc.scalar.activation(out=ge[:], in_=lgt[:, 0:4], func=AF.Exp, bias=ngm[:]), reads=[lgt, ngm], writes=[ge])
                kb.op("dve", lambda: nc.vector.tensor_reduce(out=gs[:], in_=ge[:], axis=AX.X, op=ALU.add), reads=[ge], writes=[gs])
                kb.op("dve", lambda: nc.vector.reciprocal(out=gs[:], in_=gs[:]), reads=[gs], writes=[gs])
                kb.op("dve", lambda: nc.vector.tensor_scalar(out=oh[:], in0=lgt[:, 0:4], scalar1=gmx[:], scalar2=None, op0=ALU.is_equal), reads=[lgt, gmx], writes=[oh])
                kb.op("dve", lambda: nc.vector.tensor_scalar(out=oh[:], in0=oh[:], scalar1=-1.0, scalar2=1e30, op0=ALU.add, op1=ALU.mult), reads=[oh], writes=[oh])
                kb.op("dve", lambda: nc.vector.tensor_tensor(
                    out=lem[:], in0=lgt[:, 4:36].rearrange("p (g e) -> p g e", e=8), in1=oh[:].unsqueeze(2).broadcast_to([128, 4, 8]), op=ALU.add),
                    reads=[lgt, oh], writes=[lem])
                lemf = lem[:].rearrange("p g e -> p (g e)")
                kb.op("dve", lambda: nc.vector.max(out=mx8[:], in_=lem[:].rearrange("p g e -> p (g e)")), reads=[lem], writes=[mx8])
                kb.op("dve", lambda: nc.vector.tensor_tensor(out=dd[:], in0=mx8[:, 1:2], in1=mx8[:, 0:1], op=ALU.subtract), reads=[mx8], writes=[dd])
                kb.op("act", lambda: nc.scalar.activation(out=dd[:], in_=dd[:], func=AF.Exp), reads=[dd], writes=[dd])
                kb.op("dve", lambda: nc.vector.tensor_scalar(out=w1[:], in0=dd[:], scalar1=1.0, scalar2=None, op0=ALU.add), reads=[dd], writes=[w1])
                kb.op("dve", lambda: nc.vector.reciprocal(out=w1[:], in_=w1[:]), reads=[w1], writes=[w1])
                kb.op("dve", lambda: nc.vector.tensor_tensor(out=w2[:], in0=dd[:], in1=w1[:], op=ALU.mult), reads=[dd, w1], writes=[w2])
                kb.op("dve", lambda: nc.vector.tensor_scalar(
                    out=tt1[:], in0=lem[:].rearrange("p g e -> p (g e)"), scalar1=mx8[:, 0:1], scalar2=w1[:], op0=ALU.is_equal, op1=ALU.mult),
                    reads=[lem, mx8, w1], writes=[tt1])
                kb.op("dve", lambda: nc.vector.tensor_scalar(
                    out=tt2[:], in0=lem[:].rearrange("p g e -> p (g e)"), scalar1=mx8[:, 1:2], scalar2=w2[:], op0=ALU.is_equal, op1=ALU.mult),
                    reads=[lem, mx8, w2], writes=[tt2])
                kb.op("dve", lambda: nc.vector.tensor_tensor(out=tt1[:], in0=tt1[:], in1=tt2[:], op=ALU.add), reads=[tt1, tt2], writes=[tt1])
                kb.op("dve", lambda: nc.vector.tensor_scalar(out=wgt[:], in0=tt1[:], scalar1=gs[:], scalar2=None, op0=ALU.mult), reads=[tt1, gs], writes=[wgt])
                kb.dma("sp", cx.WG_t[:, ti, :], wgt[:], reads=[wgt], writes=[], acc_writes=[cx.WG_t])


def stage_moe(cx, kb, nc, l, modv, yout=None):
    with kb.scope():
        ones, eps = ln_setup(cx, kb, nc)
        gam, bet = load_ln_params(cx, kb, nc, l, 1)
        wgT = kb.sbuf("wgT", [32, T], F32)
        wgl = kb.sbuf("wgl", [128, NT, 32], F32)
        id32 = kb.sbuf("id32", [128, 128], F32)
        sel = kb.sbuf("sel", [32, 32, 128], F32)
        kb.dma("sp", wgl[:], cx.WG_t[:], reads=[cx.WG_t], writes=[wgl])
        kb.dma("sp", id32[:], cx.m_ident32[:], reads=[cx.m_ident32], writes=[id32])
        kb.dma("sp", sel[:], cx.m_sel[:], reads=[cx.m_sel], writes=[sel])
        ptr = [kb.psum("ptr%d" % i, [128, 512]) for i in range(2)]
        for ti in range(NT):
            p_ = ptr[ti % 2]
            kb.mm([lambda p_=p_, ti=ti: nc.tensor.matmul(p_[:32, 0:128], lhsT=wgl[:, ti, :], rhs=id32[:], start=True, stop=True)],
                  reads=[wgl, id32], writes=[p_])
            kb.op("act", lambda p_=p_, ti=ti: nc.scalar.copy(out=wgT[:, ti * 128:(ti + 1) * 128], in_=p_[:32, 0:128]),
                  reads=[p_], writes=[], acc_writes=[wgT])
        precast_expert_weights(cx, kb, nc, l)
        hx = kb.sbuf("hxm", [128, 8, 512], BF16)
        yacc = kb.sbuf("yacc", [128, 8, 512], F32)
        hid = kb.sbuf("hid", [128, 16, 512], BF16)
        wdg = kb.sbuf("wdg", [128, 8, 2, 1024], BF16)
        wgs = [kb.sbuf("wgs%d" % i, [128, 8, 256], BF16) for i in range(2)]
        wus = [kb.sbuf("wus%d" % i, [128, 8, 256], BF16) for i in range(2)]
        xb = kb.sbuf("xbe", [128, 8, 512], F32)
        rsq = kb.sbuf("rsqe", [128, 8, 512], F32)
        mean = kb.sbuf("meane", [128, 512], F32)
        var = kb.sbuf("vare", [128, 512], F32)
        reps = [kb.sbuf("reps%d" % i, [128, 512], F32) for i in range(2)]
        ssb = [kb.sbuf("ssb%d" % i, [128, 512], F32) for i in range(2)]
        tsb = [kb.sbuf("tsb%d" % i, [128, 512], F32) for i in range(2)]
        ytmp = [kb.sbuf("ytmp%d" % i, [128, 512], F32) for i in range(2)]
        pg = [kb.psum("pg%d" % i, [128, 512]) for i in range(2)]
        pu = [kb.psum("pu%d" % i, [128, 512]) for i in range(2)]
        py = ptr
        ps1 = kb.psum("ps1e", [128, 512])
        ps2 = kb.psum("ps2e", [128, 512])
        prep = ps1
        nw = 0
        it = 0
        for bi, (t0, nb) in enumerate(BLKS):
            col = blkcol(t0)
            kb.dma("sp", hx[:, :, :nb], cx.HX2[:, t0:t0 + nb].rearrange("(kc p) t -> p kc t", p=128), reads=[cx.HX2], writes=[hx])
            kb.dma("sp", xb[:, :, :nb], cx.XT[:, t0:t0 + nb].rearrange("(kc p) t -> p kc t", p=128), reads=[cx.XT], writes=[xb])
            for g in range(4):
                kb.dma("sp", wdg[:].rearrange("p e hc d -> p (e hc d)"), cx.WDB[g], reads=[cx.WDB], writes=[wdg])
                for e_ in range(8):
                    e = g * 8 + e_
                    wg_, wu_ = wgs[nw % 2], wus[nw % 2]
                    nw += 1
                    kb.dma("sp", wg_[:].rearrange("p kc f -> p (kc f)"), cx.WGB[e], reads=[cx.WGB], writes=[wg_])
                    kb.dma("act", wu_[:].rearrange("p kc f -> p (kc f)"), cx.WUB[e], reads=[cx.WUB], writes=[wu_])
                    rp = reps[e % 2]
                    kb.mm([lambda e=e: nc.tensor.matmul(prep[:, :nb], lhsT=sel[:, e, :], rhs=wgT[:, t0:t0 + nb], start=True, stop=True)],
                          reads=[sel, wgT], writes=[prep])
                    kb.op("act", lambda rp=rp: nc.scalar.copy(out=rp[:, :nb], in_=prep[:, :nb]), reads=[prep], writes=[rp])
                    for hc in range(2):
                        a = it % 2
                        it += 1
                        kb.mm([(lambda kc=kc, a=a, hc=hc, wg_=wg_: nc.tensor.matmul(
                            pg[a][:, :nb], lhsT=wg_[:, kc, hc * 128:(hc + 1) * 128], rhs=hx[:, kc, :nb],
                            start=(kc == 0), stop=(kc == 7))) for kc in range(8)], reads=[wg_, hx], writes=[pg[a]])
                        kb.mm([(lambda kc=kc, a=a, hc=hc, wu_=wu_: nc.tensor.matmul(
                            pu[a][:, :nb], lhsT=wu_[:, kc, hc * 128:(hc + 1) * 128], rhs=hx[:, kc, :nb],
                            start=(kc == 0), stop=(kc == 7))) for kc in range(8)], reads=[wu_, hx], writes=[pu[a]])
                        kb.op("act", lambda a=a: nc.scalar.activation(out=ssb[a][:, :nb], in_=pg[a][:, :nb], func=AF.Silu),
                              reads=[pg[a]], writes=[ssb[a]])
                        kb.op("act", lambda a=a: nc.scalar.copy(out=tsb[a][:, :nb], in_=pu[a][:, :nb]), reads=[pu[a]], writes=[tsb[a]])
                        kb.op("dve", lambda a=a: nc.vector.tensor_tensor(out=tsb[a][:, :nb], in0=tsb[a][:, :nb], in1=ssb[a][:, :nb], op=ALU.mult),
                              reads=[tsb[a], ssb[a]], writes=[tsb[a]])
                        kb.op("pool", lambda a=a, rp=rp, e_=e_, hc=hc: nc.gpsimd.tensor_tensor(
                            out=hid[:, e_ * 2 + hc, :nb], in0=tsb[a][:, :nb], in1=rp[:, :nb], op=ALU.mult),
                            reads=[tsb[a], rp], writes=[], acc_writes=[hid])
                for oc in range(8):
                    p_ = py[oc % 2]
                    fns = []
                    for j in range(16):
                        fns.append(lambda j=j, oc=oc, p_=p_: nc.tensor.matmul(
                            p_[:, :nb], lhsT=wdg[:, j // 2, j % 2, oc * 128:(oc + 1) * 128], rhs=hid[:, j, :nb],
                            start=(j == 0), stop=(j == 15)))
                    kb.mm(fns, reads=[wdg, hid], writes=[p_])
                    if g == 0:
                        kb.op("act", lambda oc=oc, p_=p_: nc.scalar.copy(out=yacc[:, oc, :nb], in_=p_[:, :nb]), reads=[p_], writes=[], acc_writes=[yacc])
                    else:
                        yt = ytmp[oc % 2]
                        kb.op("act", lambda yt=yt, p_=p_: nc.scalar.copy(out=yt[:, :nb], in_=p_[:, :nb]), reads=[p_], writes=[yt])
                        eng = "dve" if oc % 2 == 0 else "pool"
                        h_ = nc.vector if eng == "dve" else nc.gpsimd
                        kb.op(eng, lambda h_=h_, oc=oc, yt=yt: h_.tensor_tensor(
                            out=yacc[:, oc, :nb], in0=yacc[:, oc, :nb], in1=yt[:, :nb], op=ALU.add),
                            reads=[yt, yacc], writes=[], acc_writes=[yacc])
            for oc in range(8):
                kb.op("pool", lambda oc=oc: nc.gpsimd.tensor_scalar(
                    out=yacc[:, oc, :nb], in0=yacc[:, oc, :nb], scalar1=modv[:, 40 + oc, col:col + 1], scalar2=None, op0=ALU.mult),
                    reads=[yacc, modv], writes=[yacc])
                kb.op("dve", lambda oc=oc: nc.vector.scalar_tensor_tensor(
                    out=yacc[:, oc, :nb], in0=xb[:, oc, :nb], scalar=ALPHA, in1=yacc[:, oc, :nb], op0=ALU.mult, op1=ALU.add),
                    reads=[xb, yacc], writes=[yacc])
            ln_block(kb, nc, yacc, nb, gam, bet, yacc, ones, eps, (rsq, ps1, ps2, mean, var))
            kb.dma("sp", cx.XT[:, t0:t0 + nb].rearrange("(kc p) t -> p kc t", p=128), yacc[:, :, :nb],
                   reads=[yacc], writes=[], acc_writes=[cx.XT])
            if yout is not None and t0 < TL:
                kb.dma("sp", yout[:, t0:t0 + nb].rearrange("(kc p) t -> p kc t", p=128), yacc[:, :, :nb],
                       reads=[yacc], writes=[], acc_writes=[yout])


def precast_expert_weights(cx, kb, nc, l):
    with kb.scope():
        stw = [kb.sbuf("pcw%d" % i, [128, 8, 256], BF16) for i in range(4)]
        std = [kb.sbuf("pcd%d" % i, [128, 16, 1024], BF16) for i in range(2)]
        n = 0
        for g in range(4):
            d_ = std[g % 2]
            kb.dma("pool", d_[:], cx.moe_w_down[l, g].rearrange("e (hc p) d -> p (e hc) d", p=128), reads=[cx.moe_w_down], writes=[d_])
            kb.dma("sp", cx.WDB[g], d_[:].rearrange("p j d -> p (j d)"), reads=[d_], writes=[], acc_writes=[cx.WDB])
            for e_ in range(8):
                e = g * 8 + e_
                for src, dst in ((cx.moe_w_gate, cx.WGB), (cx.moe_w_up, cx.WUB)):
                    w_ = stw[n % 4]
                    n += 1
                    kb.dma("pool", w_[:], src[l, g, e_].rearrange("(kc p) f -> p kc f", p=128), reads=[src], writes=[w_])
                    kb.dma("sp", dst[e], w_[:].rearrange("p kc f -> p (kc f)"), reads=[w_], writes=[], acc_writes=[dst])


def rope_tables():
    t = np.arange(TL)
    row = (t // 64).astype(np.float32)
    colp = (t % 64).astype(np.float32)
    nf = 16
    inv = (np.float32(10000.0) ** (-np.arange(nf, dtype=np.float32) / np.float32(nf))).astype(np.float32)
    cos = np.ones((128, T), np.float32)
    sin = np.zeros((128, T), np.float32)
    for q in range(128):
        d = q % 64
        pos = row if d < 32 else colp
        ang = (pos * inv[d % 16]).astype(np.float32)
        cos[q, :TL] = np.cos(ang).astype(np.float32)
        sin[q, :TL] = np.sin(ang).astype(np.float32)
    pt = np.zeros((128, 128), np.float32)
    for m in range(128):
        if (m % 32) < 16:
            pt[m + 16, m] = -1.0
        else:
            pt[m - 16, m] = 1.0
    return cos, sin, pt


_CONST = {}
_NAIDX = {}


def consts():
    if not _CONST:
        cos, sin, pt = rope_tables()
        _CONST.update(rope_cos=cos, rope_sin=sin, rope_pt=pt)
        _CONST.update(scan_masks())
        _CONST.update(wa_masks())
        sel = np.zeros((32, 32, 128), np.float32)
        for e in range(32):
            sel[e, e, :] = 1.0
        _CONST["m_sel"] = sel
        _CONST["m_ident32"] = np.eye(128, dtype=np.float32)
        _k, _valid, _ri, _ci = na_consts()
        _CONST["m_navalid"] = _valid
        _NAIDX["ri"] = _ri
        _NAIDX["ci"] = _ci
    return _CONST


def core_inputs(inp, b):
    f = lambda a: np.ascontiguousarray(a, dtype=np.float32)
    m = {}
    m["xin"] = f(np.concatenate([inp["x"][b].T, inp["ctx"][b].T], axis=1))
    cv = np.stack([inp["c"][b].reshape(8, 128).T, inp["c_ctx"].reshape(8, 128).T], axis=2)
    m["cvec"] = f(cv)
    m["w_mod"] = f(inp["w_mod"])
    m["b_modT"] = f(inp["b_mod"].reshape(2, 48, 128).transpose(0, 2, 1))
    m["w_in"] = f(inp["w_in"])
    m["hg_gamma"] = f(inp["hg_gamma"])
    m["gla_lr_w"] = f(inp["gla_lr_w"])
    m["gla_lr_b"] = f(inp["gla_lr_b"])
    m["wa_sink"] = f(inp["wa_sink"])
    for k in ("w_branch", "w_out", "moe_w_group", "moe_b_group", "moe_w_expert", "moe_b_expert",
              "moe_w_gate", "moe_w_up", "moe_w_down"):
        m[k] = f(inp[k])
    m["ln_gT"] = f(inp["ln_g"].reshape(2, 2, 8, 128).transpose(0, 1, 3, 2))
    m["ln_bT"] = f(inp["ln_b"].reshape(2, 2, 8, 128).transpose(0, 1, 3, 2))
    consts()
    m["na_bias"] = f(inp["na_rpb"][:, :, _NAIDX["ri"], _NAIDX["ci"]])
    m.update(consts())
    return m


FUSED = 1


def build_program(layers, final):
    nc = bass.Bass("TRN2", target_bir_lowering=False)
    cx = Ctx()
    declare_inputs(cx, nc)
    if final:
        yout = Buf(nc.dram_tensor("yout", [D, TL], F32, kind="ExternalOutput"), "yout")
    else:
        yout = None
    with ExitStack() as st:
        kb = KB(nc, st)
        declare_scratch(cx, kb, debug=False, as_output=(() if final else ("XT",)))
        modT = [kb.sbuf("modT%d" % l, [128, 48, 2], F32) for l in range(2)]
        stage_mod(cx, kb, nc, modT)
        first = True
        for l in layers:
            xsrc = cx.xin if first else cx.XT
            first = False
            with kb.scope():
                lbt = [kb.sbuf("lbt%d" % i, [128, 1024], F32) for i in range(2)]
                omlt = [kb.sbuf("omlt%d" % i, [128, 1024], F32) for i in range(2)]
                stage_lb(cx, kb, nc, lbt, omlt)
                hxT = kb.sbuf("hxT", [128, 8, T], BF16)
                stage_modulate(cx, kb, nc, xsrc, modT[l], 0, 1, hxT)
                stage_inproj_fm(cx, kb, nc, l, hxT)
                stage_inproj_tm(cx, kb, nc, l, hxT, lbt[l], omlt[l])
            stage_gla_gate(cx, kb, nc, l)
            stage_scan(cx, kb, nc, "gla")
            stage_scan(cx, kb, nc, "hgrn")
            stage_wa(cx, kb, nc, l)
            stage_na(cx, kb, nc, l)
            stage_merge(cx, kb, nc, l, xsrc, modT[l])
            stage_moe(cx, kb, nc, l, modT[l], yout if (final and l == layers[-1]) else None)
        if final:
            kb.finish([yout])
        else:
            kb.finish([cx.XT])
    return nc


def kernel(**inputs):
    inp = {k: np.asarray(v) for k, v in inputs.items()}
    in_maps = [core_inputs(inp, b) for b in range(4)]
    if FUSED:
        nc = build_program([0, 1], True)
        res = run_bass_kernel_spmd(nc, in_maps, core_ids=list(range(4)))
    else:
        nc0 = build_program([0], False)
        res0 = run_bass_kernel_spmd(nc0, in_maps, core_ids=list(range(4)))
        for b in range(4):
            in_maps[b]["xin"] = np.ascontiguousarray(res0.results[b]["XT"])
        nc1 = build_program([1], True)
        res = run_bass_kernel_spmd(nc1, in_maps, core_ids=list(range(4)))
    out = np.stack([np.ascontiguousarray(res.results[b]["yout"].T) for b in range(4)], axis=0)
    return out.astype(np.float32)
```

```python
import numpy as np
from contextlib import ExitStack
import concourse.bass as bass
import concourse.mybir as mybir
from concourse.bass_utils import run_bass_kernel_spmd
import os

F32 = mybir.dt.float32
BF16 = mybir.dt.bfloat16
U32 = mybir.dt.uint32
AF = mybir.ActivationFunctionType
ALU = mybir.AluOpType
AX = mybir.AxisListType


class Buf:
    __slots__ = ("t", "w", "r", "name")

    def __init__(self, t, name=""):
        self.t = t
        self.w = []
        self.r = []
        self.name = name

    def __getitem__(self, idx):
        return self.t[idx]


class Dep:
    __slots__ = ("w", "r")

    def __init__(self):
        self.w = []
        self.r = []


class Eng:
    def __init__(self, name, h, sem):
        self.name = name
        self.h = h
        self.sem = sem
        self.count = 0
        self.seen = {}


class KB:
    def __init__(self, nc, stack, n_dma_sems=8):
        self.nc = nc
        self.stack = stack
        self.sems = {}
        self.engs = {}
        for name, h in (("pe", nc.tensor), ("act", nc.scalar), ("dve", nc.vector),
                        ("pool", nc.gpsimd), ("sp", nc.sync)):
            s = stack.enter_context(nc.semaphore("s_" + name))
            self.sems[id(s)] = s
            self.engs[name] = Eng(name, h, s)
        self.dma_pool = {}
        for q in ("sp", "pool", "act"):
            lst = []
            for i in range(n_dma_sems):
                s = stack.enter_context(nc.semaphore("d_%s%d" % (q, i)))
                self.sems[id(s)] = s
                lst.append([s, 0])
            self.dma_pool[q] = [lst, 0]
        self.ninstr = 0

    def scope(self):
        kb = self

        class _Scope:
            def __enter__(self_s):
                self_s.prev = kb.stack
                self_s.st = ExitStack()
                self_s.st.__enter__()
                kb.stack = self_s.st
                kb.nscope = getattr(kb, "nscope", 0) + 1
                return self_s

            def __exit__(self_s, *a):
                if a[0] is None:
                    kb.barrier()
                kb.stack = self_s.prev
                return self_s.st.__exit__(*a)
        return _Scope()

    def sbuf(self, name, shape, dtype):
        name = "%s_s%d" % (name, getattr(self, "nscope", 0))
        t = self.stack.enter_context(self.nc.sbuf_tensor(name, list(shape), dtype))
        return Buf(t, name)

    def psum(self, name, shape, dtype=F32):
        name = "%s_s%d" % (name, getattr(self, "nscope", 0))
        t = self.stack.enter_context(self.nc.psum_tensor(name, list(shape), dtype))
        return Buf(t, name)

    def dram(self, name, shape, dtype, kind="Internal"):
        t = self.nc.dram_tensor(name, list(shape), dtype, kind=kind)
        return Buf(t, name)

    def _wait(self, e, deps):
        need = {}
        for (sid, val) in deps:
            if need.get(sid, 0) < val:
                need[sid] = val
        for sid, val in need.items():
            if e.seen.get(sid, 0) < val:
                e.h.wait_ge(self.sems[sid], val)
                e.seen[sid] = val
                self.ninstr += 1

    def _collect(self, reads, writes, acc_writes=()):
        deps = []
        for b in reads:
            deps += b.w
        for b in writes:
            deps += b.w
            deps += b.r
        for b in acc_writes:
            deps += b.r
        return deps

    @staticmethod
    def _compact(lst):
        mx = {}
        for (sid, v) in lst:
            if mx.get(sid, 0) < v:
                mx[sid] = v
        return list(mx.items())

    def _commit(self, token, reads, writes, acc_writes=()):
        for b in acc_writes:
            b.w.append(token)
            if len(b.w) > 64:
                b.w = self._compact(b.w)
        for b in reads:
            b.r.append(token)
            if len(b.r) > 64:
                mx = {}
                for (sid, v) in b.r:
                    if mx.get(sid, 0) < v:
                        mx[sid] = v
                b.r = list(mx.items())
        for b in writes:
            b.w = [token]
            b.r = []

    def _cutcheck(self):
        import os
        cut = os.environ.get("KB_CUT")
        self.nops = getattr(self, "nops", 0) + 1
        skip = os.environ.get("KB_SKIP")
        if skip and self.nops in [int(x) for x in skip.split(",")]:
            return True
        return cut is not None and self.nops > int(cut)

    def op(self, eng, fn, reads=(), writes=(), same_engine_sync=True, acc_writes=()):
        if self._cutcheck():
            return None
        e = self.engs[eng]
        deps = self._collect(reads, writes, acc_writes)
        if not same_engine_sync:
            deps = [d for d in deps if d[0] != id(e.sem)]
        self._wait(e, deps)
        ins = fn()
        e.count += 1
        ins.then_inc(e.sem, 1)
        self.ninstr += 1
        token = (id(e.sem), e.count)
        self._commit(token, reads, writes, acc_writes)
        return token

    def mm(self, fns, reads=(), writes=()):
        if self._cutcheck():
            return None
        e = self.engs["pe"]
        deps = self._collect(reads, writes)
        deps = [d for d in deps if d[0] != id(e.sem)]
        self._wait(e, deps)
        ins = None
        for fn in fns:
            ins = fn()
            self.ninstr += 1
        e.count += 1
        ins.then_inc(e.sem, 1)
        token = (id(e.sem), e.count)
        self._commit(token, reads, writes)
        return token

    def dma(self, q, out, in_, reads=(), writes=(), acc_writes=(), **kw):
        e = self.engs[q]
        pool = self.dma_pool[q]
        lst, idx = pool
        ent = lst[idx % len(lst)]
        pool[1] = idx + 1
        s, total = ent
        deps = self._collect(reads, writes, acc_writes)
        deps.append((id(s), total))
        import os
        if os.environ.get("DMA_SERIAL") and getattr(self, "_last_dma", {}).get(q):
            deps.append(self._last_dma[q])
        self._wait(e, deps)
        ins = e.h.dma_start(out=out, in_=in_, **kw)
        ins.then_inc(s, 16)
        ent[1] = total + 16
        self.ninstr += 1
        token = (id(s), total + 16)
        if not hasattr(self, "_last_dma"):
            self._last_dma = {}
        self._last_dma[q] = token
        self._commit(token, reads, writes, acc_writes)
        return token

    def init_memory(self, nbytes=196608):
        nc = self.nc
        with self.scope():
            n = nbytes // 4
            big = self.sbuf("initbig", [128, n], F32)
            q = n // 4
            self.op("dve", lambda: nc.vector.memset(big[:, 0:q], 0.0), writes=[])
            self.op("pool", lambda: nc.gpsimd.memset(big[:, q:2 * q], 0.0), writes=[])
            self.op("dve", lambda: nc.vector.memset(big[:, 2 * q:3 * q], 0.0), writes=[])
            self.op("pool", lambda: nc.gpsimd.memset(big[:, 3 * q:n], 0.0), writes=[])
            pss = [self.psum("initps%d" % i, [128, 512]) for i in range(8)]
            for p in pss:
                self.op("dve", lambda p=p: nc.vector.memset(p[:], 0.0), writes=[])

    def barrier(self):
        allt = []
        for e in self.engs.values():
            if e.count:
                allt.append((id(e.sem), e.count))
        for q, (lst, _) in self.dma_pool.items():
            for s, total in lst:
                if total:
                    allt.append((id(s), total))
        for e in self.engs.values():
            self._wait(e, allt)

    def finish(self, tokens_bufs):
        e = self.engs["sp"]
        deps = []
        for b in tokens_bufs:
            deps += b.w
        self._wait(e, deps)

T = 4352
TL = 4096

D = 1024
TL = 4096
TC = 256
T = TL + TC
NT = T // 128
BLKS = [(i * 512, 512) for i in range(8)] + [(4096, 256)]
DIN = 10528
ALPHA = (2.0 * 2) ** 0.25

COLS = dict(a_q=(0, 256), a_k=(256, 256), a_v=(512, 512), a_g=(1024, 512), a_lr=(1536, 32),
            b_q=(1568, 512), b_k=(2080, 512), b_v=(2592, 512),
            c_q=(3104, 512), c_k=(3616, 128), c_v=(3744, 128),
            d_q=(3872, 512), d_f=(4384, 1024), d_i=(5408, 512), d_g=(5920, 512), gate=(6432, 4096))


def blkcol(t0):
    return 1 if t0 >= TL else 0


class Ctx:
    pass


def declare_inputs(cx, nc):
    def din(name, shape, dt=F32):
        return Buf(nc.dram_tensor(name, list(shape), dt, kind="ExternalInput"), name)
    cx.xin = din("xin", [D, T])
    cx.cvec = din("cvec", [128, 8, 2])
    cx.w_mod = din("w_mod", [2, D, 6 * D])
    cx.b_modT = din("b_modT", [2, 128, 48])
    cx.w_in = din("w_in", [2, D, DIN])
    cx.rope_cos = din("rope_cos", [128, T])
    cx.rope_sin = din("rope_sin", [128, T])
    cx.rope_pt = din("rope_pt", [128, 128])
    cx.hg_gamma = din("hg_gamma", [2, 2, 512])
    cx.gla_lr_w = din("gla_lr_w", [2, 2, 16, 256])
    cx.gla_lr_b = din("gla_lr_b", [2, 2, 256])
    for nm in ("mg", "mh"):
        setattr(cx, nm + "_mq", din(nm + "_mq", [128, 2, 130]))
        setattr(cx, nm + "_mk", din(nm + "_mk", [128, 2, 128]))
    cx.m_ma = din("m_ma", [128, 2, 128])
    cx.m_ident = din("m_ident", [128, 128])
    cx.m_mau = din("m_mau", [128, 2, 128], U32)
    cx.m_wa = din("m_wa", [128, 2, 128])
    cx.wa_sink = din("wa_sink", [2, 8])
    cx.m_navalid = din("m_navalid", [128, 21, 128])
    cx.w_branch = din("w_branch", [2, 4, 512, 1024])
    cx.w_out = din("w_out", [2, 1024, 1024])
    cx.ln_gT = din("ln_gT", [2, 2, 128, 8])
    cx.ln_bT = din("ln_bT", [2, 2, 128, 8])
    cx.moe_w_group = din("moe_w_group", [2, 1024, 4])
    cx.moe_b_group = din("moe_b_group", [2, 4])
    cx.moe_w_expert = din("moe_w_expert", [2, 1024, 32])
    cx.moe_b_expert = din("moe_b_expert", [2, 32])
    cx.moe_w_gate = din("moe_w_gate", [2, 4, 8, 1024, 256])
    cx.moe_w_up = din("moe_w_up", [2, 4, 8, 1024, 256])
    cx.moe_w_down = din("moe_w_down", [2, 4, 8, 256, 1024])
    cx.m_sel = din("m_sel", [32, 32, 128])
    cx.m_ident32 = din("m_ident32", [128, 128])
    cx.na_bias = din("na_bias", [2, 8, 128, 21, 128])


def declare_scratch(cx, kb, debug, as_input=(), as_output=()):
    kind = "ExternalOutput" if debug else "Internal"

    def ds(name, shape, dt):
        b = kb.dram(name, shape, dt, kind=("ExternalInput" if name in as_input else ("ExternalOutput" if name in as_output else kind)))
        setattr(cx, name, b)
        return b
    ds("XT", [D, T], F32)
    ds("QA", [256, T], BF16)
    ds("GA", [512, T], BF16)
    ds("LR1", [33, T], F32)
    ds("QB", [512, T], BF16)
    ds("KB_", [512, T], BF16)
    ds("QC", [512, T], BF16)
    ds("KC", [128, T], BF16)
    ds("QD", [512, T], BF16)
    ds("GD", [512, T], BF16)
    ds("GATE", [4096, T], BF16)
    ds("KVA_t", [128, NT, 768], BF16)
    ds("VB_t", [128, NT, 512], BF16)
    ds("VC_t", [128, NT, 128], BF16)
    ds("VD_t", [128, NT, 512], BF16)
    ds("LGD_t", [128, NT, 1024], F32)
    ds("KD_t", [128, NT, 1024], BF16)
    ds("LGA_t", [128, NT, 512], F32)
    ds("MODT", [2, 128, 96], F32)
    ds("HX2", [D, T], BF16)
    ds("WGB", [32, 128, 2048], BF16)
    ds("WUB", [32, 128, 2048], BF16)
    ds("WDB", [4, 128, 16384], BF16)
    ds("WG_t", [128, NT, 32], F32)
    ds("WGT", [32, T], F32)
    ds("BR_A", [512, T], BF16)
    ds("BR_B", [512, T], BF16)
    ds("BR_C", [512, T], BF16)
    ds("BR_D", [512, T], BF16)


def stage_mod(cx, kb, nc, modT):
    with kb.scope():
        cv = kb.sbuf("cv", [128, 8, 2], F32)
        sc = kb.sbuf("sc", [128, 8, 2], F32)
        kb.dma("sp", cv[:], cx.cvec[:], reads=[cx.cvec], writes=[cv])
        kb.op("act", lambda: nc.scalar.activation(out=sc[:], in_=cv[:], func=AF.Silu), reads=[cv], writes=[sc])
        wbufs = [kb.sbuf("wm%d" % i, [128, 8, 1024], F32) for i in range(2)]
        bm = kb.sbuf("bm", [128, 48], F32)
        ps = kb.psum("ps_mod", [128, 96])
        wi = 0
        for l in range(2):
            kb.dma("sp", bm[:], cx.b_modT[l], reads=[cx.b_modT], writes=[bm])
            for g in range(6):
                wb = wbufs[wi % 2]
                wi += 1
                src = cx.w_mod[l, :, g * 1024:(g + 1) * 1024].rearrange("(kc p) c -> p kc c", p=128)
                kb.dma("sp", wb[:], src, reads=[cx.w_mod], writes=[wb])
                for j in range(8):
                    jj = g * 8 + j
                    fns = []
                    for kc in range(8):
                        fns.append(lambda kc=kc, j=j, jj=jj, wb=wb: nc.tensor.matmul(
                            ps[:, 2 * jj:2 * jj + 2], lhsT=wb[:, kc, j * 128:(j + 1) * 128], rhs=sc[:, kc, :],
                            start=(kc == 0), stop=(kc == 7)))
                    kb.mm(fns, reads=[wb, sc], writes=[ps])
            m = modT[l]
            kb.op("dve", lambda m=m: nc.vector.tensor_tensor(
                out=m[:], in0=ps[:].rearrange("p (j c) -> p j c", c=2),
                in1=bm[:].unsqueeze(2).broadcast_to([128, 48, 2]), op=ALU.add),
                reads=[ps, bm], writes=[m])
            kb.dma("sp", cx.MODT[l], m[:].rearrange("p j c -> p (j c)"), reads=[m], writes=[cx.MODT])


def stage_modulate(cx, kb, nc, src_dram, modv, shift_idx, scale_idx, hxT, hx32=None):
    with kb.scope():
        onep = kb.sbuf("onep", [128, 8, 2], F32)
        kb.op("dve", lambda: nc.vector.tensor_scalar(
            out=onep[:], in0=modv[:, scale_idx * 8:(scale_idx + 1) * 8, :], scalar1=1.0, scalar2=None, op0=ALU.add),
            reads=[modv], writes=[onep])
        xb = [kb.sbuf("xb%d" % i, [128, 8, 512], F32) for i in range(2)]
        for bi, (t0, nb) in enumerate(BLKS):
            col = blkcol(t0)
            x = xb[bi % 2]
            kb.dma("sp", x[:, :, :nb], src_dram[:, t0:t0 + nb].rearrange("(kc p) t -> p kc t", p=128),
                   reads=[src_dram], writes=[x])
            for kc in range(8):
                eng = "dve" if kc % 2 == 0 else "pool"
                h = nc.vector if eng == "dve" else nc.gpsimd
                kb.op(eng, lambda h=h, kc=kc, x=x: h.tensor_scalar(
                    out=hxT[:, kc, t0:t0 + nb], in0=x[:, kc, :nb],
                    scalar1=onep[:, kc, col:col + 1], scalar2=modv[:, shift_idx * 8 + kc, col:col + 1],
                    op0=ALU.mult, op1=ALU.add), reads=[x, onep, modv], writes=[hxT])


FM_GROUPS = [
    ("a_q", "QA", "copy"), ("a_g", "GA", "silu"), ("a_lr", "LR1", "copy32"),
    ("b_q", "QB", "copy"), ("b_k", "KB_", "copy"),
    ("c_q", "QC", "rope"), ("c_k", "KC", "rope"),
    ("d_q", "QD", "silu"), ("d_g", "GD", "silu"), ("gate", "GATE", "sigmoid"),
]


def stage_inproj_fm(cx, kb, nc, l, hxT, only=None):
    with kb.scope():
        wsl = [kb.sbuf("wsl%d" % i, [128, 8, 512], BF16) for i in range(2)]
        stg = [kb.sbuf("stg%d" % i, [128, T], BF16) for i in range(2)]
        stg32 = kb.sbuf("stg32", [32, T], F32)
        pss = [kb.psum("psf%d" % i, [128, 512]) for i in range(4)]
        psr = [kb.psum("psr%d" % i, [128, 512]) for i in range(2)]
        cosT = kb.sbuf("cosT", [128, T], F32)
        sinT = kb.sbuf("sinT", [128, T], F32)
        ptm = kb.sbuf("ptm", [128, 128], BF16)
        tmpb = [kb.sbuf("tmpb%d" % i, [128, 512], BF16) for i in range(2)]
        t1 = [kb.sbuf("t1_%d" % i, [128, 512], F32) for i in range(2)]
        t2 = [kb.sbuf("t2_%d" % i, [128, 512], F32) for i in range(2)]
        ones = kb.sbuf("ones1", [1, T], F32)
        kb.dma("sp", cosT[:], cx.rope_cos[:], reads=[cx.rope_cos], writes=[cosT])
        kb.dma("sp", sinT[:], cx.rope_sin[:], reads=[cx.rope_sin], writes=[sinT])
        kb.dma("pool", ptm[:], cx.rope_pt[:], reads=[cx.rope_pt], writes=[ptm])
        kb.op("dve", lambda: nc.vector.memset(ones[:], 1.0), writes=[ones])
        kb.dma("sp", cx.LR1[32:33, :], ones[:], reads=[ones], writes=[], acc_writes=[cx.LR1])
        nsl = 0
        nps = 0
        nst = 0
        nrp = 0
        for (name, dest, kind) in FM_GROUPS:
            if only is not None and name not in only:
                continue
            c0, nc_ = COLS[name]
            dbuf = getattr(cx, dest)
            for s0 in range(0, nc_, 512):
                sw = min(512, nc_ - s0)
                w = wsl[nsl % 2]
                nsl += 1
                kb.dma("pool", w[:, :, :sw],
                       cx.w_in[l, :, c0 + s0:c0 + s0 + sw].rearrange("(kc p) c -> p kc c", p=128),
                       reads=[cx.w_in], writes=[w])
                for m0 in range(0, sw, 128):
                    mw = min(128, sw - m0)
                    if kind == "copy32":
                        st = stg32
                    else:
                        st = stg[nst % 2]
                        nst += 1
                    for bi, (t0, nb) in enumerate(BLKS):
                        ps = pss[nps % 4]
                        nps += 1
                        fns = [(lambda kc=kc, ps=ps, w=w: nc.tensor.matmul(
                            ps[:mw, :nb], lhsT=w[:, kc, m0:m0 + mw], rhs=hxT[:, kc, t0:t0 + nb],
                            start=(kc == 0), stop=(kc == 7))) for kc in range(8)]
                        kb.mm(fns, reads=[w, hxT], writes=[ps])
                        o = st[:mw, t0:t0 + nb]
                        if kind in ("copy", "copy32"):
                            if nps % 2 == 0:
                                kb.op("dve", lambda ps=ps, o=o: nc.vector.tensor_copy(out=o, in_=ps[:mw, :nb]),
                                      reads=[ps], writes=[st])
                            else:
                                kb.op("act", lambda ps=ps, o=o: nc.scalar.copy(out=o, in_=ps[:mw, :nb]),
                                      reads=[ps], writes=[st])
                        elif kind == "silu":
                            kb.op("act", lambda ps=ps, o=o: nc.scalar.activation(out=o, in_=ps[:mw, :nb], func=AF.Silu),
                                  reads=[ps], writes=[st])
                        elif kind == "sigmoid":
                            kb.op("act", lambda ps=ps, o=o: nc.scalar.activation(out=o, in_=ps[:mw, :nb], func=AF.Sigmoid),
                                  reads=[ps], writes=[st])
                        elif kind == "rope":
                            tb = tmpb[nrp % 2]
                            a1 = t1[nrp % 2]
                            a2 = t2[nrp % 2]
                            pr = psr[nrp % 2]
                            nrp += 1
                            kb.op("act", lambda ps=ps, tb=tb: nc.scalar.copy(out=tb[:mw, :nb], in_=ps[:mw, :nb]),
                                  reads=[ps], writes=[tb])
                            kb.mm([lambda pr=pr, tb=tb: nc.tensor.matmul(pr[:mw, :nb], lhsT=ptm[:mw, :mw], rhs=tb[:mw, :nb],
                                                                         start=True, stop=True)],
                                  reads=[ptm, tb], writes=[pr])
                            kb.op("dve", lambda tb=tb, a1=a1: nc.vector.tensor_tensor(
                                out=a1[:mw, :nb], in0=tb[:mw, :nb], in1=cosT[:mw, t0:t0 + nb], op=ALU.mult),
                                reads=[tb, cosT], writes=[a1])
                            kb.op("dve", lambda pr=pr, a2=a2: nc.vector.tensor_tensor(
                                out=a2[:mw, :nb], in0=pr[:mw, :nb], in1=sinT[:mw, t0:t0 + nb], op=ALU.mult),
                                reads=[pr, sinT], writes=[a2])
                            kb.op("pool", lambda a1=a1, a2=a2, o=o: nc.gpsimd.tensor_tensor(
                                out=o, in0=a1[:mw, :nb], in1=a2[:mw, :nb], op=ALU.add),
                                reads=[a1, a2], writes=[st])
                    kb.dma("sp", dbuf[s0 + m0:s0 + m0 + mw, :], st[:mw, :], reads=[st], writes=[], acc_writes=[dbuf])


TM_GROUPS = [
    (256, 512, "KVA_t", 0, "copy"), (768, 256, "KVA_t", 512, "copy"),
    (2592, 512, "VB_t", 0, "copy"), (3744, 128, "VC_t", 0, "copy"),
    (5408, 512, "VD_t", 0, "copy"),
    (4384, 512, None, 0, "hgf"), (4896, 512, None, 512, "hgf"),
]


def stage_inproj_tm(cx, kb, nc, l, hxT, lbt, omlt, only=None):
    with kb.scope():
        wsl = [kb.sbuf("wtl%d" % i, [128, 8, 512], BF16) for i in range(2)]
        pss = [kb.psum("pst%d" % i, [128, 512]) for i in range(4)]
        stb = [kb.sbuf("stb%d" % i, [128, 512], BF16) for i in range(4)]
        e1 = [kb.sbuf("e1_%d" % i, [128, 512], F32) for i in range(2)]
        r1 = [kb.sbuf("r1_%d" % i, [128, 512], F32) for i in range(2)]
        lg = [kb.sbuf("lg_%d" % i, [128, 512], F32) for i in range(2)]
        kk = [kb.sbuf("kk_%d" % i, [128, 512], BF16) for i in range(2)]
        nsl = nps = nst = nh = 0
        for (c0, ncol, dest, doff, kind) in TM_GROUPS:
            if only is not None and kind not in only:
                continue
            w = wsl[nsl % 2]
            nsl += 1
            kb.dma("pool", w[:, :, :ncol], cx.w_in[l, :, c0:c0 + ncol].rearrange("(kc p) c -> p kc c", p=128),
                   reads=[cx.w_in], writes=[w])
            for ti in range(NT):
                ts = slice(ti * 128, (ti + 1) * 128)
                ps = pss[nps % 4]
                nps += 1
                fns = [(lambda kc=kc, ps=ps, w=w: nc.tensor.matmul(
                    ps[:, :ncol], lhsT=hxT[:, kc, ts], rhs=w[:, kc, :ncol],
                    start=(kc == 0), stop=(kc == 7))) for kc in range(8)]
                kb.mm(fns, reads=[w, hxT], writes=[ps])
                if kind == "copy":
                    st = stb[nst % 4]
                    nst += 1
                    if nst % 2 == 0:
                        kb.op("dve", lambda ps=ps, st=st: nc.vector.tensor_copy(out=st[:, :ncol], in_=ps[:, :ncol]),
                              reads=[ps], writes=[st])
                    else:
                        kb.op("act", lambda ps=ps, st=st: nc.scalar.copy(out=st[:, :ncol], in_=ps[:, :ncol]),
                              reads=[ps], writes=[st])
                    db = getattr(cx, dest)
                    kb.dma("sp", db[:, ti, doff:doff + ncol], st[:, :ncol], reads=[st], writes=[], acc_writes=[db])
                else:
                    e = e1[nh % 2]
                    r = r1[nh % 2]
                    g = lg[nh % 2]
                    k = kk[nh % 2]
                    nh += 1
                    cs = slice(doff, doff + 512)
                    kb.op("act", lambda ps=ps, e=e: nc.scalar.activation(out=e[:], in_=ps[:], func=AF.Exp, scale=-1.0),
                          reads=[ps], writes=[e])
                    kb.op("pool", lambda e=e: nc.gpsimd.tensor_scalar(
                        out=e[:], in0=e[:], scalar1=1.0, scalar2=None, op0=ALU.add),
                        reads=[e], writes=[e])
                    kb.op("dve", lambda e=e, r=r: nc.vector.reciprocal(out=r[:], in_=e[:]),
                          reads=[e], writes=[r])
                    kb.op("dve", lambda r=r: nc.vector.tensor_tensor(out=r[:], in0=r[:], in1=omlt[:, cs], op=ALU.mult),
                          reads=[r, omlt], writes=[r])
                    kb.op("pool", lambda r=r: nc.gpsimd.tensor_tensor(out=r[:], in0=r[:], in1=lbt[:, cs], op=ALU.add),
                          reads=[r, lbt], writes=[r])
                    kb.op("act", lambda r=r, g=g: nc.scalar.activation(out=g[:], in_=r[:], func=AF.Ln),
                          reads=[r], writes=[g])
                    kb.op("pool", lambda r=r, k=k: nc.gpsimd.tensor_scalar(
                        out=k[:], in0=r[:], scalar1=-1.0, scalar2=1.0, op0=ALU.mult, op1=ALU.add),
                        reads=[r], writes=[k])
                    kb.dma("sp", cx.LGD_t[:, ti, cs], g[:], reads=[g], writes=[], acc_writes=[cx.LGD_t])
                    kb.dma("sp", cx.KD_t[:, ti, cs], k[:], reads=[k], writes=[], acc_writes=[cx.KD_t])


def stage_lb(cx, kb, nc, lbt, omlt):
    with kb.scope():
        gam = kb.sbuf("gam", [128, 2, 2, 512], F32)
        kb.dma("sp", gam[:].rearrange("p a b c -> p (a b c)"),
               cx.hg_gamma[:].rearrange("a b c -> (a b c)").partition_broadcast(128),
               reads=[cx.hg_gamma], writes=[gam])
        d = kb.sbuf("gamd", [128, 2, 512], F32)
        kb.op("dve", lambda: nc.vector.tensor_tensor(out=d[:], in0=gam[:, :, 0, :], in1=gam[:, :, 1, :], op=ALU.subtract),
              reads=[gam], writes=[d])
        kb.op("act", lambda: nc.scalar.activation(out=d[:], in_=d[:], func=AF.Exp), reads=[d], writes=[d])
        l1 = lbt[1]
        kb.op("dve", lambda: nc.vector.tensor_scalar(
            out=d[:], in0=d[:], scalar1=1.0, scalar2=None, op0=ALU.add), reads=[d], writes=[d])
        kb.op("dve", lambda: nc.vector.reciprocal(out=l1[:], in_=d[:].rearrange("p a c -> p (a c)")),
              reads=[d], writes=[l1])
        l0 = lbt[0]
        kb.op("dve", lambda: nc.vector.memset(l0[:], 0.0), writes=[l0])
        for l in range(2):
            o = omlt[l]
            lb = lbt[l]
            kb.op("dve", lambda o=o, lb=lb: nc.vector.tensor_scalar(
                out=o[:], in0=lb[:], scalar1=-1.0, scalar2=1.0, op0=ALU.mult, op1=ALU.add),
                reads=[lb], writes=[o])


def stage_gla_gate(cx, kb, nc, l):
    with kb.scope():
        wlr = kb.sbuf("wlr", [33, 512], F32)
        lr1 = kb.sbuf("lr1", [33, T], F32)
        kb.op("dve", lambda: nc.vector.memset(wlr[:], 0.0), writes=[wlr])
        kb.dma("sp", wlr[0:16, 0:256], cx.gla_lr_w[l, 0], reads=[cx.gla_lr_w], writes=[wlr])
        kb.dma("sp", wlr[16:32, 256:512], cx.gla_lr_w[l, 1], reads=[cx.gla_lr_w], writes=[wlr])
        kb.dma("sp", wlr[32:33, :], cx.gla_lr_b[l].rearrange("a c -> (a c)").unsqueeze(0),
               reads=[cx.gla_lr_b], writes=[wlr])
        kb.dma("sp", lr1[:], cx.LR1[:], reads=[cx.LR1], writes=[lr1])
        pss = [kb.psum("psg%d" % i, [128, 512]) for i in range(2)]
        ee = [kb.sbuf("ge%d" % i, [128, 512], F32) for i in range(2)]
        gg = [kb.sbuf("gg%d" % i, [128, 512], F32) for i in range(2)]
        for ti in range(NT):
            ts = slice(ti * 128, (ti + 1) * 128)
            ps = pss[ti % 2]
            e = ee[ti % 2]
            g = gg[ti % 2]
            kb.mm([lambda ps=ps: nc.tensor.matmul(ps[:], lhsT=lr1[:, ts], rhs=wlr[:], start=True, stop=True)],
                  reads=[lr1, wlr], writes=[ps])
            kb.op("act", lambda ps=ps, e=e: nc.scalar.activation(out=e[:], in_=ps[:], func=AF.Exp, scale=-1.0),
                  reads=[ps], writes=[e])
            kb.op("act", lambda e=e, g=g: nc.scalar.activation(out=g[:], in_=e[:], func=AF.Ln, bias=1.0),
                  reads=[e], writes=[g])
            kb.dma("sp", cx.LGA_t[:, ti, :], g[:], reads=[g], writes=[], acc_writes=[cx.LGA_t])

SCAN_CFG = {
    "gla": dict(H=4, dk=64, qsrc="QA", qscale=0.125, ksrc="KVA_t", kz=0, kcol0=0, vsrc="KVA_t", vcol0=256,
                gsrc="LGA_t", gz=256, gate="GA", dest="BR_A", mask="mg"),
    "hgrn": dict(H=4, dk=128, qsrc="QD", qscale=1.0, ksrc="KD_t", kz=512, kcol0=0, vsrc="VD_t", vcol0=0,
                 gsrc="LGD_t", gz=512, gate="GD", dest="BR_D", mask="mh"),
}


def view(b):
    return Buf(b.t, b.name + "_v")


def stage_scan(cx, kb, nc, which, heads=None):
    cfg = SCAN_CFG[which]
    H, dk = cfg["H"], cfg["dk"]
    dv = 128
    qsrc, ksrc, vsrc, gsrc = (getattr(cx, cfg[k]) for k in ("qsrc", "ksrc", "vsrc", "gsrc"))
    gate, dest = getattr(cx, cfg["gate"]), getattr(cx, cfg["dest"])
    mq_d, mk_d = getattr(cx, cfg["mask"] + "_mq"), getattr(cx, cfg["mask"] + "_mk")
    with kb.scope():
        mq = kb.sbuf("mq", [128, 2, 130], F32)
        mk = kb.sbuf("mk", [128, 2, 128], F32)
        ma = kb.sbuf("ma", [128, 2, 128], F32)
        ident = kb.sbuf("ident", [128, 128], BF16)
        onesdv = kb.sbuf("onesdv", [128, 128], F32)
        kb.dma("sp", mq[:], mq_d[:], reads=[mq_d], writes=[mq])
        kb.dma("sp", mk[:], mk_d[:], reads=[mk_d], writes=[mk])
        kb.dma("sp", ma[:], cx.m_ma[:], reads=[cx.m_ma], writes=[ma])
        mau = kb.sbuf("mau", [128, 2, 128], U32)
        kb.dma("sp", mau[:], cx.m_mau[:], reads=[cx.m_mau], writes=[mau])
        kb.dma("pool", ident[:], cx.m_ident[:], reads=[cx.m_ident], writes=[ident])
        kb.op("dve", lambda: nc.vector.memset(onesdv[:], 1.0 / dv), writes=[onesdv])
        vt = kb.sbuf("vt", [128, NT, dv], BF16)
        qT = kb.sbuf("qT", [128, T], BF16)
        gsil = kb.sbuf("gsil", [128, T], BF16)
        obr = kb.sbuf("obr", [128, T], BF16)
        gts = [kb.sbuf("gt%d" % i, [128, NT, dk], F32) for i in range(2)]
        kts = [kb.sbuf("kt%d" % i, [128, NT, dk], BF16) for i in range(2)]
        qt = [kb.sbuf("qt%d" % z, [128, T], BF16) for z in range(2)]
        atm = [kb.sbuf("atm%d" % z, [128, NT, 128], BF16) for z in range(2)]
        Sp = [kb.sbuf("Sp%d" % z, [128, 2 * NT, dv], BF16) for z in range(2)]
        for z in range(2):
            kb.op("pool", lambda z=z: nc.gpsimd.memset(atm[z][:], 0.0), writes=[atm[z]])
        Sst = [kb.sbuf("Sst%d" % i, [128, dv], F32) for i in range(2)]
        R = 3
        eq = [kb.sbuf("eq%d" % i, [128, 128], F32) for i in range(R)]
        ek = [kb.sbuf("ek%d" % i, [128, 128], F32) for i in range(R)]
        ekh = [kb.sbuf("ekh%d" % i, [128, 128], F32) for i in range(R)]
        ktl = [kb.sbuf("ktl%d" % i, [128, 128], BF16) for i in range(R)]
        khat = [kb.sbuf("khat%d" % i, [128, 256], BF16) for i in range(R)]
        ktr = [kb.sbuf("ktr%d" % i, [128, 128], F32) for i in range(R)]
        atr = [kb.sbuf("atr%d" % i, [128, 128], F32) for i in range(R)]
        usb = [kb.sbuf("usb%d" % i, [128, 256], F32) for i in range(R)]
        osb = [kb.sbuf("osb%d" % i, [128, 512], F32) for i in range(2)]
        cm = kb.sbuf("cm", [128, 2], F32)
        kb.op("dve", lambda: nc.vector.memset(cm[:], 0.0), writes=[cm])
        kb.op("dve", lambda: nc.vector.memset(cm[0:64, 0:1], 1.0), writes=[cm])
        kb.op("dve", lambda: nc.vector.memset(cm[64:128, 1:2], 1.0), writes=[cm])
        er2 = [kb.sbuf("er%d" % i, [128, 2], F32) for i in range(R)]
        ed2 = [kb.sbuf("ed%d" % i, [128, 2], F32) for i in range(R)]
        psA = [kb.psum("psA%d" % i, [128, 512]) for i in range(2)]
        psB = [kb.psum("psB%d" % i, [128, 512]) for i in range(2)]
        psA_ec = [view(p) for p in psA]
        import os
        if True:
            psK = [kb.psum("psK%d" % i, [128, 512]) for i in range(2)]
        else:
            psK = psA
        psA_kt = [view(p) for p in psK]
        psA_e2 = [view(p) for p in psK]
        psB_u = [view(p) for p in psB]
        psB_at = [view(p) for p in psB]
        psO = [kb.psum("psO%d" % i, [128, 512]) for i in range(1)] * 2
        psM = [kb.psum("psM%d" % i, [128, 512]) for i in range(1)] * 2
        sq = [kb.sbuf("sq%d" % i, [128, 512], F32) for i in range(2)]
        sd = [kb.sbuf("sd%d" % i, [128, 512], F32) for i in range(2)]
        on = [kb.sbuf("on%d" % i, [128, 512], F32) for i in range(2)]
        eps_t = kb.sbuf("eps_t", [128, 1], F32)
        kb.op("dve", lambda: nc.vector.memset(eps_t[:], 1e-6), writes=[eps_t])

        it = 0
        ghz = 0
        import os
        for h in (range(H) if heads is None else heads):
            vc0 = cfg["vcol0"] + h * dv
            kb.dma("sp", vt[:], vsrc[:, :, vc0:vc0 + dv], reads=[vsrc], writes=[vt])
            kb.dma("sp", qT[:dk, :], qsrc[h * dk:(h + 1) * dk, :], reads=[qsrc], writes=[qT])
            kb.dma("sp", gsil[:], gate[h * dv:(h + 1) * dv, :], reads=[gate], writes=[gsil])
            for z in range(2):
                gt = gts[ghz % 2]
                kt = kts[ghz % 2]
                ghz += 1
                gc0 = z * cfg["gz"] + h * dk
                kc0 = cfg["kcol0"] + z * cfg["kz"] + h * dk
                kb.dma("sp", gt[:], gsrc[:, :, gc0:gc0 + dk], reads=[gsrc], writes=[gt])
                if os.environ.get("KTMEMSET"):
                    kb.op("dve", lambda kt=kt: nc.vector.memset(kt[:], 0.0), writes=[kt])
                kb.dma("sp", kt[:], ksrc[:, :, kc0:kc0 + dk], reads=[ksrc], writes=[kt])
                order = [32, 33] + list(range(32)) if z == 0 else [33, 32] + list(range(31, -1, -1))
                import os
                DBG = os.environ.get("SCAN_DBG", "")
                if DBG == "loads":
                    continue
                if DBG.startswith("nt"):
                    order = order[:int(DBG[2:])]
                S = Sst[0]
                kb.op("dve", lambda S=S: nc.vector.memset(S[:], 0.0), writes=[S])
                si = 0
                def p1(ti, a, r):
                    ts = slice(ti * 128, (ti + 1) * 128)
                    pa, pb = psA[a], psB[a]
                    pk = psK[a]
                    kb.mm([lambda pa=pa, gt=gt: nc.tensor.matmul(pa[:dk, 0:130], lhsT=gt[:, ti, :], rhs=mq[:, z, :],
                                                                 start=True, stop=True)],
                          reads=[gt, mq], writes=[psA_ec[a]])
                    kb.mm([lambda pk=pk, kt=kt: nc.tensor.matmul(pk[:dk, 256:384], lhsT=(ident[:, :dk] if os.environ.get("A2ID") else kt[:, ti, :]), rhs=ident[:],
                                                                 start=True, stop=True)],
                          reads=[kt, ident], writes=[psA_kt[a]])
                    kb.mm([lambda pk=pk, gt=gt: nc.tensor.matmul(pk[:, 384:384 + dk], lhsT=mk[:, z, :], rhs=gt[:, ti, :],
                                                                 start=True, stop=True)],
                          reads=[gt, mk], writes=[psA_e2[a]])
                    kb.op("act", lambda pa=pa, o=eq[r]: nc.scalar.activation(out=o[:dk, :], in_=pa[:dk, 0:128], func=AF.Exp),
                          reads=[psA_ec[a]], writes=[eq[r]])
                    kb.op("act", lambda pa=pa, o=ek[r]: nc.scalar.activation(out=o[:dk, :], in_=pa[:dk, 0:128], func=AF.Exp, scale=-1.0),
                          reads=[psA_ec[a]], writes=[ek[r]])
                    kb.op("act", lambda pa=pa, o=er2[r]: nc.scalar.activation(out=o[:dk, :], in_=pa[:dk, 128:130], func=AF.Exp, scale=0.5),
                          reads=[psA_ec[a]], writes=[er2[r]])
                    kb.op("act", lambda pa=pa, o=ed2[r]: nc.scalar.activation(out=o[:dk, :], in_=pa[:dk, 128:130], func=AF.Exp),
                          reads=[psA_ec[a]], writes=[ed2[r]])
                    kb.op("act", lambda pk=pk, o=ekh[r]: nc.scalar.activation(out=o[:, :dk], in_=pk[:, 384:384 + dk], func=AF.Exp),
                          reads=[psA_e2[a]], writes=[ekh[r]])
                    qz = qt[z]
                    kb.op("dve", lambda qz=qz, e=eq[r]: nc.vector.scalar_tensor_tensor(
                        out=qz[:dk, ts], in0=qT[:dk, ts], scalar=cfg["qscale"], in1=e[:dk, :], op0=ALU.mult, op1=ALU.mult),
                        reads=[qT, eq[r]], writes=[], acc_writes=[qz])
                    kb.op("act", lambda pk=pk, o=ktr[r]: nc.scalar.copy(out=o[:dk, :], in_=pk[:dk, 256:384]),
                          reads=[psA_kt[a]], writes=[ktr[r]])
                    kb.op("dve", lambda e=ek[r], o=ktl[r], kr=ktr[r]: nc.vector.tensor_tensor(
                        out=o[:dk, :], in0=kr[:dk, :], in1=e[:dk, :], op=ALU.mult),
                        reads=[ktr[r], ek[r]], writes=[ktl[r]])
                    for c in range(2):
                        kb.op("dve", lambda kt=kt, e=ekh[r], o=khat[r], c=c: nc.vector.scalar_tensor_tensor(
                            out=o[:, c * 128:c * 128 + dk], in0=kt[:, ti, :], scalar=cm[:, c:c + 1], in1=e[:, :dk],
                            op0=ALU.mult, op1=ALU.mult),
                            reads=[kt, ekh[r], cm], writes=[], acc_writes=[khat[r]])
                    return None

                def p2(ti, a, r, S, si):
                    ts = slice(ti * 128, (ti + 1) * 128)
                    pa, pb = psA[a], psB[a]
                    qz = qt[z]
                    kb.mm([(lambda c=c, pb=pb, kh=khat[r]: nc.tensor.matmul(
                        pb[:dk, c * 128:(c + 1) * 128], lhsT=kh[:, c * 128:c * 128 + dk],
                        rhs=vt[:, ti, :], start=True, stop=True)) for c in range(2)],
                        reads=[khat[r], vt], writes=[psB_u[a]])
                    kb.mm([lambda pb=pb, k_=ktl[r], qz=qz: nc.tensor.matmul(
                        pb[:, 256:384], lhsT=k_[:dk, :], rhs=qz[:dk, ts], start=True, stop=True)],
                        reads=[ktl[r], qz], writes=[psB_at[a]])
                    az = atm[z]
                    kb.op("act", lambda pb=pb, o=atr[r]: nc.scalar.copy(out=o[:], in_=pb[:, 256:384]),
                          reads=[psB_at[a]], writes=[atr[r]])
                    kb.op("dve", lambda az=az, o=atr[r]: nc.vector.copy_predicated(
                        out=az[:, ti, :], mask=mau[:, z, :], data=o[:]),
                        reads=[atr[r], mau], writes=[], acc_writes=[az])
                    kb.op("act", lambda pb=pb, o=usb[r]: nc.scalar.copy(out=o[:dk, :], in_=pb[:dk, 0:256]),
                          reads=[psB_u[a]], writes=[usb[r]])
                    spz = Sp[z]
                    for c in ((0, 1) if z == 0 else (1, 0)):
                        ci = 2 * ti + c
                        kb.op("act", lambda S=S, spz=spz, ci=ci, e=er2[r], c=c: nc.scalar.activation(
                            out=spz[:dk, ci, :], in_=S[:dk, :], func=AF.Copy, scale=e[:dk, c:c + 1]),
                            reads=[S, er2[r]], writes=[], acc_writes=[spz])
                        S2 = Sst[(si + 1) % 2]
                        si += 1
                        kb.op("dve", lambda S=S, S2=S2, e=ed2[r], u=usb[r], c=c: nc.vector.scalar_tensor_tensor(
                            out=S2[:dk, :], in0=S[:dk, :], scalar=e[:dk, c:c + 1], in1=u[:dk, c * 128:(c + 1) * 128],
                            op0=ALU.mult, op1=ALU.add),
                            reads=[S, ed2[r], usb[r]], writes=[S2])
                        S = S2
                    return S, si

                sched = []
                for ti in order:
                    sched.append((ti, it % 2, it % R))
                    it += 1
                for idx in range(len(sched) + 1):
                    if idx < len(sched):
                        p1(*sched[idx])
                    if idx >= 1:
                        S, si = p2(*sched[idx - 1], S, si)
            groups = [list(range(g * 4, g * 4 + 4)) for g in range(8)] + [[32, 33]]
            if DBG:
                groups = []
            for gi, tiles in enumerate(groups):
                po = psO[gi % 2]
                pm = psM[gi % 2]
                nb = len(tiles) * 128
                t0 = tiles[0] * 128
                fns = []
                for k_, ti in enumerate(tiles):
                    cs = k_ * 128
                    for z in range(2):
                        fns.append(lambda po=po, ti=ti, cs=cs, z=z: nc.tensor.matmul(
                            po[:, cs:cs + 128], lhsT=vt[:, ti, :], rhs=atm[z][:, ti, :], start=(z == 0), stop=False))
                        for c in range(2):
                            fns.append(lambda po=po, ti=ti, cs=cs, z=z, c=c: nc.tensor.matmul(
                                po[:, cs + c * 64:cs + (c + 1) * 64], lhsT=Sp[z][:dk, 2 * ti + c, :],
                                rhs=qt[z][:dk, ti * 128 + c * 64:ti * 128 + (c + 1) * 64],
                                start=False, stop=(z == 1)))
                kb.mm(fns, reads=[vt, atm[0], atm[1], Sp[0], Sp[1], qt[0], qt[1]], writes=[po])
                s_, d_, o_ = sq[gi % 2], sd[gi % 2], on[gi % 2]
                kb.op("act", lambda po=po, s_=s_: nc.scalar.activation(out=s_[:, :nb], in_=po[:, :nb], func=AF.Square),
                      reads=[po], writes=[s_])
                kb.mm([lambda pm=pm, s_=s_: nc.tensor.matmul(pm[:, :nb], lhsT=onesdv[:], rhs=s_[:, :nb], start=True, stop=True)],
                      reads=[onesdv, s_], writes=[pm])
                kb.op("act", lambda pm=pm, d_=d_: nc.scalar.activation(out=d_[:, :nb], in_=pm[:, :nb], func=AF.Sqrt, bias=eps_t[:]),
                      reads=[pm, eps_t], writes=[d_])
                kb.op("dve", lambda d_=d_: nc.vector.reciprocal(out=d_[:, :nb], in_=d_[:, :nb]), reads=[d_], writes=[d_])
                ob_ = osb[gi % 2]
                kb.op("act", lambda po=po, ob_=ob_: nc.scalar.copy(out=ob_[:, :nb], in_=po[:, :nb]), reads=[po], writes=[ob_])
                kb.op("dve", lambda ob_=ob_, d_=d_, o_=o_: nc.vector.tensor_tensor(
                    out=o_[:, :nb], in0=ob_[:, :nb], in1=d_[:, :nb], op=ALU.mult), reads=[ob_, d_], writes=[o_])
                kb.op("pool", lambda o_=o_: nc.gpsimd.tensor_tensor(
                    out=obr[:, t0:t0 + nb], in0=o_[:, :nb], in1=gsil[:, t0:t0 + nb], op=ALU.mult),
                    reads=[o_, gsil], writes=[], acc_writes=[obr])
            kb.dma("sp", dest[h * dv:(h + 1) * dv, :], obr[:], reads=[obr], writes=[], acc_writes=[dest])


def scan_masks():
    j = np.arange(128)[:, None]
    i = np.arange(128)[None, :]
    same = (j // 64 == i // 64).astype(np.float32)
    out = {}
    for name, gs in (("mg", -1.0 / 16.0), ("mh", 1.0)):
        mq = np.zeros((128, 2, 130), np.float32)
        mk = np.zeros((128, 2, 128), np.float32)
        mq[:, 0, :128] = gs * same * ((j <= i).astype(np.float32) - 0.5)
        mq[:, 1, :128] = gs * same * ((j >= i).astype(np.float32) - 0.5)
        for z in range(2):
            for c in range(2):
                mq[:, z, 128 + c] = gs * (np.arange(128) // 64 == c)
        mk[:, 0, :] = gs * same * (j > i)
        mk[:, 1, :] = gs * same * (j < i)
        out[name + "_mq"] = mq
        out[name + "_mk"] = mk
    ma = np.zeros((128, 2, 128), np.float32)
    ma[:, 0, :] = same * (j <= i)
    ma[:, 1, :] = same * (j >= i)
    out["m_ma"] = ma
    out["m_mau"] = ma.astype(np.uint32)
    out["m_ident"] = np.eye(128, dtype=np.float32)
    return out


def stage_wa(cx, kb, nc, l, groups=None):
    with kb.scope():
        wm = kb.sbuf("wm", [128, 2, 128], BF16)
        kb.dma("pool", wm[:], cx.m_wa[:], reads=[cx.m_wa], writes=[wm])
        ones64 = kb.sbuf("ones64", [128, 64], BF16)
        kb.op("dve", lambda: nc.vector.memset(ones64[:], 1.0), writes=[ones64])
        sk = kb.sbuf("sk", [64, 8], F32)
        kb.dma("sp", sk[:], cx.wa_sink[l].partition_broadcast(64), reads=[cx.wa_sink], writes=[sk])
        kb.op("act", lambda: nc.scalar.activation(out=sk[:], in_=sk[:], func=AF.Exp), reads=[sk], writes=[sk])
        q4 = kb.sbuf("q4", [64, 4, T], BF16)
        kT = kb.sbuf("kTc", [64, T], BF16)
        vt = kb.sbuf("vtc", [128, NT, 64], BF16)
        ost = kb.sbuf("ost", [64, 4, T], BF16)
        pS = [kb.psum("pS%d" % i, [128, 512]) for i in range(4)]
        pO = [kb.psum("pO%d" % i, [128, 512]) for i in range(2)]
        pD = [kb.psum("pD%d" % i, [128, 512]) for i in range(2)]
        R = 10
        pt = [kb.sbuf("pt%d" % i, [128, 4, 128], BF16) for i in range(R)]
        ptm = [kb.sbuf("ptm%d" % i, [128, 4, 128], BF16) for i in range(R)]
        osb = [kb.sbuf("osbw%d" % i, [64, 4, 128], F32) for i in range(2)]
        dsb = [kb.sbuf("dsbw%d" % i, [64, 4, 128], F32) for i in range(2)]
        it = 0
        for g in (range(2) if groups is None else groups):
            kb.dma("sp", q4[:], cx.QC[g * 256:(g + 1) * 256, :].rearrange("(hh d) t -> d hh t", d=64),
                   reads=[cx.QC], writes=[q4])
            kb.dma("sp", kT[:], cx.KC[g * 64:(g + 1) * 64, :], reads=[cx.KC], writes=[kT])
            kb.dma("sp", vt[:], cx.VC_t[:, :, g * 64:(g + 1) * 64], reads=[cx.VC_t], writes=[vt])
            def p1(qb):
                nonlocal it
                if qb < 32:
                    keys = []
                    if qb > 0:
                        keys.append((qb - 1, 0))
                    keys.append((qb, None))
                    if qb < 31:
                        keys.append((qb + 1, 1))
                    keys += [(32, None), (33, None)]
                else:
                    keys = [(32, None), (33, None)]
                qs = slice(qb * 128, (qb + 1) * 128)
                st_ = []
                for ki, (kt_i, mi) in enumerate(keys):
                    ps = pS[it % 4]
                    p_ = pt[it % R]
                    pm_ = ptm[it % R]
                    it += 1
                    kb.mm([lambda ps=ps, kt_i=kt_i: nc.tensor.matmul(
                        ps[:, :], lhsT=kT[:, kt_i * 128:(kt_i + 1) * 128], rhs=q4[:, :, qs], start=True, stop=True)],
                        reads=[kT, q4], writes=[ps])
                    kb.op("act", lambda ps=ps, p_=p_: nc.scalar.activation(
                        out=p_[:].rearrange("p h q -> p (h q)"), in_=ps[:, :], func=AF.Exp, scale=0.125),
                        reads=[ps], writes=[p_])
                    src = p_
                    if mi is not None:
                        eng = "dve" if it % 2 == 0 else "pool"
                        h_ = nc.vector if eng == "dve" else nc.gpsimd
                        kb.op(eng, lambda h_=h_, p_=p_, pm_=pm_, mi=mi: h_.tensor_tensor(
                            out=pm_[:], in0=p_[:], in1=wm[:, mi, :].unsqueeze(1).broadcast_to([128, 4, 128]), op=ALU.mult),
                            reads=[p_, wm], writes=[pm_])
                        src = pm_
                    st_.append((kt_i, src))
                return (qb, st_)

            def p2(state):
                qb, st_ = state
                qs = slice(qb * 128, (qb + 1) * 128)
                a = qb % 2
                po, pd = pO[a], pD[a]
                nk = len(st_)
                for ki, (kt_i, src) in enumerate(st_):
                    kb.mm([lambda po=po, src=src, kt_i=kt_i, ki=ki: nc.tensor.matmul(
                        po[:64, :], lhsT=vt[:, kt_i, :], rhs=src[:].rearrange("p h q -> p (h q)"),
                        start=(ki == 0), stop=(ki == nk - 1)),
                        lambda pd=pd, src=src, ki=ki: nc.tensor.matmul(
                        pd[:64, :], lhsT=ones64[:], rhs=src[:].rearrange("p h q -> p (h q)"),
                        start=(ki == 0), stop=(ki == nk - 1))],
                        reads=[vt, src, ones64], writes=[po, pd])
                o_, d_ = osb[a], dsb[a]
                kb.op("act", lambda po=po, o_=o_: nc.scalar.copy(out=o_[:].rearrange("p h q -> p (h q)"), in_=po[:64, :]),
                      reads=[po], writes=[o_])
                kb.op("act", lambda pd=pd, d_=d_: nc.scalar.copy(out=d_[:].rearrange("p h q -> p (h q)"), in_=pd[:64, :]),
                      reads=[pd], writes=[d_])
                kb.op("dve", lambda d_=d_: nc.vector.tensor_tensor(
                    out=d_[:], in0=d_[:], in1=sk[:, g * 4:(g + 1) * 4].unsqueeze(2).broadcast_to([64, 4, 128]), op=ALU.add),
                    reads=[d_, sk], writes=[d_])
                kb.op("dve", lambda d_=d_: nc.vector.reciprocal(out=d_[:], in_=d_[:]), reads=[d_], writes=[d_])
                kb.op("pool", lambda o_=o_, d_=d_: nc.gpsimd.tensor_tensor(
                    out=ost[:, :, qs], in0=o_[:], in1=d_[:], op=ALU.mult),
                    reads=[o_, d_], writes=[], acc_writes=[ost])

            prev = None
            for qb in range(NT):
                cur = p1(qb)
                if prev is not None:
                    p2(prev)
                prev = cur
            p2(prev)
            kb.dma("sp", cx.BR_C[g * 256:(g + 1) * 256, :].rearrange("(hh d) t -> d hh t", d=64), ost[:],
                   reads=[ost], writes=[], acc_writes=[cx.BR_C])


def wa_masks():
    kk = np.arange(128)[:, None]
    qq = np.arange(128)[None, :]
    m = np.zeros((128, 2, 128), np.float32)
    m[:, 0, :] = (kk >= qq)
    m[:, 1, :] = (kk <= qq)
    return {"m_wa": m}


def na_patterns():
    W = 64
    tok = np.arange(128)
    lr, lc = tok // W, tok % W
    pats = []
    keys = []
    for m in range(32):
        qr = 2 * m + lr[None, :]
        qc = lc[None, :]
        rs = np.clip(qr - 4, 0, 56)
        cs = np.clip(qc - 8, 0, 48)
        lst = []
        for kt_i in range(32):
            kr = 2 * kt_i + lr[:, None]
            kc = lc[:, None]
            valid = (kr >= rs) & (kr < rs + 8) & (kc >= cs) & (kc < cs + 16)
            if not valid.any():
                continue
            ri = np.clip(kr - qr + 7, 0, 14)
            ci = np.clip(kc - qc + 15, 0, 30)
            if 2 <= m <= 29:
                pid = kt_i - m + 2
                if m == 2:
                    pats.append((valid, ri, ci))
            else:
                pid = len(pats)
                pats.append((valid, ri, ci))
            lst.append((kt_i, pid))
        keys.append(lst)
    return keys, pats


def na_consts():
    keys, pats = na_patterns()
    n0 = len(keys[0]) + len(keys[1])
    order = list(range(n0, n0 + 5)) + list(range(0, n0)) + list(range(n0 + 5, len(pats)))
    remap = {old: new for new, old in enumerate(order)}
    pats2 = [pats[o] for o in order]
    keys2 = []
    for m, lst in enumerate(keys):
        if 2 <= m <= 29:
            keys2.append(lst)
        else:
            keys2.append([(k, remap[p]) for (k, p) in lst])
    valid = np.stack([p[0] for p in pats2], 1).astype(np.float32)
    ri = np.stack([p[1] for p in pats2], 1)
    ci = np.stack([p[2] for p in pats2], 1)
    return keys2, valid, ri, ci


def stage_na(cx, kb, nc, l, heads=None):
    keys_tab, valid_np, _, _ = na_consts()
    npat = valid_np.shape[1]
    with kb.scope():
        vmask = kb.sbuf("vmask", [128, npat, 128], F32)
        kb.dma("sp", vmask[:], cx.m_navalid[:], reads=[cx.m_navalid], writes=[vmask])
        ones64 = kb.sbuf("ones64n", [128, 64], BF16)
        kb.op("dve", lambda: nc.vector.memset(ones64[:], 1.0), writes=[ones64])
        qT = kb.sbuf("qTn", [64, T], BF16)
        kT = kb.sbuf("kTn", [64, T], BF16)
        vt = kb.sbuf("vtn", [128, NT, 64], BF16)
        ost = kb.sbuf("ostn", [64, T], BF16)
        bias = kb.sbuf("biasn", [128, npat, 128], F32)
        EB = kb.sbuf("EBn", [128, npat, 128], BF16)
        pS = [kb.psum("pSn%d" % i, [128, 512]) for i in range(4)]
        pO = [kb.psum("pOn%d" % i, [128, 512]) for i in range(2)]
        pD = [kb.psum("pDn%d" % i, [128, 512]) for i in range(2)]
        R = 6
        pt = [kb.sbuf("ptn%d" % i, [128, 4, 128], BF16) for i in range(R)]
        osb = [kb.sbuf("osbn%d" % i, [64, 256], F32) for i in range(2)]
        it = 0
        for h in (range(8) if heads is None else heads):
            kb.dma("sp", bias[:], cx.na_bias[l, h], reads=[cx.na_bias], writes=[bias])
            kb.op("act", lambda: nc.scalar.activation(out=bias[:], in_=bias[:], func=AF.Exp), reads=[bias], writes=[bias])
            kb.op("pool", lambda: nc.gpsimd.tensor_tensor(out=EB[:], in0=bias[:], in1=vmask[:], op=ALU.mult),
                  reads=[bias, vmask], writes=[EB])
            kb.dma("sp", qT[:], cx.QB[h * 64:(h + 1) * 64, :], reads=[cx.QB], writes=[qT])
            kb.dma("sp", kT[:], cx.KB_[h * 64:(h + 1) * 64, :], reads=[cx.KB_], writes=[kT])
            kb.dma("sp", vt[:], cx.VB_t[:, :, h * 64:(h + 1) * 64], reads=[cx.VB_t], writes=[vt])
            def p1(qb):
                nonlocal it
                if qb < 32:
                    keys = list(keys_tab[qb]) + [(32, None), (33, None)]
                else:
                    keys = [(32, None), (33, None)]
                qs = slice(qb * 128, (qb + 1) * 128)
                grp = [keys[i:i + 4] for i in range(0, len(keys), 4)]
                st_ = []
                for gk in grp:
                    ps = pS[it % 4]
                    p_ = pt[it % R]
                    it += 1
                    ng = len(gk)
                    kb.mm([(lambda ps=ps, j=j, kt_i=kt_i: nc.tensor.matmul(
                        ps[:, j * 128:(j + 1) * 128], lhsT=kT[:, kt_i * 128:(kt_i + 1) * 128], rhs=qT[:, qs],
                        start=True, stop=True)) for j, (kt_i, _) in enumerate(gk)],
                        reads=[kT, qT], writes=[ps])
                    kb.op("act", lambda ps=ps, p_=p_, ng=ng: nc.scalar.activation(
                        out=p_[:, :ng, :].rearrange("p h q -> p (h q)"), in_=ps[:, :ng * 128], func=AF.Exp, scale=0.125),
                        reads=[ps], writes=[p_])
                    nloc = sum(1 for (_, pid) in gk if pid is not None)
                    if nloc:
                        pid0 = gk[0][1]
                        eng = "dve" if it % 2 == 0 else "pool"
                        h_ = nc.vector if eng == "dve" else nc.gpsimd
                        kb.op(eng, lambda h_=h_, p_=p_, pid0=pid0, nloc=nloc: h_.tensor_tensor(
                            out=p_[:, :nloc, :], in0=p_[:, :nloc, :], in1=EB[:, pid0:pid0 + nloc, :], op=ALU.mult),
                            reads=[p_, EB], writes=[p_])
                    st_.append((gk, p_))
                return (qb, st_, len(keys))

            def p2(state):
                qb, st_, nk = state
                qs = slice(qb * 128, (qb + 1) * 128)
                a = qb % 2
                po = pO[a]
                pd = pD[a]
                done = 0
                for gk, p_ in st_:
                    fns = []
                    for j, (kt_i, _) in enumerate(gk):
                        first = (done == 0)
                        last = (done == nk - 1)
                        fns.append(lambda po=po, p_=p_, j=j, kt_i=kt_i, first=first, last=last: nc.tensor.matmul(
                            po[:64, 0:128], lhsT=vt[:, kt_i, :], rhs=p_[:, j, :], start=first, stop=last))
                        fns.append(lambda pd=pd, p_=p_, j=j, first=first, last=last: nc.tensor.matmul(
                            pd[:64, 0:128], lhsT=ones64[:], rhs=p_[:, j, :], start=first, stop=last))
                        done += 1
                    kb.mm(fns, reads=[vt, p_, ones64], writes=[po, pd])
                o_ = osb[a]
                kb.op("act", lambda po=po, o_=o_: nc.scalar.copy(out=o_[:, 0:128], in_=po[:64, 0:128]), reads=[po], writes=[o_])
                kb.op("act", lambda pd=pd, o_=o_: nc.scalar.copy(out=o_[:, 128:256], in_=pd[:64, 0:128]), reads=[pd, o_], writes=[o_])
                kb.op("dve", lambda o_=o_: nc.vector.reciprocal(out=o_[:, 128:256], in_=o_[:, 128:256]), reads=[o_], writes=[o_])
                kb.op("pool", lambda o_=o_: nc.gpsimd.tensor_tensor(
                    out=ost[:, qs], in0=o_[:, 0:128], in1=o_[:, 128:256], op=ALU.mult),
                    reads=[o_], writes=[], acc_writes=[ost])

            prev = None
            for qb in range(NT):
                cur = p1(qb)
                if prev is not None:
                    p2(prev)
                prev = cur
            p2(prev)
            kb.dma("sp", cx.BR_B[h * 64:(h + 1) * 64, :], ost[:], reads=[ost], writes=[], acc_writes=[cx.BR_B])

LN_EPS = 1e-5
MB = 256
MBLKS = [(i * MB, MB) for i in range(T // MB)]


def ln_setup(cx, kb, nc):
    ones = kb.sbuf("ln_ones", [128, 128], F32)
    kb.op("dve", lambda: nc.vector.memset(ones[:], 1.0 / D), writes=[ones])
    eps = kb.sbuf("ln_eps", [128, 1], F32)
    kb.op("dve", lambda: nc.vector.memset(eps[:], LN_EPS), writes=[eps])
    return ones, eps


def ln_block(kb, nc, r, nb, gam, bet, out, ones, eps, scr):
    rsq, ps1, ps2, mean, var = scr
    kb.op("act", lambda: nc.scalar.activation(out=rsq[:, :, :nb], in_=r[:, :, :nb], func=AF.Square), reads=[r], writes=[rsq])
    kb.mm([(lambda oc=oc: nc.tensor.matmul(ps1[:, :nb], lhsT=ones[:], rhs=r[:, oc, :nb], start=(oc == 0), stop=(oc == 7)))
           for oc in range(8)], reads=[ones, r], writes=[ps1])
    kb.mm([(lambda oc=oc: nc.tensor.matmul(ps2[:, :nb], lhsT=ones[:], rhs=rsq[:, oc, :nb], start=(oc == 0), stop=(oc == 7)))
           for oc in range(8)], reads=[ones, rsq], writes=[ps2])
    kb.op("act", lambda: nc.scalar.copy(out=mean[:, :nb], in_=ps1[:, :nb]), reads=[ps1], writes=[mean])
    kb.op("act", lambda: nc.scalar.copy(out=var[:, :nb], in_=ps2[:, :nb]), reads=[ps2], writes=[var])
    m2 = rsq
    kb.op("dve", lambda: nc.vector.tensor_tensor(out=m2[:, 0, :nb], in0=mean[:, :nb], in1=mean[:, :nb], op=ALU.mult),
          reads=[mean, rsq], writes=[rsq])
    kb.op("dve", lambda: nc.vector.tensor_tensor(out=var[:, :nb], in0=var[:, :nb], in1=m2[:, 0, :nb], op=ALU.subtract),
          reads=[var, rsq], writes=[var])
    kb.op("act", lambda: nc.scalar.activation(out=var[:, :nb], in_=var[:, :nb], func=AF.Sqrt, bias=eps[:]),
          reads=[var, eps], writes=[var])
    kb.op("dve", lambda: nc.vector.reciprocal(out=var[:, :nb], in_=var[:, :nb]), reads=[var], writes=[var])
    kb.op("dve", lambda: nc.vector.tensor_tensor(
        out=r[:, :, :nb], in0=r[:, :, :nb], in1=mean[:, :nb].unsqueeze(1).broadcast_to([128, 8, nb]), op=ALU.subtract),
        reads=[r, mean], writes=[r])
    kb.op("pool", lambda: nc.gpsimd.tensor_tensor(
        out=r[:, :, :nb], in0=r[:, :, :nb], in1=var[:, :nb].unsqueeze(1).broadcast_to([128, 8, nb]), op=ALU.mult),
        reads=[r, var], writes=[r])
    for oc in range(8):
        eng = "dve" if oc % 2 == 0 else "pool"
        h_ = nc.vector if eng == "dve" else nc.gpsimd
        kb.op(eng, lambda h_=h_, oc=oc: h_.tensor_scalar(
            out=out[:, oc, :nb], in0=r[:, oc, :nb], scalar1=gam[:, oc:oc + 1], scalar2=bet[:, oc:oc + 1],
            op0=ALU.mult, op1=ALU.add), reads=[r, gam, bet], writes=[out])


def load_ln_params(cx, kb, nc, l, which):
    gam = kb.sbuf("ln_g", [128, 8], F32)
    bet = kb.sbuf("ln_b", [128, 8], F32)
    kb.dma("sp", gam[:], cx.ln_gT[l, which], reads=[cx.ln_gT], writes=[gam])
    kb.dma("sp", bet[:], cx.ln_bT[l, which], reads=[cx.ln_bT], writes=[bet])
    return gam, bet


def stage_merge(cx, kb, nc, l, xsrc, modv):
    with kb.scope():
        ones, eps = ln_setup(cx, kb, nc)
        gam, bet = load_ln_params(cx, kb, nc, l, 0)
        wbr = kb.sbuf("wbr", [128, 4, 4, 1024], BF16)
        wout = kb.sbuf("wout", [128, 8, 1024], BF16)
        for i in range(4):
            kb.dma("pool", wbr[:, i], cx.w_branch[l, i].rearrange("(kc p) c -> p kc c", p=128), reads=[cx.w_branch], writes=[wbr])
        kb.dma("pool", wout[:], cx.w_out[l].rearrange("(kc p) c -> p kc c", p=128), reads=[cx.w_out], writes=[wout])
        wr = kb.sbuf("wr", [128, 8, 36], F32)
        kb.dma("sp", wr[:, :, 0:4], cx.moe_w_group[l].rearrange("(kc p) c -> p kc c", p=128), reads=[cx.moe_w_group], writes=[wr])
        kb.dma("sp", wr[:, :, 4:36], cx.moe_w_expert[l].rearrange("(kc p) c -> p kc c", p=128), reads=[cx.moe_w_expert], writes=[wr])
        rb = kb.sbuf("rb", [128, 36], F32)
        kb.dma("sp", rb[:, 0:4], cx.moe_b_group[l].partition_broadcast(128), reads=[cx.moe_b_group], writes=[rb])
        kb.dma("sp", rb[:, 4:36], cx.moe_b_expert[l].partition_broadcast(128), reads=[cx.moe_b_expert], writes=[rb])
        onep4 = kb.sbuf("onep4", [128, 8, 2], F32)
        kb.op("dve", lambda: nc.vector.tensor_scalar(out=onep4[:], in0=modv[:, 32:40, :], scalar1=1.0, scalar2=None, op0=ALU.add),
              reads=[modv], writes=[onep4])
        brt = [kb.sbuf("brt%d" % i, [128, 4, 4, MB], BF16) for i in range(1)]
        gtt = kb.sbuf("gtt", [128, 32, MB], BF16)
        xb = kb.sbuf("xbm", [128, 8, MB], F32)
        mrg = kb.sbuf("mrg", [128, 8, MB], BF16)
        r = kb.sbuf("rm", [128, 8, MB], F32)
        hx2b = kb.sbuf("hx2b", [128, 8, MB], BF16)
        rsq = kb.sbuf("rsqm", [128, 8, MB], F32)
        x1 = r
        hx2f = rsq
        mean = kb.sbuf("meanm", [128, MB], F32)
        var = kb.sbuf("varm", [128, MB], F32)
        csb = [kb.sbuf("csb%d" % i, [128, MB], F32) for i in range(4)]
        acc = kb.sbuf("accm", [128, MB], F32)
        mixs = kb.sbuf("mixs", [128, MB], F32)
        pbr = [kb.psum("pbr%d" % i, [128, MB]) for i in range(4)]
        pmx = kb.psum("pmx", [128, MB])
        ps1 = kb.psum("ps1m", [128, MB])
        ps2 = kb.psum("ps2m", [128, MB])
        prt = kb.psum("prt", [128, MB])
        brs = [cx.BR_A, cx.BR_B, cx.BR_C, cx.BR_D]
        lgt = kb.sbuf("lgt", [128, 36], F32)
        gmx = kb.sbuf("gmx", [128, 1], F32)
        ngm = kb.sbuf("ngm", [128, 1], F32)
        ge = kb.sbuf("ge", [128, 4], F32)
        gs = kb.sbuf("gsr", [128, 1], F32)
        oh = kb.sbuf("oh", [128, 4], F32)
        lem = kb.sbuf("lem", [128, 4, 8], F32)
        mx8 = kb.sbuf("mx8", [128, 8], F32)
        dd = kb.sbuf("dd", [128, 1], F32)
        w1 = kb.sbuf("w1", [128, 1], F32)
        w2 = kb.sbuf("w2", [128, 1], F32)
        tt1 = kb.sbuf("tt1", [128, 32], F32)
        tt2 = kb.sbuf("tt2", [128, 32], F32)
        wgt = kb.sbuf("wgt", [128, 32], F32)
        for bi, (t0, nb) in enumerate(MBLKS):
            col = blkcol(t0)
            b_ = brt[0]
            for i in range(4):
                kb.dma("sp", b_[:, i, :, :nb], brs[i][:, t0:t0 + nb].rearrange("(kc p) t -> p kc t", p=128),
                       reads=[brs[i]], writes=[b_])
            kb.dma("sp", gtt[:, :, :nb], cx.GATE[:, t0:t0 + nb].rearrange("(j p) t -> p j t", p=128), reads=[cx.GATE], writes=[gtt])
            kb.dma("sp", xb[:, :, :nb], xsrc[:, t0:t0 + nb].rearrange("(kc p) t -> p kc t", p=128), reads=[xsrc], writes=[xb])
            for oc in range(8):
                for i in range(4):
                    kb.mm([(lambda i=i, kc=kc, oc=oc: nc.tensor.matmul(
                        pbr[i][:, :nb], lhsT=wbr[:, i, kc, oc * 128:(oc + 1) * 128], rhs=b_[:, i, kc, :nb],
                        start=(kc == 0), stop=(kc == 3))) for kc in range(4)], reads=[wbr, b_], writes=[pbr[i]])
                    kb.op("act", lambda i=i: nc.scalar.copy(out=csb[i][:, :nb], in_=pbr[i][:, :nb]), reads=[pbr[i]], writes=[csb[i]])
                    eng = "dve" if i % 2 == 0 else "pool"
                    h_ = nc.vector if eng == "dve" else nc.gpsimd
                    kb.op(eng, lambda h_=h_, i=i, oc=oc: h_.tensor_tensor(
                        out=csb[i][:, :nb], in0=csb[i][:, :nb], in1=gtt[:, i * 8 + oc, :nb], op=ALU.mult),
                        reads=[csb[i], gtt], writes=[csb[i]])
                kb.op("dve", lambda: nc.vector.tensor_tensor(out=acc[:, :nb], in0=csb[0][:, :nb], in1=csb[1][:, :nb], op=ALU.add),
                      reads=[csb[0], csb[1]], writes=[acc])
                kb.op("pool", lambda: nc.gpsimd.tensor_tensor(out=csb[2][:, :nb], in0=csb[2][:, :nb], in1=csb[3][:, :nb], op=ALU.add),
                      reads=[csb[2], csb[3]], writes=[csb[2]])
                kb.op("dve", lambda oc=oc: nc.vector.tensor_tensor(out=mrg[:, oc, :nb], in0=acc[:, :nb], in1=csb[2][:, :nb], op=ALU.add),
                      reads=[acc, csb[2]], writes=[mrg])
            for oc in range(8):
                kb.mm([(lambda kc=kc, oc=oc: nc.tensor.matmul(
                    pmx[:, :nb], lhsT=wout[:, kc, oc * 128:(oc + 1) * 128], rhs=mrg[:, kc, :nb],
                    start=(kc == 0), stop=(kc == 7))) for kc in range(8)], reads=[wout, mrg], writes=[pmx])
                kb.op("act", lambda: nc.scalar.copy(out=mixs[:, :nb], in_=pmx[:, :nb]), reads=[pmx], writes=[mixs])
                kb.op("pool", lambda oc=oc: nc.gpsimd.tensor_scalar(
                    out=mixs[:, :nb], in0=mixs[:, :nb], scalar1=modv[:, 16 + oc, col:col + 1], scalar2=None, op0=ALU.mult),
                    reads=[mixs, modv], writes=[mixs])
                kb.op("dve", lambda oc=oc: nc.vector.scalar_tensor_tensor(
                    out=r[:, oc, :nb], in0=xb[:, oc, :nb], scalar=ALPHA, in1=mixs[:, :nb], op0=ALU.mult, op1=ALU.add),
                    reads=[xb, mixs], writes=[r])
            ln_block(kb, nc, r, nb, gam, bet, x1, ones, eps, (rsq, ps1, ps2, mean, var))
            kb.dma("sp", cx.XT[:, t0:t0 + nb].rearrange("(kc p) t -> p kc t", p=128), x1[:, :, :nb],
                   reads=[x1], writes=[], acc_writes=[cx.XT])
            for oc in range(8):
                eng = "dve" if oc % 2 == 0 else "pool"
                h_ = nc.vector if eng == "dve" else nc.gpsimd
                kb.op(eng, lambda h_=h_, oc=oc: h_.tensor_scalar(
                    out=hx2f[:, oc, :nb], in0=x1[:, oc, :nb], scalar1=onep4[:, oc, col:col + 1],
                    scalar2=modv[:, 24 + oc, col:col + 1], op0=ALU.mult, op1=ALU.add),
                    reads=[x1, onep4, modv], writes=[hx2f])
            kb.op("act", lambda: nc.scalar.copy(out=hx2b[:, :, :nb], in_=hx2f[:, :, :nb]), reads=[hx2f], writes=[hx2b])
            kb.dma("sp", cx.HX2[:, t0:t0 + nb].rearrange("(kc p) t -> p kc t", p=128), hx2b[:, :, :nb],
                   reads=[hx2b], writes=[], acc_writes=[cx.HX2])
            for tj in range(nb // 128):
                ti = t0 // 128 + tj
                tsl = slice(tj * 128, (tj + 1) * 128)
                kb.mm([(lambda kc=kc: nc.tensor.matmul(prt[:, 0:36], lhsT=hx2f[:, kc, tsl], rhs=wr[:, kc, :],
                                                       start=(kc == 0), stop=(kc == 7))) for kc in range(8)],
                      reads=[hx2f, wr], writes=[prt])
                kb.op("act", lambda: nc.scalar.copy(out=lgt[:], in_=prt[:, 0:36]), reads=[prt], writes=[lgt])
                kb.op("dve", lambda: nc.vector.tensor_tensor(out=lgt[:], in0=lgt[:], in1=rb[:], op=ALU.add), reads=[lgt, rb], writes=[lgt])
                kb.op("dve", lambda: nc.vector.tensor_reduce(out=gmx[:], in_=lgt[:, 0:4], axis=AX.X, op=ALU.max), reads=[lgt], writes=[gmx])
                kb.op("dve", lambda: nc.vector.tensor_scalar(out=ngm[:], in0=gmx[:], scalar1=-1.0, scalar2=None, op0=ALU.mult), reads=[gmx], writes=[ngm])
                kb.op("act", lambda: nc.scalar.activation(out=ge[:], in_=lgt[:, 0:4], func=AF.Exp, bias=ngm[:]), reads=[lgt, ngm], writes=[ge])
                kb.op("dve", lambda: nc.vector.tensor_reduce(out=gs[:], in_=ge[:], axis=AX.X, op=ALU.add), reads=[ge], writes=[gs])
                kb.op("dve", lambda: nc.vector.reciprocal(out=gs[:], in_=gs[:]), reads=[gs], writes=[gs])
                kb.op("dve", lambda: nc.vector.tensor_scalar(out=oh[:], in0=lgt[:, 0:4], scalar1=gmx[:], scalar2=None, op0=ALU.is_equal), reads=[lgt, gmx], writes=[oh])
                kb.op("dve", lambda: nc.vector.tensor_scalar(out=oh[:], in0=oh[:], scalar1=-1.0, scalar2=1e30, op0=ALU.add, op1=ALU.mult), reads=[oh], writes=[oh])
                kb.op("dve", lambda: nc.vector.tensor_tensor(
                    out=lem[:], in0=lgt[:, 4:36].rearrange("p (g e) -> p g e", e=8), in1=oh[:].unsqueeze(2).broadcast_to([128, 4, 8]), op=ALU.add),
                    reads=[lgt, oh], writes=[lem])
                lemf = lem[:].rearrange("p g e -> p (g e)")
                kb.op("dve", lambda: nc.vector.max(out=mx8[:], in_=lem[:].rearrange("p g e -> p (g e)")), reads=[lem], writes=[mx8])
                kb.op("dve", lambda: nc.vector.tensor_tensor(out=dd[:], in0=mx8[:, 1:2], in1=mx8[:, 0:1], op=ALU.subtract), reads=[mx8], writes=[dd])
                kb.op("act", lambda: nc.scalar.activation(out=dd[:], in_=dd[:], func=AF.Exp), reads=[dd], writes=[dd])
                kb.op("dve", lambda: nc.vector.tensor_scalar(out=w1[:], in0=dd[:], scalar1=1.0, scalar2=None, op0=ALU.add), reads=[dd], writes=[w1])
                kb.op("dve", lambda: nc.vector.reciprocal(out=w1[:], in_=w1[:]), reads=[w1], writes=[w1])
                kb.op("dve", lambda: nc.vector.tensor_tensor(out=w2[:], in0=dd[:], in1=w1[:], op=ALU.mult), reads=[dd, w1], writes=[w2])
                kb.op("dve", lambda: nc.vector.tensor_scalar(
                    out=tt1[:], in0=lem[:].rearrange("p g e -> p (g e)"), scalar1=mx8[:, 0:1], scalar2=w1[:], op0=ALU.is_equal, op1=ALU.mult),
                    reads=[lem, mx8, w1], writes=[tt1])
                kb.op("dve", lambda: nc.vector.tensor_scalar(
                    out=tt2[:], in0=lem[:].rearrange("p g e -> p (g e)"), scalar1=mx8[:, 1:2], scalar2=w2[:], op0=ALU.is_equal, op1=ALU.mult),
                    reads=[lem, mx8, w2], writes=[tt2])
                kb.op("dve", lambda: nc.vector.tensor_tensor(out=tt1[:], in0=tt1[:], in1=tt2[:], op=ALU.add), reads=[tt1, tt2], writes=[tt1])
                kb.op("dve", lambda: nc.vector.tensor_scalar(out=wgt[:], in0=tt1[:], scalar1=gs[:], scalar2=None, op0=ALU.mult), reads=[tt1, gs], writes=[wgt])
                kb.dma("sp", cx.WG_t[:, ti, :], wgt[:], reads=[wgt], writes=[], acc_writes=[cx.WG_t])


def stage_moe(cx, kb, nc, l, modv, yout=None):
    with kb.scope():
        ones, eps = ln_setup(cx, kb, nc)
        gam, bet = load_ln_params(cx, kb, nc, l, 1)
        wgT = kb.sbuf("wgT", [32, T], F32)
        wgl = kb.sbuf("wgl", [128, NT, 32], F32)
        id32 = kb.sbuf("id32", [128, 128], F32)
        sel = kb.sbuf("sel", [32, 32, 128], F32)
        kb.dma("sp", wgl[:], cx.WG_t[:], reads=[cx.WG_t], writes=[wgl])
        kb.dma("sp", id32[:], cx.m_ident32[:], reads=[cx.m_ident32], writes=[id32])
        kb.dma("sp", sel[:], cx.m_sel[:], reads=[cx.m_sel], writes=[sel])
        ptr = [kb.psum("ptr%d" % i, [128, 512]) for i in range(2)]
        for ti in range(NT):
            p_ = ptr[ti % 2]
            kb.mm([lambda p_=p_, ti=ti: nc.tensor.matmul(p_[:32, 0:128], lhsT=wgl[:, ti, :], rhs=id32[:], start=True, stop=True)],
                  reads=[wgl, id32], writes=[p_])
            kb.op("act", lambda p_=p_, ti=ti: nc.scalar.copy(out=wgT[:, ti * 128:(ti + 1) * 128], in_=p_[:32, 0:128]),
                  reads=[p_], writes=[], acc_writes=[wgT])
        precast_expert_weights(cx, kb, nc, l)
        hx = kb.sbuf("hxm", [128, 8, 512], BF16)
        yacc = kb.sbuf("yacc", [128, 8, 512], F32)
        hid = kb.sbuf("hid", [128, 16, 512], BF16)
        wdg = kb.sbuf("wdg", [128, 8, 2, 1024], BF16)
        wgs = [kb.sbuf("wgs%d" % i, [128, 8, 256], BF16) for i in range(2)]
        wus = [kb.sbuf("wus%d" % i, [128, 8, 256], BF16) for i in range(2)]
        xb = kb.sbuf("xbe", [128, 8, 512], F32)
        rsq = kb.sbuf("rsqe", [128, 8, 512], F32)
        mean = kb.sbuf("meane", [128, 512], F32)
        var = kb.sbuf("vare", [128, 512], F32)
        reps = [kb.sbuf("reps%d" % i, [128, 512], F32) for i in range(2)]
        ssb = [kb.sbuf("ssb%d" % i, [128, 512], F32) for i in range(2)]
        tsb = [kb.sbuf("tsb%d" % i, [128, 512], F32) for i in range(2)]
        ytmp = [kb.sbuf("ytmp%d" % i, [128, 512], F32) for i in range(2)]
        pg = [kb.psum("pg%d" % i, [128, 512]) for i in range(2)]
        pu = [kb.psum("pu%d" % i, [128, 512]) for i in range(2)]
        py = ptr
        ps1 = kb.psum("ps1e", [128, 512])
        ps2 = kb.psum("ps2e", [128, 512])
        prep = ps1
        nw = 0
        it = 0
        for bi, (t0, nb) in enumerate(BLKS):
            col = blkcol(t0)
            kb.dma("sp", hx[:, :, :nb], cx.HX2[:, t0:t0 + nb].rearrange("(kc p) t -> p kc t", p=128), reads=[cx.HX2], writes=[hx])
            kb.dma("sp", xb[:, :, :nb], cx.XT[:, t0:t0 + nb].rearrange("(kc p) t -> p kc t", p=128), reads=[cx.XT], writes=[xb])
            for g in range(4):
                kb.dma("sp", wdg[:].rearrange("p e hc d -> p (e hc d)"), cx.WDB[g], reads=[cx.WDB], writes=[wdg])
                for e_ in range(8):
                    e = g * 8 + e_
                    wg_, wu_ = wgs[nw % 2], wus[nw % 2]
                    nw += 1
                    kb.dma("sp", wg_[:].rearrange("p kc f -> p (kc f)"), cx.WGB[e], reads=[cx.WGB], writes=[wg_])
                    kb.dma("act", wu_[:].rearrange("p kc f -> p (kc f)"), cx.WUB[e], reads=[cx.WUB], writes=[wu_])
                    rp = reps[e % 2]
                    kb.mm([lambda e=e: nc.tensor.matmul(prep[:, :nb], lhsT=sel[:, e, :], rhs=wgT[:, t0:t0 + nb], start=True, stop=True)],
                          reads=[sel, wgT], writes=[prep])
                    kb.op("act", lambda rp=rp: nc.scalar.copy(out=rp[:, :nb], in_=prep[:, :nb]), reads=[prep], writes=[rp])
                    for hc in range(2):
                        a = it % 2
                        it += 1
                        kb.mm([(lambda kc=kc, a=a, hc=hc, wg_=wg_: nc.tensor.matmul(
                            pg[a][:, :nb], lhsT=wg_[:, kc, hc * 128:(hc + 1) * 128], rhs=hx[:, kc, :nb],
                            start=(kc == 0), stop=(kc == 7))) for kc in range(8)], reads=[wg_, hx], writes=[pg[a]])
                        kb.mm([(lambda kc=kc, a=a, hc=hc, wu_=wu_: nc.tensor.matmul(
                            pu[a][:, :nb], lhsT=wu_[:, kc, hc * 128:(hc + 1) * 128], rhs=hx[:, kc, :nb],
                            start=(kc == 0), stop=(kc == 7))) for kc in range(8)], reads=[wu_, hx], writes=[pu[a]])
                        kb.op("act", lambda a=a: nc.scalar.activation(out=ssb[a][:, :nb], in_=pg[a][:, :nb], func=AF.Silu),
                              reads=[pg[a]], writes=[ssb[a]])
                        kb.op("act", lambda a=a: nc.scalar.copy(out=tsb[a][:, :nb], in_=pu[a][:, :nb]), reads=[pu[a]], writes=[tsb[a]])
                        kb.op("dve", lambda a=a: nc.vector.tensor_tensor(out=tsb[a][:, :nb], in0=tsb[a][:, :nb], in1=ssb[a][:, :nb], op=ALU.mult),
                              reads=[tsb[a], ssb[a]], writes=[tsb[a]])
                        kb.op("pool", lambda a=a, rp=rp, e_=e_, hc=hc: nc.gpsimd.tensor_tensor(
                            out=hid[:, e_ * 2 + hc, :nb], in0=tsb[a][:, :nb], in1=rp[:, :nb], op=ALU.mult),
                            reads=[tsb[a], rp], writes=[], acc_writes=[hid])
                for oc in range(8):
                    p_ = py[oc % 2]
                    fns = []
                    for j in range(16):
                        fns.append(lambda j=j, oc=oc, p_=p_: nc.tensor.matmul(
                            p_[:, :nb], lhsT=wdg[:, j // 2, j % 2, oc * 128:(oc + 1) * 128], rhs=hid[:, j, :nb],
                            start=(j == 0), stop=(j == 15)))
                    kb.mm(fns, reads=[wdg, hid], writes=[p_])
                    if g == 0:
                        kb.op("act", lambda oc=oc, p_=p_: nc.scalar.copy(out=yacc[:, oc, :nb], in_=p_[:, :nb]), reads=[p_], writes=[], acc_writes=[yacc])
                    else:
                        yt = ytmp[oc % 2]
                        kb.op("act", lambda yt=yt, p_=p_: nc.scalar.copy(out=yt[:, :nb], in_=p_[:, :nb]), reads=[p_], writes=[yt])
                        eng = "dve" if oc % 2 == 0 else "pool"
                        h_ = nc.vector if eng == "dve" else nc.gpsimd
                        kb.op(eng, lambda h_=h_, oc=oc, yt=yt: h_.tensor_tensor(
                            out=yacc[:, oc, :nb], in0=yacc[:, oc, :nb], in1=yt[:, :nb], op=ALU.add),
                            reads=[yt, yacc], writes=[], acc_writes=[yacc])
            for oc in range(8):
                kb.op("pool", lambda oc=oc: nc.gpsimd.tensor_scalar(
                    out=yacc[:, oc, :nb], in0=yacc[:, oc, :nb], scalar1=modv[:, 40 + oc, col:col + 1], scalar2=None, op0=ALU.mult),
                    reads=[yacc, modv], writes=[yacc])
                kb.op("dve", lambda oc=oc: nc.vector.scalar_tensor_tensor(
                    out=yacc[:, oc, :nb], in0=xb[:, oc, :nb], scalar=ALPHA, in1=yacc[:, oc, :nb], op0=ALU.mult, op1=ALU.add),
                    reads=[xb, yacc], writes=[yacc])
            ln_block(kb, nc, yacc, nb, gam, bet, yacc, ones, eps, (rsq, ps1, ps2, mean, var))
            kb.dma("sp", cx.XT[:, t0:t0 + nb].rearrange("(kc p) t -> p kc t", p=128), yacc[:, :, :nb],
                   reads=[yacc], writes=[], acc_writes=[cx.XT])
            if yout is not None and t0 < TL:
                kb.dma("sp", yout[:, t0:t0 + nb].rearrange("(kc p) t -> p kc t", p=128), yacc[:, :, :nb],
                       reads=[yacc], writes=[], acc_writes=[yout])


def precast_expert_weights(cx, kb, nc, l):
    with kb.scope():
        stw = [kb.sbuf("pcw%d" % i, [128, 8, 256], BF16) for i in range(4)]
        std = [kb.sbuf("pcd%d" % i, [128, 16, 1024], BF16) for i in range(2)]
        n = 0
        for g in range(4):
            d_ = std[g % 2]
            kb.dma("pool", d_[:], cx.moe_w_down[l, g].rearrange("e (hc p) d -> p (e hc) d", p=128), reads=[cx.moe_w_down], writes=[d_])
            kb.dma("sp", cx.WDB[g], d_[:].rearrange("p j d -> p (j d)"), reads=[d_], writes=[], acc_writes=[cx.WDB])
            for e_ in range(8):
                e = g * 8 + e_
                for src, dst in ((cx.moe_w_gate, cx.WGB), (cx.moe_w_up, cx.WUB)):
                    w_ = stw[n % 4]
                    n += 1
                    kb.dma("pool", w_[:], src[l, g, e_].rearrange("(kc p) f -> p kc f", p=128), reads=[src], writes=[w_])
                    kb.dma("sp", dst[e], w_[:].rearrange("p kc f -> p (kc f)"), reads=[w_], writes=[], acc_writes=[dst])


def rope_tables():
    t = np.arange(TL)
    row = (t // 64).astype(np.float32)
    colp = (t % 64).astype(np.float32)
    nf = 16
    inv = (np.float32(10000.0) ** (-np.arange(nf, dtype=np.float32) / np.float32(nf))).astype(np.float32)
    cos = np.ones((128, T), np.float32)
    sin = np.zeros((128, T), np.float32)
    for q in range(128):
        d = q % 64
        pos = row if d < 32 else colp
        ang = (pos * inv[d % 16]).astype(np.float32)
        cos[q, :TL] = np.cos(ang).astype(np.float32)
        sin[q, :TL] = np.sin(ang).astype(np.float32)
    pt = np.zeros((128, 128), np.float32)
    for m in range(128):
        if (m % 32) < 16:
            pt[m + 16, m] = -1.0
        else:
            pt[m - 16, m] = 1.0
    return cos, sin, pt


_CONST = {}
_NAIDX = {}


def consts():
    if not _CONST:
        cos, sin, pt = rope_tables()
        _CONST.update(rope_cos=cos, rope_sin=sin, rope_pt=pt)
        _CONST.update(scan_masks())
        _CONST.update(wa_masks())
        sel = np.zeros((32, 32, 128), np.float32)
        for e in range(32):
            sel[e, e, :] = 1.0
        _CONST["m_sel"] = sel
        _CONST["m_ident32"] = np.eye(128, dtype=np.float32)
        _k, _valid, _ri, _ci = na_consts()
        _CONST["m_navalid"] = _valid
        _NAIDX["ri"] = _ri
        _NAIDX["ci"] = _ci
    return _CONST


def core_inputs(inp, b):
    f = lambda a: np.ascontiguousarray(a, dtype=np.float32)
    m = {}
    m["xin"] = f(np.concatenate([inp["x"][b].T, inp["ctx"][b].T], axis=1))
    cv = np.stack([inp["c"][b].reshape(8, 128).T, inp["c_ctx"].reshape(8, 128).T], axis=2)
    m["cvec"] = f(cv)
    m["w_mod"] = f(inp["w_mod"])
    m["b_modT"] = f(inp["b_mod"].reshape(2, 48, 128).transpose(0, 2, 1))
    m["w_in"] = f(inp["w_in"])
    m["hg_gamma"] = f(inp["hg_gamma"])
    m["gla_lr_w"] = f(inp["gla_lr_w"])
    m["gla_lr_b"] = f(inp["gla_lr_b"])
    m["wa_sink"] = f(inp["wa_sink"])
    for k in ("w_branch", "w_out", "moe_w_group", "moe_b_group", "moe_w_expert", "moe_b_expert",
              "moe_w_gate", "moe_w_up", "moe_w_down"):
        m[k] = f(inp[k])
    m["ln_gT"] = f(inp["ln_g"].reshape(2, 2, 8, 128).transpose(0, 1, 3, 2))
    m["ln_bT"] = f(inp["ln_b"].reshape(2, 2, 8, 128).transpose(0, 1, 3, 2))
    consts()
    m["na_bias"] = f(inp["na_rpb"][:, :, _NAIDX["ri"], _NAIDX["ci"]])
    m.update(consts())
    return m


FUSED = 1


def build_program(layers, final):
    nc = bass.Bass("TRN2", target_bir_lowering=False)
    cx = Ctx()
    declare_inputs(cx, nc)
    if final:
        yout = Buf(nc.dram_tensor("yout", [D, TL], F32, kind="ExternalOutput"), "yout")
    else:
        yout = None
    with ExitStack() as st:
        kb = KB(nc, st)
        declare_scratch(cx, kb, debug=False, as_output=(() if final else ("XT",)))
        modT = [kb.sbuf("modT%d" % l, [128, 48, 2], F32) for l in range(2)]
        stage_mod(cx, kb, nc, modT)
        first = True
        for l in layers:
            xsrc = cx.xin if first else cx.XT
            first = False
            with kb.scope():
                lbt = [kb.sbuf("lbt%d" % i, [128, 1024], F32) for i in range(2)]
                omlt = [kb.sbuf("omlt%d" % i, [128, 1024], F32) for i in range(2)]
                stage_lb(cx, kb, nc, lbt, omlt)
                hxT = kb.sbuf("hxT", [128, 8, T], BF16)
                stage_modulate(cx, kb, nc, xsrc, modT[l], 0, 1, hxT)
                stage_inproj_fm(cx, kb, nc, l, hxT)
                stage_inproj_tm(cx, kb, nc, l, hxT, lbt[l], omlt[l])
            stage_gla_gate(cx, kb, nc, l)
            stage_scan(cx, kb, nc, "gla")
            stage_scan(cx, kb, nc, "hgrn")
            stage_wa(cx, kb, nc, l)
            stage_na(cx, kb, nc, l)
            stage_merge(cx, kb, nc, l, xsrc, modT[l])
            stage_moe(cx, kb, nc, l, modT[l], yout if (final and l == layers[-1]) else None)
        if final:
            kb.finish([yout])
        else:
            kb.finish([cx.XT])
    return nc


def kernel(**inputs):
    inp = {k: np.asarray(v) for k, v in inputs.items()}
    in_maps = [core_inputs(inp, b) for b in range(4)]
    if FUSED:
        nc = build_program([0, 1], True)
        res = run_bass_kernel_spmd(nc, in_maps, core_ids=list(range(4)))
    else:
        nc0 = build_program([0], False)
        res0 = run_bass_kernel_spmd(nc0, in_maps, core_ids=list(range(4)))
        for b in range(4):
            in_maps[b]["xin"] = np.ascontiguousarray(res0.results[b]["XT"])
        nc1 = build_program([1], True)
        res = run_bass_kernel_spmd(nc1, in_maps, core_ids=list(range(4)))
    out = np.stack([np.ascontiguousarray(res.results[b]["yout"].T) for b in range(4)], axis=0)
    return out.astype(np.float32)
```

```python
import numpy as np
from contextlib import ExitStack
import concourse.bass as bass
import concourse.mybir as mybir
from concourse.bass_utils import run_bass_kernel_spmd
import os

F32 = mybir.dt.float32
BF16 = mybir.dt.bfloat16
U32 = mybir.dt.uint32
AF = mybir.ActivationFunctionType
ALU = mybir.AluOpType
AX = mybir.AxisListType


class Buf:
    __slots__ = ("t", "w", "r", "name")

    def __init__(self, t, name=""):
        self.t = t
        self.w = []
        self.r = []
        self.name = name

    def __getitem__(self, idx):
        return self.t[idx]


class Dep:
    __slots__ = ("w", "r")

    def __init__(self):
        self.w = []
        self.r = []


class Eng:
    def __init__(self, name, h, sem):
        self.name = name
        self.h = h
        self.sem = sem
        self.count = 0
        self.seen = {}


class KB:
    def __init__(self, nc, stack, n_dma_sems=8):
        self.nc = nc
        self.stack = stack
        self.sems = {}
        self.engs = {}
        for name, h in (("pe", nc.tensor), ("act", nc.scalar), ("dve", nc.vector),
                        ("pool", nc.gpsimd), ("sp", nc.sync)):
            s = stack.enter_context(nc.semaphore("s_" + name))
            self.sems[id(s)] = s
            self.engs[name] = Eng(name, h, s)
        self.dma_pool = {}
        for q in ("sp", "pool", "act"):
            lst = []
            for i in range(n_dma_sems):
                s = stack.enter_context(nc.semaphore("d_%s%d" % (q, i)))
                self.sems[id(s)] = s
                lst.append([s, 0])
            self.dma_pool[q] = [lst, 0]
        self.ninstr = 0

    def scope(self):
        kb = self

        class _Scope:
            def __enter__(self_s):
                self_s.prev = kb.stack
                self_s.st = ExitStack()
                self_s.st.__enter__()
                kb.stack = self_s.st
                kb.nscope = getattr(kb, "nscope", 0) + 1
                return self_s

            def __exit__(self_s, *a):
                if a[0] is None:
                    kb.barrier()
                kb.stack = self_s.prev
                return self_s.st.__exit__(*a)
        return _Scope()

    def sbuf(self, name, shape, dtype):
        name = "%s_s%d" % (name, getattr(self, "nscope", 0))
        t = self.stack.enter_context(self.nc.sbuf_tensor(name, list(shape), dtype))
        return Buf(t, name)

    def psum(self, name, shape, dtype=F32):
        name = "%s_s%d" % (name, getattr(self, "nscope", 0))
        t = self.stack.enter_context(self.nc.psum_tensor(name, list(shape), dtype))
        return Buf(t, name)

    def dram(self, name, shape, dtype, kind="Internal"):
        t = self.nc.dram_tensor(name, list(shape), dtype, kind=kind)
        return Buf(t, name)

    def _wait(self, e, deps):
        need = {}
        for (sid, val) in deps:
            if need.get(sid, 0) < val:
                need[sid] = val
        for sid, val in need.items():
            if e.seen.get(sid, 0) < val:
                e.h.wait_ge(self.sems[sid], val)
                e.seen[sid] = val
                self.ninstr += 1

    def _collect(self, reads, writes, acc_writes=()):
        deps = []
        for b in reads:
            deps += b.w
        for b in writes:
            deps += b.w
            deps += b.r
        for b in acc_writes:
            deps += b.r
        return deps

    @staticmethod
    def _compact(lst):
        mx = {}
        for (sid, v) in lst:
            if mx.get(sid, 0) < v:
                mx[sid] = v
        return list(mx.items())

    def _commit(self, token, reads, writes, acc_writes=()):
        for b in acc_writes:
            b.w.append(token)
            if len(b.w) > 64:
                b.w = self._compact(b.w)
        for b in reads:
            b.r.append(token)
            if len(b.r) > 64:
                mx = {}
                for (sid, v) in b.r:
                    if mx.get(sid, 0) < v:
                        mx[sid] = v
                b.r = list(mx.items())
        for b in writes:
            b.w = [token]
            b.r = []

    def _cutcheck(self):
        import os
        cut = os.environ.get("KB_CUT")
        self.nops = getattr(self, "nops", 0) + 1
        skip = os.environ.get("KB_SKIP")
        if skip and self.nops in [int(x) for x in skip.split(",")]:
            return True
        return cut is not None and self.nops > int(cut)

    def op(self, eng, fn, reads=(), writes=(), same_engine_sync=True, acc_writes=()):
        if self._cutcheck():
            return None
        e = self.engs[eng]
        deps = self._collect(reads, writes, acc_writes)
        if not same_engine_sync:
            deps = [d for d in deps if d[0] != id(e.sem)]
        self._wait(e, deps)
        ins = fn()
        e.count += 1
        ins.then_inc(e.sem, 1)
        self.ninstr += 1
        token = (id(e.sem), e.count)
        self._commit(token, reads, writes, acc_writes)
        return token

    def mm(self, fns, reads=(), writes=()):
        if self._cutcheck():
            return None
        e = self.engs["pe"]
        deps = self._collect(reads, writes)
        deps = [d for d in deps if d[0] != id(e.sem)]
        self._wait(e, deps)
        ins = None
        for fn in fns:
            ins = fn()
            self.ninstr += 1
        e.count += 1
        ins.then_inc(e.sem, 1)
        token = (id(e.sem), e.count)
        self._commit(token, reads, writes)
        return token

    def dma(self, q, out, in_, reads=(), writes=(), acc_writes=(), **kw):
        e = self.engs[q]
        pool = self.dma_pool[q]
        lst, idx = pool
        ent = lst[idx % len(lst)]
        pool[1] = idx + 1
        s, total = ent
        deps = self._collect(reads, writes, acc_writes)
        deps.append((id(s), total))
        import os
        if os.environ.get("DMA_SERIAL") and getattr(self, "_last_dma", {}).get(q):
            deps.append(self._last_dma[q])
        self._wait(e, deps)
        ins = e.h.dma_start(out=out, in_=in_, **kw)
        ins.then_inc(s, 16)
        ent[1] = total + 16
        self.ninstr += 1
        token = (id(s), total + 16)
        if not hasattr(self, "_last_dma"):
            self._last_dma = {}
        self._last_dma[q] = token
        self._commit(token, reads, writes, acc_writes)
        return token

    def init_memory(self, nbytes=196608):
        nc = self.nc
        with self.scope():
            n = nbytes // 4
            big = self.sbuf("initbig", [128, n], F32)
            q = n // 4
            self.op("dve", lambda: nc.vector.memset(big[:, 0:q], 0.0), writes=[])
            self.op("pool", lambda: nc.gpsimd.memset(big[:, q:2 * q], 0.0), writes=[])
            self.op("dve", lambda: nc.vector.memset(big[:, 2 * q:3 * q], 0.0), writes=[])
            self.op("pool", lambda: nc.gpsimd.memset(big[:, 3 * q:n], 0.0), writes=[])
            pss = [self.psum("initps%d" % i, [128, 512]) for i in range(8)]
            for p in pss:
                self.op("dve", lambda p=p: nc.vector.memset(p[:], 0.0), writes=[])

    def barrier(self):
        allt = []
        for e in self.engs.values():
            if e.count:
                allt.append((id(e.sem), e.count))
        for q, (lst, _) in self.dma_pool.items():
            for s, total in lst:
                if total:
                    allt.append((id(s), total))
        for e in self.engs.values():
            self._wait(e, allt)

    def finish(self, tokens_bufs):
        e = self.engs["sp"]
        deps = []
        for b in tokens_bufs:
            deps += b.w
        self._wait(e, deps)

T = 4352
TL = 4096

D = 1024
TL = 4096
TC = 256
T = TL + TC
NT = T // 128
BLKS = [(i * 512, 512) for i in range(8)] + [(4096, 256)]
DIN = 10528
ALPHA = (2.0 * 2) ** 0.25

COLS = dict(a_q=(0, 256), a_k=(256, 256), a_v=(512, 512), a_g=(1024, 512), a_lr=(1536, 32),
            b_q=(1568, 512), b_k=(2080, 512), b_v=(2592, 512),
            c_q=(3104, 512), c_k=(3616, 128), c_v=(3744, 128),
            d_q=(3872, 512), d_f=(4384, 1024), d_i=(5408, 512), d_g=(5920, 512), gate=(6432, 4096))


def blkcol(t0):
    return 1 if t0 >= TL else 0


class Ctx:
    pass


def declare_inputs(cx, nc):
    def din(name, shape, dt=F32):
        return Buf(nc.dram_tensor(name, list(shape), dt, kind="ExternalInput"), name)
    cx.xin = din("xin", [D, T])
    cx.cvec = din("cvec", [128, 8, 2])
    cx.w_mod = din("w_mod", [2, D, 6 * D])
    cx.b_modT = din("b_modT", [2, 128, 48])
    cx.w_in = din("w_in", [2, D, DIN])
    cx.rope_cos = din("rope_cos", [128, T])
    cx.rope_sin = din("rope_sin", [128, T])
    cx.rope_pt = din("rope_pt", [128, 128])
    cx.hg_gamma = din("hg_gamma", [2, 2, 512])
    cx.gla_lr_w = din("gla_lr_w", [2, 2, 16, 256])
    cx.gla_lr_b = din("gla_lr_b", [2, 2, 256])
    for nm in ("mg", "mh"):
        setattr(cx, nm + "_mq", din(nm + "_mq", [128, 2, 130]))
        setattr(cx, nm + "_mk", din(nm + "_mk", [128, 2, 128]))
    cx.m_ma = din("m_ma", [128, 2, 128])
    cx.m_ident = din("m_ident", [128, 128])
    cx.m_mau = din("m_mau", [128, 2, 128], U32)
    cx.m_wa = din("m_wa", [128, 2, 128])
    cx.wa_sink = din("wa_sink", [2, 8])
    cx.m_navalid = din("m_navalid", [128, 21, 128])
    cx.w_branch = din("w_branch", [2, 4, 512, 1024])
    cx.w_out = din("w_out", [2, 1024, 1024])
    cx.ln_gT = din("ln_gT", [2, 2, 128, 8])
    cx.ln_bT = din("ln_bT", [2, 2, 128, 8])
    cx.moe_w_group = din("moe_w_group", [2, 1024, 4])
    cx.moe_b_group = din("moe_b_group", [2, 4])
    cx.moe_w_expert = din("moe_w_expert", [2, 1024, 32])
    cx.moe_b_expert = din("moe_b_expert", [2, 32])
    cx.moe_w_gate = din("moe_w_gate", [2, 4, 8, 1024, 256])
    cx.moe_w_up = din("moe_w_up", [2, 4, 8, 1024, 256])
    cx.moe_w_down = din("moe_w_down", [2, 4, 8, 256, 1024])
    cx.m_sel = din("m_sel", [32, 32, 128])
    cx.m_ident32 = din("m_ident32", [128, 128])
    cx.na_bias = din("na_bias", [2, 8, 128, 21, 128])


def declare_scratch(cx, kb, debug, as_input=(), as_output=()):
    kind = "ExternalOutput" if debug else "Internal"

    def ds(name, shape, dt):
        b = kb.dram(name, shape, dt, kind=("ExternalInput" if name in as_input else ("ExternalOutput" if name in as_output else kind)))
        setattr(cx, name, b)
        return b
    ds("XT", [D, T], F32)
    ds("QA", [256, T], BF16)
    ds("GA", [512, T], BF16)
    ds("LR1", [33, T], F32)
    ds("QB", [512, T], BF16)
    ds("KB_", [512, T], BF16)
    ds("QC", [512, T], BF16)
    ds("KC", [128, T], BF16)
    ds("QD", [512, T], BF16)
    ds("GD", [512, T], BF16)
    ds("GATE", [4096, T], BF16)
    ds("KVA_t", [128, NT, 768], BF16)
    ds("VB_t", [128, NT, 512], BF16)
    ds("VC_t", [128, NT, 128], BF16)
    ds("VD_t", [128, NT, 512], BF16)
    ds("LGD_t", [128, NT, 1024], F32)
    ds("KD_t", [128, NT, 1024], BF16)
    ds("LGA_t", [128, NT, 512], F32)
    ds("MODT", [2, 128, 96], F32)
    ds("HX2", [D, T], BF16)
    ds("WGB", [32, 128, 2048], BF16)
    ds("WUB", [32, 128, 2048], BF16)
    ds("WDB", [4, 128, 16384], BF16)
    ds("WG_t", [128, NT, 32], F32)
    ds("WGT", [32, T], F32)
    ds("BR_A", [512, T], BF16)
    ds("BR_B", [512, T], BF16)
    ds("BR_C", [512, T], BF16)
    ds("BR_D", [512, T], BF16)


def stage_mod(cx, kb, nc, modT):
    with kb.scope():
        cv = kb.sbuf("cv", [128, 8, 2], F32)
        sc = kb.sbuf("sc", [128, 8, 2], F32)
        kb.dma("sp", cv[:], cx.cvec[:], reads=[cx.cvec], writes=[cv])
        kb.op("act", lambda: nc.scalar.activation(out=sc[:], in_=cv[:], func=AF.Silu), reads=[cv], writes=[sc])
        wbufs = [kb.sbuf("wm%d" % i, [128, 8, 1024], F32) for i in range(2)]
        bm = kb.sbuf("bm", [128, 48], F32)
        ps = kb.psum("ps_mod", [128, 96])
        wi = 0
        for l in range(2):
            kb.dma("sp", bm[:], cx.b_modT[l], reads=[cx.b_modT], writes=[bm])
            for g in range(6):
                wb = wbufs[wi % 2]
                wi += 1
                src = cx.w_mod[l, :, g * 1024:(g + 1) * 1024].rearrange("(kc p) c -> p kc c", p=128)
                kb.dma("sp", wb[:], src, reads=[cx.w_mod], writes=[wb])
                for j in range(8):
                    jj = g * 8 + j
                    fns = []
                    for kc in range(8):
                        fns.append(lambda kc=kc, j=j, jj=jj, wb=wb: nc.tensor.matmul(
                            ps[:, 2 * jj:2 * jj + 2], lhsT=wb[:, kc, j * 128:(j + 1) * 128], rhs=sc[:, kc, :],
                            start=(kc == 0), stop=(kc == 7)))
                    kb.mm(fns, reads=[wb, sc], writes=[ps])
            m = modT[l]
            kb.op("dve", lambda m=m: nc.vector.tensor_tensor(
                out=m[:], in0=ps[:].rearrange("p (j c) -> p j c", c=2),
                in1=bm[:].unsqueeze(2).broadcast_to([128, 48, 2]), op=ALU.add),
                reads=[ps, bm], writes=[m])
            kb.dma("sp", cx.MODT[l], m[:].rearrange("p j c -> p (j c)"), reads=[m], writes=[cx.MODT])


def stage_modulate(cx, kb, nc, src_dram, modv, shift_idx, scale_idx, hxT, hx32=None):
    with kb.scope():
        onep = kb.sbuf("onep", [128, 8, 2], F32)
        kb.op("dve", lambda: nc.vector.tensor_scalar(
            out=onep[:], in0=modv[:, scale_idx * 8:(scale_idx + 1) * 8, :], scalar1=1.0, scalar2=None, op0=ALU.add),
            reads=[modv], writes=[onep])
        xb = [kb.sbuf("xb%d" % i, [128, 8, 512], F32) for i in range(2)]
        for bi, (t0, nb) in enumerate(BLKS):
            col = blkcol(t0)
            x = xb[bi % 2]
            kb.dma("sp", x[:, :, :nb], src_dram[:, t0:t0 + nb].rearrange("(kc p) t -> p kc t", p=128),
                   reads=[src_dram], writes=[x])
            for kc in range(8):
                eng = "dve" if kc % 2 == 0 else "pool"
                h = nc.vector if eng == "dve" else nc.gpsimd
                kb.op(eng, lambda h=h, kc=kc, x=x: h.tensor_scalar(
                    out=hxT[:, kc, t0:t0 + nb], in0=x[:, kc, :nb],
                    scalar1=onep[:, kc, col:col + 1], scalar2=modv[:, shift_idx * 8 + kc, col:col + 1],
                    op0=ALU.mult, op1=ALU.add), reads=[x, onep, modv], writes=[hxT])


FM_GROUPS = [
    ("a_q", "QA", "copy"), ("a_g", "GA", "silu"), ("a_lr", "LR1", "copy32"),
    ("b_q", "QB", "copy"), ("b_k", "KB_", "copy"),
    ("c_q", "QC", "rope"), ("c_k", "KC", "rope"),
    ("d_q", "QD", "silu"), ("d_g", "GD", "silu"), ("gate", "GATE", "sigmoid"),
]


def stage_inproj_fm(cx, kb, nc, l, hxT, only=None):
    with kb.scope():
        wsl = [kb.sbuf("wsl%d" % i, [128, 8, 512], BF16) for i in range(2)]
        stg = [kb.sbuf("stg%d" % i, [128, T], BF16) for i in range(2)]
        stg32 = kb.sbuf("stg32", [32, T], F32)
        pss = [kb.psum("psf%d" % i, [128, 512]) for i in range(4)]
        psr = [kb.psum("psr%d" % i, [128, 512]) for i in range(2)]
        cosT = kb.sbuf("cosT", [128, T], F32)
        sinT = kb.sbuf("sinT", [128, T], F32)
        ptm = kb.sbuf("ptm", [128, 128], BF16)
        tmpb = [kb.sbuf("tmpb%d" % i, [128, 512], BF16) for i in range(2)]
        t1 = [kb.sbuf("t1_%d" % i, [128, 512], F32) for i in range(2)]
        t2 = [kb.sbuf("t2_%d" % i, [128, 512], F32) for i in range(2)]
        ones = kb.sbuf("ones1", [1, T], F32)
        kb.dma("sp", cosT[:], cx.rope_cos[:], reads=[cx.rope_cos], writes=[cosT])
        kb.dma("sp", sinT[:], cx.rope_sin[:], reads=[cx.rope_sin], writes=[sinT])
        kb.dma("pool", ptm[:], cx.rope_pt[:], reads=[cx.rope_pt], writes=[ptm])
        kb.op("dve", lambda: nc.vector.memset(ones[:], 1.0), writes=[ones])
        kb.dma("sp", cx.LR1[32:33, :], ones[:], reads=[ones], writes=[], acc_writes=[cx.LR1])
        nsl = 0
        nps = 0
        nst = 0
        nrp = 0
        for (name, dest, kind) in FM_GROUPS:
            if only is not None and name not in only:
                continue
            c0, nc_ = COLS[name]
            dbuf = getattr(cx, dest)
            for s0 in range(0, nc_, 512):
                sw = min(512, nc_ - s0)
                w = wsl[nsl % 2]
                nsl += 1
                kb.dma("pool", w[:, :, :sw],
                       cx.w_in[l, :, c0 + s0:c0 + s0 + sw].rearrange("(kc p) c -> p kc c", p=128),
                       reads=[cx.w_in], writes=[w])
                for m0 in range(0, sw, 128):
                    mw = min(128, sw - m0)
                    if kind == "copy32":
                        st = stg32
                    else:
                        st = stg[nst % 2]
                        nst += 1
                    for bi, (t0, nb) in enumerate(BLKS):
                        ps = pss[nps % 4]
                        nps += 1
                        fns = [(lambda kc=kc, ps=ps, w=w: nc.tensor.matmul(
                            ps[:mw, :nb], lhsT=w[:, kc, m0:m0 + mw], rhs=hxT[:, kc, t0:t0 + nb],
                            start=(kc == 0), stop=(kc == 7))) for kc in range(8)]
                        kb.mm(fns, reads=[w, hxT], writes=[ps])
                        o = st[:mw, t0:t0 + nb]
                        if kind in ("copy", "copy32"):
                            if nps % 2 == 0:
                                kb.op("dve", lambda ps=ps, o=o: nc.vector.tensor_copy(out=o, in_=ps[:mw, :nb]),
                                      reads=[ps], writes=[st])
                            else:
                                kb.op("act", lambda ps=ps, o=o: nc.scalar.copy(out=o, in_=ps[:mw, :nb]),
                                      reads=[ps], writes=[st])
                        elif kind == "silu":
                            kb.op("act", lambda ps=ps, o=o: nc.scalar.activation(out=o, in_=ps[:mw, :nb], func=AF.Silu),
                                  reads=[ps], writes=[st])
                        elif kind == "sigmoid":
                            kb.op("act", lambda ps=ps, o=o: nc.scalar.activation(out=o, in_=ps[:mw, :nb], func=AF.Sigmoid),
                                  reads=[ps], writes=[st])
                        elif kind == "rope":
                            tb = tmpb[nrp % 2]
                            a1 = t1[nrp % 2]
                            a2 = t2[nrp % 2]
                            pr = psr[nrp % 2]
                            nrp += 1
                            kb.op("act", lambda ps=ps, tb=tb: nc.scalar.copy(out=tb[:mw, :nb], in_=ps[:mw, :nb]),
                                  reads=[ps], writes=[tb])
                            kb.mm([lambda pr=pr, tb=tb: nc.tensor.matmul(pr[:mw, :nb], lhsT=ptm[:mw, :mw], rhs=tb[:mw, :nb],
                                                                         start=True, stop=True)],
                                  reads=[ptm, tb], writes=[pr])
                            kb.op("dve", lambda tb=tb, a1=a1: nc.vector.tensor_tensor(
                                out=a1[:mw, :nb], in0=tb[:mw, :nb], in1=cosT[:mw, t0:t0 + nb], op=ALU.mult),
                                reads=[tb, cosT], writes=[a1])
                            kb.op("dve", lambda pr=pr, a2=a2: nc.vector.tensor_tensor(
                                out=a2[:mw, :nb], in0=pr[:mw, :nb], in1=sinT[:mw, t0:t0 + nb], op=ALU.mult),
                                reads=[pr, sinT], writes=[a2])
                            kb.op("pool", lambda a1=a1, a2=a2, o=o: nc.gpsimd.tensor_tensor(
                                out=o, in0=a1[:mw, :nb], in1=a2[:mw, :nb], op=ALU.add),
                                reads=[a1, a2], writes=[st])
                    kb.dma("sp", dbuf[s0 + m0:s0 + m0 + mw, :], st[:mw, :], reads=[st], writes=[], acc_writes=[dbuf])


TM_GROUPS = [
    (256, 512, "KVA_t", 0, "copy"), (768, 256, "KVA_t", 512, "copy"),
    (2592, 512, "VB_t", 0, "copy"), (3744, 128, "VC_t", 0, "copy"),
    (5408, 512, "VD_t", 0, "copy"),
    (4384, 512, None, 0, "hgf"), (4896, 512, None, 512, "hgf"),
]


def stage_inproj_tm(cx, kb, nc, l, hxT, lbt, omlt, only=None):
    with kb.scope():
        wsl = [kb.sbuf("wtl%d" % i, [128, 8, 512], BF16) for i in range(2)]
        pss = [kb.psum("pst%d" % i, [128, 512]) for i in range(4)]
        stb = [kb.sbuf("stb%d" % i, [128, 512], BF16) for i in range(4)]
        e1 = [kb.sbuf("e1_%d" % i, [128, 512], F32) for i in range(2)]
        r1 = [kb.sbuf("r1_%d" % i, [128, 512], F32) for i in range(2)]
        lg = [kb.sbuf("lg_%d" % i, [128, 512], F32) for i in range(2)]
        kk = [kb.sbuf("kk_%d" % i, [128, 512], BF16) for i in range(2)]
        nsl = nps = nst = nh = 0
        for (c0, ncol, dest, doff, kind) in TM_GROUPS:
            if only is not None and kind not in only:
                continue
            w = wsl[nsl % 2]
            nsl += 1
            kb.dma("pool", w[:, :, :ncol], cx.w_in[l, :, c0:c0 + ncol].rearrange("(kc p) c -> p kc c", p=128),
                   reads=[cx.w_in], writes=[w])
            for ti in range(NT):
                ts = slice(ti * 128, (ti + 1) * 128)
                ps = pss[nps % 4]
                nps += 1
                fns = [(lambda kc=kc, ps=ps, w=w: nc.tensor.matmul(
                    ps[:, :ncol], lhsT=hxT[:, kc, ts], rhs=w[:, kc, :ncol],
                    start=(kc == 0), stop=(kc == 7))) for kc in range(8)]
                kb.mm(fns, reads=[w, hxT], writes=[ps])
                if kind == "copy":
                    st = stb[nst % 4]
                    nst += 1
                    if nst % 2 == 0:
                        kb.op("dve", lambda ps=ps, st=st: nc.vector.tensor_copy(out=st[:, :ncol], in_=ps[:, :ncol]),
                              reads=[ps], writes=[st])
                    else:
                        kb.op("act", lambda ps=ps, st=st: nc.scalar.copy(out=st[:, :ncol], in_=ps[:, :ncol]),
                              reads=[ps], writes=[st])
                    db = getattr(cx, dest)
                    kb.dma("sp", db[:, ti, doff:doff + ncol], st[:, :ncol], reads=[st], writes=[], acc_writes=[db])
                else:
                    e = e1[nh % 2]
                    r = r1[nh % 2]
                    g = lg[nh % 2]
                    k = kk[nh % 2]
                    nh += 1
                    cs = slice(doff, doff + 512)
                    kb.op("act", lambda ps=ps, e=e: nc.scalar.activation(out=e[:], in_=ps[:], func=AF.Exp, scale=-1.0),
                          reads=[ps], writes=[e])
                    kb.op("pool", lambda e=e: nc.gpsimd.tensor_scalar(
                        out=e[:], in0=e[:], scalar1=1.0, scalar2=None, op0=ALU.add),
                        reads=[e], writes=[e])
                    kb.op("dve", lambda e=e, r=r: nc.vector.reciprocal(out=r[:], in_=e[:]),
                          reads=[e], writes=[r])
                    kb.op("dve", lambda r=r: nc.vector.tensor_tensor(out=r[:], in0=r[:], in1=omlt[:, cs], op=ALU.mult),
                          reads=[r, omlt], writes=[r])
                    kb.op("pool", lambda r=r: nc.gpsimd.tensor_tensor(out=r[:], in0=r[:], in1=lbt[:, cs], op=ALU.add),
                          reads=[r, lbt], writes=[r])
                    kb.op("act", lambda r=r, g=g: nc.scalar.activation(out=g[:], in_=r[:], func=AF.Ln),
                          reads=[r], writes=[g])
                    kb.op("pool", lambda r=r, k=k: nc.gpsimd.tensor_scalar(
                        out=k[:], in0=r[:], scalar1=-1.0, scalar2=1.0, op0=ALU.mult, op1=ALU.add),
                        reads=[r], writes=[k])
                    kb.dma("sp", cx.LGD_t[:, ti, cs], g[:], reads=[g], writes=[], acc_writes=[cx.LGD_t])
                    kb.dma("sp", cx.KD_t[:, ti, cs], k[:], reads=[k], writes=[], acc_writes=[cx.KD_t])


def stage_lb(cx, kb, nc, lbt, omlt):
    with kb.scope():
        gam = kb.sbuf("gam", [128, 2, 2, 512], F32)
        kb.dma("sp", gam[:].rearrange("p a b c -> p (a b c)"),
               cx.hg_gamma[:].rearrange("a b c -> (a b c)").partition_broadcast(128),
               reads=[cx.hg_gamma], writes=[gam])
        d = kb.sbuf("gamd", [128, 2, 512], F32)
        kb.op("dve", lambda: nc.vector.tensor_tensor(out=d[:], in0=gam[:, :, 0, :], in1=gam[:, :, 1, :], op=ALU.subtract),
              reads=[gam], writes=[d])
        kb.op("act", lambda: nc.scalar.activation(out=d[:], in_=d[:], func=AF.Exp), reads=[d], writes=[d])
        l1 = lbt[1]
        kb.op("dve", lambda: nc.vector.tensor_scalar(
            out=d[:], in0=d[:], scalar1=1.0, scalar2=None, op0=ALU.add), reads=[d], writes=[d])
        kb.op("dve", lambda: nc.vector.reciprocal(out=l1[:], in_=d[:].rearrange("p a c -> p (a c)")),
              reads=[d], writes=[l1])
        l0 = lbt[0]
        kb.op("dve", lambda: nc.vector.memset(l0[:], 0.0), writes=[l0])
        for l in range(2):
            o = omlt[l]
            lb = lbt[l]
            kb.op("dve", lambda o=o, lb=lb: nc.vector.tensor_scalar(
                out=o[:], in0=lb[:], scalar1=-1.0, scalar2=1.0, op0=ALU.mult, op1=ALU.add),
                reads=[lb], writes=[o])


def stage_gla_gate(cx, kb, nc, l):
    with kb.scope():
        wlr = kb.sbuf("wlr", [33, 512], F32)
        lr1 = kb.sbuf("lr1", [33, T], F32)
        kb.op("dve", lambda: nc.vector.memset(wlr[:], 0.0), writes=[wlr])
        kb.dma("sp", wlr[0:16, 0:256], cx.gla_lr_w[l, 0], reads=[cx.gla_lr_w], writes=[wlr])
        kb.dma("sp", wlr[16:32, 256:512], cx.gla_lr_w[l, 1], reads=[cx.gla_lr_w], writes=[wlr])
        kb.dma("sp", wlr[32:33, :], cx.gla_lr_b[l].rearrange("a c -> (a c)").unsqueeze(0),
               reads=[cx.gla_lr_b], writes=[wlr])
        kb.dma("sp", lr1[:], cx.LR1[:], reads=[cx.LR1], writes=[lr1])
        pss = [kb.psum("psg%d" % i, [128, 512]) for i in range(2)]
        ee = [kb.sbuf("ge%d" % i, [128, 512], F32) for i in range(2)]
        gg = [kb.sbuf("gg%d" % i, [128, 512], F32) for i in range(2)]
        for ti in range(NT):
            ts = slice(ti * 128, (ti + 1) * 128)
            ps = pss[ti % 2]
            e = ee[ti % 2]
            g = gg[ti % 2]
            kb.mm([lambda ps=ps: nc.tensor.matmul(ps[:], lhsT=lr1[:, ts], rhs=wlr[:], start=True, stop=True)],
                  reads=[lr1, wlr], writes=[ps])
            kb.op("act", lambda ps=ps, e=e: nc.scalar.activation(out=e[:], in_=ps[:], func=AF.Exp, scale=-1.0),
                  reads=[ps], writes=[e])
            kb.op("act", lambda e=e, g=g: nc.scalar.activation(out=g[:], in_=e[:], func=AF.Ln, bias=1.0),
                  reads=[e], writes=[g])
            kb.dma("sp", cx.LGA_t[:, ti, :], g[:], reads=[g], writes=[], acc_writes=[cx.LGA_t])

SCAN_CFG = {
    "gla": dict(H=4, dk=64, qsrc="QA", qscale=0.125, ksrc="KVA_t", kz=0, kcol0=0, vsrc="KVA_t", vcol0=256,
                gsrc="LGA_t", gz=256, gate="GA", dest="BR_A", mask="mg"),
    "hgrn": dict(H=4, dk=128, qsrc="QD", qscale=1.0, ksrc="KD_t", kz=512, kcol0=0, vsrc="VD_t", vcol0=0,
                 gsrc="LGD_t", gz=512, gate="GD", dest="BR_D", mask="mh"),
}


def view(b):
    return Buf(b.t, b.name + "_v")


def stage_scan(cx, kb, nc, which, heads=None):
    cfg = SCAN_CFG[which]
    H, dk = cfg["H"], cfg["dk"]
    dv = 128
    qsrc, ksrc, vsrc, gsrc = (getattr(cx, cfg[k]) for k in ("qsrc", "ksrc", "vsrc", "gsrc"))
    gate, dest = getattr(cx, cfg["gate"]), getattr(cx, cfg["dest"])
    mq_d, mk_d = getattr(cx, cfg["mask"] + "_mq"), getattr(cx, cfg["mask"] + "_mk")
    with kb.scope():
        mq = kb.sbuf("mq", [128, 2, 130], F32)
        mk = kb.sbuf("mk", [128, 2, 128], F32)
        ma = kb.sbuf("ma", [128, 2, 128], F32)
        ident = kb.sbuf("ident", [128, 128], BF16)
        onesdv = kb.sbuf("onesdv", [128, 128], F32)
        kb.dma("sp", mq[:], mq_d[:], reads=[mq_d], writes=[mq])
        kb.dma("sp", mk[:], mk_d[:], reads=[mk_d], writes=[mk])
        kb.dma("sp", ma[:], cx.m_ma[:], reads=[cx.m_ma], writes=[ma])
        mau = kb.sbuf("mau", [128, 2, 128], U32)
        kb.dma("sp", mau[:], cx.m_mau[:], reads=[cx.m_mau], writes=[mau])
        kb.dma("pool", ident[:], cx.m_ident[:], reads=[cx.m_ident], writes=[ident])
        kb.op("dve", lambda: nc.vector.memset(onesdv[:], 1.0 / dv), writes=[onesdv])
        vt = kb.sbuf("vt", [128, NT, dv], BF16)
        qT = kb.sbuf("qT", [128, T], BF16)
        gsil = kb.sbuf("gsil", [128, T], BF16)
        obr = kb.sbuf("obr", [128, T], BF16)
        gts = [kb.sbuf("gt%d" % i, [128, NT, dk], F32) for i in range(2)]
        kts = [kb.sbuf("kt%d" % i, [128, NT, dk], BF16) for i in range(2)]
        qt = [kb.sbuf("qt%d" % z, [128, T], BF16) for z in range(2)]
        atm = [kb.sbuf("atm%d" % z, [128, NT, 128], BF16) for z in range(2)]
        Sp = [kb.sbuf("Sp%d" % z, [128, 2 * NT, dv], BF16) for z in range(2)]
        for z in range(2):
            kb.op("pool", lambda z=z: nc.gpsimd.memset(atm[z][:], 0.0), writes=[atm[z]])
        Sst = [kb.sbuf("Sst%d" % i, [128, dv], F32) for i in range(2)]
        R = 3
        eq = [kb.sbuf("eq%d" % i, [128, 128], F32) for i in range(R)]
        ek = [kb.sbuf("ek%d" % i, [128, 128], F32) for i in range(R)]
        ekh = [kb.sbuf("ekh%d" % i, [128, 128], F32) for i in range(R)]
        ktl = [kb.sbuf("ktl%d" % i, [128, 128], BF16) for i in range(R)]
        khat = [kb.sbuf("khat%d" % i, [128, 256], BF16) for i in range(R)]
        ktr = [kb.sbuf("ktr%d" % i, [128, 128], F32) for i in range(R)]
        atr = [kb.sbuf("atr%d" % i, [128, 128], F32) for i in range(R)]
        usb = [kb.sbuf("usb%d" % i, [128, 256], F32) for i in range(R)]
        osb = [kb.sbuf("osb%d" % i, [128, 512], F32) for i in range(2)]
        cm = kb.sbuf("cm", [128, 2], F32)
        kb.op("dve", lambda: nc.vector.memset(cm[:], 0.0), writes=[cm])
        kb.op("dve", lambda: nc.vector.memset(cm[0:64, 0:1], 1.0), writes=[cm])
        kb.op("dve", lambda: nc.vector.memset(cm[64:128, 1:2], 1.0), writes=[cm])
        er2 = [kb.sbuf("er%d" % i, [128, 2], F32) for i in range(R)]
        ed2 = [kb.sbuf("ed%d" % i, [128, 2], F32) for i in range(R)]
        psA = [kb.psum("psA%d" % i, [128, 512]) for i in range(2)]
        psB = [kb.psum("psB%d" % i, [128, 512]) for i in range(2)]
        psA_ec = [view(p) for p in psA]
        import os
        if True:
            psK = [kb.psum("psK%d" % i, [128, 512]) for i in range(2)]
        else:
            psK = psA
        psA_kt = [view(p) for p in psK]
        psA_e2 = [view(p) for p in psK]
        psB_u = [view(p) for p in psB]
        psB_at = [view(p) for p in psB]
        psO = [kb.psum("psO%d" % i, [128, 512]) for i in range(1)] * 2
        psM = [kb.psum("psM%d" % i, [128, 512]) for i in range(1)] * 2
        sq = [kb.sbuf("sq%d" % i, [128, 512], F32) for i in range(2)]
        sd = [kb.sbuf("sd%d" % i, [128, 512], F32) for i in range(2)]
        on = [kb.sbuf("on%d" % i, [128, 512], F32) for i in range(2)]
        eps_t = kb.sbuf("eps_t", [128, 1], F32)
        kb.op("dve", lambda: nc.vector.memset(eps_t[:], 1e-6), writes=[eps_t])

        it = 0
        ghz = 0
        import os
        for h in (range(H) if heads is None else heads):
            vc0 = cfg["vcol0"] + h * dv
            kb.dma("sp", vt[:], vsrc[:, :, vc0:vc0 + dv], reads=[vsrc], writes=[vt])
            kb.dma("sp", qT[:dk, :], qsrc[h * dk:(h + 1) * dk, :], reads=[qsrc], writes=[qT])
            kb.dma("sp", gsil[:], gate[h * dv:(h + 1) * dv, :], reads=[gate], writes=[gsil])
            for z in range(2):
                gt = gts[ghz % 2]
                kt = kts[ghz % 2]
                ghz += 1
                gc0 = z * cfg["gz"] + h * dk
                kc0 = cfg["kcol0"] + z * cfg["kz"] + h * dk
                kb.dma("sp", gt[:], gsrc[:, :, gc0:gc0 + dk], reads=[gsrc], writes=[gt])
                if os.environ.get("KTMEMSET"):
                    kb.op("dve", lambda kt=kt: nc.vector.memset(kt[:], 0.0), writes=[kt])
                kb.dma("sp", kt[:], ksrc[:, :, kc0:kc0 + dk], reads=[ksrc], writes=[kt])
                order = [32, 33] + list(range(32)) if z == 0 else [33, 32] + list(range(31, -1, -1))
                import os
                DBG = os.environ.get("SCAN_DBG", "")
                if DBG == "loads":
                    continue
                if DBG.startswith("nt"):
                    order = order[:int(DBG[2:])]
                S = Sst[0]
                kb.op("dve", lambda S=S: nc.vector.memset(S[:], 0.0), writes=[S])
                si = 0
                def p1(ti, a, r):
                    ts = slice(ti * 128, (ti + 1) * 128)
                    pa, pb = psA[a], psB[a]
                    pk = psK[a]
                    kb.mm([lambda pa=pa, gt=gt: nc.tensor.matmul(pa[:dk, 0:130], lhsT=gt[:, ti, :], rhs=mq[:, z, :],
                                                                 start=True, stop=True)],
                          reads=[gt, mq], writes=[psA_ec[a]])
                    kb.mm([lambda pk=pk, kt=kt: nc.tensor.matmul(pk[:dk, 256:384], lhsT=(ident[:, :dk] if os.environ.get("A2ID") else kt[:, ti, :]), rhs=ident[:],
                                                                 start=True, stop=True)],
                          reads=[kt, ident], writes=[psA_kt[a]])
                    kb.mm([lambda pk=pk, gt=gt: nc.tensor.matmul(pk[:, 384:384 + dk], lhsT=mk[:, z, :], rhs=gt[:, ti, :],
                                                                 start=True, stop=True)],
                          reads=[gt, mk], writes=[psA_e2[a]])
                    kb.op("act", lambda pa=pa, o=eq[r]: nc.scalar.activation(out=o[:dk, :], in_=pa[:dk, 0:128], func=AF.Exp),
                          reads=[psA_ec[a]], writes=[eq[r]])
                    kb.op("act", lambda pa=pa, o=ek[r]: nc.scalar.activation(out=o[:dk, :], in_=pa[:dk, 0:128], func=AF.Exp, scale=-1.0),
                          reads=[psA_ec[a]], writes=[ek[r]])
                    kb.op("act", lambda pa=pa, o=er2[r]: nc.scalar.activation(out=o[:dk, :], in_=pa[:dk, 128:130], func=AF.Exp, scale=0.5),
                          reads=[psA_ec[a]], writes=[er2[r]])
                    kb.op("act", lambda pa=pa, o=ed2[r]: nc.scalar.activation(out=o[:dk, :], in_=pa[:dk, 128:130], func=AF.Exp),
                          reads=[psA_ec[a]], writes=[ed2[r]])
                    kb.op("act", lambda pk=pk, o=ekh[r]: nc.scalar.activation(out=o[:, :dk], in_=pk[:, 384:384 + dk], func=AF.Exp),
                          reads=[psA_e2[a]], writes=[ekh[r]])
                    qz = qt[z]
                    kb.op("dve", lambda qz=qz, e=eq[r]: nc.vector.scalar_tensor_tensor(
                        out=qz[:dk, ts], in0=qT[:dk, ts], scalar=cfg["qscale"], in1=e[:dk, :], op0=ALU.mult, op1=ALU.mult),
                        reads=[qT, eq[r]], writes=[], acc_writes=[qz])
                    kb.op("act", lambda pk=pk, o=ktr[r]: nc.scalar.copy(out=o[:dk, :], in_=pk[:dk, 256:384]),
                          reads=[psA_kt[a]], writes=[ktr[r]])
                    kb.op("dve", lambda e=ek[r], o=ktl[r], kr=ktr[r]: nc.vector.tensor_tensor(
                        out=o[:dk, :], in0=kr[:dk, :], in1=e[:dk, :], op=ALU.mult),
                        reads=[ktr[r], ek[r]], writes=[ktl[r]])
                    for c in range(2):
                        kb.op("dve", lambda kt=kt, e=ekh[r], o=khat[r], c=c: nc.vector.scalar_tensor_tensor(
                            out=o[:, c * 128:c * 128 + dk], in0=kt[:, ti, :], scalar=cm[:, c:c + 1], in1=e[:, :dk],
                            op0=ALU.mult, op1=ALU.mult),
                            reads=[kt, ekh[r], cm], writes=[], acc_writes=[khat[r]])
                    return None

                def p2(ti, a, r, S, si):
                    ts = slice(ti * 128, (ti + 1) * 128)
                    pa, pb = psA[a], psB[a]
                    qz = qt[z]
                    kb.mm([(lambda c=c, pb=pb, kh=khat[r]: nc.tensor.matmul(
                        pb[:dk, c * 128:(c + 1) * 128], lhsT=kh[:, c * 128:c * 128 + dk],
                        rhs=vt[:, ti, :], start=True, stop=True)) for c in range(2)],
                        reads=[khat[r], vt], writes=[psB_u[a]])
                    kb.mm([lambda pb=pb, k_=ktl[r], qz=qz: nc.tensor.matmul(
                        pb[:, 256:384], lhsT=k_[:dk, :], rhs=qz[:dk, ts], start=True, stop=True)],
                        reads=[ktl[r], qz], writes=[psB_at[a]])
                    az = atm[z]
                    kb.op("act", lambda pb=pb, o=atr[r]: nc.scalar.copy(out=o[:], in_=pb[:, 256:384]),
                          reads=[psB_at[a]], writes=[atr[r]])
                    kb.op("dve", lambda az=az, o=atr[r]: nc.vector.copy_predicated(
                        out=az[:, ti, :], mask=mau[:, z, :], data=o[:]),
                        reads=[atr[r], mau], writes=[], acc_writes=[az])
                    kb.op("act", lambda pb=pb, o=usb[r]: nc.scalar.copy(out=o[:dk, :], in_=pb[:dk, 0:256]),
                          reads=[psB_u[a]], writes=[usb[r]])
                    spz = Sp[z]
                    for c in ((0, 1) if z == 0 else (1, 0)):
                        ci = 2 * ti + c
                        kb.op("act", lambda S=S, spz=spz, ci=ci, e=er2[r], c=c: nc.scalar.activation(
                            out=spz[:dk, ci, :], in_=S[:dk, :], func=AF.Copy, scale=e[:dk, c:c + 1]),
                            reads=[S, er2[r]], writes=[], acc_writes=[spz])
                        S2 = Sst[(si + 1) % 2]
                        si += 1
                        kb.op("dve", lambda S=S, S2=S2, e=ed2[r], u=usb[r], c=c: nc.vector.scalar_tensor_tensor(
                            out=S2[:dk, :], in0=S[:dk, :], scalar=e[:dk, c:c + 1], in1=u[:dk, c * 128:(c + 1) * 128],
                            op0=ALU.mult, op1=ALU.add),
                            reads=[S, ed2[r], usb[r]], writes=[S2])
                        S = S2
                    return S, si

                sched = []
                for ti in order:
                    sched.append((ti, it % 2, it % R))
                    it += 1
                for idx in range(len(sched) + 1):
                    if idx < len(sched):
                        p1(*sched[idx])
                    if idx >= 1:
                        S, si = p2(*sched[idx - 1], S, si)
            groups = [list(range(g * 4, g * 4 + 4)) for g in range(8)] + [[32, 33]]
            if DBG:
                groups = []
            for gi, tiles in enumerate(groups):
                po = psO[gi % 2]
                pm = psM[gi % 2]
                nb = len(tiles) * 128
                t0 = tiles[0] * 128
                fns = []
                for k_, ti in enumerate(tiles):
                    cs = k_ * 128
                    for z in range(2):
                        fns.append(lambda po=po, ti=ti, cs=cs, z=z: nc.tensor.matmul(
                            po[:, cs:cs + 128], lhsT=vt[:, ti, :], rhs=atm[z][:, ti, :], start=(z == 0), stop=False))
                        for c in range(2):
                            fns.append(lambda po=po, ti=ti, cs=cs, z=z, c=c: nc.tensor.matmul(
                                po[:, cs + c * 64:cs + (c + 1) * 64], lhsT=Sp[z][:dk, 2 * ti + c, :],
                                rhs=qt[z][:dk, ti * 128 + c * 64:ti * 128 + (c + 1) * 64],
                                start=False, stop=(z == 1)))
                kb.mm(fns, reads=[vt, atm[0], atm[1], Sp[0], Sp[1], qt[0], qt[1]], writes=[po])
                s_, d_, o_ = sq[gi % 2], sd[gi % 2], on[gi % 2]
                kb.op("act", lambda po=po, s_=s_: nc.scalar.activation(out=s_[:, :nb], in_=po[:, :nb], func=AF.Square),
                      reads=[po], writes=[s_])
                kb.mm([lambda pm=pm, s_=s_: nc.tensor.matmul(pm[:, :nb], lhsT=onesdv[:], rhs=s_[:, :nb], start=True, stop=True)],
                      reads=[onesdv, s_], writes=[pm])
                kb.op("act", lambda pm=pm, d_=d_: nc.scalar.activation(out=d_[:, :nb], in_=pm[:, :nb], func=AF.Sqrt, bias=eps_t[:]),
                      reads=[pm, eps_t], writes=[d_])
                kb.op("dve", lambda d_=d_: nc.vector.reciprocal(out=d_[:, :nb], in_=d_[:, :nb]), reads=[d_], writes=[d_])
                ob_ = osb[gi % 2]
                kb.op("act", lambda po=po, ob_=ob_: nc.scalar.copy(out=ob_[:, :nb], in_=po[:, :nb]), reads=[po], writes=[ob_])
                kb.op("dve", lambda ob_=ob_, d_=d_, o_=o_: nc.vector.tensor_tensor(
                    out=o_[:, :nb], in0=ob_[:, :nb], in1=d_[:, :nb], op=ALU.mult), reads=[ob_, d_], writes=[o_])
                kb.op("pool", lambda o_=o_: nc.gpsimd.tensor_tensor(
                    out=obr[:, t0:t0 + nb], in0=o_[:, :nb], in1=gsil[:, t0:t0 + nb], op=ALU.mult),
                    reads=[o_, gsil], writes=[], acc_writes=[obr])
            kb.dma("sp", dest[h * dv:(h + 1) * dv, :], obr[:], reads=[obr], writes=[], acc_writes=[dest])


def scan_masks():
    j = np.arange(128)[:, None]
    i = np.arange(128)[None, :]
    same = (j // 64 == i // 64).astype(np.float32)
    out = {}
    for name, gs in (("mg", -1.0 / 16.0), ("mh", 1.0)):
        mq = np.zeros((128, 2, 130), np.float32)
        mk = np.zeros((128, 2, 128), np.float32)
        mq[:, 0, :128] = gs * same * ((j <= i).astype(np.float32) - 0.5)
        mq[:, 1, :128] = gs * same * ((j >= i).astype(np.float32) - 0.5)
        for z in range(2):
            for c in range(2):
                mq[:, z, 128 + c] = gs * (np.arange(128) // 64 == c)
        mk[:, 0, :] = gs * same * (j > i)
        mk[:, 1, :] = gs * same * (j < i)
        out[name + "_mq"] = mq
        out[name + "_mk"] = mk
    ma = np.zeros((128, 2, 128), np.float32)
    ma[:, 0, :] = same * (j <= i)
    ma[:, 1, :] = same * (j >= i)
    out["m_ma"] = ma
    out["m_mau"] = ma.astype(np.uint32)
    out["m_ident"] = np.eye(128, dtype=np.float32)
    return out


def stage_wa(cx, kb, nc, l, groups=None):
    with kb.scope():
        wm = kb.sbuf("wm", [128, 2, 128], BF16)
        kb.dma("pool", wm[:], cx.m_wa[:], reads=[cx.m_wa], writes=[wm])
        ones64 = kb.sbuf("ones64", [128, 64], BF16)
        kb.op("dve", lambda: nc.vector.memset(ones64[:], 1.0), writes=[ones64])
        sk = kb.sbuf("sk", [64, 8], F32)
        kb.dma("sp", sk[:], cx.wa_sink[l].partition_broadcast(64), reads=[cx.wa_sink], writes=[sk])
        kb.op("act", lambda: nc.scalar.activation(out=sk[:], in_=sk[:], func=AF.Exp), reads=[sk], writes=[sk])
        q4 = kb.sbuf("q4", [64, 4, T], BF16)
        kT = kb.sbuf("kTc", [64, T], BF16)
        vt = kb.sbuf("vtc", [128, NT, 64], BF16)
        ost = kb.sbuf("ost", [64, 4, T], BF16)
        pS = [kb.psum("pS%d" % i, [128, 512]) for i in range(4)]
        pO = [kb.psum("pO%d" % i, [128, 512]) for i in range(2)]
        pD = [kb.psum("pD%d" % i, [128, 512]) for i in range(2)]
        R = 10
        pt = [kb.sbuf("pt%d" % i, [128, 4, 128], BF16) for i in range(R)]
        ptm = [kb.sbuf("ptm%d" % i, [128, 4, 128], BF16) for i in range(R)]
        osb = [kb.sbuf("osbw%d" % i, [64, 4, 128], F32) for i in range(2)]
        dsb = [kb.sbuf("dsbw%d" % i, [64, 4, 128], F32) for i in range(2)]
        it = 0
        for g in (range(2) if groups is None else groups):
            kb.dma("sp", q4[:], cx.QC[g * 256:(g + 1) * 256, :].rearrange("(hh d) t -> d hh t", d=64),
                   reads=[cx.QC], writes=[q4])
            kb.dma("sp", kT[:], cx.KC[g * 64:(g + 1) * 64, :], reads=[cx.KC], writes=[kT])
            kb.dma("sp", vt[:], cx.VC_t[:, :, g * 64:(g + 1) * 64], reads=[cx.VC_t], writes=[vt])
            def p1(qb):
                nonlocal it
                if qb < 32:
                    keys = []
                    if qb > 0:
                        keys.append((qb - 1, 0))
                    keys.append((qb, None))
                    if qb < 31:
                        keys.append((qb + 1, 1))
                    keys += [(32, None), (33, None)]
                else:
                    keys = [(32, None), (33, None)]
                qs = slice(qb * 128, (qb + 1) * 128)
                st_ = []
                for ki, (kt_i, mi) in enumerate(keys):
                    ps = pS[it % 4]
                    p_ = pt[it % R]
                    pm_ = ptm[it % R]
                    it += 1
                    kb.mm([lambda ps=ps, kt_i=kt_i: nc.tensor.matmul(
                        ps[:, :], lhsT=kT[:, kt_i * 128:(kt_i + 1) * 128], rhs=q4[:, :, qs], start=True, stop=True)],
                        reads=[kT, q4], writes=[ps])
                    kb.op("act", lambda ps=ps, p_=p_: nc.scalar.activation(
                        out=p_[:].rearrange("p h q -> p (h q)"), in_=ps[:, :], func=AF.Exp, scale=0.125),
                        reads=[ps], writes=[p_])
                    src = p_
                    if mi is not None:
                        eng = "dve" if it % 2 == 0 else "pool"
                        h_ = nc.vector if eng == "dve" else nc.gpsimd
                        kb.op(eng, lambda h_=h_, p_=p_, pm_=pm_, mi=mi: h_.tensor_tensor(
                            out=pm_[:], in0=p_[:], in1=wm[:, mi, :].unsqueeze(1).broadcast_to([128, 4, 128]), op=ALU.mult),
                            reads=[p_, wm], writes=[pm_])
                        src = pm_
                    st_.append((kt_i, src))
                return (qb, st_)

            def p2(state):
                qb, st_ = state
                qs = slice(qb * 128, (qb + 1) * 128)
                a = qb % 2
                po, pd = pO[a], pD[a]
                nk = len(st_)
                for ki, (kt_i, src) in enumerate(st_):
                    kb.mm([lambda po=po, src=src, kt_i=kt_i, ki=ki: nc.tensor.matmul(
                        po[:64, :], lhsT=vt[:, kt_i, :], rhs=src[:].rearrange("p h q -> p (h q)"),
                        start=(ki == 0), stop=(ki == nk - 1)),
                        lambda pd=pd, src=src, ki=ki: nc.tensor.matmul(
                        pd[:64, :], lhsT=ones64[:], rhs=src[:].rearrange("p h q -> p (h q)"),
                        start=(ki == 0), stop=(ki == nk - 1))],
                        reads=[vt, src, ones64], writes=[po, pd])
                o_, d_ = osb[a], dsb[a]
                kb.op("act", lambda po=po, o_=o_: nc.scalar.copy(out=o_[:].rearrange("p h q -> p (h q)"), in_=po[:64, :]),
                      reads=[po], writes=[o_])
                kb.op("act", lambda pd=pd, d_=d_: nc.scalar.copy(out=d_[:].rearrange("p h q -> p (h q)"), in_=pd[:64, :]),
                      reads=[pd], writes=[d_])
                kb.op("dve", lambda d_=d_: nc.vector.tensor_tensor(
                    out=d_[:], in0=d_[:], in1=sk[:, g * 4:(g + 1) * 4].unsqueeze(2).broadcast_to([64, 4, 128]), op=ALU.add),
                    reads=[d_, sk], writes=[d_])
                kb.op("dve", lambda d_=d_: nc.vector.reciprocal(out=d_[:], in_=d_[:]), reads=[d_], writes=[d_])
                kb.op("pool", lambda o_=o_, d_=d_: nc.gpsimd.tensor_tensor(
                    out=ost[:, :, qs], in0=o_[:], in1=d_[:], op=ALU.mult),
                    reads=[o_, d_], writes=[], acc_writes=[ost])

            prev = None
            for qb in range(NT):
                cur = p1(qb)
                if prev is not None:
                    p2(prev)
                prev = cur
            p2(prev)
            kb.dma("sp", cx.BR_C[g * 256:(g + 1) * 256, :].rearrange("(hh d) t -> d hh t", d=64), ost[:],
                   reads=[ost], writes=[], acc_writes=[cx.BR_C])


def wa_masks():
    kk = np.arange(128)[:, None]
    qq = np.arange(128)[None, :]
    m = np.zeros((128, 2, 128), np.float32)
    m[:, 0, :] = (kk >= qq)
    m[:, 1, :] = (kk <= qq)
    return {"m_wa": m}


def na_patterns():
    W = 64
    tok = np.arange(128)
    lr, lc = tok // W, tok % W
    pats = []
    keys = []
    for m in range(32):
        qr = 2 * m + lr[None, :]
        qc = lc[None, :]
        rs = np.clip(qr - 4, 0, 56)
        cs = np.clip(qc - 8, 0, 48)
        lst = []
        for kt_i in range(32):
            kr = 2 * kt_i + lr[:, None]
            kc = lc[:, None]
            valid = (kr >= rs) & (kr < rs + 8) & (kc >= cs) & (kc < cs + 16)
            if not valid.any():
                continue
            ri = np.clip(kr - qr + 7, 0, 14)
            ci = np.clip(kc - qc + 15, 0, 30)
            if 2 <= m <= 29:
                pid = kt_i - m + 2
                if m == 2:
                    pats.append((valid, ri, ci))
            else:
                pid = len(pats)
                pats.append((valid, ri, ci))
            lst.append((kt_i, pid))
        keys.append(lst)
    return keys, pats


def na_consts():
    keys, pats = na_patterns()
    n0 = len(keys[0]) + len(keys[1])
    order = list(range(n0, n0 + 5)) + list(range(0, n0)) + list(range(n0 + 5, len(pats)))
    remap = {old: new for new, old in enumerate(order)}
    pats2 = [pats[o] for o in order]
    keys2 = []
    for m, lst in enumerate(keys):
        if 2 <= m <= 29:
            keys2.append(lst)
        else:
            keys2.append([(k, remap[p]) for (k, p) in lst])
    valid = np.stack([p[0] for p in pats2], 1).astype(np.float32)
    ri = np.stack([p[1] for p in pats2], 1)
    ci = np.stack([p[2] for p in pats2], 1)
    return keys2, valid, ri, ci


def stage_na(cx, kb, nc, l, heads=None):
    keys_tab, valid_np, _, _ = na_consts()
    npat = valid_np.shape[1]
    with kb.scope():
        vmask = kb.sbuf("vmask", [128, npat, 128], F32)
        kb.dma("sp", vmask[:], cx.m_navalid[:], reads=[cx.m_navalid], writes=[vmask])
        ones64 = kb.sbuf("ones64n", [128, 64], BF16)
        kb.op("dve", lambda: nc.vector.memset(ones64[:], 1.0), writes=[ones64])
        qT = kb.sbuf("qTn", [64, T], BF16)
        kT = kb.sbuf("kTn", [64, T], BF16)
        vt = kb.sbuf("vtn", [128, NT, 64], BF16)
        ost = kb.sbuf("ostn", [64, T], BF16)
        bias = kb.sbuf("biasn", [128, npat, 128], F32)
        EB = kb.sbuf("EBn", [128, npat, 128], BF16)
        pS = [kb.psum("pSn%d" % i, [128, 512]) for i in range(4)]
        pO = [kb.psum("pOn%d" % i, [128, 512]) for i in range(2)]
        pD = [kb.psum("pDn%d" % i, [128, 512]) for i in range(2)]
        R = 6
        pt = [kb.sbuf("ptn%d" % i, [128, 4, 128], BF16) for i in range(R)]
        osb = [kb.sbuf("osbn%d" % i, [64, 256], F32) for i in range(2)]
        it = 0
        for h in (range(8) if heads is None else heads):
            kb.dma("sp", bias[:], cx.na_bias[l, h], reads=[cx.na_bias], writes=[bias])
            kb.op("act", lambda: nc.scalar.activation(out=bias[:], in_=bias[:], func=AF.Exp), reads=[bias], writes=[bias])
            kb.op("pool", lambda: nc.gpsimd.tensor_tensor(out=EB[:], in0=bias[:], in1=vmask[:], op=ALU.mult),
                  reads=[bias, vmask], writes=[EB])
            kb.dma("sp", qT[:], cx.QB[h * 64:(h + 1) * 64, :], reads=[cx.QB], writes=[qT])
            kb.dma("sp", kT[:], cx.KB_[h * 64:(h + 1) * 64, :], reads=[cx.KB_], writes=[kT])
            kb.dma("sp", vt[:], cx.VB_t[:, :, h * 64:(h + 1) * 64], reads=[cx.VB_t], writes=[vt])
            def p1(qb):
                nonlocal it
                if qb < 32:
                    keys = list(keys_tab[qb]) + [(32, None), (33, None)]
                else:
                    keys = [(32, None), (33, None)]
                qs = slice(qb * 128, (qb + 1) * 128)
                grp = [keys[i:i + 4] for i in range(0, len(keys), 4)]
                st_ = []
                for gk in grp:
                    ps = pS[it % 4]
                    p_ = pt[it % R]
                    it += 1
                    ng = len(gk)
                    kb.mm([(lambda ps=ps, j=j, kt_i=kt_i: nc.tensor.matmul(
                        ps[:, j * 128:(j + 1) * 128], lhsT=kT[:, kt_i * 128:(kt_i + 1) * 128], rhs=qT[:, qs],
                        start=True, stop=True)) for j, (kt_i, _) in enumerate(gk)],
                        reads=[kT, qT], writes=[ps])
                    kb.op("act", lambda ps=ps, p_=p_, ng=ng: nc.scalar.activation(
                        out=p_[:, :ng, :].rearrange("p h q -> p (h q)"), in_=ps[:, :ng * 128], func=AF.Exp, scale=0.125),
                        reads=[ps], writes=[p_])
                    nloc = sum(1 for (_, pid) in gk if pid is not None)
                    if nloc:
                        pid0 = gk[0][1]
                        eng = "dve" if it % 2 == 0 else "pool"
                        h_ = nc.vector if eng == "dve" else nc.gpsimd
                        kb.op(eng, lambda h_=h_, p_=p_, pid0=pid0, nloc=nloc: h_.tensor_tensor(
                            out=p_[:, :nloc, :], in0=p_[:, :nloc, :], in1=EB[:, pid0:pid0 + nloc, :], op=ALU.mult),
                            reads=[p_, EB], writes=[p_])
                    st_.append((gk, p_))
                return (qb, st_, len(keys))

            def p2(state):
                qb, st_, nk = state
                qs = slice(qb * 128, (qb + 1) * 128)
                a = qb % 2
                po = pO[a]
                pd = pD[a]
                done = 0
                for gk, p_ in st_:
                    fns = []
                    for j, (kt_i, _) in enumerate(gk):
                        first = (done == 0)
                        last = (done == nk - 1)
                        fns.append(lambda po=po, p_=p_, j=j, kt_i=kt_i, first=first, last=last: nc.tensor.matmul(
                            po[:64, 0:128], lhsT=vt[:, kt_i, :], rhs=p_[:, j, :], start=first, stop=last))
                        fns.append(lambda pd=pd, p_=p_, j=j, first=first, last=last: nc.tensor.matmul(
                            pd[:64, 0:128], lhsT=ones64[:], rhs=p_[:, j, :], start=first, stop=last))
                        done += 1
                    kb.mm(fns, reads=[vt, p_, ones64], writes=[po, pd])
                o_ = osb[a]
                kb.op("act", lambda po=po, o_=o_: nc.scalar.copy(out=o_[:, 0:128], in_=po[:64, 0:128]), reads=[po], writes=[o_])
                kb.op("act", lambda pd=pd, o_=o_: nc.scalar.copy(out=o_[:, 128:256], in_=pd[:64, 0:128]), reads=[pd, o_], writes=[o_])
                kb.op("dve", lambda o_=o_: nc.vector.reciprocal(out=o_[:, 128:256], in_=o_[:, 128:256]), reads=[o_], writes=[o_])
                kb.op("pool", lambda o_=o_: nc.gpsimd.tensor_tensor(
                    out=ost[:, qs], in0=o_[:, 0:128], in1=o_[:, 128:256], op=ALU.mult),
                    reads=[o_], writes=[], acc_writes=[ost])

            prev = None
            for qb in range(NT):
                cur = p1(qb)
                if prev is not None:
                    p2(prev)
                prev = cur
            p2(prev)
            kb.dma("sp", cx.BR_B[h * 64:(h + 1) * 64, :], ost[:], reads=[ost], writes=[], acc_writes=[cx.BR_B])

LN_EPS = 1e-5
MB = 512
MBLKS = [(i * MB, min(MB, T - i * MB)) for i in range((T + MB - 1) // MB)]


def ln_setup(cx, kb, nc):
    ones = kb.sbuf("ln_ones", [128, 128], F32)
    kb.op("dve", lambda: nc.vector.memset(ones[:], 1.0 / D), writes=[ones])
    eps = kb.sbuf("ln_eps", [128, 1], F32)
    kb.op("dve", lambda: nc.vector.memset(eps[:], LN_EPS), writes=[eps])
    return ones, eps


def ln_block(kb, nc, r, nb, gam, bet, out, ones, eps, scr):
    rsq, ps1, ps2, mean, var = scr
    kb.op("act", lambda: nc.scalar.activation(out=rsq[:, :, :nb], in_=r[:, :, :nb], func=AF.Square), reads=[r], writes=[rsq])
    kb.mm([(lambda oc=oc: nc.tensor.matmul(ps1[:, :nb], lhsT=ones[:], rhs=r[:, oc, :nb], start=(oc == 0), stop=(oc == 7)))
           for oc in range(8)], reads=[ones, r], writes=[ps1])
    kb.mm([(lambda oc=oc: nc.tensor.matmul(ps2[:, :nb], lhsT=ones[:], rhs=rsq[:, oc, :nb], start=(oc == 0), stop=(oc == 7)))
           for oc in range(8)], reads=[ones, rsq], writes=[ps2])
    kb.op("act", lambda: nc.scalar.copy(out=mean[:, :nb], in_=ps1[:, :nb]), reads=[ps1], writes=[mean])
    kb.op("act", lambda: nc.scalar.copy(out=var[:, :nb], in_=ps2[:, :nb]), reads=[ps2], writes=[var])
    m2 = rsq
    kb.op("dve", lambda: nc.vector.tensor_tensor(out=m2[:, 0, :nb], in0=mean[:, :nb], in1=mean[:, :nb], op=ALU.mult),
          reads=[mean, rsq], writes=[rsq])
    kb.op("dve", lambda: nc.vector.tensor_tensor(out=var[:, :nb], in0=var[:, :nb], in1=m2[:, 0, :nb], op=ALU.subtract),
          reads=[var, rsq], writes=[var])
    kb.op("act", lambda: nc.scalar.activation(out=var[:, :nb], in_=var[:, :nb], func=AF.Sqrt, bias=eps[:]),
          reads=[var, eps], writes=[var])
    kb.op("dve", lambda: nc.vector.reciprocal(out=var[:, :nb], in_=var[:, :nb]), reads=[var], writes=[var])
    kb.op("dve", lambda: nc.vector.tensor_tensor(
        out=r[:, :, :nb], in0=r[:, :, :nb], in1=mean[:, :nb].unsqueeze(1).broadcast_to([128, 8, nb]), op=ALU.subtract),
        reads=[r, mean], writes=[r])
    kb.op("pool", lambda: nc.gpsimd.tensor_tensor(
        out=r[:, :, :nb], in0=r[:, :, :nb], in1=var[:, :nb].unsqueeze(1).broadcast_to([128, 8, nb]), op=ALU.mult),
        reads=[r, var], writes=[r])
    for oc in range(8):
        eng = "dve" if oc % 2 == 0 else "pool"
        h_ = nc.vector if eng == "dve" else nc.gpsimd
        kb.op(eng, lambda h_=h_, oc=oc: h_.tensor_scalar(
            out=out[:, oc, :nb], in0=r[:, oc, :nb], scalar1=gam[:, oc:oc + 1], scalar2=bet[:, oc:oc + 1],
            op0=ALU.mult, op1=ALU.add), reads=[r, gam, bet], writes=[out])


def load_ln_params(cx, kb, nc, l, which):
    gam = kb.sbuf("ln_g", [128, 8], F32)
    bet = kb.sbuf("ln_b", [128, 8], F32)
    kb.dma("sp", gam[:], cx.ln_gT[l, which], reads=[cx.ln_gT], writes=[gam])
    kb.dma("sp", bet[:], cx.ln_bT[l, which], reads=[cx.ln_bT], writes=[bet])
    return gam, bet


def stage_merge(cx, kb, nc, l, xsrc, modv):
    with kb.scope():
        ones, eps = ln_setup(cx, kb, nc)
        gam, bet = load_ln_params(cx, kb, nc, l, 0)
        wbr = kb.sbuf("wbr", [128, 4, 4, 1024], BF16)
        wout = kb.sbuf("wout", [128, 8, 1024], BF16)
        for i in range(4):
            kb.dma("pool", wbr[:, i], cx.w_branch[l, i].rearrange("(kc p) c -> p kc c", p=128), reads=[cx.w_branch], writes=[wbr])
        kb.dma("pool", wout[:], cx.w_out[l].rearrange("(kc p) c -> p kc c", p=128), reads=[cx.w_out], writes=[wout])
        wr = kb.sbuf("wr", [128, 8, 36], F32)
        kb.dma("sp", wr[:, :, 0:4], cx.moe_w_group[l].rearrange("(kc p) c -> p kc c", p=128), reads=[cx.moe_w_group], writes=[wr])
        kb.dma("sp", wr[:, :, 4:36], cx.moe_w_expert[l].rearrange("(kc p) c -> p kc c", p=128), reads=[cx.moe_w_expert], writes=[wr])
        rb = kb.sbuf("rb", [128, 36], F32)
        kb.dma("sp", rb[:, 0:4], cx.moe_b_group[l].partition_broadcast(128), reads=[cx.moe_b_group], writes=[rb])
        kb.dma("sp", rb[:, 4:36], cx.moe_b_expert[l].partition_broadcast(128), reads=[cx.moe_b_expert], writes=[rb])
        onep4 = kb.sbuf("onep4", [128, 8, 2], F32)
        kb.op("dve", lambda: nc.vector.tensor_scalar(out=onep4[:], in0=modv[:, 32:40, :], scalar1=1.0, scalar2=None, op0=ALU.add),
              reads=[modv], writes=[onep4])
        brt = [kb.sbuf("brt%d" % i, [128, 4, 4, MB], BF16) for i in range(1)]
        gtt = kb.sbuf("gtt", [128, 32, MB], BF16)
        xb = kb.sbuf("xbm", [128, 8, MB], F32)
        mrg = kb.sbuf("mrg", [128, 8, MB], BF16)
        r = kb.sbuf("rm", [128, 8, MB], F32)
        hx2b = kb.sbuf("hx2b", [128, 8, MB], BF16)
        rsq = kb.sbuf("rsqm", [128, 8, MB], F32)
        x1 = r
        hx2f = rsq
        mean = kb.sbuf("meanm", [128, MB], F32)
        var = kb.sbuf("varm", [128, MB], F32)
        csb = [kb.sbuf("csb%d" % i, [128, MB], F32) for i in range(4)]
        acc = kb.sbuf("accm", [128, MB], F32)
        mixs = kb.sbuf("mixs", [128, MB], F32)
        pbr = [kb.psum("pbr%d" % i, [128, MB]) for i in range(4)]
        pmx = kb.psum("pmx", [128, MB])
        ps1 = kb.psum("ps1m", [128, MB])
        ps2 = kb.psum("ps2m", [128, MB])
        prt = kb.psum("prt", [128, MB])
        brs = [cx.BR_A, cx.BR_B, cx.BR_C, cx.BR_D]
        lgt = kb.sbuf("lgt", [128, 36], F32)
        gmx = kb.sbuf("gmx", [128, 1], F32)
        ngm = kb.sbuf("ngm", [128, 1], F32)
        ge = kb.sbuf("ge", [128, 4], F32)
        gs = kb.sbuf("gsr", [128, 1], F32)
        oh = kb.sbuf("oh", [128, 4], F32)
        lem = kb.sbuf("lem", [128, 4, 8], F32)
        mx8 = kb.sbuf("mx8", [128, 8], F32)
        dd = kb.sbuf("dd", [128, 1], F32)
        w1 = kb.sbuf("w1", [128, 1], F32)
        w2 = kb.sbuf("w2", [128, 1], F32)
        tt1 = kb.sbuf("tt1", [128, 32], F32)
        tt2 = kb.sbuf("tt2", [128, 32], F32)
        wgt = kb.sbuf("wgt", [128, 32], F32)
        for bi, (t0, nb) in enumerate(MBLKS):
            col = blkcol(t0)
            b_ = brt[0]
            for i in range(4):
                kb.dma("sp", b_[:, i, :, :nb], brs[i][:, t0:t0 + nb].rearrange("(kc p) t -> p kc t", p=128),
                       reads=[brs[i]], writes=[b_])
            kb.dma("sp", gtt[:, :, :nb], cx.GATE[:, t0:t0 + nb].rearrange("(j p) t -> p j t", p=128), reads=[cx.GATE], writes=[gtt])
            kb.dma("sp", xb[:, :, :nb], xsrc[:, t0:t0 + nb].rearrange("(kc p) t -> p kc t", p=128), reads=[xsrc], writes=[xb])
            for oc in range(8):
                for i in range(4):
                    kb.mm([(lambda i=i, kc=kc, oc=oc: nc.tensor.matmul(
                        pbr[i][:, :nb], lhsT=wbr[:, i, kc, oc * 128:(oc + 1) * 128], rhs=b_[:, i, kc, :nb],
                        start=(kc == 0), stop=(kc == 3))) for kc in range(4)], reads=[wbr, b_], writes=[pbr[i]])
                    kb.op("act", lambda i=i: nc.scalar.copy(out=csb[i][:, :nb], in_=pbr[i][:, :nb]), reads=[pbr[i]], writes=[csb[i]])
                    eng = "dve" if i % 2 == 0 else "pool"
                    h_ = nc.vector if eng == "dve" else nc.gpsimd
                    kb.op(eng, lambda h_=h_, i=i, oc=oc: h_.tensor_tensor(
                        out=csb[i][:, :nb], in0=csb[i][:, :nb], in1=gtt[:, i * 8 + oc, :nb], op=ALU.mult),
                        reads=[csb[i], gtt], writes=[csb[i]])
                kb.op("dve", lambda: nc.vector.tensor_tensor(out=acc[:, :nb], in0=csb[0][:, :nb], in1=csb[1][:, :nb], op=ALU.add),
                      reads=[csb[0], csb[1]], writes=[acc])
                kb.op("pool", lambda: nc.gpsimd.tensor_tensor(out=csb[2][:, :nb], in0=csb[2][:, :nb], in1=csb[3][:, :nb], op=ALU.add),
                      reads=[csb[2], csb[3]], writes=[csb[2]])
                kb.op("dve", lambda oc=oc: nc.vector.tensor_tensor(out=mrg[:, oc, :nb], in0=acc[:, :nb], in1=csb[2][:, :nb], op=ALU.add),
                      reads=[acc, csb[2]], writes=[mrg])
            for oc in range(8):
                kb.mm([(lambda kc=kc, oc=oc: nc.tensor.matmul(
                    pmx[:, :nb], lhsT=wout[:, kc, oc * 128:(oc + 1) * 128], rhs=mrg[:, kc, :nb],
                    start=(kc == 0), stop=(kc == 7))) for kc in range(8)], reads=[wout, mrg], writes=[pmx])
                kb.op("act", lambda: nc.scalar.copy(out=mixs[:, :nb], in_=pmx[:, :nb]), reads=[pmx], writes=[mixs])
                kb.op("pool", lambda oc=oc: nc.gpsimd.tensor_scalar(
                    out=mixs[:, :nb], in0=mixs[:, :nb], scalar1=modv[:, 16 + oc, col:col + 1], scalar2=None, op0=ALU.mult),
                    reads=[mixs, modv], writes=[mixs])
                kb.op("dve", lambda oc=oc: nc.vector.scalar_tensor_tensor(
                    out=r[:, oc, :nb], in0=xb[:, oc, :nb], scalar=ALPHA, in1=mixs[:, :nb], op0=ALU.mult, op1=ALU.add),
                    reads=[xb, mixs], writes=[r])
            ln_block(kb, nc, r, nb, gam, bet, x1, ones, eps, (rsq, ps1, ps2, mean, var))
            kb.dma("sp", cx.XT[:, t0:t0 + nb].rearrange("(kc p) t -> p kc t", p=128), x1[:, :, :nb],
                   reads=[x1], writes=[], acc_writes=[cx.XT])
            for oc in range(8):
                eng = "dve" if oc % 2 == 0 else "pool"
                h_ = nc.vector if eng == "dve" else nc.gpsimd
                kb.op(eng, lambda h_=h_, oc=oc: h_.tensor_scalar(
                    out=hx2f[:, oc, :nb], in0=x1[:, oc, :nb], scalar1=onep4[:, oc, col:col + 1],
                    scalar2=modv[:, 24 + oc, col:col + 1], op0=ALU.mult, op1=ALU.add),
                    reads=[x1, onep4, modv], writes=[hx2f])
            kb.op("act", lambda: nc.scalar.copy(out=hx2b[:, :, :nb], in_=hx2f[:, :, :nb]), reads=[hx2f], writes=[hx2b])
            kb.dma("sp", cx.HX2[:, t0:t0 + nb].rearrange("(kc p) t -> p kc t", p=128), hx2b[:, :, :nb],
                   reads=[hx2b], writes=[], acc_writes=[cx.HX2])
            for tj in range(nb // 128):
                ti = t0 // 128 + tj
                tsl = slice(tj * 128, (tj + 1) * 128)
                kb.mm([(lambda kc=kc: nc.tensor.matmul(prt[:, 0:36], lhsT=hx2f[:, kc, tsl], rhs=wr[:, kc, :],
                                                       start=(kc == 0), stop=(kc == 7))) for kc in range(8)],
                      reads=[hx2f, wr], writes=[prt])
                kb.op("act", lambda: nc.scalar.copy(out=lgt[:], in_=prt[:, 0:36]), reads=[prt], writes=[lgt])
                kb.op("dve", lambda: nc.vector.tensor_tensor(out=lgt[:], in0=lgt[:], in1=rb[:], op=ALU.add), reads=[lgt, rb], writes=[lgt])
                kb.op("dve", lambda: nc.vector.tensor_reduce(out=gmx[:], in_=lgt[:, 0:4], axis=AX.X, op=ALU.max), reads=[lgt], writes=[gmx])
                kb.op("dve", lambda: nc.vector.tensor_scalar(out=ngm[:], in0=gmx[:], scalar1=-1.0, scalar2=None, op0=ALU.mult), reads=[gmx], writes=[ngm])
                kb.op("act", lambda: nc.scalar.activation(out=ge[:], in_=lgt[:, 0:4], func=AF.Exp, bias=ngm[:]), reads=[lgt, ngm], writes=[ge])
                kb.op("dve", lambda: nc.vector.tensor_reduce(out=gs[:], in_=ge[:], axis=AX.X, op=ALU.add), reads=[ge], writes=[gs])
                kb.op("dve", lambda: nc.vector.reciprocal(out=gs[:], in_=gs[:]), reads=[gs], writes=[gs])
                kb.op("dve", lambda: nc.vector.tensor_scalar(out=oh[:], in0=lgt[:, 0:4], scalar1=gmx[:], scalar2=None, op0=ALU.is_equal), reads=[lgt, gmx], writes=[oh])
                kb.op("dve", lambda: nc.vector.tensor_scalar(out=oh[:], in0=oh[:], scalar1=-1.0, scalar2=1e30, op0=ALU.add, op1=ALU.mult), reads=[oh], writes=[oh])
                kb.op("dve", lambda: nc.vector.tensor_tensor(
                    out=lem[:], in0=lgt[:, 4:36].rearrange("p (g e) -> p g e", e=8), in1=oh[:].unsqueeze(2).broadcast_to([128, 4, 8]), op=ALU.add),
                    reads=[lgt, oh], writes=[lem])
                lemf = lem[:].rearrange("p g e -> p (g e)")
                kb.op("dve", lambda: nc.vector.max(out=mx8[:], in_=lem[:].rearrange("p g e -> p (g e)")), reads=[lem], writes=[mx8])
                kb.op("dve", lambda: nc.vector.tensor_tensor(out=dd[:], in0=mx8[:, 1:2], in1=mx8[:, 0:1], op=ALU.subtract), reads=[mx8], writes=[dd])
                kb.op("act", lambda: nc.scalar.activation(out=dd[:], in_=dd[:], func=AF.Exp), reads=[dd], writes=[dd])
                kb.op("dve", lambda: nc.vector.tensor_scalar(out=w1[:], in0=dd[:], scalar1=1.0, scalar2=None, op0=ALU.add), reads=[dd], writes=[w1])
                kb.op("dve", lambda: nc.vector.reciprocal(out=w1[:], in_=w1[:]), reads=[w1], writes=[w1])
                kb.op("dve", lambda: nc.vector.tensor_tensor(out=w2[:], in0=dd[:], in1=w1[:], op=ALU.mult), reads=[dd, w1], writes=[w2])
                kb.op("dve", lambda: nc.vector.tensor_scalar(
                    out=tt1[:], in0=lem[:].rearrange("p g e -> p (g e)"), scalar1=mx8[:, 0:1], scalar2=w1[:], op0=ALU.is_equal, op1=ALU.mult),
                    reads=[lem, mx8, w1], writes=[tt1])
                kb.op("dve", lambda: nc.vector.tensor_scalar(
                    out=tt2[:], in0=lem[:].rearrange("p g e -> p (g e)"), scalar1=mx8[:, 1:2], scalar2=w2[:], op0=ALU.is_equal, op1=ALU.mult),
                    reads=[lem, mx8, w2], writes=[tt2])
                kb.op("dve", lambda: nc.vector.tensor_tensor(out=tt1[:], in0=tt1[:], in1=tt2[:], op=ALU.add), reads=[tt1, tt2], writes=[tt1])
                kb.op("dve", lambda: nc.vector.tensor_scalar(out=wgt[:], in0=tt1[:], scalar1=gs[:], scalar2=None, op0=ALU.mult), reads=[tt1, gs], writes=[wgt])
                kb.dma("sp", cx.WG_t[:, ti, :], wgt[:], reads=[wgt], writes=[], acc_writes=[cx.WG_t])


def stage_moe(cx, kb, nc, l, modv, yout=None):
    with kb.scope():
        ones, eps = ln_setup(cx, kb, nc)
        gam, bet = load_ln_params(cx, kb, nc, l, 1)
        wgT = kb.sbuf("wgT", [32, T], F32)
        wgl = kb.sbuf("wgl", [128, NT, 32], F32)
        id32 = kb.sbuf("id32", [128, 128], F32)
        sel = kb.sbuf("sel", [32, 32, 128], F32)
        kb.dma("sp", wgl[:], cx.WG_t[:], reads=[cx.WG_t], writes=[wgl])
        kb.dma("sp", id32[:], cx.m_ident32[:], reads=[cx.m_ident32], writes=[id32])
        kb.dma("sp", sel[:], cx.m_sel[:], reads=[cx.m_sel], writes=[sel])
        ptr = [kb.psum("ptr%d" % i, [128, 512]) for i in range(2)]
        for ti in range(NT):
            p_ = ptr[ti % 2]
            kb.mm([lambda p_=p_, ti=ti: nc.tensor.matmul(p_[:32, 0:128], lhsT=wgl[:, ti, :], rhs=id32[:], start=True, stop=True)],
                  reads=[wgl, id32], writes=[p_])
            kb.op("act", lambda p_=p_, ti=ti: nc.scalar.copy(out=wgT[:, ti * 128:(ti + 1) * 128], in_=p_[:32, 0:128]),
                  reads=[p_], writes=[], acc_writes=[wgT])
        precast_expert_weights(cx, kb, nc, l)
        hx = kb.sbuf("hxm", [128, 8, 512], BF16)
        yacc = kb.sbuf("yacc", [128, 8, 512], F32)
        hid = kb.sbuf("hid", [128, 16, 512], BF16)
        wdg = kb.sbuf("wdg", [128, 8, 2, 1024], BF16)
        wgs = [kb.sbuf("wgs%d" % i, [128, 8, 256], BF16) for i in range(2)]
        wus = [kb.sbuf("wus%d" % i, [128, 8, 256], BF16) for i in range(2)]
        xb = kb.sbuf("xbe", [128, 8, 512], F32)
        rsq = kb.sbuf("rsqe", [128, 8, 512], F32)
        mean = kb.sbuf("meane", [128, 512], F32)
        var = kb.sbuf("vare", [128, 512], F32)
        reps = [kb.sbuf("reps%d" % i, [128, 512], F32) for i in range(2)]
        ssb = [kb.sbuf("ssb%d" % i, [128, 512], F32) for i in range(2)]
        tsb = [kb.sbuf("tsb%d" % i, [128, 512], F32) for i in range(2)]
        ytmp = [kb.sbuf("ytmp%d" % i, [128, 512], F32) for i in range(2)]
        pg = [kb.psum("pg%d" % i, [128, 512]) for i in range(2)]
        pu = [kb.psum("pu%d" % i, [128, 512]) for i in range(2)]
        py = ptr
        ps1 = kb.psum("ps1e", [128, 512])
        ps2 = kb.psum("ps2e", [128, 512])
        prep = ps1
        nw = 0
        it = 0
        for bi, (t0, nb) in enumerate(BLKS):
            col = blkcol(t0)
            kb.dma("sp", hx[:, :, :nb], cx.HX2[:, t0:t0 + nb].rearrange("(kc p) t -> p kc t", p=128), reads=[cx.HX2], writes=[hx])
            kb.dma("sp", xb[:, :, :nb], cx.XT[:, t0:t0 + nb].rearrange("(kc p) t -> p kc t", p=128), reads=[cx.XT], writes=[xb])
            for g in range(4):
                kb.dma("sp", wdg[:].rearrange("p e hc d -> p (e hc d)"), cx.WDB[g], reads=[cx.WDB], writes=[wdg])
                for e_ in range(8):
                    e = g * 8 + e_
                    wg_, wu_ = wgs[nw % 2], wus[nw % 2]
                    nw += 1
                    kb.dma("sp", wg_[:].rearrange("p kc f -> p (kc f)"), cx.WGB[e], reads=[cx.WGB], writes=[wg_])
                    kb.dma("act", wu_[:].rearrange("p kc f -> p (kc f)"), cx.WUB[e], reads=[cx.WUB], writes=[wu_])
                    rp = reps[e % 2]
                    kb.mm([lambda e=e: nc.tensor.matmul(prep[:, :nb], lhsT=sel[:, e, :], rhs=wgT[:, t0:t0 + nb], start=True, stop=True)],
                          reads=[sel, wgT], writes=[prep])
                    kb.op("act", lambda rp=rp: nc.scalar.copy(out=rp[:, :nb], in_=prep[:, :nb]), reads=[prep], writes=[rp])
                    for hc in range(2):
                        a = it % 2
                        it += 1
                        kb.mm([(lambda kc=kc, a=a, hc=hc, wg_=wg_: nc.tensor.matmul(
                            pg[a][:, :nb], lhsT=wg_[:, kc, hc * 128:(hc + 1) * 128], rhs=hx[:, kc, :nb],
                            start=(kc == 0), stop=(kc == 7))) for kc in range(8)], reads=[wg_, hx], writes=[pg[a]])
                        kb.mm([(lambda kc=kc, a=a, hc=hc, wu_=wu_: nc.tensor.matmul(
                            pu[a][:, :nb], lhsT=wu_[:, kc, hc * 128:(hc + 1) * 128], rhs=hx[:, kc, :nb],
                            start=(kc == 0), stop=(kc == 7))) for kc in range(8)], reads=[wu_, hx], writes=[pu[a]])
                        kb.op("act", lambda a=a: nc.scalar.activation(out=ssb[a][:, :nb], in_=pg[a][:, :nb], func=AF.Silu),
                              reads=[pg[a]], writes=[ssb[a]])
                        kb.op("act", lambda a=a: nc.scalar.copy(out=tsb[a][:, :nb], in_=pu[a][:, :nb]), reads=[pu[a]], writes=[tsb[a]])
                        kb.op("dve", lambda a=a: nc.vector.tensor_tensor(out=tsb[a][:, :nb], in0=tsb[a][:, :nb], in1=ssb[a][:, :nb], op=ALU.mult),
                              reads=[tsb[a], ssb[a]], writes=[tsb[a]])
                        kb.op("pool", lambda a=a, rp=rp, e_=e_, hc=hc: nc.gpsimd.tensor_tensor(
                            out=hid[:, e_ * 2 + hc, :nb], in0=tsb[a][:, :nb], in1=rp[:, :nb], op=ALU.mult),
                            reads=[tsb[a], rp], writes=[], acc_writes=[hid])
                for oc in range(8):
                    p_ = py[oc % 2]
                    fns = []
                    for j in range(16):
                        fns.append(lambda j=j, oc=oc, p_=p_: nc.tensor.matmul(
                            p_[:, :nb], lhsT=wdg[:, j // 2, j % 2, oc * 128:(oc + 1) * 128], rhs=hid[:, j, :nb],
                            start=(j == 0), stop=(j == 15)))
                    kb.mm(fns, reads=[wdg, hid], writes=[p_])
                    if g == 0:
                        kb.op("act", lambda oc=oc, p_=p_: nc.scalar.copy(out=yacc[:, oc, :nb], in_=p_[:, :nb]), reads=[p_], writes=[], acc_writes=[yacc])
                    else:
                        yt = ytmp[oc % 2]
                        kb.op("act", lambda yt=yt, p_=p_: nc.scalar.copy(out=yt[:, :nb], in_=p_[:, :nb]), reads=[p_], writes=[yt])
                        eng = "dve" if oc % 2 == 0 else "pool"
                        h_ = nc.vector if eng == "dve" else nc.gpsimd
                        kb.op(eng, lambda h_=h_, oc=oc, yt=yt: h_.tensor_tensor(
                            out=yacc[:, oc, :nb], in0=yacc[:, oc, :nb], in1=yt[:, :nb], op=ALU.add),
                            reads=[yt, yacc], writes=[], acc_writes=[yacc])
            for oc in range(8):
                kb.op("pool", lambda oc=oc: nc.gpsimd.tensor_scalar(
                    out=yacc[:, oc, :nb], in0=yacc[:, oc, :nb], scalar1=modv[:, 40 + oc, col:col + 1], scalar2=None, op0=ALU.mult),
                    reads=[yacc, modv], writes=[yacc])
                kb.op("dve", lambda oc=oc: nc.vector.scalar_tensor_tensor(
                    out=yacc[:, oc, :nb], in0=xb[:, oc, :nb], scalar=ALPHA, in1=yacc[:, oc, :nb], op0=ALU.mult, op1=ALU.add),
                    reads=[xb, yacc], writes=[yacc])
            ln_block(kb, nc, yacc, nb, gam, bet, yacc, ones, eps, (rsq, ps1, ps2, mean, var))
            kb.dma("sp", cx.XT[:, t0:t0 + nb].rearrange("(kc p) t -> p kc t", p=128), yacc[:, :, :nb],
                   reads=[yacc], writes=[], acc_writes=[cx.XT])
            if yout is not None and t0 < TL:
                kb.dma("sp", yout[:, t0:t0 + nb].rearrange("(kc p) t -> p kc t", p=128), yacc[:, :, :nb],
                       reads=[yacc], writes=[], acc_writes=[yout])


def precast_expert_weights(cx, kb, nc, l):
    with kb.scope():
        stw = [kb.sbuf("pcw%d" % i, [128, 8, 256], BF16) for i in range(4)]
        std = [kb.sbuf("pcd%d" % i, [128, 16, 1024], BF16) for i in range(2)]
        n = 0
        for g in range(4):
            d_ = std[g % 2]
            kb.dma("pool", d_[:], cx.moe_w_down[l, g].rearrange("e (hc p) d -> p (e hc) d", p=128), reads=[cx.moe_w_down], writes=[d_])
            kb.dma("sp", cx.WDB[g], d_[:].rearrange("p j d -> p (j d)"), reads=[d_], writes=[], acc_writes=[cx.WDB])
            for e_ in range(8):
                e = g * 8 + e_
                for src, dst in ((cx.moe_w_gate, cx.WGB), (cx.moe_w_up, cx.WUB)):
                    w_ = stw[n % 4]
                    n += 1
                    kb.dma("pool", w_[:], src[l, g, e_].rearrange("(kc p) f -> p kc f", p=128), reads=[src], writes=[w_])
                    kb.dma("sp", dst[e], w_[:].rearrange("p kc f -> p (kc f)"), reads=[w_], writes=[], acc_writes=[dst])


def rope_tables():
    t = np.arange(TL)
    row = (t // 64).astype(np.float32)
    colp = (t % 64).astype(np.float32)
    nf = 16
    inv = (np.float32(10000.0) ** (-np.arange(nf, dtype=np.float32) / np.float32(nf))).astype(np.float32)
    cos = np.ones((128, T), np.float32)
    sin = np.zeros((128, T), np.float32)
    for q in range(128):
        d = q % 64
        pos = row if d < 32 else colp
        ang = (pos * inv[d % 16]).astype(np.float32)
        cos[q, :TL] = np.cos(ang).astype(np.float32)
        sin[q, :TL] = np.sin(ang).astype(np.float32)
    pt = np.zeros((128, 128), np.float32)
    for m in range(128):
        if (m % 32) < 16:
            pt[m + 16, m] = -1.0
        else:
            pt[m - 16, m] = 1.0
    return cos, sin, pt


_CONST = {}
_NAIDX = {}


def consts():
    if not _CONST:
        cos, sin, pt = rope_tables()
        _CONST.update(rope_cos=cos, rope_sin=sin, rope_pt=pt)
        _CONST.update(scan_masks())
        _CONST.update(wa_masks())
        sel = np.zeros((32, 32, 128), np.float32)
        for e in range(32):
            sel[e, e, :] = 1.0
        _CONST["m_sel"] = sel
        _CONST["m_ident32"] = np.eye(128, dtype=np.float32)
        _k, _valid, _ri, _ci = na_consts()
        _CONST["m_navalid"] = _valid
        _NAIDX["ri"] = _ri
        _NAIDX["ci"] = _ci
    return _CONST


def core_inputs(inp, b):
    f = lambda a: np.ascontiguousarray(a, dtype=np.float32)
    m = {}
    m["xin"] = f(np.concatenate([inp["x"][b].T, inp["ctx"][b].T], axis=1))
    cv = np.stack([inp["c"][b].reshape(8, 128).T, inp["c_ctx"].reshape(8, 128).T], axis=2)
    m["cvec"] = f(cv)
    m["w_mod"] = f(inp["w_mod"])
    m["b_modT"] = f(inp["b_mod"].reshape(2, 48, 128).transpose(0, 2, 1))
    m["w_in"] = f(inp["w_in"])
    m["hg_gamma"] = f(inp["hg_gamma"])
    m["gla_lr_w"] = f(inp["gla_lr_w"])
    m["gla_lr_b"] = f(inp["gla_lr_b"])
    m["wa_sink"] = f(inp["wa_sink"])
    for k in ("w_branch", "w_out", "moe_w_group", "moe_b_group", "moe_w_expert", "moe_b_expert",
              "moe_w_gate", "moe_w_up", "moe_w_down"):
        m[k] = f(inp[k])
    m["ln_gT"] = f(inp["ln_g"].reshape(2, 2, 8, 128).transpose(0, 1, 3, 2))
    m["ln_bT"] = f(inp["ln_b"].reshape(2, 2, 8, 128).transpose(0, 1, 3, 2))
    consts()
    m["na_bias"] = f(inp["na_rpb"][:, :, _NAIDX["ri"], _NAIDX["ci"]])
    m.update(consts())
    return m


FUSED = 1


def build_program(layers, final):
    nc = bass.Bass("TRN2", target_bir_lowering=False)
    cx = Ctx()
    declare_inputs(cx, nc)
    if final:
        yout = Buf(nc.dram_tensor("yout", [D, TL], F32, kind="ExternalOutput"), "yout")
    else:
        yout = None
    with ExitStack() as st:
        kb = KB(nc, st)
        declare_scratch(cx, kb, debug=False, as_output=(() if final else ("XT",)))
        modT = [kb.sbuf("modT%d" % l, [128, 48, 2], F32) for l in range(2)]
        stage_mod(cx, kb, nc, modT)
        first = True
        for l in layers:
            xsrc = cx.xin if first else cx.XT
            first = False
            with kb.scope():
                lbt = [kb.sbuf("lbt%d" % i, [128, 1024], F32) for i in range(2)]
                omlt = [kb.sbuf("omlt%d" % i, [128, 1024], F32) for i in range(2)]
                stage_lb(cx, kb, nc, lbt, omlt)
                hxT = kb.sbuf("hxT", [128, 8, T], BF16)
                stage_modulate(cx, kb, nc, xsrc, modT[l], 0, 1, hxT)
                stage_inproj_fm(cx, kb, nc, l, hxT)
                stage_inproj_tm(cx, kb, nc, l, hxT, lbt[l], omlt[l])
            stage_gla_gate(cx, kb, nc, l)
            stage_scan(cx, kb, nc, "gla")
            stage_scan(cx, kb, nc, "hgrn")
            stage_wa(cx, kb, nc, l)
            stage_na(cx, kb, nc, l)
            stage_merge(cx, kb, nc, l, xsrc, modT[l])
            stage_moe(cx, kb, nc, l, modT[l], yout if (final and l == layers[-1]) else None)
        if final:
            kb.finish([yout])
        else:
            kb.finish([cx.XT])
    return nc


def kernel(**inputs):
    inp = {k: np.asarray(v) for k, v in inputs.items()}
    in_maps = [core_inputs(inp, b) for b in range(4)]
    if FUSED:
        nc = build_program([0, 1], True)
        res = run_bass_kernel_spmd(nc, in_maps, core_ids=list(range(4)))
    else:
        nc0 = build_program([0], False)
        res0 = run_bass_kernel_spmd(nc0, in_maps, core_ids=list(range(4)))
        for b in range(4):
            in_maps[b]["xin"] = np.ascontiguousarray(res0.results[b]["XT"])
        nc1 = build_program([1], True)
        res = run_bass_kernel_spmd(nc1, in_maps, core_ids=list(range(4)))
    out = np.stack([np.ascontiguousarray(res.results[b]["yout"].T) for b in range(4)], axis=0)
    return out.astype(np.float32)
```

```python
import numpy as np
from contextlib import ExitStack
import concourse.bass as bass
import concourse.mybir as mybir
from concourse.bass_utils import run_bass_kernel_spmd
import os

F32 = mybir.dt.float32
BF16 = mybir.dt.bfloat16
U32 = mybir.dt.uint32
AF = mybir.ActivationFunctionType
ALU = mybir.AluOpType
AX = mybir.AxisListType


class Buf:
    __slots__ = ("t", "w", "r", "name")

    def __init__(self, t, name=""):
        self.t = t
        self.w = []
        self.r = []
        self.name = name

    def __getitem__(self, idx):
        return self.t[idx]


class Dep:
    __slots__ = ("w", "r")

    def __init__(self):
        self.w = []
        self.r = []


class Eng:
    def __init__(self, name, h, sem):
        self.name = name
        self.h = h
        self.sem = sem
        self.count = 0
        self.seen = {}


class KB:
    def __init__(self, nc, stack, n_dma_sems=8):
        self.nc = nc
        self.stack = stack
        self.sems = {}
        self.engs = {}
        for name, h in (("pe", nc.tensor), ("act", nc.scalar), ("dve", nc.vector),
                        ("pool", nc.gpsimd), ("sp", nc.sync)):
            s = stack.enter_context(nc.semaphore("s_" + name))
            self.sems[id(s)] = s
            self.engs[name] = Eng(name, h, s)
        self.dma_pool = {}
        for q in ("sp", "pool", "act"):
            lst = []
            for i in range(n_dma_sems):
                s = stack.enter_context(nc.semaphore("d_%s%d" % (q, i)))
                self.sems[id(s)] = s
                lst.append([s, 0])
            self.dma_pool[q] = [lst, 0]
        self.ninstr = 0

    def scope(self):
        kb = self

        class _Scope:
            def __enter__(self_s):
                self_s.prev = kb.stack
                self_s.st = ExitStack()
                self_s.st.__enter__()
                kb.stack = self_s.st
                kb.nscope = getattr(kb, "nscope", 0) + 1
                return self_s

            def __exit__(self_s, *a):
                if a[0] is None:
                    kb.barrier()
                kb.stack = self_s.prev
                return self_s.st.__exit__(*a)
        return _Scope()

    def sbuf(self, name, shape, dtype):
        name = "%s_s%d" % (name, getattr(self, "nscope", 0))
        t = self.stack.enter_context(self.nc.sbuf_tensor(name, list(shape), dtype))
        return Buf(t, name)

    def psum(self, name, shape, dtype=F32):
        name = "%s_s%d" % (name, getattr(self, "nscope", 0))
        t = self.stack.enter_context(self.nc.psum_tensor(name, list(shape), dtype))
        return Buf(t, name)

    def dram(self, name, shape, dtype, kind="Internal"):
        t = self.nc.dram_tensor(name, list(shape), dtype, kind=kind)
        return Buf(t, name)

    def _wait(self, e, deps):
        need = {}
        for (sid, val) in deps:
            if need.get(sid, 0) < val:
                need[sid] = val
        for sid, val in need.items():
            if e.seen.get(sid, 0) < val:
                e.h.wait_ge(self.sems[sid], val)
                e.seen[sid] = val
                self.ninstr += 1

    def _collect(self, reads, writes, acc_writes=()):
        deps = []
        for b in reads:
            deps += b.w
        for b in writes:
            deps += b.w
            deps += b.r
        for b in acc_writes:
            deps += b.r
        return deps

    @staticmethod
    def _compact(lst):
        mx = {}
        for (sid, v) in lst:
            if mx.get(sid, 0) < v:
                mx[sid] = v
        return list(mx.items())

    def _commit(self, token, reads, writes, acc_writes=()):
        for b in acc_writes:
            b.w.append(token)
            if len(b.w) > 64:
                b.w = self._compact(b.w)
        for b in reads:
            b.r.append(token)
            if len(b.r) > 64:
                mx = {}
                for (sid, v) in b.r:
                    if mx.get(sid, 0) < v:
                        mx[sid] = v
                b.r = list(mx.items())
        for b in writes:
            b.w = [token]
            b.r = []

    def _cutcheck(self):
        import os
        cut = os.environ.get("KB_CUT")
        self.nops = getattr(self, "nops", 0) + 1
        skip = os.environ.get("KB_SKIP")
        if skip and self.nops in [int(x) for x in skip.split(",")]:
            return True
        return cut is not None and self.nops > int(cut)

    def op(self, eng, fn, reads=(), writes=(), same_engine_sync=True, acc_writes=()):
        if self._cutcheck():
            return None
        e = self.engs[eng]
        deps = self._collect(reads, writes, acc_writes)
        if not same_engine_sync:
            deps = [d for d in deps if d[0] != id(e.sem)]
        self._wait(e, deps)
        ins = fn()
        e.count += 1
        ins.then_inc(e.sem, 1)
        self.ninstr += 1
        token = (id(e.sem), e.count)
        self._commit(token, reads, writes, acc_writes)
        return token

    def mm(self, fns, reads=(), writes=()):
        if self._cutcheck():
            return None
        e = self.engs["pe"]
        deps = self._collect(reads, writes)
        deps = [d for d in deps if d[0] != id(e.sem)]
        self._wait(e, deps)
        ins = None
        for fn in fns:
            ins = fn()
            self.ninstr += 1
        e.count += 1
        ins.then_inc(e.sem, 1)
        token = (id(e.sem), e.count)
        self._commit(token, reads, writes)
        return token

    def dma(self, q, out, in_, reads=(), writes=(), acc_writes=(), **kw):
        e = self.engs[q]
        pool = self.dma_pool[q]
        lst, idx = pool
        ent = lst[idx % len(lst)]
        pool[1] = idx + 1
        s, total = ent
        deps = self._collect(reads, writes, acc_writes)
        deps.append((id(s), total))
        import os
        if os.environ.get("DMA_SERIAL") and getattr(self, "_last_dma", {}).get(q):
            deps.append(self._last_dma[q])
        self._wait(e, deps)
        ins = e.h.dma_start(out=out, in_=in_, **kw)
        ins.then_inc(s, 16)
        ent[1] = total + 16
        self.ninstr += 1
        token = (id(s), total + 16)
        if not hasattr(self, "_last_dma"):
            self._last_dma = {}
        self._last_dma[q] = token
        self._commit(token, reads, writes, acc_writes)
        return token

    def init_memory(self, nbytes=196608):
        nc = self.nc
        with self.scope():
            n = nbytes // 4
            big = self.sbuf("initbig", [128, n], F32)
            q = n // 4
            self.op("dve", lambda: nc.vector.memset(big[:, 0:q], 0.0), writes=[])
            self.op("pool", lambda: nc.gpsimd.memset(big[:, q:2 * q], 0.0), writes=[])
            self.op("dve", lambda: nc.vector.memset(big[:, 2 * q:3 * q], 0.0), writes=[])
            self.op("pool", lambda: nc.gpsimd.memset(big[:, 3 * q:n], 0.0), writes=[])
            pss = [self.psum("initps%d" % i, [128, 512]) for i in range(8)]
            for p in pss:
                self.op("dve", lambda p=p: nc.vector.memset(p[:], 0.0), writes=[])

    def barrier(self):
        allt = []
        for e in self.engs.values():
            if e.count:
                allt.append((id(e.sem), e.count))
        for q, (lst, _) in self.dma_pool.items():
            for s, total in lst:
                if total:
                    allt.append((id(s), total))
        for e in self.engs.values():
            self._wait(e, allt)

    def finish(self, tokens_bufs):
        e = self.engs["sp"]
        deps = []
        for b in tokens_bufs:
            deps += b.w
        self._wait(e, deps)

T = 4352
TL = 4096

D = 1024
TL = 4096
TC = 256
T = TL + TC
NT = T // 128
BLKS = [(i * 512, 512) for i in range(8)] + [(4096, 256)]
DIN = 10528
ALPHA = (2.0 * 2) ** 0.25

COLS = dict(a_q=(0, 256), a_k=(256, 256), a_v=(512, 512), a_g=(1024, 512), a_lr=(1536, 32),
            b_q=(1568, 512), b_k=(2080, 512), b_v=(2592, 512),
            c_q=(3104, 512), c_k=(3616, 128), c_v=(3744, 128),
            d_q=(3872, 512), d_f=(4384, 1024), d_i=(5408, 512), d_g=(5920, 512), gate=(6432, 4096))


def blkcol(t0):
    return 1 if t0 >= TL else 0


class Ctx:
    pass


def declare_inputs(cx, nc):
    def din(name, shape, dt=F32):
        return Buf(nc.dram_tensor(name, list(shape), dt, kind="ExternalInput"), name)
    cx.xin = din("xin", [D, T])
    cx.cvec = din("cvec", [128, 8, 2])
    cx.w_mod = din("w_mod", [2, D, 6 * D])
    cx.b_modT = din("b_modT", [2, 128, 48])
    cx.w_in = din("w_in", [2, D, DIN])
    cx.rope_cos = din("rope_cos", [128, T])
    cx.rope_sin = din("rope_sin", [128, T])
    cx.rope_pt = din("rope_pt", [128, 128])
    cx.hg_gamma = din("hg_gamma", [2, 2, 512])
    cx.gla_lr_w = din("gla_lr_w", [2, 2, 16, 256])
    cx.gla_lr_b = din("gla_lr_b", [2, 2, 256])
    for nm in ("mg", "mh"):
        setattr(cx, nm + "_mq", din(nm + "_mq", [128, 2, 130]))
        setattr(cx, nm + "_mk", din(nm + "_mk", [128, 2, 128]))
    cx.m_ma = din("m_ma", [128, 2, 128])
    cx.m_ident = din("m_ident", [128, 128])
    cx.m_mau = din("m_mau", [128, 2, 128], U32)
    cx.m_wa = din("m_wa", [128, 2, 128])
    cx.wa_sink = din("wa_sink", [2, 8])
    cx.m_navalid = din("m_navalid", [128, 21, 128])
    cx.w_branch = din("w_branch", [2, 4, 512, 1024])
    cx.w_out = din("w_out", [2, 1024, 1024])
    cx.ln_gT = din("ln_gT", [2, 2, 128, 8])
    cx.ln_bT = din("ln_bT", [2, 2, 128, 8])
    cx.moe_w_group = din("moe_w_group", [2, 1024, 4])
    cx.moe_b_group = din("moe_b_group", [2, 4])
    cx.moe_w_expert = din("moe_w_expert", [2, 1024, 32])
    cx.moe_b_expert = din("moe_b_expert", [2, 32])
    cx.moe_w_gate = din("moe_w_gate", [2, 4, 8, 1024, 256])
    cx.moe_w_up = din("moe_w_up", [2, 4, 8, 1024, 256])
    cx.moe_w_down = din("moe_w_down", [2, 4, 8, 256, 1024])
    cx.m_sel = din("m_sel", [32, 32, 128])
    cx.m_ident32 = din("m_ident32", [128, 128])
    cx.na_bias = din("na_bias", [2, 8, 128, 21, 128])


def declare_scratch(cx, kb, debug, as_input=(), as_output=()):
    kind = "ExternalOutput" if debug else "Internal"

    def ds(name, shape, dt):
        b = kb.dram(name, shape, dt, kind=("ExternalInput" if name in as_input else ("ExternalOutput" if name in as_output else kind)))
        setattr(cx, name, b)
        return b
    ds("XT", [D, T], F32)
    ds("QA", [256, T], BF16)
    ds("GA", [512, T], BF16)
    ds("LR1", [33, T], F32)
    ds("QB", [512, T], BF16)
    ds("KB_", [512, T], BF16)
    ds("QC", [512, T], BF16)
    ds("KC", [128, T], BF16)
    ds("QD", [512, T], BF16)
    ds("GD", [512, T], BF16)
    ds("GATE", [4096, T], BF16)
    ds("KVA_t", [128, NT, 768], BF16)
    ds("VB_t", [128, NT, 512], BF16)
    ds("VC_t", [128, NT, 128], BF16)
    ds("VD_t", [128, NT, 512], BF16)
    ds("LGD_t", [128, NT, 1024], F32)
    ds("KD_t", [128, NT, 1024], BF16)
    ds("LGA_t", [128, NT, 512], F32)
    ds("MODT", [2, 128, 96], F32)
    ds("HX2", [D, T], BF16)
    ds("WGB", [32, 128, 2048], BF16)
    ds("WUB", [32, 128, 2048], BF16)
    ds("WDB", [4, 128, 16384], BF16)
    ds("WG_t", [128, NT, 32], F32)
    ds("WGT", [32, T], F32)
    ds("BR_A", [512, T], BF16)
    ds("BR_B", [512, T], BF16)
    ds("BR_C", [512, T], BF16)
    ds("BR_D", [512, T], BF16)


def stage_mod(cx, kb, nc, modT):
    with kb.scope():
        cv = kb.sbuf("cv", [128, 8, 2], F32)
        sc = kb.sbuf("sc", [128, 8, 2], F32)
        kb.dma("sp", cv[:], cx.cvec[:], reads=[cx.cvec], writes=[cv])
        kb.op("act", lambda: nc.scalar.activation(out=sc[:], in_=cv[:], func=AF.Silu), reads=[cv], writes=[sc])
        wbufs = [kb.sbuf("wm%d" % i, [128, 8, 1024], F32) for i in range(2)]
        bm = kb.sbuf("bm", [128, 48], F32)
        ps = kb.psum("ps_mod", [128, 96])
        wi = 0
        for l in range(2):
            kb.dma("sp", bm[:], cx.b_modT[l], reads=[cx.b_modT], writes=[bm])
            for g in range(6):
                wb = wbufs[wi % 2]
                wi += 1
                src = cx.w_mod[l, :, g * 1024:(g + 1) * 1024].rearrange("(kc p) c -> p kc c", p=128)
                kb.dma("sp", wb[:], src, reads=[cx.w_mod], writes=[wb])
                for j in range(8):
                    jj = g * 8 + j
                    fns = []
                    for kc in range(8):
                        fns.append(lambda kc=kc, j=j, jj=jj, wb=wb: nc.tensor.matmul(
                            ps[:, 2 * jj:2 * jj + 2], lhsT=wb[:, kc, j * 128:(j + 1) * 128], rhs=sc[:, kc, :],
                            start=(kc == 0), stop=(kc == 7)))
                    kb.mm(fns, reads=[wb, sc], writes=[ps])
            m = modT[l]
            kb.op("dve", lambda m=m: nc.vector.tensor_tensor(
                out=m[:], in0=ps[:].rearrange("p (j c) -> p j c", c=2),
                in1=bm[:].unsqueeze(2).broadcast_to([128, 48, 2]), op=ALU.add),
                reads=[ps, bm], writes=[m])
            kb.dma("sp", cx.MODT[l], m[:].rearrange("p j c -> p (j c)"), reads=[m], writes=[cx.MODT])


def stage_modulate(cx, kb, nc, src_dram, modv, shift_idx, scale_idx, hxT, hx32=None):
    with kb.scope():
        onep = kb.sbuf("onep", [128, 8, 2], F32)
        kb.op("dve", lambda: nc.vector.tensor_scalar(
            out=onep[:], in0=modv[:, scale_idx * 8:(scale_idx + 1) * 8, :], scalar1=1.0, scalar2=None, op0=ALU.add),
            reads=[modv], writes=[onep])
        xb = [kb.sbuf("xb%d" % i, [128, 8, 512], F32) for i in range(2)]
        for bi, (t0, nb) in enumerate(BLKS):
            col = blkcol(t0)
            x = xb[bi % 2]
            kb.dma("sp", x[:, :, :nb], src_dram[:, t0:t0 + nb].rearrange("(kc p) t -> p kc t", p=128),
                   reads=[src_dram], writes=[x])
            for kc in range(8):
                eng = "dve" if kc % 2 == 0 else "pool"
                h = nc.vector if eng == "dve" else nc.gpsimd
                kb.op(eng, lambda h=h, kc=kc, x=x: h.tensor_scalar(
                    out=hxT[:, kc, t0:t0 + nb], in0=x[:, kc, :nb],
                    scalar1=onep[:, kc, col:col + 1], scalar2=modv[:, shift_idx * 8 + kc, col:col + 1],
                    op0=ALU.mult, op1=ALU.add), reads=[x, onep, modv], writes=[hxT])


FM_GROUPS = [
    ("a_q", "QA", "copy"), ("a_g", "GA", "silu"), ("a_lr", "LR1", "copy32"),
    ("b_q", "QB", "copy"), ("b_k", "KB_", "copy"),
    ("c_q", "QC", "rope"), ("c_k", "KC", "rope"),
    ("d_q", "QD", "silu"), ("d_g", "GD", "silu"), ("gate", "GATE", "sigmoid"),
]


def stage_inproj_fm(cx, kb, nc, l, hxT, only=None):
    with kb.scope():
        wsl = [kb.sbuf("wsl%d" % i, [128, 8, 512], BF16) for i in range(2)]
        stg = [kb.sbuf("stg%d" % i, [128, T], BF16) for i in range(2)]
        stg32 = kb.sbuf("stg32", [32, T], F32)
        pss = [kb.psum("psf%d" % i, [128, 512]) for i in range(4)]
        psr = [kb.psum("psr%d" % i, [128, 512]) for i in range(2)]
        cosT = kb.sbuf("cosT", [128, T], F32)
        sinT = kb.sbuf("sinT", [128, T], F32)
        ptm = kb.sbuf("ptm", [128, 128], BF16)
        tmpb = [kb.sbuf("tmpb%d" % i, [128, 512], BF16) for i in range(2)]
        t1 = [kb.sbuf("t1_%d" % i, [128, 512], F32) for i in range(2)]
        t2 = [kb.sbuf("t2_%d" % i, [128, 512], F32) for i in range(2)]
        ones = kb.sbuf("ones1", [1, T], F32)
        kb.dma("sp", cosT[:], cx.rope_cos[:], reads=[cx.rope_cos], writes=[cosT])
        kb.dma("sp", sinT[:], cx.rope_sin[:], reads=[cx.rope_sin], writes=[sinT])
        kb.dma("pool", ptm[:], cx.rope_pt[:], reads=[cx.rope_pt], writes=[ptm])
        kb.op("dve", lambda: nc.vector.memset(ones[:], 1.0), writes=[ones])
        kb.dma("sp", cx.LR1[32:33, :], ones[:], reads=[ones], writes=[], acc_writes=[cx.LR1])
        nsl = 0
        nps = 0
        nst = 0
        nrp = 0
        for (name, dest, kind) in FM_GROUPS:
            if only is not None and name not in only:
                continue
            c0, nc_ = COLS[name]
            dbuf = getattr(cx, dest)
            for s0 in range(0, nc_, 512):
                sw = min(512, nc_ - s0)
                w = wsl[nsl % 2]
                nsl += 1
                kb.dma("pool", w[:, :, :sw],
                       cx.w_in[l, :, c0 + s0:c0 + s0 + sw].rearrange("(kc p) c -> p kc c", p=128),
                       reads=[cx.w_in], writes=[w])
                for m0 in range(0, sw, 128):
                    mw = min(128, sw - m0)
                    if kind == "copy32":
                        st = stg32
                    else:
                        st = stg[nst % 2]
                        nst += 1
                    for bi, (t0, nb) in enumerate(BLKS):
                        ps = pss[nps % 4]
                        nps += 1
                        fns = [(lambda kc=kc, ps=ps, w=w: nc.tensor.matmul(
                            ps[:mw, :nb], lhsT=w[:, kc, m0:m0 + mw], rhs=hxT[:, kc, t0:t0 + nb],
                            start=(kc == 0), stop=(kc == 7))) for kc in range(8)]
                        kb.mm(fns, reads=[w, hxT], writes=[ps])
                        o = st[:mw, t0:t0 + nb]
                        if kind in ("copy", "copy32"):
                            if nps % 2 == 0:
                                kb.op("dve", lambda ps=ps, o=o: nc.vector.tensor_copy(out=o, in_=ps[:mw, :nb]),
                                      reads=[ps], writes=[st])
                            else:
                                kb.op("act", lambda ps=ps, o=o: nc.scalar.copy(out=o, in_=ps[:mw, :nb]),
                                      reads=[ps], writes=[st])
                        elif kind == "silu":
                            kb.op("act", lambda ps=ps, o=o: nc.scalar.activation(out=o, in_=ps[:mw, :nb], func=AF.Silu),
                                  reads=[ps], writes=[st])
                        elif kind == "sigmoid":
                            kb.op("act", lambda ps=ps, o=o: nc.scalar.activation(out=o, in_=ps[:mw, :nb], func=AF.Sigmoid),
                                  reads=[ps], writes=[st])
                        elif kind == "rope":
                            tb = tmpb[nrp % 2]
                            a1 = t1[nrp % 2]
                            a2 = t2[nrp % 2]
                            pr = psr[nrp % 2]
                            nrp += 1
                            kb.op("act", lambda ps=ps, tb=tb: nc.scalar.copy(out=tb[:mw, :nb], in_=ps[:mw, :nb]),
                                  reads=[ps], writes=[tb])
                            kb.mm([lambda pr=pr, tb=tb: nc.tensor.matmul(pr[:mw, :nb], lhsT=ptm[:mw, :mw], rhs=tb[:mw, :nb],
                                                                         start=True, stop=True)],
                                  reads=[ptm, tb], writes=[pr])
                            kb.op("dve", lambda tb=tb, a1=a1: nc.vector.tensor_tensor(
                                out=a1[:mw, :nb], in0=tb[:mw, :nb], in1=cosT[:mw, t0:t0 + nb], op=ALU.mult),
                                reads=[tb, cosT], writes=[a1])
                            kb.op("dve", lambda pr=pr, a2=a2: nc.vector.tensor_tensor(
                                out=a2[:mw, :nb], in0=pr[:mw, :nb], in1=sinT[:mw, t0:t0 + nb], op=ALU.mult),
                                reads=[pr, sinT], writes=[a2])
                            kb.op("pool", lambda a1=a1, a2=a2, o=o: nc.gpsimd.tensor_tensor(
                                out=o, in0=a1[:mw, :nb], in1=a2[:mw, :nb], op=ALU.add),
                                reads=[a1, a2], writes=[st])
                    kb.dma("sp", dbuf[s0 + m0:s0 + m0 + mw, :], st[:mw, :], reads=[st], writes=[], acc_writes=[dbuf])


TM_GROUPS = [
    (256, 512, "KVA_t", 0, "copy"), (768, 256, "KVA_t", 512, "copy"),
    (2592, 512, "VB_t", 0, "copy"), (3744, 128, "VC_t", 0, "copy"),
    (5408, 512, "VD_t", 0, "copy"),
    (4384, 512, None, 0, "hgf"), (4896, 512, None, 512, "hgf"),
]


def stage_inproj_tm(cx, kb, nc, l, hxT, lbt, omlt, only=None):
    with kb.scope():
        wsl = [kb.sbuf("wtl%d" % i, [128, 8, 512], BF16) for i in range(2)]
        pss = [kb.psum("pst%d" % i, [128, 512]) for i in range(4)]
        stb = [kb.sbuf("stb%d" % i, [128, 512], BF16) for i in range(4)]
        e1 = [kb.sbuf("e1_%d" % i, [128, 512], F32) for i in range(2)]
        r1 = [kb.sbuf("r1_%d" % i, [128, 512], F32) for i in range(2)]
        lg = [kb.sbuf("lg_%d" % i, [128, 512], F32) for i in range(2)]
        kk = [kb.sbuf("kk_%d" % i, [128, 512], BF16) for i in range(2)]
        nsl = nps = nst = nh = 0
        for (c0, ncol, dest, doff, kind) in TM_GROUPS:
            if only is not None and kind not in only:
                continue
            w = wsl[nsl % 2]
            nsl += 1
            kb.dma("pool", w[:, :, :ncol], cx.w_in[l, :, c0:c0 + ncol].rearrange("(kc p) c -> p kc c", p=128),
                   reads=[cx.w_in], writes=[w])
            for ti in range(NT):
                ts = slice(ti * 128, (ti + 1) * 128)
                ps = pss[nps % 4]
                nps += 1
                fns = [(lambda kc=kc, ps=ps, w=w: nc.tensor.matmul(
                    ps[:, :ncol], lhsT=hxT[:, kc, ts], rhs=w[:, kc, :ncol],
                    start=(kc == 0), stop=(kc == 7))) for kc in range(8)]
                kb.mm(fns, reads=[w, hxT], writes=[ps])
                if kind == "copy":
                    st = stb[nst % 4]
                    nst += 1
                    if nst % 2 == 0:
                        kb.op("dve", lambda ps=ps, st=st: nc.vector.tensor_copy(out=st[:, :ncol], in_=ps[:, :ncol]),
                              reads=[ps], writes=[st])
                    else:
                        kb.op("act", lambda ps=ps, st=st: nc.scalar.copy(out=st[:, :ncol], in_=ps[:, :ncol]),
                              reads=[ps], writes=[st])
                    db = getattr(cx, dest)
                    kb.dma("sp", db[:, ti, doff:doff + ncol], st[:, :ncol], reads=[st], writes=[], acc_writes=[db])
                else:
                    e = e1[nh % 2]
                    r = r1[nh % 2]
                    g = lg[nh % 2]
                    k = kk[nh % 2]
                    nh += 1
                    cs = slice(doff, doff + 512)
                    kb.op("act", lambda ps=ps, e=e: nc.scalar.activation(out=e[:], in_=ps[:], func=AF.Exp, scale=-1.0),
                          reads=[ps], writes=[e])
                    kb.op("pool", lambda e=e: nc.gpsimd.tensor_scalar(
                        out=e[:], in0=e[:], scalar1=1.0, scalar2=None, op0=ALU.add),
                        reads=[e], writes=[e])
                    kb.op("dve", lambda e=e, r=r: nc.vector.reciprocal(out=r[:], in_=e[:]),
                          reads=[e], writes=[r])
                    kb.op("dve", lambda r=r: nc.vector.tensor_tensor(out=r[:], in0=r[:], in1=omlt[:, cs], op=ALU.mult),
                          reads=[r, omlt], writes=[r])
                    kb.op("pool", lambda r=r: nc.gpsimd.tensor_tensor(out=r[:], in0=r[:], in1=lbt[:, cs], op=ALU.add),
                          reads=[r, lbt], writes=[r])
                    kb.op("act", lambda r=r, g=g: nc.scalar.activation(out=g[:], in_=r[:], func=AF.Ln),
                          reads=[r], writes=[g])
                    kb.op("pool", lambda r=r, k=k: nc.gpsimd.tensor_scalar(
                        out=k[:], in0=r[:], scalar1=-1.0, scalar2=1.0, op0=ALU.mult, op1=ALU.add),
                        reads=[r], writes=[k])
                    kb.dma("sp", cx.LGD_t[:, ti, cs], g[:], reads=[g], writes=[], acc_writes=[cx.LGD_t])
                    kb.dma("sp", cx.KD_t[:, ti, cs], k[:], reads=[k], writes=[], acc_writes=[cx.KD_t])


def stage_lb(cx, kb, nc, lbt, omlt):
    with kb.scope():
        gam = kb.sbuf("gam", [128, 2, 2, 512], F32)
        kb.dma("sp", gam[:].rearrange("p a b c -> p (a b c)"),
               cx.hg_gamma[:].rearrange("a b c -> (a b c)").partition_broadcast(128),
               reads=[cx.hg_gamma], writes=[gam])
        d = kb.sbuf("gamd", [128, 2, 512], F32)
        kb.op("dve", lambda: nc.vector.tensor_tensor(out=d[:], in0=gam[:, :, 0, :], in1=gam[:, :, 1, :], op=ALU.subtract),
              reads=[gam], writes=[d])
        kb.op("act", lambda: nc.scalar.activation(out=d[:], in_=d[:], func=AF.Exp), reads=[d], writes=[d])
        l1 = lbt[1]
        kb.op("dve", lambda: nc.vector.tensor_scalar(
            out=d[:], in0=d[:], scalar1=1.0, scalar2=None, op0=ALU.add), reads=[d], writes=[d])
        kb.op("dve", lambda: nc.vector.reciprocal(out=l1[:], in_=d[:].rearrange("p a c -> p (a c)")),
              reads=[d], writes=[l1])
        l0 = lbt[0]
        kb.op("dve", lambda: nc.vector.memset(l0[:], 0.0), writes=[l0])
        for l in range(2):
            o = omlt[l]
            lb = lbt[l]
            kb.op("dve", lambda o=o, lb=lb: nc.vector.tensor_scalar(
                out=o[:], in0=lb[:], scalar1=-1.0, scalar2=1.0, op0=ALU.mult, op1=ALU.add),
                reads=[lb], writes=[o])


def stage_gla_gate(cx, kb, nc, l):
    with kb.scope():
        wlr = kb.sbuf("wlr", [33, 512], F32)
        lr1 = kb.sbuf("lr1", [33, T], F32)
        kb.op("dve", lambda: nc.vector.memset(wlr[:], 0.0), writes=[wlr])
        kb.dma("sp", wlr[0:16, 0:256], cx.gla_lr_w[l, 0], reads=[cx.gla_lr_w], writes=[wlr])
        kb.dma("sp", wlr[16:32, 256:512], cx.gla_lr_w[l, 1], reads=[cx.gla_lr_w], writes=[wlr])
        kb.dma("sp", wlr[32:33, :], cx.gla_lr_b[l].rearrange("a c -> (a c)").unsqueeze(0),
               reads=[cx.gla_lr_b], writes=[wlr])
        kb.dma("sp", lr1[:], cx.LR1[:], reads=[cx.LR1], writes=[lr1])
        pss = [kb.psum("psg%d" % i, [128, 512]) for i in range(2)]
        ee = [kb.sbuf("ge%d" % i, [128, 512], F32) for i in range(2)]
        gg = [kb.sbuf("gg%d" % i, [128, 512], F32) for i in range(2)]
        for ti in range(NT):
            ts = slice(ti * 128, (ti + 1) * 128)
            ps = pss[ti % 2]
            e = ee[ti % 2]
            g = gg[ti % 2]
            kb.mm([lambda ps=ps: nc.tensor.matmul(ps[:], lhsT=lr1[:, ts], rhs=wlr[:], start=True, stop=True)],
                  reads=[lr1, wlr], writes=[ps])
            kb.op("act", lambda ps=ps, e=e: nc.scalar.activation(out=e[:], in_=ps[:], func=AF.Exp, scale=-1.0),
                  reads=[ps], writes=[e])
            kb.op("act", lambda e=e, g=g: nc.scalar.activation(out=g[:], in_=e[:], func=AF.Ln, bias=1.0),
                  reads=[e], writes=[g])
            kb.dma("sp", cx.LGA_t[:, ti, :], g[:], reads=[g], writes=[], acc_writes=[cx.LGA_t])

SCAN_CFG = {
    "gla": dict(H=4, dk=64, qsrc="QA", qscale=0.125, ksrc="KVA_t", kz=0, kcol0=0, vsrc="KVA_t", vcol0=256,
                gsrc="LGA_t", gz=256, gate="GA", dest="BR_A", mask="mg"),
    "hgrn": dict(H=4, dk=128, qsrc="QD", qscale=1.0, ksrc="KD_t", kz=512, kcol0=0, vsrc="VD_t", vcol0=0,
                 gsrc="LGD_t", gz=512, gate="GD", dest="BR_D", mask="mh"),
}


def view(b):
    return Buf(b.t, b.name + "_v")


def stage_scan(cx, kb, nc, which, heads=None):
    cfg = SCAN_CFG[which]
    H, dk = cfg["H"], cfg["dk"]
    dv = 128
    qsrc, ksrc, vsrc, gsrc = (getattr(cx, cfg[k]) for k in ("qsrc", "ksrc", "vsrc", "gsrc"))
    gate, dest = getattr(cx, cfg["gate"]), getattr(cx, cfg["dest"])
    mq_d, mk_d = getattr(cx, cfg["mask"] + "_mq"), getattr(cx, cfg["mask"] + "_mk")
    with kb.scope():
        mq = kb.sbuf("mq", [128, 2, 130], F32)
        mk = kb.sbuf("mk", [128, 2, 128], F32)
        ma = kb.sbuf("ma", [128, 2, 128], F32)
        ident = kb.sbuf("ident", [128, 128], BF16)
        onesdv = kb.sbuf("onesdv", [128, 128], F32)
        kb.dma("sp", mq[:], mq_d[:], reads=[mq_d], writes=[mq])
        kb.dma("sp", mk[:], mk_d[:], reads=[mk_d], writes=[mk])
        kb.dma("sp", ma[:], cx.m_ma[:], reads=[cx.m_ma], writes=[ma])
        mau = kb.sbuf("mau", [128, 2, 128], U32)
        kb.dma("sp", mau[:], cx.m_mau[:], reads=[cx.m_mau], writes=[mau])
        kb.dma("pool", ident[:], cx.m_ident[:], reads=[cx.m_ident], writes=[ident])
        kb.op("dve", lambda: nc.vector.memset(onesdv[:], 1.0 / dv), writes=[onesdv])
        vt = kb.sbuf("vt", [128, NT, dv], BF16)
        qT = kb.sbuf("qT", [128, T], BF16)
        gsil = kb.sbuf("gsil", [128, T], BF16)
        obr = kb.sbuf("obr", [128, T], BF16)
        gts = [kb.sbuf("gt%d" % i, [128, NT, dk], F32) for i in range(2)]
        kts = [kb.sbuf("kt%d" % i, [128, NT, dk], BF16) for i in range(2)]
        qt = [kb.sbuf("qt%d" % z, [128, T], BF16) for z in range(2)]
        atm = [kb.sbuf("atm%d" % z, [128, NT, 128], BF16) for z in range(2)]
        Sp = [kb.sbuf("Sp%d" % z, [128, 2 * NT, dv], BF16) for z in range(2)]
        for z in range(2):
            kb.op("pool", lambda z=z: nc.gpsimd.memset(atm[z][:], 0.0), writes=[atm[z]])
        Sst = [kb.sbuf("Sst%d" % i, [128, dv], F32) for i in range(2)]
        R = 3
        eq = [kb.sbuf("eq%d" % i, [128, 128], F32) for i in range(R)]
        ek = [kb.sbuf("ek%d" % i, [128, 128], F32) for i in range(R)]
        ekh = [kb.sbuf("ekh%d" % i, [128, 128], F32) for i in range(R)]
        ktl = [kb.sbuf("ktl%d" % i, [128, 128], BF16) for i in range(R)]
        khat = [kb.sbuf("khat%d" % i, [128, 256], BF16) for i in range(R)]
        ktr = [kb.sbuf("ktr%d" % i, [128, 128], F32) for i in range(R)]
        atr = [kb.sbuf("atr%d" % i, [128, 128], F32) for i in range(R)]
        usb = [kb.sbuf("usb%d" % i, [128, 256], F32) for i in range(R)]
        osb = [kb.sbuf("osb%d" % i, [128, 512], F32) for i in range(2)]
        cm = kb.sbuf("cm", [128, 2], F32)
        kb.op("dve", lambda: nc.vector.memset(cm[:], 0.0), writes=[cm])
        kb.op("dve", lambda: nc.vector.memset(cm[0:64, 0:1], 1.0), writes=[cm])
        kb.op("dve", lambda: nc.vector.memset(cm[64:128, 1:2], 1.0), writes=[cm])
        er2 = [kb.sbuf("er%d" % i, [128, 2], F32) for i in range(R)]
        ed2 = [kb.sbuf("ed%d" % i, [128, 2], F32) for i in range(R)]
        psA = [kb.psum("psA%d" % i, [128, 512]) for i in range(2)]
        psB = [kb.psum("psB%d" % i, [128, 512]) for i in range(2)]
        psA_ec = [view(p) for p in psA]
        import os
        if True:
            psK = [kb.psum("psK%d" % i, [128, 512]) for i in range(2)]
        else:
            psK = psA
        psA_kt = [view(p) for p in psK]
        psA_e2 = [view(p) for p in psK]
        psB_u = [view(p) for p in psB]
        psB_at = [view(p) for p in psB]
        psO = [kb.psum("psO%d" % i, [128, 512]) for i in range(1)] * 2
        psM = [kb.psum("psM%d" % i, [128, 512]) for i in range(1)] * 2
        sq = [kb.sbuf("sq%d" % i, [128, 512], F32) for i in range(2)]
        sd = [kb.sbuf("sd%d" % i, [128, 512], F32) for i in range(2)]
        on = [kb.sbuf("on%d" % i, [128, 512], F32) for i in range(2)]
        eps_t = kb.sbuf("eps_t", [128, 1], F32)
        kb.op("dve", lambda: nc.vector.memset(eps_t[:], 1e-6), writes=[eps_t])

        it = 0
        ghz = 0
        import os
        for h in (range(H) if heads is None else heads):
            vc0 = cfg["vcol0"] + h * dv
            kb.dma("sp", vt[:], vsrc[:, :, vc0:vc0 + dv], reads=[vsrc], writes=[vt])
            kb.dma("sp", qT[:dk, :], qsrc[h * dk:(h + 1) * dk, :], reads=[qsrc], writes=[qT])
            kb.dma("sp", gsil[:], gate[h * dv:(h + 1) * dv, :], reads=[gate], writes=[gsil])
            for z in range(2):
                gt = gts[ghz % 2]
                kt = kts[ghz % 2]
                ghz += 1
                gc0 = z * cfg["gz"] + h * dk
                kc0 = cfg["kcol0"] + z * cfg["kz"] + h * dk
                kb.dma("sp", gt[:], gsrc[:, :, gc0:gc0 + dk], reads=[gsrc], writes=[gt])
                if os.environ.get("KTMEMSET"):
                    kb.op("dve", lambda kt=kt: nc.vector.memset(kt[:], 0.0), writes=[kt])
                kb.dma("sp", kt[:], ksrc[:, :, kc0:kc0 + dk], reads=[ksrc], writes=[kt])
                order = [32, 33] + list(range(32)) if z == 0 else [33, 32] + list(range(31, -1, -1))
                import os
                DBG = os.environ.get("SCAN_DBG", "")
                if DBG == "loads":
                    continue
                if DBG.startswith("nt"):
                    order = order[:int(DBG[2:])]
                S = Sst[0]
                kb.op("dve", lambda S=S: nc.vector.memset(S[:], 0.0), writes=[S])
                si = 0
                def p1(ti, a, r):
                    ts = slice(ti * 128, (ti + 1) * 128)
                    pa, pb = psA[a], psB[a]
                    pk = psK[a]
                    kb.mm([lambda pa=pa, gt=gt: nc.tensor.matmul(pa[:dk, 0:130], lhsT=gt[:, ti, :], rhs=mq[:, z, :],
                                                                 start=True, stop=True)],
                          reads=[gt, mq], writes=[psA_ec[a]])
                    kb.mm([lambda pk=pk, kt=kt: nc.tensor.matmul(pk[:dk, 256:384], lhsT=(ident[:, :dk] if os.environ.get("A2ID") else kt[:, ti, :]), rhs=ident[:],
                                                                 start=True, stop=True)],
                          reads=[kt, ident], writes=[psA_kt[a]])
                    kb.mm([lambda pk=pk, gt=gt: nc.tensor.matmul(pk[:, 384:384 + dk], lhsT=mk[:, z, :], rhs=gt[:, ti, :],
                                                                 start=True, stop=True)],
                          reads=[gt, mk], writes=[psA_e2[a]])
                    kb.op("act", lambda pa=pa, o=eq[r]: nc.scalar.activation(out=o[:dk, :], in_=pa[:dk, 0:128], func=AF.Exp),
                          reads=[psA_ec[a]], writes=[eq[r]])
                    kb.op("act", lambda pa=pa, o=ek[r]: nc.scalar.activation(out=o[:dk, :], in_=pa[:dk, 0:128], func=AF.Exp, scale=-1.0),
                          reads=[psA_ec[a]], writes=[ek[r]])
                    kb.op("act", lambda pa=pa, o=er2[r]: nc.scalar.activation(out=o[:dk, :], in_=pa[:dk, 128:130], func=AF.Exp, scale=0.5),
                          reads=[psA_ec[a]], writes=[er2[r]])
                    kb.op("act", lambda pa=pa, o=ed2[r]: nc.scalar.activation(out=o[:dk, :], in_=pa[:dk, 128:130], func=AF.Exp),
                          reads=[psA_ec[a]], writes=[ed2[r]])
                    kb.op("act", lambda pk=pk, o=ekh[r]: nc.scalar.activation(out=o[:, :dk], in_=pk[:, 384:384 + dk], func=AF.Exp),
                          reads=[psA_e2[a]], writes=[ekh[r]])
                    qz = qt[z]
                    kb.op("dve", lambda qz=qz, e=eq[r]: nc.vector.scalar_tensor_tensor(
                        out=qz[:dk, ts], in0=qT[:dk, ts], scalar=cfg["qscale"], in1=e[:dk, :], op0=ALU.mult, op1=ALU.mult),
                        reads=[qT, eq[r]], writes=[], acc_writes=[qz])
                    kb.op("act", lambda pk=pk, o=ktr[r]: nc.scalar.copy(out=o[:dk, :], in_=pk[:dk, 256:384]),
                          reads=[psA_kt[a]], writes=[ktr[r]])
                    kb.op("dve", lambda e=ek[r], o=ktl[r], kr=ktr[r]: nc.vector.tensor_tensor(
                        out=o[:dk, :], in0=kr[:dk, :], in1=e[:dk, :], op=ALU.mult),
                        reads=[ktr[r], ek[r]], writes=[ktl[r]])
                    for c in range(2):
                        kb.op("dve", lambda kt=kt, e=ekh[r], o=khat[r], c=c: nc.vector.scalar_tensor_tensor(
                            out=o[:, c * 128:c * 128 + dk], in0=kt[:, ti, :], scalar=cm[:, c:c + 1], in1=e[:, :dk],
                            op0=ALU.mult, op1=ALU.mult),
                            reads=[kt, ekh[r], cm], writes=[], acc_writes=[khat[r]])
                    return None

                def p2(ti, a, r, S, si):
                    ts = slice(ti * 128, (ti + 1) * 128)
                    pa, pb = psA[a], psB[a]
                    qz = qt[z]
                    kb.mm([(lambda c=c, pb=pb, kh=khat[r]: nc.tensor.matmul(
                        pb[:dk, c * 128:(c + 1) * 128], lhsT=kh[:, c * 128:c * 128 + dk],
                        rhs=vt[:, ti, :], start=True, stop=True)) for c in range(2)],
                        reads=[khat[r], vt], writes=[psB_u[a]])
                    kb.mm([lambda pb=pb, k_=ktl[r], qz=qz: nc.tensor.matmul(
                        pb[:, 256:384], lhsT=k_[:dk, :], rhs=qz[:dk, ts], start=True, stop=True)],
                        reads=[ktl[r], qz], writes=[psB_at[a]])
                    az = atm[z]
                    kb.op("act", lambda pb=pb, o=atr[r]: nc.scalar.copy(out=o[:], in_=pb[:, 256:384]),
                          reads=[psB_at[a]], writes=[atr[r]])
                    kb.op("dve", lambda az=az, o=atr[r]: nc.vector.copy_predicated(
                        out=az[:, ti, :], mask=mau[:, z, :], data=o[:]),
                        reads=[atr[r], mau], writes=[], acc_writes=[az])
                    kb.op("act", lambda pb=pb, o=usb[r]: nc.scalar.copy(out=o[:dk, :], in_=pb[:dk, 0:256]),
                          reads=[psB_u[a]], writes=[usb[r]])
                    spz = Sp[z]
                    for c in ((0, 1) if z == 0 else (1, 0)):
                        ci = 2 * ti + c
                        kb.op("act", lambda S=S, spz=spz, ci=ci, e=er2[r], c=c: nc.scalar.activation(
                            out=spz[:dk, ci, :], in_=S[:dk, :], func=AF.Copy, scale=e[:dk, c:c + 1]),
                            reads=[S, er2[r]], writes=[], acc_writes=[spz])
                        S2 = Sst[(si + 1) % 2]
                        si += 1
                        kb.op("dve", lambda S=S, S2=S2, e=ed2[r], u=usb[r], c=c: nc.vector.scalar_tensor_tensor(
                            out=S2[:dk, :], in0=S[:dk, :], scalar=e[:dk, c:c + 1], in1=u[:dk, c * 128:(c + 1) * 128],
                            op0=ALU.mult, op1=ALU.add),
                            reads=[S, ed2[r], usb[r]], writes=[S2])
                        S = S2
                    return S, si

                sched = []
                for ti in order:
                    sched.append((ti, it % 2, it % R))
                    it += 1
                for idx in range(len(sched) + 1):
                    if idx < len(sched):
                        p1(*sched[idx])
                    if idx >= 1:
                        S, si = p2(*sched[idx - 1], S, si)
            groups = [list(range(g * 4, g * 4 + 4)) for g in range(8)] + [[32, 33]]
            if DBG:
                groups = []
            for gi, tiles in enumerate(groups):
                po = psO[gi % 2]
                pm = psM[gi % 2]
                nb = len(tiles) * 128
                t0 = tiles[0] * 128
                fns = []
                for k_, ti in enumerate(tiles):
                    cs = k_ * 128
                    for z in range(2):
                        fns.append(lambda po=po, ti=ti, cs=cs, z=z: nc.tensor.matmul(
                            po[:, cs:cs + 128], lhsT=vt[:, ti, :], rhs=atm[z][:, ti, :], start=(z == 0), stop=False))
                        for c in range(2):
                            fns.append(lambda po=po, ti=ti, cs=cs, z=z, c=c: nc.tensor.matmul(
                                po[:, cs + c * 64:cs + (c + 1) * 64], lhsT=Sp[z][:dk, 2 * ti + c, :],
                                rhs=qt[z][:dk, ti * 128 + c * 64:ti * 128 + (c + 1) * 64],
                                start=False, stop=(z == 1)))
                kb.mm(fns, reads=[vt, atm[0], atm[1], Sp[0], Sp[1], qt[0], qt[1]], writes=[po])
                s_, d_, o_ = sq[gi % 2], sd[gi % 2], on[gi % 2]
                kb.op("act", lambda po=po, s_=s_: nc.scalar.activation(out=s_[:, :nb], in_=po[:, :nb], func=AF.Square),
                      reads=[po], writes=[s_])
                kb.mm([lambda pm=pm, s_=s_: nc.tensor.matmul(pm[:, :nb], lhsT=onesdv[:], rhs=s_[:, :nb], start=True, stop=True)],
                      reads=[onesdv, s_], writes=[pm])
                kb.op("act", lambda pm=pm, d_=d_: nc.scalar.activation(out=d_[:, :nb], in_=pm[:, :nb], func=AF.Sqrt, bias=eps_t[:]),
                      reads=[pm, eps_t], writes=[d_])
                kb.op("dve", lambda d_=d_: nc.vector.reciprocal(out=d_[:, :nb], in_=d_[:, :nb]), reads=[d_], writes=[d_])
                ob_ = osb[gi % 2]
                kb.op("act", lambda po=po, ob_=ob_: nc.scalar.copy(out=ob_[:, :nb], in_=po[:, :nb]), reads=[po], writes=[ob_])
                kb.op("dve", lambda ob_=ob_, d_=d_, o_=o_: nc.vector.tensor_tensor(
                    out=o_[:, :nb], in0=ob_[:, :nb], in1=d_[:, :nb], op=ALU.mult), reads=[ob_, d_], writes=[o_])
                kb.op("pool", lambda o_=o_: nc.gpsimd.tensor_tensor(
                    out=obr[:, t0:t0 + nb], in0=o_[:, :nb], in1=gsil[:, t0:t0 + nb], op=ALU.mult),
                    reads=[o_, gsil], writes=[], acc_writes=[obr])
            kb.dma("sp", dest[h * dv:(h + 1) * dv, :], obr[:], reads=[obr], writes=[], acc_writes=[dest])


def scan_masks():
    j = np.arange(128)[:, None]
    i = np.arange(128)[None, :]
    same = (j // 64 == i // 64).astype(np.float32)
    out = {}
    for name, gs in (("mg", -1.0 / 16.0), ("mh", 1.0)):
        mq = np.zeros((128, 2, 130), np.float32)
        mk = np.zeros((128, 2, 128), np.float32)
        mq[:, 0, :128] = gs * same * ((j <= i).astype(np.float32) - 0.5)
        mq[:, 1, :128] = gs * same * ((j >= i).astype(np.float32) - 0.5)
        for z in range(2):
            for c in range(2):
                mq[:, z, 128 + c] = gs * (np.arange(128) // 64 == c)
        mk[:, 0, :] = gs * same * (j > i)
        mk[:, 1, :] = gs * same * (j < i)
        out[name + "_mq"] = mq
        out[name + "_mk"] = mk
    ma = np.zeros((128, 2, 128), np.float32)
    ma[:, 0, :] = same * (j <= i)
    ma[:, 1, :] = same * (j >= i)
    out["m_ma"] = ma
    out["m_mau"] = ma.astype(np.uint32)
    out["m_ident"] = np.eye(128, dtype=np.float32)
    return out


def stage_wa(cx, kb, nc, l, groups=None):
    with kb.scope():
        wm = kb.sbuf("wm", [128, 2, 128], BF16)
        kb.dma("pool", wm[:], cx.m_wa[:], reads=[cx.m_wa], writes=[wm])
        ones64 = kb.sbuf("ones64", [128, 64], BF16)
        kb.op("dve", lambda: nc.vector.memset(ones64[:], 1.0), writes=[ones64])
        sk = kb.sbuf("sk", [64, 8], F32)
        kb.dma("sp", sk[:], cx.wa_sink[l].partition_broadcast(64), reads=[cx.wa_sink], writes=[sk])
        kb.op("act", lambda: nc.scalar.activation(out=sk[:], in_=sk[:], func=AF.Exp), reads=[sk], writes=[sk])
        q4 = kb.sbuf("q4", [64, 4, T], BF16)
        kT = kb.sbuf("kTc", [64, T], BF16)
        vt = kb.sbuf("vtc", [128, NT, 64], BF16)
        ost = kb.sbuf("ost", [64, 4, T], BF16)
        pS = [kb.psum("pS%d" % i, [128, 512]) for i in range(4)]
        pO = [kb.psum("pO%d" % i, [128, 512]) for i in range(2)]
        pD = [kb.psum("pD%d" % i, [128, 512]) for i in range(2)]
        R = 10
        pt = [kb.sbuf("pt%d" % i, [128, 4, 128], BF16) for i in range(R)]
        ptm = [kb.sbuf("ptm%d" % i, [128, 4, 128], BF16) for i in range(R)]
        osb = [kb.sbuf("osbw%d" % i, [64, 4, 128], F32) for i in range(2)]
        dsb = [kb.sbuf("dsbw%d" % i, [64, 4, 128], F32) for i in range(2)]
        it = 0
        for g in (range(2) if groups is None else groups):
            kb.dma("sp", q4[:], cx.QC[g * 256:(g + 1) * 256, :].rearrange("(hh d) t -> d hh t", d=64),
                   reads=[cx.QC], writes=[q4])
            kb.dma("sp", kT[:], cx.KC[g * 64:(g + 1) * 64, :], reads=[cx.KC], writes=[kT])
            kb.dma("sp", vt[:], cx.VC_t[:, :, g * 64:(g + 1) * 64], reads=[cx.VC_t], writes=[vt])
            def p1(qb):
                nonlocal it
                if qb < 32:
                    keys = []
                    if qb > 0:
                        keys.append((qb - 1, 0))
                    keys.append((qb, None))
                    if qb < 31:
                        keys.append((qb + 1, 1))
                    keys += [(32, None), (33, None)]
                else:
                    keys = [(32, None), (33, None)]
                qs = slice(qb * 128, (qb + 1) * 128)
                st_ = []
                for ki, (kt_i, mi) in enumerate(keys):
                    ps = pS[it % 4]
                    p_ = pt[it % R]
                    pm_ = ptm[it % R]
                    it += 1
                    kb.mm([lambda ps=ps, kt_i=kt_i: nc.tensor.matmul(
                        ps[:, :], lhsT=kT[:, kt_i * 128:(kt_i + 1) * 128], rhs=q4[:, :, qs], start=True, stop=True)],
                        reads=[kT, q4], writes=[ps])
                    kb.op("act", lambda ps=ps, p_=p_: nc.scalar.activation(
                        out=p_[:].rearrange("p h q -> p (h q)"), in_=ps[:, :], func=AF.Exp, scale=0.125),
                        reads=[ps], writes=[p_])
                    src = p_
                    if mi is not None:
                        eng = "dve" if it % 2 == 0 else "pool"
                        h_ = nc.vector if eng == "dve" else nc.gpsimd
                        kb.op(eng, lambda h_=h_, p_=p_, pm_=pm_, mi=mi: h_.tensor_tensor(
                            out=pm_[:], in0=p_[:], in1=wm[:, mi, :].unsqueeze(1).broadcast_to([128, 4, 128]), op=ALU.mult),
                            reads=[p_, wm], writes=[pm_])
                        src = pm_
                    st_.append((kt_i, src))
                return (qb, st_)

            def p2(state):
                qb, st_ = state
                qs = slice(qb * 128, (qb + 1) * 128)
                a = qb % 2
                po, pd = pO[a], pD[a]
                nk = len(st_)
                for ki, (kt_i, src) in enumerate(st_):
                    kb.mm([lambda po=po, src=src, kt_i=kt_i, ki=ki: nc.tensor.matmul(
                        po[:64, :], lhsT=vt[:, kt_i, :], rhs=src[:].rearrange("p h q -> p (h q)"),
                        start=(ki == 0), stop=(ki == nk - 1)),
                        lambda pd=pd, src=src, ki=ki: nc.tensor.matmul(
                        pd[:64, :], lhsT=ones64[:], rhs=src[:].rearrange("p h q -> p (h q)"),
                        start=(ki == 0), stop=(ki == nk - 1))],
                        reads=[vt, src, ones64], writes=[po, pd])
                o_, d_ = osb[a], dsb[a]
                kb.op("act", lambda po=po, o_=o_: nc.scalar.copy(out=o_[:].rearrange("p h q -> p (h q)"), in_=po[:64, :]),
                      reads=[po], writes=[o_])
                kb.op("act", lambda pd=pd, d_=d_: nc.scalar.copy(out=d_[:].rearrange("p h q -> p (h q)"), in_=pd[:64, :]),
                      reads=[pd], writes=[d_])
                kb.op("dve", lambda d_=d_: nc.vector.tensor_tensor(
                    out=d_[:], in0=d_[:], in1=sk[:, g * 4:(g + 1) * 4].unsqueeze(2).broadcast_to([64, 4, 128]), op=ALU.add),
                    reads=[d_, sk], writes=[d_])
                kb.op("dve", lambda d_=d_: nc.vector.reciprocal(out=d_[:], in_=d_[:]), reads=[d_], writes=[d_])
                kb.op("pool", lambda o_=o_, d_=d_: nc.gpsimd.tensor_tensor(
                    out=ost[:, :, qs], in0=o_[:], in1=d_[:], op=ALU.mult),
                    reads=[o_, d_], writes=[], acc_writes=[ost])

            prev = None
            for qb in range(NT):
                cur = p1(qb)
                if prev is not None:
                    p2(prev)
                prev = cur
            p2(prev)
            kb.dma("sp", cx.BR_C[g * 256:(g + 1) * 256, :].rearrange("(hh d) t -> d hh t", d=64), ost[:],
                   reads=[ost], writes=[], acc_writes=[cx.BR_C])


def wa_masks():
    kk = np.arange(128)[:, None]
    qq = np.arange(128)[None, :]
    m = np.zeros((128, 2, 128), np.float32)
    m[:, 0, :] = (kk >= qq)
    m[:, 1, :] = (kk <= qq)
    return {"m_wa": m}


def na_patterns():
    W = 64
    tok = np.arange(128)
    lr, lc = tok // W, tok % W
    pats = []
    keys = []
    for m in range(32):
        qr = 2 * m + lr[None, :]
        qc = lc[None, :]
        rs = np.clip(qr - 4, 0, 56)
        cs = np.clip(qc - 8, 0, 48)
        lst = []
        for kt_i in range(32):
            kr = 2 * kt_i + lr[:, None]
            kc = lc[:, None]
            valid = (kr >= rs) & (kr < rs + 8) & (kc >= cs) & (kc < cs + 16)
            if not valid.any():
                continue
            ri = np.clip(kr - qr + 7, 0, 14)
            ci = np.clip(kc - qc + 15, 0, 30)
            if 2 <= m <= 29:
                pid = kt_i - m + 2
                if m == 2:
                    pats.append((valid, ri, ci))
            else:
                pid = len(pats)
                pats.append((valid, ri, ci))
            lst.append((kt_i, pid))
        keys.append(lst)
    return keys, pats


def na_consts():
    keys, pats = na_patterns()
    n0 = len(keys[0]) + len(keys[1])
    order = list(range(n0, n0 + 5)) + list(range(0, n0)) + list(range(n0 + 5, len(pats)))
    remap = {old: new for new, old in enumerate(order)}
    pats2 = [pats[o] for o in order]
    keys2 = []
    for m, lst in enumerate(keys):
        if 2 <= m <= 29:
            keys2.append(lst)
        else:
            keys2.append([(k, remap[p]) for (k, p) in lst])
    valid = np.stack([p[0] for p in pats2], 1).astype(np.float32)
    ri = np.stack([p[1] for p in pats2], 1)
    ci = np.stack([p[2] for p in pats2], 1)
    return keys2, valid, ri, ci


def stage_na(cx, kb, nc, l, heads=None):
    keys_tab, valid_np, _, _ = na_consts()
    npat = valid_np.shape[1]
    with kb.scope():
        vmask = kb.sbuf("vmask", [128, npat, 128], F32)
        kb.dma("sp", vmask[:], cx.m_navalid[:], reads=[cx.m_navalid], writes=[vmask])
        ones64 = kb.sbuf("ones64n", [128, 64], BF16)
        kb.op("dve", lambda: nc.vector.memset(ones64[:], 1.0), writes=[ones64])
        qT = kb.sbuf("qTn", [64, T], BF16)
        kT = kb.sbuf("kTn", [64, T], BF16)
        vt = kb.sbuf("vtn", [128, NT, 64], BF16)
        ost = kb.sbuf("ostn", [64, T], BF16)
        bias = kb.sbuf("biasn", [128, npat, 128], F32)
        EB = kb.sbuf("EBn", [128, npat, 128], BF16)
        pS = [kb.psum("pSn%d" % i, [128, 512]) for i in range(4)]
        pO = [kb.psum("pOn%d" % i, [128, 512]) for i in range(2)]
        pD = [kb.psum("pDn%d" % i, [128, 512]) for i in range(2)]
        R = 6
        pt = [kb.sbuf("ptn%d" % i, [128, 4, 128], BF16) for i in range(R)]
        osb = [kb.sbuf("osbn%d" % i, [64, 256], F32) for i in range(2)]
        it = 0
        for h in (range(8) if heads is None else heads):
            kb.dma("sp", bias[:], cx.na_bias[l, h], reads=[cx.na_bias], writes=[bias])
            kb.op("act", lambda: nc.scalar.activation(out=bias[:], in_=bias[:], func=AF.Exp), reads=[bias], writes=[bias])
            kb.op("pool", lambda: nc.gpsimd.tensor_tensor(out=EB[:], in0=bias[:], in1=vmask[:], op=ALU.mult),
                  reads=[bias, vmask], writes=[EB])
            kb.dma("sp", qT[:], cx.QB[h * 64:(h + 1) * 64, :], reads=[cx.QB], writes=[qT])
            kb.dma("sp", kT[:], cx.KB_[h * 64:(h + 1) * 64, :], reads=[cx.KB_], writes=[kT])
            kb.dma("sp", vt[:], cx.VB_t[:, :, h * 64:(h + 1) * 64], reads=[cx.VB_t], writes=[vt])
            def p1(qb):
                nonlocal it
                if qb < 32:
                    keys = list(keys_tab[qb]) + [(32, None), (33, None)]
                else:
                    keys = [(32, None), (33, None)]
                qs = slice(qb * 128, (qb + 1) * 128)
                grp = [keys[i:i + 4] for i in range(0, len(keys), 4)]
                st_ = []
                for gk in grp:
                    ps = pS[it % 4]
                    p_ = pt[it % R]
                    it += 1
                    ng = len(gk)
                    kb.mm([(lambda ps=ps, j=j, kt_i=kt_i: nc.tensor.matmul(
                        ps[:, j * 128:(j + 1) * 128], lhsT=kT[:, kt_i * 128:(kt_i + 1) * 128], rhs=qT[:, qs],
                        start=True, stop=True)) for j, (kt_i, _) in enumerate(gk)],
                        reads=[kT, qT], writes=[ps])
                    kb.op("act", lambda ps=ps, p_=p_, ng=ng: nc.scalar.activation(
                        out=p_[:, :ng, :].rearrange("p h q -> p (h q)"), in_=ps[:, :ng * 128], func=AF.Exp, scale=0.125),
                        reads=[ps], writes=[p_])
                    nloc = sum(1 for (_, pid) in gk if pid is not None)
                    if nloc:
                        pid0 = gk[0][1]
                        eng = "dve" if it % 2 == 0 else "pool"
                        h_ = nc.vector if eng == "dve" else nc.gpsimd
                        kb.op(eng, lambda h_=h_, p_=p_, pid0=pid0, nloc=nloc: h_.tensor_tensor(
                            out=p_[:, :nloc, :], in0=p_[:, :nloc, :], in1=EB[:, pid0:pid0 + nloc, :], op=ALU.mult),
                            reads=[p_, EB], writes=[p_])
                    st_.append((gk, p_))
                return (qb, st_, len(keys))

            def p2(state):
                qb, st_, nk = state
                qs = slice(qb * 128, (qb + 1) * 128)
                a = qb % 2
                po = pO[a]
                pd = pD[a]
                done = 0
                for gk, p_ in st_:
                    fns = []
                    for j, (kt_i, _) in enumerate(gk):
                        first = (done == 0)
                        last = (done == nk - 1)
                        fns.append(lambda po=po, p_=p_, j=j, kt_i=kt_i, first=first, last=last: nc.tensor.matmul(
                            po[:64, 0:128], lhsT=vt[:, kt_i, :], rhs=p_[:, j, :], start=first, stop=last))
                        fns.append(lambda pd=pd, p_=p_, j=j, first=first, last=last: nc.tensor.matmul(
                            pd[:64, 0:128], lhsT=ones64[:], rhs=p_[:, j, :], start=first, stop=last))
                        done += 1
                    kb.mm(fns, reads=[vt, p_, ones64], writes=[po, pd])
                o_ = osb[a]
                kb.op("act", lambda po=po, o_=o_: nc.scalar.copy(out=o_[:, 0:128], in_=po[:64, 0:128]), reads=[po], writes=[o_])
                kb.op("act", lambda pd=pd, o_=o_: nc.scalar.copy(out=o_[:, 128:256], in_=pd[:64, 0:128]), reads=[pd, o_], writes=[o_])
                kb.op("dve", lambda o_=o_: nc.vector.reciprocal(out=o_[:, 128:256], in_=o_[:, 128:256]), reads=[o_], writes=[o_])
                kb.op("pool", lambda o_=o_: nc.gpsimd.tensor_tensor(
                    out=ost[:, qs], in0=o_[:, 0:128], in1=o_[:, 128:256], op=ALU.mult),
                    reads=[o_], writes=[], acc_writes=[ost])

            prev = None
            for qb in range(NT):
                cur = p1(qb)
                if prev is not None:
                    p2(prev)
                prev = cur
            p2(prev)
            kb.dma("sp", cx.BR_B[h * 64:(h + 1) * 64, :], ost[:], reads=[ost], writes=[], acc_writes=[cx.BR_B])

LN_EPS = 1e-5
MB = 512
MBLKS = [(i * MB, min(MB, T - i * MB)) for i in range((T + MB - 1) // MB)]


def ln_setup(cx, kb, nc):
    ones = kb.sbuf("ln_ones", [128, 128], F32)
    kb.op("dve", lambda: nc.vector.memset(ones[:], 1.0 / D), writes=[ones])
    eps = kb.sbuf("ln_eps", [128, 1], F32)
    kb.op("dve", lambda: nc.vector.memset(eps[:], LN_EPS), writes=[eps])
    return ones, eps


def ln_block(kb, nc, r, nb, gam, bet, out, ones, eps, scr):
    rsq, ps1, ps2, mean, var = scr
    kb.op("act", lambda: nc.scalar.activation(out=rsq[:, :, :nb], in_=r[:, :, :nb], func=AF.Square), reads=[r], writes=[rsq])
    kb.mm([(lambda oc=oc: nc.tensor.matmul(ps1[:, :nb], lhsT=ones[:], rhs=r[:, oc, :nb], start=(oc == 0), stop=(oc == 7)))
           for oc in range(8)], reads=[ones, r], writes=[ps1])
    kb.mm([(lambda oc=oc: nc.tensor.matmul(ps2[:, :nb], lhsT=ones[:], rhs=rsq[:, oc, :nb], start=(oc == 0), stop=(oc == 7)))
           for oc in range(8)], reads=[ones, rsq], writes=[ps2])
    kb.op("act", lambda: nc.scalar.copy(out=mean[:, :nb], in_=ps1[:, :nb]), reads=[ps1], writes=[mean])
    kb.op("act", lambda: nc.scalar.copy(out=var[:, :nb], in_=ps2[:, :nb]), reads=[ps2], writes=[var])
    m2 = rsq
    kb.op("dve", lambda: nc.vector.tensor_tensor(out=m2[:, 0, :nb], in0=mean[:, :nb], in1=mean[:, :nb], op=ALU.mult),
          reads=[mean, rsq], writes=[rsq])
    kb.op("dve", lambda: nc.vector.tensor_tensor(out=var[:, :nb], in0=var[:, :nb], in1=m2[:, 0, :nb], op=ALU.subtract),
          reads=[var, rsq], writes=[var])
    kb.op("act", lambda: nc.scalar.activation(out=var[:, :nb], in_=var[:, :nb], func=AF.Sqrt, bias=eps[:]),
          reads=[var, eps], writes=[var])
    kb.op("dve", lambda: nc.vector.reciprocal(out=var[:, :nb], in_=var[:, :nb]), reads=[var], writes=[var])
    kb.op("dve", lambda: nc.vector.tensor_tensor(
        out=r[:, :, :nb], in0=r[:, :, :nb], in1=mean[:, :nb].unsqueeze(1).broadcast_to([128, 8, nb]), op=ALU.subtract),
        reads=[r, mean], writes=[r])
    kb.op("pool", lambda: nc.gpsimd.tensor_tensor(
        out=r[:, :, :nb], in0=r[:, :, :nb], in1=var[:, :nb].unsqueeze(1).broadcast_to([128, 8, nb]), op=ALU.mult),
        reads=[r, var], writes=[r])
    for oc in range(8):
        eng = "dve" if oc % 2 == 0 else "pool"
        h_ = nc.vector if eng == "dve" else nc.gpsimd
        kb.op(eng, lambda h_=h_, oc=oc: h_.tensor_scalar(
            out=out[:, oc, :nb], in0=r[:, oc, :nb], scalar1=gam[:, oc:oc + 1], scalar2=bet[:, oc:oc + 1],
            op0=ALU.mult, op1=ALU.add), reads=[r, gam, bet], writes=[out])


def load_ln_params(cx, kb, nc, l, which):
    gam = kb.sbuf("ln_g", [128, 8], F32)
    bet = kb.sbuf("ln_b", [128, 8], F32)
    kb.dma("sp", gam[:], cx.ln_gT[l, which], reads=[cx.ln_gT], writes=[gam])
    kb.dma("sp", bet[:], cx.ln_bT[l, which], reads=[cx.ln_bT], writes=[bet])
    return gam, bet


def stage_merge(cx, kb, nc, l, xsrc, modv):
    with kb.scope():
        ones, eps = ln_setup(cx, kb, nc)
        gam, bet = load_ln_params(cx, kb, nc, l, 0)
        wbr = kb.sbuf("wbr", [128, 4, 4, 1024], BF16)
        wout = kb.sbuf("wout", [128, 8, 1024], BF16)
        for i in range(4):
            kb.dma("pool", wbr[:, i], cx.w_branch[l, i].rearrange("(kc p) c -> p kc c", p=128), reads=[cx.w_branch], writes=[wbr])
        kb.dma("pool", wout[:], cx.w_out[l].rearrange("(kc p) c -> p kc c", p=128), reads=[cx.w_out], writes=[wout])
        wr = kb.sbuf("wr", [128, 8, 36], F32)
        kb.dma("sp", wr[:, :, 0:4], cx.moe_w_group[l].rearrange("(kc p) c -> p kc c", p=128), reads=[cx.moe_w_group], writes=[wr])
        kb.dma("sp", wr[:, :, 4:36], cx.moe_w_expert[l].rearrange("(kc p) c -> p kc c", p=128), reads=[cx.moe_w_expert], writes=[wr])
        rb = kb.sbuf("rb", [128, 36], F32)
        kb.dma("sp", rb[:, 0:4], cx.moe_b_group[l].partition_broadcast(128), reads=[cx.moe_b_group], writes=[rb])
        kb.dma("sp", rb[:, 4:36], cx.moe_b_expert[l].partition_broadcast(128), reads=[cx.moe_b_expert], writes=[rb])
        onep4 = kb.sbuf("onep4", [128, 8, 2], F32)
        kb.op("dve", lambda: nc.vector.tensor_scalar(out=onep4[:], in0=modv[:, 32:40, :], scalar1=1.0, scalar2=None, op0=ALU.add),
              reads=[modv], writes=[onep4])
        brt = [kb.sbuf("brt%d" % i, [128, 4, 4, MB], BF16) for i in range(1)]
        gtt = kb.sbuf("gtt", [128, 32, MB], BF16)
        xb = kb.sbuf("xbm", [128, 8, MB], F32)
        mrg = kb.sbuf("mrg", [128, 8, MB], BF16)
        r = kb.sbuf("rm", [128, 8, MB], F32)
        hx2b = kb.sbuf("hx2b", [128, 8, MB], BF16)
        rsq = kb.sbuf("rsqm", [128, 8, MB], F32)
        x1 = r
        hx2f = rsq
        mean = kb.sbuf("meanm", [128, MB], F32)
        var = kb.sbuf("varm", [128, MB], F32)
        csb = [kb.sbuf("csb%d" % i, [128, MB], F32) for i in range(4)]
        acc = kb.sbuf("accm", [128, MB], F32)
        mixs = kb.sbuf("mixs", [128, MB], F32)
        pbr = [kb.psum("pbr%d" % i, [128, MB]) for i in range(4)]
        pmx = kb.psum("pmx", [128, MB])
        ps1 = kb.psum("ps1m", [128, MB])
        ps2 = kb.psum("ps2m", [128, MB])
        prt = kb.psum("prt", [128, MB])
        brs = [cx.BR_A, cx.BR_B, cx.BR_C, cx.BR_D]
        lgt = kb.sbuf("lgt", [128, 36], F32)
        gmx = kb.sbuf("gmx", [128, 1], F32)
        ngm = kb.sbuf("ngm", [128, 1], F32)
        ge = kb.sbuf("ge", [128, 4], F32)
        gs = kb.sbuf("gsr", [128, 1], F32)
        oh = kb.sbuf("oh", [128, 4], F32)
        lem = kb.sbuf("lem", [128, 4, 8], F32)
        mx8 = kb.sbuf("mx8", [128, 8], F32)
        dd = kb.sbuf("dd", [128, 1], F32)
        w1 = kb.sbuf("w1", [128, 1], F32)
        w2 = kb.sbuf("w2", [128, 1], F32)
        tt1 = kb.sbuf("tt1", [128, 32], F32)
        tt2 = kb.sbuf("tt2", [128, 32], F32)
        wgt = kb.sbuf("wgt", [128, 32], F32)
        for bi, (t0, nb) in enumerate(MBLKS):
            col = blkcol(t0)
            b_ = brt[0]
            for i in range(4):
                kb.dma("sp", b_[:, i, :, :nb], brs[i][:, t0:t0 + nb].rearrange("(kc p) t -> p kc t", p=128),
                       reads=[brs[i]], writes=[b_])
            kb.dma("sp", gtt[:, :, :nb], cx.GATE[:, t0:t0 + nb].rearrange("(j p) t -> p j t", p=128), reads=[cx.GATE], writes=[gtt])
            kb.dma("sp", xb[:, :, :nb], xsrc[:, t0:t0 + nb].rearrange("(kc p) t -> p kc t", p=128), reads=[xsrc], writes=[xb])
            for oc in range(8):
                for i in range(4):
                    kb.mm([(lambda i=i, kc=kc, oc=oc: nc.tensor.matmul(
                        pbr[i][:, :nb], lhsT=wbr[:, i, kc, oc * 128:(oc + 1) * 128], rhs=b_[:, i, kc, :nb],
                        start=(kc == 0), stop=(kc == 3))) for kc in range(4)], reads=[wbr, b_], writes=[pbr[i]])
                    kb.op("act", lambda i=i: nc.scalar.copy(out=csb[i][:, :nb], in_=pbr[i][:, :nb]), reads=[pbr[i]], writes=[csb[i]])
                    eng = "dve" if i % 2 == 0 else "pool"
                    h_ = nc.vector if eng == "dve" else nc.gpsimd
                    kb.op(eng, lambda h_=h_, i=i, oc=oc: h_.tensor_tensor(
                        out=csb[i][:, :nb], in0=csb[i][:, :nb], in1=gtt[:, i * 8 + oc, :nb], op=ALU.mult),
                        reads=[csb[i], gtt], writes=[csb[i]])
                kb.op("dve", lambda: nc.vector.tensor_tensor(out=acc[:, :nb], in0=csb[0][:, :nb], in1=csb[1][:, :nb], op=ALU.add),
                      reads=[csb[0], csb[1]], writes=[acc])
                kb.op("pool", lambda: nc.gpsimd.tensor_tensor(out=csb[2][:, :nb], in0=csb[2][:, :nb], in1=csb[3][:, :nb], op=ALU.add),
                      reads=[csb[2], csb[3]], writes=[csb[2]])
                kb.op("dve", lambda oc=oc: nc.vector.tensor_tensor(out=mrg[:, oc, :nb], in0=acc[:, :nb], in1=csb[2][:, :nb], op=ALU.add),
                      reads=[acc, csb[2]], writes=[mrg])
            for oc in range(8):
                kb.mm([(lambda kc=kc, oc=oc: nc.tensor.matmul(
                    pmx[:, :nb], lhsT=wout[:, kc, oc * 128:(oc + 1) * 128], rhs=mrg[:, kc, :nb],
                    start=(kc == 0), stop=(kc == 7))) for kc in range(8)], reads=[wout, mrg], writes=[pmx])
                kb.op("act", lambda: nc.scalar.copy(out=mixs[:, :nb], in_=pmx[:, :nb]), reads=[pmx], writes=[mixs])
                kb.op("pool", lambda oc=oc: nc.gpsimd.tensor_scalar(
                    out=mixs[:, :nb], in0=mixs[:, :nb], scalar1=modv[:, 16 + oc, col:col + 1], scalar2=None, op0=ALU.mult),
                    reads=[mixs, modv], writes=[mixs])
                kb.op("dve", lambda oc=oc: nc.vector.scalar_tensor_tensor(
                    out=r[:, oc, :nb], in0=xb[:, oc, :nb], scalar=ALPHA, in1=mixs[:, :nb], op0=ALU.mult, op1=ALU.add),
                    reads=[xb, mixs], writes=[r])
            ln_block(kb, nc, r, nb, gam, bet, x1, ones, eps, (rsq, ps1, ps2, mean, var))
            kb.dma("sp", cx.XT[:, t0:t0 + nb].rearrange("(kc p) t -> p kc t", p=128), x1[:, :, :nb],
                   reads=[x1], writes=[], acc_writes=[cx.XT])
            for oc in range(8):
                eng = "dve" if oc % 2 == 0 else "pool"
                h_ = nc.vector if eng == "dve" else nc.gpsimd
                kb.op(eng, lambda h_=h_, oc=oc: h_.tensor_scalar(
                    out=hx2f[:, oc, :nb], in0=x1[:, oc, :nb], scalar1=onep4[:, oc, col:col + 1],
                    scalar2=modv[:, 24 + oc, col:col + 1], op0=ALU.mult, op1=ALU.add),
                    reads=[x1, onep4, modv], writes=[hx2f])
            kb.op("act", lambda: nc.scalar.copy(out=hx2b[:, :, :nb], in_=hx2f[:, :, :nb]), reads=[hx2f], writes=[hx2b])
            kb.dma("sp", cx.HX2[:, t0:t0 + nb].rearrange("(kc p) t -> p kc t", p=128), hx2b[:, :, :nb],
                   reads=[hx2b], writes=[], acc_writes=[cx.HX2])
            for tj in range(nb // 128):
                ti = t0 // 128 + tj
                tsl = slice(tj * 128, (tj + 1) * 128)
                kb.mm([(lambda kc=kc: nc.tensor.matmul(prt[:, 0:36], lhsT=hx2f[:, kc, tsl], rhs=wr[:, kc, :],
                                                       start=(kc == 0), stop=(kc == 7))) for kc in range(8)],
                      reads=[hx2f, wr], writes=[prt])
                kb.op("act", lambda: nc.scalar.copy(out=lgt[:], in_=prt[:, 0:36]), reads=[prt], writes=[lgt])
                kb.op("dve", lambda: nc.vector.tensor_tensor(out=lgt[:], in0=lgt[:], in1=rb[:], op=ALU.add), reads=[lgt, rb], writes=[lgt])
                kb.op("dve", lambda: nc.vector.tensor_reduce(out=gmx[:], in_=lgt[:, 0:4], axis=AX.X, op=ALU.max), reads=[lgt], writes=[gmx])
                kb.op("dve", lambda: nc.vector.tensor_scalar(out=ngm[:], in0=gmx[:], scalar1=-1.0, scalar2=None, op0=ALU.mult), reads=[gmx], writes=[ngm])
                kb.op("act", lambda: nc.scalar.activation(out=ge[:], in_=lgt[:, 0:4], func=AF.Exp, bias=ngm[:]), reads=[lgt, ngm], writes=[ge])
                kb.op("dve", lambda: nc.vector.tensor_reduce(out=gs[:], in_=ge[:], axis=AX.X, op=ALU.add), reads=[ge], writes=[gs])
                kb.op("dve", lambda: nc.vector.reciprocal(out=gs[:], in_=gs[:]), reads=[gs], writes=[gs])
                kb.op("dve", lambda: nc.vector.tensor_scalar(out=oh[:], in0=lgt[:, 0:4], scalar1=gmx[:], scalar2=None, op0=ALU.is_equal), reads=[lgt, gmx], writes=[oh])
                kb.op("dve", lambda: nc.vector.tensor_scalar(out=oh[:], in0=oh[:], scalar1=-1.0, scalar2=1e30, op0=ALU.add, op1=ALU.mult), reads=[oh], writes=[oh])
                kb.op("dve", lambda: nc.vector.tensor_tensor(
                    out=lem[:], in0=lgt[:, 4:36].rearrange("p (g e) -> p g e", e=8), in1=oh[:].unsqueeze(2).broadcast_to([128, 4, 8]), op=ALU.add),
                    reads=[lgt, oh], writes=[lem])
                lemf = lem[:].rearrange("p g e -> p (g e)")
                kb.op("dve", lambda: nc.vector.max(out=mx8[:], in_=lem[:].rearrange("p g e -> p (g e)")), reads=[lem], writes=[mx8])
                kb.op("dve", lambda: nc.vector.tensor_tensor(out=dd[:], in0=mx8[:, 1:2], in1=mx8[:, 0:1], op=ALU.subtract), reads=[mx8], writes=[dd])
                kb.op("act", lambda: nc.scalar.activation(out=dd[:], in_=dd[:], func=AF.Exp), reads=[dd], writes=[dd])
                kb.op("dve", lambda: nc.vector.tensor_scalar(out=w1[:], in0=dd[:], scalar1=1.0, scalar2=None, op0=ALU.add), reads=[dd], writes=[w1])
                kb.op("dve", lambda: nc.vector.reciprocal(out=w1[:], in_=w1[:]), reads=[w1], writes=[w1])
                kb.op("dve", lambda: nc.vector.tensor_tensor(out=w2[:], in0=dd[:], in1=w1[:], op=ALU.mult), reads=[dd, w1], writes=[w2])
                kb.op("dve", lambda: nc.vector.tensor_scalar(
                    out=tt1[:], in0=lem[:].rearrange("p g e -> p (g e)"), scalar1=mx8[:, 0:1], scalar2=w1[:], op0=ALU.is_equal, op1=ALU.mult),
                    reads=[lem, mx8, w1], writes=[tt1])
                kb.op("dve", lambda: nc.vector.tensor_scalar(
                    out=tt2[:], in0=lem[:].rearrange("p g e -> p (g e)"), scalar1=mx8[:, 1:2], scalar2=w2[:], op0=ALU.is_equal, op1=ALU.mult),
                    reads=[lem, mx8, w2], writes=[tt2])
                kb.op("dve", lambda: nc.vector.tensor_tensor(out=tt1[:], in0=tt1[:], in1=tt2[:], op=ALU.add), reads=[tt1, tt2], writes=[tt1])
                kb.op("dve", lambda: nc.vector.tensor_scalar(out=wgt[:], in0=tt1[:], scalar1=gs[:], scalar2=None, op0=ALU.mult), reads=[tt1, gs], writes=[wgt])
                kb.dma("sp", cx.WG_t[:, ti, :], wgt[:], reads=[wgt], writes=[], acc_writes=[cx.WG_t])


def stage_moe(cx, kb, nc, l, modv, yout=None):
    with kb.scope():
        ones, eps = ln_setup(cx, kb, nc)
        gam, bet = load_ln_params(cx, kb, nc, l, 1)
        wgT = kb.sbuf("wgT", [32, T], F32)
        sel = kb.sbuf("sel", [32, 32, 128], F32)
        kb.dma("sp", sel[:], cx.m_sel[:], reads=[cx.m_sel], writes=[sel])
        ptr = [kb.psum("ptr%d" % i, [128, 512]) for i in range(2)]
        with kb.scope():
            wgl = kb.sbuf("wgl", [128, NT, 32], F32)
            id32 = kb.sbuf("id32", [128, 128], F32)
            kb.dma("sp", wgl[:], cx.WG_t[:], reads=[cx.WG_t], writes=[wgl])
            kb.dma("sp", id32[:], cx.m_ident32[:], reads=[cx.m_ident32], writes=[id32])
            for ti in range(NT):
                p_ = ptr[ti % 2]
                kb.mm([lambda p_=p_, ti=ti: nc.tensor.matmul(p_[:32, 0:128], lhsT=wgl[:, ti, :], rhs=id32[:], start=True, stop=True)],
                      reads=[wgl, id32], writes=[p_])
                kb.op("act", lambda p_=p_, ti=ti: nc.scalar.copy(out=wgT[:, ti * 128:(ti + 1) * 128], in_=p_[:32, 0:128]),
                      reads=[p_], writes=[], acc_writes=[wgT])
        precast_expert_weights(cx, kb, nc, l)
        hx = kb.sbuf("hxm", [128, 8, 512], BF16)
        yacc = kb.sbuf("yacc", [128, 8, 512], F32)
        hid = kb.sbuf("hid", [128, 16, 512], BF16)
        wdg = kb.sbuf("wdg", [128, 8, 2, 1024], BF16)
        NWB = 4
        wgs = [kb.sbuf("wgs%d" % i, [128, 8, 256], BF16) for i in range(NWB)]
        wus = [kb.sbuf("wus%d" % i, [128, 8, 256], BF16) for i in range(NWB)]
        xb = kb.sbuf("xbe", [128, 8, 512], F32)
        rsq = kb.sbuf("rsqe", [128, 8, 512], F32)
        mean = kb.sbuf("meane", [128, 512], F32)
        var = kb.sbuf("vare", [128, 512], F32)
        reps = [kb.sbuf("reps%d" % i, [128, 512], F32) for i in range(2)]
        ssb = [kb.sbuf("ssb%d" % i, [128, 512], F32) for i in range(2)]
        tsb = [kb.sbuf("tsb%d" % i, [128, 512], F32) for i in range(2)]
        ytmp = [kb.sbuf("ytmp%d" % i, [128, 512], F32) for i in range(2)]
        pg = [kb.psum("pg%d" % i, [128, 512]) for i in range(2)]
        pu = [kb.psum("pu%d" % i, [128, 512]) for i in range(2)]
        py = ptr
        ps1 = kb.psum("ps1e", [128, 512])
        ps2 = kb.psum("ps2e", [128, 512])
        prep = ps1
        nw = 0
        it = 0
        for bi, (t0, nb) in enumerate(BLKS):
            col = blkcol(t0)
            kb.dma("sp", hx[:, :, :nb], cx.HX2[:, t0:t0 + nb].rearrange("(kc p) t -> p kc t", p=128), reads=[cx.HX2], writes=[hx])
            kb.dma("sp", xb[:, :, :nb], cx.XT[:, t0:t0 + nb].rearrange("(kc p) t -> p kc t", p=128), reads=[cx.XT], writes=[xb])
            for g in range(4):
                kb.dma("sp", wdg[:].rearrange("p e hc d -> p (e hc d)"), cx.WDB[g], reads=[cx.WDB], writes=[wdg])
                for e_ in range(8):
                    e = g * 8 + e_
                    wg_, wu_ = wgs[nw % NWB], wus[nw % NWB]
                    nw += 1
                    kb.dma("sp", wg_[:].rearrange("p kc f -> p (kc f)"), cx.WGB[e], reads=[cx.WGB], writes=[wg_])
                    kb.dma("act", wu_[:].rearrange("p kc f -> p (kc f)"), cx.WUB[e], reads=[cx.WUB], writes=[wu_])
                    rp = reps[e % 2]
                    kb.mm([lambda e=e: nc.tensor.matmul(prep[:, :nb], lhsT=sel[:, e, :], rhs=wgT[:, t0:t0 + nb], start=True, stop=True)],
                          reads=[sel, wgT], writes=[prep])
                    kb.op("act", lambda rp=rp: nc.scalar.copy(out=rp[:, :nb], in_=prep[:, :nb]), reads=[prep], writes=[rp])
                    for hc in range(2):
                        a = it % 2
                        it += 1
                        kb.mm([(lambda kc=kc, a=a, hc=hc, wg_=wg_: nc.tensor.matmul(
                            pg[a][:, :nb], lhsT=wg_[:, kc, hc * 128:(hc + 1) * 128], rhs=hx[:, kc, :nb],
                            start=(kc == 0), stop=(kc == 7))) for kc in range(8)], reads=[wg_, hx], writes=[pg[a]])
                        kb.mm([(lambda kc=kc, a=a, hc=hc, wu_=wu_: nc.tensor.matmul(
                            pu[a][:, :nb], lhsT=wu_[:, kc, hc * 128:(hc + 1) * 128], rhs=hx[:, kc, :nb],
                            start=(kc == 0), stop=(kc == 7))) for kc in range(8)], reads=[wu_, hx], writes=[pu[a]])
                        kb.op("act", lambda a=a: nc.scalar.activation(out=ssb[a][:, :nb], in_=pg[a][:, :nb], func=AF.Silu),
                              reads=[pg[a]], writes=[ssb[a]])
                        kb.op("act", lambda a=a: nc.scalar.copy(out=tsb[a][:, :nb], in_=pu[a][:, :nb]), reads=[pu[a]], writes=[tsb[a]])
                        kb.op("dve", lambda a=a: nc.vector.tensor_tensor(out=tsb[a][:, :nb], in0=tsb[a][:, :nb], in1=ssb[a][:, :nb], op=ALU.mult),
                              reads=[tsb[a], ssb[a]], writes=[tsb[a]])
                        kb.op("pool", lambda a=a, rp=rp, e_=e_, hc=hc: nc.gpsimd.tensor_tensor(
                            out=hid[:, e_ * 2 + hc, :nb], in0=tsb[a][:, :nb], in1=rp[:, :nb], op=ALU.mult),
                            reads=[tsb[a], rp], writes=[], acc_writes=[hid])
                for oc in range(8):
                    p_ = py[oc % 2]
                    fns = []
                    for j in range(16):
                        fns.append(lambda j=j, oc=oc, p_=p_: nc.tensor.matmul(
                            p_[:, :nb], lhsT=wdg[:, j // 2, j % 2, oc * 128:(oc + 1) * 128], rhs=hid[:, j, :nb],
                            start=(j == 0), stop=(j == 15)))
                    kb.mm(fns, reads=[wdg, hid], writes=[p_])
                    if g == 0:
                        kb.op("act", lambda oc=oc, p_=p_: nc.scalar.copy(out=yacc[:, oc, :nb], in_=p_[:, :nb]), reads=[p_], writes=[], acc_writes=[yacc])
                    else:
                        yt = ytmp[oc % 2]
                        kb.op("act", lambda yt=yt, p_=p_: nc.scalar.copy(out=yt[:, :nb], in_=p_[:, :nb]), reads=[p_], writes=[yt])
                        eng = "dve" if oc % 2 == 0 else "pool"
                        h_ = nc.vector if eng == "dve" else nc.gpsimd
                        kb.op(eng, lambda h_=h_, oc=oc, yt=yt: h_.tensor_tensor(
                            out=yacc[:, oc, :nb], in0=yacc[:, oc, :nb], in1=yt[:, :nb], op=ALU.add),
                            reads=[yt, yacc], writes=[], acc_writes=[yacc])
            for oc in range(8):
                kb.op("pool", lambda oc=oc: nc.gpsimd.tensor_scalar(
                    out=yacc[:, oc, :nb], in0=yacc[:, oc, :nb], scalar1=modv[:, 40 + oc, col:col + 1], scalar2=None, op0=ALU.mult),
                    reads=[yacc, modv], writes=[yacc])
                kb.op("dve", lambda oc=oc: nc.vector.scalar_tensor_tensor(
                    out=yacc[:, oc, :nb], in0=xb[:, oc, :nb], scalar=ALPHA, in1=yacc[:, oc, :nb], op0=ALU.mult, op1=ALU.add),
                    reads=[xb, yacc], writes=[yacc])
            ln_block(kb, nc, yacc, nb, gam, bet, yacc, ones, eps, (rsq, ps1, ps2, mean, var))
            kb.dma("sp", cx.XT[:, t0:t0 + nb].rearrange("(kc p) t -> p kc t", p=128), yacc[:, :, :nb],
                   reads=[yacc], writes=[], acc_writes=[cx.XT])
            if yout is not None and t0 < TL:
                kb.dma("sp", yout[:, t0:t0 + nb].rearrange("(kc p) t -> p kc t", p=128), yacc[:, :, :nb],
                       reads=[yacc], writes=[], acc_writes=[yout])


def precast_expert_weights(cx, kb, nc, l):
    with kb.scope():
        stw = [kb.sbuf("pcw%d" % i, [128, 8, 256], BF16) for i in range(4)]
        std = [kb.sbuf("pcd%d" % i, [128, 16, 1024], BF16) for i in range(2)]
        n = 0
        for g in range(4):
            d_ = std[g % 2]
            kb.dma("pool", d_[:], cx.moe_w_down[l, g].rearrange("e (hc p) d -> p (e hc) d", p=128), reads=[cx.moe_w_down], writes=[d_])
            kb.dma("sp", cx.WDB[g], d_[:].rearrange("p j d -> p (j d)"), reads=[d_], writes=[], acc_writes=[cx.WDB])
            for e_ in range(8):
                e = g * 8 + e_
                for src, dst in ((cx.moe_w_gate, cx.WGB), (cx.moe_w_up, cx.WUB)):
                    w_ = stw[n % 4]
                    n += 1
                    kb.dma("pool", w_[:], src[l, g, e_].rearrange("(kc p) f -> p kc f", p=128), reads=[src], writes=[w_])
                    kb.dma("sp", dst[e], w_[:].rearrange("p kc f -> p (kc f)"), reads=[w_], writes=[], acc_writes=[dst])


def rope_tables():
    t = np.arange(TL)
    row = (t // 64).astype(np.float32)
    colp = (t % 64).astype(np.float32)
    nf = 16
    inv = (np.float32(10000.0) ** (-np.arange(nf, dtype=np.float32) / np.float32(nf))).astype(np.float32)
    cos = np.ones((128, T), np.float32)
    sin = np.zeros((128, T), np.float32)
    for q in range(128):
        d = q % 64
        pos = row if d < 32 else colp
        ang = (pos * inv[d % 16]).astype(np.float32)
        cos[q, :TL] = np.cos(ang).astype(np.float32)
        sin[q, :TL] = np.sin(ang).astype(np.float32)
    pt = np.zeros((128, 128), np.float32)
    for m in range(128):
        if (m % 32) < 16:
            pt[m + 16, m] = -1.0
        else:
            pt[m - 16, m] = 1.0
    return cos, sin, pt


_CONST = {}
_NAIDX = {}


def consts():
    if not _CONST:
        cos, sin, pt = rope_tables()
        _CONST.update(rope_cos=cos, rope_sin=sin, rope_pt=pt)
        _CONST.update(scan_masks())
        _CONST.update(wa_masks())
        sel = np.zeros((32, 32, 128), np.float32)
        for e in range(32):
            sel[e, e, :] = 1.0
        _CONST["m_sel"] = sel
        _CONST["m_ident32"] = np.eye(128, dtype=np.float32)
        _k, _valid, _ri, _ci = na_consts()
        _CONST["m_navalid"] = _valid
        _NAIDX["ri"] = _ri
        _NAIDX["ci"] = _ci
    return _CONST


def core_inputs(inp, b):
    f = lambda a: np.ascontiguousarray(a, dtype=np.float32)
    m = {}
    m["xin"] = f(np.concatenate([inp["x"][b].T, inp["ctx"][b].T], axis=1))
    cv = np.stack([inp["c"][b].reshape(8, 128).T, inp["c_ctx"].reshape(8, 128).T], axis=2)
    m["cvec"] = f(cv)
    m["w_mod"] = f(inp["w_mod"])
    m["b_modT"] = f(inp["b_mod"].reshape(2, 48, 128).transpose(0, 2, 1))
    m["w_in"] = f(inp["w_in"])
    m["hg_gamma"] = f(inp["hg_gamma"])
    m["gla_lr_w"] = f(inp["gla_lr_w"])
    m["gla_lr_b"] = f(inp["gla_lr_b"])
    m["wa_sink"] = f(inp["wa_sink"])
    for k in ("w_branch", "w_out", "moe_w_group", "moe_b_group", "moe_w_expert", "moe_b_expert",
              "moe_w_gate", "moe_w_up", "moe_w_down"):
        m[k] = f(inp[k])
    m["ln_gT"] = f(inp["ln_g"].reshape(2, 2, 8, 128).transpose(0, 1, 3, 2))
    m["ln_bT"] = f(inp["ln_b"].reshape(2, 2, 8, 128).transpose(0, 1, 3, 2))
    consts()
    m["na_bias"] = f(inp["na_rpb"][:, :, _NAIDX["ri"], _NAIDX["ci"]])
    m.update(consts())
    return m


FUSED = 1


def build_program(layers, final):
    nc = bass.Bass("TRN2", target_bir_lowering=False)
    cx = Ctx()
    declare_inputs(cx, nc)
    if final:
        yout = Buf(nc.dram_tensor("yout", [D, TL], F32, kind="ExternalOutput"), "yout")
    else:
        yout = None
    with ExitStack() as st:
        kb = KB(nc, st)
        declare_scratch(cx, kb, debug=False, as_output=(() if final else ("XT",)))
        modT = [kb.sbuf("modT%d" % l, [128, 48, 2], F32) for l in range(2)]
        stage_mod(cx, kb, nc, modT)
        first = True
        for l in layers:
            xsrc = cx.xin if first else cx.XT
            first = False
            with kb.scope():
                lbt = [kb.sbuf("lbt%d" % i, [128, 1024], F32) for i in range(2)]
                omlt = [kb.sbuf("omlt%d" % i, [128, 1024], F32) for i in range(2)]
                stage_lb(cx, kb, nc, lbt, omlt)
                hxT = kb.sbuf("hxT", [128, 8, T], BF16)
                stage_modulate(cx, kb, nc, xsrc, modT[l], 0, 1, hxT)
                stage_inproj_fm(cx, kb, nc, l, hxT)
                stage_inproj_tm(cx, kb, nc, l, hxT, lbt[l], omlt[l])
            stage_gla_gate(cx, kb, nc, l)
            stage_scan(cx, kb, nc, "gla")
            stage_scan(cx, kb, nc, "hgrn")
            stage_wa(cx, kb, nc, l)
            stage_na(cx, kb, nc, l)
            stage_merge(cx, kb, nc, l, xsrc, modT[l])
            stage_moe(cx, kb, nc, l, modT[l], yout if (final and l == layers[-1]) else None)
        if final:
            kb.finish([yout])
        else:
            kb.finish([cx.XT])
    return nc


def kernel(**inputs):
    inp = {k: np.asarray(v) for k, v in inputs.items()}
    in_maps = [core_inputs(inp, b) for b in range(4)]
    if FUSED:
        nc = build_program([0, 1], True)
        res = run_bass_kernel_spmd(nc, in_maps, core_ids=list(range(4)))
    else:
        nc0 = build_program([0], False)
        res0 = run_bass_kernel_spmd(nc0, in_maps, core_ids=list(range(4)))
        for b in range(4):
            in_maps[b]["xin"] = np.ascontiguousarray(res0.results[b]["XT"])
        nc1 = build_program([1], True)
        res = run_bass_kernel_spmd(nc1, in_maps, core_ids=list(range(4)))
    out = np.stack([np.ascontiguousarray(res.results[b]["yout"].T) for b in range(4)], axis=0)
    return out.astype(np.float32)
```

```python
import numpy as np
from contextlib import ExitStack
import concourse.bass as bass
import concourse.mybir as mybir
from concourse.bass_utils import run_bass_kernel_spmd
import os

F32 = mybir.dt.float32
BF16 = mybir.dt.bfloat16
U32 = mybir.dt.uint32
AF = mybir.ActivationFunctionType
ALU = mybir.AluOpType
AX = mybir.AxisListType


class Buf:
    __slots__ = ("t", "w", "r", "name")

    def __init__(self, t, name=""):
        self.t = t
        self.w = []
        self.r = []
        self.name = name

    def __getitem__(self, idx):
        return self.t[idx]


class Dep:
    __slots__ = ("w", "r")

    def __init__(self):
        self.w = []
        self.r = []


class Eng:
    def __init__(self, name, h, sem):
        self.name = name
        self.h = h
        self.sem = sem
        self.count = 0
        self.seen = {}


class KB:
    def __init__(self, nc, stack, n_dma_sems=8):
        self.nc = nc
        self.stack = stack
        self.sems = {}
        self.engs = {}
        for name, h in (("pe", nc.tensor), ("act", nc.scalar), ("dve", nc.vector),
                        ("pool", nc.gpsimd), ("sp", nc.sync)):
            s = stack.enter_context(nc.semaphore("s_" + name))
            self.sems[id(s)] = s
            self.engs[name] = Eng(name, h, s)
        self.dma_pool = {}
        for q in ("sp", "pool", "act"):
            lst = []
            for i in range(n_dma_sems):
                s = stack.enter_context(nc.semaphore("d_%s%d" % (q, i)))
                self.sems[id(s)] = s
                lst.append([s, 0])
            self.dma_pool[q] = [lst, 0]
        self.ninstr = 0

    def scope(self):
        kb = self

        class _Scope:
            def __enter__(self_s):
                self_s.prev = kb.stack
                self_s.st = ExitStack()
                self_s.st.__enter__()
                kb.stack = self_s.st
                kb.nscope = getattr(kb, "nscope", 0) + 1
                return self_s

            def __exit__(self_s, *a):
                if a[0] is None:
                    kb.barrier()
                kb.stack = self_s.prev
                return self_s.st.__exit__(*a)
        return _Scope()

    def sbuf(self, name, shape, dtype):
        name = "%s_s%d" % (name, getattr(self, "nscope", 0))
        t = self.stack.enter_context(self.nc.sbuf_tensor(name, list(shape), dtype))
        return Buf(t, name)

    def psum(self, name, shape, dtype=F32):
        name = "%s_s%d" % (name, getattr(self, "nscope", 0))
        t = self.stack.enter_context(self.nc.psum_tensor(name, list(shape), dtype))
        return Buf(t, name)

    def dram(self, name, shape, dtype, kind="Internal"):
        t = self.nc.dram_tensor(name, list(shape), dtype, kind=kind)
        return Buf(t, name)

    def _wait(self, e, deps):
        need = {}
        for (sid, val) in deps:
            if need.get(sid, 0) < val:
                need[sid] = val
        for sid, val in need.items():
            if e.seen.get(sid, 0) < val:
                e.h.wait_ge(self.sems[sid], val)
                e.seen[sid] = val
                self.ninstr += 1

    def _collect(self, reads, writes, acc_writes=()):
        deps = []
        for b in reads:
            deps += b.w
        for b in writes:
            deps += b.w
            deps += b.r
        for b in acc_writes:
            deps += b.r
        return deps

    @staticmethod
    def _compact(lst):
        mx = {}
        for (sid, v) in lst:
            if mx.get(sid, 0) < v:
                mx[sid] = v
        return list(mx.items())

    def _commit(self, token, reads, writes, acc_writes=()):
        for b in acc_writes:
            b.w.append(token)
            if len(b.w) > 64:
                b.w = self._compact(b.w)
        for b in reads:
            b.r.append(token)
            if len(b.r) > 64:
                mx = {}
                for (sid, v) in b.r:
                    if mx.get(sid, 0) < v:
                        mx[sid] = v
                b.r = list(mx.items())
        for b in writes:
            b.w = [token]
            b.r = []

    def _cutcheck(self):
        import os
        cut = os.environ.get("KB_CUT")
        self.nops = getattr(self, "nops", 0) + 1
        skip = os.environ.get("KB_SKIP")
        if skip and self.nops in [int(x) for x in skip.split(",")]:
            return True
        return cut is not None and self.nops > int(cut)

    def op(self, eng, fn, reads=(), writes=(), same_engine_sync=True, acc_writes=()):
        if self._cutcheck():
            return None
        e = self.engs[eng]
        deps = self._collect(reads, writes, acc_writes)
        if not same_engine_sync:
            deps = [d for d in deps if d[0] != id(e.sem)]
        self._wait(e, deps)
        ins = fn()
        e.count += 1
        ins.then_inc(e.sem, 1)
        self.ninstr += 1
        token = (id(e.sem), e.count)
        self._commit(token, reads, writes, acc_writes)
        return token

    def mm(self, fns, reads=(), writes=()):
        if self._cutcheck():
            return None
        e = self.engs["pe"]
        deps = self._collect(reads, writes)
        deps = [d for d in deps if d[0] != id(e.sem)]
        self._wait(e, deps)
        ins = None
        for fn in fns:
            ins = fn()
            self.ninstr += 1
        e.count += 1
        ins.then_inc(e.sem, 1)
        token = (id(e.sem), e.count)
        self._commit(token, reads, writes)
        return token

    def dma(self, q, out, in_, reads=(), writes=(), acc_writes=(), **kw):
        e = self.engs[q]
        pool = self.dma_pool[q]
        lst, idx = pool
        ent = lst[idx % len(lst)]
        pool[1] = idx + 1
        s, total = ent
        deps = self._collect(reads, writes, acc_writes)
        deps.append((id(s), total))
        import os
        if os.environ.get("DMA_SERIAL") and getattr(self, "_last_dma", {}).get(q):
            deps.append(self._last_dma[q])
        self._wait(e, deps)
        ins = e.h.dma_start(out=out, in_=in_, **kw)
        ins.then_inc(s, 16)
        ent[1] = total + 16
        self.ninstr += 1
        token = (id(s), total + 16)
        if not hasattr(self, "_last_dma"):
            self._last_dma = {}
        self._last_dma[q] = token
        self._commit(token, reads, writes, acc_writes)
        return token

    def init_memory(self, nbytes=196608):
        nc = self.nc
        with self.scope():
            n = nbytes // 4
            big = self.sbuf("initbig", [128, n], F32)
            q = n // 4
            self.op("dve", lambda: nc.vector.memset(big[:, 0:q], 0.0), writes=[])
            self.op("pool", lambda: nc.gpsimd.memset(big[:, q:2 * q], 0.0), writes=[])
            self.op("dve", lambda: nc.vector.memset(big[:, 2 * q:3 * q], 0.0), writes=[])
            self.op("pool", lambda: nc.gpsimd.memset(big[:, 3 * q:n], 0.0), writes=[])
            pss = [self.psum("initps%d" % i, [128, 512]) for i in range(8)]
            for p in pss:
                self.op("dve", lambda p=p: nc.vector.memset(p[:], 0.0), writes=[])

    def barrier(self):
        allt = []
        for e in self.engs.values():
            if e.count:
                allt.append((id(e.sem), e.count))
        for q, (lst, _) in self.dma_pool.items():
            for s, total in lst:
                if total:
                    allt.append((id(s), total))
        for e in self.engs.values():
            self._wait(e, allt)

    def finish(self, tokens_bufs):
        e = self.engs["sp"]
        deps = []
        for b in tokens_bufs:
            deps += b.w
        self._wait(e, deps)

T = 4352
TL = 4096

D = 1024
TL = 4096
TC = 256
T = TL + TC
NT = T // 128
BLKS = [(i * 512, 512) for i in range(8)] + [(4096, 256)]
DIN = 10528
ALPHA = (2.0 * 2) ** 0.25

COLS = dict(a_q=(0, 256), a_k=(256, 256), a_v=(512, 512), a_g=(1024, 512), a_lr=(1536, 32),
            b_q=(1568, 512), b_k=(2080, 512), b_v=(2592, 512),
            c_q=(3104, 512), c_k=(3616, 128), c_v=(3744, 128),
            d_q=(3872, 512), d_f=(4384, 1024), d_i=(5408, 512), d_g=(5920, 512), gate=(6432, 4096))


def blkcol(t0):
    return 1 if t0 >= TL else 0


class Ctx:
    pass


def declare_inputs(cx, nc):
    def din(name, shape, dt=F32):
        return Buf(nc.dram_tensor(name, list(shape), dt, kind="ExternalInput"), name)
    cx.xin = din("xin", [D, T])
    cx.cvec = din("cvec", [128, 8, 2])
    cx.w_mod = din("w_mod", [2, D, 6 * D])
    cx.b_modT = din("b_modT", [2, 128, 48])
    cx.w_in = din("w_in", [2, D, DIN])
    cx.rope_cos = din("rope_cos", [128, T])
    cx.rope_sin = din("rope_sin", [128, T])
    cx.rope_pt = din("rope_pt", [128, 128])
    cx.hg_gamma = din("hg_gamma", [2, 2, 512])
    cx.gla_lr_w = din("gla_lr_w", [2, 2, 16, 256])
    cx.gla_lr_b = din("gla_lr_b", [2, 2, 256])
    for nm in ("mg", "mh"):
        setattr(cx, nm + "_mq", din(nm + "_mq", [128, 2, 130]))
        setattr(cx, nm + "_mk", din(nm + "_mk", [128, 2, 128]))
    cx.m_ma = din("m_ma", [128, 2, 128])
    cx.m_ident = din("m_ident", [128, 128])
    cx.m_mau = din("m_mau", [128, 2, 128], U32)
    cx.m_wa = din("m_wa", [128, 2, 128])
    cx.wa_sink = din("wa_sink", [2, 8])
    cx.m_navalid = din("m_navalid", [128, 21, 128])
    cx.w_branch = din("w_branch", [2, 4, 512, 1024])
    cx.w_out = din("w_out", [2, 1024, 1024])
    cx.ln_gT = din("ln_gT", [2, 2, 128, 8])
    cx.ln_bT = din("ln_bT", [2, 2, 128, 8])
    cx.moe_w_group = din("moe_w_group", [2, 1024, 4])
    cx.moe_b_group = din("moe_b_group", [2, 4])
    cx.moe_w_expert = din("moe_w_expert", [2, 1024, 32])
    cx.moe_b_expert = din("moe_b_expert", [2, 32])
    cx.moe_w_gate = din("moe_w_gate", [2, 4, 8, 1024, 256])
    cx.moe_w_up = din("moe_w_up", [2, 4, 8, 1024, 256])
    cx.moe_w_down = din("moe_w_down", [2, 4, 8, 256, 1024])
    cx.m_sel = din("m_sel", [32, 32, 128])
    cx.m_ident32 = din("m_ident32", [128, 128])
    cx.na_bias = din("na_bias", [2, 8, 128, 21, 128])


def declare_scratch(cx, kb, debug, as_input=(), as_output=()):
    kind = "ExternalOutput" if debug else "Internal"

    def ds(name, shape, dt):
        b = kb.dram(name, shape, dt, kind=("ExternalInput" if name in as_input else ("ExternalOutput" if name in as_output else kind)))
        setattr(cx, name, b)
        return b
    ds("XT", [D, T], F32)
    ds("QA", [256, T], BF16)
    ds("GA", [512, T], BF16)
    ds("LR1", [33, T], F32)
    ds("QB", [512, T], BF16)
    ds("KB_", [512, T], BF16)
    ds("QC", [512, T], BF16)
    ds("KC", [128, T], BF16)
    ds("QD", [512, T], BF16)
    ds("GD", [512, T], BF16)
    ds("GATE", [4096, T], BF16)
    ds("KVA_t", [128, NT, 768], BF16)
    ds("VB_t", [128, NT, 512], BF16)
    ds("VC_t", [128, NT, 128], BF16)
    ds("VD_t", [128, NT, 512], BF16)
    ds("LGD_t", [128, NT, 1024], F32)
    ds("KD_t", [128, NT, 1024], BF16)
    ds("LGA_t", [128, NT, 512], F32)
    ds("MODT", [2, 128, 96], F32)
    ds("HX2", [D, T], BF16)
    ds("WGB", [32, 128, 2048], BF16)
    ds("WUB", [32, 128, 2048], BF16)
    ds("WDB", [4, 128, 16384], BF16)
    ds("WG_t", [128, NT, 32], F32)
    ds("WGT", [32, T], F32)
    ds("BR_A", [512, T], BF16)
    ds("BR_B", [512, T], BF16)
    ds("BR_C", [512, T], BF16)
    ds("BR_D", [512, T], BF16)


def stage_mod(cx, kb, nc, modT):
    with kb.scope():
        cv = kb.sbuf("cv", [128, 8, 2], F32)
        sc = kb.sbuf("sc", [128, 8, 2], F32)
        kb.dma("sp", cv[:], cx.cvec[:], reads=[cx.cvec], writes=[cv])
        kb.op("act", lambda: nc.scalar.activation(out=sc[:], in_=cv[:], func=AF.Silu), reads=[cv], writes=[sc])
        wbufs = [kb.sbuf("wm%d" % i, [128, 8, 1024], F32) for i in range(2)]
        bm = kb.sbuf("bm", [128, 48], F32)
        ps = kb.psum("ps_mod", [128, 96])
        wi = 0
        for l in range(2):
            kb.dma("sp", bm[:], cx.b_modT[l], reads=[cx.b_modT], writes=[bm])
            for g in range(6):
                wb = wbufs[wi % 2]
                wi += 1
                src = cx.w_mod[l, :, g * 1024:(g + 1) * 1024].rearrange("(kc p) c -> p kc c", p=128)
                kb.dma("sp", wb[:], src, reads=[cx.w_mod], writes=[wb])
                for j in range(8):
                    jj = g * 8 + j
                    fns = []
                    for kc in range(8):
                        fns.append(lambda kc=kc, j=j, jj=jj, wb=wb: nc.tensor.matmul(
                            ps[:, 2 * jj:2 * jj + 2], lhsT=wb[:, kc, j * 128:(j + 1) * 128], rhs=sc[:, kc, :],
                            start=(kc == 0), stop=(kc == 7)))
                    kb.mm(fns, reads=[wb, sc], writes=[ps])
            m = modT[l]
            kb.op("dve", lambda m=m: nc.vector.tensor_tensor(
                out=m[:], in0=ps[:].rearrange("p (j c) -> p j c", c=2),
                in1=bm[:].unsqueeze(2).broadcast_to([128, 48, 2]), op=ALU.add),
                reads=[ps, bm], writes=[m])
            kb.dma("sp", cx.MODT[l], m[:].rearrange("p j c -> p (j c)"), reads=[m], writes=[cx.MODT])


def stage_modulate(cx, kb, nc, src_dram, modv, shift_idx, scale_idx, hxT, hx32=None):
    with kb.scope():
        onep = kb.sbuf("onep", [128, 8, 2], F32)
        kb.op("dve", lambda: nc.vector.tensor_scalar(
            out=onep[:], in0=modv[:, scale_idx * 8:(scale_idx + 1) * 8, :], scalar1=1.0, scalar2=None, op0=ALU.add),
            reads=[modv], writes=[onep])
        xb = [kb.sbuf("xb%d" % i, [128, 8, 512], F32) for i in range(2)]
        for bi, (t0, nb) in enumerate(BLKS):
            col = blkcol(t0)
            x = xb[bi % 2]
            kb.dma("sp", x[:, :, :nb], src_dram[:, t0:t0 + nb].rearrange("(kc p) t -> p kc t", p=128),
                   reads=[src_dram], writes=[x])
            for kc in range(8):
                eng = "dve" if kc % 2 == 0 else "pool"
                h = nc.vector if eng == "dve" else nc.gpsimd
                kb.op(eng, lambda h=h, kc=kc, x=x: h.tensor_scalar(
                    out=hxT[:, kc, t0:t0 + nb], in0=x[:, kc, :nb],
                    scalar1=onep[:, kc, col:col + 1], scalar2=modv[:, shift_idx * 8 + kc, col:col + 1],
                    op0=ALU.mult, op1=ALU.add), reads=[x, onep, modv], writes=[hxT])


FM_GROUPS = [
    ("a_q", "QA", "copy"), ("a_g", "GA", "silu"), ("a_lr", "LR1", "copy32"),
    ("b_q", "QB", "copy"), ("b_k", "KB_", "copy"),
    ("c_q", "QC", "rope"), ("c_k", "KC", "rope"),
    ("d_q", "QD", "silu"), ("d_g", "GD", "silu"), ("gate", "GATE", "sigmoid"),
]


def stage_inproj_fm(cx, kb, nc, l, hxT, only=None):
    with kb.scope():
        wsl = [kb.sbuf("wsl%d" % i, [128, 8, 512], BF16) for i in range(2)]
        stg = [kb.sbuf("stg%d" % i, [128, T], BF16) for i in range(2)]
        stg32 = kb.sbuf("stg32", [32, T], F32)
        pss = [kb.psum("psf%d" % i, [128, 512]) for i in range(4)]
        psr = [kb.psum("psr%d" % i, [128, 512]) for i in range(2)]
        cosT = kb.sbuf("cosT", [128, T], F32)
        sinT = kb.sbuf("sinT", [128, T], F32)
        ptm = kb.sbuf("ptm", [128, 128], BF16)
        tmpb = [kb.sbuf("tmpb%d" % i, [128, 512], BF16) for i in range(2)]
        t1 = [kb.sbuf("t1_%d" % i, [128, 512], F32) for i in range(2)]
        t2 = [kb.sbuf("t2_%d" % i, [128, 512], F32) for i in range(2)]
        ones = kb.sbuf("ones1", [1, T], F32)
        kb.dma("sp", cosT[:], cx.rope_cos[:], reads=[cx.rope_cos], writes=[cosT])
        kb.dma("sp", sinT[:], cx.rope_sin[:], reads=[cx.rope_sin], writes=[sinT])
        kb.dma("pool", ptm[:], cx.rope_pt[:], reads=[cx.rope_pt], writes=[ptm])
        kb.op("dve", lambda: nc.vector.memset(ones[:], 1.0), writes=[ones])
        kb.dma("sp", cx.LR1[32:33, :], ones[:], reads=[ones], writes=[], acc_writes=[cx.LR1])
        nsl = 0
        nps = 0
        nst = 0
        nrp = 0
        for (name, dest, kind) in FM_GROUPS:
            if only is not None and name not in only:
                continue
            c0, nc_ = COLS[name]
            dbuf = getattr(cx, dest)
            for s0 in range(0, nc_, 512):
                sw = min(512, nc_ - s0)
                w = wsl[nsl % 2]
                nsl += 1
                kb.dma("pool", w[:, :, :sw],
                       cx.w_in[l, :, c0 + s0:c0 + s0 + sw].rearrange("(kc p) c -> p kc c", p=128),
                       reads=[cx.w_in], writes=[w])
                for m0 in range(0, sw, 128):
                    mw = min(128, sw - m0)
                    if kind == "copy32":
                        st = stg32
                    else:
                        st = stg[nst % 2]
                        nst += 1
                    for bi, (t0, nb) in enumerate(BLKS):
                        ps = pss[nps % 4]
                        nps += 1
                        fns = [(lambda kc=kc, ps=ps, w=w: nc.tensor.matmul(
                            ps[:mw, :nb], lhsT=w[:, kc, m0:m0 + mw], rhs=hxT[:, kc, t0:t0 + nb],
                            start=(kc == 0), stop=(kc == 7))) for kc in range(8)]
                        kb.mm(fns, reads=[w, hxT], writes=[ps])
                        o = st[:mw, t0:t0 + nb]
                        if kind in ("copy", "copy32"):
                            if nps % 2 == 0:
                                kb.op("dve", lambda ps=ps, o=o: nc.vector.tensor_copy(out=o, in_=ps[:mw, :nb]),
                                      reads=[ps], writes=[st])
                            else:
                                kb.op("act", lambda ps=ps, o=o: nc.scalar.copy(out=o, in_=ps[:mw, :nb]),
                                      reads=[ps], writes=[st])
                        elif kind == "silu":
                            kb.op("act", lambda ps=ps, o=o: nc.scalar.activation(out=o, in_=ps[:mw, :nb], func=AF.Silu),
                                  reads=[ps], writes=[st])
                        elif kind == "sigmoid":
                            kb.op("act", lambda ps=ps, o=o: nc.scalar.activation(out=o, in_=ps[:mw, :nb], func=AF.Sigmoid),
                                  reads=[ps], writes=[st])
                        elif kind == "rope":
                            tb = tmpb[nrp % 2]
                            a1 = t1[nrp % 2]
                            a2 = t2[nrp % 2]
                            pr = psr[nrp % 2]
                            nrp += 1
                            kb.op("act", lambda ps=ps, tb=tb: nc.scalar.copy(out=tb[:mw, :nb], in_=ps[:mw, :nb]),
                                  reads=[ps], writes=[tb])
                            kb.mm([lambda pr=pr, tb=tb: nc.tensor.matmul(pr[:mw, :nb], lhsT=ptm[:mw, :mw], rhs=tb[:mw, :nb],
                                                                         start=True, stop=True)],
                                  reads=[ptm, tb], writes=[pr])
                            kb.op("dve", lambda tb=tb, a1=a1: nc.vector.tensor_tensor(
                                out=a1[:mw, :nb], in0=tb[:mw, :nb], in1=cosT[:mw, t0:t0 + nb], op=ALU.mult),
                                reads=[tb, cosT], writes=[a1])
                            kb.op("dve", lambda pr=pr, a2=a2: nc.vector.tensor_tensor(
                                out=a2[:mw, :nb], in0=pr[:mw, :nb], in1=sinT[:mw, t0:t0 + nb], op=ALU.mult),
                                reads=[pr, sinT], writes=[a2])
                            kb.op("pool", lambda a1=a1, a2=a2, o=o: nc.gpsimd.tensor_tensor(
                                out=o, in0=a1[:mw, :nb], in1=a2[:mw, :nb], op=ALU.add),
                                reads=[a1, a2], writes=[st])
                    kb.dma("sp", dbuf[s0 + m0:s0 + m0 + mw, :], st[:mw, :], reads=[st], writes=[], acc_writes=[dbuf])


TM_GROUPS = [
    (256, 512, "KVA_t", 0, "copy"), (768, 256, "KVA_t", 512, "copy"),
    (2592, 512, "VB_t", 0, "copy"), (3744, 128, "VC_t", 0, "copy"),
    (5408, 512, "VD_t", 0, "copy"),
    (4384, 512, None, 0, "hgf"), (4896, 512, None, 512, "hgf"),
]


def stage_inproj_tm(cx, kb, nc, l, hxT, lbt, omlt, only=None):
    with kb.scope():
        wsl = [kb.sbuf("wtl%d" % i, [128, 8, 512], BF16) for i in range(2)]
        pss = [kb.psum("pst%d" % i, [128, 512]) for i in range(4)]
        stb = [kb.sbuf("stb%d" % i, [128, 512], BF16) for i in range(4)]
        e1 = [kb.sbuf("e1_%d" % i, [128, 512], F32) for i in range(2)]
        r1 = [kb.sbuf("r1_%d" % i, [128, 512], F32) for i in range(2)]
        lg = [kb.sbuf("lg_%d" % i, [128, 512], F32) for i in range(2)]
        kk = [kb.sbuf("kk_%d" % i, [128, 512], BF16) for i in range(2)]
        nsl = nps = nst = nh = 0
        for (c0, ncol, dest, doff, kind) in TM_GROUPS:
            if only is not None and kind not in only:
                continue
            w = wsl[nsl % 2]
            nsl += 1
            kb.dma("pool", w[:, :, :ncol], cx.w_in[l, :, c0:c0 + ncol].rearrange("(kc p) c -> p kc c", p=128),
                   reads=[cx.w_in], writes=[w])
            for ti in range(NT):
                ts = slice(ti * 128, (ti + 1) * 128)
                ps = pss[nps % 4]
                nps += 1
                fns = [(lambda kc=kc, ps=ps, w=w: nc.tensor.matmul(
                    ps[:, :ncol], lhsT=hxT[:, kc, ts], rhs=w[:, kc, :ncol],
                    start=(kc == 0), stop=(kc == 7))) for kc in range(8)]
                kb.mm(fns, reads=[w, hxT], writes=[ps])
                if kind == "copy":
                    st = stb[nst % 4]
                    nst += 1
                    if nst % 2 == 0:
                        kb.op("dve", lambda ps=ps, st=st: nc.vector.tensor_copy(out=st[:, :ncol], in_=ps[:, :ncol]),
                              reads=[ps], writes=[st])
                    else:
                        kb.op("act", lambda ps=ps, st=st: nc.scalar.copy(out=st[:, :ncol], in_=ps[:, :ncol]),
                              reads=[ps], writes=[st])
                    db = getattr(cx, dest)
                    kb.dma("sp", db[:, ti, doff:doff + ncol], st[:, :ncol], reads=[st], writes=[], acc_writes=[db])
                else:
                    e = e1[nh % 2]
                    r = r1[nh % 2]
                    g = lg[nh % 2]
                    k = kk[nh % 2]
                    nh += 1
                    cs = slice(doff, doff + 512)
                    kb.op("act", lambda ps=ps, e=e: nc.scalar.activation(out=e[:], in_=ps[:], func=AF.Exp, scale=-1.0),
                          reads=[ps], writes=[e])
                    kb.op("pool", lambda e=e: nc.gpsimd.tensor_scalar(
                        out=e[:], in0=e[:], scalar1=1.0, scalar2=None, op0=ALU.add),
                        reads=[e], writes=[e])
                    kb.op("dve", lambda e=e, r=r: nc.vector.reciprocal(out=r[:], in_=e[:]),
                          reads=[e], writes=[r])
                    kb.op("dve", lambda r=r: nc.vector.tensor_tensor(out=r[:], in0=r[:], in1=omlt[:, cs], op=ALU.mult),
                          reads=[r, omlt], writes=[r])
                    kb.op("pool", lambda r=r: nc.gpsimd.tensor_tensor(out=r[:], in0=r[:], in1=lbt[:, cs], op=ALU.add),
                          reads=[r, lbt], writes=[r])
                    kb.op("act", lambda r=r, g=g: nc.scalar.activation(out=g[:], in_=r[:], func=AF.Ln),
                          reads=[r], writes=[g])
                    kb.op("pool", lambda r=r, k=k: nc.gpsimd.tensor_scalar(
                        out=k[:], in0=r[:], scalar1=-1.0, scalar2=1.0, op0=ALU.mult, op1=ALU.add),
                        reads=[r], writes=[k])
                    kb.dma("sp", cx.LGD_t[:, ti, cs], g[:], reads=[g], writes=[], acc_writes=[cx.LGD_t])
                    kb.dma("sp", cx.KD_t[:, ti, cs], k[:], reads=[k], writes=[], acc_writes=[cx.KD_t])


def stage_lb(cx, kb, nc, lbt, omlt):
    with kb.scope():
        gam = kb.sbuf("gam", [128, 2, 2, 512], F32)
        kb.dma("sp", gam[:].rearrange("p a b c -> p (a b c)"),
               cx.hg_gamma[:].rearrange("a b c -> (a b c)").partition_broadcast(128),
               reads=[cx.hg_gamma], writes=[gam])
        d = kb.sbuf("gamd", [128, 2, 512], F32)
        kb.op("dve", lambda: nc.vector.tensor_tensor(out=d[:], in0=gam[:, :, 0, :], in1=gam[:, :, 1, :], op=ALU.subtract),
              reads=[gam], writes=[d])
        kb.op("act", lambda: nc.scalar.activation(out=d[:], in_=d[:], func=AF.Exp), reads=[d], writes=[d])
        l1 = lbt[1]
        kb.op("dve", lambda: nc.vector.tensor_scalar(
            out=d[:], in0=d[:], scalar1=1.0, scalar2=None, op0=ALU.add), reads=[d], writes=[d])
        kb.op("dve", lambda: nc.vector.reciprocal(out=l1[:], in_=d[:].rearrange("p a c -> p (a c)")),
              reads=[d], writes=[l1])
        l0 = lbt[0]
        kb.op("dve", lambda: nc.vector.memset(l0[:], 0.0), writes=[l0])
        for l in range(2):
            o = omlt[l]
            lb = lbt[l]
            kb.op("dve", lambda o=o, lb=lb: nc.vector.tensor_scalar(
                out=o[:], in0=lb[:], scalar1=-1.0, scalar2=1.0, op0=ALU.mult, op1=ALU.add),
                reads=[lb], writes=[o])


def stage_gla_gate(cx, kb, nc, l):
    with kb.scope():
        wlr = kb.sbuf("wlr", [33, 512], F32)
        lr1 = kb.sbuf("lr1", [33, T], F32)
        kb.op("dve", lambda: nc.vector.memset(wlr[:], 0.0), writes=[wlr])
        kb.dma("sp", wlr[0:16, 0:256], cx.gla_lr_w[l, 0], reads=[cx.gla_lr_w], writes=[wlr])
        kb.dma("sp", wlr[16:32, 256:512], cx.gla_lr_w[l, 1], reads=[cx.gla_lr_w], writes=[wlr])
        kb.dma("sp", wlr[32:33, :], cx.gla_lr_b[l].rearrange("a c -> (a c)").unsqueeze(0),
               reads=[cx.gla_lr_b], writes=[wlr])
        kb.dma("sp", lr1[:], cx.LR1[:], reads=[cx.LR1], writes=[lr1])
        pss = [kb.psum("psg%d" % i, [128, 512]) for i in range(2)]
        ee = [kb.sbuf("ge%d" % i, [128, 512], F32) for i in range(2)]
        gg = [kb.sbuf("gg%d" % i, [128, 512], F32) for i in range(2)]
        for ti in range(NT):
            ts = slice(ti * 128, (ti + 1) * 128)
            ps = pss[ti % 2]
            e = ee[ti % 2]
            g = gg[ti % 2]
            kb.mm([lambda ps=ps: nc.tensor.matmul(ps[:], lhsT=lr1[:, ts], rhs=wlr[:], start=True, stop=True)],
                  reads=[lr1, wlr], writes=[ps])
            kb.op("act", lambda ps=ps, e=e: nc.scalar.activation(out=e[:], in_=ps[:], func=AF.Exp, scale=-1.0),
                  reads=[ps], writes=[e])
            kb.op("act", lambda e=e, g=g: nc.scalar.activation(out=g[:], in_=e[:], func=AF.Ln, bias=1.0),
                  reads=[e], writes=[g])
            kb.dma("sp", cx.LGA_t[:, ti, :], g[:], reads=[g], writes=[], acc_writes=[cx.LGA_t])

SCAN_CFG = {
    "gla": dict(H=4, dk=64, qsrc="QA", qscale=0.125, ksrc="KVA_t", kz=0, kcol0=0, vsrc="KVA_t", vcol0=256,
                gsrc="LGA_t", gz=256, gate="GA", dest="BR_A", mask="mg"),
    "hgrn": dict(H=4, dk=128, qsrc="QD", qscale=1.0, ksrc="KD_t", kz=512, kcol0=0, vsrc="VD_t", vcol0=0,
                 gsrc="LGD_t", gz=512, gate="GD", dest="BR_D", mask="mh"),
}


def view(b):
    return Buf(b.t, b.name + "_v")


def stage_scan(cx, kb, nc, which, heads=None):
    cfg = SCAN_CFG[which]
    H, dk = cfg["H"], cfg["dk"]
    dv = 128
    qsrc, ksrc, vsrc, gsrc = (getattr(cx, cfg[k]) for k in ("qsrc", "ksrc", "vsrc", "gsrc"))
    gate, dest = getattr(cx, cfg["gate"]), getattr(cx, cfg["dest"])
    mq_d, mk_d = getattr(cx, cfg["mask"] + "_mq"), getattr(cx, cfg["mask"] + "_mk")
    with kb.scope():
        mq = kb.sbuf("mq", [128, 2, 130], F32)
        mk = kb.sbuf("mk", [128, 2, 128], F32)
        ma = kb.sbuf("ma", [128, 2, 128], F32)
        ident = kb.sbuf("ident", [128, 128], BF16)
        onesdv = kb.sbuf("onesdv", [128, 128], F32)
        kb.dma("sp", mq[:], mq_d[:], reads=[mq_d], writes=[mq])
        kb.dma("sp", mk[:], mk_d[:], reads=[mk_d], writes=[mk])
        kb.dma("sp", ma[:], cx.m_ma[:], reads=[cx.m_ma], writes=[ma])
        mau = kb.sbuf("mau", [128, 2, 128], U32)
        kb.dma("sp", mau[:], cx.m_mau[:], reads=[cx.m_mau], writes=[mau])
        kb.dma("pool", ident[:], cx.m_ident[:], reads=[cx.m_ident], writes=[ident])
        kb.op("dve", lambda: nc.vector.memset(onesdv[:], 1.0 / dv), writes=[onesdv])
        vt = kb.sbuf("vt", [128, NT, dv], BF16)
        qT = kb.sbuf("qT", [128, T], BF16)
        gsil = kb.sbuf("gsil", [128, T], BF16)
        obr = kb.sbuf("obr", [128, T], BF16)
        gts = [kb.sbuf("gt%d" % i, [128, NT, dk], F32) for i in range(2)]
        kts = [kb.sbuf("kt%d" % i, [128, NT, dk], BF16) for i in range(2)]
        qt = [kb.sbuf("qt%d" % z, [128, T], BF16) for z in range(2)]
        atm = [kb.sbuf("atm%d" % z, [128, NT, 128], BF16) for z in range(2)]
        Sp = [kb.sbuf("Sp%d" % z, [128, 2 * NT, dv], BF16) for z in range(2)]
        for z in range(2):
            kb.op("pool", lambda z=z: nc.gpsimd.memset(atm[z][:], 0.0), writes=[atm[z]])
        Sst = [kb.sbuf("Sst%d" % i, [128, dv], F32) for i in range(2)]
        R = 3
        eq = [kb.sbuf("eq%d" % i, [128, 128], F32) for i in range(R)]
        ek = [kb.sbuf("ek%d" % i, [128, 128], F32) for i in range(R)]
        ekh = [kb.sbuf("ekh%d" % i, [128, 128], F32) for i in range(R)]
        ktl = [kb.sbuf("ktl%d" % i, [128, 128], BF16) for i in range(R)]
        khat = [kb.sbuf("khat%d" % i, [128, 256], BF16) for i in range(R)]
        ktr = [kb.sbuf("ktr%d" % i, [128, 128], F32) for i in range(R)]
        atr = [kb.sbuf("atr%d" % i, [128, 128], F32) for i in range(R)]
        usb = [kb.sbuf("usb%d" % i, [128, 256], F32) for i in range(R)]
        osb = [kb.sbuf("osb%d" % i, [128, 512], F32) for i in range(2)]
        cm = kb.sbuf("cm", [128, 2], F32)
        kb.op("dve", lambda: nc.vector.memset(cm[:], 0.0), writes=[cm])
        kb.op("dve", lambda: nc.vector.memset(cm[0:64, 0:1], 1.0), writes=[cm])
        kb.op("dve", lambda: nc.vector.memset(cm[64:128, 1:2], 1.0), writes=[cm])
        er2 = [kb.sbuf("er%d" % i, [128, 2], F32) for i in range(R)]
        ed2 = [kb.sbuf("ed%d" % i, [128, 2], F32) for i in range(R)]
        psA = [kb.psum("psA%d" % i, [128, 512]) for i in range(2)]
        psB = [kb.psum("psB%d" % i, [128, 512]) for i in range(2)]
        psA_ec = [view(p) for p in psA]
        import os
        if True:
            psK = [kb.psum("psK%d" % i, [128, 512]) for i in range(2)]
        else:
            psK = psA
        psA_kt = [view(p) for p in psK]
        psA_e2 = [view(p) for p in psK]
        psB_u = [view(p) for p in psB]
        psB_at = [view(p) for p in psB]
        psO = [kb.psum("psO%d" % i, [128, 512]) for i in range(1)] * 2
        psM = [kb.psum("psM%d" % i, [128, 512]) for i in range(1)] * 2
        sq = [kb.sbuf("sq%d" % i, [128, 512], F32) for i in range(2)]
        sd = [kb.sbuf("sd%d" % i, [128, 512], F32) for i in range(2)]
        on = [kb.sbuf("on%d" % i, [128, 512], F32) for i in range(2)]
        eps_t = kb.sbuf("eps_t", [128, 1], F32)
        kb.op("dve", lambda: nc.vector.memset(eps_t[:], 1e-6), writes=[eps_t])

        it = 0
        ghz = 0
        import os
        for h in (range(H) if heads is None else heads):
            vc0 = cfg["vcol0"] + h * dv
            kb.dma("sp", vt[:], vsrc[:, :, vc0:vc0 + dv], reads=[vsrc], writes=[vt])
            kb.dma("sp", qT[:dk, :], qsrc[h * dk:(h + 1) * dk, :], reads=[qsrc], writes=[qT])
            kb.dma("sp", gsil[:], gate[h * dv:(h + 1) * dv, :], reads=[gate], writes=[gsil])
            for z in range(2):
                gt = gts[ghz % 2]
                kt = kts[ghz % 2]
                ghz += 1
                gc0 = z * cfg["gz"] + h * dk
                kc0 = cfg["kcol0"] + z * cfg["kz"] + h * dk
                kb.dma("sp", gt[:], gsrc[:, :, gc0:gc0 + dk], reads=[gsrc], writes=[gt])
                if os.environ.get("KTMEMSET"):
                    kb.op("dve", lambda kt=kt: nc.vector.memset(kt[:], 0.0), writes=[kt])
                kb.dma("sp", kt[:], ksrc[:, :, kc0:kc0 + dk], reads=[ksrc], writes=[kt])
                order = [32, 33] + list(range(32)) if z == 0 else [33, 32] + list(range(31, -1, -1))
                import os
                DBG = os.environ.get("SCAN_DBG", "")
                if DBG == "loads":
                    continue
                if DBG.startswith("nt"):
                    order = order[:int(DBG[2:])]
                S = Sst[0]
                kb.op("dve", lambda S=S: nc.vector.memset(S[:], 0.0), writes=[S])
                si = 0
                def p1(ti, a, r):
                    ts = slice(ti * 128, (ti + 1) * 128)
                    pa, pb = psA[a], psB[a]
                    pk = psK[a]
                    kb.mm([lambda pa=pa, gt=gt: nc.tensor.matmul(pa[:dk, 0:130], lhsT=gt[:, ti, :], rhs=mq[:, z, :],
                                                                 start=True, stop=True)],
                          reads=[gt, mq], writes=[psA_ec[a]])
                    kb.mm([lambda pk=pk, kt=kt: nc.tensor.matmul(pk[:dk, 256:384], lhsT=(ident[:, :dk] if os.environ.get("A2ID") else kt[:, ti, :]), rhs=ident[:],
                                                                 start=True, stop=True)],
                          reads=[kt, ident], writes=[psA_kt[a]])
                    kb.mm([lambda pk=pk, gt=gt: nc.tensor.matmul(pk[:, 384:384 + dk], lhsT=mk[:, z, :], rhs=gt[:, ti, :],
                                                                 start=True, stop=True)],
                          reads=[gt, mk], writes=[psA_e2[a]])
                    kb.op("act", lambda pa=pa, o=eq[r]: nc.scalar.activation(out=o[:dk, :], in_=pa[:dk, 0:128], func=AF.Exp),
                          reads=[psA_ec[a]], writes=[eq[r]])
                    kb.op("act", lambda pa=pa, o=ek[r]: nc.scalar.activation(out=o[:dk, :], in_=pa[:dk, 0:128], func=AF.Exp, scale=-1.0),
                          reads=[psA_ec[a]], writes=[ek[r]])
                    kb.op("act", lambda pa=pa, o=er2[r]: nc.scalar.activation(out=o[:dk, :], in_=pa[:dk, 128:130], func=AF.Exp, scale=0.5),
                          reads=[psA_ec[a]], writes=[er2[r]])
                    kb.op("act", lambda pa=pa, o=ed2[r]: nc.scalar.activation(out=o[:dk, :], in_=pa[:dk, 128:130], func=AF.Exp),
                          reads=[psA_ec[a]], writes=[ed2[r]])
                    kb.op("act", lambda pk=pk, o=ekh[r]: nc.scalar.activation(out=o[:, :dk], in_=pk[:, 384:384 + dk], func=AF.Exp),
                          reads=[psA_e2[a]], writes=[ekh[r]])
                    qz = qt[z]
                    kb.op("dve", lambda qz=qz, e=eq[r]: nc.vector.scalar_tensor_tensor(
                        out=qz[:dk, ts], in0=qT[:dk, ts], scalar=cfg["qscale"], in1=e[:dk, :], op0=ALU.mult, op1=ALU.mult),
                        reads=[qT, eq[r]], writes=[], acc_writes=[qz])
                    kb.op("act", lambda pk=pk, o=ktr[r]: nc.scalar.copy(out=o[:dk, :], in_=pk[:dk, 256:384]),
                          reads=[psA_kt[a]], writes=[ktr[r]])
                    kb.op("dve", lambda e=ek[r], o=ktl[r], kr=ktr[r]: nc.vector.tensor_tensor(
                        out=o[:dk, :], in0=kr[:dk, :], in1=e[:dk, :], op=ALU.mult),
                        reads=[ktr[r], ek[r]], writes=[ktl[r]])
                    for c in range(2):
                        kb.op("dve", lambda kt=kt, e=ekh[r], o=khat[r], c=c: nc.vector.scalar_tensor_tensor(
                            out=o[:, c * 128:c * 128 + dk], in0=kt[:, ti, :], scalar=cm[:, c:c + 1], in1=e[:, :dk],
                            op0=ALU.mult, op1=ALU.mult),
                            reads=[kt, ekh[r], cm], writes=[], acc_writes=[khat[r]])
                    return None

                def p2(ti, a, r, S, si):
                    ts = slice(ti * 128, (ti + 1) * 128)
                    pa, pb = psA[a], psB[a]
                    qz = qt[z]
                    kb.mm([(lambda c=c, pb=pb, kh=khat[r]: nc.tensor.matmul(
                        pb[:dk, c * 128:(c + 1) * 128], lhsT=kh[:, c * 128:c * 128 + dk],
                        rhs=vt[:, ti, :], start=True, stop=True)) for c in range(2)],
                        reads=[khat[r], vt], writes=[psB_u[a]])
                    kb.mm([lambda pb=pb, k_=ktl[r], qz=qz: nc.tensor.matmul(
                        pb[:, 256:384], lhsT=k_[:dk, :], rhs=qz[:dk, ts], start=True, stop=True)],
                        reads=[ktl[r], qz], writes=[psB_at[a]])
                    az = atm[z]
                    kb.op("act", lambda pb=pb, o=atr[r]: nc.scalar.copy(out=o[:], in_=pb[:, 256:384]),
                          reads=[psB_at[a]], writes=[atr[r]])
                    kb.op("dve", lambda az=az, o=atr[r]: nc.vector.copy_predicated(
                        out=az[:, ti, :], mask=mau[:, z, :], data=o[:]),
                        reads=[atr[r], mau], writes=[], acc_writes=[az])
                    kb.op("act", lambda pb=pb, o=usb[r]: nc.scalar.copy(out=o[:dk, :], in_=pb[:dk, 0:256]),
                          reads=[psB_u[a]], writes=[usb[r]])
                    spz = Sp[z]
                    for c in ((0, 1) if z == 0 else (1, 0)):
                        ci = 2 * ti + c
                        kb.op("act", lambda S=S, spz=spz, ci=ci, e=er2[r], c=c: nc.scalar.activation(
                            out=spz[:dk, ci, :], in_=S[:dk, :], func=AF.Copy, scale=e[:dk, c:c + 1]),
                            reads=[S, er2[r]], writes=[], acc_writes=[spz])
                        S2 = Sst[(si + 1) % 2]
                        si += 1
                        kb.op("dve", lambda S=S, S2=S2, e=ed2[r], u=usb[r], c=c: nc.vector.scalar_tensor_tensor(
                            out=S2[:dk, :], in0=S[:dk, :], scalar=e[:dk, c:c + 1], in1=u[:dk, c * 128:(c + 1) * 128],
                            op0=ALU.mult, op1=ALU.add),
                            reads=[S, ed2[r], usb[r]], writes=[S2])
                        S = S2
                    return S, si

                sched = []
                for ti in order:
                    sched.append((ti, it % 2, it % R))
                    it += 1
                for idx in range(len(sched) + 1):
                    if idx < len(sched):
                        p1(*sched[idx])
                    if idx >= 1:
                        S, si = p2(*sched[idx - 1], S, si)
            groups = [list(range(g * 4, g * 4 + 4)) for g in range(8)] + [[32, 33]]
            if DBG:
                groups = []
            for gi, tiles in enumerate(groups):
                po = psO[gi % 2]
                pm = psM[gi % 2]
                nb = len(tiles) * 128
                t0 = tiles[0] * 128
                fns = []
                for k_, ti in enumerate(tiles):
                    cs = k_ * 128
                    for z in range(2):
                        fns.append(lambda po=po, ti=ti, cs=cs, z=z: nc.tensor.matmul(
                            po[:, cs:cs + 128], lhsT=vt[:, ti, :], rhs=atm[z][:, ti, :], start=(z == 0), stop=False))
                        for c in range(2):
                            fns.append(lambda po=po, ti=ti, cs=cs, z=z, c=c: nc.tensor.matmul(
                                po[:, cs + c * 64:cs + (c + 1) * 64], lhsT=Sp[z][:dk, 2 * ti + c, :],
                                rhs=qt[z][:dk, ti * 128 + c * 64:ti * 128 + (c + 1) * 64],
                                start=False, stop=(z == 1)))
                kb.mm(fns, reads=[vt, atm[0], atm[1], Sp[0], Sp[1], qt[0], qt[1]], writes=[po])
                s_, d_, o_ = sq[gi % 2], sd[gi % 2], on[gi % 2]
                kb.op("act", lambda po=po, s_=s_: nc.scalar.activation(out=s_[:, :nb], in_=po[:, :nb], func=AF.Square),
                      reads=[po], writes=[s_])
                kb.mm([lambda pm=pm, s_=s_: nc.tensor.matmul(pm[:, :nb], lhsT=onesdv[:], rhs=s_[:, :nb], start=True, stop=True)],
                      reads=[onesdv, s_], writes=[pm])
                kb.op("act", lambda pm=pm, d_=d_: nc.scalar.activation(out=d_[:, :nb], in_=pm[:, :nb], func=AF.Sqrt, bias=eps_t[:]),
                      reads=[pm, eps_t], writes=[d_])
                kb.op("dve", lambda d_=d_: nc.vector.reciprocal(out=d_[:, :nb], in_=d_[:, :nb]), reads=[d_], writes=[d_])
                ob_ = osb[gi % 2]
                kb.op("act", lambda po=po, ob_=ob_: nc.scalar.copy(out=ob_[:, :nb], in_=po[:, :nb]), reads=[po], writes=[ob_])
                kb.op("dve", lambda ob_=ob_, d_=d_, o_=o_: nc.vector.tensor_tensor(
                    out=o_[:, :nb], in0=ob_[:, :nb], in1=d_[:, :nb], op=ALU.mult), reads=[ob_, d_], writes=[o_])
                kb.op("pool", lambda o_=o_: nc.gpsimd.tensor_tensor(
                    out=obr[:, t0:t0 + nb], in0=o_[:, :nb], in1=gsil[:, t0:t0 + nb], op=ALU.mult),
                    reads=[o_, gsil], writes=[], acc_writes=[obr])
            kb.dma("sp", dest[h * dv:(h + 1) * dv, :], obr[:], reads=[obr], writes=[], acc_writes=[dest])


def scan_masks():
    j = np.arange(128)[:, None]
    i = np.arange(128)[None, :]
    same = (j // 64 == i // 64).astype(np.float32)
    out = {}
    for name, gs in (("mg", -1.0 / 16.0), ("mh", 1.0)):
        mq = np.zeros((128, 2, 130), np.float32)
        mk = np.zeros((128, 2, 128), np.float32)
        mq[:, 0, :128] = gs * same * ((j <= i).astype(np.float32) - 0.5)
        mq[:, 1, :128] = gs * same * ((j >= i).astype(np.float32) - 0.5)
        for z in range(2):
            for c in range(2):
                mq[:, z, 128 + c] = gs * (np.arange(128) // 64 == c)
        mk[:, 0, :] = gs * same * (j > i)
        mk[:, 1, :] = gs * same * (j < i)
        out[name + "_mq"] = mq
        out[name + "_mk"] = mk
    ma = np.zeros((128, 2, 128), np.float32)
    ma[:, 0, :] = same * (j <= i)
    ma[:, 1, :] = same * (j >= i)
    out["m_ma"] = ma
    out["m_mau"] = ma.astype(np.uint32)
    out["m_ident"] = np.eye(128, dtype=np.float32)
    return out


def stage_wa(cx, kb, nc, l, groups=None):
    with kb.scope():
        wm = kb.sbuf("wm", [128, 2, 128], BF16)
        kb.dma("pool", wm[:], cx.m_wa[:], reads=[cx.m_wa], writes=[wm])
        ones64 = kb.sbuf("ones64", [128, 64], BF16)
        kb.op("dve", lambda: nc.vector.memset(ones64[:], 1.0), writes=[ones64])
        sk = kb.sbuf("sk", [64, 8], F32)
        kb.dma("sp", sk[:], cx.wa_sink[l].partition_broadcast(64), reads=[cx.wa_sink], writes=[sk])
        kb.op("act", lambda: nc.scalar.activation(out=sk[:], in_=sk[:], func=AF.Exp), reads=[sk], writes=[sk])
        q4 = kb.sbuf("q4", [64, 4, T], BF16)
        kT = kb.sbuf("kTc", [64, T], BF16)
        vt = kb.sbuf("vtc", [128, NT, 64], BF16)
        ost = kb.sbuf("ost", [64, 4, T], BF16)
        pS = [kb.psum("pS%d" % i, [128, 512]) for i in range(4)]
        pO = [kb.psum("pO%d" % i, [128, 512]) for i in range(2)]
        pD = [kb.psum("pD%d" % i, [128, 512]) for i in range(2)]
        R = 10
        pt = [kb.sbuf("pt%d" % i, [128, 4, 128], BF16) for i in range(R)]
        ptm = [kb.sbuf("ptm%d" % i, [128, 4, 128], BF16) for i in range(R)]
        osb = [kb.sbuf("osbw%d" % i, [64, 4, 128], F32) for i in range(2)]
        dsb = [kb.sbuf("dsbw%d" % i, [64, 4, 128], F32) for i in range(2)]
        it = 0
        for g in (range(2) if groups is None else groups):
            kb.dma("sp", q4[:], cx.QC[g * 256:(g + 1) * 256, :].rearrange("(hh d) t -> d hh t", d=64),
                   reads=[cx.QC], writes=[q4])
            kb.dma("sp", kT[:], cx.KC[g * 64:(g + 1) * 64, :], reads=[cx.KC], writes=[kT])
            kb.dma("sp", vt[:], cx.VC_t[:, :, g * 64:(g + 1) * 64], reads=[cx.VC_t], writes=[vt])
            def p1(qb):
                nonlocal it
                if qb < 32:
                    keys = []
                    if qb > 0:
                        keys.append((qb - 1, 0))
                    keys.append((qb, None))
                    if qb < 31:
                        keys.append((qb + 1, 1))
                    keys += [(32, None), (33, None)]
                else:
                    keys = [(32, None), (33, None)]
                qs = slice(qb * 128, (qb + 1) * 128)
                st_ = []
                for ki, (kt_i, mi) in enumerate(keys):
                    ps = pS[it % 4]
                    p_ = pt[it % R]
                    pm_ = ptm[it % R]
                    it += 1
                    kb.mm([lambda ps=ps, kt_i=kt_i: nc.tensor.matmul(
                        ps[:, :], lhsT=kT[:, kt_i * 128:(kt_i + 1) * 128], rhs=q4[:, :, qs], start=True, stop=True)],
                        reads=[kT, q4], writes=[ps])
                    kb.op("act", lambda ps=ps, p_=p_: nc.scalar.activation(
                        out=p_[:].rearrange("p h q -> p (h q)"), in_=ps[:, :], func=AF.Exp, scale=0.125),
                        reads=[ps], writes=[p_])
                    src = p_
                    if mi is not None:
                        eng = "dve" if it % 2 == 0 else "pool"
                        h_ = nc.vector if eng == "dve" else nc.gpsimd
                        kb.op(eng, lambda h_=h_, p_=p_, pm_=pm_, mi=mi: h_.tensor_tensor(
                            out=pm_[:], in0=p_[:], in1=wm[:, mi, :].unsqueeze(1).broadcast_to([128, 4, 128]), op=ALU.mult),
                            reads=[p_, wm], writes=[pm_])
                        src = pm_
                    st_.append((kt_i, src))
                return (qb, st_)

            def p2(state):
                qb, st_ = state
                qs = slice(qb * 128, (qb + 1) * 128)
                a = qb % 2
                po, pd = pO[a], pD[a]
                nk = len(st_)
                for ki, (kt_i, src) in enumerate(st_):
                    kb.mm([lambda po=po, src=src, kt_i=kt_i, ki=ki: nc.tensor.matmul(
                        po[:64, :], lhsT=vt[:, kt_i, :], rhs=src[:].rearrange("p h q -> p (h q)"),
                        start=(ki == 0), stop=(ki == nk - 1)),
                        lambda pd=pd, src=src, ki=ki: nc.tensor.matmul(
                        pd[:64, :], lhsT=ones64[:], rhs=src[:].rearrange("p h q -> p (h q)"),
                        start=(ki == 0), stop=(ki == nk - 1))],
                        reads=[vt, src, ones64], writes=[po, pd])
                o_, d_ = osb[a], dsb[a]
                kb.op("act", lambda po=po, o_=o_: nc.scalar.copy(out=o_[:].rearrange("p h q -> p (h q)"), in_=po[:64, :]),
                      reads=[po], writes=[o_])
                kb.op("act", lambda pd=pd, d_=d_: nc.scalar.copy(out=d_[:].rearrange("p h q -> p (h q)"), in_=pd[:64, :]),
                      reads=[pd], writes=[d_])
                kb.op("dve", lambda d_=d_: nc.vector.tensor_tensor(
                    out=d_[:], in0=d_[:], in1=sk[:, g * 4:(g + 1) * 4].unsqueeze(2).broadcast_to([64, 4, 128]), op=ALU.add),
                    reads=[d_, sk], writes=[d_])
                kb.op("dve", lambda d_=d_: nc.vector.reciprocal(out=d_[:], in_=d_[:]), reads=[d_], writes=[d_])
                kb.op("pool", lambda o_=o_, d_=d_: nc.gpsimd.tensor_tensor(
                    out=ost[:, :, qs], in0=o_[:], in1=d_[:], op=ALU.mult),
                    reads=[o_, d_], writes=[], acc_writes=[ost])

            prev = None
            for qb in range(NT):
                cur = p1(qb)
                if prev is not None:
                    p2(prev)
                prev = cur
            p2(prev)
            kb.dma("sp", cx.BR_C[g * 256:(g + 1) * 256, :].rearrange("(hh d) t -> d hh t", d=64), ost[:],
                   reads=[ost], writes=[], acc_writes=[cx.BR_C])


def wa_masks():
    kk = np.arange(128)[:, None]
    qq = np.arange(128)[None, :]
    m = np.zeros((128, 2, 128), np.float32)
    m[:, 0, :] = (kk >= qq)
    m[:, 1, :] = (kk <= qq)
    return {"m_wa": m}


def na_patterns():
    W = 64
    tok = np.arange(128)
    lr, lc = tok // W, tok % W
    pats = []
    keys = []
    for m in range(32):
        qr = 2 * m + lr[None, :]
        qc = lc[None, :]
        rs = np.clip(qr - 4, 0, 56)
        cs = np.clip(qc - 8, 0, 48)
        lst = []
        for kt_i in range(32):
            kr = 2 * kt_i + lr[:, None]
            kc = lc[:, None]
            valid = (kr >= rs) & (kr < rs + 8) & (kc >= cs) & (kc < cs + 16)
            if not valid.any():
                continue
            ri = np.clip(kr - qr + 7, 0, 14)
            ci = np.clip(kc - qc + 15, 0, 30)
            if 2 <= m <= 29:
                pid = kt_i - m + 2
                if m == 2:
                    pats.append((valid, ri, ci))
            else:
                pid = len(pats)
                pats.append((valid, ri, ci))
            lst.append((kt_i, pid))
        keys.append(lst)
    return keys, pats


def na_consts():
    keys, pats = na_patterns()
    n0 = len(keys[0]) + len(keys[1])
    order = list(range(n0, n0 + 5)) + list(range(0, n0)) + list(range(n0 + 5, len(pats)))
    remap = {old: new for new, old in enumerate(order)}
    pats2 = [pats[o] for o in order]
    keys2 = []
    for m, lst in enumerate(keys):
        if 2 <= m <= 29:
            keys2.append(lst)
        else:
            keys2.append([(k, remap[p]) for (k, p) in lst])
    valid = np.stack([p[0] for p in pats2], 1).astype(np.float32)
    ri = np.stack([p[1] for p in pats2], 1)
    ci = np.stack([p[2] for p in pats2], 1)
    return keys2, valid, ri, ci


def stage_na(cx, kb, nc, l, heads=None, bg_factory=None):
    keys_tab, valid_np, _, _ = na_consts()
    npat = valid_np.shape[1]
    with kb.scope():
        vmask = kb.sbuf("vmask", [128, npat, 128], F32)
        kb.dma("sp", vmask[:], cx.m_navalid[:], reads=[cx.m_navalid], writes=[vmask])
        ones64 = kb.sbuf("ones64n", [128, 64], BF16)
        kb.op("dve", lambda: nc.vector.memset(ones64[:], 1.0), writes=[ones64])
        qT = kb.sbuf("qTn", [64, T], BF16)
        kT = kb.sbuf("kTn", [64, T], BF16)
        vt = kb.sbuf("vtn", [128, NT, 64], BF16)
        ost = kb.sbuf("ostn", [64, T], BF16)
        bias = kb.sbuf("biasn", [128, npat, 128], F32)
        EB = kb.sbuf("EBn", [128, npat, 128], BF16)
        pS = [kb.psum("pSn%d" % i, [128, 512]) for i in range(4)]
        pO = [kb.psum("pOn%d" % i, [128, 512]) for i in range(2)]
        pD = [kb.psum("pDn%d" % i, [128, 512]) for i in range(2)]
        R = 6
        pt = [kb.sbuf("ptn%d" % i, [128, 4, 128], BF16) for i in range(R)]
        osb = [kb.sbuf("osbn%d" % i, [64, 256], F32) for i in range(2)]
        it = 0
        bg = None
        if bg_factory is not None:
            stw = [kb.sbuf("pcw%d" % i, [128, 8, 256], BF16) for i in range(4)]
            std = [kb.sbuf("pcd%d" % i, [128, 16, 1024], BF16) for i in range(2)]
            bg = bg_factory(stw, std)
        for h in (range(8) if heads is None else heads):
            kb.dma("sp", bias[:], cx.na_bias[l, h], reads=[cx.na_bias], writes=[bias])
            kb.op("act", lambda: nc.scalar.activation(out=bias[:], in_=bias[:], func=AF.Exp), reads=[bias], writes=[bias])
            kb.op("pool", lambda: nc.gpsimd.tensor_tensor(out=EB[:], in0=bias[:], in1=vmask[:], op=ALU.mult),
                  reads=[bias, vmask], writes=[EB])
            kb.dma("sp", qT[:], cx.QB[h * 64:(h + 1) * 64, :], reads=[cx.QB], writes=[qT])
            kb.dma("sp", kT[:], cx.KB_[h * 64:(h + 1) * 64, :], reads=[cx.KB_], writes=[kT])
            kb.dma("sp", vt[:], cx.VB_t[:, :, h * 64:(h + 1) * 64], reads=[cx.VB_t], writes=[vt])
            def p1(qb):
                nonlocal it
                if qb < 32:
                    keys = list(keys_tab[qb]) + [(32, None), (33, None)]
                else:
                    keys = [(32, None), (33, None)]
                qs = slice(qb * 128, (qb + 1) * 128)
                grp = [keys[i:i + 4] for i in range(0, len(keys), 4)]
                st_ = []
                for gk in grp:
                    ps = pS[it % 4]
                    p_ = pt[it % R]
                    it += 1
                    ng = len(gk)
                    kb.mm([(lambda ps=ps, j=j, kt_i=kt_i: nc.tensor.matmul(
                        ps[:, j * 128:(j + 1) * 128], lhsT=kT[:, kt_i * 128:(kt_i + 1) * 128], rhs=qT[:, qs],
                        start=True, stop=True)) for j, (kt_i, _) in enumerate(gk)],
                        reads=[kT, qT], writes=[ps])
                    kb.op("act", lambda ps=ps, p_=p_, ng=ng: nc.scalar.activation(
                        out=p_[:, :ng, :].rearrange("p h q -> p (h q)"), in_=ps[:, :ng * 128], func=AF.Exp, scale=0.125),
                        reads=[ps], writes=[p_])
                    nloc = sum(1 for (_, pid) in gk if pid is not None)
                    if nloc:
                        pid0 = gk[0][1]
                        eng = "dve" if it % 2 == 0 else "pool"
                        h_ = nc.vector if eng == "dve" else nc.gpsimd
                        kb.op(eng, lambda h_=h_, p_=p_, pid0=pid0, nloc=nloc: h_.tensor_tensor(
                            out=p_[:, :nloc, :], in0=p_[:, :nloc, :], in1=EB[:, pid0:pid0 + nloc, :], op=ALU.mult),
                            reads=[p_, EB], writes=[p_])
                    st_.append((gk, p_))
                return (qb, st_, len(keys))

            def p2(state):
                qb, st_, nk = state
                qs = slice(qb * 128, (qb + 1) * 128)
                a = qb % 2
                po = pO[a]
                pd = pD[a]
                done = 0
                for gk, p_ in st_:
                    fns = []
                    for j, (kt_i, _) in enumerate(gk):
                        first = (done == 0)
                        last = (done == nk - 1)
                        fns.append(lambda po=po, p_=p_, j=j, kt_i=kt_i, first=first, last=last: nc.tensor.matmul(
                            po[:64, 0:128], lhsT=vt[:, kt_i, :], rhs=p_[:, j, :], start=first, stop=last))
                        fns.append(lambda pd=pd, p_=p_, j=j, first=first, last=last: nc.tensor.matmul(
                            pd[:64, 0:128], lhsT=ones64[:], rhs=p_[:, j, :], start=first, stop=last))
                        done += 1
                    kb.mm(fns, reads=[vt, p_, ones64], writes=[po, pd])
                o_ = osb[a]
                kb.op("act", lambda po=po, o_=o_: nc.scalar.copy(out=o_[:, 0:128], in_=po[:64, 0:128]), reads=[po], writes=[o_])
                kb.op("act", lambda pd=pd, o_=o_: nc.scalar.copy(out=o_[:, 128:256], in_=pd[:64, 0:128]), reads=[pd, o_], writes=[o_])
                kb.op("dve", lambda o_=o_: nc.vector.reciprocal(out=o_[:, 128:256], in_=o_[:, 128:256]), reads=[o_], writes=[o_])
                kb.op("pool", lambda o_=o_: nc.gpsimd.tensor_tensor(
                    out=ost[:, qs], in0=o_[:, 0:128], in1=o_[:, 128:256], op=ALU.mult),
                    reads=[o_], writes=[], acc_writes=[ost])

            prev = None
            for qb in range(NT):
                cur = p1(qb)
                if prev is not None:
                    p2(prev)
                prev = cur
                if bg is not None and qb % 2 == 1:
                    next(bg, None)
            p2(prev)
            kb.dma("sp", cx.BR_B[h * 64:(h + 1) * 64, :], ost[:], reads=[ost], writes=[], acc_writes=[cx.BR_B])
        if bg is not None:
            for _ in bg:
                pass

LN_EPS = 1e-5
MB = 512
MBLKS = [(i * MB, min(MB, T - i * MB)) for i in range((T + MB - 1) // MB)]


def ln_setup(cx, kb, nc):
    ones = kb.sbuf("ln_ones", [128, 128], F32)
    kb.op("dve", lambda: nc.vector.memset(ones[:], 1.0 / D), writes=[ones])
    eps = kb.sbuf("ln_eps", [128, 1], F32)
    kb.op("dve", lambda: nc.vector.memset(eps[:], LN_EPS), writes=[eps])
    return ones, eps


def ln_block(kb, nc, r, nb, gam, bet, out, ones, eps, scr):
    rsq, ps1, ps2, mean, var = scr
    kb.op("act", lambda: nc.scalar.activation(out=rsq[:, :, :nb], in_=r[:, :, :nb], func=AF.Square), reads=[r], writes=[rsq])
    kb.mm([(lambda oc=oc: nc.tensor.matmul(ps1[:, :nb], lhsT=ones[:], rhs=r[:, oc, :nb], start=(oc == 0), stop=(oc == 7)))
           for oc in range(8)], reads=[ones, r], writes=[ps1])
    kb.mm([(lambda oc=oc: nc.tensor.matmul(ps2[:, :nb], lhsT=ones[:], rhs=rsq[:, oc, :nb], start=(oc == 0), stop=(oc == 7)))
           for oc in range(8)], reads=[ones, rsq], writes=[ps2])
    kb.op("act", lambda: nc.scalar.copy(out=mean[:, :nb], in_=ps1[:, :nb]), reads=[ps1], writes=[mean])
    kb.op("act", lambda: nc.scalar.copy(out=var[:, :nb], in_=ps2[:, :nb]), reads=[ps2], writes=[var])
    m2 = rsq
    kb.op("dve", lambda: nc.vector.tensor_tensor(out=m2[:, 0, :nb], in0=mean[:, :nb], in1=mean[:, :nb], op=ALU.mult),
          reads=[mean, rsq], writes=[rsq])
    kb.op("dve", lambda: nc.vector.tensor_tensor(out=var[:, :nb], in0=var[:, :nb], in1=m2[:, 0, :nb], op=ALU.subtract),
          reads=[var, rsq], writes=[var])
    kb.op("act", lambda: nc.scalar.activation(out=var[:, :nb], in_=var[:, :nb], func=AF.Sqrt, bias=eps[:]),
          reads=[var, eps], writes=[var])
    kb.op("dve", lambda: nc.vector.reciprocal(out=var[:, :nb], in_=var[:, :nb]), reads=[var], writes=[var])
    kb.op("dve", lambda: nc.vector.tensor_tensor(
        out=r[:, :, :nb], in0=r[:, :, :nb], in1=mean[:, :nb].unsqueeze(1).broadcast_to([128, 8, nb]), op=ALU.subtract),
        reads=[r, mean], writes=[r])
    kb.op("pool", lambda: nc.gpsimd.tensor_tensor(
        out=r[:, :, :nb], in0=r[:, :, :nb], in1=var[:, :nb].unsqueeze(1).broadcast_to([128, 8, nb]), op=ALU.mult),
        reads=[r, var], writes=[r])
    for oc in range(8):
        eng = "dve" if oc % 2 == 0 else "pool"
        h_ = nc.vector if eng == "dve" else nc.gpsimd
        kb.op(eng, lambda h_=h_, oc=oc: h_.tensor_scalar(
            out=out[:, oc, :nb], in0=r[:, oc, :nb], scalar1=gam[:, oc:oc + 1], scalar2=bet[:, oc:oc + 1],
            op0=ALU.mult, op1=ALU.add), reads=[r, gam, bet], writes=[out])


def load_ln_params(cx, kb, nc, l, which):
    gam = kb.sbuf("ln_g", [128, 8], F32)
    bet = kb.sbuf("ln_b", [128, 8], F32)
    kb.dma("sp", gam[:], cx.ln_gT[l, which], reads=[cx.ln_gT], writes=[gam])
    kb.dma("sp", bet[:], cx.ln_bT[l, which], reads=[cx.ln_bT], writes=[bet])
    return gam, bet


def stage_merge(cx, kb, nc, l, xsrc, modv):
    with kb.scope():
        ones, eps = ln_setup(cx, kb, nc)
        gam, bet = load_ln_params(cx, kb, nc, l, 0)
        wbr = kb.sbuf("wbr", [128, 4, 4, 1024], BF16)
        wout = kb.sbuf("wout", [128, 8, 1024], BF16)
        for i in range(4):
            kb.dma("pool", wbr[:, i], cx.w_branch[l, i].rearrange("(kc p) c -> p kc c", p=128), reads=[cx.w_branch], writes=[wbr])
        kb.dma("pool", wout[:], cx.w_out[l].rearrange("(kc p) c -> p kc c", p=128), reads=[cx.w_out], writes=[wout])
        wr = kb.sbuf("wr", [128, 8, 36], F32)
        kb.dma("sp", wr[:, :, 0:4], cx.moe_w_group[l].rearrange("(kc p) c -> p kc c", p=128), reads=[cx.moe_w_group], writes=[wr])
        kb.dma("sp", wr[:, :, 4:36], cx.moe_w_expert[l].rearrange("(kc p) c -> p kc c", p=128), reads=[cx.moe_w_expert], writes=[wr])
        rb = kb.sbuf("rb", [128, 36], F32)
        kb.dma("sp", rb[:, 0:4], cx.moe_b_group[l].partition_broadcast(128), reads=[cx.moe_b_group], writes=[rb])
        kb.dma("sp", rb[:, 4:36], cx.moe_b_expert[l].partition_broadcast(128), reads=[cx.moe_b_expert], writes=[rb])
        onep4 = kb.sbuf("onep4", [128, 8, 2], F32)
        kb.op("dve", lambda: nc.vector.tensor_scalar(out=onep4[:], in0=modv[:, 32:40, :], scalar1=1.0, scalar2=None, op0=ALU.add),
              reads=[modv], writes=[onep4])
        brt = [kb.sbuf("brt%d" % i, [128, 4, 4, MB], BF16) for i in range(1)]
        gtt = kb.sbuf("gtt", [128, 32, MB], BF16)
        xb = kb.sbuf("xbm", [128, 8, MB], F32)
        mrg = kb.sbuf("mrg", [128, 8, MB], BF16)
        r = kb.sbuf("rm", [128, 8, MB], F32)
        hx2b = kb.sbuf("hx2b", [128, 8, MB], BF16)
        rsq = kb.sbuf("rsqm", [128, 8, MB], F32)
        x1 = r
        hx2f = rsq
        mean = kb.sbuf("meanm", [128, MB], F32)
        var = kb.sbuf("varm", [128, MB], F32)
        csb = [kb.sbuf("csb%d" % i, [128, MB], F32) for i in range(4)]
        acc = kb.sbuf("accm", [128, MB], F32)
        mixs = kb.sbuf("mixs", [128, MB], F32)
        pbr = [kb.psum("pbr%d" % i, [128, MB]) for i in range(4)]
        pmx = kb.psum("pmx", [128, MB])
        ps1 = kb.psum("ps1m", [128, MB])
        ps2 = kb.psum("ps2m", [128, MB])
        prt = kb.psum("prt", [128, MB])
        brs = [cx.BR_A, cx.BR_B, cx.BR_C, cx.BR_D]
        lgt = kb.sbuf("lgt", [128, 36], F32)
        gmx = kb.sbuf("gmx", [128, 1], F32)
        ngm = kb.sbuf("ngm", [128, 1], F32)
        ge = kb.sbuf("ge", [128, 4], F32)
        gs = kb.sbuf("gsr", [128, 1], F32)
        oh = kb.sbuf("oh", [128, 4], F32)
        lem = kb.sbuf("lem", [128, 4, 8], F32)
        mx8 = kb.sbuf("mx8", [128, 8], F32)
        dd = kb.sbuf("dd", [128, 1], F32)
        w1 = kb.sbuf("w1", [128, 1], F32)
        w2 = kb.sbuf("w2", [128, 1], F32)
        tt1 = kb.sbuf("tt1", [128, 32], F32)
        tt2 = kb.sbuf("tt2", [128, 32], F32)
        wgt = kb.sbuf("wgt", [128, 32], F32)
        for bi, (t0, nb) in enumerate(MBLKS):
            col = blkcol(t0)
            b_ = brt[0]
            for i in range(4):
                kb.dma("sp", b_[:, i, :, :nb], brs[i][:, t0:t0 + nb].rearrange("(kc p) t -> p kc t", p=128),
                       reads=[brs[i]], writes=[b_])
            kb.dma("sp", gtt[:, :, :nb], cx.GATE[:, t0:t0 + nb].rearrange("(j p) t -> p j t", p=128), reads=[cx.GATE], writes=[gtt])
            kb.dma("sp", xb[:, :, :nb], xsrc[:, t0:t0 + nb].rearrange("(kc p) t -> p kc t", p=128), reads=[xsrc], writes=[xb])
            for oc in range(8):
                for i in range(4):
                    kb.mm([(lambda i=i, kc=kc, oc=oc: nc.tensor.matmul(
                        pbr[i][:, :nb], lhsT=wbr[:, i, kc, oc * 128:(oc + 1) * 128], rhs=b_[:, i, kc, :nb],
                        start=(kc == 0), stop=(kc == 3))) for kc in range(4)], reads=[wbr, b_], writes=[pbr[i]])
                    kb.op("act", lambda i=i: nc.scalar.copy(out=csb[i][:, :nb], in_=pbr[i][:, :nb]), reads=[pbr[i]], writes=[csb[i]])
                    eng = "dve" if i % 2 == 0 else "pool"
                    h_ = nc.vector if eng == "dve" else nc.gpsimd
                    kb.op(eng, lambda h_=h_, i=i, oc=oc: h_.tensor_tensor(
                        out=csb[i][:, :nb], in0=csb[i][:, :nb], in1=gtt[:, i * 8 + oc, :nb], op=ALU.mult),
                        reads=[csb[i], gtt], writes=[csb[i]])
                kb.op("dve", lambda: nc.vector.tensor_tensor(out=acc[:, :nb], in0=csb[0][:, :nb], in1=csb[1][:, :nb], op=ALU.add),
                      reads=[csb[0], csb[1]], writes=[acc])
                kb.op("pool", lambda: nc.gpsimd.tensor_tensor(out=csb[2][:, :nb], in0=csb[2][:, :nb], in1=csb[3][:, :nb], op=ALU.add),
                      reads=[csb[2], csb[3]], writes=[csb[2]])
                kb.op("dve", lambda oc=oc: nc.vector.tensor_tensor(out=mrg[:, oc, :nb], in0=acc[:, :nb], in1=csb[2][:, :nb], op=ALU.add),
                      reads=[acc, csb[2]], writes=[mrg])
            for oc in range(8):
                kb.mm([(lambda kc=kc, oc=oc: nc.tensor.matmul(
                    pmx[:, :nb], lhsT=wout[:, kc, oc * 128:(oc + 1) * 128], rhs=mrg[:, kc, :nb],
                    start=(kc == 0), stop=(kc == 7))) for kc in range(8)], reads=[wout, mrg], writes=[pmx])
                kb.op("act", lambda: nc.scalar.copy(out=mixs[:, :nb], in_=pmx[:, :nb]), reads=[pmx], writes=[mixs])
                kb.op("pool", lambda oc=oc: nc.gpsimd.tensor_scalar(
                    out=mixs[:, :nb], in0=mixs[:, :nb], scalar1=modv[:, 16 + oc, col:col + 1], scalar2=None, op0=ALU.mult),
                    reads=[mixs, modv], writes=[mixs])
                kb.op("dve", lambda oc=oc: nc.vector.scalar_tensor_tensor(
                    out=r[:, oc, :nb], in0=xb[:, oc, :nb], scalar=ALPHA, in1=mixs[:, :nb], op0=ALU.mult, op1=ALU.add),
                    reads=[xb, mixs], writes=[r])
            ln_block(kb, nc, r, nb, gam, bet, x1, ones, eps, (rsq, ps1, ps2, mean, var))
            kb.dma("sp", cx.XT[:, t0:t0 + nb].rearrange("(kc p) t -> p kc t", p=128), x1[:, :, :nb],
                   reads=[x1], writes=[], acc_writes=[cx.XT])
            for oc in range(8):
                eng = "dve" if oc % 2 == 0 else "pool"
                h_ = nc.vector if eng == "dve" else nc.gpsimd
                kb.op(eng, lambda h_=h_, oc=oc: h_.tensor_scalar(
                    out=hx2f[:, oc, :nb], in0=x1[:, oc, :nb], scalar1=onep4[:, oc, col:col + 1],
                    scalar2=modv[:, 24 + oc, col:col + 1], op0=ALU.mult, op1=ALU.add),
                    reads=[x1, onep4, modv], writes=[hx2f])
            kb.op("act", lambda: nc.scalar.copy(out=hx2b[:, :, :nb], in_=hx2f[:, :, :nb]), reads=[hx2f], writes=[hx2b])
            kb.dma("sp", cx.HX2[:, t0:t0 + nb].rearrange("(kc p) t -> p kc t", p=128), hx2b[:, :, :nb],
                   reads=[hx2b], writes=[], acc_writes=[cx.HX2])
            for tj in range(nb // 128):
                ti = t0 // 128 + tj
                tsl = slice(tj * 128, (tj + 1) * 128)
                kb.mm([(lambda kc=kc: nc.tensor.matmul(prt[:, 0:36], lhsT=hx2f[:, kc, tsl], rhs=wr[:, kc, :],
                                                       start=(kc == 0), stop=(kc == 7))) for kc in range(8)],
                      reads=[hx2f, wr], writes=[prt])
                kb.op("act", lambda: nc.scalar.copy(out=lgt[:], in_=prt[:, 0:36]), reads=[prt], writes=[lgt])
                kb.op("dve", lambda: nc.vector.tensor_tensor(out=lgt[:], in0=lgt[:], in1=rb[:], op=ALU.add), reads=[lgt, rb], writes=[lgt])
                kb.op("dve", lambda: nc.vector.tensor_reduce(out=gmx[:], in_=lgt[:, 0:4], axis=AX.X, op=ALU.max), reads=[lgt], writes=[gmx])
                kb.op("dve", lambda: nc.vector.tensor_scalar(out=ngm[:], in0=gmx[:], scalar1=-1.0, scalar2=None, op0=ALU.mult), reads=[gmx], writes=[ngm])
                kb.op("act", lambda: nc.scalar.activation(out=ge[:], in_=lgt[:, 0:4], func=AF.Exp, bias=ngm[:]), reads=[lgt, ngm], writes=[ge])
                kb.op("dve", lambda: nc.vector.tensor_reduce(out=gs[:], in_=ge[:], axis=AX.X, op=ALU.add), reads=[ge], writes=[gs])
                kb.op("dve", lambda: nc.vector.reciprocal(out=gs[:], in_=gs[:]), reads=[gs], writes=[gs])
                kb.op("dve", lambda: nc.vector.tensor_scalar(out=oh[:], in0=lgt[:, 0:4], scalar1=gmx[:], scalar2=None, op0=ALU.is_equal), reads=[lgt, gmx], writes=[oh])
                kb.op("dve", lambda: nc.vector.tensor_scalar(out=oh[:], in0=oh[:], scalar1=-1.0, scalar2=1e30, op0=ALU.add, op1=ALU.mult), reads=[oh], writes=[oh])
                kb.op("dve", lambda: nc.vector.tensor_tensor(
                    out=lem[:], in0=lgt[:, 4:36].rearrange("p (g e) -> p g e", e=8), in1=oh[:].unsqueeze(2).broadcast_to([128, 4, 8]), op=ALU.add),
                    reads=[lgt, oh], writes=[lem])
                lemf = lem[:].rearrange("p g e -> p (g e)")
                kb.op("dve", lambda: nc.vector.max(out=mx8[:], in_=lem[:].rearrange("p g e -> p (g e)")), reads=[lem], writes=[mx8])
                kb.op("dve", lambda: nc.vector.tensor_tensor(out=dd[:], in0=mx8[:, 1:2], in1=mx8[:, 0:1], op=ALU.subtract), reads=[mx8], writes=[dd])
                kb.op("act", lambda: nc.scalar.activation(out=dd[:], in_=dd[:], func=AF.Exp), reads=[dd], writes=[dd])
                kb.op("dve", lambda: nc.vector.tensor_scalar(out=w1[:], in0=dd[:], scalar1=1.0, scalar2=None, op0=ALU.add), reads=[dd], writes=[w1])
                kb.op("dve", lambda: nc.vector.reciprocal(out=w1[:], in_=w1[:]), reads=[w1], writes=[w1])
                kb.op("dve", lambda: nc.vector.tensor_tensor(out=w2[:], in0=dd[:], in1=w1[:], op=ALU.mult), reads=[dd, w1], writes=[w2])
                kb.op("dve", lambda: nc.vector.tensor_scalar(
                    out=tt1[:], in0=lem[:].rearrange("p g e -> p (g e)"), scalar1=mx8[:, 0:1], scalar2=w1[:], op0=ALU.is_equal, op1=ALU.mult),
                    reads=[lem, mx8, w1], writes=[tt1])
                kb.op("dve", lambda: nc.vector.tensor_scalar(
                    out=tt2[:], in0=lem[:].rearrange("p g e -> p (g e)"), scalar1=mx8[:, 1:2], scalar2=w2[:], op0=ALU.is_equal, op1=ALU.mult),
                    reads=[lem, mx8, w2], writes=[tt2])
                kb.op("dve", lambda: nc.vector.tensor_tensor(out=tt1[:], in0=tt1[:], in1=tt2[:], op=ALU.add), reads=[tt1, tt2], writes=[tt1])
                kb.op("dve", lambda: nc.vector.tensor_scalar(out=wgt[:], in0=tt1[:], scalar1=gs[:], scalar2=None, op0=ALU.mult), reads=[tt1, gs], writes=[wgt])
                kb.dma("sp", cx.WG_t[:, ti, :], wgt[:], reads=[wgt], writes=[], acc_writes=[cx.WG_t])


def stage_moe(cx, kb, nc, l, modv, yout=None, precast=True):
    with kb.scope():
        ones, eps = ln_setup(cx, kb, nc)
        gam, bet = load_ln_params(cx, kb, nc, l, 1)
        wgT = kb.sbuf("wgT", [32, T], F32)
        sel = kb.sbuf("sel", [32, 32, 128], F32)
        kb.dma("sp", sel[:], cx.m_sel[:], reads=[cx.m_sel], writes=[sel])
        ptr = [kb.psum("ptr%d" % i, [128, 512]) for i in range(2)]
        with kb.scope():
            wgl = kb.sbuf("wgl", [128, NT, 32], F32)
            id32 = kb.sbuf("id32", [128, 128], F32)
            kb.dma("sp", wgl[:], cx.WG_t[:], reads=[cx.WG_t], writes=[wgl])
            kb.dma("sp", id32[:], cx.m_ident32[:], reads=[cx.m_ident32], writes=[id32])
            for ti in range(NT):
                p_ = ptr[ti % 2]
                kb.mm([lambda p_=p_, ti=ti: nc.tensor.matmul(p_[:32, 0:128], lhsT=wgl[:, ti, :], rhs=id32[:], start=True, stop=True)],
                      reads=[wgl, id32], writes=[p_])
                kb.op("act", lambda p_=p_, ti=ti: nc.scalar.copy(out=wgT[:, ti * 128:(ti + 1) * 128], in_=p_[:32, 0:128]),
                      reads=[p_], writes=[], acc_writes=[wgT])
        if precast:
            precast_expert_weights(cx, kb, nc, l)
        hx = kb.sbuf("hxm", [128, 8, 512], BF16)
        yacc = kb.sbuf("yacc", [128, 8, 512], F32)
        hid = kb.sbuf("hid", [128, 16, 512], BF16)
        wdg = kb.sbuf("wdg", [128, 8, 2, 1024], BF16)
        NWB = 4
        wgs = [kb.sbuf("wgs%d" % i, [128, 8, 256], BF16) for i in range(NWB)]
        wus = [kb.sbuf("wus%d" % i, [128, 8, 256], BF16) for i in range(NWB)]
        xb = kb.sbuf("xbe", [128, 8, 512], F32)
        rsq = kb.sbuf("rsqe", [128, 8, 512], F32)
        mean = kb.sbuf("meane", [128, 512], F32)
        var = kb.sbuf("vare", [128, 512], F32)
        reps = [kb.sbuf("reps%d" % i, [128, 512], F32) for i in range(2)]
        ssb = [kb.sbuf("ssb%d" % i, [128, 512], F32) for i in range(2)]
        tsb = [kb.sbuf("tsb%d" % i, [128, 512], F32) for i in range(2)]
        ytmp = [kb.sbuf("ytmp%d" % i, [128, 512], F32) for i in range(2)]
        pg = [kb.psum("pg%d" % i, [128, 512]) for i in range(2)]
        pu = [kb.psum("pu%d" % i, [128, 512]) for i in range(2)]
        py = ptr
        ps1 = kb.psum("ps1e", [128, 512])
        ps2 = kb.psum("ps2e", [128, 512])
        prep = ps1
        nw = 0
        it = 0
        for bi, (t0, nb) in enumerate(BLKS):
            col = blkcol(t0)
            kb.dma("sp", hx[:, :, :nb], cx.HX2[:, t0:t0 + nb].rearrange("(kc p) t -> p kc t", p=128), reads=[cx.HX2], writes=[hx])
            kb.dma("sp", xb[:, :, :nb], cx.XT[:, t0:t0 + nb].rearrange("(kc p) t -> p kc t", p=128), reads=[cx.XT], writes=[xb])
            for g in range(4):
                kb.dma("sp", wdg[:].rearrange("p e hc d -> p (e hc d)"), cx.WDB[g], reads=[cx.WDB], writes=[wdg])
                for e_ in range(8):
                    e = g * 8 + e_
                    wg_, wu_ = wgs[nw % NWB], wus[nw % NWB]
                    nw += 1
                    kb.dma("sp", wg_[:].rearrange("p kc f -> p (kc f)"), cx.WGB[e], reads=[cx.WGB], writes=[wg_])
                    kb.dma("act", wu_[:].rearrange("p kc f -> p (kc f)"), cx.WUB[e], reads=[cx.WUB], writes=[wu_])
                    rp = reps[e % 2]
                    kb.mm([lambda e=e: nc.tensor.matmul(prep[:, :nb], lhsT=sel[:, e, :], rhs=wgT[:, t0:t0 + nb], start=True, stop=True)],
                          reads=[sel, wgT], writes=[prep])
                    kb.op("act", lambda rp=rp: nc.scalar.copy(out=rp[:, :nb], in_=prep[:, :nb]), reads=[prep], writes=[rp])
                    for hc in range(2):
                        a = it % 2
                        it += 1
                        kb.mm([(lambda kc=kc, a=a, hc=hc, wg_=wg_: nc.tensor.matmul(
                            pg[a][:, :nb], lhsT=wg_[:, kc, hc * 128:(hc + 1) * 128], rhs=hx[:, kc, :nb],
                            start=(kc == 0), stop=(kc == 7))) for kc in range(8)], reads=[wg_, hx], writes=[pg[a]])
                        kb.mm([(lambda kc=kc, a=a, hc=hc, wu_=wu_: nc.tensor.matmul(
                            pu[a][:, :nb], lhsT=wu_[:, kc, hc * 128:(hc + 1) * 128], rhs=hx[:, kc, :nb],
                            start=(kc == 0), stop=(kc == 7))) for kc in range(8)], reads=[wu_, hx], writes=[pu[a]])
                        kb.op("act", lambda a=a: nc.scalar.activation(out=ssb[a][:, :nb], in_=pg[a][:, :nb], func=AF.Silu),
                              reads=[pg[a]], writes=[ssb[a]])
                        kb.op("act", lambda a=a: nc.scalar.copy(out=tsb[a][:, :nb], in_=pu[a][:, :nb]), reads=[pu[a]], writes=[tsb[a]])
                        kb.op("dve", lambda a=a: nc.vector.tensor_tensor(out=tsb[a][:, :nb], in0=tsb[a][:, :nb], in1=ssb[a][:, :nb], op=ALU.mult),
                              reads=[tsb[a], ssb[a]], writes=[tsb[a]])
                        kb.op("pool", lambda a=a, rp=rp, e_=e_, hc=hc: nc.gpsimd.tensor_tensor(
                            out=hid[:, e_ * 2 + hc, :nb], in0=tsb[a][:, :nb], in1=rp[:, :nb], op=ALU.mult),
                            reads=[tsb[a], rp], writes=[], acc_writes=[hid])
                for oc in range(8):
                    p_ = py[oc % 2]
                    fns = []
                    for j in range(16):
                        fns.append(lambda j=j, oc=oc, p_=p_: nc.tensor.matmul(
                            p_[:, :nb], lhsT=wdg[:, j // 2, j % 2, oc * 128:(oc + 1) * 128], rhs=hid[:, j, :nb],
                            start=(j == 0), stop=(j == 15)))
                    kb.mm(fns, reads=[wdg, hid], writes=[p_])
                    if g == 0:
                        kb.op("act", lambda oc=oc, p_=p_: nc.scalar.copy(out=yacc[:, oc, :nb], in_=p_[:, :nb]), reads=[p_], writes=[], acc_writes=[yacc])
                    else:
                        yt = ytmp[oc % 2]
                        kb.op("act", lambda yt=yt, p_=p_: nc.scalar.copy(out=yt[:, :nb], in_=p_[:, :nb]), reads=[p_], writes=[yt])
                        eng = "dve" if oc % 2 == 0 else "pool"
                        h_ = nc.vector if eng == "dve" else nc.gpsimd
                        kb.op(eng, lambda h_=h_, oc=oc, yt=yt: h_.tensor_tensor(
                            out=yacc[:, oc, :nb], in0=yacc[:, oc, :nb], in1=yt[:, :nb], op=ALU.add),
                            reads=[yt, yacc], writes=[], acc_writes=[yacc])
            for oc in range(8):
                kb.op("pool", lambda oc=oc: nc.gpsimd.tensor_scalar(
                    out=yacc[:, oc, :nb], in0=yacc[:, oc, :nb], scalar1=modv[:, 40 + oc, col:col + 1], scalar2=None, op0=ALU.mult),
                    reads=[yacc, modv], writes=[yacc])
                kb.op("dve", lambda oc=oc: nc.vector.scalar_tensor_tensor(
                    out=yacc[:, oc, :nb], in0=xb[:, oc, :nb], scalar=ALPHA, in1=yacc[:, oc, :nb], op0=ALU.mult, op1=ALU.add),
                    reads=[xb, yacc], writes=[yacc])
            ln_block(kb, nc, yacc, nb, gam, bet, yacc, ones, eps, (rsq, ps1, ps2, mean, var))
            kb.dma("sp", cx.XT[:, t0:t0 + nb].rearrange("(kc p) t -> p kc t", p=128), yacc[:, :, :nb],
                   reads=[yacc], writes=[], acc_writes=[cx.XT])
            if yout is not None and t0 < TL:
                kb.dma("sp", yout[:, t0:t0 + nb].rearrange("(kc p) t -> p kc t", p=128), yacc[:, :, :nb],
                       reads=[yacc], writes=[], acc_writes=[yout])


def precast_expert_weights(cx, kb, nc, l):
    with kb.scope():
        stw = [kb.sbuf("pcw%d" % i, [128, 8, 256], BF16) for i in range(4)]
        std = [kb.sbuf("pcd%d" % i, [128, 16, 1024], BF16) for i in range(2)]
        n = 0
        for g in range(4):
            d_ = std[g % 2]
            kb.dma("pool", d_[:], cx.moe_w_down[l, g].rearrange("e (hc p) d -> p (e hc) d", p=128), reads=[cx.moe_w_down], writes=[d_])
            kb.dma("sp", cx.WDB[g], d_[:].rearrange("p j d -> p (j d)"), reads=[d_], writes=[], acc_writes=[cx.WDB])
            for e_ in range(8):
                e = g * 8 + e_
                for src, dst in ((cx.moe_w_gate, cx.WGB), (cx.moe_w_up, cx.WUB)):
                    w_ = stw[n % 4]
                    n += 1
                    kb.dma("pool", w_[:], src[l, g, e_].rearrange("(kc p) f -> p kc f", p=128), reads=[src], writes=[w_])
                    kb.dma("sp", dst[e], w_[:].rearrange("p kc f -> p (kc f)"), reads=[w_], writes=[], acc_writes=[dst])


def precast_gen(cx, kb, nc, l, stw, std):
    pend = []
    n = 0
    for g in range(4):
        d_ = std[g % 2]
        kb.dma("pool", d_[:], cx.moe_w_down[l, g].rearrange("e (hc p) d -> p (e hc) d", p=128), reads=[cx.moe_w_down], writes=[d_])
        pend.append((cx.WDB, g, d_, "p j d -> p (j d)"))
        yield
        for e_ in range(8):
            e = g * 8 + e_
            for src, dst in ((cx.moe_w_gate, cx.WGB), (cx.moe_w_up, cx.WUB)):
                w_ = stw[n % len(stw)]
                n += 1
                kb.dma("pool", w_[:], src[l, g, e_].rearrange("(kc p) f -> p kc f", p=128), reads=[src], writes=[w_])
                pend.append((dst, e, w_, "p kc f -> p (kc f)"))
                yield
                while len(pend) > 2:
                    dst_, idx_, buf_, pat_ = pend.pop(0)
                    kb.dma("sp", dst_[idx_], buf_[:].rearrange(pat_), reads=[buf_], writes=[], acc_writes=[dst_])
    while pend:
        dst_, idx_, buf_, pat_ = pend.pop(0)
        kb.dma("sp", dst_[idx_], buf_[:].rearrange(pat_), reads=[buf_], writes=[], acc_writes=[dst_])
        yield


def rope_tables():
    t = np.arange(TL)
    row = (t // 64).astype(np.float32)
    colp = (t % 64).astype(np.float32)
    nf = 16
    inv = (np.float32(10000.0) ** (-np.arange(nf, dtype=np.float32) / np.float32(nf))).astype(np.float32)
    cos = np.ones((128, T), np.float32)
    sin = np.zeros((128, T), np.float32)
    for q in range(128):
        d = q % 64
        pos = row if d < 32 else colp
        ang = (pos * inv[d % 16]).astype(np.float32)
        cos[q, :TL] = np.cos(ang).astype(np.float32)
        sin[q, :TL] = np.sin(ang).astype(np.float32)
    pt = np.zeros((128, 128), np.float32)
    for m in range(128):
        if (m % 32) < 16:
            pt[m + 16, m] = -1.0
        else:
            pt[m - 16, m] = 1.0
    return cos, sin, pt


_CONST = {}
_NAIDX = {}


def consts():
    if not _CONST:
        cos, sin, pt = rope_tables()
        _CONST.update(rope_cos=cos, rope_sin=sin, rope_pt=pt)
        _CONST.update(scan_masks())
        _CONST.update(wa_masks())
        sel = np.zeros((32, 32, 128), np.float32)
        for e in range(32):
            sel[e, e, :] = 1.0
        _CONST["m_sel"] = sel
        _CONST["m_ident32"] = np.eye(128, dtype=np.float32)
        _k, _valid, _ri, _ci = na_consts()
        _CONST["m_navalid"] = _valid
        _NAIDX["ri"] = _ri
        _NAIDX["ci"] = _ci
    return _CONST


def core_inputs(inp, b):
    f = lambda a: np.ascontiguousarray(a, dtype=np.float32)
    m = {}
    m["xin"] = f(np.concatenate([inp["x"][b].T, inp["ctx"][b].T], axis=1))
    cv = np.stack([inp["c"][b].reshape(8, 128).T, inp["c_ctx"].reshape(8, 128).T], axis=2)
    m["cvec"] = f(cv)
    m["w_mod"] = f(inp["w_mod"])
    m["b_modT"] = f(inp["b_mod"].reshape(2, 48, 128).transpose(0, 2, 1))
    m["w_in"] = f(inp["w_in"])
    m["hg_gamma"] = f(inp["hg_gamma"])
    m["gla_lr_w"] = f(inp["gla_lr_w"])
    m["gla_lr_b"] = f(inp["gla_lr_b"])
    m["wa_sink"] = f(inp["wa_sink"])
    for k in ("w_branch", "w_out", "moe_w_group", "moe_b_group", "moe_w_expert", "moe_b_expert",
              "moe_w_gate", "moe_w_up", "moe_w_down"):
        m[k] = f(inp[k])
    m["ln_gT"] = f(inp["ln_g"].reshape(2, 2, 8, 128).transpose(0, 1, 3, 2))
    m["ln_bT"] = f(inp["ln_b"].reshape(2, 2, 8, 128).transpose(0, 1, 3, 2))
    consts()
    m["na_bias"] = f(inp["na_rpb"][:, :, _NAIDX["ri"], _NAIDX["ci"]])
    m.update(consts())
    return m


FUSED = 1


def build_program(layers, final):
    nc = bass.Bass("TRN2", target_bir_lowering=False)
    cx = Ctx()
    declare_inputs(cx, nc)
    if final:
        yout = Buf(nc.dram_tensor("yout", [D, TL], F32, kind="ExternalOutput"), "yout")
    else:
        yout = None
    with ExitStack() as st:
        kb = KB(nc, st)
        declare_scratch(cx, kb, debug=False, as_output=(() if final else ("XT",)))
        modT = [kb.sbuf("modT%d" % l, [128, 48, 2], F32) for l in range(2)]
        stage_mod(cx, kb, nc, modT)
        first = True
        for l in layers:
            xsrc = cx.xin if first else cx.XT
            first = False
            with kb.scope():
                lbt = [kb.sbuf("lbt%d" % i, [128, 1024], F32) for i in range(2)]
                omlt = [kb.sbuf("omlt%d" % i, [128, 1024], F32) for i in range(2)]
                stage_lb(cx, kb, nc, lbt, omlt)
                hxT = kb.sbuf("hxT", [128, 8, T], BF16)
                stage_modulate(cx, kb, nc, xsrc, modT[l], 0, 1, hxT)
                stage_inproj_fm(cx, kb, nc, l, hxT)
                stage_inproj_tm(cx, kb, nc, l, hxT, lbt[l], omlt[l])
            stage_gla_gate(cx, kb, nc, l)
            stage_scan(cx, kb, nc, "gla")
            stage_scan(cx, kb, nc, "hgrn")
            stage_wa(cx, kb, nc, l)
            stage_na(cx, kb, nc, l, bg_factory=(lambda stw, std, l=l: precast_gen(cx, kb, nc, l, stw, std)))
            stage_merge(cx, kb, nc, l, xsrc, modT[l])
            stage_moe(cx, kb, nc, l, modT[l], yout if (final and l == layers[-1]) else None, precast=False)
        if final:
            kb.finish([yout])
        else:
            kb.finish([cx.XT])
    return nc


def kernel(**inputs):
    inp = {k: np.asarray(v) for k, v in inputs.items()}
    in_maps = [core_inputs(inp, b) for b in range(4)]
    if FUSED:
        nc = build_program([0, 1], True)
        res = run_bass_kernel_spmd(nc, in_maps, core_ids=list(range(4)))
    else:
        nc0 = build_program([0], False)
        res0 = run_bass_kernel_spmd(nc0, in_maps, core_ids=list(range(4)))
        for b in range(4):
            in_maps[b]["xin"] = np.ascontiguousarray(res0.results[b]["XT"])
        nc1 = build_program([1], True)
        res = run_bass_kernel_spmd(nc1, in_maps, core_ids=list(range(4)))
    out = np.stack([np.ascontiguousarray(res.results[b]["yout"].T) for b in range(4)], axis=0)
    return out.astype(np.float32)
```

```python
import numpy as np
from contextlib import ExitStack
import concourse.bass as bass
import concourse.mybir as mybir
from concourse.bass_utils import run_bass_kernel_spmd
import os

F32 = mybir.dt.float32
BF16 = mybir.dt.bfloat16
U32 = mybir.dt.uint32
AF = mybir.ActivationFunctionType
ALU = mybir.AluOpType
AX = mybir.AxisListType


class Buf:
    __slots__ = ("t", "w", "r", "name")

    def __init__(self, t, name=""):
        self.t = t
        self.w = []
        self.r = []
        self.name = name

    def __getitem__(self, idx):
        return self.t[idx]


class Dep:
    __slots__ = ("w", "r")

    def __init__(self):
        self.w = []
        self.r = []


class Eng:
    def __init__(self, name, h, sem):
        self.name = name
        self.h = h
        self.sem = sem
        self.count = 0
        self.seen = {}


class KB:
    def __init__(self, nc, stack, n_dma_sems=8):
        self.nc = nc
        self.stack = stack
        self.sems = {}
        self.engs = {}
        for name, h in (("pe", nc.tensor), ("act", nc.scalar), ("dve", nc.vector),
                        ("pool", nc.gpsimd), ("sp", nc.sync)):
            s = stack.enter_context(nc.semaphore("s_" + name))
            self.sems[id(s)] = s
            self.engs[name] = Eng(name, h, s)
        self.dma_pool = {}
        for q in ("sp", "pool", "act"):
            lst = []
            for i in range(n_dma_sems):
                s = stack.enter_context(nc.semaphore("d_%s%d" % (q, i)))
                self.sems[id(s)] = s
                lst.append([s, 0])
            self.dma_pool[q] = [lst, 0]
        self.ninstr = 0

    def scope(self):
        kb = self

        class _Scope:
            def __enter__(self_s):
                self_s.prev = kb.stack
                self_s.st = ExitStack()
                self_s.st.__enter__()
                kb.stack = self_s.st
                kb.nscope = getattr(kb, "nscope", 0) + 1
                return self_s

            def __exit__(self_s, *a):
                if a[0] is None:
                    kb.barrier()
                kb.stack = self_s.prev
                return self_s.st.__exit__(*a)
        return _Scope()

    def sbuf(self, name, shape, dtype):
        name = "%s_s%d" % (name, getattr(self, "nscope", 0))
        t = self.stack.enter_context(self.nc.sbuf_tensor(name, list(shape), dtype))
        return Buf(t, name)

    def psum(self, name, shape, dtype=F32):
        name = "%s_s%d" % (name, getattr(self, "nscope", 0))
        t = self.stack.enter_context(self.nc.psum_tensor(name, list(shape), dtype))
        return Buf(t, name)

    def dram(self, name, shape, dtype, kind="Internal"):
        t = self.nc.dram_tensor(name, list(shape), dtype, kind=kind)
        return Buf(t, name)

    def _wait(self, e, deps):
        need = {}
        for (sid, val) in deps:
            if need.get(sid, 0) < val:
                need[sid] = val
        for sid, val in need.items():
            if e.seen.get(sid, 0) < val:
                e.h.wait_ge(self.sems[sid], val)
                e.seen[sid] = val
                self.ninstr += 1

    def _collect(self, reads, writes, acc_writes=()):
        deps = []
        for b in reads:
            deps += b.w
        for b in writes:
            deps += b.w
            deps += b.r
        for b in acc_writes:
            deps += b.r
        return deps

    @staticmethod
    def _compact(lst):
        mx = {}
        for (sid, v) in lst:
            if mx.get(sid, 0) < v:
                mx[sid] = v
        return list(mx.items())

    def _commit(self, token, reads, writes, acc_writes=()):
        for b in acc_writes:
            b.w.append(token)
            if len(b.w) > 64:
                b.w = self._compact(b.w)
        for b in reads:
            b.r.append(token)
            if len(b.r) > 64:
                mx = {}
                for (sid, v) in b.r:
                    if mx.get(sid, 0) < v:
                        mx[sid] = v
                b.r = list(mx.items())
        for b in writes:
            b.w = [token]
            b.r = []

    def _cutcheck(self):
        import os
        cut = os.environ.get("KB_CUT")
        self.nops = getattr(self, "nops", 0) + 1
        skip = os.environ.get("KB_SKIP")
        if skip and self.nops in [int(x) for x in skip.split(",")]:
            return True
        return cut is not None and self.nops > int(cut)

    def op(self, eng, fn, reads=(), writes=(), same_engine_sync=True, acc_writes=()):
        if self._cutcheck():
            return None
        e = self.engs[eng]
        deps = self._collect(reads, writes, acc_writes)
        if not same_engine_sync:
            deps = [d for d in deps if d[0] != id(e.sem)]
        self._wait(e, deps)
        ins = fn()
        e.count += 1
        ins.then_inc(e.sem, 1)
        self.ninstr += 1
        token = (id(e.sem), e.count)
        self._commit(token, reads, writes, acc_writes)
        return token

    def mm(self, fns, reads=(), writes=()):
        if self._cutcheck():
            return None
        e = self.engs["pe"]
        deps = self._collect(reads, writes)
        deps = [d for d in deps if d[0] != id(e.sem)]
        self._wait(e, deps)
        ins = None
        for fn in fns:
            ins = fn()
            self.ninstr += 1
        e.count += 1
        ins.then_inc(e.sem, 1)
        token = (id(e.sem), e.count)
        self._commit(token, reads, writes)
        return token

    def dma(self, q, out, in_, reads=(), writes=(), acc_writes=(), **kw):
        e = self.engs[q]
        pool = self.dma_pool[q]
        lst, idx = pool
        ent = lst[idx % len(lst)]
        pool[1] = idx + 1
        s, total = ent
        deps = self._collect(reads, writes, acc_writes)
        deps.append((id(s), total))
        import os
        if os.environ.get("DMA_SERIAL") and getattr(self, "_last_dma", {}).get(q):
            deps.append(self._last_dma[q])
        self._wait(e, deps)
        ins = e.h.dma_start(out=out, in_=in_, **kw)
        ins.then_inc(s, 16)
        ent[1] = total + 16
        self.ninstr += 1
        token = (id(s), total + 16)
        if not hasattr(self, "_last_dma"):
            self._last_dma = {}
        self._last_dma[q] = token
        self._commit(token, reads, writes, acc_writes)
        return token

    def init_memory(self, nbytes=196608):
        nc = self.nc
        with self.scope():
            n = nbytes // 4
            big = self.sbuf("initbig", [128, n], F32)
            q = n // 4
            self.op("dve", lambda: nc.vector.memset(big[:, 0:q], 0.0), writes=[])
            self.op("pool", lambda: nc.gpsimd.memset(big[:, q:2 * q], 0.0), writes=[])
            self.op("dve", lambda: nc.vector.memset(big[:, 2 * q:3 * q], 0.0), writes=[])
            self.op("pool", lambda: nc.gpsimd.memset(big[:, 3 * q:n], 0.0), writes=[])
            pss = [self.psum("initps%d" % i, [128, 512]) for i in range(8)]
            for p in pss:
                self.op("dve", lambda p=p: nc.vector.memset(p[:], 0.0), writes=[])

    def barrier(self):
        allt = []
        for e in self.engs.values():
            if e.count:
                allt.append((id(e.sem), e.count))
        for q, (lst, _) in self.dma_pool.items():
            for s, total in lst:
                if total:
                    allt.append((id(s), total))
        for e in self.engs.values():
            self._wait(e, allt)

    def finish(self, tokens_bufs):
        e = self.engs["sp"]
        deps = []
        for b in tokens_bufs:
            deps += b.w
        self._wait(e, deps)

T = 4352
TL = 4096

D = 1024
TL = 4096
TC = 256
T = TL + TC
NT = T // 128
BLKS = [(i * 512, 512) for i in range(8)] + [(4096, 256)]
DIN = 10528
ALPHA = (2.0 * 2) ** 0.25

COLS = dict(a_q=(0, 256), a_k=(256, 256), a_v=(512, 512), a_g=(1024, 512), a_lr=(1536, 32),
            b_q=(1568, 512), b_k=(2080, 512), b_v=(2592, 512),
            c_q=(3104, 512), c_k=(3616, 128), c_v=(3744, 128),
            d_q=(3872, 512), d_f=(4384, 1024), d_i=(5408, 512), d_g=(5920, 512), gate=(6432, 4096))


def blkcol(t0):
    return 1 if t0 >= TL else 0


class Ctx:
    pass


def declare_inputs(cx, nc):
    def din(name, shape, dt=F32):
        return Buf(nc.dram_tensor(name, list(shape), dt, kind="ExternalInput"), name)
    cx.xin = din("xin", [D, T])
    cx.cvec = din("cvec", [128, 8, 2])
    cx.w_mod = din("w_mod", [2, D, 6 * D])
    cx.b_modT = din("b_modT", [2, 128, 48])
    cx.w_in = din("w_in", [2, D, DIN])
    cx.rope_cos = din("rope_cos", [128, T])
    cx.rope_sin = din("rope_sin", [128, T])
    cx.rope_pt = din("rope_pt", [128, 128])
    cx.hg_gamma = din("hg_gamma", [2, 2, 512])
    cx.gla_lr_w = din("gla_lr_w", [2, 2, 16, 256])
    cx.gla_lr_b = din("gla_lr_b", [2, 2, 256])
    for nm in ("mg", "mh"):
        setattr(cx, nm + "_mq", din(nm + "_mq", [128, 2, 130]))
        setattr(cx, nm + "_mk", din(nm + "_mk", [128, 2, 128]))
    cx.m_ma = din("m_ma", [128, 2, 128])
    cx.m_ident = din("m_ident", [128, 128])
    cx.m_mau = din("m_mau", [128, 2, 128], U32)
    cx.m_wa = din("m_wa", [128, 2, 128])
    cx.wa_sink = din("wa_sink", [2, 8])
    cx.m_navalid = din("m_navalid", [128, 21, 128])
    cx.w_branch = din("w_branch", [2, 4, 512, 1024])
    cx.w_out = din("w_out", [2, 1024, 1024])
    cx.ln_gT = din("ln_gT", [2, 2, 128, 8])
    cx.ln_bT = din("ln_bT", [2, 2, 128, 8])
    cx.moe_w_group = din("moe_w_group", [2, 1024, 4])
    cx.moe_b_group = din("moe_b_group", [2, 4])
    cx.moe_w_expert = din("moe_w_expert", [2, 1024, 32])
    cx.moe_b_expert = din("moe_b_expert", [2, 32])
    cx.moe_w_gate = din("moe_w_gate", [2, 4, 8, 1024, 256])
    cx.moe_w_up = din("moe_w_up", [2, 4, 8, 1024, 256])
    cx.moe_w_down = din("moe_w_down", [2, 4, 8, 256, 1024])
    cx.m_sel = din("m_sel", [32, 32, 128])
    cx.m_ident32 = din("m_ident32", [128, 128])
    cx.na_bias = din("na_bias", [2, 8, 128, 21, 128])


def declare_scratch(cx, kb, debug, as_input=(), as_output=()):
    kind = "ExternalOutput" if debug else "Internal"

    def ds(name, shape, dt):
        b = kb.dram(name, shape, dt, kind=("ExternalInput" if name in as_input else ("ExternalOutput" if name in as_output else kind)))
        setattr(cx, name, b)
        return b
    ds("XT", [D, T], F32)
    ds("QA", [256, T], BF16)
    ds("GA", [512, T], BF16)
    ds("LR1", [33, T], F32)
    ds("QB", [512, T], BF16)
    ds("KB_", [512, T], BF16)
    ds("QC", [512, T], BF16)
    ds("KC", [128, T], BF16)
    ds("QD", [512, T], BF16)
    ds("GD", [512, T], BF16)
    ds("GATE", [4096, T], BF16)
    ds("KVA_t", [128, NT, 768], BF16)
    ds("VB_t", [128, NT, 512], BF16)
    ds("VC_t", [128, NT, 128], BF16)
    ds("VD_t", [128, NT, 512], BF16)
    ds("LGD_t", [128, NT, 1024], F32)
    ds("KD_t", [128, NT, 1024], BF16)
    ds("LGA_t", [128, NT, 512], F32)
    ds("MODT", [2, 128, 96], F32)
    ds("HX2", [D, T], BF16)
    ds("WGB", [32, 128, 2048], BF16)
    ds("WUB", [32, 128, 2048], BF16)
    ds("WDB", [4, 128, 16384], BF16)
    ds("WG_t", [128, NT, 32], F32)
    ds("WGT", [32, T], F32)
    ds("BR_A", [512, T], BF16)
    ds("BR_B", [512, T], BF16)
    ds("BR_C", [512, T], BF16)
    ds("BR_D", [512, T], BF16)


def stage_mod(cx, kb, nc, modT):
    with kb.scope():
        cv = kb.sbuf("cv", [128, 8, 2], F32)
        sc = kb.sbuf("sc", [128, 8, 2], F32)
        kb.dma("sp", cv[:], cx.cvec[:], reads=[cx.cvec], writes=[cv])
        kb.op("act", lambda: nc.scalar.activation(out=sc[:], in_=cv[:], func=AF.Silu), reads=[cv], writes=[sc])
        wbufs = [kb.sbuf("wm%d" % i, [128, 8, 1024], F32) for i in range(2)]
        bm = kb.sbuf("bm", [128, 48], F32)
        ps = kb.psum("ps_mod", [128, 96])
        wi = 0
        for l in range(2):
            kb.dma("sp", bm[:], cx.b_modT[l], reads=[cx.b_modT], writes=[bm])
            for g in range(6):
                wb = wbufs[wi % 2]
                wi += 1
                src = cx.w_mod[l, :, g * 1024:(g + 1) * 1024].rearrange("(kc p) c -> p kc c", p=128)
                kb.dma("sp", wb[:], src, reads=[cx.w_mod], writes=[wb])
                for j in range(8):
                    jj = g * 8 + j
                    fns = []
                    for kc in range(8):
                        fns.append(lambda kc=kc, j=j, jj=jj, wb=wb: nc.tensor.matmul(
                            ps[:, 2 * jj:2 * jj + 2], lhsT=wb[:, kc, j * 128:(j + 1) * 128], rhs=sc[:, kc, :],
                            start=(kc == 0), stop=(kc == 7)))
                    kb.mm(fns, reads=[wb, sc], writes=[ps])
            m = modT[l]
            kb.op("dve", lambda m=m: nc.vector.tensor_tensor(
                out=m[:], in0=ps[:].rearrange("p (j c) -> p j c", c=2),
                in1=bm[:].unsqueeze(2).broadcast_to([128, 48, 2]), op=ALU.add),
                reads=[ps, bm], writes=[m])
            kb.dma("sp", cx.MODT[l], m[:].rearrange("p j c -> p (j c)"), reads=[m], writes=[cx.MODT])


def stage_modulate(cx, kb, nc, src_dram, modv, shift_idx, scale_idx, hxT, hx32=None):
    with kb.scope():
        onep = kb.sbuf("onep", [128, 8, 2], F32)
        kb.op("dve", lambda: nc.vector.tensor_scalar(
            out=onep[:], in0=modv[:, scale_idx * 8:(scale_idx + 1) * 8, :], scalar1=1.0, scalar2=None, op0=ALU.add),
            reads=[modv], writes=[onep])
        xb = [kb.sbuf("xb%d" % i, [128, 8, 512], F32) for i in range(2)]
        for bi, (t0, nb) in enumerate(BLKS):
            col = blkcol(t0)
            x = xb[bi % 2]
            kb.dma("sp", x[:, :, :nb], src_dram[:, t0:t0 + nb].rearrange("(kc p) t -> p kc t", p=128),
                   reads=[src_dram], writes=[x])
            for kc in range(8):
                eng = "dve" if kc % 2 == 0 else "pool"
                h = nc.vector if eng == "dve" else nc.gpsimd
                kb.op(eng, lambda h=h, kc=kc, x=x: h.tensor_scalar(
                    out=hxT[:, kc, t0:t0 + nb], in0=x[:, kc, :nb],
                    scalar1=onep[:, kc, col:col + 1], scalar2=modv[:, shift_idx * 8 + kc, col:col + 1],
                    op0=ALU.mult, op1=ALU.add), reads=[x, onep, modv], writes=[hxT])


FM_GROUPS = [
    ("a_q", "QA", "copy"), ("a_g", "GA", "silu"), ("a_lr", "LR1", "copy32"),
    ("b_q", "QB", "copy"), ("b_k", "KB_", "copy"),
    ("c_q", "QC", "rope"), ("c_k", "KC", "rope"),
    ("d_q", "QD", "silu"), ("d_g", "GD", "silu"), ("gate", "GATE", "sigmoid"),
]


def stage_inproj_fm(cx, kb, nc, l, hxT, only=None):
    with kb.scope():
        wsl = [kb.sbuf("wsl%d" % i, [128, 8, 512], BF16) for i in range(2)]
        stg = [kb.sbuf("stg%d" % i, [128, T], BF16) for i in range(2)]
        stg32 = kb.sbuf("stg32", [32, T], F32)
        pss = [kb.psum("psf%d" % i, [128, 512]) for i in range(4)]
        psr = [kb.psum("psr%d" % i, [128, 512]) for i in range(2)]
        cosT = kb.sbuf("cosT", [128, T], F32)
        sinT = kb.sbuf("sinT", [128, T], F32)
        ptm = kb.sbuf("ptm", [128, 128], BF16)
        tmpb = [kb.sbuf("tmpb%d" % i, [128, 512], BF16) for i in range(2)]
        t1 = [kb.sbuf("t1_%d" % i, [128, 512], F32) for i in range(2)]
        t2 = [kb.sbuf("t2_%d" % i, [128, 512], F32) for i in range(2)]
        ones = kb.sbuf("ones1", [1, T], F32)
        kb.dma("sp", cosT[:], cx.rope_cos[:], reads=[cx.rope_cos], writes=[cosT])
        kb.dma("sp", sinT[:], cx.rope_sin[:], reads=[cx.rope_sin], writes=[sinT])
        kb.dma("pool", ptm[:], cx.rope_pt[:], reads=[cx.rope_pt], writes=[ptm])
        kb.op("dve", lambda: nc.vector.memset(ones[:], 1.0), writes=[ones])
        kb.dma("sp", cx.LR1[32:33, :], ones[:], reads=[ones], writes=[], acc_writes=[cx.LR1])
        nsl = 0
        nps = 0
        nst = 0
        nrp = 0
        for (name, dest, kind) in FM_GROUPS:
            if only is not None and name not in only:
                continue
            c0, nc_ = COLS[name]
            dbuf = getattr(cx, dest)
            for s0 in range(0, nc_, 512):
                sw = min(512, nc_ - s0)
                w = wsl[nsl % 2]
                nsl += 1
                kb.dma("pool", w[:, :, :sw],
                       cx.w_in[l, :, c0 + s0:c0 + s0 + sw].rearrange("(kc p) c -> p kc c", p=128),
                       reads=[cx.w_in], writes=[w])
                for m0 in range(0, sw, 128):
                    mw = min(128, sw - m0)
                    if kind == "copy32":
                        st = stg32
                    else:
                        st = stg[nst % 2]
                        nst += 1
                    for bi, (t0, nb) in enumerate(BLKS):
                        ps = pss[nps % 4]
                        nps += 1
                        fns = [(lambda kc=kc, ps=ps, w=w: nc.tensor.matmul(
                            ps[:mw, :nb], lhsT=w[:, kc, m0:m0 + mw], rhs=hxT[:, kc, t0:t0 + nb],
                            start=(kc == 0), stop=(kc == 7))) for kc in range(8)]
                        kb.mm(fns, reads=[w, hxT], writes=[ps])
                        o = st[:mw, t0:t0 + nb]
                        if kind in ("copy", "copy32"):
                            if nps % 2 == 0:
                                kb.op("dve", lambda ps=ps, o=o: nc.vector.tensor_copy(out=o, in_=ps[:mw, :nb]),
                                      reads=[ps], writes=[st])
                            else:
                                kb.op("act", lambda ps=ps, o=o: nc.scalar.copy(out=o, in_=ps[:mw, :nb]),
                                      reads=[ps], writes=[st])
                        elif kind == "silu":
                            kb.op("act", lambda ps=ps, o=o: nc.scalar.activation(out=o, in_=ps[:mw, :nb], func=AF.Silu),
                                  reads=[ps], writes=[st])
                        elif kind == "sigmoid":
                            kb.op("act", lambda ps=ps, o=o: nc.scalar.activation(out=o, in_=ps[:mw, :nb], func=AF.Sigmoid),
                                  reads=[ps], writes=[st])
                        elif kind == "rope":
                            tb = tmpb[nrp % 2]
                            a1 = t1[nrp % 2]
                            a2 = t2[nrp % 2]
                            pr = psr[nrp % 2]
                            nrp += 1
                            kb.op("act", lambda ps=ps, tb=tb: nc.scalar.copy(out=tb[:mw, :nb], in_=ps[:mw, :nb]),
                                  reads=[ps], writes=[tb])
                            kb.mm([lambda pr=pr, tb=tb: nc.tensor.matmul(pr[:mw, :nb], lhsT=ptm[:mw, :mw], rhs=tb[:mw, :nb],
                                                                         start=True, stop=True)],
                                  reads=[ptm, tb], writes=[pr])
                            kb.op("dve", lambda tb=tb, a1=a1: nc.vector.tensor_tensor(
                                out=a1[:mw, :nb], in0=tb[:mw, :nb], in1=cosT[:mw, t0:t0 + nb], op=ALU.mult),
                                reads=[tb, cosT], writes=[a1])
                            kb.op("dve", lambda pr=pr, a2=a2: nc.vector.tensor_tensor(
                                out=a2[:mw, :nb], in0=pr[:mw, :nb], in1=sinT[:mw, t0:t0 + nb], op=ALU.mult),
                                reads=[pr, sinT], writes=[a2])
                            kb.op("pool", lambda a1=a1, a2=a2, o=o: nc.gpsimd.tensor_tensor(
                                out=o, in0=a1[:mw, :nb], in1=a2[:mw, :nb], op=ALU.add),
                                reads=[a1, a2], writes=[st])
                    kb.dma("sp", dbuf[s0 + m0:s0 + m0 + mw, :], st[:mw, :], reads=[st], writes=[], acc_writes=[dbuf])


TM_GROUPS = [
    (256, 512, "KVA_t", 0, "copy"), (768, 256, "KVA_t", 512, "copy"),
    (2592, 512, "VB_t", 0, "copy"), (3744, 128, "VC_t", 0, "copy"),
    (5408, 512, "VD_t", 0, "copy"),
    (4384, 512, None, 0, "hgf"), (4896, 512, None, 512, "hgf"),
]


def stage_inproj_tm(cx, kb, nc, l, hxT, lbt, omlt, only=None):
    with kb.scope():
        wsl = [kb.sbuf("wtl%d" % i, [128, 8, 512], BF16) for i in range(2)]
        pss = [kb.psum("pst%d" % i, [128, 512]) for i in range(4)]
        stb = [kb.sbuf("stb%d" % i, [128, 512], BF16) for i in range(4)]
        e1 = [kb.sbuf("e1_%d" % i, [128, 512], F32) for i in range(2)]
        r1 = [kb.sbuf("r1_%d" % i, [128, 512], F32) for i in range(2)]
        lg = [kb.sbuf("lg_%d" % i, [128, 512], F32) for i in range(2)]
        kk = [kb.sbuf("kk_%d" % i, [128, 512], BF16) for i in range(2)]
        nsl = nps = nst = nh = 0
        for (c0, ncol, dest, doff, kind) in TM_GROUPS:
            if only is not None and kind not in only:
                continue
            w = wsl[nsl % 2]
            nsl += 1
            kb.dma("pool", w[:, :, :ncol], cx.w_in[l, :, c0:c0 + ncol].rearrange("(kc p) c -> p kc c", p=128),
                   reads=[cx.w_in], writes=[w])
            for ti in range(NT):
                ts = slice(ti * 128, (ti + 1) * 128)
                ps = pss[nps % 4]
                nps += 1
                fns = [(lambda kc=kc, ps=ps, w=w: nc.tensor.matmul(
                    ps[:, :ncol], lhsT=hxT[:, kc, ts], rhs=w[:, kc, :ncol],
                    start=(kc == 0), stop=(kc == 7))) for kc in range(8)]
                kb.mm(fns, reads=[w, hxT], writes=[ps])
                if kind == "copy":
                    st = stb[nst % 4]
                    nst += 1
                    if nst % 2 == 0:
                        kb.op("dve", lambda ps=ps, st=st: nc.vector.tensor_copy(out=st[:, :ncol], in_=ps[:, :ncol]),
                              reads=[ps], writes=[st])
                    else:
                        kb.op("act", lambda ps=ps, st=st: nc.scalar.copy(out=st[:, :ncol], in_=ps[:, :ncol]),
                              reads=[ps], writes=[st])
                    db = getattr(cx, dest)
                    kb.dma("sp", db[:, ti, doff:doff + ncol], st[:, :ncol], reads=[st], writes=[], acc_writes=[db])
                else:
                    e = e1[nh % 2]
                    r = r1[nh % 2]
                    g = lg[nh % 2]
                    k = kk[nh % 2]
                    nh += 1
                    cs = slice(doff, doff + 512)
                    kb.op("act", lambda ps=ps, e=e: nc.scalar.activation(out=e[:], in_=ps[:], func=AF.Exp, scale=-1.0),
                          reads=[ps], writes=[e])
                    kb.op("pool", lambda e=e: nc.gpsimd.tensor_scalar(
                        out=e[:], in0=e[:], scalar1=1.0, scalar2=None, op0=ALU.add),
                        reads=[e], writes=[e])
                    kb.op("dve", lambda e=e, r=r: nc.vector.reciprocal(out=r[:], in_=e[:]),
                          reads=[e], writes=[r])
                    kb.op("dve", lambda r=r: nc.vector.tensor_tensor(out=r[:], in0=r[:], in1=omlt[:, cs], op=ALU.mult),
                          reads=[r, omlt], writes=[r])
                    kb.op("pool", lambda r=r: nc.gpsimd.tensor_tensor(out=r[:], in0=r[:], in1=lbt[:, cs], op=ALU.add),
                          reads=[r, lbt], writes=[r])
                    kb.op("act", lambda r=r, g=g: nc.scalar.activation(out=g[:], in_=r[:], func=AF.Ln),
                          reads=[r], writes=[g])
                    kb.op("pool", lambda r=r, k=k: nc.gpsimd.tensor_scalar(
                        out=k[:], in0=r[:], scalar1=-1.0, scalar2=1.0, op0=ALU.mult, op1=ALU.add),
                        reads=[r], writes=[k])
                    kb.dma("sp", cx.LGD_t[:, ti, cs], g[:], reads=[g], writes=[], acc_writes=[cx.LGD_t])
                    kb.dma("sp", cx.KD_t[:, ti, cs], k[:], reads=[k], writes=[], acc_writes=[cx.KD_t])


def stage_lb(cx, kb, nc, lbt, omlt):
    with kb.scope():
        gam = kb.sbuf("gam", [128, 2, 2, 512], F32)
        kb.dma("sp", gam[:].rearrange("p a b c -> p (a b c)"),
               cx.hg_gamma[:].rearrange("a b c -> (a b c)").partition_broadcast(128),
               reads=[cx.hg_gamma], writes=[gam])
        d = kb.sbuf("gamd", [128, 2, 512], F32)
        kb.op("dve", lambda: nc.vector.tensor_tensor(out=d[:], in0=gam[:, :, 0, :], in1=gam[:, :, 1, :], op=ALU.subtract),
              reads=[gam], writes=[d])
        kb.op("act", lambda: nc.scalar.activation(out=d[:], in_=d[:], func=AF.Exp), reads=[d], writes=[d])
        l1 = lbt[1]
        kb.op("dve", lambda: nc.vector.tensor_scalar(
            out=d[:], in0=d[:], scalar1=1.0, scalar2=None, op0=ALU.add), reads=[d], writes=[d])
        kb.op("dve", lambda: nc.vector.reciprocal(out=l1[:], in_=d[:].rearrange("p a c -> p (a c)")),
              reads=[d], writes=[l1])
        l0 = lbt[0]
        kb.op("dve", lambda: nc.vector.memset(l0[:], 0.0), writes=[l0])
        for l in range(2):
            o = omlt[l]
            lb = lbt[l]
            kb.op("dve", lambda o=o, lb=lb: nc.vector.tensor_scalar(
                out=o[:], in0=lb[:], scalar1=-1.0, scalar2=1.0, op0=ALU.mult, op1=ALU.add),
                reads=[lb], writes=[o])


def stage_gla_gate(cx, kb, nc, l):
    with kb.scope():
        wlr = kb.sbuf("wlr", [33, 512], F32)
        lr1 = kb.sbuf("lr1", [33, T], F32)
        kb.op("dve", lambda: nc.vector.memset(wlr[:], 0.0), writes=[wlr])
        kb.dma("sp", wlr[0:16, 0:256], cx.gla_lr_w[l, 0], reads=[cx.gla_lr_w], writes=[wlr])
        kb.dma("sp", wlr[16:32, 256:512], cx.gla_lr_w[l, 1], reads=[cx.gla_lr_w], writes=[wlr])
        kb.dma("sp", wlr[32:33, :], cx.gla_lr_b[l].rearrange("a c -> (a c)").unsqueeze(0),
               reads=[cx.gla_lr_b], writes=[wlr])
        kb.dma("sp", lr1[:], cx.LR1[:], reads=[cx.LR1], writes=[lr1])
        pss = [kb.psum("psg%d" % i, [128, 512]) for i in range(2)]
        ee = [kb.sbuf("ge%d" % i, [128, 512], F32) for i in range(2)]
        gg = [kb.sbuf("gg%d" % i, [128, 512], F32) for i in range(2)]
        for ti in range(NT):
            ts = slice(ti * 128, (ti + 1) * 128)
            ps = pss[ti % 2]
            e = ee[ti % 2]
            g = gg[ti % 2]
            kb.mm([lambda ps=ps: nc.tensor.matmul(ps[:], lhsT=lr1[:, ts], rhs=wlr[:], start=True, stop=True)],
                  reads=[lr1, wlr], writes=[ps])
            kb.op("act", lambda ps=ps, e=e: nc.scalar.activation(out=e[:], in_=ps[:], func=AF.Exp, scale=-1.0),
                  reads=[ps], writes=[e])
            kb.op("act", lambda e=e, g=g: nc.scalar.activation(out=g[:], in_=e[:], func=AF.Ln, bias=1.0),
                  reads=[e], writes=[g])
            kb.dma("sp", cx.LGA_t[:, ti, :], g[:], reads=[g], writes=[], acc_writes=[cx.LGA_t])

SCAN_CFG = {
    "gla": dict(H=4, dk=64, qsrc="QA", qscale=0.125, ksrc="KVA_t", kz=0, kcol0=0, vsrc="KVA_t", vcol0=256,
                gsrc="LGA_t", gz=256, gate="GA", dest="BR_A", mask="mg"),
    "hgrn": dict(H=4, dk=128, qsrc="QD", qscale=1.0, ksrc="KD_t", kz=512, kcol0=0, vsrc="VD_t", vcol0=0,
                 gsrc="LGD_t", gz=512, gate="GD", dest="BR_D", mask="mh"),
}


def view(b):
    return Buf(b.t, b.name + "_v")


def stage_scan(cx, kb, nc, which, heads=None):
    cfg = SCAN_CFG[which]
    H, dk = cfg["H"], cfg["dk"]
    dv = 128
    qsrc, ksrc, vsrc, gsrc = (getattr(cx, cfg[k]) for k in ("qsrc", "ksrc", "vsrc", "gsrc"))
    gate, dest = getattr(cx, cfg["gate"]), getattr(cx, cfg["dest"])
    mq_d, mk_d = getattr(cx, cfg["mask"] + "_mq"), getattr(cx, cfg["mask"] + "_mk")
    with kb.scope():
        mq = kb.sbuf("mq", [128, 2, 130], F32)
        mk = kb.sbuf("mk", [128, 2, 128], F32)
        ma = kb.sbuf("ma", [128, 2, 128], F32)
        ident = kb.sbuf("ident", [128, 128], BF16)
        onesdv = kb.sbuf("onesdv", [128, 128], F32)
        kb.dma("sp", mq[:], mq_d[:], reads=[mq_d], writes=[mq])
        kb.dma("sp", mk[:], mk_d[:], reads=[mk_d], writes=[mk])
        kb.dma("sp", ma[:], cx.m_ma[:], reads=[cx.m_ma], writes=[ma])
        mau = kb.sbuf("mau", [128, 2, 128], U32)
        kb.dma("sp", mau[:], cx.m_mau[:], reads=[cx.m_mau], writes=[mau])
        kb.dma("pool", ident[:], cx.m_ident[:], reads=[cx.m_ident], writes=[ident])
        kb.op("dve", lambda: nc.vector.memset(onesdv[:], 1.0 / dv), writes=[onesdv])
        vt = kb.sbuf("vt", [128, NT, dv], BF16)
        qT = kb.sbuf("qT", [128, T], BF16)
        gsil = kb.sbuf("gsil", [128, T], BF16)
        obr = kb.sbuf("obr", [128, T], BF16)
        gts = [kb.sbuf("gt%d" % i, [128, NT, dk], F32) for i in range(2)]
        kts = [kb.sbuf("kt%d" % i, [128, NT, dk], BF16) for i in range(2)]
        qt = [kb.sbuf("qt%d" % z, [128, T], BF16) for z in range(2)]
        atm = [kb.sbuf("atm%d" % z, [128, NT, 128], BF16) for z in range(2)]
        Sp = [kb.sbuf("Sp%d" % z, [128, 2 * NT, dv], BF16) for z in range(2)]
        for z in range(2):
            kb.op("pool", lambda z=z: nc.gpsimd.memset(atm[z][:], 0.0), writes=[atm[z]])
        Sst = [kb.sbuf("Sst%d" % i, [128, dv], F32) for i in range(2)]
        R = 3
        eq = [kb.sbuf("eq%d" % i, [128, 128], F32) for i in range(R)]
        ek = [kb.sbuf("ek%d" % i, [128, 128], F32) for i in range(R)]
        ekh = [kb.sbuf("ekh%d" % i, [128, 128], F32) for i in range(R)]
        ktl = [kb.sbuf("ktl%d" % i, [128, 128], BF16) for i in range(R)]
        khat = [kb.sbuf("khat%d" % i, [128, 256], BF16) for i in range(R)]
        ktr = [kb.sbuf("ktr%d" % i, [128, 128], F32) for i in range(R)]
        atr = [kb.sbuf("atr%d" % i, [128, 128], F32) for i in range(R)]
        usb = [kb.sbuf("usb%d" % i, [128, 256], F32) for i in range(R)]
        osb = [kb.sbuf("osb%d" % i, [128, 512], F32) for i in range(2)]
        cm = kb.sbuf("cm", [128, 2], F32)
        kb.op("dve", lambda: nc.vector.memset(cm[:], 0.0), writes=[cm])
        kb.op("dve", lambda: nc.vector.memset(cm[0:64, 0:1], 1.0), writes=[cm])
        kb.op("dve", lambda: nc.vector.memset(cm[64:128, 1:2], 1.0), writes=[cm])
        er2 = [kb.sbuf("er%d" % i, [128, 2], F32) for i in range(R)]
        ed2 = [kb.sbuf("ed%d" % i, [128, 2], F32) for i in range(R)]
        psA = [kb.psum("psA%d" % i, [128, 512]) for i in range(2)]
        psB = [kb.psum("psB%d" % i, [128, 512]) for i in range(2)]
        psA_ec = [view(p) for p in psA]
        import os
        if True:
            psK = [kb.psum("psK%d" % i, [128, 512]) for i in range(2)]
        else:
            psK = psA
        psA_kt = [view(p) for p in psK]
        psA_e2 = [view(p) for p in psK]
        psB_u = [view(p) for p in psB]
        psB_at = [view(p) for p in psB]
        psO = [kb.psum("psO%d" % i, [128, 512]) for i in range(1)] * 2
        psM = [kb.psum("psM%d" % i, [128, 512]) for i in range(1)] * 2
        sq = [kb.sbuf("sq%d" % i, [128, 512], F32) for i in range(2)]
        sd = [kb.sbuf("sd%d" % i, [128, 512], F32) for i in range(2)]
        on = [kb.sbuf("on%d" % i, [128, 512], F32) for i in range(2)]
        eps_t = kb.sbuf("eps_t", [128, 1], F32)
        kb.op("dve", lambda: nc.vector.memset(eps_t[:], 1e-6), writes=[eps_t])

        it = 0
        ghz = 0
        import os
        for h in (range(H) if heads is None else heads):
            vc0 = cfg["vcol0"] + h * dv
            kb.dma("sp", vt[:], vsrc[:, :, vc0:vc0 + dv], reads=[vsrc], writes=[vt])
            kb.dma("sp", qT[:dk, :], qsrc[h * dk:(h + 1) * dk, :], reads=[qsrc], writes=[qT])
            kb.dma("sp", gsil[:], gate[h * dv:(h + 1) * dv, :], reads=[gate], writes=[gsil])
            for z in range(2):
                gt = gts[ghz % 2]
                kt = kts[ghz % 2]
                ghz += 1
                gc0 = z * cfg["gz"] + h * dk
                kc0 = cfg["kcol0"] + z * cfg["kz"] + h * dk
                kb.dma("sp", gt[:], gsrc[:, :, gc0:gc0 + dk], reads=[gsrc], writes=[gt])
                if os.environ.get("KTMEMSET"):
                    kb.op("dve", lambda kt=kt: nc.vector.memset(kt[:], 0.0), writes=[kt])
                kb.dma("sp", kt[:], ksrc[:, :, kc0:kc0 + dk], reads=[ksrc], writes=[kt])
                order = [32, 33] + list(range(32)) if z == 0 else [33, 32] + list(range(31, -1, -1))
                import os
                DBG = os.environ.get("SCAN_DBG", "")
                if DBG == "loads":
                    continue
                if DBG.startswith("nt"):
                    order = order[:int(DBG[2:])]
                S = Sst[0]
                kb.op("dve", lambda S=S: nc.vector.memset(S[:], 0.0), writes=[S])
                si = 0
                def p1(ti, a, r):
                    ts = slice(ti * 128, (ti + 1) * 128)
                    pa, pb = psA[a], psB[a]
                    pk = psK[a]
                    kb.mm([lambda pa=pa, gt=gt: nc.tensor.matmul(pa[:dk, 0:130], lhsT=gt[:, ti, :], rhs=mq[:, z, :],
                                                                 start=True, stop=True)],
                          reads=[gt, mq], writes=[psA_ec[a]])
                    kb.mm([lambda pk=pk, kt=kt: nc.tensor.matmul(pk[:dk, 256:384], lhsT=(ident[:, :dk] if os.environ.get("A2ID") else kt[:, ti, :]), rhs=ident[:],
                                                                 start=True, stop=True)],
                          reads=[kt, ident], writes=[psA_kt[a]])
                    kb.mm([lambda pk=pk, gt=gt: nc.tensor.matmul(pk[:, 384:384 + dk], lhsT=mk[:, z, :], rhs=gt[:, ti, :],
                                                                 start=True, stop=True)],
                          reads=[gt, mk], writes=[psA_e2[a]])
                    kb.op("act", lambda pa=pa, o=eq[r]: nc.scalar.activation(out=o[:dk, :], in_=pa[:dk, 0:128], func=AF.Exp),
                          reads=[psA_ec[a]], writes=[eq[r]])
                    kb.op("act", lambda pa=pa, o=ek[r]: nc.scalar.activation(out=o[:dk, :], in_=pa[:dk, 0:128], func=AF.Exp, scale=-1.0),
                          reads=[psA_ec[a]], writes=[ek[r]])
                    kb.op("act", lambda pa=pa, o=er2[r]: nc.scalar.activation(out=o[:dk, :], in_=pa[:dk, 128:130], func=AF.Exp, scale=0.5),
                          reads=[psA_ec[a]], writes=[er2[r]])
                    kb.op("act", lambda pa=pa, o=ed2[r]: nc.scalar.activation(out=o[:dk, :], in_=pa[:dk, 128:130], func=AF.Exp),
                          reads=[psA_ec[a]], writes=[ed2[r]])
                    kb.op("act", lambda pk=pk, o=ekh[r]: nc.scalar.activation(out=o[:, :dk], in_=pk[:, 384:384 + dk], func=AF.Exp),
                          reads=[psA_e2[a]], writes=[ekh[r]])
                    qz = qt[z]
                    kb.op("dve", lambda qz=qz, e=eq[r]: nc.vector.scalar_tensor_tensor(
                        out=qz[:dk, ts], in0=qT[:dk, ts], scalar=cfg["qscale"], in1=e[:dk, :], op0=ALU.mult, op1=ALU.mult),
                        reads=[qT, eq[r]], writes=[], acc_writes=[qz])
                    kb.op("act", lambda pk=pk, o=ktr[r]: nc.scalar.copy(out=o[:dk, :], in_=pk[:dk, 256:384]),
                          reads=[psA_kt[a]], writes=[ktr[r]])
                    kb.op("dve", lambda e=ek[r], o=ktl[r], kr=ktr[r]: nc.vector.tensor_tensor(
                        out=o[:dk, :], in0=kr[:dk, :], in1=e[:dk, :], op=ALU.mult),
                        reads=[ktr[r], ek[r]], writes=[ktl[r]])
                    for c in range(2):
                        kb.op("dve", lambda kt=kt, e=ekh[r], o=khat[r], c=c: nc.vector.scalar_tensor_tensor(
                            out=o[:, c * 128:c * 128 + dk], in0=kt[:, ti, :], scalar=cm[:, c:c + 1], in1=e[:, :dk],
                            op0=ALU.mult, op1=ALU.mult),
                            reads=[kt, ekh[r], cm], writes=[], acc_writes=[khat[r]])
                    return None

                def p2(ti, a, r, S, si):
                    ts = slice(ti * 128, (ti + 1) * 128)
                    pa, pb = psA[a], psB[a]
                    qz = qt[z]
                    kb.mm([(lambda c=c, pb=pb, kh=khat[r]: nc.tensor.matmul(
                        pb[:dk, c * 128:(c + 1) * 128], lhsT=kh[:, c * 128:c * 128 + dk],
                        rhs=vt[:, ti, :], start=True, stop=True)) for c in range(2)],
                        reads=[khat[r], vt], writes=[psB_u[a]])
                    kb.mm([lambda pb=pb, k_=ktl[r], qz=qz: nc.tensor.matmul(
                        pb[:, 256:384], lhsT=k_[:dk, :], rhs=qz[:dk, ts], start=True, stop=True)],
                        reads=[ktl[r], qz], writes=[psB_at[a]])
                    az = atm[z]
                    kb.op("act", lambda pb=pb, o=atr[r]: nc.scalar.copy(out=o[:], in_=pb[:, 256:384]),
                          reads=[psB_at[a]], writes=[atr[r]])
                    kb.op("dve", lambda az=az, o=atr[r]: nc.vector.copy_predicated(
                        out=az[:, ti, :], mask=mau[:, z, :], data=o[:]),
                        reads=[atr[r], mau], writes=[], acc_writes=[az])
                    kb.op("act", lambda pb=pb, o=usb[r]: nc.scalar.copy(out=o[:dk, :], in_=pb[:dk, 0:256]),
                          reads=[psB_u[a]], writes=[usb[r]])
                    spz = Sp[z]
                    for c in ((0, 1) if z == 0 else (1, 0)):
                        ci = 2 * ti + c
                        kb.op("act", lambda S=S, spz=spz, ci=ci, e=er2[r], c=c: nc.scalar.activation(
                            out=spz[:dk, ci, :], in_=S[:dk, :], func=AF.Copy, scale=e[:dk, c:c + 1]),
                            reads=[S, er2[r]], writes=[], acc_writes=[spz])
                        S2 = Sst[(si + 1) % 2]
                        si += 1
                        kb.op("dve", lambda S=S, S2=S2, e=ed2[r], u=usb[r], c=c: nc.vector.scalar_tensor_tensor(
                            out=S2[:dk, :], in0=S[:dk, :], scalar=e[:dk, c:c + 1], in1=u[:dk, c * 128:(c + 1) * 128],
                            op0=ALU.mult, op1=ALU.add),
                            reads=[S, ed2[r], usb[r]], writes=[S2])
                        S = S2
                    return S, si

                sched = []
                for ti in order:
                    sched.append((ti, it % 2, it % R))
                    it += 1
                for idx in range(len(sched) + 1):
                    if idx < len(sched):
                        p1(*sched[idx])
                    if idx >= 1:
                        S, si = p2(*sched[idx - 1], S, si)
            groups = [list(range(g * 4, g * 4 + 4)) for g in range(8)] + [[32, 33]]
            if DBG:
                groups = []
            for gi, tiles in enumerate(groups):
                po = psO[gi % 2]
                pm = psM[gi % 2]
                nb = len(tiles) * 128
                t0 = tiles[0] * 128
                fns = []
                for k_, ti in enumerate(tiles):
                    cs = k_ * 128
                    for z in range(2):
                        fns.append(lambda po=po, ti=ti, cs=cs, z=z: nc.tensor.matmul(
                            po[:, cs:cs + 128], lhsT=vt[:, ti, :], rhs=atm[z][:, ti, :], start=(z == 0), stop=False))
                        for c in range(2):
                            fns.append(lambda po=po, ti=ti, cs=cs, z=z, c=c: nc.tensor.matmul(
                                po[:, cs + c * 64:cs + (c + 1) * 64], lhsT=Sp[z][:dk, 2 * ti + c, :],
                                rhs=qt[z][:dk, ti * 128 + c * 64:ti * 128 + (c + 1) * 64],
                                start=False, stop=(z == 1)))
                kb.mm(fns, reads=[vt, atm[0], atm[1], Sp[0], Sp[1], qt[0], qt[1]], writes=[po])
                s_, d_, o_ = sq[gi % 2], sd[gi % 2], on[gi % 2]
                kb.op("act", lambda po=po, s_=s_: nc.scalar.activation(out=s_[:, :nb], in_=po[:, :nb], func=AF.Square),
                      reads=[po], writes=[s_])
                kb.mm([lambda pm=pm, s_=s_: nc.tensor.matmul(pm[:, :nb], lhsT=onesdv[:], rhs=s_[:, :nb], start=True, stop=True)],
                      reads=[onesdv, s_], writes=[pm])
                kb.op("act", lambda pm=pm, d_=d_: nc.scalar.activation(out=d_[:, :nb], in_=pm[:, :nb], func=AF.Sqrt, bias=eps_t[:]),
                      reads=[pm, eps_t], writes=[d_])
                kb.op("dve", lambda d_=d_: nc.vector.reciprocal(out=d_[:, :nb], in_=d_[:, :nb]), reads=[d_], writes=[d_])
                ob_ = osb[gi % 2]
                kb.op("act", lambda po=po, ob_=ob_: nc.scalar.copy(out=ob_[:, :nb], in_=po[:, :nb]), reads=[po], writes=[ob_])
                kb.op("dve", lambda ob_=ob_, d_=d_, o_=o_: nc.vector.tensor_tensor(
                    out=o_[:, :nb], in0=ob_[:, :nb], in1=d_[:, :nb], op=ALU.mult), reads=[ob_, d_], writes=[o_])
                kb.op("pool", lambda o_=o_: nc.gpsimd.tensor_tensor(
                    out=obr[:, t0:t0 + nb], in0=o_[:, :nb], in1=gsil[:, t0:t0 + nb], op=ALU.mult),
                    reads=[o_, gsil], writes=[], acc_writes=[obr])
            kb.dma("sp", dest[h * dv:(h + 1) * dv, :], obr[:], reads=[obr], writes=[], acc_writes=[dest])


def scan_masks():
    j = np.arange(128)[:, None]
    i = np.arange(128)[None, :]
    same = (j // 64 == i // 64).astype(np.float32)
    out = {}
    for name, gs in (("mg", -1.0 / 16.0), ("mh", 1.0)):
        mq = np.zeros((128, 2, 130), np.float32)
        mk = np.zeros((128, 2, 128), np.float32)
        mq[:, 0, :128] = gs * same * ((j <= i).astype(np.float32) - 0.5)
        mq[:, 1, :128] = gs * same * ((j >= i).astype(np.float32) - 0.5)
        for z in range(2):
            for c in range(2):
                mq[:, z, 128 + c] = gs * (np.arange(128) // 64 == c)
        mk[:, 0, :] = gs * same * (j > i)
        mk[:, 1, :] = gs * same * (j < i)
        out[name + "_mq"] = mq
        out[name + "_mk"] = mk
    ma = np.zeros((128, 2, 128), np.float32)
    ma[:, 0, :] = same * (j <= i)
    ma[:, 1, :] = same * (j >= i)
    out["m_ma"] = ma
    out["m_mau"] = ma.astype(np.uint32)
    out["m_ident"] = np.eye(128, dtype=np.float32)
    return out


def stage_wa(cx, kb, nc, l, groups=None):
    with kb.scope():
        wm = kb.sbuf("wm", [128, 2, 128], BF16)
        kb.dma("pool", wm[:], cx.m_wa[:], reads=[cx.m_wa], writes=[wm])
        ones64 = kb.sbuf("ones64", [128, 64], BF16)
        kb.op("dve", lambda: nc.vector.memset(ones64[:], 1.0), writes=[ones64])
        sk = kb.sbuf("sk", [64, 8], F32)
        kb.dma("sp", sk[:], cx.wa_sink[l].partition_broadcast(64), reads=[cx.wa_sink], writes=[sk])
        kb.op("act", lambda: nc.scalar.activation(out=sk[:], in_=sk[:], func=AF.Exp), reads=[sk], writes=[sk])
        q4 = kb.sbuf("q4", [64, 4, T], BF16)
        kT = kb.sbuf("kTc", [64, T], BF16)
        vt = kb.sbuf("vtc", [128, NT, 64], BF16)
        ost = kb.sbuf("ost", [64, 4, T], BF16)
        pS = [kb.psum("pS%d" % i, [128, 512]) for i in range(4)]
        pO = [kb.psum("pO%d" % i, [128, 512]) for i in range(2)]
        pD = [kb.psum("pD%d" % i, [128, 512]) for i in range(2)]
        R = 10
        pt = [kb.sbuf("pt%d" % i, [128, 4, 128], BF16) for i in range(R)]
        ptm = [kb.sbuf("ptm%d" % i, [128, 4, 128], BF16) for i in range(R)]
        osb = [kb.sbuf("osbw%d" % i, [64, 4, 128], F32) for i in range(2)]
        dsb = [kb.sbuf("dsbw%d" % i, [64, 4, 128], F32) for i in range(2)]
        it = 0
        for g in (range(2) if groups is None else groups):
            kb.dma("sp", q4[:], cx.QC[g * 256:(g + 1) * 256, :].rearrange("(hh d) t -> d hh t", d=64),
                   reads=[cx.QC], writes=[q4])
            kb.dma("sp", kT[:], cx.KC[g * 64:(g + 1) * 64, :], reads=[cx.KC], writes=[kT])
            kb.dma("sp", vt[:], cx.VC_t[:, :, g * 64:(g + 1) * 64], reads=[cx.VC_t], writes=[vt])
            def p1(qb):
                nonlocal it
                if qb < 32:
                    keys = []
                    if qb > 0:
                        keys.append((qb - 1, 0))
                    keys.append((qb, None))
                    if qb < 31:
                        keys.append((qb + 1, 1))
                    keys += [(32, None), (33, None)]
                else:
                    keys = [(32, None), (33, None)]
                qs = slice(qb * 128, (qb + 1) * 128)
                st_ = []
                for ki, (kt_i, mi) in enumerate(keys):
                    ps = pS[it % 4]
                    p_ = pt[it % R]
                    pm_ = ptm[it % R]
                    it += 1
                    kb.mm([lambda ps=ps, kt_i=kt_i: nc.tensor.matmul(
                        ps[:, :], lhsT=kT[:, kt_i * 128:(kt_i + 1) * 128], rhs=q4[:, :, qs], start=True, stop=True)],
                        reads=[kT, q4], writes=[ps])
                    kb.op("act", lambda ps=ps, p_=p_: nc.scalar.activation(
                        out=p_[:].rearrange("p h q -> p (h q)"), in_=ps[:, :], func=AF.Exp, scale=0.125),
                        reads=[ps], writes=[p_])
                    src = p_
                    if mi is not None:
                        eng = "dve" if it % 2 == 0 else "pool"
                        h_ = nc.vector if eng == "dve" else nc.gpsimd
                        kb.op(eng, lambda h_=h_, p_=p_, pm_=pm_, mi=mi: h_.tensor_tensor(
                            out=pm_[:], in0=p_[:], in1=wm[:, mi, :].unsqueeze(1).broadcast_to([128, 4, 128]), op=ALU.mult),
                            reads=[p_, wm], writes=[pm_])
                        src = pm_
                    st_.append((kt_i, src))
                return (qb, st_)

            def p2(state):
                qb, st_ = state
                qs = slice(qb * 128, (qb + 1) * 128)
                a = qb % 2
                po, pd = pO[a], pD[a]
                nk = len(st_)
                for ki, (kt_i, src) in enumerate(st_):
                    kb.mm([lambda po=po, src=src, kt_i=kt_i, ki=ki: nc.tensor.matmul(
                        po[:64, :], lhsT=vt[:, kt_i, :], rhs=src[:].rearrange("p h q -> p (h q)"),
                        start=(ki == 0), stop=(ki == nk - 1)),
                        lambda pd=pd, src=src, ki=ki: nc.tensor.matmul(
                        pd[:64, :], lhsT=ones64[:], rhs=src[:].rearrange("p h q -> p (h q)"),
                        start=(ki == 0), stop=(ki == nk - 1))],
                        reads=[vt, src, ones64], writes=[po, pd])
                o_, d_ = osb[a], dsb[a]
                kb.op("act", lambda po=po, o_=o_: nc.scalar.copy(out=o_[:].rearrange("p h q -> p (h q)"), in_=po[:64, :]),
                      reads=[po], writes=[o_])
                kb.op("act", lambda pd=pd, d_=d_: nc.scalar.copy(out=d_[:].rearrange("p h q -> p (h q)"), in_=pd[:64, :]),
                      reads=[pd], writes=[d_])
                kb.op("dve", lambda d_=d_: nc.vector.tensor_tensor(
                    out=d_[:], in0=d_[:], in1=sk[:, g * 4:(g + 1) * 4].unsqueeze(2).broadcast_to([64, 4, 128]), op=ALU.add),
                    reads=[d_, sk], writes=[d_])
                kb.op("dve", lambda d_=d_: nc.vector.reciprocal(out=d_[:], in_=d_[:]), reads=[d_], writes=[d_])
                kb.op("pool", lambda o_=o_, d_=d_: nc.gpsimd.tensor_tensor(
                    out=ost[:, :, qs], in0=o_[:], in1=d_[:], op=ALU.mult),
                    reads=[o_, d_], writes=[], acc_writes=[ost])

            prev = None
            for qb in range(NT):
                cur = p1(qb)
                if prev is not None:
                    p2(prev)
                prev = cur
            p2(prev)
            kb.dma("sp", cx.BR_C[g * 256:(g + 1) * 256, :].rearrange("(hh d) t -> d hh t", d=64), ost[:],
                   reads=[ost], writes=[], acc_writes=[cx.BR_C])


def wa_masks():
    kk = np.arange(128)[:, None]
    qq = np.arange(128)[None, :]
    m = np.zeros((128, 2, 128), np.float32)
    m[:, 0, :] = (kk >= qq)
    m[:, 1, :] = (kk <= qq)
    return {"m_wa": m}


def na_patterns():
    W = 64
    tok = np.arange(128)
    lr, lc = tok // W, tok % W
    pats = []
    keys = []
    for m in range(32):
        qr = 2 * m + lr[None, :]
        qc = lc[None, :]
        rs = np.clip(qr - 4, 0, 56)
        cs = np.clip(qc - 8, 0, 48)
        lst = []
        for kt_i in range(32):
            kr = 2 * kt_i + lr[:, None]
            kc = lc[:, None]
            valid = (kr >= rs) & (kr < rs + 8) & (kc >= cs) & (kc < cs + 16)
            if not valid.any():
                continue
            ri = np.clip(kr - qr + 7, 0, 14)
            ci = np.clip(kc - qc + 15, 0, 30)
            if 2 <= m <= 29:
                pid = kt_i - m + 2
                if m == 2:
                    pats.append((valid, ri, ci))
            else:
                pid = len(pats)
                pats.append((valid, ri, ci))
            lst.append((kt_i, pid))
        keys.append(lst)
    return keys, pats


def na_consts():
    keys, pats = na_patterns()
    n0 = len(keys[0]) + len(keys[1])
    order = list(range(n0, n0 + 5)) + list(range(0, n0)) + list(range(n0 + 5, len(pats)))
    remap = {old: new for new, old in enumerate(order)}
    pats2 = [pats[o] for o in order]
    keys2 = []
    for m, lst in enumerate(keys):
        if 2 <= m <= 29:
            keys2.append(lst)
        else:
            keys2.append([(k, remap[p]) for (k, p) in lst])
    valid = np.stack([p[0] for p in pats2], 1).astype(np.float32)
    ri = np.stack([p[1] for p in pats2], 1)
    ci = np.stack([p[2] for p in pats2], 1)
    return keys2, valid, ri, ci


def stage_na(cx, kb, nc, l, heads=None, bg_factory=None):
    keys_tab, valid_np, _, _ = na_consts()
    npat = valid_np.shape[1]
    with kb.scope():
        vmask = kb.sbuf("vmask", [128, npat, 128], F32)
        kb.dma("sp", vmask[:], cx.m_navalid[:], reads=[cx.m_navalid], writes=[vmask])
        ones64 = kb.sbuf("ones64n", [128, 64], BF16)
        kb.op("dve", lambda: nc.vector.memset(ones64[:], 1.0), writes=[ones64])
        qT = kb.sbuf("qTn", [64, T], BF16)
        kT = kb.sbuf("kTn", [64, T], BF16)
        vt = kb.sbuf("vtn", [128, NT, 64], BF16)
        ost = kb.sbuf("ostn", [64, T], BF16)
        bias = kb.sbuf("biasn", [128, npat, 128], F32)
        EB = kb.sbuf("EBn", [128, npat, 128], BF16)
        pS = [kb.psum("pSn%d" % i, [128, 512]) for i in range(4)]
        pO = [kb.psum("pOn%d" % i, [128, 512]) for i in range(2)]
        pD = [kb.psum("pDn%d" % i, [128, 512]) for i in range(2)]
        R = 6
        pt = [kb.sbuf("ptn%d" % i, [128, 4, 128], BF16) for i in range(R)]
        osb = [kb.sbuf("osbn%d" % i, [64, 256], F32) for i in range(2)]
        it = 0
        bg = None
        if bg_factory is not None:
            stw = [kb.sbuf("pcw%d" % i, [128, 8, 256], BF16) for i in range(4)]
            std = [kb.sbuf("pcd%d" % i, [128, 16, 1024], BF16) for i in range(2)]
            bg = bg_factory(stw, std)
        for h in (range(8) if heads is None else heads):
            kb.dma("sp", bias[:], cx.na_bias[l, h], reads=[cx.na_bias], writes=[bias])
            kb.op("act", lambda: nc.scalar.activation(out=bias[:], in_=bias[:], func=AF.Exp), reads=[bias], writes=[bias])
            kb.op("pool", lambda: nc.gpsimd.tensor_tensor(out=EB[:], in0=bias[:], in1=vmask[:], op=ALU.mult),
                  reads=[bias, vmask], writes=[EB])
            kb.dma("sp", qT[:], cx.QB[h * 64:(h + 1) * 64, :], reads=[cx.QB], writes=[qT])
            kb.dma("sp", kT[:], cx.KB_[h * 64:(h + 1) * 64, :], reads=[cx.KB_], writes=[kT])
            kb.dma("sp", vt[:], cx.VB_t[:, :, h * 64:(h + 1) * 64], reads=[cx.VB_t], writes=[vt])
            def p1(qb):
                nonlocal it
                if qb < 32:
                    keys = list(keys_tab[qb]) + [(32, None), (33, None)]
                else:
                    keys = [(32, None), (33, None)]
                qs = slice(qb * 128, (qb + 1) * 128)
                grp = [keys[i:i + 4] for i in range(0, len(keys), 4)]
                st_ = []
                for gk in grp:
                    ps = pS[it % 4]
                    p_ = pt[it % R]
                    it += 1
                    ng = len(gk)
                    kb.mm([(lambda ps=ps, j=j, kt_i=kt_i: nc.tensor.matmul(
                        ps[:, j * 128:(j + 1) * 128], lhsT=kT[:, kt_i * 128:(kt_i + 1) * 128], rhs=qT[:, qs],
                        start=True, stop=True)) for j, (kt_i, _) in enumerate(gk)],
                        reads=[kT, qT], writes=[ps])
                    kb.op("act", lambda ps=ps, p_=p_, ng=ng: nc.scalar.activation(
                        out=p_[:, :ng, :].rearrange("p h q -> p (h q)"), in_=ps[:, :ng * 128], func=AF.Exp, scale=0.125),
                        reads=[ps], writes=[p_])
                    nloc = sum(1 for (_, pid) in gk if pid is not None)
                    if nloc:
                        pid0 = gk[0][1]
                        eng = "dve" if it % 2 == 0 else "pool"
                        h_ = nc.vector if eng == "dve" else nc.gpsimd
                        kb.op(eng, lambda h_=h_, p_=p_, pid0=pid0, nloc=nloc: h_.tensor_tensor(
                            out=p_[:, :nloc, :], in0=p_[:, :nloc, :], in1=EB[:, pid0:pid0 + nloc, :], op=ALU.mult),
                            reads=[p_, EB], writes=[p_])
                    st_.append((gk, p_))
                return (qb, st_, len(keys))

            def p2(state):
                qb, st_, nk = state
                qs = slice(qb * 128, (qb + 1) * 128)
                a = qb % 2
                po = pO[a]
                pd = pD[a]
                done = 0
                for gk, p_ in st_:
                    fns = []
                    for j, (kt_i, _) in enumerate(gk):
                        first = (done == 0)
                        last = (done == nk - 1)
                        fns.append(lambda po=po, p_=p_, j=j, kt_i=kt_i, first=first, last=last: nc.tensor.matmul(
                            po[:64, 0:128], lhsT=vt[:, kt_i, :], rhs=p_[:, j, :], start=first, stop=last))
                        fns.append(lambda pd=pd, p_=p_, j=j, first=first, last=last: nc.tensor.matmul(
                            pd[:64, 0:128], lhsT=ones64[:], rhs=p_[:, j, :], start=first, stop=last))
                        done += 1
                    kb.mm(fns, reads=[vt, p_, ones64], writes=[po, pd])
                o_ = osb[a]
                kb.op("act", lambda po=po, o_=o_: nc.scalar.copy(out=o_[:, 0:128], in_=po[:64, 0:128]), reads=[po], writes=[o_])
                kb.op("act", lambda pd=pd, o_=o_: nc.scalar.copy(out=o_[:, 128:256], in_=pd[:64, 0:128]), reads=[pd, o_], writes=[o_])
                kb.op("dve", lambda o_=o_: nc.vector.reciprocal(out=o_[:, 128:256], in_=o_[:, 128:256]), reads=[o_], writes=[o_])
                kb.op("pool", lambda o_=o_: nc.gpsimd.tensor_tensor(
                    out=ost[:, qs], in0=o_[:, 0:128], in1=o_[:, 128:256], op=ALU.mult),
                    reads=[o_], writes=[], acc_writes=[ost])

            prev = None
            for qb in range(NT):
                cur = p1(qb)
                if prev is not None:
                    p2(prev)
                prev = cur
                if bg is not None and qb % 2 == 1:
                    next(bg, None)
            p2(prev)
            kb.dma("sp", cx.BR_B[h * 64:(h + 1) * 64, :], ost[:], reads=[ost], writes=[], acc_writes=[cx.BR_B])
        if bg is not None:
            for _ in bg:
                pass

LN_EPS = 1e-5
MB = 512
MBLKS = [(i * MB, min(MB, T - i * MB)) for i in range((T + MB - 1) // MB)]


def ln_setup(cx, kb, nc):
    ones = kb.sbuf("ln_ones", [128, 128], F32)
    kb.op("dve", lambda: nc.vector.memset(ones[:], 1.0 / D), writes=[ones])
    eps = kb.sbuf("ln_eps", [128, 1], F32)
    kb.op("dve", lambda: nc.vector.memset(eps[:], LN_EPS), writes=[eps])
    return ones, eps


def ln_block(kb, nc, r, nb, gam, bet, out, ones, eps, scr):
    rsq, ps1, ps2, mean, var = scr
    kb.op("act", lambda: nc.scalar.activation(out=rsq[:, :, :nb], in_=r[:, :, :nb], func=AF.Square), reads=[r], writes=[rsq])
    kb.mm([(lambda oc=oc: nc.tensor.matmul(ps1[:, :nb], lhsT=ones[:], rhs=r[:, oc, :nb], start=(oc == 0), stop=(oc == 7)))
           for oc in range(8)], reads=[ones, r], writes=[ps1])
    kb.mm([(lambda oc=oc: nc.tensor.matmul(ps2[:, :nb], lhsT=ones[:], rhs=rsq[:, oc, :nb], start=(oc == 0), stop=(oc == 7)))
           for oc in range(8)], reads=[ones, rsq], writes=[ps2])
    kb.op("act", lambda: nc.scalar.copy(out=mean[:, :nb], in_=ps1[:, :nb]), reads=[ps1], writes=[mean])
    kb.op("act", lambda: nc.scalar.copy(out=var[:, :nb], in_=ps2[:, :nb]), reads=[ps2], writes=[var])
    m2 = rsq
    kb.op("dve", lambda: nc.vector.tensor_tensor(out=m2[:, 0, :nb], in0=mean[:, :nb], in1=mean[:, :nb], op=ALU.mult),
          reads=[mean, rsq], writes=[rsq])
    kb.op("dve", lambda: nc.vector.tensor_tensor(out=var[:, :nb], in0=var[:, :nb], in1=m2[:, 0, :nb], op=ALU.subtract),
          reads=[var, rsq], writes=[var])
    kb.op("act", lambda: nc.scalar.activation(out=var[:, :nb], in_=var[:, :nb], func=AF.Sqrt, bias=eps[:]),
          reads=[var, eps], writes=[var])
    kb.op("dve", lambda: nc.vector.reciprocal(out=var[:, :nb], in_=var[:, :nb]), reads=[var], writes=[var])
    kb.op("dve", lambda: nc.vector.tensor_tensor(
        out=r[:, :, :nb], in0=r[:, :, :nb], in1=mean[:, :nb].unsqueeze(1).broadcast_to([128, 8, nb]), op=ALU.subtract),
        reads=[r, mean], writes=[r])
    kb.op("pool", lambda: nc.gpsimd.tensor_tensor(
        out=r[:, :, :nb], in0=r[:, :, :nb], in1=var[:, :nb].unsqueeze(1).broadcast_to([128, 8, nb]), op=ALU.mult),
        reads=[r, var], writes=[r])
    for oc in range(8):
        eng = "dve" if oc % 2 == 0 else "pool"
        h_ = nc.vector if eng == "dve" else nc.gpsimd
        kb.op(eng, lambda h_=h_, oc=oc: h_.tensor_scalar(
            out=out[:, oc, :nb], in0=r[:, oc, :nb], scalar1=gam[:, oc:oc + 1], scalar2=bet[:, oc:oc + 1],
            op0=ALU.mult, op1=ALU.add), reads=[r, gam, bet], writes=[out])


def load_ln_params(cx, kb, nc, l, which):
    gam = kb.sbuf("ln_g", [128, 8], F32)
    bet = kb.sbuf("ln_b", [128, 8], F32)
    kb.dma("sp", gam[:], cx.ln_gT[l, which], reads=[cx.ln_gT], writes=[gam])
    kb.dma("sp", bet[:], cx.ln_bT[l, which], reads=[cx.ln_bT], writes=[bet])
    return gam, bet


def stage_merge(cx, kb, nc, l, xsrc, modv):
    with kb.scope():
        ones, eps = ln_setup(cx, kb, nc)
        gam, bet = load_ln_params(cx, kb, nc, l, 0)
        wbr = kb.sbuf("wbr", [128, 4, 4, 1024], BF16)
        wout = kb.sbuf("wout", [128, 8, 1024], BF16)
        for i in range(4):
            kb.dma("pool", wbr[:, i], cx.w_branch[l, i].rearrange("(kc p) c -> p kc c", p=128), reads=[cx.w_branch], writes=[wbr])
        kb.dma("pool", wout[:], cx.w_out[l].rearrange("(kc p) c -> p kc c", p=128), reads=[cx.w_out], writes=[wout])
        wr = kb.sbuf("wr", [128, 8, 36], F32)
        kb.dma("sp", wr[:, :, 0:4], cx.moe_w_group[l].rearrange("(kc p) c -> p kc c", p=128), reads=[cx.moe_w_group], writes=[wr])
        kb.dma("sp", wr[:, :, 4:36], cx.moe_w_expert[l].rearrange("(kc p) c -> p kc c", p=128), reads=[cx.moe_w_expert], writes=[wr])
        rb = kb.sbuf("rb", [128, 36], F32)
        kb.dma("sp", rb[:, 0:4], cx.moe_b_group[l].partition_broadcast(128), reads=[cx.moe_b_group], writes=[rb])
        kb.dma("sp", rb[:, 4:36], cx.moe_b_expert[l].partition_broadcast(128), reads=[cx.moe_b_expert], writes=[rb])
        onep4 = kb.sbuf("onep4", [128, 8, 2], F32)
        kb.op("dve", lambda: nc.vector.tensor_scalar(out=onep4[:], in0=modv[:, 32:40, :], scalar1=1.0, scalar2=None, op0=ALU.add),
              reads=[modv], writes=[onep4])
        brt = [kb.sbuf("brt%d" % i, [128, 4, 4, MB], BF16) for i in range(1)]
        gtt = kb.sbuf("gtt", [128, 32, MB], BF16)
        xb = kb.sbuf("xbm", [128, 8, MB], F32)
        mrg = kb.sbuf("mrg", [128, 8, MB], BF16)
        r = kb.sbuf("rm", [128, 8, MB], F32)
        hx2b = kb.sbuf("hx2b", [128, 8, MB], BF16)
        rsq = kb.sbuf("rsqm", [128, 8, MB], F32)
        x1 = r
        hx2f = rsq
        mean = kb.sbuf("meanm", [128, MB], F32)
        var = kb.sbuf("varm", [128, MB], F32)
        csb = [kb.sbuf("csb%d" % i, [128, MB], F32) for i in range(4)]
        acc = kb.sbuf("accm", [128, MB], F32)
        mixs = kb.sbuf("mixs", [128, MB], F32)
        pbr = [kb.psum("pbr%d" % i, [128, MB]) for i in range(4)]
        pmx = kb.psum("pmx", [128, MB])
        ps1 = kb.psum("ps1m", [128, MB])
        ps2 = kb.psum("ps2m", [128, MB])
        prt = kb.psum("prt", [128, MB])
        brs = [cx.BR_A, cx.BR_B, cx.BR_C, cx.BR_D]
        lgt = kb.sbuf("lgt", [128, 36], F32)
        gmx = kb.sbuf("gmx", [128, 1], F32)
        ngm = kb.sbuf("ngm", [128, 1], F32)
        ge = kb.sbuf("ge", [128, 4], F32)
        gs = kb.sbuf("gsr", [128, 1], F32)
        oh = kb.sbuf("oh", [128, 4], F32)
        lem = kb.sbuf("lem", [128, 4, 8], F32)
        mx8 = kb.sbuf("mx8", [128, 8], F32)
        dd = kb.sbuf("dd", [128, 1], F32)
        w1 = kb.sbuf("w1", [128, 1], F32)
        w2 = kb.sbuf("w2", [128, 1], F32)
        tt1 = kb.sbuf("tt1", [128, 32], F32)
        tt2 = kb.sbuf("tt2", [128, 32], F32)
        wgt = kb.sbuf("wgt", [128, 32], F32)
        for bi, (t0, nb) in enumerate(MBLKS):
            col = blkcol(t0)
            b_ = brt[0]
            for i in range(4):
                kb.dma("sp", b_[:, i, :, :nb], brs[i][:, t0:t0 + nb].rearrange("(kc p) t -> p kc t", p=128),
                       reads=[brs[i]], writes=[b_])
            kb.dma("sp", gtt[:, :, :nb], cx.GATE[:, t0:t0 + nb].rearrange("(j p) t -> p j t", p=128), reads=[cx.GATE], writes=[gtt])
            kb.dma("sp", xb[:, :, :nb], xsrc[:, t0:t0 + nb].rearrange("(kc p) t -> p kc t", p=128), reads=[xsrc], writes=[xb])
            for oc in range(8):
                for i in range(4):
                    kb.mm([(lambda i=i, kc=kc, oc=oc: nc.tensor.matmul(
                        pbr[i][:, :nb], lhsT=wbr[:, i, kc, oc * 128:(oc + 1) * 128], rhs=b_[:, i, kc, :nb],
                        start=(kc == 0), stop=(kc == 3))) for kc in range(4)], reads=[wbr, b_], writes=[pbr[i]])
                    kb.op("act", lambda i=i: nc.scalar.copy(out=csb[i][:, :nb], in_=pbr[i][:, :nb]), reads=[pbr[i]], writes=[csb[i]])
                    eng = "dve" if i % 2 == 0 else "pool"
                    h_ = nc.vector if eng == "dve" else nc.gpsimd
                    kb.op(eng, lambda h_=h_, i=i, oc=oc: h_.tensor_tensor(
                        out=csb[i][:, :nb], in0=csb[i][:, :nb], in1=gtt[:, i * 8 + oc, :nb], op=ALU.mult),
                        reads=[csb[i], gtt], writes=[csb[i]])
                kb.op("dve", lambda: nc.vector.tensor_tensor(out=acc[:, :nb], in0=csb[0][:, :nb], in1=csb[1][:, :nb], op=ALU.add),
                      reads=[csb[0], csb[1]], writes=[acc])
                kb.op("pool", lambda: nc.gpsimd.tensor_tensor(out=csb[2][:, :nb], in0=csb[2][:, :nb], in1=csb[3][:, :nb], op=ALU.add),
                      reads=[csb[2], csb[3]], writes=[csb[2]])
                kb.op("dve", lambda oc=oc: nc.vector.tensor_tensor(out=mrg[:, oc, :nb], in0=acc[:, :nb], in1=csb[2][:, :nb], op=ALU.add),
                      reads=[acc, csb[2]], writes=[mrg])
            for oc in range(8):
                kb.mm([(lambda kc=kc, oc=oc: nc.tensor.matmul(
                    pmx[:, :nb], lhsT=wout[:, kc, oc * 128:(oc + 1) * 128], rhs=mrg[:, kc, :nb],
                    start=(kc == 0), stop=(kc == 7))) for kc in range(8)], reads=[wout, mrg], writes=[pmx])
                kb.op("act", lambda: nc.scalar.copy(out=mixs[:, :nb], in_=pmx[:, :nb]), reads=[pmx], writes=[mixs])
                kb.op("pool", lambda oc=oc: nc.gpsimd.tensor_scalar(
                    out=mixs[:, :nb], in0=mixs[:, :nb], scalar1=modv[:, 16 + oc, col:col + 1], scalar2=None, op0=ALU.mult),
                    reads=[mixs, modv], writes=[mixs])
                kb.op("dve", lambda oc=oc: nc.vector.scalar_tensor_tensor(
                    out=r[:, oc, :nb], in0=xb[:, oc, :nb], scalar=ALPHA, in1=mixs[:, :nb], op0=ALU.mult, op1=ALU.add),
                    reads=[xb, mixs], writes=[r])
            ln_block(kb, nc, r, nb, gam, bet, x1, ones, eps, (rsq, ps1, ps2, mean, var))
            kb.dma("sp", cx.XT[:, t0:t0 + nb].rearrange("(kc p) t -> p kc t", p=128), x1[:, :, :nb],
                   reads=[x1], writes=[], acc_writes=[cx.XT])
            for oc in range(8):
                eng = "dve" if oc % 2 == 0 else "pool"
                h_ = nc.vector if eng == "dve" else nc.gpsimd
                kb.op(eng, lambda h_=h_, oc=oc: h_.tensor_scalar(
                    out=hx2f[:, oc, :nb], in0=x1[:, oc, :nb], scalar1=onep4[:, oc, col:col + 1],
                    scalar2=modv[:, 24 + oc, col:col + 1], op0=ALU.mult, op1=ALU.add),
                    reads=[x1, onep4, modv], writes=[hx2f])
            kb.op("act", lambda: nc.scalar.copy(out=hx2b[:, :, :nb], in_=hx2f[:, :, :nb]), reads=[hx2f], writes=[hx2b])
            kb.dma("sp", cx.HX2[:, t0:t0 + nb].rearrange("(kc p) t -> p kc t", p=128), hx2b[:, :, :nb],
                   reads=[hx2b], writes=[], acc_writes=[cx.HX2])
            for tj in range(nb // 128):
                ti = t0 // 128 + tj
                tsl = slice(tj * 128, (tj + 1) * 128)
                kb.mm([(lambda kc=kc: nc.tensor.matmul(prt[:, 0:36], lhsT=hx2f[:, kc, tsl], rhs=wr[:, kc, :],
                                                       start=(kc == 0), stop=(kc == 7))) for kc in range(8)],
                      reads=[hx2f, wr], writes=[prt])
                kb.op("act", lambda: nc.scalar.copy(out=lgt[:], in_=prt[:, 0:36]), reads=[prt], writes=[lgt])
                kb.op("dve", lambda: nc.vector.tensor_tensor(out=lgt[:], in0=lgt[:], in1=rb[:], op=ALU.add), reads=[lgt, rb], writes=[lgt])
                kb.op("dve", lambda: nc.vector.tensor_reduce(out=gmx[:], in_=lgt[:, 0:4], axis=AX.X, op=ALU.max), reads=[lgt], writes=[gmx])
                kb.op("dve", lambda: nc.vector.tensor_scalar(out=ngm[:], in0=gmx[:], scalar1=-1.0, scalar2=None, op0=ALU.mult), reads=[gmx], writes=[ngm])
                kb.op("act", lambda: nc.scalar.activation(out=ge[:], in_=lgt[:, 0:4], func=AF.Exp, bias=ngm[:]), reads=[lgt, ngm], writes=[ge])
                kb.op("dve", lambda: nc.vector.tensor_reduce(out=gs[:], in_=ge[:], axis=AX.X, op=ALU.add), reads=[ge], writes=[gs])
                kb.op("dve", lambda: nc.vector.reciprocal(out=gs[:], in_=gs[:]), reads=[gs], writes=[gs])
                kb.op("dve", lambda: nc.vector.tensor_scalar(out=oh[:], in0=lgt[:, 0:4], scalar1=gmx[:], scalar2=None, op0=ALU.is_equal), reads=[lgt, gmx], writes=[oh])
                kb.op("dve", lambda: nc.vector.tensor_scalar(out=oh[:], in0=oh[:], scalar1=-1.0, scalar2=1e30, op0=ALU.add, op1=ALU.mult), reads=[oh], writes=[oh])
                kb.op("dve", lambda: nc.vector.tensor_tensor(
                    out=lem[:], in0=lgt[:, 4:36].rearrange("p (g e) -> p g e", e=8), in1=oh[:].unsqueeze(2).broadcast_to([128, 4, 8]), op=ALU.add),
                    reads=[lgt, oh], writes=[lem])
                lemf = lem[:].rearrange("p g e -> p (g e)")
                kb.op("dve", lambda: nc.vector.max(out=mx8[:], in_=lem[:].rearrange("p g e -> p (g e)")), reads=[lem], writes=[mx8])
                kb.op("dve", lambda: nc.vector.tensor_tensor(out=dd[:], in0=mx8[:, 1:2], in1=mx8[:, 0:1], op=ALU.subtract), reads=[mx8], writes=[dd])
                kb.op("act", lambda: nc.scalar.activation(out=dd[:], in_=dd[:], func=AF.Exp), reads=[dd], writes=[dd])
                kb.op("dve", lambda: nc.vector.tensor_scalar(out=w1[:], in0=dd[:], scalar1=1.0, scalar2=None, op0=ALU.add), reads=[dd], writes=[w1])
                kb.op("dve", lambda: nc.vector.reciprocal(out=w1[:], in_=w1[:]), reads=[w1], writes=[w1])
                kb.op("dve", lambda: nc.vector.tensor_tensor(out=w2[:], in0=dd[:], in1=w1[:], op=ALU.mult), reads=[dd, w1], writes=[w2])
                kb.op("dve", lambda: nc.vector.tensor_scalar(
                    out=tt1[:], in0=lem[:].rearrange("p g e -> p (g e)"), scalar1=mx8[:, 0:1], scalar2=w1[:], op0=ALU.is_equal, op1=ALU.mult),
                    reads=[lem, mx8, w1], writes=[tt1])
                kb.op("dve", lambda: nc.vector.tensor_scalar(
                    out=tt2[:], in0=lem[:].rearrange("p g e -> p (g e)"), scalar1=mx8[:, 1:2], scalar2=w2[:], op0=ALU.is_equal, op1=ALU.mult),
                    reads=[lem, mx8, w2], writes=[tt2])
                kb.op("dve", lambda: nc.vector.tensor_tensor(out=tt1[:], in0=tt1[:], in1=tt2[:], op=ALU.add), reads=[tt1, tt2], writes=[tt1])
                kb.op("dve", lambda: nc.vector.tensor_scalar(out=wgt[:], in0=tt1[:], scalar1=gs[:], scalar2=None, op0=ALU.mult), reads=[tt1, gs], writes=[wgt])
                kb.dma("sp", cx.WG_t[:, ti, :], wgt[:], reads=[wgt], writes=[], acc_writes=[cx.WG_t])


def stage_moe(cx, kb, nc, l, modv, yout=None, precast=True):
    with kb.scope():
        ones, eps = ln_setup(cx, kb, nc)
        gam, bet = load_ln_params(cx, kb, nc, l, 1)
        wgT = kb.sbuf("wgT", [32, T], F32)
        sel = kb.sbuf("sel", [32, 32, 128], F32)
        kb.dma("sp", sel[:], cx.m_sel[:], reads=[cx.m_sel], writes=[sel])
        ptr = [kb.psum("ptr%d" % i, [128, 512]) for i in range(2)]
        with kb.scope():
            wgl = kb.sbuf("wgl", [128, NT, 32], F32)
            id32 = kb.sbuf("id32", [128, 128], F32)
            kb.dma("sp", wgl[:], cx.WG_t[:], reads=[cx.WG_t], writes=[wgl])
            kb.dma("sp", id32[:], cx.m_ident32[:], reads=[cx.m_ident32], writes=[id32])
            for ti in range(NT):
                p_ = ptr[ti % 2]
                kb.mm([lambda p_=p_, ti=ti: nc.tensor.matmul(p_[:32, 0:128], lhsT=wgl[:, ti, :], rhs=id32[:], start=True, stop=True)],
                      reads=[wgl, id32], writes=[p_])
                kb.op("act", lambda p_=p_, ti=ti: nc.scalar.copy(out=wgT[:, ti * 128:(ti + 1) * 128], in_=p_[:32, 0:128]),
                      reads=[p_], writes=[], acc_writes=[wgT])
        if precast:
            precast_expert_weights(cx, kb, nc, l)
        hx = kb.sbuf("hxm", [128, 8, 512], BF16)
        yacc = kb.sbuf("yacc", [128, 8, 512], F32)
        hid = kb.sbuf("hid", [128, 16, 512], BF16)
        wdg = kb.sbuf("wdg", [128, 8, 2, 1024], BF16)
        NWB = 4
        wgs = [kb.sbuf("wgs%d" % i, [128, 8, 256], BF16) for i in range(NWB)]
        wus = [kb.sbuf("wus%d" % i, [128, 8, 256], BF16) for i in range(NWB)]
        xb = kb.sbuf("xbe", [128, 8, 512], F32)
        rsq = kb.sbuf("rsqe", [128, 8, 512], F32)
        mean = kb.sbuf("meane", [128, 512], F32)
        var = kb.sbuf("vare", [128, 512], F32)
        reps = [kb.sbuf("reps%d" % i, [128, 512], F32) for i in range(2)]
        ssb = [kb.sbuf("ssb%d" % i, [128, 512], F32) for i in range(3)]
        tsb = [kb.sbuf("tsb%d" % i, [128, 512], F32) for i in range(3)]
        ytmp = [kb.sbuf("ytmp%d" % i, [128, 512], F32) for i in range(2)]
        NPS = 3
        pg = [kb.psum("pg%d" % i, [128, 512]) for i in range(NPS)]
        pu = [kb.psum("pu%d" % i, [128, 512]) for i in range(NPS)]
        py = ptr
        ps1 = pg[0]
        ps2 = pu[0]
        prep = ptr[1]
        nw = 0
        it = 0
        for bi, (t0, nb) in enumerate(BLKS):
            col = blkcol(t0)
            kb.dma("sp", hx[:, :, :nb], cx.HX2[:, t0:t0 + nb].rearrange("(kc p) t -> p kc t", p=128), reads=[cx.HX2], writes=[hx])
            kb.dma("sp", xb[:, :, :nb], cx.XT[:, t0:t0 + nb].rearrange("(kc p) t -> p kc t", p=128), reads=[cx.XT], writes=[xb])
            for g in range(4):
                kb.dma("sp", wdg[:].rearrange("p e hc d -> p (e hc d)"), cx.WDB[g], reads=[cx.WDB], writes=[wdg])
                for e_ in range(8):
                    e = g * 8 + e_
                    wg_, wu_ = wgs[nw % NWB], wus[nw % NWB]
                    nw += 1
                    kb.dma("sp", wg_[:].rearrange("p kc f -> p (kc f)"), cx.WGB[e], reads=[cx.WGB], writes=[wg_])
                    kb.dma("act", wu_[:].rearrange("p kc f -> p (kc f)"), cx.WUB[e], reads=[cx.WUB], writes=[wu_])
                    rp = reps[e % 2]
                    kb.mm([lambda e=e: nc.tensor.matmul(prep[:, :nb], lhsT=sel[:, e, :], rhs=wgT[:, t0:t0 + nb], start=True, stop=True)],
                          reads=[sel, wgT], writes=[prep])
                    kb.op("act", lambda rp=rp: nc.scalar.copy(out=rp[:, :nb], in_=prep[:, :nb]), reads=[prep], writes=[rp])
                    for hc in range(2):
                        a = it % NPS
                        it += 1
                        kb.mm([(lambda kc=kc, a=a, hc=hc, wg_=wg_: nc.tensor.matmul(
                            pg[a][:, :nb], lhsT=wg_[:, kc, hc * 128:(hc + 1) * 128], rhs=hx[:, kc, :nb],
                            start=(kc == 0), stop=(kc == 7))) for kc in range(8)], reads=[wg_, hx], writes=[pg[a]])
                        kb.mm([(lambda kc=kc, a=a, hc=hc, wu_=wu_: nc.tensor.matmul(
                            pu[a][:, :nb], lhsT=wu_[:, kc, hc * 128:(hc + 1) * 128], rhs=hx[:, kc, :nb],
                            start=(kc == 0), stop=(kc == 7))) for kc in range(8)], reads=[wu_, hx], writes=[pu[a]])
                        kb.op("act", lambda a=a: nc.scalar.activation(out=ssb[a][:, :nb], in_=pg[a][:, :nb], func=AF.Silu),
                              reads=[pg[a]], writes=[ssb[a]])
                        kb.op("act", lambda a=a: nc.scalar.copy(out=tsb[a][:, :nb], in_=pu[a][:, :nb]), reads=[pu[a]], writes=[tsb[a]])
                        kb.op("dve", lambda a=a: nc.vector.tensor_tensor(out=tsb[a][:, :nb], in0=tsb[a][:, :nb], in1=ssb[a][:, :nb], op=ALU.mult),
                              reads=[tsb[a], ssb[a]], writes=[tsb[a]])
                        kb.op("pool", lambda a=a, rp=rp, e_=e_, hc=hc: nc.gpsimd.tensor_tensor(
                            out=hid[:, e_ * 2 + hc, :nb], in0=tsb[a][:, :nb], in1=rp[:, :nb], op=ALU.mult),
                            reads=[tsb[a], rp], writes=[], acc_writes=[hid])
                for oc in range(8):
                    p_ = py[oc % 2]
                    fns = []
                    for j in range(16):
                        fns.append(lambda j=j, oc=oc, p_=p_: nc.tensor.matmul(
                            p_[:, :nb], lhsT=wdg[:, j // 2, j % 2, oc * 128:(oc + 1) * 128], rhs=hid[:, j, :nb],
                            start=(j == 0), stop=(j == 15)))
                    kb.mm(fns, reads=[wdg, hid], writes=[p_])
                    if g == 0:
                        kb.op("act", lambda oc=oc, p_=p_: nc.scalar.copy(out=yacc[:, oc, :nb], in_=p_[:, :nb]), reads=[p_], writes=[], acc_writes=[yacc])
                    else:
                        yt = ytmp[oc % 2]
                        kb.op("act", lambda yt=yt, p_=p_: nc.scalar.copy(out=yt[:, :nb], in_=p_[:, :nb]), reads=[p_], writes=[yt])
                        eng = "dve" if oc % 2 == 0 else "pool"
                        h_ = nc.vector if eng == "dve" else nc.gpsimd
                        kb.op(eng, lambda h_=h_, oc=oc, yt=yt: h_.tensor_tensor(
                            out=yacc[:, oc, :nb], in0=yacc[:, oc, :nb], in1=yt[:, :nb], op=ALU.add),
                            reads=[yt, yacc], writes=[], acc_writes=[yacc])
            for oc in range(8):
                kb.op("pool", lambda oc=oc: nc.gpsimd.tensor_scalar(
                    out=yacc[:, oc, :nb], in0=yacc[:, oc, :nb], scalar1=modv[:, 40 + oc, col:col + 1], scalar2=None, op0=ALU.mult),
                    reads=[yacc, modv], writes=[yacc])
                kb.op("dve", lambda oc=oc: nc.vector.scalar_tensor_tensor(
                    out=yacc[:, oc, :nb], in0=xb[:, oc, :nb], scalar=ALPHA, in1=yacc[:, oc, :nb], op0=ALU.mult, op1=ALU.add),
                    reads=[xb, yacc], writes=[yacc])
            ln_block(kb, nc, yacc, nb, gam, bet, yacc, ones, eps, (rsq, ps1, ps2, mean, var))
            kb.dma("sp", cx.XT[:, t0:t0 + nb].rearrange("(kc p) t -> p kc t", p=128), yacc[:, :, :nb],
                   reads=[yacc], writes=[], acc_writes=[cx.XT])
            if yout is not None and t0 < TL:
                kb.dma("sp", yout[:, t0:t0 + nb].rearrange("(kc p) t -> p kc t", p=128), yacc[:, :, :nb],
                       reads=[yacc], writes=[], acc_writes=[yout])


def precast_expert_weights(cx, kb, nc, l):
    with kb.scope():
        stw = [kb.sbuf("pcw%d" % i, [128, 8, 256], BF16) for i in range(4)]
        std = [kb.sbuf("pcd%d" % i, [128, 16, 1024], BF16) for i in range(2)]
        n = 0
        for g in range(4):
            d_ = std[g % 2]
            kb.dma("pool", d_[:], cx.moe_w_down[l, g].rearrange("e (hc p) d -> p (e hc) d", p=128), reads=[cx.moe_w_down], writes=[d_])
            kb.dma("sp", cx.WDB[g], d_[:].rearrange("p j d -> p (j d)"), reads=[d_], writes=[], acc_writes=[cx.WDB])
            for e_ in range(8):
                e = g * 8 + e_
                for src, dst in ((cx.moe_w_gate, cx.WGB), (cx.moe_w_up, cx.WUB)):
                    w_ = stw[n % 4]
                    n += 1
                    kb.dma("pool", w_[:], src[l, g, e_].rearrange("(kc p) f -> p kc f", p=128), reads=[src], writes=[w_])
                    kb.dma("sp", dst[e], w_[:].rearrange("p kc f -> p (kc f)"), reads=[w_], writes=[], acc_writes=[dst])


def precast_gen(cx, kb, nc, l, stw, std):
    pend = []
    n = 0
    for g in range(4):
        d_ = std[g % 2]
        kb.dma("pool", d_[:], cx.moe_w_down[l, g].rearrange("e (hc p) d -> p (e hc) d", p=128), reads=[cx.moe_w_down], writes=[d_])
        pend.append((cx.WDB, g, d_, "p j d -> p (j d)"))
        yield
        for e_ in range(8):
            e = g * 8 + e_
            for src, dst in ((cx.moe_w_gate, cx.WGB), (cx.moe_w_up, cx.WUB)):
                w_ = stw[n % len(stw)]
                n += 1
                kb.dma("pool", w_[:], src[l, g, e_].rearrange("(kc p) f -> p kc f", p=128), reads=[src], writes=[w_])
                pend.append((dst, e, w_, "p kc f -> p (kc f)"))
                yield
                while len(pend) > 2:
                    dst_, idx_, buf_, pat_ = pend.pop(0)
                    kb.dma("sp", dst_[idx_], buf_[:].rearrange(pat_), reads=[buf_], writes=[], acc_writes=[dst_])
    while pend:
        dst_, idx_, buf_, pat_ = pend.pop(0)
        kb.dma("sp", dst_[idx_], buf_[:].rearrange(pat_), reads=[buf_], writes=[], acc_writes=[dst_])
        yield


def rope_tables():
    t = np.arange(TL)
    row = (t // 64).astype(np.float32)
    colp = (t % 64).astype(np.float32)
    nf = 16
    inv = (np.float32(10000.0) ** (-np.arange(nf, dtype=np.float32) / np.float32(nf))).astype(np.float32)
    cos = np.ones((128, T), np.float32)
    sin = np.zeros((128, T), np.float32)
    for q in range(128):
        d = q % 64
        pos = row if d < 32 else colp
        ang = (pos * inv[d % 16]).astype(np.float32)
        cos[q, :TL] = np.cos(ang).astype(np.float32)
        sin[q, :TL] = np.sin(ang).astype(np.float32)
    pt = np.zeros((128, 128), np.float32)
    for m in range(128):
        if (m % 32) < 16:
            pt[m + 16, m] = -1.0
        else:
            pt[m - 16, m] = 1.0
    return cos, sin, pt


_CONST = {}
_NAIDX = {}


def consts():
    if not _CONST:
        cos, sin, pt = rope_tables()
        _CONST.update(rope_cos=cos, rope_sin=sin, rope_pt=pt)
        _CONST.update(scan_masks())
        _CONST.update(wa_masks())
        sel = np.zeros((32, 32, 128), np.float32)
        for e in range(32):
            sel[e, e, :] = 1.0
        _CONST["m_sel"] = sel
        _CONST["m_ident32"] = np.eye(128, dtype=np.float32)
        _k, _valid, _ri, _ci = na_consts()
        _CONST["m_navalid"] = _valid
        _NAIDX["ri"] = _ri
        _NAIDX["ci"] = _ci
    return _CONST


def core_inputs(inp, b):
    f = lambda a: np.ascontiguousarray(a, dtype=np.float32)
    m = {}
    m["xin"] = f(np.concatenate([inp["x"][b].T, inp["ctx"][b].T], axis=1))
    cv = np.stack([inp["c"][b].reshape(8, 128).T, inp["c_ctx"].reshape(8, 128).T], axis=2)
    m["cvec"] = f(cv)
    m["w_mod"] = f(inp["w_mod"])
    m["b_modT"] = f(inp["b_mod"].reshape(2, 48, 128).transpose(0, 2, 1))
    m["w_in"] = f(inp["w_in"])
    m["hg_gamma"] = f(inp["hg_gamma"])
    m["gla_lr_w"] = f(inp["gla_lr_w"])
    m["gla_lr_b"] = f(inp["gla_lr_b"])
    m["wa_sink"] = f(inp["wa_sink"])
    for k in ("w_branch", "w_out", "moe_w_group", "moe_b_group", "moe_w_expert", "moe_b_expert",
              "moe_w_gate", "moe_w_up", "moe_w_down"):
        m[k] = f(inp[k])
    m["ln_gT"] = f(inp["ln_g"].reshape(2, 2, 8, 128).transpose(0, 1, 3, 2))
    m["ln_bT"] = f(inp["ln_b"].reshape(2, 2, 8, 128).transpose(0, 1, 3, 2))
    consts()
    m["na_bias"] = f(inp["na_rpb"][:, :, _NAIDX["ri"], _NAIDX["ci"]])
    m.update(consts())
    return m


FUSED = 1


def build_program(layers, final):
    nc = bass.Bass("TRN2", target_bir_lowering=False)
    cx = Ctx()
    declare_inputs(cx, nc)
    if final:
        yout = Buf(nc.dram_tensor("yout", [D, TL], F32, kind="ExternalOutput"), "yout")
    else:
        yout = None
    with ExitStack() as st:
        kb = KB(nc, st)
        declare_scratch(cx, kb, debug=False, as_output=(() if final else ("XT",)))
        modT = [kb.sbuf("modT%d" % l, [128, 48, 2], F32) for l in range(2)]
        stage_mod(cx, kb, nc, modT)
        first = True
        for l in layers:
            xsrc = cx.xin if first else cx.XT
            first = False
            with kb.scope():
                lbt = [kb.sbuf("lbt%d" % i, [128, 1024], F32) for i in range(2)]
                omlt = [kb.sbuf("omlt%d" % i, [128, 1024], F32) for i in range(2)]
                stage_lb(cx, kb, nc, lbt, omlt)
                hxT = kb.sbuf("hxT", [128, 8, T], BF16)
                stage_modulate(cx, kb, nc, xsrc, modT[l], 0, 1, hxT)
                stage_inproj_fm(cx, kb, nc, l, hxT)
                stage_inproj_tm(cx, kb, nc, l, hxT, lbt[l], omlt[l])
            stage_gla_gate(cx, kb, nc, l)
            stage_scan(cx, kb, nc, "gla")
            stage_scan(cx, kb, nc, "hgrn")
            stage_wa(cx, kb, nc, l)
            stage_na(cx, kb, nc, l, bg_factory=(lambda stw, std, l=l: precast_gen(cx, kb, nc, l, stw, std)))
            stage_merge(cx, kb, nc, l, xsrc, modT[l])
            stage_moe(cx, kb, nc, l, modT[l], yout if (final and l == layers[-1]) else None, precast=False)
        if final:
            kb.finish([yout])
        else:
            kb.finish([cx.XT])
    return nc


def kernel(**inputs):
    inp = {k: np.asarray(v) for k, v in inputs.items()}
    in_maps = [core_inputs(inp, b) for b in range(4)]
    if FUSED:
        nc = build_program([0, 1], True)
        res = run_bass_kernel_spmd(nc, in_maps, core_ids=list(range(4)))
    else:
        nc0 = build_program([0], False)
        res0 = run_bass_kernel_spmd(nc0, in_maps, core_ids=list(range(4)))
        for b in range(4):
            in_maps[b]["xin"] = np.ascontiguousarray(res0.results[b]["XT"])
        nc1 = build_program([1], True)
        res = run_bass_kernel_spmd(nc1, in_maps, core_ids=list(range(4)))
    out = np.stack([np.ascontiguousarray(res.results[b]["yout"].T) for b in range(4)], axis=0)
    return out.astype(np.float32)
```

```python
import numpy as np
from contextlib import ExitStack
import concourse.bass as bass
import concourse.mybir as mybir
from concourse.bass_utils import run_bass_kernel_spmd
import os

F32 = mybir.dt.float32
BF16 = mybir.dt.bfloat16
U32 = mybir.dt.uint32
AF = mybir.ActivationFunctionType
ALU = mybir.AluOpType
AX = mybir.AxisListType


class Buf:
    __slots__ = ("t", "w", "r", "name")

    def __init__(self, t, name=""):
        self.t = t
        self.w = []
        self.r = []
        self.name = name

    def __getitem__(self, idx):
        return self.t[idx]


class Dep:
    __slots__ = ("w", "r")

    def __init__(self):
        self.w = []
        self.r = []


class Eng:
    def __init__(self, name, h, sem):
        self.name = name
        self.h = h
        self.sem = sem
        self.count = 0
        self.seen = {}


class KB:
    def __init__(self, nc, stack, n_dma_sems=8):
        self.nc = nc
        self.stack = stack
        self.sems = {}
        self.engs = {}
        for name, h in (("pe", nc.tensor), ("act", nc.scalar), ("dve", nc.vector),
                        ("pool", nc.gpsimd), ("sp", nc.sync)):
            s = stack.enter_context(nc.semaphore("s_" + name))
            self.sems[id(s)] = s
            self.engs[name] = Eng(name, h, s)
        self.dma_pool = {}
        for q in ("sp", "pool", "act"):
            lst = []
            for i in range(n_dma_sems):
                s = stack.enter_context(nc.semaphore("d_%s%d" % (q, i)))
                self.sems[id(s)] = s
                lst.append([s, 0])
            self.dma_pool[q] = [lst, 0]
        self.ninstr = 0

    def scope(self):
        kb = self

        class _Scope:
            def __enter__(self_s):
                self_s.prev = kb.stack
                self_s.st = ExitStack()
                self_s.st.__enter__()
                kb.stack = self_s.st
                kb.nscope = getattr(kb, "nscope", 0) + 1
                return self_s

            def __exit__(self_s, *a):
                if a[0] is None:
                    kb.barrier()
                kb.stack = self_s.prev
                return self_s.st.__exit__(*a)
        return _Scope()

    def sbuf(self, name, shape, dtype):
        name = "%s_s%d" % (name, getattr(self, "nscope", 0))
        t = self.stack.enter_context(self.nc.sbuf_tensor(name, list(shape), dtype))
        return Buf(t, name)

    def psum(self, name, shape, dtype=F32):
        name = "%s_s%d" % (name, getattr(self, "nscope", 0))
        t = self.stack.enter_context(self.nc.psum_tensor(name, list(shape), dtype))
        return Buf(t, name)

    def dram(self, name, shape, dtype, kind="Internal"):
        t = self.nc.dram_tensor(name, list(shape), dtype, kind=kind)
        return Buf(t, name)

    def _wait(self, e, deps):
        need = {}
        for (sid, val) in deps:
            if need.get(sid, 0) < val:
                need[sid] = val
        for sid, val in need.items():
            if e.seen.get(sid, 0) < val:
                e.h.wait_ge(self.sems[sid], val)
                e.seen[sid] = val
                self.ninstr += 1

    def _collect(self, reads, writes, acc_writes=()):
        deps = []
        for b in reads:
            deps += b.w
        for b in writes:
            deps += b.w
            deps += b.r
        for b in acc_writes:
            deps += b.r
        return deps

    @staticmethod
    def _compact(lst):
        mx = {}
        for (sid, v) in lst:
            if mx.get(sid, 0) < v:
                mx[sid] = v
        return list(mx.items())

    def _commit(self, token, reads, writes, acc_writes=()):
        for b in acc_writes:
            b.w.append(token)
            if len(b.w) > 64:
                b.w = self._compact(b.w)
        for b in reads:
            b.r.append(token)
            if len(b.r) > 64:
                mx = {}
                for (sid, v) in b.r:
                    if mx.get(sid, 0) < v:
                        mx[sid] = v
                b.r = list(mx.items())
        for b in writes:
            b.w = [token]
            b.r = []

    def _cutcheck(self):
        import os
        cut = os.environ.get("KB_CUT")
        self.nops = getattr(self, "nops", 0) + 1
        skip = os.environ.get("KB_SKIP")
        if skip and self.nops in [int(x) for x in skip.split(",")]:
            return True
        return cut is not None and self.nops > int(cut)

    def op(self, eng, fn, reads=(), writes=(), same_engine_sync=True, acc_writes=()):
        if self._cutcheck():
            return None
        e = self.engs[eng]
        deps = self._collect(reads, writes, acc_writes)
        if not same_engine_sync:
            deps = [d for d in deps if d[0] != id(e.sem)]
        self._wait(e, deps)
        ins = fn()
        e.count += 1
        ins.then_inc(e.sem, 1)
        self.ninstr += 1
        token = (id(e.sem), e.count)
        self._commit(token, reads, writes, acc_writes)
        return token

    def mm(self, fns, reads=(), writes=()):
        if self._cutcheck():
            return None
        e = self.engs["pe"]
        deps = self._collect(reads, writes)
        deps = [d for d in deps if d[0] != id(e.sem)]
        self._wait(e, deps)
        ins = None
        for fn in fns:
            ins = fn()
            self.ninstr += 1
        e.count += 1
        ins.then_inc(e.sem, 1)
        token = (id(e.sem), e.count)
        self._commit(token, reads, writes)
        return token

    def dma(self, q, out, in_, reads=(), writes=(), acc_writes=(), **kw):
        e = self.engs[q]
        pool = self.dma_pool[q]
        lst, idx = pool
        ent = lst[idx % len(lst)]
        pool[1] = idx + 1
        s, total = ent
        deps = self._collect(reads, writes, acc_writes)
        deps.append((id(s), total))
        import os
        if os.environ.get("DMA_SERIAL") and getattr(self, "_last_dma", {}).get(q):
            deps.append(self._last_dma[q])
        self._wait(e, deps)
        ins = e.h.dma_start(out=out, in_=in_, **kw)
        ins.then_inc(s, 16)
        ent[1] = total + 16
        self.ninstr += 1
        token = (id(s), total + 16)
        if not hasattr(self, "_last_dma"):
            self._last_dma = {}
        self._last_dma[q] = token
        self._commit(token, reads, writes, acc_writes)
        return token

    def init_memory(self, nbytes=196608):
        nc = self.nc
        with self.scope():
            n = nbytes // 4
            big = self.sbuf("initbig", [128, n], F32)
            q = n // 4
            self.op("dve", lambda: nc.vector.memset(big[:, 0:q], 0.0), writes=[])
            self.op("pool", lambda: nc.gpsimd.memset(big[:, q:2 * q], 0.0), writes=[])
            self.op("dve", lambda: nc.vector.memset(big[:, 2 * q:3 * q], 0.0), writes=[])
            self.op("pool", lambda: nc.gpsimd.memset(big[:, 3 * q:n], 0.0), writes=[])
            pss = [self.psum("initps%d" % i, [128, 512]) for i in range(8)]
            for p in pss:
                self.op("dve", lambda p=p: nc.vector.memset(p[:], 0.0), writes=[])

    def barrier(self):
        allt = []
        for e in self.engs.values():
            if e.count:
                allt.append((id(e.sem), e.count))
        for q, (lst, _) in self.dma_pool.items():
            for s, total in lst:
                if total:
                    allt.append((id(s), total))
        for e in self.engs.values():
            self._wait(e, allt)

    def finish(self, tokens_bufs):
        e = self.engs["sp"]
        deps = []
        for b in tokens_bufs:
            deps += b.w
        self._wait(e, deps)

T = 4352
TL = 4096

D = 1024
TL = 4096
TC = 256
T = TL + TC
NT = T // 128
BLKS = [(i * 512, 512) for i in range(8)] + [(4096, 256)]
DIN = 10528
ALPHA = (2.0 * 2) ** 0.25

COLS = dict(a_q=(0, 256), a_k=(256, 256), a_v=(512, 512), a_g=(1024, 512), a_lr=(1536, 32),
            b_q=(1568, 512), b_k=(2080, 512), b_v=(2592, 512),
            c_q=(3104, 512), c_k=(3616, 128), c_v=(3744, 128),
            d_q=(3872, 512), d_f=(4384, 1024), d_i=(5408, 512), d_g=(5920, 512), gate=(6432, 4096))


def blkcol(t0):
    return 1 if t0 >= TL else 0


class Ctx:
    pass


def declare_inputs(cx, nc):
    def din(name, shape, dt=F32):
        return Buf(nc.dram_tensor(name, list(shape), dt, kind="ExternalInput"), name)
    cx.xin = din("xin", [D, T])
    cx.cvec = din("cvec", [128, 8, 2])
    cx.w_mod = din("w_mod", [2, D, 6 * D])
    cx.b_modT = din("b_modT", [2, 128, 48])
    cx.w_in = din("w_in", [2, D, DIN])
    cx.rope_cos = din("rope_cos", [128, T])
    cx.rope_sin = din("rope_sin", [128, T])
    cx.rope_pt = din("rope_pt", [128, 128])
    cx.hg_gamma = din("hg_gamma", [2, 2, 512])
    cx.gla_lr_w = din("gla_lr_w", [2, 2, 16, 256])
    cx.gla_lr_b = din("gla_lr_b", [2, 2, 256])
    for nm in ("mg", "mh"):
        setattr(cx, nm + "_mq", din(nm + "_mq", [128, 2, 130]))
        setattr(cx, nm + "_mk", din(nm + "_mk", [128, 2, 128]))
    cx.m_ma = din("m_ma", [128, 2, 128])
    cx.m_ident = din("m_ident", [128, 128])
    cx.m_mau = din("m_mau", [128, 2, 128], U32)
    cx.m_wa = din("m_wa", [128, 2, 128])
    cx.wa_sink = din("wa_sink", [2, 8])
    cx.m_navalid = din("m_navalid", [128, 21, 128])
    cx.w_branch = din("w_branch", [2, 4, 512, 1024])
    cx.w_out = din("w_out", [2, 1024, 1024])
    cx.ln_gT = din("ln_gT", [2, 2, 128, 8])
    cx.ln_bT = din("ln_bT", [2, 2, 128, 8])
    cx.moe_w_group = din("moe_w_group", [2, 1024, 4])
    cx.moe_b_group = din("moe_b_group", [2, 4])
    cx.moe_w_expert = din("moe_w_expert", [2, 1024, 32])
    cx.moe_b_expert = din("moe_b_expert", [2, 32])
    cx.moe_w_gate = din("moe_w_gate", [2, 4, 8, 1024, 256])
    cx.moe_w_up = din("moe_w_up", [2, 4, 8, 1024, 256])
    cx.moe_w_down = din("moe_w_down", [2, 4, 8, 256, 1024])
    cx.m_sel = din("m_sel", [32, 32, 128])
    cx.m_ident32 = din("m_ident32", [128, 128])
    cx.na_bias = din("na_bias", [2, 8, 128, 21, 128])


def declare_scratch(cx, kb, debug, as_input=(), as_output=()):
    kind = "ExternalOutput" if debug else "Internal"

    def ds(name, shape, dt):
        b = kb.dram(name, shape, dt, kind=("ExternalInput" if name in as_input else ("ExternalOutput" if name in as_output else kind)))
        setattr(cx, name, b)
        return b
    ds("XT", [D, T], F32)
    ds("QA", [256, T], BF16)
    ds("GA", [512, T], BF16)
    ds("LR1", [33, T], F32)
    ds("QB", [512, T], BF16)
    ds("KB_", [512, T], BF16)
    ds("QC", [512, T], BF16)
    ds("KC", [128, T], BF16)
    ds("QD", [512, T], BF16)
    ds("GD", [512, T], BF16)
    ds("GATE", [4096, T], BF16)
    ds("KVA_t", [128, NT, 768], BF16)
    ds("VB_t", [128, NT, 512], BF16)
    ds("VC_t", [128, NT, 128], BF16)
    ds("VD_t", [128, NT, 512], BF16)
    ds("LGD_t", [128, NT, 1024], F32)
    ds("KD_t", [128, NT, 1024], BF16)
    ds("LGA_t", [128, NT, 512], F32)
    ds("MODT", [2, 128, 96], F32)
    ds("HX2", [D, T], BF16)
    ds("WGB", [32, 128, 2048], BF16)
    ds("WUB", [32, 128, 2048], BF16)
    ds("WDB", [4, 128, 16384], BF16)
    ds("WG_t", [128, NT, 32], F32)
    ds("WGT", [32, T], F32)
    ds("BR_A", [512, T], BF16)
    ds("BR_B", [512, T], BF16)
    ds("BR_C", [512, T], BF16)
    ds("BR_D", [512, T], BF16)


def stage_mod(cx, kb, nc, modT):
    with kb.scope():
        cv = kb.sbuf("cv", [128, 8, 2], F32)
        sc = kb.sbuf("sc", [128, 8, 2], F32)
        kb.dma("sp", cv[:], cx.cvec[:], reads=[cx.cvec], writes=[cv])
        kb.op("act", lambda: nc.scalar.activation(out=sc[:], in_=cv[:], func=AF.Silu), reads=[cv], writes=[sc])
        wbufs = [kb.sbuf("wm%d" % i, [128, 8, 1024], F32) for i in range(2)]
        bm = kb.sbuf("bm", [128, 48], F32)
        ps = kb.psum("ps_mod", [128, 96])
        wi = 0
        for l in range(2):
            kb.dma("sp", bm[:], cx.b_modT[l], reads=[cx.b_modT], writes=[bm])
            for g in range(6):
                wb = wbufs[wi % 2]
                wi += 1
                src = cx.w_mod[l, :, g * 1024:(g + 1) * 1024].rearrange("(kc p) c -> p kc c", p=128)
                kb.dma("sp", wb[:], src, reads=[cx.w_mod], writes=[wb])
                for j in range(8):
                    jj = g * 8 + j
                    fns = []
                    for kc in range(8):
                        fns.append(lambda kc=kc, j=j, jj=jj, wb=wb: nc.tensor.matmul(
                            ps[:, 2 * jj:2 * jj + 2], lhsT=wb[:, kc, j * 128:(j + 1) * 128], rhs=sc[:, kc, :],
                            start=(kc == 0), stop=(kc == 7)))
                    kb.mm(fns, reads=[wb, sc], writes=[ps])
            m = modT[l]
            kb.op("dve", lambda m=m: nc.vector.tensor_tensor(
                out=m[:], in0=ps[:].rearrange("p (j c) -> p j c", c=2),
                in1=bm[:].unsqueeze(2).broadcast_to([128, 48, 2]), op=ALU.add),
                reads=[ps, bm], writes=[m])
            kb.dma("sp", cx.MODT[l], m[:].rearrange("p j c -> p (j c)"), reads=[m], writes=[cx.MODT])


def stage_modulate(cx, kb, nc, src_dram, modv, shift_idx, scale_idx, hxT, hx32=None):
    with kb.scope():
        onep = kb.sbuf("onep", [128, 8, 2], F32)
        kb.op("dve", lambda: nc.vector.tensor_scalar(
            out=onep[:], in0=modv[:, scale_idx * 8:(scale_idx + 1) * 8, :], scalar1=1.0, scalar2=None, op0=ALU.add),
            reads=[modv], writes=[onep])
        xb = [kb.sbuf("xb%d" % i, [128, 8, 512], F32) for i in range(2)]
        for bi, (t0, nb) in enumerate(BLKS):
            col = blkcol(t0)
            x = xb[bi % 2]
            kb.dma("sp", x[:, :, :nb], src_dram[:, t0:t0 + nb].rearrange("(kc p) t -> p kc t", p=128),
                   reads=[src_dram], writes=[x])
            for kc in range(8):
                eng = "dve" if kc % 2 == 0 else "pool"
                h = nc.vector if eng == "dve" else nc.gpsimd
                kb.op(eng, lambda h=h, kc=kc, x=x: h.tensor_scalar(
                    out=hxT[:, kc, t0:t0 + nb], in0=x[:, kc, :nb],
                    scalar1=onep[:, kc, col:col + 1], scalar2=modv[:, shift_idx * 8 + kc, col:col + 1],
                    op0=ALU.mult, op1=ALU.add), reads=[x, onep, modv], writes=[hxT])


FM_GROUPS = [
    ("a_q", "QA", "copy"), ("a_g", "GA", "silu"), ("a_lr", "LR1", "copy32"),
    ("b_q", "QB", "copy"), ("b_k", "KB_", "copy"),
    ("c_q", "QC", "rope"), ("c_k", "KC", "rope"),
    ("d_q", "QD", "silu"), ("d_g", "GD", "silu"), ("gate", "GATE", "sigmoid"),
]


def stage_inproj_fm(cx, kb, nc, l, hxT, only=None):
    with kb.scope():
        wsl = [kb.sbuf("wsl%d" % i, [128, 8, 512], BF16) for i in range(2)]
        stg = [kb.sbuf("stg%d" % i, [128, T], BF16) for i in range(2)]
        stg32 = kb.sbuf("stg32", [32, T], F32)
        pss = [kb.psum("psf%d" % i, [128, 512]) for i in range(4)]
        psr = [kb.psum("psr%d" % i, [128, 512]) for i in range(2)]
        cosT = kb.sbuf("cosT", [128, T], F32)
        sinT = kb.sbuf("sinT", [128, T], F32)
        ptm = kb.sbuf("ptm", [128, 128], BF16)
        tmpb = [kb.sbuf("tmpb%d" % i, [128, 512], BF16) for i in range(2)]
        t1 = [kb.sbuf("t1_%d" % i, [128, 512], F32) for i in range(2)]
        t2 = [kb.sbuf("t2_%d" % i, [128, 512], F32) for i in range(2)]
        ones = kb.sbuf("ones1", [1, T], F32)
        kb.dma("sp", cosT[:], cx.rope_cos[:], reads=[cx.rope_cos], writes=[cosT])
        kb.dma("sp", sinT[:], cx.rope_sin[:], reads=[cx.rope_sin], writes=[sinT])
        kb.dma("pool", ptm[:], cx.rope_pt[:], reads=[cx.rope_pt], writes=[ptm])
        kb.op("dve", lambda: nc.vector.memset(ones[:], 1.0), writes=[ones])
        kb.dma("sp", cx.LR1[32:33, :], ones[:], reads=[ones], writes=[], acc_writes=[cx.LR1])
        nsl = 0
        nps = 0
        nst = 0
        nrp = 0
        for (name, dest, kind) in FM_GROUPS:
            if only is not None and name not in only:
                continue
            c0, nc_ = COLS[name]
            dbuf = getattr(cx, dest)
            for s0 in range(0, nc_, 512):
                sw = min(512, nc_ - s0)
                w = wsl[nsl % 2]
                nsl += 1
                kb.dma("pool", w[:, :, :sw],
                       cx.w_in[l, :, c0 + s0:c0 + s0 + sw].rearrange("(kc p) c -> p kc c", p=128),
                       reads=[cx.w_in], writes=[w])
                for m0 in range(0, sw, 128):
                    mw = min(128, sw - m0)
                    if kind == "copy32":
                        st = stg32
                    else:
                        st = stg[nst % 2]
                        nst += 1
                    for bi, (t0, nb) in enumerate(BLKS):
                        ps = pss[nps % 4]
                        nps += 1
                        fns = [(lambda kc=kc, ps=ps, w=w: nc.tensor.matmul(
                            ps[:mw, :nb], lhsT=w[:, kc, m0:m0 + mw], rhs=hxT[:, kc, t0:t0 + nb],
                            start=(kc == 0), stop=(kc == 7))) for kc in range(8)]
                        kb.mm(fns, reads=[w, hxT], writes=[ps])
                        o = st[:mw, t0:t0 + nb]
                        if kind in ("copy", "copy32"):
                            if nps % 2 == 0:
                                kb.op("dve", lambda ps=ps, o=o: nc.vector.tensor_copy(out=o, in_=ps[:mw, :nb]),
                                      reads=[ps], writes=[st])
                            else:
                                kb.op("act", lambda ps=ps, o=o: nc.scalar.copy(out=o, in_=ps[:mw, :nb]),
                                      reads=[ps], writes=[st])
                        elif kind == "silu":
                            kb.op("act", lambda ps=ps, o=o: nc.scalar.activation(out=o, in_=ps[:mw, :nb], func=AF.Silu),
                                  reads=[ps], writes=[st])
                        elif kind == "sigmoid":
                            kb.op("act", lambda ps=ps, o=o: nc.scalar.activation(out=o, in_=ps[:mw, :nb], func=AF.Sigmoid),
                                  reads=[ps], writes=[st])
                        elif kind == "rope":
                            tb = tmpb[nrp % 2]
                            a1 = t1[nrp % 2]
                            a2 = t2[nrp % 2]
                            pr = psr[nrp % 2]
                            nrp += 1
                            kb.op("act", lambda ps=ps, tb=tb: nc.scalar.copy(out=tb[:mw, :nb], in_=ps[:mw, :nb]),
                                  reads=[ps], writes=[tb])
                            kb.mm([lambda pr=pr, tb=tb: nc.tensor.matmul(pr[:mw, :nb], lhsT=ptm[:mw, :mw], rhs=tb[:mw, :nb],
                                                                         start=True, stop=True)],
                                  reads=[ptm, tb], writes=[pr])
                            kb.op("dve", lambda tb=tb, a1=a1: nc.vector.tensor_tensor(
                                out=a1[:mw, :nb], in0=tb[:mw, :nb], in1=cosT[:mw, t0:t0 + nb], op=ALU.mult),
                                reads=[tb, cosT], writes=[a1])
                            kb.op("dve", lambda pr=pr, a2=a2: nc.vector.tensor_tensor(
                                out=a2[:mw, :nb], in0=pr[:mw, :nb], in1=sinT[:mw, t0:t0 + nb], op=ALU.mult),
                                reads=[pr, sinT], writes=[a2])
                            kb.op("pool", lambda a1=a1, a2=a2, o=o: nc.gpsimd.tensor_tensor(
                                out=o, in0=a1[:mw, :nb], in1=a2[:mw, :nb], op=ALU.add),
                                reads=[a1, a2], writes=[st])
                    kb.dma("sp", dbuf[s0 + m0:s0 + m0 + mw, :], st[:mw, :], reads=[st], writes=[], acc_writes=[dbuf])


TM_GROUPS = [
    (256, 512, "KVA_t", 0, "copy"), (768, 256, "KVA_t", 512, "copy"),
    (2592, 512, "VB_t", 0, "copy"), (3744, 128, "VC_t", 0, "copy"),
    (5408, 512, "VD_t", 0, "copy"),
    (4384, 512, None, 0, "hgf"), (4896, 512, None, 512, "hgf"),
]


def stage_inproj_tm(cx, kb, nc, l, hxT, lbt, omlt, only=None):
    with kb.scope():
        wsl = [kb.sbuf("wtl%d" % i, [128, 8, 512], BF16) for i in range(2)]
        pss = [kb.psum("pst%d" % i, [128, 512]) for i in range(4)]
        stb = [kb.sbuf("stb%d" % i, [128, 512], BF16) for i in range(4)]
        e1 = [kb.sbuf("e1_%d" % i, [128, 512], F32) for i in range(2)]
        r1 = [kb.sbuf("r1_%d" % i, [128, 512], F32) for i in range(2)]
        lg = [kb.sbuf("lg_%d" % i, [128, 512], F32) for i in range(2)]
        kk = [kb.sbuf("kk_%d" % i, [128, 512], BF16) for i in range(2)]
        nsl = nps = nst = nh = 0
        for (c0, ncol, dest, doff, kind) in TM_GROUPS:
            if only is not None and kind not in only:
                continue
            w = wsl[nsl % 2]
            nsl += 1
            kb.dma("pool", w[:, :, :ncol], cx.w_in[l, :, c0:c0 + ncol].rearrange("(kc p) c -> p kc c", p=128),
                   reads=[cx.w_in], writes=[w])
            for ti in range(NT):
                ts = slice(ti * 128, (ti + 1) * 128)
                ps = pss[nps % 4]
                nps += 1
                fns = [(lambda kc=kc, ps=ps, w=w: nc.tensor.matmul(
                    ps[:, :ncol], lhsT=hxT[:, kc, ts], rhs=w[:, kc, :ncol],
                    start=(kc == 0), stop=(kc == 7))) for kc in range(8)]
                kb.mm(fns, reads=[w, hxT], writes=[ps])
                if kind == "copy":
                    st = stb[nst % 4]
                    nst += 1
                    if nst % 2 == 0:
                        kb.op("dve", lambda ps=ps, st=st: nc.vector.tensor_copy(out=st[:, :ncol], in_=ps[:, :ncol]),
                              reads=[ps], writes=[st])
                    else:
                        kb.op("act", lambda ps=ps, st=st: nc.scalar.copy(out=st[:, :ncol], in_=ps[:, :ncol]),
                              reads=[ps], writes=[st])
                    db = getattr(cx, dest)
                    kb.dma("sp", db[:, ti, doff:doff + ncol], st[:, :ncol], reads=[st], writes=[], acc_writes=[db])
                else:
                    e = e1[nh % 2]
                    r = r1[nh % 2]
                    g = lg[nh % 2]
                    k = kk[nh % 2]
                    nh += 1
                    cs = slice(doff, doff + 512)
                    kb.op("act", lambda ps=ps, e=e: nc.scalar.activation(out=e[:], in_=ps[:], func=AF.Exp, scale=-1.0),
                          reads=[ps], writes=[e])
                    kb.op("pool", lambda e=e: nc.gpsimd.tensor_scalar(
                        out=e[:], in0=e[:], scalar1=1.0, scalar2=None, op0=ALU.add),
                        reads=[e], writes=[e])
                    kb.op("dve", lambda e=e, r=r: nc.vector.reciprocal(out=r[:], in_=e[:]),
                          reads=[e], writes=[r])
                    kb.op("dve", lambda r=r: nc.vector.tensor_tensor(out=r[:], in0=r[:], in1=omlt[:, cs], op=ALU.mult),
                          reads=[r, omlt], writes=[r])
                    kb.op("pool", lambda r=r: nc.gpsimd.tensor_tensor(out=r[:], in0=r[:], in1=lbt[:, cs], op=ALU.add),
                          reads=[r, lbt], writes=[r])
                    kb.op("act", lambda r=r, g=g: nc.scalar.activation(out=g[:], in_=r[:], func=AF.Ln),
                          reads=[r], writes=[g])
                    kb.op("pool", lambda r=r, k=k: nc.gpsimd.tensor_scalar(
                        out=k[:], in0=r[:], scalar1=-1.0, scalar2=1.0, op0=ALU.mult, op1=ALU.add),
                        reads=[r], writes=[k])
                    kb.dma("sp", cx.LGD_t[:, ti, cs], g[:], reads=[g], writes=[], acc_writes=[cx.LGD_t])
                    kb.dma("sp", cx.KD_t[:, ti, cs], k[:], reads=[k], writes=[], acc_writes=[cx.KD_t])


def stage_lb(cx, kb, nc, lbt, omlt):
    with kb.scope():
        gam = kb.sbuf("gam", [128, 2, 2, 512], F32)
        kb.dma("sp", gam[:].rearrange("p a b c -> p (a b c)"),
               cx.hg_gamma[:].rearrange("a b c -> (a b c)").partition_broadcast(128),
               reads=[cx.hg_gamma], writes=[gam])
        d = kb.sbuf("gamd", [128, 2, 512], F32)
        kb.op("dve", lambda: nc.vector.tensor_tensor(out=d[:], in0=gam[:, :, 0, :], in1=gam[:, :, 1, :], op=ALU.subtract),
              reads=[gam], writes=[d])
        kb.op("act", lambda: nc.scalar.activation(out=d[:], in_=d[:], func=AF.Exp), reads=[d], writes=[d])
        l1 = lbt[1]
        kb.op("dve", lambda: nc.vector.tensor_scalar(
            out=d[:], in0=d[:], scalar1=1.0, scalar2=None, op0=ALU.add), reads=[d], writes=[d])
        kb.op("dve", lambda: nc.vector.reciprocal(out=l1[:], in_=d[:].rearrange("p a c -> p (a c)")),
              reads=[d], writes=[l1])
        l0 = lbt[0]
        kb.op("dve", lambda: nc.vector.memset(l0[:], 0.0), writes=[l0])
        for l in range(2):
            o = omlt[l]
            lb = lbt[l]
            kb.op("dve", lambda o=o, lb=lb: nc.vector.tensor_scalar(
                out=o[:], in0=lb[:], scalar1=-1.0, scalar2=1.0, op0=ALU.mult, op1=ALU.add),
                reads=[lb], writes=[o])


def stage_gla_gate(cx, kb, nc, l):
    with kb.scope():
        wlr = kb.sbuf("wlr", [33, 512], F32)
        lr1 = kb.sbuf("lr1", [33, T], F32)
        kb.op("dve", lambda: nc.vector.memset(wlr[:], 0.0), writes=[wlr])
        kb.dma("sp", wlr[0:16, 0:256], cx.gla_lr_w[l, 0], reads=[cx.gla_lr_w], writes=[wlr])
        kb.dma("sp", wlr[16:32, 256:512], cx.gla_lr_w[l, 1], reads=[cx.gla_lr_w], writes=[wlr])
        kb.dma("sp", wlr[32:33, :], cx.gla_lr_b[l].rearrange("a c -> (a c)").unsqueeze(0),
               reads=[cx.gla_lr_b], writes=[wlr])
        kb.dma("sp", lr1[:], cx.LR1[:], reads=[cx.LR1], writes=[lr1])
        pss = [kb.psum("psg%d" % i, [128, 512]) for i in range(2)]
        ee = [kb.sbuf("ge%d" % i, [128, 512], F32) for i in range(2)]
        gg = [kb.sbuf("gg%d" % i, [128, 512], F32) for i in range(2)]
        for ti in range(NT):
            ts = slice(ti * 128, (ti + 1) * 128)
            ps = pss[ti % 2]
            e = ee[ti % 2]
            g = gg[ti % 2]
            kb.mm([lambda ps=ps: nc.tensor.matmul(ps[:], lhsT=lr1[:, ts], rhs=wlr[:], start=True, stop=True)],
                  reads=[lr1, wlr], writes=[ps])
            kb.op("act", lambda ps=ps, e=e: nc.scalar.activation(out=e[:], in_=ps[:], func=AF.Exp, scale=-1.0),
                  reads=[ps], writes=[e])
            kb.op("act", lambda e=e, g=g: nc.scalar.activation(out=g[:], in_=e[:], func=AF.Ln, bias=1.0),
                  reads=[e], writes=[g])
            kb.dma("sp", cx.LGA_t[:, ti, :], g[:], reads=[g], writes=[], acc_writes=[cx.LGA_t])

SCAN_CFG = {
    "gla": dict(H=4, dk=64, qsrc="QA", qscale=0.125, ksrc="KVA_t", kz=0, kcol0=0, vsrc="KVA_t", vcol0=256,
                gsrc="LGA_t", gz=256, gate="GA", dest="BR_A", mask="mg"),
    "hgrn": dict(H=4, dk=128, qsrc="QD", qscale=1.0, ksrc="KD_t", kz=512, kcol0=0, vsrc="VD_t", vcol0=0,
                 gsrc="LGD_t", gz=512, gate="GD", dest="BR_D", mask="mh"),
}


def view(b):
    return Buf(b.t, b.name + "_v")


def stage_scan(cx, kb, nc, which, heads=None):
    cfg = SCAN_CFG[which]
    H, dk = cfg["H"], cfg["dk"]
    dv = 128
    qsrc, ksrc, vsrc, gsrc = (getattr(cx, cfg[k]) for k in ("qsrc", "ksrc", "vsrc", "gsrc"))
    gate, dest = getattr(cx, cfg["gate"]), getattr(cx, cfg["dest"])
    mq_d, mk_d = getattr(cx, cfg["mask"] + "_mq"), getattr(cx, cfg["mask"] + "_mk")
    with kb.scope():
        mq = kb.sbuf("mq", [128, 2, 130], F32)
        mk = kb.sbuf("mk", [128, 2, 128], F32)
        ma = kb.sbuf("ma", [128, 2, 128], F32)
        ident = kb.sbuf("ident", [128, 128], BF16)
        onesdv = kb.sbuf("onesdv", [128, 128], F32)
        kb.dma("sp", mq[:], mq_d[:], reads=[mq_d], writes=[mq])
        kb.dma("sp", mk[:], mk_d[:], reads=[mk_d], writes=[mk])
        kb.dma("sp", ma[:], cx.m_ma[:], reads=[cx.m_ma], writes=[ma])
        mau = kb.sbuf("mau", [128, 2, 128], U32)
        kb.dma("sp", mau[:], cx.m_mau[:], reads=[cx.m_mau], writes=[mau])
        kb.dma("pool", ident[:], cx.m_ident[:], reads=[cx.m_ident], writes=[ident])
        kb.op("dve", lambda: nc.vector.memset(onesdv[:], 1.0 / dv), writes=[onesdv])
        vt = kb.sbuf("vt", [128, NT, dv], BF16)
        qT = kb.sbuf("qT", [128, T], BF16)
        gsil = kb.sbuf("gsil", [128, T], BF16)
        obr = kb.sbuf("obr", [128, T], BF16)
        gts = [kb.sbuf("gt%d" % i, [128, NT, dk], F32) for i in range(2)]
        kts = [kb.sbuf("kt%d" % i, [128, NT, dk], BF16) for i in range(2)]
        qt = [kb.sbuf("qt%d" % z, [128, T], BF16) for z in range(2)]
        atm = [kb.sbuf("atm%d" % z, [128, NT, 128], BF16) for z in range(2)]
        Sp = [kb.sbuf("Sp%d" % z, [128, 2 * NT, dv], BF16) for z in range(2)]
        for z in range(2):
            kb.op("pool", lambda z=z: nc.gpsimd.memset(atm[z][:], 0.0), writes=[atm[z]])
        Sst = [kb.sbuf("Sst%d" % i, [128, dv], F32) for i in range(2)]
        R = 3
        eq = [kb.sbuf("eq%d" % i, [128, 128], F32) for i in range(R)]
        ek = [kb.sbuf("ek%d" % i, [128, 128], F32) for i in range(R)]
        ekh = [kb.sbuf("ekh%d" % i, [128, 128], F32) for i in range(R)]
        ktl = [kb.sbuf("ktl%d" % i, [128, 128], BF16) for i in range(R)]
        khat = [kb.sbuf("khat%d" % i, [128, 256], BF16) for i in range(R)]
        ktr = [kb.sbuf("ktr%d" % i, [128, 128], F32) for i in range(R)]
        atr = [kb.sbuf("atr%d" % i, [128, 128], F32) for i in range(R)]
        usb = [kb.sbuf("usb%d" % i, [128, 256], F32) for i in range(R)]
        osb = [kb.sbuf("osb%d" % i, [128, 512], F32) for i in range(2)]
        cm = kb.sbuf("cm", [128, 2], F32)
        kb.op("dve", lambda: nc.vector.memset(cm[:], 0.0), writes=[cm])
        kb.op("dve", lambda: nc.vector.memset(cm[0:64, 0:1], 1.0), writes=[cm])
        kb.op("dve", lambda: nc.vector.memset(cm[64:128, 1:2], 1.0), writes=[cm])
        er2 = [kb.sbuf("er%d" % i, [128, 2], F32) for i in range(R)]
        ed2 = [kb.sbuf("ed%d" % i, [128, 2], F32) for i in range(R)]
        psA = [kb.psum("psA%d" % i, [128, 512]) for i in range(2)]
        psB = [kb.psum("psB%d" % i, [128, 512]) for i in range(2)]
        psA_ec = [view(p) for p in psA]
        import os
        if True:
            psK = [kb.psum("psK%d" % i, [128, 512]) for i in range(2)]
        else:
            psK = psA
        psA_kt = [view(p) for p in psK]
        psA_e2 = [view(p) for p in psK]
        psB_u = [view(p) for p in psB]
        psB_at = [view(p) for p in psB]
        psO = [kb.psum("psO%d" % i, [128, 512]) for i in range(1)] * 2
        psM = [kb.psum("psM%d" % i, [128, 512]) for i in range(1)] * 2
        sq = [kb.sbuf("sq%d" % i, [128, 512], F32) for i in range(2)]
        sd = [kb.sbuf("sd%d" % i, [128, 512], F32) for i in range(2)]
        on = [kb.sbuf("on%d" % i, [128, 512], F32) for i in range(2)]
        eps_t = kb.sbuf("eps_t", [128, 1], F32)
        kb.op("dve", lambda: nc.vector.memset(eps_t[:], 1e-6), writes=[eps_t])

        it = 0
        ghz = 0
        import os
        for h in (range(H) if heads is None else heads):
            vc0 = cfg["vcol0"] + h * dv
            kb.dma("sp", vt[:], vsrc[:, :, vc0:vc0 + dv], reads=[vsrc], writes=[vt])
            kb.dma("sp", qT[:dk, :], qsrc[h * dk:(h + 1) * dk, :], reads=[qsrc], writes=[qT])
            kb.dma("sp", gsil[:], gate[h * dv:(h + 1) * dv, :], reads=[gate], writes=[gsil])
            for z in range(2):
                gt = gts[ghz % 2]
                kt = kts[ghz % 2]
                ghz += 1
                gc0 = z * cfg["gz"] + h * dk
                kc0 = cfg["kcol0"] + z * cfg["kz"] + h * dk
                kb.dma("sp", gt[:], gsrc[:, :, gc0:gc0 + dk], reads=[gsrc], writes=[gt])
                if os.environ.get("KTMEMSET"):
                    kb.op("dve", lambda kt=kt: nc.vector.memset(kt[:], 0.0), writes=[kt])
                kb.dma("sp", kt[:], ksrc[:, :, kc0:kc0 + dk], reads=[ksrc], writes=[kt])
                order = [32, 33] + list(range(32)) if z == 0 else [33, 32] + list(range(31, -1, -1))
                import os
                DBG = os.environ.get("SCAN_DBG", "")
                if DBG == "loads":
                    continue
                if DBG.startswith("nt"):
                    order = order[:int(DBG[2:])]
                S = Sst[0]
                kb.op("dve", lambda S=S: nc.vector.memset(S[:], 0.0), writes=[S])
                si = 0
                def p1(ti, a, r):
                    ts = slice(ti * 128, (ti + 1) * 128)
                    pa, pb = psA[a], psB[a]
                    pk = psK[a]
                    kb.mm([lambda pa=pa, gt=gt: nc.tensor.matmul(pa[:dk, 0:130], lhsT=gt[:, ti, :], rhs=mq[:, z, :],
                                                                 start=True, stop=True)],
                          reads=[gt, mq], writes=[psA_ec[a]])
                    kb.mm([lambda pk=pk, kt=kt: nc.tensor.matmul(pk[:dk, 256:384], lhsT=(ident[:, :dk] if os.environ.get("A2ID") else kt[:, ti, :]), rhs=ident[:],
                                                                 start=True, stop=True)],
                          reads=[kt, ident], writes=[psA_kt[a]])
                    kb.mm([lambda pk=pk, gt=gt: nc.tensor.matmul(pk[:, 384:384 + dk], lhsT=mk[:, z, :], rhs=gt[:, ti, :],
                                                                 start=True, stop=True)],
                          reads=[gt, mk], writes=[psA_e2[a]])
                    kb.op("act", lambda pa=pa, o=eq[r]: nc.scalar.activation(out=o[:dk, :], in_=pa[:dk, 0:128], func=AF.Exp),
                          reads=[psA_ec[a]], writes=[eq[r]])
                    kb.op("act", lambda pa=pa, o=ek[r]: nc.scalar.activation(out=o[:dk, :], in_=pa[:dk, 0:128], func=AF.Exp, scale=-1.0),
                          reads=[psA_ec[a]], writes=[ek[r]])
                    kb.op("act", lambda pa=pa, o=er2[r]: nc.scalar.activation(out=o[:dk, :], in_=pa[:dk, 128:130], func=AF.Exp, scale=0.5),
                          reads=[psA_ec[a]], writes=[er2[r]])
                    kb.op("act", lambda pa=pa, o=ed2[r]: nc.scalar.activation(out=o[:dk, :], in_=pa[:dk, 128:130], func=AF.Exp),
                          reads=[psA_ec[a]], writes=[ed2[r]])
                    kb.op("act", lambda pk=pk, o=ekh[r]: nc.scalar.activation(out=o[:, :dk], in_=pk[:, 384:384 + dk], func=AF.Exp),
                          reads=[psA_e2[a]], writes=[ekh[r]])
                    qz = qt[z]
                    kb.op("dve", lambda qz=qz, e=eq[r]: nc.vector.scalar_tensor_tensor(
                        out=qz[:dk, ts], in0=qT[:dk, ts], scalar=cfg["qscale"], in1=e[:dk, :], op0=ALU.mult, op1=ALU.mult),
                        reads=[qT, eq[r]], writes=[], acc_writes=[qz])
                    kb.op("act", lambda pk=pk, o=ktr[r]: nc.scalar.copy(out=o[:dk, :], in_=pk[:dk, 256:384]),
                          reads=[psA_kt[a]], writes=[ktr[r]])
                    kb.op("dve", lambda e=ek[r], o=ktl[r], kr=ktr[r]: nc.vector.tensor_tensor(
                        out=o[:dk, :], in0=kr[:dk, :], in1=e[:dk, :], op=ALU.mult),
                        reads=[ktr[r], ek[r]], writes=[ktl[r]])
                    for c in range(2):
                        kb.op("dve", lambda kt=kt, e=ekh[r], o=khat[r], c=c: nc.vector.scalar_tensor_tensor(
                            out=o[:, c * 128:c * 128 + dk], in0=kt[:, ti, :], scalar=cm[:, c:c + 1], in1=e[:, :dk],
                            op0=ALU.mult, op1=ALU.mult),
                            reads=[kt, ekh[r], cm], writes=[], acc_writes=[khat[r]])
                    return None

                def p2(ti, a, r, S, si):
                    ts = slice(ti * 128, (ti + 1) * 128)
                    pa, pb = psA[a], psB[a]
                    qz = qt[z]
                    kb.mm([(lambda c=c, pb=pb, kh=khat[r]: nc.tensor.matmul(
                        pb[:dk, c * 128:(c + 1) * 128], lhsT=kh[:, c * 128:c * 128 + dk],
                        rhs=vt[:, ti, :], start=True, stop=True)) for c in range(2)],
                        reads=[khat[r], vt], writes=[psB_u[a]])
                    kb.mm([lambda pb=pb, k_=ktl[r], qz=qz: nc.tensor.matmul(
                        pb[:, 256:384], lhsT=k_[:dk, :], rhs=qz[:dk, ts], start=True, stop=True)],
                        reads=[ktl[r], qz], writes=[psB_at[a]])
                    az = atm[z]
                    kb.op("act", lambda pb=pb, o=atr[r]: nc.scalar.copy(out=o[:], in_=pb[:, 256:384]),
                          reads=[psB_at[a]], writes=[atr[r]])
                    kb.op("dve", lambda az=az, o=atr[r]: nc.vector.copy_predicated(
                        out=az[:, ti, :], mask=mau[:, z, :], data=o[:]),
                        reads=[atr[r], mau], writes=[], acc_writes=[az])
                    kb.op("act", lambda pb=pb, o=usb[r]: nc.scalar.copy(out=o[:dk, :], in_=pb[:dk, 0:256]),
                          reads=[psB_u[a]], writes=[usb[r]])
                    spz = Sp[z]
                    for c in ((0, 1) if z == 0 else (1, 0)):
                        ci = 2 * ti + c
                        kb.op("act", lambda S=S, spz=spz, ci=ci, e=er2[r], c=c: nc.scalar.activation(
                            out=spz[:dk, ci, :], in_=S[:dk, :], func=AF.Copy, scale=e[:dk, c:c + 1]),
                            reads=[S, er2[r]], writes=[], acc_writes=[spz])
                        S2 = Sst[(si + 1) % 2]
                        si += 1
                        kb.op("dve", lambda S=S, S2=S2, e=ed2[r], u=usb[r], c=c: nc.vector.scalar_tensor_tensor(
                            out=S2[:dk, :], in0=S[:dk, :], scalar=e[:dk, c:c + 1], in1=u[:dk, c * 128:(c + 1) * 128],
                            op0=ALU.mult, op1=ALU.add),
                            reads=[S, ed2[r], usb[r]], writes=[S2])
                        S = S2
                    return S, si

                sched = []
                for ti in order:
                    sched.append((ti, it % 2, it % R))
                    it += 1
                for idx in range(len(sched) + 1):
                    if idx < len(sched):
                        p1(*sched[idx])
                    if idx >= 1:
                        S, si = p2(*sched[idx - 1], S, si)
            groups = [list(range(g * 4, g * 4 + 4)) for g in range(8)] + [[32, 33]]
            if DBG:
                groups = []
            for gi, tiles in enumerate(groups):
                po = psO[gi % 2]
                pm = psM[gi % 2]
                nb = len(tiles) * 128
                t0 = tiles[0] * 128
                fns = []
                for k_, ti in enumerate(tiles):
                    cs = k_ * 128
                    for z in range(2):
                        fns.append(lambda po=po, ti=ti, cs=cs, z=z: nc.tensor.matmul(
                            po[:, cs:cs + 128], lhsT=vt[:, ti, :], rhs=atm[z][:, ti, :], start=(z == 0), stop=False))
                        for c in range(2):
                            fns.append(lambda po=po, ti=ti, cs=cs, z=z, c=c: nc.tensor.matmul(
                                po[:, cs + c * 64:cs + (c + 1) * 64], lhsT=Sp[z][:dk, 2 * ti + c, :],
                                rhs=qt[z][:dk, ti * 128 + c * 64:ti * 128 + (c + 1) * 64],
                                start=False, stop=(z == 1)))
                kb.mm(fns, reads=[vt, atm[0], atm[1], Sp[0], Sp[1], qt[0], qt[1]], writes=[po])
                s_, d_, o_ = sq[gi % 2], sd[gi % 2], on[gi % 2]
                kb.op("act", lambda po=po, s_=s_: nc.scalar.activation(out=s_[:, :nb], in_=po[:, :nb], func=AF.Square),
                      reads=[po], writes=[s_])
                kb.mm([lambda pm=pm, s_=s_: nc.tensor.matmul(pm[:, :nb], lhsT=onesdv[:], rhs=s_[:, :nb], start=True, stop=True)],
                      reads=[onesdv, s_], writes=[pm])
                kb.op("act", lambda pm=pm, d_=d_: nc.scalar.activation(out=d_[:, :nb], in_=pm[:, :nb], func=AF.Sqrt, bias=eps_t[:]),
                      reads=[pm, eps_t], writes=[d_])
                kb.op("dve", lambda d_=d_: nc.vector.reciprocal(out=d_[:, :nb], in_=d_[:, :nb]), reads=[d_], writes=[d_])
                ob_ = osb[gi % 2]
                kb.op("act", lambda po=po, ob_=ob_: nc.scalar.copy(out=ob_[:, :nb], in_=po[:, :nb]), reads=[po], writes=[ob_])
                kb.op("dve", lambda ob_=ob_, d_=d_, o_=o_: nc.vector.tensor_tensor(
                    out=o_[:, :nb], in0=ob_[:, :nb], in1=d_[:, :nb], op=ALU.mult), reads=[ob_, d_], writes=[o_])
                kb.op("pool", lambda o_=o_: nc.gpsimd.tensor_tensor(
                    out=obr[:, t0:t0 + nb], in0=o_[:, :nb], in1=gsil[:, t0:t0 + nb], op=ALU.mult),
                    reads=[o_, gsil], writes=[], acc_writes=[obr])
            kb.dma("sp", dest[h * dv:(h + 1) * dv, :], obr[:], reads=[obr], writes=[], acc_writes=[dest])


def scan_masks():
    j = np.arange(128)[:, None]
    i = np.arange(128)[None, :]
    same = (j // 64 == i // 64).astype(np.float32)
    out = {}
    for name, gs in (("mg", -1.0 / 16.0), ("mh", 1.0)):
        mq = np.zeros((128, 2, 130), np.float32)
        mk = np.zeros((128, 2, 128), np.float32)
        mq[:, 0, :128] = gs * same * ((j <= i).astype(np.float32) - 0.5)
        mq[:, 1, :128] = gs * same * ((j >= i).astype(np.float32) - 0.5)
        for z in range(2):
            for c in range(2):
                mq[:, z, 128 + c] = gs * (np.arange(128) // 64 == c)
        mk[:, 0, :] = gs * same * (j > i)
        mk[:, 1, :] = gs * same * (j < i)
        out[name + "_mq"] = mq
        out[name + "_mk"] = mk
    ma = np.zeros((128, 2, 128), np.float32)
    ma[:, 0, :] = same * (j <= i)
    ma[:, 1, :] = same * (j >= i)
    out["m_ma"] = ma
    out["m_mau"] = ma.astype(np.uint32)
    out["m_ident"] = np.eye(128, dtype=np.float32)
    return out


def stage_wa(cx, kb, nc, l, groups=None):
    with kb.scope():
        wm = kb.sbuf("wm", [128, 2, 128], BF16)
        kb.dma("pool", wm[:], cx.m_wa[:], reads=[cx.m_wa], writes=[wm])
        ones64 = kb.sbuf("ones64", [128, 64], BF16)
        kb.op("dve", lambda: nc.vector.memset(ones64[:], 1.0), writes=[ones64])
        sk = kb.sbuf("sk", [64, 8], F32)
        kb.dma("sp", sk[:], cx.wa_sink[l].partition_broadcast(64), reads=[cx.wa_sink], writes=[sk])
        kb.op("act", lambda: nc.scalar.activation(out=sk[:], in_=sk[:], func=AF.Exp), reads=[sk], writes=[sk])
        q4 = kb.sbuf("q4", [64, 4, T], BF16)
        kT = kb.sbuf("kTc", [64, T], BF16)
        vt = kb.sbuf("vtc", [128, NT, 64], BF16)
        ost = kb.sbuf("ost", [64, 4, T], BF16)
        pS = [kb.psum("pS%d" % i, [128, 512]) for i in range(4)]
        pO = [kb.psum("pO%d" % i, [128, 512]) for i in range(2)]
        pD = [kb.psum("pD%d" % i, [128, 512]) for i in range(2)]
        R = 10
        pt = [kb.sbuf("pt%d" % i, [128, 4, 128], BF16) for i in range(R)]
        ptm = [kb.sbuf("ptm%d" % i, [128, 4, 128], BF16) for i in range(R)]
        osb = [kb.sbuf("osbw%d" % i, [64, 4, 128], F32) for i in range(2)]
        dsb = [kb.sbuf("dsbw%d" % i, [64, 4, 128], F32) for i in range(2)]
        it = 0
        for g in (range(2) if groups is None else groups):
            kb.dma("sp", q4[:], cx.QC[g * 256:(g + 1) * 256, :].rearrange("(hh d) t -> d hh t", d=64),
                   reads=[cx.QC], writes=[q4])
            kb.dma("sp", kT[:], cx.KC[g * 64:(g + 1) * 64, :], reads=[cx.KC], writes=[kT])
            kb.dma("sp", vt[:], cx.VC_t[:, :, g * 64:(g + 1) * 64], reads=[cx.VC_t], writes=[vt])
            def p1(qb):
                nonlocal it
                if qb < 32:
                    keys = []
                    if qb > 0:
                        keys.append((qb - 1, 0))
                    keys.append((qb, None))
                    if qb < 31:
                        keys.append((qb + 1, 1))
                    keys += [(32, None), (33, None)]
                else:
                    keys = [(32, None), (33, None)]
                qs = slice(qb * 128, (qb + 1) * 128)
                st_ = []
                for ki, (kt_i, mi) in enumerate(keys):
                    ps = pS[it % 4]
                    p_ = pt[it % R]
                    pm_ = ptm[it % R]
                    it += 1
                    kb.mm([lambda ps=ps, kt_i=kt_i: nc.tensor.matmul(
                        ps[:, :], lhsT=kT[:, kt_i * 128:(kt_i + 1) * 128], rhs=q4[:, :, qs], start=True, stop=True)],
                        reads=[kT, q4], writes=[ps])
                    kb.op("act", lambda ps=ps, p_=p_: nc.scalar.activation(
                        out=p_[:].rearrange("p h q -> p (h q)"), in_=ps[:, :], func=AF.Exp, scale=0.125),
                        reads=[ps], writes=[p_])
                    src = p_
                    if mi is not None:
                        eng = "dve" if it % 2 == 0 else "pool"
                        h_ = nc.vector if eng == "dve" else nc.gpsimd
                        kb.op(eng, lambda h_=h_, p_=p_, pm_=pm_, mi=mi: h_.tensor_tensor(
                            out=pm_[:], in0=p_[:], in1=wm[:, mi, :].unsqueeze(1).broadcast_to([128, 4, 128]), op=ALU.mult),
                            reads=[p_, wm], writes=[pm_])
                        src = pm_
                    st_.append((kt_i, src))
                return (qb, st_)

            def p2(state):
                qb, st_ = state
                qs = slice(qb * 128, (qb + 1) * 128)
                a = qb % 2
                po, pd = pO[a], pD[a]
                nk = len(st_)
                for ki, (kt_i, src) in enumerate(st_):
                    kb.mm([lambda po=po, src=src, kt_i=kt_i, ki=ki: nc.tensor.matmul(
                        po[:64, :], lhsT=vt[:, kt_i, :], rhs=src[:].rearrange("p h q -> p (h q)"),
                        start=(ki == 0), stop=(ki == nk - 1)),
                        lambda pd=pd, src=src, ki=ki: nc.tensor.matmul(
                        pd[:64, :], lhsT=ones64[:], rhs=src[:].rearrange("p h q -> p (h q)"),
                        start=(ki == 0), stop=(ki == nk - 1))],
                        reads=[vt, src, ones64], writes=[po, pd])
                o_, d_ = osb[a], dsb[a]
                kb.op("act", lambda po=po, o_=o_: nc.scalar.copy(out=o_[:].rearrange("p h q -> p (h q)"), in_=po[:64, :]),
                      reads=[po], writes=[o_])
                kb.op("act", lambda pd=pd, d_=d_: nc.scalar.copy(out=d_[:].rearrange("p h q -> p (h q)"), in_=pd[:64, :]),
                      reads=[pd], writes=[d_])
                kb.op("dve", lambda d_=d_: nc.vector.tensor_tensor(
                    out=d_[:], in0=d_[:], in1=sk[:, g * 4:(g + 1) * 4].unsqueeze(2).broadcast_to([64, 4, 128]), op=ALU.add),
                    reads=[d_, sk], writes=[d_])
                kb.op("dve", lambda d_=d_: nc.vector.reciprocal(out=d_[:], in_=d_[:]), reads=[d_], writes=[d_])
                kb.op("pool", lambda o_=o_, d_=d_: nc.gpsimd.tensor_tensor(
                    out=ost[:, :, qs], in0=o_[:], in1=d_[:], op=ALU.mult),
                    reads=[o_, d_], writes=[], acc_writes=[ost])

            prev = None
            for qb in range(NT):
                cur = p1(qb)
                if prev is not None:
                    p2(prev)
                prev = cur
            p2(prev)
            kb.dma("sp", cx.BR_C[g * 256:(g + 1) * 256, :].rearrange("(hh d) t -> d hh t", d=64), ost[:],
                   reads=[ost], writes=[], acc_writes=[cx.BR_C])


def wa_masks():
    kk = np.arange(128)[:, None]
    qq = np.arange(128)[None, :]
    m = np.zeros((128, 2, 128), np.float32)
    m[:, 0, :] = (kk >= qq)
    m[:, 1, :] = (kk <= qq)
    return {"m_wa": m}


def na_patterns():
    W = 64
    tok = np.arange(128)
    lr, lc = tok // W, tok % W
    pats = []
    keys = []
    for m in range(32):
        qr = 2 * m + lr[None, :]
        qc = lc[None, :]
        rs = np.clip(qr - 4, 0, 56)
        cs = np.clip(qc - 8, 0, 48)
        lst = []
        for kt_i in range(32):
            kr = 2 * kt_i + lr[:, None]
            kc = lc[:, None]
            valid = (kr >= rs) & (kr < rs + 8) & (kc >= cs) & (kc < cs + 16)
            if not valid.any():
                continue
            ri = np.clip(kr - qr + 7, 0, 14)
            ci = np.clip(kc - qc + 15, 0, 30)
            if 2 <= m <= 29:
                pid = kt_i - m + 2
                if m == 2:
                    pats.append((valid, ri, ci))
            else:
                pid = len(pats)
                pats.append((valid, ri, ci))
            lst.append((kt_i, pid))
        keys.append(lst)
    return keys, pats


def na_consts():
    keys, pats = na_patterns()
    n0 = len(keys[0]) + len(keys[1])
    order = list(range(n0, n0 + 5)) + list(range(0, n0)) + list(range(n0 + 5, len(pats)))
    remap = {old: new for new, old in enumerate(order)}
    pats2 = [pats[o] for o in order]
    keys2 = []
    for m, lst in enumerate(keys):
        if 2 <= m <= 29:
            keys2.append(lst)
        else:
            keys2.append([(k, remap[p]) for (k, p) in lst])
    valid = np.stack([p[0] for p in pats2], 1).astype(np.float32)
    ri = np.stack([p[1] for p in pats2], 1)
    ci = np.stack([p[2] for p in pats2], 1)
    return keys2, valid, ri, ci


def stage_na(cx, kb, nc, l, heads=None, bg_factory=None):
    keys_tab, valid_np, _, _ = na_consts()
    npat = valid_np.shape[1]
    with kb.scope():
        vmask = kb.sbuf("vmask", [128, npat, 128], F32)
        kb.dma("sp", vmask[:], cx.m_navalid[:], reads=[cx.m_navalid], writes=[vmask])
        ones64 = kb.sbuf("ones64n", [128, 64], BF16)
        kb.op("dve", lambda: nc.vector.memset(ones64[:], 1.0), writes=[ones64])
        qT = kb.sbuf("qTn", [64, T], BF16)
        kT = kb.sbuf("kTn", [64, T], BF16)
        vt = kb.sbuf("vtn", [128, NT, 64], BF16)
        ost = kb.sbuf("ostn", [64, T], BF16)
        bias = kb.sbuf("biasn", [128, npat, 128], F32)
        EB = kb.sbuf("EBn", [128, npat, 128], BF16)
        pS = [kb.psum("pSn%d" % i, [128, 512]) for i in range(4)]
        pO = [kb.psum("pOn%d" % i, [128, 512]) for i in range(2)]
        pD = [kb.psum("pDn%d" % i, [128, 512]) for i in range(2)]
        R = 6
        pt = [kb.sbuf("ptn%d" % i, [128, 4, 128], BF16) for i in range(R)]
        osb = [kb.sbuf("osbn%d" % i, [64, 256], F32) for i in range(2)]
        it = 0
        bg = None
        if bg_factory is not None:
            stw = [kb.sbuf("pcw%d" % i, [128, 8, 256], BF16) for i in range(4)]
            std = [kb.sbuf("pcd%d" % i, [128, 16, 1024], BF16) for i in range(2)]
            bg = bg_factory(stw, std)
        for h in (range(8) if heads is None else heads):
            kb.dma("sp", bias[:], cx.na_bias[l, h], reads=[cx.na_bias], writes=[bias])
            kb.op("act", lambda: nc.scalar.activation(out=bias[:], in_=bias[:], func=AF.Exp), reads=[bias], writes=[bias])
            kb.op("pool", lambda: nc.gpsimd.tensor_tensor(out=EB[:], in0=bias[:], in1=vmask[:], op=ALU.mult),
                  reads=[bias, vmask], writes=[EB])
            kb.dma("sp", qT[:], cx.QB[h * 64:(h + 1) * 64, :], reads=[cx.QB], writes=[qT])
            kb.dma("sp", kT[:], cx.KB_[h * 64:(h + 1) * 64, :], reads=[cx.KB_], writes=[kT])
            kb.dma("sp", vt[:], cx.VB_t[:, :, h * 64:(h + 1) * 64], reads=[cx.VB_t], writes=[vt])
            def p1(qb):
                nonlocal it
                if qb < 32:
                    keys = list(keys_tab[qb]) + [(32, None), (33, None)]
                else:
                    keys = [(32, None), (33, None)]
                qs = slice(qb * 128, (qb + 1) * 128)
                grp = [keys[i:i + 4] for i in range(0, len(keys), 4)]
                st_ = []
                for gk in grp:
                    ps = pS[it % 4]
                    p_ = pt[it % R]
                    it += 1
                    ng = len(gk)
                    kb.mm([(lambda ps=ps, j=j, kt_i=kt_i: nc.tensor.matmul(
                        ps[:, j * 128:(j + 1) * 128], lhsT=kT[:, kt_i * 128:(kt_i + 1) * 128], rhs=qT[:, qs],
                        start=True, stop=True)) for j, (kt_i, _) in enumerate(gk)],
                        reads=[kT, qT], writes=[ps])
                    kb.op("act", lambda ps=ps, p_=p_, ng=ng: nc.scalar.activation(
                        out=p_[:, :ng, :].rearrange("p h q -> p (h q)"), in_=ps[:, :ng * 128], func=AF.Exp, scale=0.125),
                        reads=[ps], writes=[p_])
                    nloc = sum(1 for (_, pid) in gk if pid is not None)
                    if nloc:
                        pid0 = gk[0][1]
                        eng = "dve" if it % 2 == 0 else "pool"
                        h_ = nc.vector if eng == "dve" else nc.gpsimd
                        kb.op(eng, lambda h_=h_, p_=p_, pid0=pid0, nloc=nloc: h_.tensor_tensor(
                            out=p_[:, :nloc, :], in0=p_[:, :nloc, :], in1=EB[:, pid0:pid0 + nloc, :], op=ALU.mult),
                            reads=[p_, EB], writes=[p_])
                    st_.append((gk, p_))
                return (qb, st_, len(keys))

            def p2(state):
                qb, st_, nk = state
                qs = slice(qb * 128, (qb + 1) * 128)
                a = qb % 2
                po = pO[a]
                pd = pD[a]
                done = 0
                for gk, p_ in st_:
                    fns = []
                    for j, (kt_i, _) in enumerate(gk):
                        first = (done == 0)
                        last = (done == nk - 1)
                        fns.append(lambda po=po, p_=p_, j=j, kt_i=kt_i, first=first, last=last: nc.tensor.matmul(
                            po[:64, 0:128], lhsT=vt[:, kt_i, :], rhs=p_[:, j, :], start=first, stop=last))
                        fns.append(lambda pd=pd, p_=p_, j=j, first=first, last=last: nc.tensor.matmul(
                            pd[:64, 0:128], lhsT=ones64[:], rhs=p_[:, j, :], start=first, stop=last))
                        done += 1
                    kb.mm(fns, reads=[vt, p_, ones64], writes=[po, pd])
                o_ = osb[a]
                kb.op("act", lambda po=po, o_=o_: nc.scalar.copy(out=o_[:, 0:128], in_=po[:64, 0:128]), reads=[po], writes=[o_])
                kb.op("act", lambda pd=pd, o_=o_: nc.scalar.copy(out=o_[:, 128:256], in_=pd[:64, 0:128]), reads=[pd, o_], writes=[o_])
                kb.op("dve", lambda o_=o_: nc.vector.reciprocal(out=o_[:, 128:256], in_=o_[:, 128:256]), reads=[o_], writes=[o_])
                kb.op("pool", lambda o_=o_: nc.gpsimd.tensor_tensor(
                    out=ost[:, qs], in0=o_[:, 0:128], in1=o_[:, 128:256], op=ALU.mult),
                    reads=[o_], writes=[], acc_writes=[ost])

            prev = None
            for qb in range(NT):
                cur = p1(qb)
                if prev is not None:
                    p2(prev)
                prev = cur
                if bg is not None and qb % 2 == 1:
                    next(bg, None)
            p2(prev)
            kb.dma("sp", cx.BR_B[h * 64:(h + 1) * 64, :], ost[:], reads=[ost], writes=[], acc_writes=[cx.BR_B])
        if bg is not None:
            for _ in bg:
                pass

LN_EPS = 1e-5
MB = 512
MBLKS = [(i * MB, min(MB, T - i * MB)) for i in range((T + MB - 1) // MB)]


def ln_setup(cx, kb, nc):
    ones = kb.sbuf("ln_ones", [128, 128], F32)
    kb.op("dve", lambda: nc.vector.memset(ones[:], 1.0 / D), writes=[ones])
    eps = kb.sbuf("ln_eps", [128, 1], F32)
    kb.op("dve", lambda: nc.vector.memset(eps[:], LN_EPS), writes=[eps])
    return ones, eps


def ln_block(kb, nc, r, nb, gam, bet, out, ones, eps, scr):
    rsq, ps1, ps2, mean, var = scr
    kb.op("act", lambda: nc.scalar.activation(out=rsq[:, :, :nb], in_=r[:, :, :nb], func=AF.Square), reads=[r], writes=[rsq])
    kb.mm([(lambda oc=oc: nc.tensor.matmul(ps1[:, :nb], lhsT=ones[:], rhs=r[:, oc, :nb], start=(oc == 0), stop=(oc == 7)))
           for oc in range(8)], reads=[ones, r], writes=[ps1])
    kb.mm([(lambda oc=oc: nc.tensor.matmul(ps2[:, :nb], lhsT=ones[:], rhs=rsq[:, oc, :nb], start=(oc == 0), stop=(oc == 7)))
           for oc in range(8)], reads=[ones, rsq], writes=[ps2])
    kb.op("act", lambda: nc.scalar.copy(out=mean[:, :nb], in_=ps1[:, :nb]), reads=[ps1], writes=[mean])
    kb.op("act", lambda: nc.scalar.copy(out=var[:, :nb], in_=ps2[:, :nb]), reads=[ps2], writes=[var])
    m2 = rsq
    kb.op("dve", lambda: nc.vector.tensor_tensor(out=m2[:, 0, :nb], in0=mean[:, :nb], in1=mean[:, :nb], op=ALU.mult),
          reads=[mean, rsq], writes=[rsq])
    kb.op("dve", lambda: nc.vector.tensor_tensor(out=var[:, :nb], in0=var[:, :nb], in1=m2[:, 0, :nb], op=ALU.subtract),
          reads=[var, rsq], writes=[var])
    kb.op("act", lambda: nc.scalar.activation(out=var[:, :nb], in_=var[:, :nb], func=AF.Sqrt, bias=eps[:]),
          reads=[var, eps], writes=[var])
    kb.op("dve", lambda: nc.vector.reciprocal(out=var[:, :nb], in_=var[:, :nb]), reads=[var], writes=[var])
    kb.op("dve", lambda: nc.vector.tensor_tensor(
        out=r[:, :, :nb], in0=r[:, :, :nb], in1=mean[:, :nb].unsqueeze(1).broadcast_to([128, 8, nb]), op=ALU.subtract),
        reads=[r, mean], writes=[r])
    kb.op("pool", lambda: nc.gpsimd.tensor_tensor(
        out=r[:, :, :nb], in0=r[:, :, :nb], in1=var[:, :nb].unsqueeze(1).broadcast_to([128, 8, nb]), op=ALU.mult),
        reads=[r, var], writes=[r])
    for oc in range(8):
        eng = "dve" if oc % 2 == 0 else "pool"
        h_ = nc.vector if eng == "dve" else nc.gpsimd
        kb.op(eng, lambda h_=h_, oc=oc: h_.tensor_scalar(
            out=out[:, oc, :nb], in0=r[:, oc, :nb], scalar1=gam[:, oc:oc + 1], scalar2=bet[:, oc:oc + 1],
            op0=ALU.mult, op1=ALU.add), reads=[r, gam, bet], writes=[out])


def load_ln_params(cx, kb, nc, l, which):
    gam = kb.sbuf("ln_g", [128, 8], F32)
    bet = kb.sbuf("ln_b", [128, 8], F32)
    kb.dma("sp", gam[:], cx.ln_gT[l, which], reads=[cx.ln_gT], writes=[gam])
    kb.dma("sp", bet[:], cx.ln_bT[l, which], reads=[cx.ln_bT], writes=[bet])
    return gam, bet


def stage_merge(cx, kb, nc, l, xsrc, modv):
    with kb.scope():
        ones, eps = ln_setup(cx, kb, nc)
        gam, bet = load_ln_params(cx, kb, nc, l, 0)
        wbr = kb.sbuf("wbr", [128, 4, 4, 1024], BF16)
        wout = kb.sbuf("wout", [128, 8, 1024], BF16)
        for i in range(4):
            kb.dma("pool", wbr[:, i], cx.w_branch[l, i].rearrange("(kc p) c -> p kc c", p=128), reads=[cx.w_branch], writes=[wbr])
        kb.dma("pool", wout[:], cx.w_out[l].rearrange("(kc p) c -> p kc c", p=128), reads=[cx.w_out], writes=[wout])
        wr = kb.sbuf("wr", [128, 8, 36], F32)
        kb.dma("sp", wr[:, :, 0:4], cx.moe_w_group[l].rearrange("(kc p) c -> p kc c", p=128), reads=[cx.moe_w_group], writes=[wr])
        kb.dma("sp", wr[:, :, 4:36], cx.moe_w_expert[l].rearrange("(kc p) c -> p kc c", p=128), reads=[cx.moe_w_expert], writes=[wr])
        rb = kb.sbuf("rb", [128, 36], F32)
        kb.dma("sp", rb[:, 0:4], cx.moe_b_group[l].partition_broadcast(128), reads=[cx.moe_b_group], writes=[rb])
        kb.dma("sp", rb[:, 4:36], cx.moe_b_expert[l].partition_broadcast(128), reads=[cx.moe_b_expert], writes=[rb])
        onep4 = kb.sbuf("onep4", [128, 8, 2], F32)
        kb.op("dve", lambda: nc.vector.tensor_scalar(out=onep4[:], in0=modv[:, 32:40, :], scalar1=1.0, scalar2=None, op0=ALU.add),
              reads=[modv], writes=[onep4])
        brt = [kb.sbuf("brt%d" % i, [128, 4, 4, MB], BF16) for i in range(1)]
        gtt = kb.sbuf("gtt", [128, 32, MB], BF16)
        xb = kb.sbuf("xbm", [128, 8, MB], F32)
        mrg = kb.sbuf("mrg", [128, 8, MB], BF16)
        r = kb.sbuf("rm", [128, 8, MB], F32)
        hx2b = kb.sbuf("hx2b", [128, 8, MB], BF16)
        rsq = kb.sbuf("rsqm", [128, 8, MB], F32)
        x1 = r
        hx2f = rsq
        mean = kb.sbuf("meanm", [128, MB], F32)
        var = kb.sbuf("varm", [128, MB], F32)
        csb = [kb.sbuf("csb%d" % i, [128, MB], F32) for i in range(4)]
        acc = kb.sbuf("accm", [128, MB], F32)
        mixs = kb.sbuf("mixs", [128, MB], F32)
        pbr = [kb.psum("pbr%d" % i, [128, MB]) for i in range(4)]
        pmx = kb.psum("pmx", [128, MB])
        ps1 = kb.psum("ps1m", [128, MB])
        ps2 = kb.psum("ps2m", [128, MB])
        prt = kb.psum("prt", [128, MB])
        brs = [cx.BR_A, cx.BR_B, cx.BR_C, cx.BR_D]
        lgt = kb.sbuf("lgt", [128, 36], F32)
        gmx = kb.sbuf("gmx", [128, 1], F32)
        ngm = kb.sbuf("ngm", [128, 1], F32)
        ge = kb.sbuf("ge", [128, 4], F32)
        gs = kb.sbuf("gsr", [128, 1], F32)
        oh = kb.sbuf("oh", [128, 4], F32)
        lem = kb.sbuf("lem", [128, 4, 8], F32)
        mx8 = kb.sbuf("mx8", [128, 8], F32)
        dd = kb.sbuf("dd", [128, 1], F32)
        w1 = kb.sbuf("w1", [128, 1], F32)
        w2 = kb.sbuf("w2", [128, 1], F32)
        tt1 = kb.sbuf("tt1", [128, 32], F32)
        tt2 = kb.sbuf("tt2", [128, 32], F32)
        wgt = kb.sbuf("wgt", [128, 32], F32)
        for bi, (t0, nb) in enumerate(MBLKS):
            col = blkcol(t0)
            b_ = brt[0]
            for i in range(4):
                kb.dma("sp", b_[:, i, :, :nb], brs[i][:, t0:t0 + nb].rearrange("(kc p) t -> p kc t", p=128),
                       reads=[brs[i]], writes=[b_])
            kb.dma("sp", gtt[:, :, :nb], cx.GATE[:, t0:t0 + nb].rearrange("(j p) t -> p j t", p=128), reads=[cx.GATE], writes=[gtt])
            kb.dma("sp", xb[:, :, :nb], xsrc[:, t0:t0 + nb].rearrange("(kc p) t -> p kc t", p=128), reads=[xsrc], writes=[xb])
            for oc in range(8):
                for i in range(4):
                    kb.mm([(lambda i=i, kc=kc, oc=oc: nc.tensor.matmul(
                        pbr[i][:, :nb], lhsT=wbr[:, i, kc, oc * 128:(oc + 1) * 128], rhs=b_[:, i, kc, :nb],
                        start=(kc == 0), stop=(kc == 3))) for kc in range(4)], reads=[wbr, b_], writes=[pbr[i]])
                    kb.op("act", lambda i=i: nc.scalar.copy(out=csb[i][:, :nb], in_=pbr[i][:, :nb]), reads=[pbr[i]], writes=[csb[i]])
                    eng = "dve" if i % 2 == 0 else "pool"
                    h_ = nc.vector if eng == "dve" else nc.gpsimd
                    kb.op(eng, lambda h_=h_, i=i, oc=oc: h_.tensor_tensor(
                        out=csb[i][:, :nb], in0=csb[i][:, :nb], in1=gtt[:, i * 8 + oc, :nb], op=ALU.mult),
                        reads=[csb[i], gtt], writes=[csb[i]])
                kb.op("dve", lambda: nc.vector.tensor_tensor(out=acc[:, :nb], in0=csb[0][:, :nb], in1=csb[1][:, :nb], op=ALU.add),
                      reads=[csb[0], csb[1]], writes=[acc])
                kb.op("pool", lambda: nc.gpsimd.tensor_tensor(out=csb[2][:, :nb], in0=csb[2][:, :nb], in1=csb[3][:, :nb], op=ALU.add),
                      reads=[csb[2], csb[3]], writes=[csb[2]])
                kb.op("dve", lambda oc=oc: nc.vector.tensor_tensor(out=mrg[:, oc, :nb], in0=acc[:, :nb], in1=csb[2][:, :nb], op=ALU.add),
                      reads=[acc, csb[2]], writes=[mrg])
            for oc in range(8):
                kb.mm([(lambda kc=kc, oc=oc: nc.tensor.matmul(
                    pmx[:, :nb], lhsT=wout[:, kc, oc * 128:(oc + 1) * 128], rhs=mrg[:, kc, :nb],
                    start=(kc == 0), stop=(kc == 7))) for kc in range(8)], reads=[wout, mrg], writes=[pmx])
                kb.op("act", lambda: nc.scalar.copy(out=mixs[:, :nb], in_=pmx[:, :nb]), reads=[pmx], writes=[mixs])
                kb.op("pool", lambda oc=oc: nc.gpsimd.tensor_scalar(
                    out=mixs[:, :nb], in0=mixs[:, :nb], scalar1=modv[:, 16 + oc, col:col + 1], scalar2=None, op0=ALU.mult),
                    reads=[mixs, modv], writes=[mixs])
                kb.op("dve", lambda oc=oc: nc.vector.scalar_tensor_tensor(
                    out=r[:, oc, :nb], in0=xb[:, oc, :nb], scalar=ALPHA, in1=mixs[:, :nb], op0=ALU.mult, op1=ALU.add),
                    reads=[xb, mixs], writes=[r])
            ln_block(kb, nc, r, nb, gam, bet, x1, ones, eps, (rsq, ps1, ps2, mean, var))
            kb.dma("sp", cx.XT[:, t0:t0 + nb].rearrange("(kc p) t -> p kc t", p=128), x1[:, :, :nb],
                   reads=[x1], writes=[], acc_writes=[cx.XT])
            for oc in range(8):
                eng = "dve" if oc % 2 == 0 else "pool"
                h_ = nc.vector if eng == "dve" else nc.gpsimd
                kb.op(eng, lambda h_=h_, oc=oc: h_.tensor_scalar(
                    out=hx2f[:, oc, :nb], in0=x1[:, oc, :nb], scalar1=onep4[:, oc, col:col + 1],
                    scalar2=modv[:, 24 + oc, col:col + 1], op0=ALU.mult, op1=ALU.add),
                    reads=[x1, onep4, modv], writes=[hx2f])
            kb.op("act", lambda: nc.scalar.copy(out=hx2b[:, :, :nb], in_=hx2f[:, :, :nb]), reads=[hx2f], writes=[hx2b])
            kb.dma("sp", cx.HX2[:, t0:t0 + nb].rearrange("(kc p) t -> p kc t", p=128), hx2b[:, :, :nb],
                   reads=[hx2b], writes=[], acc_writes=[cx.HX2])
            for tj in range(nb // 128):
                ti = t0 // 128 + tj
                tsl = slice(tj * 128, (tj + 1) * 128)
                kb.mm([(lambda kc=kc: nc.tensor.matmul(prt[:, 0:36], lhsT=hx2f[:, kc, tsl], rhs=wr[:, kc, :],
                                                       start=(kc == 0), stop=(kc == 7))) for kc in range(8)],
                      reads=[hx2f, wr], writes=[prt])
                kb.op("act", lambda: nc.scalar.copy(out=lgt[:], in_=prt[:, 0:36]), reads=[prt], writes=[lgt])
                kb.op("dve", lambda: nc.vector.tensor_tensor(out=lgt[:], in0=lgt[:], in1=rb[:], op=ALU.add), reads=[lgt, rb], writes=[lgt])
                kb.op("dve", lambda: nc.vector.tensor_reduce(out=gmx[:], in_=lgt[:, 0:4], axis=AX.X, op=ALU.max), reads=[lgt], writes=[gmx])
                kb.op("dve", lambda: nc.vector.tensor_scalar(out=ngm[:], in0=gmx[:], scalar1=-1.0, scalar2=None, op0=ALU.mult), reads=[gmx], writes=[ngm])
                kb.op("act", lambda: nc.scalar.activation(out=ge[:], in_=lgt[:, 0:4], func=AF.Exp, bias=ngm[:]), reads=[lgt, ngm], writes=[ge])
                kb.op("dve", lambda: nc.vector.tensor_reduce(out=gs[:], in_=ge[:], axis=AX.X, op=ALU.add), reads=[ge], writes=[gs])
                kb.op("dve", lambda: nc.vector.reciprocal(out=gs[:], in_=gs[:]), reads=[gs], writes=[gs])
                kb.op("dve", lambda: nc.vector.tensor_scalar(out=oh[:], in0=lgt[:, 0:4], scalar1=gmx[:], scalar2=None, op0=ALU.is_equal), reads=[lgt, gmx], writes=[oh])
                kb.op("dve", lambda: nc.vector.tensor_scalar(out=oh[:], in0=oh[:], scalar1=-1.0, scalar2=1e30, op0=ALU.add, op1=ALU.mult), reads=[oh], writes=[oh])
                kb.op("dve", lambda: nc.vector.tensor_tensor(
                    out=lem[:], in0=lgt[:, 4:36].rearrange("p (g e) -> p g e", e=8), in1=oh[:].unsqueeze(2).broadcast_to([128, 4, 8]), op=ALU.add),
                    reads=[lgt, oh], writes=[lem])
                lemf = lem[:].rearrange("p g e -> p (g e)")
                kb.op("dve", lambda: nc.vector.max(out=mx8[:], in_=lem[:].rearrange("p g e -> p (g e)")), reads=[lem], writes=[mx8])
                kb.op("dve", lambda: nc.vector.tensor_tensor(out=dd[:], in0=mx8[:, 1:2], in1=mx8[:, 0:1], op=ALU.subtract), reads=[mx8], writes=[dd])
                kb.op("act", lambda: nc.scalar.activation(out=dd[:], in_=dd[:], func=AF.Exp), reads=[dd], writes=[dd])
                kb.op("dve", lambda: nc.vector.tensor_scalar(out=w1[:], in0=dd[:], scalar1=1.0, scalar2=None, op0=ALU.add), reads=[dd], writes=[w1])
                kb.op("dve", lambda: nc.vector.reciprocal(out=w1[:], in_=w1[:]), reads=[w1], writes=[w1])
                kb.op("dve", lambda: nc.vector.tensor_tensor(out=w2[:], in0=dd[:], in1=w1[:], op=ALU.mult), reads=[dd, w1], writes=[w2])
                kb.op("dve", lambda: nc.vector.tensor_scalar(
                    out=tt1[:], in0=lem[:].rearrange("p g e -> p (g e)"), scalar1=mx8[:, 0:1], scalar2=w1[:], op0=ALU.is_equal, op1=ALU.mult),
                    reads=[lem, mx8, w1], writes=[tt1])
                kb.op("dve", lambda: nc.vector.tensor_scalar(
                    out=tt2[:], in0=lem[:].rearrange("p g e -> p (g e)"), scalar1=mx8[:, 1:2], scalar2=w2[:], op0=ALU.is_equal, op1=ALU.mult),
                    reads=[lem, mx8, w2], writes=[tt2])
                kb.op("dve", lambda: nc.vector.tensor_tensor(out=tt1[:], in0=tt1[:], in1=tt2[:], op=ALU.add), reads=[tt1, tt2], writes=[tt1])
                kb.op("dve", lambda: nc.vector.tensor_scalar(out=wgt[:], in0=tt1[:], scalar1=gs[:], scalar2=None, op0=ALU.mult), reads=[tt1, gs], writes=[wgt])
                kb.dma("sp", cx.WG_t[:, ti, :], wgt[:], reads=[wgt], writes=[], acc_writes=[cx.WG_t])


def stage_moe(cx, kb, nc, l, modv, yout=None, precast=True):
    with kb.scope():
        ones, eps = ln_setup(cx, kb, nc)
        gam, bet = load_ln_params(cx, kb, nc, l, 1)
        wgT = kb.sbuf("wgT", [32, T], F32)
        sel = kb.sbuf("sel", [32, 32, 128], F32)
        kb.dma("sp", sel[:], cx.m_sel[:], reads=[cx.m_sel], writes=[sel])
        ptr = [kb.psum("ptr%d" % i, [128, 512]) for i in range(2)]
        with kb.scope():
            wgl = kb.sbuf("wgl", [128, NT, 32], F32)
            id32 = kb.sbuf("id32", [128, 128], F32)
            kb.dma("sp", wgl[:], cx.WG_t[:], reads=[cx.WG_t], writes=[wgl])
            kb.dma("sp", id32[:], cx.m_ident32[:], reads=[cx.m_ident32], writes=[id32])
            for ti in range(NT):
                p_ = ptr[ti % 2]
                kb.mm([lambda p_=p_, ti=ti: nc.tensor.matmul(p_[:32, 0:128], lhsT=wgl[:, ti, :], rhs=id32[:], start=True, stop=True)],
                      reads=[wgl, id32], writes=[p_])
                kb.op("act", lambda p_=p_, ti=ti: nc.scalar.copy(out=wgT[:, ti * 128:(ti + 1) * 128], in_=p_[:32, 0:128]),
                      reads=[p_], writes=[], acc_writes=[wgT])
        if precast:
            precast_expert_weights(cx, kb, nc, l)
        hx = kb.sbuf("hxm", [128, 8, 512], BF16)
        yacc = kb.sbuf("yacc", [128, 8, 512], F32)
        hid = kb.sbuf("hid", [128, 16, 512], BF16)
        wdg = kb.sbuf("wdg", [128, 8, 2, 1024], BF16)
        NWB = 4
        wgs = [kb.sbuf("wgs%d" % i, [128, 8, 256], BF16) for i in range(NWB)]
        wus = [kb.sbuf("wus%d" % i, [128, 8, 256], BF16) for i in range(NWB)]
        xb = kb.sbuf("xbe", [128, 8, 512], F32)
        rsq = kb.sbuf("rsqe", [128, 8, 512], F32)
        mean = kb.sbuf("meane", [128, 512], F32)
        var = kb.sbuf("vare", [128, 512], F32)
        reps = [kb.sbuf("reps%d" % i, [128, 512], BF16) for i in range(2)]
        ssb = [kb.sbuf("ssb%d" % i, [128, 512], BF16) for i in range(3)]
        ytmp = [kb.sbuf("ytmp%d" % i, [128, 512], F32) for i in range(2)]
        NPS = 3
        pg = [kb.psum("pg%d" % i, [128, 512]) for i in range(NPS)]
        pu = [kb.psum("pu%d" % i, [128, 512]) for i in range(NPS)]
        py = ptr
        ps1 = pg[0]
        ps2 = pu[0]
        prep = ptr[1]
        nw = 0
        it = 0
        for bi, (t0, nb) in enumerate(BLKS):
            col = blkcol(t0)
            kb.dma("sp", hx[:, :, :nb], cx.HX2[:, t0:t0 + nb].rearrange("(kc p) t -> p kc t", p=128), reads=[cx.HX2], writes=[hx])
            kb.dma("sp", xb[:, :, :nb], cx.XT[:, t0:t0 + nb].rearrange("(kc p) t -> p kc t", p=128), reads=[cx.XT], writes=[xb])
            for g in range(4):
                kb.dma("sp", wdg[:].rearrange("p e hc d -> p (e hc d)"), cx.WDB[g], reads=[cx.WDB], writes=[wdg])
                for e_ in range(8):
                    e = g * 8 + e_
                    wg_, wu_ = wgs[nw % NWB], wus[nw % NWB]
                    nw += 1
                    kb.dma("sp", wg_[:].rearrange("p kc f -> p (kc f)"), cx.WGB[e], reads=[cx.WGB], writes=[wg_])
                    kb.dma("act", wu_[:].rearrange("p kc f -> p (kc f)"), cx.WUB[e], reads=[cx.WUB], writes=[wu_])
                    rp = reps[e % 2]
                    kb.mm([lambda e=e: nc.tensor.matmul(prep[:, :nb], lhsT=sel[:, e, :], rhs=wgT[:, t0:t0 + nb], start=True, stop=True)],
                          reads=[sel, wgT], writes=[prep])
                    kb.op("act", lambda rp=rp: nc.scalar.copy(out=rp[:, :nb], in_=prep[:, :nb]), reads=[prep], writes=[rp])
                    for hc in range(2):
                        a = it % NPS
                        it += 1
                        kb.mm([(lambda kc=kc, a=a, hc=hc, wg_=wg_: nc.tensor.matmul(
                            pg[a][:, :nb], lhsT=wg_[:, kc, hc * 128:(hc + 1) * 128], rhs=hx[:, kc, :nb],
                            start=(kc == 0), stop=(kc == 7))) for kc in range(8)], reads=[wg_, hx], writes=[pg[a]])
                        kb.mm([(lambda kc=kc, a=a, hc=hc, wu_=wu_: nc.tensor.matmul(
                            pu[a][:, :nb], lhsT=wu_[:, kc, hc * 128:(hc + 1) * 128], rhs=hx[:, kc, :nb],
                            start=(kc == 0), stop=(kc == 7))) for kc in range(8)], reads=[wu_, hx], writes=[pu[a]])
                        kb.op("act", lambda a=a: nc.scalar.activation(out=ssb[a][:, :nb], in_=pg[a][:, :nb], func=AF.Silu),
                              reads=[pg[a]], writes=[ssb[a]])
                        kb.op("pool", lambda a=a, rp=rp: nc.gpsimd.tensor_tensor(
                            out=ssb[a][:, :nb], in0=ssb[a][:, :nb], in1=rp[:, :nb], op=ALU.mult),
                            reads=[ssb[a], rp], writes=[ssb[a]])
                        kb.op("dve", lambda a=a, e_=e_, hc=hc: nc.vector.tensor_tensor(
                            out=hid[:, e_ * 2 + hc, :nb], in0=pu[a][:, :nb], in1=ssb[a][:, :nb], op=ALU.mult),
                            reads=[pu[a], ssb[a]], writes=[], acc_writes=[hid])
                for oc in range(8):
                    p_ = py[oc % 2]
                    fns = []
                    for j in range(16):
                        fns.append(lambda j=j, oc=oc, p_=p_: nc.tensor.matmul(
                            p_[:, :nb], lhsT=wdg[:, j // 2, j % 2, oc * 128:(oc + 1) * 128], rhs=hid[:, j, :nb],
                            start=(j == 0), stop=(j == 15)))
                    kb.mm(fns, reads=[wdg, hid], writes=[p_])
                    if g == 0:
                        kb.op("act", lambda oc=oc, p_=p_: nc.scalar.copy(out=yacc[:, oc, :nb], in_=p_[:, :nb]), reads=[p_], writes=[], acc_writes=[yacc])
                    else:
                        yt = ytmp[oc % 2]
                        kb.op("act", lambda yt=yt, p_=p_: nc.scalar.copy(out=yt[:, :nb], in_=p_[:, :nb]), reads=[p_], writes=[yt])
                        eng = "dve" if oc % 2 == 0 else "pool"
                        h_ = nc.vector if eng == "dve" else nc.gpsimd
                        kb.op(eng, lambda h_=h_, oc=oc, yt=yt: h_.tensor_tensor(
                            out=yacc[:, oc, :nb], in0=yacc[:, oc, :nb], in1=yt[:, :nb], op=ALU.add),
                            reads=[yt, yacc], writes=[], acc_writes=[yacc])
            for oc in range(8):
                kb.op("pool", lambda oc=oc: nc.gpsimd.tensor_scalar(
                    out=yacc[:, oc, :nb], in0=yacc[:, oc, :nb], scalar1=modv[:, 40 + oc, col:col + 1], scalar2=None, op0=ALU.mult),
                    reads=[yacc, modv], writes=[yacc])
                kb.op("dve", lambda oc=oc: nc.vector.scalar_tensor_tensor(
                    out=yacc[:, oc, :nb], in0=xb[:, oc, :nb], scalar=ALPHA, in1=yacc[:, oc, :nb], op0=ALU.mult, op1=ALU.add),
                    reads=[xb, yacc], writes=[yacc])
            ln_block(kb, nc, yacc, nb, gam, bet, yacc, ones, eps, (rsq, ps1, ps2, mean, var))
            kb.dma("sp", cx.XT[:, t0:t0 + nb].rearrange("(kc p) t -> p kc t", p=128), yacc[:, :, :nb],
                   reads=[yacc], writes=[], acc_writes=[cx.XT])
            if yout is not None and t0 < TL:
                kb.dma("sp", yout[:, t0:t0 + nb].rearrange("(kc p) t -> p kc t", p=128), yacc[:, :, :nb],
                       reads=[yacc], writes=[], acc_writes=[yout])


def precast_expert_weights(cx, kb, nc, l):
    with kb.scope():
        stw = [kb.sbuf("pcw%d" % i, [128, 8, 256], BF16) for i in range(4)]
        std = [kb.sbuf("pcd%d" % i, [128, 16, 1024], BF16) for i in range(2)]
        n = 0
        for g in range(4):
            d_ = std[g % 2]
            kb.dma("pool", d_[:], cx.moe_w_down[l, g].rearrange("e (hc p) d -> p (e hc) d", p=128), reads=[cx.moe_w_down], writes=[d_])
            kb.dma("sp", cx.WDB[g], d_[:].rearrange("p j d -> p (j d)"), reads=[d_], writes=[], acc_writes=[cx.WDB])
            for e_ in range(8):
                e = g * 8 + e_
                for src, dst in ((cx.moe_w_gate, cx.WGB), (cx.moe_w_up, cx.WUB)):
                    w_ = stw[n % 4]
                    n += 1
                    kb.dma("pool", w_[:], src[l, g, e_].rearrange("(kc p) f -> p kc f", p=128), reads=[src], writes=[w_])
                    kb.dma("sp", dst[e], w_[:].rearrange("p kc f -> p (kc f)"), reads=[w_], writes=[], acc_writes=[dst])


def precast_gen(cx, kb, nc, l, stw, std):
    pend = []
    n = 0
    for g in range(4):
        d_ = std[g % 2]
        kb.dma("pool", d_[:], cx.moe_w_down[l, g].rearrange("e (hc p) d -> p (e hc) d", p=128), reads=[cx.moe_w_down], writes=[d_])
        pend.append((cx.WDB, g, d_, "p j d -> p (j d)"))
        yield
        for e_ in range(8):
            e = g * 8 + e_
            for src, dst in ((cx.moe_w_gate, cx.WGB), (cx.moe_w_up, cx.WUB)):
                w_ = stw[n % len(stw)]
                n += 1
                kb.dma("pool", w_[:], src[l, g, e_].rearrange("(kc p) f -> p kc f", p=128), reads=[src], writes=[w_])
                pend.append((dst, e, w_, "p kc f -> p (kc f)"))
                yield
                while len(pend) > 2:
                    dst_, idx_, buf_, pat_ = pend.pop(0)
                    kb.dma("sp", dst_[idx_], buf_[:].rearrange(pat_), reads=[buf_], writes=[], acc_writes=[dst_])
    while pend:
        dst_, idx_, buf_, pat_ = pend.pop(0)
        kb.dma("sp", dst_[idx_], buf_[:].rearrange(pat_), reads=[buf_], writes=[], acc_writes=[dst_])
        yield


def rope_tables():
    t = np.arange(TL)
    row = (t // 64).astype(np.float32)
    colp = (t % 64).astype(np.float32)
    nf = 16
    inv = (np.float32(10000.0) ** (-np.arange(nf, dtype=np.float32) / np.float32(nf))).astype(np.float32)
    cos = np.ones((128, T), np.float32)
    sin = np.zeros((128, T), np.float32)
    for q in range(128):
        d = q % 64
        pos = row if d < 32 else colp
        ang = (pos * inv[d % 16]).astype(np.float32)
        cos[q, :TL] = np.cos(ang).astype(np.float32)
        sin[q, :TL] = np.sin(ang).astype(np.float32)
    pt = np.zeros((128, 128), np.float32)
    for m in range(128):
        if (m % 32) < 16:
            pt[m + 16, m] = -1.0
        else:
            pt[m - 16, m] = 1.0
    return cos, sin, pt


_CONST = {}
_NAIDX = {}


def consts():
    if not _CONST:
        cos, sin, pt = rope_tables()
        _CONST.update(rope_cos=cos, rope_sin=sin, rope_pt=pt)
        _CONST.update(scan_masks())
        _CONST.update(wa_masks())
        sel = np.zeros((32, 32, 128), np.float32)
        for e in range(32):
            sel[e, e, :] = 1.0
        _CONST["m_sel"] = sel
        _CONST["m_ident32"] = np.eye(128, dtype=np.float32)
        _k, _valid, _ri, _ci = na_consts()
        _CONST["m_navalid"] = _valid
        _NAIDX["ri"] = _ri
        _NAIDX["ci"] = _ci
    return _CONST


def core_inputs(inp, b):
    f = lambda a: np.ascontiguousarray(a, dtype=np.float32)
    m = {}
    m["xin"] = f(np.concatenate([inp["x"][b].T, inp["ctx"][b].T], axis=1))
    cv = np.stack([inp["c"][b].reshape(8, 128).T, inp["c_ctx"].reshape(8, 128).T], axis=2)
    m["cvec"] = f(cv)
    m["w_mod"] = f(inp["w_mod"])
    m["b_modT"] = f(inp["b_mod"].reshape(2, 48, 128).transpose(0, 2, 1))
    m["w_in"] = f(inp["w_in"])
    m["hg_gamma"] = f(inp["hg_gamma"])
    m["gla_lr_w"] = f(inp["gla_lr_w"])
    m["gla_lr_b"] = f(inp["gla_lr_b"])
    m["wa_sink"] = f(inp["wa_sink"])
    for k in ("w_branch", "w_out", "moe_w_group", "moe_b_group", "moe_w_expert", "moe_b_expert",
              "moe_w_gate", "moe_w_up", "moe_w_down"):
        m[k] = f(inp[k])
    m["ln_gT"] = f(inp["ln_g"].reshape(2, 2, 8, 128).transpose(0, 1, 3, 2))
    m["ln_bT"] = f(inp["ln_b"].reshape(2, 2, 8, 128).transpose(0, 1, 3, 2))
    consts()
    m["na_bias"] = f(inp["na_rpb"][:, :, _NAIDX["ri"], _NAIDX["ci"]])
    m.update(consts())
    return m


FUSED = 1


def build_program(layers, final):
    nc = bass.Bass("TRN2", target_bir_lowering=False)
    cx = Ctx()
    declare_inputs(cx, nc)
    if final:
        yout = Buf(nc.dram_tensor("yout", [D, TL], F32, kind="ExternalOutput"), "yout")
    else:
        yout = None
    with ExitStack() as st:
        kb = KB(nc, st)
        declare_scratch(cx, kb, debug=False, as_output=(() if final else ("XT",)))
        modT = [kb.sbuf("modT%d" % l, [128, 48, 2], F32) for l in range(2)]
        stage_mod(cx, kb, nc, modT)
        first = True
        for l in layers:
            xsrc = cx.xin if first else cx.XT
            first = False
            with kb.scope():
                lbt = [kb.sbuf("lbt%d" % i, [128, 1024], F32) for i in range(2)]
                omlt = [kb.sbuf("omlt%d" % i, [128, 1024], F32) for i in range(2)]
                stage_lb(cx, kb, nc, lbt, omlt)
                hxT = kb.sbuf("hxT", [128, 8, T], BF16)
                stage_modulate(cx, kb, nc, xsrc, modT[l], 0, 1, hxT)
                stage_inproj_fm(cx, kb, nc, l, hxT)
                stage_inproj_tm(cx, kb, nc, l, hxT, lbt[l], omlt[l])
            stage_gla_gate(cx, kb, nc, l)
            stage_scan(cx, kb, nc, "gla")
            stage_scan(cx, kb, nc, "hgrn")
            stage_wa(cx, kb, nc, l)
            stage_na(cx, kb, nc, l, bg_factory=(lambda stw, std, l=l: precast_gen(cx, kb, nc, l, stw, std)))
            stage_merge(cx, kb, nc, l, xsrc, modT[l])
            stage_moe(cx, kb, nc, l, modT[l], yout if (final and l == layers[-1]) else None, precast=False)
        if final:
            kb.finish([yout])
        else:
            kb.finish([cx.XT])
    return nc


def kernel(**inputs):
    inp = {k: np.asarray(v) for k, v in inputs.items()}
    in_maps = [core_inputs(inp, b) for b in range(4)]
    if FUSED:
        nc = build_program([0, 1], True)
        res = run_bass_kernel_spmd(nc, in_maps, core_ids=list(range(4)))
    else:
        nc0 = build_program([0], False)
        res0 = run_bass_kernel_spmd(nc0, in_maps, core_ids=list(range(4)))
        for b in range(4):
            in_maps[b]["xin"] = np.ascontiguousarray(res0.results[b]["XT"])
        nc1 = build_program([1], True)
        res = run_bass_kernel_spmd(nc1, in_maps, core_ids=list(range(4)))
    out = np.stack([np.ascontiguousarray(res.results[b]["yout"].T) for b in range(4)], axis=0)
    return out.astype(np.float32)
```

```python
import numpy as np
from contextlib import ExitStack
import concourse.bass as bass
import concourse.mybir as mybir
from concourse.bass_utils import run_bass_kernel_spmd
import os

F32 = mybir.dt.float32
BF16 = mybir.dt.bfloat16
U32 = mybir.dt.uint32
AF = mybir.ActivationFunctionType
ALU = mybir.AluOpType
AX = mybir.AxisListType


class Buf:
    __slots__ = ("t", "w", "r", "name")

    def __init__(self, t, name=""):
        self.t = t
        self.w = []
        self.r = []
        self.name = name

    def __getitem__(self, idx):
        return self.t[idx]


class Dep:
    __slots__ = ("w", "r")

    def __init__(self):
        self.w = []
        self.r = []


class Eng:
    def __init__(self, name, h, sem):
        self.name = name
        self.h = h
        self.sem = sem
        self.count = 0
        self.seen = {}


class KB:
    def __init__(self, nc, stack, n_dma_sems=8):
        self.nc = nc
        self.stack = stack
        self.sems = {}
        self.engs = {}
        for name, h in (("pe", nc.tensor), ("act", nc.scalar), ("dve", nc.vector),
                        ("pool", nc.gpsimd), ("sp", nc.sync)):
            s = stack.enter_context(nc.semaphore("s_" + name))
            self.sems[id(s)] = s
            self.engs[name] = Eng(name, h, s)
        self.dma_pool = {}
        for q in ("sp", "pool", "act"):
            lst = []
            for i in range(n_dma_sems):
                s = stack.enter_context(nc.semaphore("d_%s%d" % (q, i)))
                self.sems[id(s)] = s
                lst.append([s, 0])
            self.dma_pool[q] = [lst, 0]
        self.ninstr = 0

    def scope(self):
        kb = self

        class _Scope:
            def __enter__(self_s):
                self_s.prev = kb.stack
                self_s.st = ExitStack()
                self_s.st.__enter__()
                kb.stack = self_s.st
                kb.nscope = getattr(kb, "nscope", 0) + 1
                return self_s

            def __exit__(self_s, *a):
                if a[0] is None:
                    kb.barrier()
                kb.stack = self_s.prev
                return self_s.st.__exit__(*a)
        return _Scope()

    def sbuf(self, name, shape, dtype):
        name = "%s_s%d" % (name, getattr(self, "nscope", 0))
        t = self.stack.enter_context(self.nc.sbuf_tensor(name, list(shape), dtype))
        return Buf(t, name)

    def psum(self, name, shape, dtype=F32):
        name = "%s_s%d" % (name, getattr(self, "nscope", 0))
        t = self.stack.enter_context(self.nc.psum_tensor(name, list(shape), dtype))
        return Buf(t, name)

    def dram(self, name, shape, dtype, kind="Internal"):
        t = self.nc.dram_tensor(name, list(shape), dtype, kind=kind)
        return Buf(t, name)

    def _wait(self, e, deps):
        need = {}
        for (sid, val) in deps:
            if need.get(sid, 0) < val:
                need[sid] = val
        for sid, val in need.items():
            if e.seen.get(sid, 0) < val:
                e.h.wait_ge(self.sems[sid], val)
                e.seen[sid] = val
                self.ninstr += 1

    def _collect(self, reads, writes, acc_writes=()):
        deps = []
        for b in reads:
            deps += b.w
        for b in writes:
            deps += b.w
            deps += b.r
        for b in acc_writes:
            deps += b.r
        return deps

    @staticmethod
    def _compact(lst):
        mx = {}
        for (sid, v) in lst:
            if mx.get(sid, 0) < v:
                mx[sid] = v
        return list(mx.items())

    def _commit(self, token, reads, writes, acc_writes=()):
        for b in acc_writes:
            b.w.append(token)
            if len(b.w) > 64:
                b.w = self._compact(b.w)
        for b in reads:
            b.r.append(token)
            if len(b.r) > 64:
                mx = {}
                for (sid, v) in b.r:
                    if mx.get(sid, 0) < v:
                        mx[sid] = v
                b.r = list(mx.items())
        for b in writes:
            b.w = [token]
            b.r = []

    def _cutcheck(self):
        import os
        cut = os.environ.get("KB_CUT")
        self.nops = getattr(self, "nops", 0) + 1
        skip = os.environ.get("KB_SKIP")
        if skip and self.nops in [int(x) for x in skip.split(",")]:
            return True
        return cut is not None and self.nops > int(cut)

    def op(self, eng, fn, reads=(), writes=(), same_engine_sync=True, acc_writes=()):
        if self._cutcheck():
            return None
        e = self.engs[eng]
        deps = self._collect(reads, writes, acc_writes)
        if not same_engine_sync:
            deps = [d for d in deps if d[0] != id(e.sem)]
        self._wait(e, deps)
        ins = fn()
        e.count += 1
        ins.then_inc(e.sem, 1)
        self.ninstr += 1
        token = (id(e.sem), e.count)
        self._commit(token, reads, writes, acc_writes)
        return token

    def mm(self, fns, reads=(), writes=()):
        if self._cutcheck():
            return None
        e = self.engs["pe"]
        deps = self._collect(reads, writes)
        deps = [d for d in deps if d[0] != id(e.sem)]
        self._wait(e, deps)
        ins = None
        for fn in fns:
            ins = fn()
            self.ninstr += 1
        e.count += 1
        ins.then_inc(e.sem, 1)
        token = (id(e.sem), e.count)
        self._commit(token, reads, writes)
        return token

    def dma(self, q, out, in_, reads=(), writes=(), acc_writes=(), **kw):
        e = self.engs[q]
        pool = self.dma_pool[q]
        lst, idx = pool
        ent = lst[idx % len(lst)]
        pool[1] = idx + 1
        s, total = ent
        deps = self._collect(reads, writes, acc_writes)
        deps.append((id(s), total))
        import os
        if os.environ.get("DMA_SERIAL") and getattr(self, "_last_dma", {}).get(q):
            deps.append(self._last_dma[q])
        self._wait(e, deps)
        ins = e.h.dma_start(out=out, in_=in_, **kw)
        ins.then_inc(s, 16)
        ent[1] = total + 16
        self.ninstr += 1
        token = (id(s), total + 16)
        if not hasattr(self, "_last_dma"):
            self._last_dma = {}
        self._last_dma[q] = token
        self._commit(token, reads, writes, acc_writes)
        return token

    def init_memory(self, nbytes=196608):
        nc = self.nc
        with self.scope():
            n = nbytes // 4
            big = self.sbuf("initbig", [128, n], F32)
            q = n // 4
            self.op("dve", lambda: nc.vector.memset(big[:, 0:q], 0.0), writes=[])
            self.op("pool", lambda: nc.gpsimd.memset(big[:, q:2 * q], 0.0), writes=[])
            self.op("dve", lambda: nc.vector.memset(big[:, 2 * q:3 * q], 0.0), writes=[])
            self.op("pool", lambda: nc.gpsimd.memset(big[:, 3 * q:n], 0.0), writes=[])
            pss = [self.psum("initps%d" % i, [128, 512]) for i in range(8)]
            for p in pss:
                self.op("dve", lambda p=p: nc.vector.memset(p[:], 0.0), writes=[])

    def barrier(self):
        allt = []
        for e in self.engs.values():
            if e.count:
                allt.append((id(e.sem), e.count))
        for q, (lst, _) in self.dma_pool.items():
            for s, total in lst:
                if total:
                    allt.append((id(s), total))
        for e in self.engs.values():
            self._wait(e, allt)

    def finish(self, tokens_bufs):
        e = self.engs["sp"]
        deps = []
        for b in tokens_bufs:
            deps += b.w
        self._wait(e, deps)

T = 4352
TL = 4096

D = 1024
TL = 4096
TC = 256
T = TL + TC
NT = T // 128
BLKS = [(i * 512, 512) for i in range(8)] + [(4096, 256)]
DIN = 10528
ALPHA = (2.0 * 2) ** 0.25

COLS = dict(a_q=(0, 256), a_k=(256, 256), a_v=(512, 512), a_g=(1024, 512), a_lr=(1536, 32),
            b_q=(1568, 512), b_k=(2080, 512), b_v=(2592, 512),
            c_q=(3104, 512), c_k=(3616, 128), c_v=(3744, 128),
            d_q=(3872, 512), d_f=(4384, 1024), d_i=(5408, 512), d_g=(5920, 512), gate=(6432, 4096))


def blkcol(t0):
    return 1 if t0 >= TL else 0


class Ctx:
    pass


def declare_inputs(cx, nc):
    def din(name, shape, dt=F32):
        return Buf(nc.dram_tensor(name, list(shape), dt, kind="ExternalInput"), name)
    cx.xin = din("xin", [D, T])
    cx.cvec = din("cvec", [128, 8, 2])
    cx.w_mod = din("w_mod", [2, D, 6 * D])
    cx.b_modT = din("b_modT", [2, 128, 48])
    cx.w_in = din("w_in", [2, D, DIN])
    cx.rope_cos = din("rope_cos", [128, T])
    cx.rope_sin = din("rope_sin", [128, T])
    cx.rope_pt = din("rope_pt", [128, 128])
    cx.hg_gamma = din("hg_gamma", [2, 2, 512])
    cx.gla_lr_w = din("gla_lr_w", [2, 2, 16, 256])
    cx.gla_lr_b = din("gla_lr_b", [2, 2, 256])
    for nm in ("mg", "mh"):
        setattr(cx, nm + "_mq", din(nm + "_mq", [128, 2, 130]))
        setattr(cx, nm + "_mk", din(nm + "_mk", [128, 2, 128]))
    cx.m_ma = din("m_ma", [128, 2, 128])
    cx.m_ident = din("m_ident", [128, 128])
    cx.m_mau = din("m_mau", [128, 2, 128], U32)
    cx.m_wa = din("m_wa", [128, 2, 128])
    cx.wa_sink = din("wa_sink", [2, 8])
    cx.m_navalid = din("m_navalid", [128, 21, 128])
    cx.w_branch = din("w_branch", [2, 4, 512, 1024])
    cx.w_out = din("w_out", [2, 1024, 1024])
    cx.ln_gT = din("ln_gT", [2, 2, 128, 8])
    cx.ln_bT = din("ln_bT", [2, 2, 128, 8])
    cx.moe_w_group = din("moe_w_group", [2, 1024, 4])
    cx.moe_b_group = din("moe_b_group", [2, 4])
    cx.moe_w_expert = din("moe_w_expert", [2, 1024, 32])
    cx.moe_b_expert = din("moe_b_expert", [2, 32])
    cx.moe_w_gate = din("moe_w_gate", [2, 4, 8, 1024, 256])
    cx.moe_w_up = din("moe_w_up", [2, 4, 8, 1024, 256])
    cx.moe_w_down = din("moe_w_down", [2, 4, 8, 256, 1024])
    cx.m_sel = din("m_sel", [32, 32, 128])
    cx.m_ident32 = din("m_ident32", [128, 128])
    cx.na_bias = din("na_bias", [2, 8, 128, 21, 128])


def declare_scratch(cx, kb, debug, as_input=(), as_output=()):
    kind = "ExternalOutput" if debug else "Internal"

    def ds(name, shape, dt):
        b = kb.dram(name, shape, dt, kind=("ExternalInput" if name in as_input else ("ExternalOutput" if name in as_output else kind)))
        setattr(cx, name, b)
        return b
    ds("XT", [D, T], F32)
    ds("QA", [256, T], BF16)
    ds("GA", [512, T], BF16)
    ds("LR1", [33, T], F32)
    ds("QB", [512, T], BF16)
    ds("KB_", [512, T], BF16)
    ds("QC", [512, T], BF16)
    ds("KC", [128, T], BF16)
    ds("QD", [512, T], BF16)
    ds("GD", [512, T], BF16)
    ds("GATE", [4096, T], BF16)
    ds("KVA_t", [128, NT, 768], BF16)
    ds("VB_t", [128, NT, 512], BF16)
    ds("VC_t", [128, NT, 128], BF16)
    ds("VD_t", [128, NT, 512], BF16)
    ds("LGD_t", [128, NT, 1024], F32)
    ds("KD_t", [128, NT, 1024], BF16)
    ds("LGA_t", [128, NT, 512], F32)
    ds("MODT", [2, 128, 96], F32)
    ds("HX2", [D, T], BF16)
    ds("WGB", [32, 128, 2048], BF16)
    ds("WUB", [32, 128, 2048], BF16)
    ds("WDB", [4, 128, 16384], BF16)
    ds("WG_t", [128, NT, 32], F32)
    ds("WGT", [32, T], F32)
    ds("BR_A", [512, T], BF16)
    ds("BR_B", [512, T], BF16)
    ds("BR_C", [512, T], BF16)
    ds("BR_D", [512, T], BF16)


def stage_mod(cx, kb, nc, modT):
    with kb.scope():
        cv = kb.sbuf("cv", [128, 8, 2], F32)
        sc = kb.sbuf("sc", [128, 8, 2], F32)
        kb.dma("sp", cv[:], cx.cvec[:], reads=[cx.cvec], writes=[cv])
        kb.op("act", lambda: nc.scalar.activation(out=sc[:], in_=cv[:], func=AF.Silu), reads=[cv], writes=[sc])
        wbufs = [kb.sbuf("wm%d" % i, [128, 8, 1024], F32) for i in range(2)]
        bm = kb.sbuf("bm", [128, 48], F32)
        ps = kb.psum("ps_mod", [128, 96])
        wi = 0
        for l in range(2):
            kb.dma("sp", bm[:], cx.b_modT[l], reads=[cx.b_modT], writes=[bm])
            for g in range(6):
                wb = wbufs[wi % 2]
                wi += 1
                src = cx.w_mod[l, :, g * 1024:(g + 1) * 1024].rearrange("(kc p) c -> p kc c", p=128)
                kb.dma("sp", wb[:], src, reads=[cx.w_mod], writes=[wb])
                for j in range(8):
                    jj = g * 8 + j
                    fns = []
                    for kc in range(8):
                        fns.append(lambda kc=kc, j=j, jj=jj, wb=wb: nc.tensor.matmul(
                            ps[:, 2 * jj:2 * jj + 2], lhsT=wb[:, kc, j * 128:(j + 1) * 128], rhs=sc[:, kc, :],
                            start=(kc == 0), stop=(kc == 7)))
                    kb.mm(fns, reads=[wb, sc], writes=[ps])
            m = modT[l]
            kb.op("dve", lambda m=m: nc.vector.tensor_tensor(
                out=m[:], in0=ps[:].rearrange("p (j c) -> p j c", c=2),
                in1=bm[:].unsqueeze(2).broadcast_to([128, 48, 2]), op=ALU.add),
                reads=[ps, bm], writes=[m])
            kb.dma("sp", cx.MODT[l], m[:].rearrange("p j c -> p (j c)"), reads=[m], writes=[cx.MODT])


def stage_modulate(cx, kb, nc, src_dram, modv, shift_idx, scale_idx, hxT, hx32=None):
    with kb.scope():
        onep = kb.sbuf("onep", [128, 8, 2], F32)
        kb.op("dve", lambda: nc.vector.tensor_scalar(
            out=onep[:], in0=modv[:, scale_idx * 8:(scale_idx + 1) * 8, :], scalar1=1.0, scalar2=None, op0=ALU.add),
            reads=[modv], writes=[onep])
        xb = [kb.sbuf("xb%d" % i, [128, 8, 512], F32) for i in range(2)]
        for bi, (t0, nb) in enumerate(BLKS):
            col = blkcol(t0)
            x = xb[bi % 2]
            kb.dma("sp", x[:, :, :nb], src_dram[:, t0:t0 + nb].rearrange("(kc p) t -> p kc t", p=128),
                   reads=[src_dram], writes=[x])
            for kc in range(8):
                eng = "dve" if kc % 2 == 0 else "pool"
                h = nc.vector if eng == "dve" else nc.gpsimd
                kb.op(eng, lambda h=h, kc=kc, x=x: h.tensor_scalar(
                    out=hxT[:, kc, t0:t0 + nb], in0=x[:, kc, :nb],
                    scalar1=onep[:, kc, col:col + 1], scalar2=modv[:, shift_idx * 8 + kc, col:col + 1],
                    op0=ALU.mult, op1=ALU.add), reads=[x, onep, modv], writes=[hxT])


FM_GROUPS = [
    ("a_q", "QA", "copy"), ("a_g", "GA", "silu"), ("a_lr", "LR1", "copy32"),
    ("b_q", "QB", "copy"), ("b_k", "KB_", "copy"),
    ("c_q", "QC", "rope"), ("c_k", "KC", "rope"),
    ("d_q", "QD", "silu"), ("d_g", "GD", "silu"), ("gate", "GATE", "sigmoid"),
]


def stage_inproj_fm(cx, kb, nc, l, hxT, only=None):
    with kb.scope():
        wsl = [kb.sbuf("wsl%d" % i, [128, 8, 512], BF16) for i in range(2)]
        stg = [kb.sbuf("stg%d" % i, [128, T], BF16) for i in range(2)]
        stg32 = kb.sbuf("stg32", [32, T], F32)
        pss = [kb.psum("psf%d" % i, [128, 512]) for i in range(4)]
        psr = [kb.psum("psr%d" % i, [128, 512]) for i in range(2)]
        cosT = kb.sbuf("cosT", [128, T], F32)
        sinT = kb.sbuf("sinT", [128, T], F32)
        ptm = kb.sbuf("ptm", [128, 128], BF16)
        tmpb = [kb.sbuf("tmpb%d" % i, [128, 512], BF16) for i in range(2)]
        t1 = [kb.sbuf("t1_%d" % i, [128, 512], F32) for i in range(2)]
        t2 = [kb.sbuf("t2_%d" % i, [128, 512], F32) for i in range(2)]
        ones = kb.sbuf("ones1", [1, T], F32)
        kb.dma("sp", cosT[:], cx.rope_cos[:], reads=[cx.rope_cos], writes=[cosT])
        kb.dma("sp", sinT[:], cx.rope_sin[:], reads=[cx.rope_sin], writes=[sinT])
        kb.dma("pool", ptm[:], cx.rope_pt[:], reads=[cx.rope_pt], writes=[ptm])
        kb.op("dve", lambda: nc.vector.memset(ones[:], 1.0), writes=[ones])
        kb.dma("sp", cx.LR1[32:33, :], ones[:], reads=[ones], writes=[], acc_writes=[cx.LR1])
        nsl = 0
        nps = 0
        nst = 0
        nrp = 0
        for (name, dest, kind) in FM_GROUPS:
            if only is not None and name not in only:
                continue
            c0, nc_ = COLS[name]
            dbuf = getattr(cx, dest)
            for s0 in range(0, nc_, 512):
                sw = min(512, nc_ - s0)
                w = wsl[nsl % 2]
                nsl += 1
                kb.dma("pool", w[:, :, :sw],
                       cx.w_in[l, :, c0 + s0:c0 + s0 + sw].rearrange("(kc p) c -> p kc c", p=128),
                       reads=[cx.w_in], writes=[w])
                for m0 in range(0, sw, 128):
                    mw = min(128, sw - m0)
                    if kind == "copy32":
                        st = stg32
                    else:
                        st = stg[nst % 2]
                        nst += 1
                    for bi, (t0, nb) in enumerate(BLKS):
                        ps = pss[nps % 4]
                        nps += 1
                        fns = [(lambda kc=kc, ps=ps, w=w: nc.tensor.matmul(
                            ps[:mw, :nb], lhsT=w[:, kc, m0:m0 + mw], rhs=hxT[:, kc, t0:t0 + nb],
                            start=(kc == 0), stop=(kc == 7))) for kc in range(8)]
                        kb.mm(fns, reads=[w, hxT], writes=[ps])
                        o = st[:mw, t0:t0 + nb]
                        if kind in ("copy", "copy32"):
                            if nps % 2 == 0:
                                kb.op("dve", lambda ps=ps, o=o: nc.vector.tensor_copy(out=o, in_=ps[:mw, :nb]),
                                      reads=[ps], writes=[st])
                            else:
                                kb.op("act", lambda ps=ps, o=o: nc.scalar.copy(out=o, in_=ps[:mw, :nb]),
                                      reads=[ps], writes=[st])
                        elif kind == "silu":
                            kb.op("act", lambda ps=ps, o=o: nc.scalar.activation(out=o, in_=ps[:mw, :nb], func=AF.Silu),
                                  reads=[ps], writes=[st])
                        elif kind == "sigmoid":
                            kb.op("act", lambda ps=ps, o=o: nc.scalar.activation(out=o, in_=ps[:mw, :nb], func=AF.Sigmoid),
                                  reads=[ps], writes=[st])
                        elif kind == "rope":
                            tb = tmpb[nrp % 2]
                            a1 = t1[nrp % 2]
                            a2 = t2[nrp % 2]
                            pr = psr[nrp % 2]
                            nrp += 1
                            kb.op("act", lambda ps=ps, tb=tb: nc.scalar.copy(out=tb[:mw, :nb], in_=ps[:mw, :nb]),
                                  reads=[ps], writes=[tb])
                            kb.mm([lambda pr=pr, tb=tb: nc.tensor.matmul(pr[:mw, :nb], lhsT=ptm[:mw, :mw], rhs=tb[:mw, :nb],
                                                                         start=True, stop=True)],
                                  reads=[ptm, tb], writes=[pr])
                            kb.op("dve", lambda tb=tb, a1=a1: nc.vector.tensor_tensor(
                                out=a1[:mw, :nb], in0=tb[:mw, :nb], in1=cosT[:mw, t0:t0 + nb], op=ALU.mult),
                                reads=[tb, cosT], writes=[a1])
                            kb.op("dve", lambda pr=pr, a2=a2: nc.vector.tensor_tensor(
                                out=a2[:mw, :nb], in0=pr[:mw, :nb], in1=sinT[:mw, t0:t0 + nb], op=ALU.mult),
                                reads=[pr, sinT], writes=[a2])
                            kb.op("pool", lambda a1=a1, a2=a2, o=o: nc.gpsimd.tensor_tensor(
                                out=o, in0=a1[:mw, :nb], in1=a2[:mw, :nb], op=ALU.add),
                                reads=[a1, a2], writes=[st])
                    kb.dma("sp", dbuf[s0 + m0:s0 + m0 + mw, :], st[:mw, :], reads=[st], writes=[], acc_writes=[dbuf])


TM_GROUPS = [
    (256, 512, "KVA_t", 0, "copy"), (768, 256, "KVA_t", 512, "copy"),
    (2592, 512, "VB_t", 0, "copy"), (3744, 128, "VC_t", 0, "copy"),
    (5408, 512, "VD_t", 0, "copy"),
    (4384, 512, None, 0, "hgf"), (4896, 512, None, 512, "hgf"),
]


def stage_inproj_tm(cx, kb, nc, l, hxT, lbt, omlt, only=None):
    with kb.scope():
        wsl = [kb.sbuf("wtl%d" % i, [128, 8, 512], BF16) for i in range(2)]
        pss = [kb.psum("pst%d" % i, [128, 512]) for i in range(4)]
        stb = [kb.sbuf("stb%d" % i, [128, 512], BF16) for i in range(4)]
        e1 = [kb.sbuf("e1_%d" % i, [128, 512], F32) for i in range(2)]
        r1 = [kb.sbuf("r1_%d" % i, [128, 512], F32) for i in range(2)]
        lg = [kb.sbuf("lg_%d" % i, [128, 512], F32) for i in range(2)]
        kk = [kb.sbuf("kk_%d" % i, [128, 512], BF16) for i in range(2)]
        nsl = nps = nst = nh = 0
        for (c0, ncol, dest, doff, kind) in TM_GROUPS:
            if only is not None and kind not in only:
                continue
            w = wsl[nsl % 2]
            nsl += 1
            kb.dma("pool", w[:, :, :ncol], cx.w_in[l, :, c0:c0 + ncol].rearrange("(kc p) c -> p kc c", p=128),
                   reads=[cx.w_in], writes=[w])
            for ti in range(NT):
                ts = slice(ti * 128, (ti + 1) * 128)
                ps = pss[nps % 4]
                nps += 1
                fns = [(lambda kc=kc, ps=ps, w=w: nc.tensor.matmul(
                    ps[:, :ncol], lhsT=hxT[:, kc, ts], rhs=w[:, kc, :ncol],
                    start=(kc == 0), stop=(kc == 7))) for kc in range(8)]
                kb.mm(fns, reads=[w, hxT], writes=[ps])
                if kind == "copy":
                    st = stb[nst % 4]
                    nst += 1
                    if nst % 2 == 0:
                        kb.op("dve", lambda ps=ps, st=st: nc.vector.tensor_copy(out=st[:, :ncol], in_=ps[:, :ncol]),
                              reads=[ps], writes=[st])
                    else:
                        kb.op("act", lambda ps=ps, st=st: nc.scalar.copy(out=st[:, :ncol], in_=ps[:, :ncol]),
                              reads=[ps], writes=[st])
                    db = getattr(cx, dest)
                    kb.dma("sp", db[:, ti, doff:doff + ncol], st[:, :ncol], reads=[st], writes=[], acc_writes=[db])
                else:
                    e = e1[nh % 2]
                    r = r1[nh % 2]
                    g = lg[nh % 2]
                    k = kk[nh % 2]
                    nh += 1
                    cs = slice(doff, doff + 512)
                    kb.op("act", lambda ps=ps, e=e: nc.scalar.activation(out=e[:], in_=ps[:], func=AF.Exp, scale=-1.0),
                          reads=[ps], writes=[e])
                    kb.op("pool", lambda e=e: nc.gpsimd.tensor_scalar(
                        out=e[:], in0=e[:], scalar1=1.0, scalar2=None, op0=ALU.add),
                        reads=[e], writes=[e])
                    kb.op("dve", lambda e=e, r=r: nc.vector.reciprocal(out=r[:], in_=e[:]),
                          reads=[e], writes=[r])
                    kb.op("dve", lambda r=r: nc.vector.tensor_tensor(out=r[:], in0=r[:], in1=omlt[:, cs], op=ALU.mult),
                          reads=[r, omlt], writes=[r])
                    kb.op("pool", lambda r=r: nc.gpsimd.tensor_tensor(out=r[:], in0=r[:], in1=lbt[:, cs], op=ALU.add),
                          reads=[r, lbt], writes=[r])
                    kb.op("act", lambda r=r, g=g: nc.scalar.activation(out=g[:], in_=r[:], func=AF.Ln),
                          reads=[r], writes=[g])
                    kb.op("pool", lambda r=r, k=k: nc.gpsimd.tensor_scalar(
                        out=k[:], in0=r[:], scalar1=-1.0, scalar2=1.0, op0=ALU.mult, op1=ALU.add),
                        reads=[r], writes=[k])
                    kb.dma("sp", cx.LGD_t[:, ti, cs], g[:], reads=[g], writes=[], acc_writes=[cx.LGD_t])
                    kb.dma("sp", cx.KD_t[:, ti, cs], k[:], reads=[k], writes=[], acc_writes=[cx.KD_t])


def stage_lb(cx, kb, nc, lbt, omlt):
    with kb.scope():
        gam = kb.sbuf("gam", [128, 2, 2, 512], F32)
        kb.dma("sp", gam[:].rearrange("p a b c -> p (a b c)"),
               cx.hg_gamma[:].rearrange("a b c -> (a b c)").partition_broadcast(128),
               reads=[cx.hg_gamma], writes=[gam])
        d = kb.sbuf("gamd", [128, 2, 512], F32)
        kb.op("dve", lambda: nc.vector.tensor_tensor(out=d[:], in0=gam[:, :, 0, :], in1=gam[:, :, 1, :], op=ALU.subtract),
              reads=[gam], writes=[d])
        kb.op("act", lambda: nc.scalar.activation(out=d[:], in_=d[:], func=AF.Exp), reads=[d], writes=[d])
        l1 = lbt[1]
        kb.op("dve", lambda: nc.vector.tensor_scalar(
            out=d[:], in0=d[:], scalar1=1.0, scalar2=None, op0=ALU.add), reads=[d], writes=[d])
        kb.op("dve", lambda: nc.vector.reciprocal(out=l1[:], in_=d[:].rearrange("p a c -> p (a c)")),
              reads=[d], writes=[l1])
        l0 = lbt[0]
        kb.op("dve", lambda: nc.vector.memset(l0[:], 0.0), writes=[l0])
        for l in range(2):
            o = omlt[l]
            lb = lbt[l]
            kb.op("dve", lambda o=o, lb=lb: nc.vector.tensor_scalar(
                out=o[:], in0=lb[:], scalar1=-1.0, scalar2=1.0, op0=ALU.mult, op1=ALU.add),
                reads=[lb], writes=[o])


def stage_gla_gate(cx, kb, nc, l):
    with kb.scope():
        wlr = kb.sbuf("wlr", [33, 512], F32)
        lr1 = kb.sbuf("lr1", [33, T], F32)
        kb.op("dve", lambda: nc.vector.memset(wlr[:], 0.0), writes=[wlr])
        kb.dma("sp", wlr[0:16, 0:256], cx.gla_lr_w[l, 0], reads=[cx.gla_lr_w], writes=[wlr])
        kb.dma("sp", wlr[16:32, 256:512], cx.gla_lr_w[l, 1], reads=[cx.gla_lr_w], writes=[wlr])
        kb.dma("sp", wlr[32:33, :], cx.gla_lr_b[l].rearrange("a c -> (a c)").unsqueeze(0),
               reads=[cx.gla_lr_b], writes=[wlr])
        kb.dma("sp", lr1[:], cx.LR1[:], reads=[cx.LR1], writes=[lr1])
        pss = [kb.psum("psg%d" % i, [128, 512]) for i in range(2)]
        ee = [kb.sbuf("ge%d" % i, [128, 512], F32) for i in range(2)]
        gg = [kb.sbuf("gg%d" % i, [128, 512], F32) for i in range(2)]
        for ti in range(NT):
            ts = slice(ti * 128, (ti + 1) * 128)
            ps = pss[ti % 2]
            e = ee[ti % 2]
            g = gg[ti % 2]
            kb.mm([lambda ps=ps: nc.tensor.matmul(ps[:], lhsT=lr1[:, ts], rhs=wlr[:], start=True, stop=True)],
                  reads=[lr1, wlr], writes=[ps])
            kb.op("act", lambda ps=ps, e=e: nc.scalar.activation(out=e[:], in_=ps[:], func=AF.Exp, scale=-1.0),
                  reads=[ps], writes=[e])
            kb.op("act", lambda e=e, g=g: nc.scalar.activation(out=g[:], in_=e[:], func=AF.Ln, bias=1.0),
                  reads=[e], writes=[g])
            kb.dma("sp", cx.LGA_t[:, ti, :], g[:], reads=[g], writes=[], acc_writes=[cx.LGA_t])

SCAN_CFG = {
    "gla": dict(H=4, dk=64, qsrc="QA", qscale=0.125, ksrc="KVA_t", kz=0, kcol0=0, vsrc="KVA_t", vcol0=256,
                gsrc="LGA_t", gz=256, gate="GA", dest="BR_A", mask="mg"),
    "hgrn": dict(H=4, dk=128, qsrc="QD", qscale=1.0, ksrc="KD_t", kz=512, kcol0=0, vsrc="VD_t", vcol0=0,
                 gsrc="LGD_t", gz=512, gate="GD", dest="BR_D", mask="mh"),
}


def view(b):
    return Buf(b.t, b.name + "_v")


def stage_scan(cx, kb, nc, which, heads=None):
    cfg = SCAN_CFG[which]
    H, dk = cfg["H"], cfg["dk"]
    dv = 128
    qsrc, ksrc, vsrc, gsrc = (getattr(cx, cfg[k]) for k in ("qsrc", "ksrc", "vsrc", "gsrc"))
    gate, dest = getattr(cx, cfg["gate"]), getattr(cx, cfg["dest"])
    mq_d, mk_d = getattr(cx, cfg["mask"] + "_mq"), getattr(cx, cfg["mask"] + "_mk")
    with kb.scope():
        mq = kb.sbuf("mq", [128, 2, 130], F32)
        mk = kb.sbuf("mk", [128, 2, 128], F32)
        ma = kb.sbuf("ma", [128, 2, 128], F32)
        ident = kb.sbuf("ident", [128, 128], BF16)
        onesdv = kb.sbuf("onesdv", [128, 128], F32)
        kb.dma("sp", mq[:], mq_d[:], reads=[mq_d], writes=[mq])
        kb.dma("sp", mk[:], mk_d[:], reads=[mk_d], writes=[mk])
        kb.dma("sp", ma[:], cx.m_ma[:], reads=[cx.m_ma], writes=[ma])
        mau = kb.sbuf("mau", [128, 2, 128], U32)
        kb.dma("sp", mau[:], cx.m_mau[:], reads=[cx.m_mau], writes=[mau])
        kb.dma("pool", ident[:], cx.m_ident[:], reads=[cx.m_ident], writes=[ident])
        kb.op("dve", lambda: nc.vector.memset(onesdv[:], 1.0 / dv), writes=[onesdv])
        vt = kb.sbuf("vt", [128, NT, dv], BF16)
        qT = kb.sbuf("qT", [128, T], BF16)
        gsil = kb.sbuf("gsil", [128, T], BF16)
        obr = kb.sbuf("obr", [128, T], BF16)
        gts = [kb.sbuf("gt%d" % i, [128, NT, dk], F32) for i in range(2)]
        kts = [kb.sbuf("kt%d" % i, [128, NT, dk], BF16) for i in range(2)]
        qt = [kb.sbuf("qt%d" % z, [128, T], BF16) for z in range(2)]
        atm = [kb.sbuf("atm%d" % z, [128, NT, 128], BF16) for z in range(2)]
        Sp = [kb.sbuf("Sp%d" % z, [128, 2 * NT, dv], BF16) for z in range(2)]
        for z in range(2):
            kb.op("pool", lambda z=z: nc.gpsimd.memset(atm[z][:], 0.0), writes=[atm[z]])
        Sst = [kb.sbuf("Sst%d" % i, [128, dv], F32) for i in range(2)]
        R = 3
        eq = [kb.sbuf("eq%d" % i, [128, 128], F32) for i in range(R)]
        ek = [kb.sbuf("ek%d" % i, [128, 128], F32) for i in range(R)]
        ekh = [kb.sbuf("ekh%d" % i, [128, 128], F32) for i in range(R)]
        ktl = [kb.sbuf("ktl%d" % i, [128, 128], BF16) for i in range(R)]
        khat = [kb.sbuf("khat%d" % i, [128, 256], BF16) for i in range(R)]
        ktr = [kb.sbuf("ktr%d" % i, [128, 128], F32) for i in range(R)]
        atr = [kb.sbuf("atr%d" % i, [128, 128], F32) for i in range(R)]
        usb = [kb.sbuf("usb%d" % i, [128, 256], F32) for i in range(R)]
        osb = [kb.sbuf("osb%d" % i, [128, 512], F32) for i in range(2)]
        cm = kb.sbuf("cm", [128, 2], F32)
        kb.op("dve", lambda: nc.vector.memset(cm[:], 0.0), writes=[cm])
        kb.op("dve", lambda: nc.vector.memset(cm[0:64, 0:1], 1.0), writes=[cm])
        kb.op("dve", lambda: nc.vector.memset(cm[64:128, 1:2], 1.0), writes=[cm])
        er2 = [kb.sbuf("er%d" % i, [128, 2], F32) for i in range(R)]
        ed2 = [kb.sbuf("ed%d" % i, [128, 2], F32) for i in range(R)]
        psA = [kb.psum("psA%d" % i, [128, 512]) for i in range(2)]
        psB = [kb.psum("psB%d" % i, [128, 512]) for i in range(2)]
        psA_ec = [view(p) for p in psA]
        import os
        if True:
            psK = [kb.psum("psK%d" % i, [128, 512]) for i in range(2)]
        else:
            psK = psA
        psA_kt = [view(p) for p in psK]
        psA_e2 = [view(p) for p in psK]
        psB_u = [view(p) for p in psB]
        psB_at = [view(p) for p in psB]
        psO = [kb.psum("psO%d" % i, [128, 512]) for i in range(1)] * 2
        psM = [kb.psum("psM%d" % i, [128, 512]) for i in range(1)] * 2
        sq = [kb.sbuf("sq%d" % i, [128, 512], F32) for i in range(2)]
        sd = [kb.sbuf("sd%d" % i, [128, 512], F32) for i in range(2)]
        on = [kb.sbuf("on%d" % i, [128, 512], F32) for i in range(2)]
        eps_t = kb.sbuf("eps_t", [128, 1], F32)
        kb.op("dve", lambda: nc.vector.memset(eps_t[:], 1e-6), writes=[eps_t])

        it = 0
        ghz = 0
        import os
        for h in (range(H) if heads is None else heads):
            vc0 = cfg["vcol0"] + h * dv
            kb.dma("sp", vt[:], vsrc[:, :, vc0:vc0 + dv], reads=[vsrc], writes=[vt])
            kb.dma("sp", qT[:dk, :], qsrc[h * dk:(h + 1) * dk, :], reads=[qsrc], writes=[qT])
            kb.dma("sp", gsil[:], gate[h * dv:(h + 1) * dv, :], reads=[gate], writes=[gsil])
            for z in range(2):
                gt = gts[ghz % 2]
                kt = kts[ghz % 2]
                ghz += 1
                gc0 = z * cfg["gz"] + h * dk
                kc0 = cfg["kcol0"] + z * cfg["kz"] + h * dk
                kb.dma("sp", gt[:], gsrc[:, :, gc0:gc0 + dk], reads=[gsrc], writes=[gt])
                if os.environ.get("KTMEMSET"):
                    kb.op("dve", lambda kt=kt: nc.vector.memset(kt[:], 0.0), writes=[kt])
                kb.dma("sp", kt[:], ksrc[:, :, kc0:kc0 + dk], reads=[ksrc], writes=[kt])
                order = [32, 33] + list(range(32)) if z == 0 else [33, 32] + list(range(31, -1, -1))
                import os
                DBG = os.environ.get("SCAN_DBG", "")
                if DBG == "loads":
                    continue
                if DBG.startswith("nt"):
                    order = order[:int(DBG[2:])]
                S = Sst[0]
                kb.op("dve", lambda S=S: nc.vector.memset(S[:], 0.0), writes=[S])
                si = 0
                def p1(ti, a, r):
                    ts = slice(ti * 128, (ti + 1) * 128)
                    pa, pb = psA[a], psB[a]
                    pk = psK[a]
                    kb.mm([lambda pa=pa, gt=gt: nc.tensor.matmul(pa[:dk, 0:130], lhsT=gt[:, ti, :], rhs=mq[:, z, :],
                                                                 start=True, stop=True)],
                          reads=[gt, mq], writes=[psA_ec[a]])
                    kb.mm([lambda pk=pk, kt=kt: nc.tensor.matmul(pk[:dk, 256:384], lhsT=(ident[:, :dk] if os.environ.get("A2ID") else kt[:, ti, :]), rhs=ident[:],
                                                                 start=True, stop=True)],
                          reads=[kt, ident], writes=[psA_kt[a]])
                    kb.mm([lambda pk=pk, gt=gt: nc.tensor.matmul(pk[:, 384:384 + dk], lhsT=mk[:, z, :], rhs=gt[:, ti, :],
                                                                 start=True, stop=True)],
                          reads=[gt, mk], writes=[psA_e2[a]])
                    kb.op("act", lambda pa=pa, o=eq[r]: nc.scalar.activation(out=o[:dk, :], in_=pa[:dk, 0:128], func=AF.Exp),
                          reads=[psA_ec[a]], writes=[eq[r]])
                    kb.op("act", lambda pa=pa, o=ek[r]: nc.scalar.activation(out=o[:dk, :], in_=pa[:dk, 0:128], func=AF.Exp, scale=-1.0),
                          reads=[psA_ec[a]], writes=[ek[r]])
                    kb.op("act", lambda pa=pa, o=er2[r]: nc.scalar.activation(out=o[:dk, :], in_=pa[:dk, 128:130], func=AF.Exp, scale=0.5),
                          reads=[psA_ec[a]], writes=[er2[r]])
                    kb.op("act", lambda pa=pa, o=ed2[r]: nc.scalar.activation(out=o[:dk, :], in_=pa[:dk, 128:130], func=AF.Exp),
                          reads=[psA_ec[a]], writes=[ed2[r]])
                    kb.op("act", lambda pk=pk, o=ekh[r]: nc.scalar.activation(out=o[:, :dk], in_=pk[:, 384:384 + dk], func=AF.Exp),
                          reads=[psA_e2[a]], writes=[ekh[r]])
                    qz = qt[z]
                    kb.op("dve", lambda qz=qz, e=eq[r]: nc.vector.scalar_tensor_tensor(
                        out=qz[:dk, ts], in0=qT[:dk, ts], scalar=cfg["qscale"], in1=e[:dk, :], op0=ALU.mult, op1=ALU.mult),
                        reads=[qT, eq[r]], writes=[], acc_writes=[qz])
                    kb.op("act", lambda pk=pk, o=ktr[r]: nc.scalar.copy(out=o[:dk, :], in_=pk[:dk, 256:384]),
                          reads=[psA_kt[a]], writes=[ktr[r]])
                    kb.op("dve", lambda e=ek[r], o=ktl[r], kr=ktr[r]: nc.vector.tensor_tensor(
                        out=o[:dk, :], in0=kr[:dk, :], in1=e[:dk, :], op=ALU.mult),
                        reads=[ktr[r], ek[r]], writes=[ktl[r]])
                    for c in range(2):
                        kb.op("dve", lambda kt=kt, e=ekh[r], o=khat[r], c=c: nc.vector.scalar_tensor_tensor(
                            out=o[:, c * 128:c * 128 + dk], in0=kt[:, ti, :], scalar=cm[:, c:c + 1], in1=e[:, :dk],
                            op0=ALU.mult, op1=ALU.mult),
                            reads=[kt, ekh[r], cm], writes=[], acc_writes=[khat[r]])
                    return None

                def p2(ti, a, r, S, si):
                    ts = slice(ti * 128, (ti + 1) * 128)
                    pa, pb = psA[a], psB[a]
                    qz = qt[z]
                    kb.mm([(lambda c=c, pb=pb, kh=khat[r]: nc.tensor.matmul(
                        pb[:dk, c * 128:(c + 1) * 128], lhsT=kh[:, c * 128:c * 128 + dk],
                        rhs=vt[:, ti, :], start=True, stop=True)) for c in range(2)],
                        reads=[khat[r], vt], writes=[psB_u[a]])
                    kb.mm([lambda pb=pb, k_=ktl[r], qz=qz: nc.tensor.matmul(
                        pb[:, 256:384], lhsT=k_[:dk, :], rhs=qz[:dk, ts], start=True, stop=True)],
                        reads=[ktl[r], qz], writes=[psB_at[a]])
                    az = atm[z]
                    kb.op("act", lambda pb=pb, o=atr[r]: nc.scalar.copy(out=o[:], in_=pb[:, 256:384]),
                          reads=[psB_at[a]], writes=[atr[r]])
                    kb.op("dve", lambda az=az, o=atr[r]: nc.vector.copy_predicated(
                        out=az[:, ti, :], mask=mau[:, z, :], data=o[:]),
                        reads=[atr[r], mau], writes=[], acc_writes=[az])
                    kb.op("act", lambda pb=pb, o=usb[r]: nc.scalar.copy(out=o[:dk, :], in_=pb[:dk, 0:256]),
                          reads=[psB_u[a]], writes=[usb[r]])
                    spz = Sp[z]
                    for c in ((0, 1) if z == 0 else (1, 0)):
                        ci = 2 * ti + c
                        kb.op("act", lambda S=S, spz=spz, ci=ci, e=er2[r], c=c: nc.scalar.activation(
                            out=spz[:dk, ci, :], in_=S[:dk, :], func=AF.Copy, scale=e[:dk, c:c + 1]),
                            reads=[S, er2[r]], writes=[], acc_writes=[spz])
                        S2 = Sst[(si + 1) % 2]
                        si += 1
                        kb.op("dve", lambda S=S, S2=S2, e=ed2[r], u=usb[r], c=c: nc.vector.scalar_tensor_tensor(
                            out=S2[:dk, :], in0=S[:dk, :], scalar=e[:dk, c:c + 1], in1=u[:dk, c * 128:(c + 1) * 128],
                            op0=ALU.mult, op1=ALU.add),
                            reads=[S, ed2[r], usb[r]], writes=[S2])
                        S = S2
                    return S, si

                sched = []
                for ti in order:
                    sched.append((ti, it % 2, it % R))
                    it += 1
                for idx in range(len(sched) + 1):
                    if idx < len(sched):
                        p1(*sched[idx])
                    if idx >= 1:
                        S, si = p2(*sched[idx - 1], S, si)
            groups = [list(range(g * 4, g * 4 + 4)) for g in range(8)] + [[32, 33]]
            if DBG:
                groups = []
            for gi, tiles in enumerate(groups):
                po = psO[gi % 2]
                pm = psM[gi % 2]
                nb = len(tiles) * 128
                t0 = tiles[0] * 128
                fns = []
                for k_, ti in enumerate(tiles):
                    cs = k_ * 128
                    for z in range(2):
                        fns.append(lambda po=po, ti=ti, cs=cs, z=z: nc.tensor.matmul(
                            po[:, cs:cs + 128], lhsT=vt[:, ti, :], rhs=atm[z][:, ti, :], start=(z == 0), stop=False))
                        for c in range(2):
                            fns.append(lambda po=po, ti=ti, cs=cs, z=z, c=c: nc.tensor.matmul(
                                po[:, cs + c * 64:cs + (c + 1) * 64], lhsT=Sp[z][:dk, 2 * ti + c, :],
                                rhs=qt[z][:dk, ti * 128 + c * 64:ti * 128 + (c + 1) * 64],
                                start=False, stop=(z == 1)))
                kb.mm(fns, reads=[vt, atm[0], atm[1], Sp[0], Sp[1], qt[0], qt[1]], writes=[po])
                s_, d_, o_ = sq[gi % 2], sd[gi % 2], on[gi % 2]
                kb.op("act", lambda po=po, s_=s_: nc.scalar.activation(out=s_[:, :nb], in_=po[:, :nb], func=AF.Square),
                      reads=[po], writes=[s_])
                kb.mm([lambda pm=pm, s_=s_: nc.tensor.matmul(pm[:, :nb], lhsT=onesdv[:], rhs=s_[:, :nb], start=True, stop=True)],
                      reads=[onesdv, s_], writes=[pm])
                kb.op("act", lambda pm=pm, d_=d_: nc.scalar.activation(out=d_[:, :nb], in_=pm[:, :nb], func=AF.Sqrt, bias=eps_t[:]),
                      reads=[pm, eps_t], writes=[d_])
                kb.op("dve", lambda d_=d_: nc.vector.reciprocal(out=d_[:, :nb], in_=d_[:, :nb]), reads=[d_], writes=[d_])
                ob_ = osb[gi % 2]
                kb.op("act", lambda po=po, ob_=ob_: nc.scalar.copy(out=ob_[:, :nb], in_=po[:, :nb]), reads=[po], writes=[ob_])
                kb.op("dve", lambda ob_=ob_, d_=d_, o_=o_: nc.vector.tensor_tensor(
                    out=o_[:, :nb], in0=ob_[:, :nb], in1=d_[:, :nb], op=ALU.mult), reads=[ob_, d_], writes=[o_])
                kb.op("pool", lambda o_=o_: nc.gpsimd.tensor_tensor(
                    out=obr[:, t0:t0 + nb], in0=o_[:, :nb], in1=gsil[:, t0:t0 + nb], op=ALU.mult),
                    reads=[o_, gsil], writes=[], acc_writes=[obr])
            kb.dma("sp", dest[h * dv:(h + 1) * dv, :], obr[:], reads=[obr], writes=[], acc_writes=[dest])


def scan_masks():
    j = np.arange(128)[:, None]
    i = np.arange(128)[None, :]
    same = (j // 64 == i // 64).astype(np.float32)
    out = {}
    for name, gs in (("mg", -1.0 / 16.0), ("mh", 1.0)):
        mq = np.zeros((128, 2, 130), np.float32)
        mk = np.zeros((128, 2, 128), np.float32)
        mq[:, 0, :128] = gs * same * ((j <= i).astype(np.float32) - 0.5)
        mq[:, 1, :128] = gs * same * ((j >= i).astype(np.float32) - 0.5)
        for z in range(2):
            for c in range(2):
                mq[:, z, 128 + c] = gs * (np.arange(128) // 64 == c)
        mk[:, 0, :] = gs * same * (j > i)
        mk[:, 1, :] = gs * same * (j < i)
        out[name + "_mq"] = mq
        out[name + "_mk"] = mk
    ma = np.zeros((128, 2, 128), np.float32)
    ma[:, 0, :] = same * (j <= i)
    ma[:, 1, :] = same * (j >= i)
    out["m_ma"] = ma
    out["m_mau"] = ma.astype(np.uint32)
    out["m_ident"] = np.eye(128, dtype=np.float32)
    return out


def stage_wa(cx, kb, nc, l, groups=None):
    with kb.scope():
        wm = kb.sbuf("wm", [128, 2, 128], BF16)
        kb.dma("pool", wm[:], cx.m_wa[:], reads=[cx.m_wa], writes=[wm])
        ones64 = kb.sbuf("ones64", [128, 64], BF16)
        kb.op("dve", lambda: nc.vector.memset(ones64[:], 1.0), writes=[ones64])
        sk = kb.sbuf("sk", [64, 8], F32)
        kb.dma("sp", sk[:], cx.wa_sink[l].partition_broadcast(64), reads=[cx.wa_sink], writes=[sk])
        kb.op("act", lambda: nc.scalar.activation(out=sk[:], in_=sk[:], func=AF.Exp), reads=[sk], writes=[sk])
        q4 = kb.sbuf("q4", [64, 4, T], BF16)
        kT = kb.sbuf("kTc", [64, T], BF16)
        vt = kb.sbuf("vtc", [128, NT, 64], BF16)
        ost = kb.sbuf("ost", [64, 4, T], BF16)
        pS = [kb.psum("pS%d" % i, [128, 512]) for i in range(4)]
        pO = [kb.psum("pO%d" % i, [128, 512]) for i in range(2)]
        pD = [kb.psum("pD%d" % i, [128, 512]) for i in range(2)]
        R = 10
        pt = [kb.sbuf("pt%d" % i, [128, 4, 128], BF16) for i in range(R)]
        ptm = [kb.sbuf("ptm%d" % i, [128, 4, 128], BF16) for i in range(R)]
        osb = [kb.sbuf("osbw%d" % i, [64, 4, 128], F32) for i in range(2)]
        dsb = [kb.sbuf("dsbw%d" % i, [64, 4, 128], F32) for i in range(2)]
        it = 0
        for g in (range(2) if groups is None else groups):
            kb.dma("sp", q4[:], cx.QC[g * 256:(g + 1) * 256, :].rearrange("(hh d) t -> d hh t", d=64),
                   reads=[cx.QC], writes=[q4])
            kb.dma("sp", kT[:], cx.KC[g * 64:(g + 1) * 64, :], reads=[cx.KC], writes=[kT])
            kb.dma("sp", vt[:], cx.VC_t[:, :, g * 64:(g + 1) * 64], reads=[cx.VC_t], writes=[vt])
            def p1(qb):
                nonlocal it
                if qb < 32:
                    keys = []
                    if qb > 0:
                        keys.append((qb - 1, 0))
                    keys.append((qb, None))
                    if qb < 31:
                        keys.append((qb + 1, 1))
                    keys += [(32, None), (33, None)]
                else:
                    keys = [(32, None), (33, None)]
                qs = slice(qb * 128, (qb + 1) * 128)
                st_ = []
                for ki, (kt_i, mi) in enumerate(keys):
                    ps = pS[it % 4]
                    p_ = pt[it % R]
                    pm_ = ptm[it % R]
                    it += 1
                    kb.mm([lambda ps=ps, kt_i=kt_i: nc.tensor.matmul(
                        ps[:, :], lhsT=kT[:, kt_i * 128:(kt_i + 1) * 128], rhs=q4[:, :, qs], start=True, stop=True)],
                        reads=[kT, q4], writes=[ps])
                    kb.op("act", lambda ps=ps, p_=p_: nc.scalar.activation(
                        out=p_[:].rearrange("p h q -> p (h q)"), in_=ps[:, :], func=AF.Exp, scale=0.125),
                        reads=[ps], writes=[p_])
                    src = p_
                    if mi is not None:
                        eng = "dve" if it % 2 == 0 else "pool"
                        h_ = nc.vector if eng == "dve" else nc.gpsimd
                        kb.op(eng, lambda h_=h_, p_=p_, pm_=pm_, mi=mi: h_.tensor_tensor(
                            out=pm_[:], in0=p_[:], in1=wm[:, mi, :].unsqueeze(1).broadcast_to([128, 4, 128]), op=ALU.mult),
                            reads=[p_, wm], writes=[pm_])
                        src = pm_
                    st_.append((kt_i, src))
                return (qb, st_)

            def p2(state):
                qb, st_ = state
                qs = slice(qb * 128, (qb + 1) * 128)
                a = qb % 2
                po, pd = pO[a], pD[a]
                nk = len(st_)
                for ki, (kt_i, src) in enumerate(st_):
                    kb.mm([lambda po=po, src=src, kt_i=kt_i, ki=ki: nc.tensor.matmul(
                        po[:64, :], lhsT=vt[:, kt_i, :], rhs=src[:].rearrange("p h q -> p (h q)"),
                        start=(ki == 0), stop=(ki == nk - 1)),
                        lambda pd=pd, src=src, ki=ki: nc.tensor.matmul(
                        pd[:64, :], lhsT=ones64[:], rhs=src[:].rearrange("p h q -> p (h q)"),
                        start=(ki == 0), stop=(ki == nk - 1))],
                        reads=[vt, src, ones64], writes=[po, pd])
                o_, d_ = osb[a], dsb[a]
                kb.op("act", lambda po=po, o_=o_: nc.scalar.copy(out=o_[:].rearrange("p h q -> p (h q)"), in_=po[:64, :]),
                      reads=[po], writes=[o_])
                kb.op("act", lambda pd=pd, d_=d_: nc.scalar.copy(out=d_[:].rearrange("p h q -> p (h q)"), in_=pd[:64, :]),
                      reads=[pd], writes=[d_])
                kb.op("dve", lambda d_=d_: nc.vector.tensor_tensor(
                    out=d_[:], in0=d_[:], in1=sk[:, g * 4:(g + 1) * 4].unsqueeze(2).broadcast_to([64, 4, 128]), op=ALU.add),
                    reads=[d_, sk], writes=[d_])
                kb.op("dve", lambda d_=d_: nc.vector.reciprocal(out=d_[:], in_=d_[:]), reads=[d_], writes=[d_])
                kb.op("pool", lambda o_=o_, d_=d_: nc.gpsimd.tensor_tensor(
                    out=ost[:, :, qs], in0=o_[:], in1=d_[:], op=ALU.mult),
                    reads=[o_, d_], writes=[], acc_writes=[ost])

            prev = None
            for qb in range(NT):
                cur = p1(qb)
                if prev is not None:
                    p2(prev)
                prev = cur
            p2(prev)
            kb.dma("sp", cx.BR_C[g * 256:(g + 1) * 256, :].rearrange("(hh d) t -> d hh t", d=64), ost[:],
                   reads=[ost], writes=[], acc_writes=[cx.BR_C])


def wa_masks():
    kk = np.arange(128)[:, None]
    qq = np.arange(128)[None, :]
    m = np.zeros((128, 2, 128), np.float32)
    m[:, 0, :] = (kk >= qq)
    m[:, 1, :] = (kk <= qq)
    return {"m_wa": m}


def na_patterns():
    W = 64
    tok = np.arange(128)
    lr, lc = tok // W, tok % W
    pats = []
    keys = []
    for m in range(32):
        qr = 2 * m + lr[None, :]
        qc = lc[None, :]
        rs = np.clip(qr - 4, 0, 56)
        cs = np.clip(qc - 8, 0, 48)
        lst = []
        for kt_i in range(32):
            kr = 2 * kt_i + lr[:, None]
            kc = lc[:, None]
            valid = (kr >= rs) & (kr < rs + 8) & (kc >= cs) & (kc < cs + 16)
            if not valid.any():
                continue
            ri = np.clip(kr - qr + 7, 0, 14)
            ci = np.clip(kc - qc + 15, 0, 30)
            if 2 <= m <= 29:
                pid = kt_i - m + 2
                if m == 2:
                    pats.append((valid, ri, ci))
            else:
                pid = len(pats)
                pats.append((valid, ri, ci))
            lst.append((kt_i, pid))
        keys.append(lst)
    return keys, pats


def na_consts():
    keys, pats = na_patterns()
    n0 = len(keys[0]) + len(keys[1])
    order = list(range(n0, n0 + 5)) + list(range(0, n0)) + list(range(n0 + 5, len(pats)))
    remap = {old: new for new, old in enumerate(order)}
    pats2 = [pats[o] for o in order]
    keys2 = []
    for m, lst in enumerate(keys):
        if 2 <= m <= 29:
            keys2.append(lst)
        else:
            keys2.append([(k, remap[p]) for (k, p) in lst])
    valid = np.stack([p[0] for p in pats2], 1).astype(np.float32)
    ri = np.stack([p[1] for p in pats2], 1)
    ci = np.stack([p[2] for p in pats2], 1)
    return keys2, valid, ri, ci


def stage_na(cx, kb, nc, l, heads=None, bg_factory=None):
    keys_tab, valid_np, _, _ = na_consts()
    npat = valid_np.shape[1]
    with kb.scope():
        vmask = kb.sbuf("vmask", [128, npat, 128], F32)
        kb.dma("sp", vmask[:], cx.m_navalid[:], reads=[cx.m_navalid], writes=[vmask])
        ones64 = kb.sbuf("ones64n", [128, 64], BF16)
        kb.op("dve", lambda: nc.vector.memset(ones64[:], 1.0), writes=[ones64])
        qT = kb.sbuf("qTn", [64, T], BF16)
        kT = kb.sbuf("kTn", [64, T], BF16)
        vt = kb.sbuf("vtn", [128, NT, 64], BF16)
        ost = kb.sbuf("ostn", [64, T], BF16)
        bias = kb.sbuf("biasn", [128, npat, 128], F32)
        EB = kb.sbuf("EBn", [128, npat, 128], BF16)
        pS = [kb.psum("pSn%d" % i, [128, 512]) for i in range(4)]
        pO = [kb.psum("pOn%d" % i, [128, 512]) for i in range(2)]
        pD = [kb.psum("pDn%d" % i, [128, 512]) for i in range(2)]
        R = 6
        pt = [kb.sbuf("ptn%d" % i, [128, 4, 128], BF16) for i in range(R)]
        osb = [kb.sbuf("osbn%d" % i, [64, 256], F32) for i in range(2)]
        it = 0
        bg = None
        if bg_factory is not None:
            stw = [kb.sbuf("pcw%d" % i, [128, 8, 256], BF16) for i in range(4)]
            std = [kb.sbuf("pcd%d" % i, [128, 16, 1024], BF16) for i in range(2)]
            bg = bg_factory(stw, std)
        for h in (range(8) if heads is None else heads):
            kb.dma("sp", bias[:], cx.na_bias[l, h], reads=[cx.na_bias], writes=[bias])
            kb.op("act", lambda: nc.scalar.activation(out=bias[:], in_=bias[:], func=AF.Exp), reads=[bias], writes=[bias])
            kb.op("pool", lambda: nc.gpsimd.tensor_tensor(out=EB[:], in0=bias[:], in1=vmask[:], op=ALU.mult),
                  reads=[bias, vmask], writes=[EB])
            kb.dma("sp", qT[:], cx.QB[h * 64:(h + 1) * 64, :], reads=[cx.QB], writes=[qT])
            kb.dma("sp", kT[:], cx.KB_[h * 64:(h + 1) * 64, :], reads=[cx.KB_], writes=[kT])
            kb.dma("sp", vt[:], cx.VB_t[:, :, h * 64:(h + 1) * 64], reads=[cx.VB_t], writes=[vt])
            def p1(qb):
                nonlocal it
                if qb < 32:
                    keys = list(keys_tab[qb]) + [(32, None), (33, None)]
                else:
                    keys = [(32, None), (33, None)]
                qs = slice(qb * 128, (qb + 1) * 128)
                grp = [keys[i:i + 4] for i in range(0, len(keys), 4)]
                st_ = []
                for gk in grp:
                    ps = pS[it % 4]
                    p_ = pt[it % R]
                    it += 1
                    ng = len(gk)
                    kb.mm([(lambda ps=ps, j=j, kt_i=kt_i: nc.tensor.matmul(
                        ps[:, j * 128:(j + 1) * 128], lhsT=kT[:, kt_i * 128:(kt_i + 1) * 128], rhs=qT[:, qs],
                        start=True, stop=True)) for j, (kt_i, _) in enumerate(gk)],
                        reads=[kT, qT], writes=[ps])
                    kb.op("act", lambda ps=ps, p_=p_, ng=ng: nc.scalar.activation(
                        out=p_[:, :ng, :].rearrange("p h q -> p (h q)"), in_=ps[:, :ng * 128], func=AF.Exp, scale=0.125),
                        reads=[ps], writes=[p_])
                    nloc = sum(1 for (_, pid) in gk if pid is not None)
                    if nloc:
                        pid0 = gk[0][1]
                        eng = "dve" if it % 2 == 0 else "pool"
                        h_ = nc.vector if eng == "dve" else nc.gpsimd
                        kb.op(eng, lambda h_=h_, p_=p_, pid0=pid0, nloc=nloc: h_.tensor_tensor(
                            out=p_[:, :nloc, :], in0=p_[:, :nloc, :], in1=EB[:, pid0:pid0 + nloc, :], op=ALU.mult),
                            reads=[p_, EB], writes=[p_])
                    st_.append((gk, p_))
                return (qb, st_, len(keys))

            def p2(state):
                qb, st_, nk = state
                qs = slice(qb * 128, (qb + 1) * 128)
                a = qb % 2
                po = pO[a]
                pd = pD[a]
                done = 0
                for gk, p_ in st_:
                    fns = []
                    for j, (kt_i, _) in enumerate(gk):
                        first = (done == 0)
                        last = (done == nk - 1)
                        fns.append(lambda po=po, p_=p_, j=j, kt_i=kt_i, first=first, last=last: nc.tensor.matmul(
                            po[:64, 0:128], lhsT=vt[:, kt_i, :], rhs=p_[:, j, :], start=first, stop=last))
                        fns.append(lambda pd=pd, p_=p_, j=j, first=first, last=last: nc.tensor.matmul(
                            pd[:64, 0:128], lhsT=ones64[:], rhs=p_[:, j, :], start=first, stop=last))
                        done += 1
                    kb.mm(fns, reads=[vt, p_, ones64], writes=[po, pd])
                o_ = osb[a]
                kb.op("act", lambda po=po, o_=o_: nc.scalar.copy(out=o_[:, 0:128], in_=po[:64, 0:128]), reads=[po], writes=[o_])
                kb.op("act", lambda pd=pd, o_=o_: nc.scalar.copy(out=o_[:, 128:256], in_=pd[:64, 0:128]), reads=[pd, o_], writes=[o_])
                kb.op("dve", lambda o_=o_: nc.vector.reciprocal(out=o_[:, 128:256], in_=o_[:, 128:256]), reads=[o_], writes=[o_])
                kb.op("pool", lambda o_=o_: nc.gpsimd.tensor_tensor(
                    out=ost[:, qs], in0=o_[:, 0:128], in1=o_[:, 128:256], op=ALU.mult),
                    reads=[o_], writes=[], acc_writes=[ost])

            prev = None
            for qb in range(NT):
                cur = p1(qb)
                if prev is not None:
                    p2(prev)
                prev = cur
                if bg is not None and qb % 2 == 1:
                    next(bg, None)
            p2(prev)
            kb.dma("sp", cx.BR_B[h * 64:(h + 1) * 64, :], ost[:], reads=[ost], writes=[], acc_writes=[cx.BR_B])
        if bg is not None:
            for _ in bg:
                pass

LN_EPS = 1e-5
MB = 512
MBLKS = [(i * MB, min(MB, T - i * MB)) for i in range((T + MB - 1) // MB)]


def ln_setup(cx, kb, nc):
    ones = kb.sbuf("ln_ones", [128, 128], F32)
    kb.op("dve", lambda: nc.vector.memset(ones[:], 1.0 / D), writes=[ones])
    eps = kb.sbuf("ln_eps", [128, 1], F32)
    kb.op("dve", lambda: nc.vector.memset(eps[:], LN_EPS), writes=[eps])
    return ones, eps


def ln_block(kb, nc, r, nb, gam, bet, out, ones, eps, scr):
    rsq, ps1, ps2, mean, var = scr
    kb.op("act", lambda: nc.scalar.activation(out=rsq[:, :, :nb], in_=r[:, :, :nb], func=AF.Square), reads=[r], writes=[rsq])
    kb.mm([(lambda oc=oc: nc.tensor.matmul(ps1[:, :nb], lhsT=ones[:], rhs=r[:, oc, :nb], start=(oc == 0), stop=(oc == 7)))
           for oc in range(8)], reads=[ones, r], writes=[ps1])
    kb.mm([(lambda oc=oc: nc.tensor.matmul(ps2[:, :nb], lhsT=ones[:], rhs=rsq[:, oc, :nb], start=(oc == 0), stop=(oc == 7)))
           for oc in range(8)], reads=[ones, rsq], writes=[ps2])
    kb.op("act", lambda: nc.scalar.copy(out=mean[:, :nb], in_=ps1[:, :nb]), reads=[ps1], writes=[mean])
    kb.op("act", lambda: nc.scalar.copy(out=var[:, :nb], in_=ps2[:, :nb]), reads=[ps2], writes=[var])
    m2 = rsq
    kb.op("dve", lambda: nc.vector.tensor_tensor(out=m2[:, 0, :nb], in0=mean[:, :nb], in1=mean[:, :nb], op=ALU.mult),
          reads=[mean, rsq], writes=[rsq])
    kb.op("dve", lambda: nc.vector.tensor_tensor(out=var[:, :nb], in0=var[:, :nb], in1=m2[:, 0, :nb], op=ALU.subtract),
          reads=[var, rsq], writes=[var])
    kb.op("act", lambda: nc.scalar.activation(out=var[:, :nb], in_=var[:, :nb], func=AF.Sqrt, bias=eps[:]),
          reads=[var, eps], writes=[var])
    kb.op("dve", lambda: nc.vector.reciprocal(out=var[:, :nb], in_=var[:, :nb]), reads=[var], writes=[var])
    kb.op("dve", lambda: nc.vector.tensor_tensor(
        out=r[:, :, :nb], in0=r[:, :, :nb], in1=mean[:, :nb].unsqueeze(1).broadcast_to([128, 8, nb]), op=ALU.subtract),
        reads=[r, mean], writes=[r])
    kb.op("pool", lambda: nc.gpsimd.tensor_tensor(
        out=r[:, :, :nb], in0=r[:, :, :nb], in1=var[:, :nb].unsqueeze(1).broadcast_to([128, 8, nb]), op=ALU.mult),
        reads=[r, var], writes=[r])
    for oc in range(8):
        eng = "dve" if oc % 2 == 0 else "pool"
        h_ = nc.vector if eng == "dve" else nc.gpsimd
        kb.op(eng, lambda h_=h_, oc=oc: h_.tensor_scalar(
            out=out[:, oc, :nb], in0=r[:, oc, :nb], scalar1=gam[:, oc:oc + 1], scalar2=bet[:, oc:oc + 1],
            op0=ALU.mult, op1=ALU.add), reads=[r, gam, bet], writes=[out])


def load_ln_params(cx, kb, nc, l, which):
    gam = kb.sbuf("ln_g", [128, 8], F32)
    bet = kb.sbuf("ln_b", [128, 8], F32)
    kb.dma("sp", gam[:], cx.ln_gT[l, which], reads=[cx.ln_gT], writes=[gam])
    kb.dma("sp", bet[:], cx.ln_bT[l, which], reads=[cx.ln_bT], writes=[bet])
    return gam, bet


def stage_merge(cx, kb, nc, l, xsrc, modv):
    with kb.scope():
        ones, eps = ln_setup(cx, kb, nc)
        gam, bet = load_ln_params(cx, kb, nc, l, 0)
        wbr = kb.sbuf("wbr", [128, 4, 4, 1024], BF16)
        wout = kb.sbuf("wout", [128, 8, 1024], BF16)
        for i in range(4):
            kb.dma("pool", wbr[:, i], cx.w_branch[l, i].rearrange("(kc p) c -> p kc c", p=128), reads=[cx.w_branch], writes=[wbr])
        kb.dma("pool", wout[:], cx.w_out[l].rearrange("(kc p) c -> p kc c", p=128), reads=[cx.w_out], writes=[wout])
        wr = kb.sbuf("wr", [128, 8, 36], F32)
        kb.dma("sp", wr[:, :, 0:4], cx.moe_w_group[l].rearrange("(kc p) c -> p kc c", p=128), reads=[cx.moe_w_group], writes=[wr])
        kb.dma("sp", wr[:, :, 4:36], cx.moe_w_expert[l].rearrange("(kc p) c -> p kc c", p=128), reads=[cx.moe_w_expert], writes=[wr])
        rb = kb.sbuf("rb", [128, 36], F32)
        kb.dma("sp", rb[:, 0:4], cx.moe_b_group[l].partition_broadcast(128), reads=[cx.moe_b_group], writes=[rb])
        kb.dma("sp", rb[:, 4:36], cx.moe_b_expert[l].partition_broadcast(128), reads=[cx.moe_b_expert], writes=[rb])
        onep4 = kb.sbuf("onep4", [128, 8, 2], F32)
        kb.op("dve", lambda: nc.vector.tensor_scalar(out=onep4[:], in0=modv[:, 32:40, :], scalar1=1.0, scalar2=None, op0=ALU.add),
              reads=[modv], writes=[onep4])
        brt = [kb.sbuf("brt%d" % i, [128, 4, 4, MB], BF16) for i in range(1)]
        gtt = kb.sbuf("gtt", [128, 32, MB], BF16)
        xb = kb.sbuf("xbm", [128, 8, MB], F32)
        mrg = kb.sbuf("mrg", [128, 8, MB], BF16)
        r = kb.sbuf("rm", [128, 8, MB], F32)
        hx2b = kb.sbuf("hx2b", [128, 8, MB], BF16)
        rsq = kb.sbuf("rsqm", [128, 8, MB], F32)
        x1 = r
        hx2f = rsq
        mean = kb.sbuf("meanm", [128, MB], F32)
        var = kb.sbuf("varm", [128, MB], F32)
        csb = [kb.sbuf("csb%d" % i, [128, MB], BF16) for i in range(4)]
        acc = kb.sbuf("accm", [128, MB], BF16)
        mixs = kb.sbuf("mixs", [128, MB], F32)
        pbr = [kb.psum("pbr%d" % i, [128, MB]) for i in range(4)]
        pmx = kb.psum("pmx", [128, MB])
        ps1 = kb.psum("ps1m", [128, MB])
        ps2 = kb.psum("ps2m", [128, MB])
        prt = kb.psum("prt", [128, MB])
        brs = [cx.BR_A, cx.BR_B, cx.BR_C, cx.BR_D]
        lgt = kb.sbuf("lgt", [128, 36], F32)
        gmx = kb.sbuf("gmx", [128, 1], F32)
        ngm = kb.sbuf("ngm", [128, 1], F32)
        ge = kb.sbuf("ge", [128, 4], F32)
        gs = kb.sbuf("gsr", [128, 1], F32)
        oh = kb.sbuf("oh", [128, 4], F32)
        lem = kb.sbuf("lem", [128, 4, 8], F32)
        mx8 = kb.sbuf("mx8", [128, 8], F32)
        dd = kb.sbuf("dd", [128, 1], F32)
        w1 = kb.sbuf("w1", [128, 1], F32)
        w2 = kb.sbuf("w2", [128, 1], F32)
        tt1 = kb.sbuf("tt1", [128, 32], F32)
        tt2 = kb.sbuf("tt2", [128, 32], F32)
        wgt = kb.sbuf("wgt", [128, 32], F32)
        for bi, (t0, nb) in enumerate(MBLKS):
            col = blkcol(t0)
            b_ = brt[0]
            for i in range(4):
                kb.dma("sp", b_[:, i, :, :nb], brs[i][:, t0:t0 + nb].rearrange("(kc p) t -> p kc t", p=128),
                       reads=[brs[i]], writes=[b_])
            kb.dma("sp", gtt[:, :, :nb], cx.GATE[:, t0:t0 + nb].rearrange("(j p) t -> p j t", p=128), reads=[cx.GATE], writes=[gtt])
            kb.dma("sp", xb[:, :, :nb], xsrc[:, t0:t0 + nb].rearrange("(kc p) t -> p kc t", p=128), reads=[xsrc], writes=[xb])
            for oc in range(8):
                for i in range(4):
                    kb.mm([(lambda i=i, kc=kc, oc=oc: nc.tensor.matmul(
                        pbr[i][:, :nb], lhsT=wbr[:, i, kc, oc * 128:(oc + 1) * 128], rhs=b_[:, i, kc, :nb],
                        start=(kc == 0), stop=(kc == 3))) for kc in range(4)], reads=[wbr, b_], writes=[pbr[i]])
                    kb.op("act", lambda i=i: nc.scalar.copy(out=csb[i][:, :nb], in_=pbr[i][:, :nb]), reads=[pbr[i]], writes=[csb[i]])
                    eng = "dve" if i % 2 == 0 else "pool"
                    h_ = nc.vector if eng == "dve" else nc.gpsimd
                    kb.op(eng, lambda h_=h_, i=i, oc=oc: h_.tensor_tensor(
                        out=csb[i][:, :nb], in0=csb[i][:, :nb], in1=gtt[:, i * 8 + oc, :nb], op=ALU.mult),
                        reads=[csb[i], gtt], writes=[csb[i]])
                kb.op("dve", lambda: nc.vector.tensor_tensor(out=acc[:, :nb], in0=csb[0][:, :nb], in1=csb[1][:, :nb], op=ALU.add),
                      reads=[csb[0], csb[1]], writes=[acc])
                kb.op("pool", lambda: nc.gpsimd.tensor_tensor(out=csb[2][:, :nb], in0=csb[2][:, :nb], in1=csb[3][:, :nb], op=ALU.add),
                      reads=[csb[2], csb[3]], writes=[csb[2]])
                kb.op("dve", lambda oc=oc: nc.vector.tensor_tensor(out=mrg[:, oc, :nb], in0=acc[:, :nb], in1=csb[2][:, :nb], op=ALU.add),
                      reads=[acc, csb[2]], writes=[mrg])
            for oc in range(8):
                kb.mm([(lambda kc=kc, oc=oc: nc.tensor.matmul(
                    pmx[:, :nb], lhsT=wout[:, kc, oc * 128:(oc + 1) * 128], rhs=mrg[:, kc, :nb],
                    start=(kc == 0), stop=(kc == 7))) for kc in range(8)], reads=[wout, mrg], writes=[pmx])
                kb.op("act", lambda: nc.scalar.copy(out=mixs[:, :nb], in_=pmx[:, :nb]), reads=[pmx], writes=[mixs])
                kb.op("pool", lambda oc=oc: nc.gpsimd.tensor_scalar(
                    out=mixs[:, :nb], in0=mixs[:, :nb], scalar1=modv[:, 16 + oc, col:col + 1], scalar2=None, op0=ALU.mult),
                    reads=[mixs, modv], writes=[mixs])
                kb.op("dve", lambda oc=oc: nc.vector.scalar_tensor_tensor(
                    out=r[:, oc, :nb], in0=xb[:, oc, :nb], scalar=ALPHA, in1=mixs[:, :nb], op0=ALU.mult, op1=ALU.add),
                    reads=[xb, mixs], writes=[r])
            ln_block(kb, nc, r, nb, gam, bet, x1, ones, eps, (rsq, ps1, ps2, mean, var))
            kb.dma("sp", cx.XT[:, t0:t0 + nb].rearrange("(kc p) t -> p kc t", p=128), x1[:, :, :nb],
                   reads=[x1], writes=[], acc_writes=[cx.XT])
            for oc in range(8):
                eng = "dve" if oc % 2 == 0 else "pool"
                h_ = nc.vector if eng == "dve" else nc.gpsimd
                kb.op(eng, lambda h_=h_, oc=oc: h_.tensor_scalar(
                    out=hx2f[:, oc, :nb], in0=x1[:, oc, :nb], scalar1=onep4[:, oc, col:col + 1],
                    scalar2=modv[:, 24 + oc, col:col + 1], op0=ALU.mult, op1=ALU.add),
                    reads=[x1, onep4, modv], writes=[hx2f])
            kb.op("act", lambda: nc.scalar.copy(out=hx2b[:, :, :nb], in_=hx2f[:, :, :nb]), reads=[hx2f], writes=[hx2b])
            kb.dma("sp", cx.HX2[:, t0:t0 + nb].rearrange("(kc p) t -> p kc t", p=128), hx2b[:, :, :nb],
                   reads=[hx2b], writes=[], acc_writes=[cx.HX2])
            for tj in range(nb // 128):
                ti = t0 // 128 + tj
                tsl = slice(tj * 128, (tj + 1) * 128)
                kb.mm([(lambda kc=kc: nc.tensor.matmul(prt[:, 0:36], lhsT=hx2f[:, kc, tsl], rhs=wr[:, kc, :],
                                                       start=(kc == 0), stop=(kc == 7))) for kc in range(8)],
                      reads=[hx2f, wr], writes=[prt])
                kb.op("act", lambda: nc.scalar.copy(out=lgt[:], in_=prt[:, 0:36]), reads=[prt], writes=[lgt])
                kb.op("dve", lambda: nc.vector.tensor_tensor(out=lgt[:], in0=lgt[:], in1=rb[:], op=ALU.add), reads=[lgt, rb], writes=[lgt])
                kb.op("dve", lambda: nc.vector.tensor_reduce(out=gmx[:], in_=lgt[:, 0:4], axis=AX.X, op=ALU.max), reads=[lgt], writes=[gmx])
                kb.op("dve", lambda: nc.vector.tensor_scalar(out=ngm[:], in0=gmx[:], scalar1=-1.0, scalar2=None, op0=ALU.mult), reads=[gmx], writes=[ngm])
                kb.op("act", lambda: nc.scalar.activation(out=ge[:], in_=lgt[:, 0:4], func=AF.Exp, bias=ngm[:]), reads=[lgt, ngm], writes=[ge])
                kb.op("dve", lambda: nc.vector.tensor_reduce(out=gs[:], in_=ge[:], axis=AX.X, op=ALU.add), reads=[ge], writes=[gs])
                kb.op("dve", lambda: nc.vector.reciprocal(out=gs[:], in_=gs[:]), reads=[gs], writes=[gs])
                kb.op("dve", lambda: nc.vector.tensor_scalar(out=oh[:], in0=lgt[:, 0:4], scalar1=gmx[:], scalar2=None, op0=ALU.is_equal), reads=[lgt, gmx], writes=[oh])
                kb.op("dve", lambda: nc.vector.tensor_scalar(out=oh[:], in0=oh[:], scalar1=-1.0, scalar2=1e30, op0=ALU.add, op1=ALU.mult), reads=[oh], writes=[oh])
                kb.op("dve", lambda: nc.vector.tensor_tensor(
                    out=lem[:], in0=lgt[:, 4:36].rearrange("p (g e) -> p g e", e=8), in1=oh[:].unsqueeze(2).broadcast_to([128, 4, 8]), op=ALU.add),
                    reads=[lgt, oh], writes=[lem])
                lemf = lem[:].rearrange("p g e -> p (g e)")
                kb.op("dve", lambda: nc.vector.max(out=mx8[:], in_=lem[:].rearrange("p g e -> p (g e)")), reads=[lem], writes=[mx8])
                kb.op("dve", lambda: nc.vector.tensor_tensor(out=dd[:], in0=mx8[:, 1:2], in1=mx8[:, 0:1], op=ALU.subtract), reads=[mx8], writes=[dd])
                kb.op("act", lambda: nc.scalar.activation(out=dd[:], in_=dd[:], func=AF.Exp), reads=[dd], writes=[dd])
                kb.op("dve", lambda: nc.vector.tensor_scalar(out=w1[:], in0=dd[:], scalar1=1.0, scalar2=None, op0=ALU.add), reads=[dd], writes=[w1])
                kb.op("dve", lambda: nc.vector.reciprocal(out=w1[:], in_=w1[:]), reads=[w1], writes=[w1])
                kb.op("dve", lambda: nc.vector.tensor_tensor(out=w2[:], in0=dd[:], in1=w1[:], op=ALU.mult), reads=[dd, w1], writes=[w2])
                kb.op("dve", lambda: nc.vector.tensor_scalar(
                    out=tt1[:], in0=lem[:].rearrange("p g e -> p (g e)"), scalar1=mx8[:, 0:1], scalar2=w1[:], op0=ALU.is_equal, op1=ALU.mult),
                    reads=[lem, mx8, w1], writes=[tt1])
                kb.op("dve", lambda: nc.vector.tensor_scalar(
                    out=tt2[:], in0=lem[:].rearrange("p g e -> p (g e)"), scalar1=mx8[:, 1:2], scalar2=w2[:], op0=ALU.is_equal, op1=ALU.mult),
                    reads=[lem, mx8, w2], writes=[tt2])
                kb.op("dve", lambda: nc.vector.tensor_tensor(out=tt1[:], in0=tt1[:], in1=tt2[:], op=ALU.add), reads=[tt1, tt2], writes=[tt1])
                kb.op("dve", lambda: nc.vector.tensor_scalar(out=wgt[:], in0=tt1[:], scalar1=gs[:], scalar2=None, op0=ALU.mult), reads=[tt1, gs], writes=[wgt])
                kb.dma("sp", cx.WG_t[:, ti, :], wgt[:], reads=[wgt], writes=[], acc_writes=[cx.WG_t])


def stage_moe(cx, kb, nc, l, modv, yout=None, precast=True):
    with kb.scope():
        ones, eps = ln_setup(cx, kb, nc)
        gam, bet = load_ln_params(cx, kb, nc, l, 1)
        wgT = kb.sbuf("wgT", [32, T], F32)
        sel = kb.sbuf("sel", [32, 32, 128], F32)
        kb.dma("sp", sel[:], cx.m_sel[:], reads=[cx.m_sel], writes=[sel])
        ptr = [kb.psum("ptr%d" % i, [128, 512]) for i in range(2)]
        with kb.scope():
            wgl = kb.sbuf("wgl", [128, NT, 32], F32)
            id32 = kb.sbuf("id32", [128, 128], F32)
            kb.dma("sp", wgl[:], cx.WG_t[:], reads=[cx.WG_t], writes=[wgl])
            kb.dma("sp", id32[:], cx.m_ident32[:], reads=[cx.m_ident32], writes=[id32])
            for ti in range(NT):
                p_ = ptr[ti % 2]
                kb.mm([lambda p_=p_, ti=ti: nc.tensor.matmul(p_[:32, 0:128], lhsT=wgl[:, ti, :], rhs=id32[:], start=True, stop=True)],
                      reads=[wgl, id32], writes=[p_])
                kb.op("act", lambda p_=p_, ti=ti: nc.scalar.copy(out=wgT[:, ti * 128:(ti + 1) * 128], in_=p_[:32, 0:128]),
                      reads=[p_], writes=[], acc_writes=[wgT])
        if precast:
            precast_expert_weights(cx, kb, nc, l)
        hx = kb.sbuf("hxm", [128, 8, 512], BF16)
        yacc = kb.sbuf("yacc", [128, 8, 512], F32)
        hid = kb.sbuf("hid", [128, 16, 512], BF16)
        wdg = kb.sbuf("wdg", [128, 8, 2, 1024], BF16)
        NWB = 4
        wgs = [kb.sbuf("wgs%d" % i, [128, 8, 256], BF16) for i in range(NWB)]
        wus = [kb.sbuf("wus%d" % i, [128, 8, 256], BF16) for i in range(NWB)]
        xb = kb.sbuf("xbe", [128, 8, 512], F32)
        rsq = kb.sbuf("rsqe", [128, 8, 512], F32)
        mean = kb.sbuf("meane", [128, 512], F32)
        var = kb.sbuf("vare", [128, 512], F32)
        reps = [kb.sbuf("reps%d" % i, [128, 512], BF16) for i in range(2)]
        ssb = [kb.sbuf("ssb%d" % i, [128, 512], BF16) for i in range(3)]
        ytmp = [kb.sbuf("ytmp%d" % i, [128, 512], F32) for i in range(2)]
        NPS = 3
        pg = [kb.psum("pg%d" % i, [128, 512]) for i in range(NPS)]
        pu = [kb.psum("pu%d" % i, [128, 512]) for i in range(NPS)]
        py = ptr
        ps1 = pg[0]
        ps2 = pu[0]
        prep = ptr[1]
        nw = 0
        it = 0
        for bi, (t0, nb) in enumerate(BLKS):
            col = blkcol(t0)
            kb.dma("sp", hx[:, :, :nb], cx.HX2[:, t0:t0 + nb].rearrange("(kc p) t -> p kc t", p=128), reads=[cx.HX2], writes=[hx])
            kb.dma("sp", xb[:, :, :nb], cx.XT[:, t0:t0 + nb].rearrange("(kc p) t -> p kc t", p=128), reads=[cx.XT], writes=[xb])
            for g in range(4):
                kb.dma("sp", wdg[:].rearrange("p e hc d -> p (e hc d)"), cx.WDB[g], reads=[cx.WDB], writes=[wdg])
                for e_ in range(8):
                    e = g * 8 + e_
                    wg_, wu_ = wgs[nw % NWB], wus[nw % NWB]
                    nw += 1
                    kb.dma("sp", wg_[:].rearrange("p kc f -> p (kc f)"), cx.WGB[e], reads=[cx.WGB], writes=[wg_])
                    kb.dma("act", wu_[:].rearrange("p kc f -> p (kc f)"), cx.WUB[e], reads=[cx.WUB], writes=[wu_])
                    rp = reps[e % 2]
                    kb.mm([lambda e=e: nc.tensor.matmul(prep[:, :nb], lhsT=sel[:, e, :], rhs=wgT[:, t0:t0 + nb], start=True, stop=True)],
                          reads=[sel, wgT], writes=[prep])
                    kb.op("act", lambda rp=rp: nc.scalar.copy(out=rp[:, :nb], in_=prep[:, :nb]), reads=[prep], writes=[rp])
                    for hc in range(2):
                        a = it % NPS
                        it += 1
                        kb.mm([(lambda kc=kc, a=a, hc=hc, wg_=wg_: nc.tensor.matmul(
                            pg[a][:, :nb], lhsT=wg_[:, kc, hc * 128:(hc + 1) * 128], rhs=hx[:, kc, :nb],
                            start=(kc == 0), stop=(kc == 7))) for kc in range(8)], reads=[wg_, hx], writes=[pg[a]])
                        kb.mm([(lambda kc=kc, a=a, hc=hc, wu_=wu_: nc.tensor.matmul(
                            pu[a][:, :nb], lhsT=wu_[:, kc, hc * 128:(hc + 1) * 128], rhs=hx[:, kc, :nb],
                            start=(kc == 0), stop=(kc == 7))) for kc in range(8)], reads=[wu_, hx], writes=[pu[a]])
                        kb.op("act", lambda a=a: nc.scalar.activation(out=ssb[a][:, :nb], in_=pg[a][:, :nb], func=AF.Silu),
                              reads=[pg[a]], writes=[ssb[a]])
                        kb.op("pool", lambda a=a, rp=rp: nc.gpsimd.tensor_tensor(
                            out=ssb[a][:, :nb], in0=ssb[a][:, :nb], in1=rp[:, :nb], op=ALU.mult),
                            reads=[ssb[a], rp], writes=[ssb[a]])
                        kb.op("dve", lambda a=a, e_=e_, hc=hc: nc.vector.tensor_tensor(
                            out=hid[:, e_ * 2 + hc, :nb], in0=pu[a][:, :nb], in1=ssb[a][:, :nb], op=ALU.mult),
                            reads=[pu[a], ssb[a]], writes=[], acc_writes=[hid])
                for oc in range(8):
                    p_ = py[oc % 2]
                    fns = []
                    for j in range(16):
                        fns.append(lambda j=j, oc=oc, p_=p_: nc.tensor.matmul(
                            p_[:, :nb], lhsT=wdg[:, j // 2, j % 2, oc * 128:(oc + 1) * 128], rhs=hid[:, j, :nb],
                            start=(j == 0), stop=(j == 15)))
                    kb.mm(fns, reads=[wdg, hid], writes=[p_])
                    if g == 0:
                        kb.op("act", lambda oc=oc, p_=p_: nc.scalar.copy(out=yacc[:, oc, :nb], in_=p_[:, :nb]), reads=[p_], writes=[], acc_writes=[yacc])
                    else:
                        yt = ytmp[oc % 2]
                        kb.op("act", lambda yt=yt, p_=p_: nc.scalar.copy(out=yt[:, :nb], in_=p_[:, :nb]), reads=[p_], writes=[yt])
                        eng = "dve" if oc % 2 == 0 else "pool"
                        h_ = nc.vector if eng == "dve" else nc.gpsimd
                        kb.op(eng, lambda h_=h_, oc=oc, yt=yt: h_.tensor_tensor(
                            out=yacc[:, oc, :nb], in0=yacc[:, oc, :nb], in1=yt[:, :nb], op=ALU.add),
                            reads=[yt, yacc], writes=[], acc_writes=[yacc])
            for oc in range(8):
                kb.op("pool", lambda oc=oc: nc.gpsimd.tensor_scalar(
                    out=yacc[:, oc, :nb], in0=yacc[:, oc, :nb], scalar1=modv[:, 40 + oc, col:col + 1], scalar2=None, op0=ALU.mult),
                    reads=[yacc, modv], writes=[yacc])
                kb.op("dve", lambda oc=oc: nc.vector.scalar_tensor_tensor(
                    out=yacc[:, oc, :nb], in0=xb[:, oc, :nb], scalar=ALPHA, in1=yacc[:, oc, :nb], op0=ALU.mult, op1=ALU.add),
                    reads=[xb, yacc], writes=[yacc])
            ln_block(kb, nc, yacc, nb, gam, bet, yacc, ones, eps, (rsq, ps1, ps2, mean, var))
            kb.dma("sp", cx.XT[:, t0:t0 + nb].rearrange("(kc p) t -> p kc t", p=128), yacc[:, :, :nb],
                   reads=[yacc], writes=[], acc_writes=[cx.XT])
            if yout is not None and t0 < TL:
                kb.dma("sp", yout[:, t0:t0 + nb].rearrange("(kc p) t -> p kc t", p=128), yacc[:, :, :nb],
                       reads=[yacc], writes=[], acc_writes=[yout])


def precast_expert_weights(cx, kb, nc, l):
    with kb.scope():
        stw = [kb.sbuf("pcw%d" % i, [128, 8, 256], BF16) for i in range(4)]
        std = [kb.sbuf("pcd%d" % i, [128, 16, 1024], BF16) for i in range(2)]
        n = 0
        for g in range(4):
            d_ = std[g % 2]
            kb.dma("pool", d_[:], cx.moe_w_down[l, g].rearrange("e (hc p) d -> p (e hc) d", p=128), reads=[cx.moe_w_down], writes=[d_])
            kb.dma("sp", cx.WDB[g], d_[:].rearrange("p j d -> p (j d)"), reads=[d_], writes=[], acc_writes=[cx.WDB])
            for e_ in range(8):
                e = g * 8 + e_
                for src, dst in ((cx.moe_w_gate, cx.WGB), (cx.moe_w_up, cx.WUB)):
                    w_ = stw[n % 4]
                    n += 1
                    kb.dma("pool", w_[:], src[l, g, e_].rearrange("(kc p) f -> p kc f", p=128), reads=[src], writes=[w_])
                    kb.dma("sp", dst[e], w_[:].rearrange("p kc f -> p (kc f)"), reads=[w_], writes=[], acc_writes=[dst])


def precast_gen(cx, kb, nc, l, stw, std):
    pend = []
    n = 0
    for g in range(4):
        d_ = std[g % 2]
        kb.dma("pool", d_[:], cx.moe_w_down[l, g].rearrange("e (hc p) d -> p (e hc) d", p=128), reads=[cx.moe_w_down], writes=[d_])
        pend.append((cx.WDB, g, d_, "p j d -> p (j d)"))
        yield
        for e_ in range(8):
            e = g * 8 + e_
            for src, dst in ((cx.moe_w_gate, cx.WGB), (cx.moe_w_up, cx.WUB)):
                w_ = stw[n % len(stw)]
                n += 1
                kb.dma("pool", w_[:], src[l, g, e_].rearrange("(kc p) f -> p kc f", p=128), reads=[src], writes=[w_])
                pend.append((dst, e, w_, "p kc f -> p (kc f)"))
                yield
                while len(pend) > 2:
                    dst_, idx_, buf_, pat_ = pend.pop(0)
                    kb.dma("sp", dst_[idx_], buf_[:].rearrange(pat_), reads=[buf_], writes=[], acc_writes=[dst_])
    while pend:
        dst_, idx_, buf_, pat_ = pend.pop(0)
        kb.dma("sp", dst_[idx_], buf_[:].rearrange(pat_), reads=[buf_], writes=[], acc_writes=[dst_])
        yield


def rope_tables():
    t = np.arange(TL)
    row = (t // 64).astype(np.float32)
    colp = (t % 64).astype(np.float32)
    nf = 16
    inv = (np.float32(10000.0) ** (-np.arange(nf, dtype=np.float32) / np.float32(nf))).astype(np.float32)
    cos = np.ones((128, T), np.float32)
    sin = np.zeros((128, T), np.float32)
    for q in range(128):
        d = q % 64
        pos = row if d < 32 else colp
        ang = (pos * inv[d % 16]).astype(np.float32)
        cos[q, :TL] = np.cos(ang).astype(np.float32)
        sin[q, :TL] = np.sin(ang).astype(np.float32)
    pt = np.zeros((128, 128), np.float32)
    for m in range(128):
        if (m % 32) < 16:
            pt[m + 16, m] = -1.0
        else:
            pt[m - 16, m] = 1.0
    return cos, sin, pt


_CONST = {}
_NAIDX = {}


def consts():
    if not _CONST:
        cos, sin, pt = rope_tables()
        _CONST.update(rope_cos=cos, rope_sin=sin, rope_pt=pt)
        _CONST.update(scan_masks())
        _CONST.update(wa_masks())
        sel = np.zeros((32, 32, 128), np.float32)
        for e in range(32):
            sel[e, e, :] = 1.0
        _CONST["m_sel"] = sel
        _CONST["m_ident32"] = np.eye(128, dtype=np.float32)
        _k, _valid, _ri, _ci = na_consts()
        _CONST["m_navalid"] = _valid
        _NAIDX["ri"] = _ri
        _NAIDX["ci"] = _ci
    return _CONST


def core_inputs(inp, b):
    f = lambda a: np.ascontiguousarray(a, dtype=np.float32)
    m = {}
    m["xin"] = f(np.concatenate([inp["x"][b].T, inp["ctx"][b].T], axis=1))
    cv = np.stack([inp["c"][b].reshape(8, 128).T, inp["c_ctx"].reshape(8, 128).T], axis=2)
    m["cvec"] = f(cv)
    m["w_mod"] = f(inp["w_mod"])
    m["b_modT"] = f(inp["b_mod"].reshape(2, 48, 128).transpose(0, 2, 1))
    m["w_in"] = f(inp["w_in"])
    m["hg_gamma"] = f(inp["hg_gamma"])
    m["gla_lr_w"] = f(inp["gla_lr_w"])
    m["gla_lr_b"] = f(inp["gla_lr_b"])
    m["wa_sink"] = f(inp["wa_sink"])
    for k in ("w_branch", "w_out", "moe_w_group", "moe_b_group", "moe_w_expert", "moe_b_expert",
              "moe_w_gate", "moe_w_up", "moe_w_down"):
        m[k] = f(inp[k])
    m["ln_gT"] = f(inp["ln_g"].reshape(2, 2, 8, 128).transpose(0, 1, 3, 2))
    m["ln_bT"] = f(inp["ln_b"].reshape(2, 2, 8, 128).transpose(0, 1, 3, 2))
    consts()
    m["na_bias"] = f(inp["na_rpb"][:, :, _NAIDX["ri"], _NAIDX["ci"]])
    m.update(consts())
    return m


FUSED = 1


def build_program(layers, final):
    nc = bass.Bass("TRN2", target_bir_lowering=False)
    cx = Ctx()
    declare_inputs(cx, nc)
    if final:
        yout = Buf(nc.dram_tensor("yout", [D, TL], F32, kind="ExternalOutput"), "yout")
    else:
        yout = None
    with ExitStack() as st:
        kb = KB(nc, st)
        declare_scratch(cx, kb, debug=False, as_output=(() if final else ("XT",)))
        modT = [kb.sbuf("modT%d" % l, [128, 48, 2], F32) for l in range(2)]
        stage_mod(cx, kb, nc, modT)
        first = True
        for l in layers:
            xsrc = cx.xin if first else cx.XT
            first = False
            with kb.scope():
                lbt = [kb.sbuf("lbt%d" % i, [128, 1024], F32) for i in range(2)]
                omlt = [kb.sbuf("omlt%d" % i, [128, 1024], F32) for i in range(2)]
                stage_lb(cx, kb, nc, lbt, omlt)
                hxT = kb.sbuf("hxT", [128, 8, T], BF16)
                stage_modulate(cx, kb, nc, xsrc, modT[l], 0, 1, hxT)
                stage_inproj_fm(cx, kb, nc, l, hxT)
                stage_inproj_tm(cx, kb, nc, l, hxT, lbt[l], omlt[l])
            stage_gla_gate(cx, kb, nc, l)
            stage_scan(cx, kb, nc, "gla")
            stage_scan(cx, kb, nc, "hgrn")
            stage_wa(cx, kb, nc, l)
            stage_na(cx, kb, nc, l, bg_factory=(lambda stw, std, l=l: precast_gen(cx, kb, nc, l, stw, std)))
            stage_merge(cx, kb, nc, l, xsrc, modT[l])
            stage_moe(cx, kb, nc, l, modT[l], yout if (final and l == layers[-1]) else None, precast=False)
        if final:
            kb.finish([yout])
        else:
            kb.finish([cx.XT])
    return nc


def kernel(**inputs):
    inp = {k: np.asarray(v) for k, v in inputs.items()}
    in_maps = [core_inputs(inp, b) for b in range(4)]
    if FUSED:
        nc = build_program([0, 1], True)
        res = run_bass_kernel_spmd(nc, in_maps, core_ids=list(range(4)))
    else:
        nc0 = build_program([0], False)
        res0 = run_bass_kernel_spmd(nc0, in_maps, core_ids=list(range(4)))
        for b in range(4):
            in_maps[b]["xin"] = np.ascontiguousarray(res0.results[b]["XT"])
        nc1 = build_program([1], True)
        res = run_bass_kernel_spmd(nc1, in_maps, core_ids=list(range(4)))
    out = np.stack([np.ascontiguousarray(res.results[b]["yout"].T) for b in range(4)], axis=0)
    return out.astype(np.float32)
```

```python
import numpy as np
from contextlib import ExitStack
import concourse.bass as bass
import concourse.mybir as mybir
from concourse.bass_utils import run_bass_kernel_spmd
import os

F32 = mybir.dt.float32
BF16 = mybir.dt.bfloat16
U32 = mybir.dt.uint32
AF = mybir.ActivationFunctionType
ALU = mybir.AluOpType
AX = mybir.AxisListType


class Buf:
    __slots__ = ("t", "w", "r", "name")

    def __init__(self, t, name=""):
        self.t = t
        self.w = []
        self.r = []
        self.name = name

    def __getitem__(self, idx):
        return self.t[idx]


class Dep:
    __slots__ = ("w", "r")

    def __init__(self):
        self.w = []
        self.r = []


class Eng:
    def __init__(self, name, h, sem):
        self.name = name
        self.h = h
        self.sem = sem
        self.count = 0
        self.seen = {}


class KB:
    def __init__(self, nc, stack, n_dma_sems=8):
        self.nc = nc
        self.stack = stack
        self.sems = {}
        self.engs = {}
        for name, h in (("pe", nc.tensor), ("act", nc.scalar), ("dve", nc.vector),
                        ("pool", nc.gpsimd), ("sp", nc.sync)):
            s = stack.enter_context(nc.semaphore("s_" + name))
            self.sems[id(s)] = s
            self.engs[name] = Eng(name, h, s)
        self.dma_pool = {}
        for q in ("sp", "pool", "act"):
            lst = []
            for i in range(n_dma_sems):
                s = stack.enter_context(nc.semaphore("d_%s%d" % (q, i)))
                self.sems[id(s)] = s
                lst.append([s, 0])
            self.dma_pool[q] = [lst, 0]
        self.ninstr = 0

    def scope(self):
        kb = self

        class _Scope:
            def __enter__(self_s):
                self_s.prev = kb.stack
                self_s.st = ExitStack()
                self_s.st.__enter__()
                kb.stack = self_s.st
                kb.nscope = getattr(kb, "nscope", 0) + 1
                return self_s

            def __exit__(self_s, *a):
                if a[0] is None:
                    kb.barrier()
                kb.stack = self_s.prev
                return self_s.st.__exit__(*a)
        return _Scope()

    def sbuf(self, name, shape, dtype):
        name = "%s_s%d" % (name, getattr(self, "nscope", 0))
        t = self.stack.enter_context(self.nc.sbuf_tensor(name, list(shape), dtype))
        return Buf(t, name)

    def psum(self, name, shape, dtype=F32):
        name = "%s_s%d" % (name, getattr(self, "nscope", 0))
        t = self.stack.enter_context(self.nc.psum_tensor(name, list(shape), dtype))
        return Buf(t, name)

    def dram(self, name, shape, dtype, kind="Internal"):
        t = self.nc.dram_tensor(name, list(shape), dtype, kind=kind)
        return Buf(t, name)

    def _wait(self, e, deps):
        need = {}
        for (sid, val) in deps:
            if need.get(sid, 0) < val:
                need[sid] = val
        for sid, val in need.items():
            if e.seen.get(sid, 0) < val:
                e.h.wait_ge(self.sems[sid], val)
                e.seen[sid] = val
                self.ninstr += 1

    def _collect(self, reads, writes, acc_writes=()):
        deps = []
        for b in reads:
            deps += b.w
        for b in writes:
            deps += b.w
            deps += b.r
        for b in acc_writes:
            deps += b.r
        return deps

    @staticmethod
    def _compact(lst):
        mx = {}
        for (sid, v) in lst:
            if mx.get(sid, 0) < v:
                mx[sid] = v
        return list(mx.items())

    def _commit(self, token, reads, writes, acc_writes=()):
        for b in acc_writes:
            b.w.append(token)
            if len(b.w) > 64:
                b.w = self._compact(b.w)
        for b in reads:
            b.r.append(token)
            if len(b.r) > 64:
                mx = {}
                for (sid, v) in b.r:
                    if mx.get(sid, 0) < v:
                        mx[sid] = v
                b.r = list(mx.items())
        for b in writes:
            b.w = [token]
            b.r = []

    def _cutcheck(self):
        import os
        cut = os.environ.get("KB_CUT")
        self.nops = getattr(self, "nops", 0) + 1
        skip = os.environ.get("KB_SKIP")
        if skip and self.nops in [int(x) for x in skip.split(",")]:
            return True
        return cut is not None and self.nops > int(cut)

    def op(self, eng, fn, reads=(), writes=(), same_engine_sync=True, acc_writes=()):
        if self._cutcheck():
            return None
        e = self.engs[eng]
        deps = self._collect(reads, writes, acc_writes)
        if not same_engine_sync:
            deps = [d for d in deps if d[0] != id(e.sem)]
        self._wait(e, deps)
        ins = fn()
        e.count += 1
        ins.then_inc(e.sem, 1)
        self.ninstr += 1
        token = (id(e.sem), e.count)
        self._commit(token, reads, writes, acc_writes)
        return token

    def mm(self, fns, reads=(), writes=()):
        if self._cutcheck():
            return None
        e = self.engs["pe"]
        deps = self._collect(reads, writes)
        deps = [d for d in deps if d[0] != id(e.sem)]
        self._wait(e, deps)
        ins = None
        for fn in fns:
            ins = fn()
            self.ninstr += 1
        e.count += 1
        ins.then_inc(e.sem, 1)
        token = (id(e.sem), e.count)
        self._commit(token, reads, writes)
        return token

    def dma(self, q, out, in_, reads=(), writes=(), acc_writes=(), **kw):
        e = self.engs[q]
        pool = self.dma_pool[q]
        lst, idx = pool
        ent = lst[idx % len(lst)]
        pool[1] = idx + 1
        s, total = ent
        deps = self._collect(reads, writes, acc_writes)
        deps.append((id(s), total))
        import os
        if os.environ.get("DMA_SERIAL") and getattr(self, "_last_dma", {}).get(q):
            deps.append(self._last_dma[q])
        self._wait(e, deps)
        ins = e.h.dma_start(out=out, in_=in_, **kw)
        ins.then_inc(s, 16)
        ent[1] = total + 16
        self.ninstr += 1
        token = (id(s), total + 16)
        if not hasattr(self, "_last_dma"):
            self._last_dma = {}
        self._last_dma[q] = token
        self._commit(token, reads, writes, acc_writes)
        return token

    def init_memory(self, nbytes=196608):
        nc = self.nc
        with self.scope():
            n = nbytes // 4
            big = self.sbuf("initbig", [128, n], F32)
            q = n // 4
            self.op("dve", lambda: nc.vector.memset(big[:, 0:q], 0.0), writes=[])
            self.op("pool", lambda: nc.gpsimd.memset(big[:, q:2 * q], 0.0), writes=[])
            self.op("dve", lambda: nc.vector.memset(big[:, 2 * q:3 * q], 0.0), writes=[])
            self.op("pool", lambda: nc.gpsimd.memset(big[:, 3 * q:n], 0.0), writes=[])
            pss = [self.psum("initps%d" % i, [128, 512]) for i in range(8)]
            for p in pss:
                self.op("dve", lambda p=p: nc.vector.memset(p[:], 0.0), writes=[])

    def barrier(self):
        allt = []
        for e in self.engs.values():
            if e.count:
                allt.append((id(e.sem), e.count))
        for q, (lst, _) in self.dma_pool.items():
            for s, total in lst:
                if total:
                    allt.append((id(s), total))
        for e in self.engs.values():
            self._wait(e, allt)

    def finish(self, tokens_bufs):
        e = self.engs["sp"]
        deps = []
        for b in tokens_bufs:
            deps += b.w
        self._wait(e, deps)

T = 4352
TL = 4096

D = 1024
TL = 4096
TC = 256
T = TL + TC
NT = T // 128
BLKS = [(i * 512, 512) for i in range(8)] + [(4096, 256)]
DIN = 10528
ALPHA = (2.0 * 2) ** 0.25

COLS = dict(a_q=(0, 256), a_k=(256, 256), a_v=(512, 512), a_g=(1024, 512), a_lr=(1536, 32),
            b_q=(1568, 512), b_k=(2080, 512), b_v=(2592, 512),
            c_q=(3104, 512), c_k=(3616, 128), c_v=(3744, 128),
            d_q=(3872, 512), d_f=(4384, 1024), d_i=(5408, 512), d_g=(5920, 512), gate=(6432, 4096))


def blkcol(t0):
    return 1 if t0 >= TL else 0


class Ctx:
    pass


def declare_inputs(cx, nc):
    def din(name, shape, dt=F32):
        return Buf(nc.dram_tensor(name, list(shape), dt, kind="ExternalInput"), name)
    cx.xin = din("xin", [D, T])
    cx.cvec = din("cvec", [128, 8, 2])
    cx.w_mod = din("w_mod", [2, D, 6 * D])
    cx.b_modT = din("b_modT", [2, 128, 48])
    cx.w_in = din("w_in", [2, D, DIN])
    cx.rope_cos = din("rope_cos", [128, T])
    cx.rope_sin = din("rope_sin", [128, T])
    cx.rope_pt = din("rope_pt", [128, 128])
    cx.hg_gamma = din("hg_gamma", [2, 2, 512])
    cx.gla_lr_w = din("gla_lr_w", [2, 2, 16, 256])
    cx.gla_lr_b = din("gla_lr_b", [2, 2, 256])
    for nm in ("mg", "mh"):
        setattr(cx, nm + "_mq", din(nm + "_mq", [128, 2, 130]))
        setattr(cx, nm + "_mk", din(nm + "_mk", [128, 2, 128]))
    cx.m_ma = din("m_ma", [128, 2, 128])
    cx.m_ident = din("m_ident", [128, 128])
    cx.m_mau = din("m_mau", [128, 2, 128], U32)
    cx.m_wa = din("m_wa", [128, 2, 128])
    cx.wa_sink = din("wa_sink", [2, 8])
    cx.m_navalid = din("m_navalid", [128, 21, 128])
    cx.w_branch = din("w_branch", [2, 4, 512, 1024])
    cx.w_out = din("w_out", [2, 1024, 1024])
    cx.ln_gT = din("ln_gT", [2, 2, 128, 8])
    cx.ln_bT = din("ln_bT", [2, 2, 128, 8])
    cx.moe_w_group = din("moe_w_group", [2, 1024, 4])
    cx.moe_b_group = din("moe_b_group", [2, 4])
    cx.moe_w_expert = din("moe_w_expert", [2, 1024, 32])
    cx.moe_b_expert = din("moe_b_expert", [2, 32])
    cx.moe_w_gate = din("moe_w_gate", [2, 4, 8, 1024, 256])
    cx.moe_w_up = din("moe_w_up", [2, 4, 8, 1024, 256])
    cx.moe_w_down = din("moe_w_down", [2, 4, 8, 256, 1024])
    cx.m_sel = din("m_sel", [32, 32, 128])
    cx.m_ident32 = din("m_ident32", [128, 128])
    cx.na_bias = din("na_bias", [2, 8, 128, 21, 128])


def declare_scratch(cx, kb, debug, as_input=(), as_output=()):
    kind = "ExternalOutput" if debug else "Internal"

    def ds(name, shape, dt):
        b = kb.dram(name, shape, dt, kind=("ExternalInput" if name in as_input else ("ExternalOutput" if name in as_output else kind)))
        setattr(cx, name, b)
        return b
    ds("XT", [D, T], F32)
    ds("QA", [256, T], BF16)
    ds("GA", [512, T], BF16)
    ds("LR1", [33, T], F32)
    ds("QB", [512, T], BF16)
    ds("KB_", [512, T], BF16)
    ds("QC", [512, T], BF16)
    ds("KC", [128, T], BF16)
    ds("QD", [512, T], BF16)
    ds("GD", [512, T], BF16)
    ds("GATE", [4096, T], BF16)
    ds("KVA_t", [128, NT, 768], BF16)
    ds("VB_t", [128, NT, 512], BF16)
    ds("VC_t", [128, NT, 128], BF16)
    ds("VD_t", [128, NT, 512], BF16)
    ds("LGD_t", [128, NT, 1024], F32)
    ds("KD_t", [128, NT, 1024], BF16)
    ds("LGA_t", [128, NT, 512], F32)
    ds("MODT", [2, 128, 96], F32)
    ds("HX2", [D, T], BF16)
    ds("WGB", [32, 128, 2048], BF16)
    ds("WUB", [32, 128, 2048], BF16)
    ds("WDB", [4, 128, 16384], BF16)
    ds("WG_t", [128, NT, 32], F32)
    ds("WGT", [32, T], F32)
    ds("BR_A", [512, T], BF16)
    ds("BR_B", [512, T], BF16)
    ds("BR_C", [512, T], BF16)
    ds("BR_D", [512, T], BF16)


def stage_mod(cx, kb, nc, modT):
    with kb.scope():
        cv = kb.sbuf("cv", [128, 8, 2], F32)
        sc = kb.sbuf("sc", [128, 8, 2], F32)
        kb.dma("sp", cv[:], cx.cvec[:], reads=[cx.cvec], writes=[cv])
        kb.op("act", lambda: nc.scalar.activation(out=sc[:], in_=cv[:], func=AF.Silu), reads=[cv], writes=[sc])
        wbufs = [kb.sbuf("wm%d" % i, [128, 8, 1024], F32) for i in range(2)]
        bm = kb.sbuf("bm", [128, 48], F32)
        ps = kb.psum("ps_mod", [128, 96])
        wi = 0
        for l in range(2):
            kb.dma("sp", bm[:], cx.b_modT[l], reads=[cx.b_modT], writes=[bm])
            for g in range(6):
                wb = wbufs[wi % 2]
                wi += 1
                src = cx.w_mod[l, :, g * 1024:(g + 1) * 1024].rearrange("(kc p) c -> p kc c", p=128)
                kb.dma("sp", wb[:], src, reads=[cx.w_mod], writes=[wb])
                for j in range(8):
                    jj = g * 8 + j
                    fns = []
                    for kc in range(8):
                        fns.append(lambda kc=kc, j=j, jj=jj, wb=wb: nc.tensor.matmul(
                            ps[:, 2 * jj:2 * jj + 2], lhsT=wb[:, kc, j * 128:(j + 1) * 128], rhs=sc[:, kc, :],
                            start=(kc == 0), stop=(kc == 7)))
                    kb.mm(fns, reads=[wb, sc], writes=[ps])
            m = modT[l]
            kb.op("dve", lambda m=m: nc.vector.tensor_tensor(
                out=m[:], in0=ps[:].rearrange("p (j c) -> p j c", c=2),
                in1=bm[:].unsqueeze(2).broadcast_to([128, 48, 2]), op=ALU.add),
                reads=[ps, bm], writes=[m])
            kb.dma("sp", cx.MODT[l], m[:].rearrange("p j c -> p (j c)"), reads=[m], writes=[cx.MODT])


def stage_modulate(cx, kb, nc, src_dram, modv, shift_idx, scale_idx, hxT, hx32=None):
    with kb.scope():
        onep = kb.sbuf("onep", [128, 8, 2], F32)
        kb.op("dve", lambda: nc.vector.tensor_scalar(
            out=onep[:], in0=modv[:, scale_idx * 8:(scale_idx + 1) * 8, :], scalar1=1.0, scalar2=None, op0=ALU.add),
            reads=[modv], writes=[onep])
        xb = [kb.sbuf("xb%d" % i, [128, 8, 512], F32) for i in range(2)]
        for bi, (t0, nb) in enumerate(BLKS):
            col = blkcol(t0)
            x = xb[bi % 2]
            kb.dma("sp", x[:, :, :nb], src_dram[:, t0:t0 + nb].rearrange("(kc p) t -> p kc t", p=128),
                   reads=[src_dram], writes=[x])
            for kc in range(8):
                eng = "dve" if kc % 2 == 0 else "pool"
                h = nc.vector if eng == "dve" else nc.gpsimd
                kb.op(eng, lambda h=h, kc=kc, x=x: h.tensor_scalar(
                    out=hxT[:, kc, t0:t0 + nb], in0=x[:, kc, :nb],
                    scalar1=onep[:, kc, col:col + 1], scalar2=modv[:, shift_idx * 8 + kc, col:col + 1],
                    op0=ALU.mult, op1=ALU.add), reads=[x, onep, modv], writes=[hxT])


FM_GROUPS = [
    ("a_q", "QA", "copy"), ("a_g", "GA", "silu"), ("a_lr", "LR1", "copy32"),
    ("b_q", "QB", "copy"), ("b_k", "KB_", "copy"),
    ("c_q", "QC", "rope"), ("c_k", "KC", "rope"),
    ("d_q", "QD", "silu"), ("d_g", "GD", "silu"), ("gate", "GATE", "sigmoid"),
]


def stage_inproj_fm(cx, kb, nc, l, hxT, only=None):
    with kb.scope():
        wsl = [kb.sbuf("wsl%d" % i, [128, 8, 512], BF16) for i in range(2)]
        stg = [kb.sbuf("stg%d" % i, [128, T], BF16) for i in range(2)]
        stg32 = kb.sbuf("stg32", [32, T], F32)
        pss = [kb.psum("psf%d" % i, [128, 512]) for i in range(4)]
        psr = [kb.psum("psr%d" % i, [128, 512]) for i in range(2)]
        cosT = kb.sbuf("cosT", [128, T], F32)
        sinT = kb.sbuf("sinT", [128, T], F32)
        ptm = kb.sbuf("ptm", [128, 128], BF16)
        tmpb = [kb.sbuf("tmpb%d" % i, [128, 512], BF16) for i in range(2)]
        t1 = [kb.sbuf("t1_%d" % i, [128, 512], F32) for i in range(2)]
        t2 = [kb.sbuf("t2_%d" % i, [128, 512], F32) for i in range(2)]
        ones = kb.sbuf("ones1", [1, T], F32)
        kb.dma("sp", cosT[:], cx.rope_cos[:], reads=[cx.rope_cos], writes=[cosT])
        kb.dma("sp", sinT[:], cx.rope_sin[:], reads=[cx.rope_sin], writes=[sinT])
        kb.dma("pool", ptm[:], cx.rope_pt[:], reads=[cx.rope_pt], writes=[ptm])
        kb.op("dve", lambda: nc.vector.memset(ones[:], 1.0), writes=[ones])
        kb.dma("sp", cx.LR1[32:33, :], ones[:], reads=[ones], writes=[], acc_writes=[cx.LR1])
        nsl = 0
        nps = 0
        nst = 0
        nrp = 0
        for (name, dest, kind) in FM_GROUPS:
            if only is not None and name not in only:
                continue
            c0, nc_ = COLS[name]
            dbuf = getattr(cx, dest)
            for s0 in range(0, nc_, 512):
                sw = min(512, nc_ - s0)
                w = wsl[nsl % 2]
                nsl += 1
                kb.dma("pool", w[:, :, :sw],
                       cx.w_in[l, :, c0 + s0:c0 + s0 + sw].rearrange("(kc p) c -> p kc c", p=128),
                       reads=[cx.w_in], writes=[w])
                for m0 in range(0, sw, 128):
                    mw = min(128, sw - m0)
                    if kind == "copy32":
                        st = stg32
                    else:
                        st = stg[nst % 2]
                        nst += 1
                    for bi, (t0, nb) in enumerate(BLKS):
                        ps = pss[nps % 4]
                        nps += 1
                        fns = [(lambda kc=kc, ps=ps, w=w: nc.tensor.matmul(
                            ps[:mw, :nb], lhsT=w[:, kc, m0:m0 + mw], rhs=hxT[:, kc, t0:t0 + nb],
                            start=(kc == 0), stop=(kc == 7))) for kc in range(8)]
                        kb.mm(fns, reads=[w, hxT], writes=[ps])
                        o = st[:mw, t0:t0 + nb]
                        if kind in ("copy", "copy32"):
                            if nps % 2 == 0:
                                kb.op("dve", lambda ps=ps, o=o: nc.vector.tensor_copy(out=o, in_=ps[:mw, :nb]),
                                      reads=[ps], writes=[st])
                            else:
                                kb.op("act", lambda ps=ps, o=o: nc.scalar.copy(out=o, in_=ps[:mw, :nb]),
                                      reads=[ps], writes=[st])
                        elif kind == "silu":
                            kb.op("act", lambda ps=ps, o=o: nc.scalar.activation(out=o, in_=ps[:mw, :nb], func=AF.Silu),
                                  reads=[ps], writes=[st])
                        elif kind == "sigmoid":
                            kb.op("act", lambda ps=ps, o=o: nc.scalar.activation(out=o, in_=ps[:mw, :nb], func=AF.Sigmoid),
                                  reads=[ps], writes=[st])
                        elif kind == "rope":
                            tb = tmpb[nrp % 2]
                            a1 = t1[nrp % 2]
                            a2 = t2[nrp % 2]
                            pr = psr[nrp % 2]
                            nrp += 1
                            kb.op("act", lambda ps=ps, tb=tb: nc.scalar.copy(out=tb[:mw, :nb], in_=ps[:mw, :nb]),
                                  reads=[ps], writes=[tb])
                            kb.mm([lambda pr=pr, tb=tb: nc.tensor.matmul(pr[:mw, :nb], lhsT=ptm[:mw, :mw], rhs=tb[:mw, :nb],
                                                                         start=True, stop=True)],
                                  reads=[ptm, tb], writes=[pr])
                            kb.op("dve", lambda tb=tb, a1=a1: nc.vector.tensor_tensor(
                                out=a1[:mw, :nb], in0=tb[:mw, :nb], in1=cosT[:mw, t0:t0 + nb], op=ALU.mult),
                                reads=[tb, cosT], writes=[a1])
                            kb.op("dve", lambda pr=pr, a2=a2: nc.vector.tensor_tensor(
                                out=a2[:mw, :nb], in0=pr[:mw, :nb], in1=sinT[:mw, t0:t0 + nb], op=ALU.mult),
                                reads=[pr, sinT], writes=[a2])
                            kb.op("pool", lambda a1=a1, a2=a2, o=o: nc.gpsimd.tensor_tensor(
                                out=o, in0=a1[:mw, :nb], in1=a2[:mw, :nb], op=ALU.add),
                                reads=[a1, a2], writes=[st])
                    kb.dma("sp", dbuf[s0 + m0:s0 + m0 + mw, :], st[:mw, :], reads=[st], writes=[], acc_writes=[dbuf])


TM_GROUPS = [
    (256, 512, "KVA_t", 0, "copy"), (768, 256, "KVA_t", 512, "copy"),
    (2592, 512, "VB_t", 0, "copy"), (3744, 128, "VC_t", 0, "copy"),
    (5408, 512, "VD_t", 0, "copy"),
    (4384, 512, None, 0, "hgf"), (4896, 512, None, 512, "hgf"),
]


def stage_inproj_tm(cx, kb, nc, l, hxT, lbt, omlt, only=None):
    with kb.scope():
        wsl = [kb.sbuf("wtl%d" % i, [128, 8, 512], BF16) for i in range(2)]
        pss = [kb.psum("pst%d" % i, [128, 512]) for i in range(4)]
        stb = [kb.sbuf("stb%d" % i, [128, 512], BF16) for i in range(4)]
        e1 = [kb.sbuf("e1_%d" % i, [128, 512], F32) for i in range(2)]
        r1 = [kb.sbuf("r1_%d" % i, [128, 512], F32) for i in range(2)]
        lg = [kb.sbuf("lg_%d" % i, [128, 512], F32) for i in range(2)]
        kk = [kb.sbuf("kk_%d" % i, [128, 512], BF16) for i in range(2)]
        nsl = nps = nst = nh = 0
        for (c0, ncol, dest, doff, kind) in TM_GROUPS:
            if only is not None and kind not in only:
                continue
            w = wsl[nsl % 2]
            nsl += 1
            kb.dma("pool", w[:, :, :ncol], cx.w_in[l, :, c0:c0 + ncol].rearrange("(kc p) c -> p kc c", p=128),
                   reads=[cx.w_in], writes=[w])
            for ti in range(NT):
                ts = slice(ti * 128, (ti + 1) * 128)
                ps = pss[nps % 4]
                nps += 1
                fns = [(lambda kc=kc, ps=ps, w=w: nc.tensor.matmul(
                    ps[:, :ncol], lhsT=hxT[:, kc, ts], rhs=w[:, kc, :ncol],
                    start=(kc == 0), stop=(kc == 7))) for kc in range(8)]
                kb.mm(fns, reads=[w, hxT], writes=[ps])
                if kind == "copy":
                    st = stb[nst % 4]
                    nst += 1
                    if nst % 2 == 0:
                        kb.op("dve", lambda ps=ps, st=st: nc.vector.tensor_copy(out=st[:, :ncol], in_=ps[:, :ncol]),
                              reads=[ps], writes=[st])
                    else:
                        kb.op("act", lambda ps=ps, st=st: nc.scalar.copy(out=st[:, :ncol], in_=ps[:, :ncol]),
                              reads=[ps], writes=[st])
                    db = getattr(cx, dest)
                    kb.dma("sp", db[:, ti, doff:doff + ncol], st[:, :ncol], reads=[st], writes=[], acc_writes=[db])
                else:
                    e = e1[nh % 2]
                    r = r1[nh % 2]
                    g = lg[nh % 2]
                    k = kk[nh % 2]
                    nh += 1
                    cs = slice(doff, doff + 512)
                    kb.op("act", lambda ps=ps, r=r: nc.scalar.activation(out=r[:], in_=ps[:], func=AF.Sigmoid),
                          reads=[ps], writes=[r])
                    kb.op("dve", lambda r=r: nc.vector.tensor_tensor(out=r[:], in0=r[:], in1=omlt[:, cs], op=ALU.mult),
                          reads=[r, omlt], writes=[r])
                    kb.op("pool", lambda r=r: nc.gpsimd.tensor_tensor(out=r[:], in0=r[:], in1=lbt[:, cs], op=ALU.add),
                          reads=[r, lbt], writes=[r])
                    kb.op("act", lambda r=r, g=g: nc.scalar.activation(out=g[:], in_=r[:], func=AF.Ln),
                          reads=[r], writes=[g])
                    kb.op("pool", lambda r=r, k=k: nc.gpsimd.tensor_scalar(
                        out=k[:], in0=r[:], scalar1=-1.0, scalar2=1.0, op0=ALU.mult, op1=ALU.add),
                        reads=[r], writes=[k])
                    kb.dma("sp", cx.LGD_t[:, ti, cs], g[:], reads=[g], writes=[], acc_writes=[cx.LGD_t])
                    kb.dma("sp", cx.KD_t[:, ti, cs], k[:], reads=[k], writes=[], acc_writes=[cx.KD_t])


def stage_lb(cx, kb, nc, lbt, omlt):
    with kb.scope():
        gam = kb.sbuf("gam", [128, 2, 2, 512], F32)
        kb.dma("sp", gam[:].rearrange("p a b c -> p (a b c)"),
               cx.hg_gamma[:].rearrange("a b c -> (a b c)").partition_broadcast(128),
               reads=[cx.hg_gamma], writes=[gam])
        d = kb.sbuf("gamd", [128, 2, 512], F32)
        kb.op("dve", lambda: nc.vector.tensor_tensor(out=d[:], in0=gam[:, :, 0, :], in1=gam[:, :, 1, :], op=ALU.subtract),
              reads=[gam], writes=[d])
        kb.op("act", lambda: nc.scalar.activation(out=d[:], in_=d[:], func=AF.Exp), reads=[d], writes=[d])
        l1 = lbt[1]
        kb.op("dve", lambda: nc.vector.tensor_scalar(
            out=d[:], in0=d[:], scalar1=1.0, scalar2=None, op0=ALU.add), reads=[d], writes=[d])
        kb.op("dve", lambda: nc.vector.reciprocal(out=l1[:], in_=d[:].rearrange("p a c -> p (a c)")),
              reads=[d], writes=[l1])
        l0 = lbt[0]
        kb.op("dve", lambda: nc.vector.memset(l0[:], 0.0), writes=[l0])
        for l in range(2):
            o = omlt[l]
            lb = lbt[l]
            kb.op("dve", lambda o=o, lb=lb: nc.vector.tensor_scalar(
                out=o[:], in0=lb[:], scalar1=-1.0, scalar2=1.0, op0=ALU.mult, op1=ALU.add),
                reads=[lb], writes=[o])


def stage_gla_gate(cx, kb, nc, l):
    with kb.scope():
        wlr = kb.sbuf("wlr", [33, 512], F32)
        lr1 = kb.sbuf("lr1", [33, T], F32)
        kb.op("dve", lambda: nc.vector.memset(wlr[:], 0.0), writes=[wlr])
        kb.dma("sp", wlr[0:16, 0:256], cx.gla_lr_w[l, 0], reads=[cx.gla_lr_w], writes=[wlr])
        kb.dma("sp", wlr[16:32, 256:512], cx.gla_lr_w[l, 1], reads=[cx.gla_lr_w], writes=[wlr])
        kb.dma("sp", wlr[32:33, :], cx.gla_lr_b[l].rearrange("a c -> (a c)").unsqueeze(0),
               reads=[cx.gla_lr_b], writes=[wlr])
        kb.dma("sp", lr1[:], cx.LR1[:], reads=[cx.LR1], writes=[lr1])
        pss = [kb.psum("psg%d" % i, [128, 512]) for i in range(2)]
        ee = [kb.sbuf("ge%d" % i, [128, 512], F32) for i in range(2)]
        gg = [kb.sbuf("gg%d" % i, [128, 512], F32) for i in range(2)]
        for ti in range(NT):
            ts = slice(ti * 128, (ti + 1) * 128)
            ps = pss[ti % 2]
            e = ee[ti % 2]
            g = gg[ti % 2]
            kb.mm([lambda ps=ps: nc.tensor.matmul(ps[:], lhsT=lr1[:, ts], rhs=wlr[:], start=True, stop=True)],
                  reads=[lr1, wlr], writes=[ps])
            kb.op("act", lambda ps=ps, e=e: nc.scalar.activation(out=e[:], in_=ps[:], func=AF.Exp, scale=-1.0),
                  reads=[ps], writes=[e])
            kb.op("act", lambda e=e, g=g: nc.scalar.activation(out=g[:], in_=e[:], func=AF.Ln, bias=1.0),
                  reads=[e], writes=[g])
            kb.dma("sp", cx.LGA_t[:, ti, :], g[:], reads=[g], writes=[], acc_writes=[cx.LGA_t])

SCAN_CFG = {
    "gla": dict(H=4, dk=64, qsrc="QA", qscale=0.125, ksrc="KVA_t", kz=0, kcol0=0, vsrc="KVA_t", vcol0=256,
                gsrc="LGA_t", gz=256, gate="GA", dest="BR_A", mask="mg"),
    "hgrn": dict(H=4, dk=128, qsrc="QD", qscale=1.0, ksrc="KD_t", kz=512, kcol0=0, vsrc="VD_t", vcol0=0,
                 gsrc="LGD_t", gz=512, gate="GD", dest="BR_D", mask="mh"),
}


def view(b):
    return Buf(b.t, b.name + "_v")


def stage_scan(cx, kb, nc, which, heads=None):
    cfg = SCAN_CFG[which]
    H, dk = cfg["H"], cfg["dk"]
    dv = 128
    qsrc, ksrc, vsrc, gsrc = (getattr(cx, cfg[k]) for k in ("qsrc", "ksrc", "vsrc", "gsrc"))
    gate, dest = getattr(cx, cfg["gate"]), getattr(cx, cfg["dest"])
    mq_d, mk_d = getattr(cx, cfg["mask"] + "_mq"), getattr(cx, cfg["mask"] + "_mk")
    with kb.scope():
        mq = kb.sbuf("mq", [128, 2, 130], F32)
        mk = kb.sbuf("mk", [128, 2, 128], F32)
        ma = kb.sbuf("ma", [128, 2, 128], F32)
        ident = kb.sbuf("ident", [128, 128], BF16)
        onesdv = kb.sbuf("onesdv", [128, 128], F32)
        kb.dma("sp", mq[:], mq_d[:], reads=[mq_d], writes=[mq])
        kb.dma("sp", mk[:], mk_d[:], reads=[mk_d], writes=[mk])
        kb.dma("sp", ma[:], cx.m_ma[:], reads=[cx.m_ma], writes=[ma])
        mau = kb.sbuf("mau", [128, 2, 128], U32)
        kb.dma("sp", mau[:], cx.m_mau[:], reads=[cx.m_mau], writes=[mau])
        kb.dma("pool", ident[:], cx.m_ident[:], reads=[cx.m_ident], writes=[ident])
        kb.op("dve", lambda: nc.vector.memset(onesdv[:], 1.0 / dv), writes=[onesdv])
        vt = kb.sbuf("vt", [128, NT, dv], BF16)
        qT = kb.sbuf("qT", [128, T], BF16)
        gsil = kb.sbuf("gsil", [128, T], BF16)
        obr = kb.sbuf("obr", [128, T], BF16)
        gts = [kb.sbuf("gt%d" % i, [128, NT, dk], F32) for i in range(2)]
        kts = [kb.sbuf("kt%d" % i, [128, NT, dk], BF16) for i in range(2)]
        qt = [kb.sbuf("qt%d" % z, [128, T], BF16) for z in range(2)]
        atm = [kb.sbuf("atm%d" % z, [128, NT, 128], BF16) for z in range(2)]
        Sp = [kb.sbuf("Sp%d" % z, [128, 2 * NT, dv], BF16) for z in range(2)]
        for z in range(2):
            kb.op("pool", lambda z=z: nc.gpsimd.memset(atm[z][:], 0.0), writes=[atm[z]])
        Sst = [kb.sbuf("Sst%d" % i, [128, dv], F32) for i in range(2)]
        R = 3
        eq = [kb.sbuf("eq%d" % i, [128, 128], F32) for i in range(R)]
        ek = [kb.sbuf("ek%d" % i, [128, 128], F32) for i in range(R)]
        ekh = [kb.sbuf("ekh%d" % i, [128, 128], F32) for i in range(R)]
        ktl = [kb.sbuf("ktl%d" % i, [128, 128], BF16) for i in range(R)]
        khat = [kb.sbuf("khat%d" % i, [128, 256], BF16) for i in range(R)]
        ktr = [kb.sbuf("ktr%d" % i, [128, 128], F32) for i in range(R)]
        atr = [kb.sbuf("atr%d" % i, [128, 128], F32) for i in range(R)]
        usb = [kb.sbuf("usb%d" % i, [128, 256], F32) for i in range(R)]
        osb = [kb.sbuf("osb%d" % i, [128, 512], F32) for i in range(2)]
        cm = kb.sbuf("cm", [128, 2], F32)
        kb.op("dve", lambda: nc.vector.memset(cm[:], 0.0), writes=[cm])
        kb.op("dve", lambda: nc.vector.memset(cm[0:64, 0:1], 1.0), writes=[cm])
        kb.op("dve", lambda: nc.vector.memset(cm[64:128, 1:2], 1.0), writes=[cm])
        er2 = [kb.sbuf("er%d" % i, [128, 2], F32) for i in range(R)]
        ed2 = [kb.sbuf("ed%d" % i, [128, 2], F32) for i in range(R)]
        psA = [kb.psum("psA%d" % i, [128, 512]) for i in range(2)]
        psB = [kb.psum("psB%d" % i, [128, 512]) for i in range(2)]
        psA_ec = [view(p) for p in psA]
        import os
        if True:
            psK = [kb.psum("psK%d" % i, [128, 512]) for i in range(2)]
        else:
            psK = psA
        psA_kt = [view(p) for p in psK]
        psA_e2 = [view(p) for p in psK]
        psB_u = [view(p) for p in psB]
        psB_at = [view(p) for p in psB]
        psO = [kb.psum("psO%d" % i, [128, 512]) for i in range(1)] * 2
        psM = [kb.psum("psM%d" % i, [128, 512]) for i in range(1)] * 2
        sq = [kb.sbuf("sq%d" % i, [128, 512], F32) for i in range(2)]
        sd = [kb.sbuf("sd%d" % i, [128, 512], F32) for i in range(2)]
        on = [kb.sbuf("on%d" % i, [128, 512], F32) for i in range(2)]
        eps_t = kb.sbuf("eps_t", [128, 1], F32)
        kb.op("dve", lambda: nc.vector.memset(eps_t[:], 1e-6), writes=[eps_t])

        it = 0
        ghz = 0
        import os
        for h in (range(H) if heads is None else heads):
            vc0 = cfg["vcol0"] + h * dv
            kb.dma("sp", vt[:], vsrc[:, :, vc0:vc0 + dv], reads=[vsrc], writes=[vt])
            kb.dma("sp", qT[:dk, :], qsrc[h * dk:(h + 1) * dk, :], reads=[qsrc], writes=[qT])
            kb.dma("sp", gsil[:], gate[h * dv:(h + 1) * dv, :], reads=[gate], writes=[gsil])
            for z in range(2):
                gt = gts[ghz % 2]
                kt = kts[ghz % 2]
                ghz += 1
                gc0 = z * cfg["gz"] + h * dk
                kc0 = cfg["kcol0"] + z * cfg["kz"] + h * dk
                kb.dma("sp", gt[:], gsrc[:, :, gc0:gc0 + dk], reads=[gsrc], writes=[gt])
                if os.environ.get("KTMEMSET"):
                    kb.op("dve", lambda kt=kt: nc.vector.memset(kt[:], 0.0), writes=[kt])
                kb.dma("sp", kt[:], ksrc[:, :, kc0:kc0 + dk], reads=[ksrc], writes=[kt])
                order = [32, 33] + list(range(32)) if z == 0 else [33, 32] + list(range(31, -1, -1))
                import os
                DBG = os.environ.get("SCAN_DBG", "")
                if DBG == "loads":
                    continue
                if DBG.startswith("nt"):
                    order = order[:int(DBG[2:])]
                S = Sst[0]
                kb.op("dve", lambda S=S: nc.vector.memset(S[:], 0.0), writes=[S])
                si = 0
                def p1(ti, a, r):
                    ts = slice(ti * 128, (ti + 1) * 128)
                    pa, pb = psA[a], psB[a]
                    pk = psK[a]
                    kb.mm([lambda pa=pa, gt=gt: nc.tensor.matmul(pa[:dk, 0:130], lhsT=gt[:, ti, :], rhs=mq[:, z, :],
                                                                 start=True, stop=True)],
                          reads=[gt, mq], writes=[psA_ec[a]])
                    kb.mm([lambda pk=pk, kt=kt: nc.tensor.matmul(pk[:dk, 256:384], lhsT=(ident[:, :dk] if os.environ.get("A2ID") else kt[:, ti, :]), rhs=ident[:],
                                                                 start=True, stop=True)],
                          reads=[kt, ident], writes=[psA_kt[a]])
                    kb.mm([lambda pk=pk, gt=gt: nc.tensor.matmul(pk[:, 384:384 + dk], lhsT=mk[:, z, :], rhs=gt[:, ti, :],
                                                                 start=True, stop=True)],
                          reads=[gt, mk], writes=[psA_e2[a]])
                    kb.op("act", lambda pa=pa, o=eq[r]: nc.scalar.activation(out=o[:dk, :], in_=pa[:dk, 0:128], func=AF.Exp),
                          reads=[psA_ec[a]], writes=[eq[r]])
                    kb.op("act", lambda pa=pa, o=ek[r]: nc.scalar.activation(out=o[:dk, :], in_=pa[:dk, 0:128], func=AF.Exp, scale=-1.0),
                          reads=[psA_ec[a]], writes=[ek[r]])
                    kb.op("act", lambda pa=pa, o=er2[r]: nc.scalar.activation(out=o[:dk, :], in_=pa[:dk, 128:130], func=AF.Exp, scale=0.5),
                          reads=[psA_ec[a]], writes=[er2[r]])
                    kb.op("act", lambda pa=pa, o=ed2[r]: nc.scalar.activation(out=o[:dk, :], in_=pa[:dk, 128:130], func=AF.Exp),
                          reads=[psA_ec[a]], writes=[ed2[r]])
                    kb.op("act", lambda pk=pk, o=ekh[r]: nc.scalar.activation(out=o[:, :dk], in_=pk[:, 384:384 + dk], func=AF.Exp),
                          reads=[psA_e2[a]], writes=[ekh[r]])
                    qz = qt[z]
                    kb.op("dve", lambda qz=qz, e=eq[r]: nc.vector.scalar_tensor_tensor(
                        out=qz[:dk, ts], in0=qT[:dk, ts], scalar=cfg["qscale"], in1=e[:dk, :], op0=ALU.mult, op1=ALU.mult),
                        reads=[qT, eq[r]], writes=[], acc_writes=[qz])
                    kb.op("act", lambda pk=pk, o=ktr[r]: nc.scalar.copy(out=o[:dk, :], in_=pk[:dk, 256:384]),
                          reads=[psA_kt[a]], writes=[ktr[r]])
                    kb.op("dve", lambda e=ek[r], o=ktl[r], kr=ktr[r]: nc.vector.tensor_tensor(
                        out=o[:dk, :], in0=kr[:dk, :], in1=e[:dk, :], op=ALU.mult),
                        reads=[ktr[r], ek[r]], writes=[ktl[r]])
                    for c in range(2):
                        kb.op("dve", lambda kt=kt, e=ekh[r], o=khat[r], c=c: nc.vector.scalar_tensor_tensor(
                            out=o[:, c * 128:c * 128 + dk], in0=kt[:, ti, :], scalar=cm[:, c:c + 1], in1=e[:, :dk],
                            op0=ALU.mult, op1=ALU.mult),
                            reads=[kt, ekh[r], cm], writes=[], acc_writes=[khat[r]])
                    return None

                def p2(ti, a, r, S, si):
                    ts = slice(ti * 128, (ti + 1) * 128)
                    pa, pb = psA[a], psB[a]
                    qz = qt[z]
                    kb.mm([(lambda c=c, pb=pb, kh=khat[r]: nc.tensor.matmul(
                        pb[:dk, c * 128:(c + 1) * 128], lhsT=kh[:, c * 128:c * 128 + dk],
                        rhs=vt[:, ti, :], start=True, stop=True)) for c in range(2)],
                        reads=[khat[r], vt], writes=[psB_u[a]])
                    kb.mm([lambda pb=pb, k_=ktl[r], qz=qz: nc.tensor.matmul(
                        pb[:, 256:384], lhsT=k_[:dk, :], rhs=qz[:dk, ts], start=True, stop=True)],
                        reads=[ktl[r], qz], writes=[psB_at[a]])
                    az = atm[z]
                    kb.op("act", lambda pb=pb, o=atr[r]: nc.scalar.copy(out=o[:], in_=pb[:, 256:384]),
                          reads=[psB_at[a]], writes=[atr[r]])
                    kb.op("dve", lambda az=az, o=atr[r]: nc.vector.copy_predicated(
                        out=az[:, ti, :], mask=mau[:, z, :], data=o[:]),
                        reads=[atr[r], mau], writes=[], acc_writes=[az])
                    kb.op("act", lambda pb=pb, o=usb[r]: nc.scalar.copy(out=o[:dk, :], in_=pb[:dk, 0:256]),
                          reads=[psB_u[a]], writes=[usb[r]])
                    spz = Sp[z]
                    for c in ((0, 1) if z == 0 else (1, 0)):
                        ci = 2 * ti + c
                        kb.op("act", lambda S=S, spz=spz, ci=ci, e=er2[r], c=c: nc.scalar.activation(
                            out=spz[:dk, ci, :], in_=S[:dk, :], func=AF.Copy, scale=e[:dk, c:c + 1]),
                            reads=[S, er2[r]], writes=[], acc_writes=[spz])
                        S2 = Sst[(si + 1) % 2]
                        si += 1
                        kb.op("dve", lambda S=S, S2=S2, e=ed2[r], u=usb[r], c=c: nc.vector.scalar_tensor_tensor(
                            out=S2[:dk, :], in0=S[:dk, :], scalar=e[:dk, c:c + 1], in1=u[:dk, c * 128:(c + 1) * 128],
                            op0=ALU.mult, op1=ALU.add),
                            reads=[S, ed2[r], usb[r]], writes=[S2])
                        S = S2
                    return S, si

                sched = []
                for ti in order:
                    sched.append((ti, it % 2, it % R))
                    it += 1
                for idx in range(len(sched) + 1):
                    if idx < len(sched):
                        p1(*sched[idx])
                    if idx >= 1:
                        S, si = p2(*sched[idx - 1], S, si)
            groups = [list(range(g * 4, g * 4 + 4)) for g in range(8)] + [[32, 33]]
            if DBG:
                groups = []
            for gi, tiles in enumerate(groups):
                po = psO[gi % 2]
                pm = psM[gi % 2]
                nb = len(tiles) * 128
                t0 = tiles[0] * 128
                fns = []
                for k_, ti in enumerate(tiles):
                    cs = k_ * 128
                    for z in range(2):
                        fns.append(lambda po=po, ti=ti, cs=cs, z=z: nc.tensor.matmul(
                            po[:, cs:cs + 128], lhsT=vt[:, ti, :], rhs=atm[z][:, ti, :], start=(z == 0), stop=False))
                        for c in range(2):
                            fns.append(lambda po=po, ti=ti, cs=cs, z=z, c=c: nc.tensor.matmul(
                                po[:, cs + c * 64:cs + (c + 1) * 64], lhsT=Sp[z][:dk, 2 * ti + c, :],
                                rhs=qt[z][:dk, ti * 128 + c * 64:ti * 128 + (c + 1) * 64],
                                start=False, stop=(z == 1)))
                kb.mm(fns, reads=[vt, atm[0], atm[1], Sp[0], Sp[1], qt[0], qt[1]], writes=[po])
                s_, d_, o_ = sq[gi % 2], sd[gi % 2], on[gi % 2]
                kb.op("act", lambda po=po, s_=s_: nc.scalar.activation(out=s_[:, :nb], in_=po[:, :nb], func=AF.Square),
                      reads=[po], writes=[s_])
                kb.mm([lambda pm=pm, s_=s_: nc.tensor.matmul(pm[:, :nb], lhsT=onesdv[:], rhs=s_[:, :nb], start=True, stop=True)],
                      reads=[onesdv, s_], writes=[pm])
                kb.op("act", lambda pm=pm, d_=d_: nc.scalar.activation(out=d_[:, :nb], in_=pm[:, :nb], func=AF.Sqrt, bias=eps_t[:]),
                      reads=[pm, eps_t], writes=[d_])
                kb.op("dve", lambda d_=d_: nc.vector.reciprocal(out=d_[:, :nb], in_=d_[:, :nb]), reads=[d_], writes=[d_])
                ob_ = osb[gi % 2]
                kb.op("act", lambda po=po, ob_=ob_: nc.scalar.copy(out=ob_[:, :nb], in_=po[:, :nb]), reads=[po], writes=[ob_])
                kb.op("dve", lambda ob_=ob_, d_=d_, o_=o_: nc.vector.tensor_tensor(
                    out=o_[:, :nb], in0=ob_[:, :nb], in1=d_[:, :nb], op=ALU.mult), reads=[ob_, d_], writes=[o_])
                kb.op("pool", lambda o_=o_: nc.gpsimd.tensor_tensor(
                    out=obr[:, t0:t0 + nb], in0=o_[:, :nb], in1=gsil[:, t0:t0 + nb], op=ALU.mult),
                    reads=[o_, gsil], writes=[], acc_writes=[obr])
            kb.dma("sp", dest[h * dv:(h + 1) * dv, :], obr[:], reads=[obr], writes=[], acc_writes=[dest])


def scan_masks():
    j = np.arange(128)[:, None]
    i = np.arange(128)[None, :]
    same = (j // 64 == i // 64).astype(np.float32)
    out = {}
    for name, gs in (("mg", -1.0 / 16.0), ("mh", 1.0)):
        mq = np.zeros((128, 2, 130), np.float32)
        mk = np.zeros((128, 2, 128), np.float32)
        mq[:, 0, :128] = gs * same * ((j <= i).astype(np.float32) - 0.5)
        mq[:, 1, :128] = gs * same * ((j >= i).astype(np.float32) - 0.5)
        for z in range(2):
            for c in range(2):
                mq[:, z, 128 + c] = gs * (np.arange(128) // 64 == c)
        mk[:, 0, :] = gs * same * (j > i)
        mk[:, 1, :] = gs * same * (j < i)
        out[name + "_mq"] = mq
        out[name + "_mk"] = mk
    ma = np.zeros((128, 2, 128), np.float32)
    ma[:, 0, :] = same * (j <= i)
    ma[:, 1, :] = same * (j >= i)
    out["m_ma"] = ma
    out["m_mau"] = ma.astype(np.uint32)
    out["m_ident"] = np.eye(128, dtype=np.float32)
    return out


def stage_wa(cx, kb, nc, l, groups=None):
    with kb.scope():
        wm = kb.sbuf("wm", [128, 2, 128], BF16)
        kb.dma("pool", wm[:], cx.m_wa[:], reads=[cx.m_wa], writes=[wm])
        ones64 = kb.sbuf("ones64", [128, 64], BF16)
        kb.op("dve", lambda: nc.vector.memset(ones64[:], 1.0), writes=[ones64])
        sk = kb.sbuf("sk", [64, 8], F32)
        kb.dma("sp", sk[:], cx.wa_sink[l].partition_broadcast(64), reads=[cx.wa_sink], writes=[sk])
        kb.op("act", lambda: nc.scalar.activation(out=sk[:], in_=sk[:], func=AF.Exp), reads=[sk], writes=[sk])
        q4 = kb.sbuf("q4", [64, 4, T], BF16)
        kT = kb.sbuf("kTc", [64, T], BF16)
        vt = kb.sbuf("vtc", [128, NT, 64], BF16)
        ost = kb.sbuf("ost", [64, 4, T], BF16)
        pS = [kb.psum("pS%d" % i, [128, 512]) for i in range(4)]
        pO = [kb.psum("pO%d" % i, [128, 512]) for i in range(2)]
        pD = [kb.psum("pD%d" % i, [128, 512]) for i in range(2)]
        R = 10
        pt = [kb.sbuf("pt%d" % i, [128, 4, 128], BF16) for i in range(R)]
        ptm = [kb.sbuf("ptm%d" % i, [128, 4, 128], BF16) for i in range(R)]
        osb = [kb.sbuf("osbw%d" % i, [64, 4, 128], F32) for i in range(2)]
        dsb = [kb.sbuf("dsbw%d" % i, [64, 4, 128], F32) for i in range(2)]
        it = 0
        for g in (range(2) if groups is None else groups):
            kb.dma("sp", q4[:], cx.QC[g * 256:(g + 1) * 256, :].rearrange("(hh d) t -> d hh t", d=64),
                   reads=[cx.QC], writes=[q4])
            kb.dma("sp", kT[:], cx.KC[g * 64:(g + 1) * 64, :], reads=[cx.KC], writes=[kT])
            kb.dma("sp", vt[:], cx.VC_t[:, :, g * 64:(g + 1) * 64], reads=[cx.VC_t], writes=[vt])
            def p1(qb):
                nonlocal it
                if qb < 32:
                    keys = []
                    if qb > 0:
                        keys.append((qb - 1, 0))
                    keys.append((qb, None))
                    if qb < 31:
                        keys.append((qb + 1, 1))
                    keys += [(32, None), (33, None)]
                else:
                    keys = [(32, None), (33, None)]
                qs = slice(qb * 128, (qb + 1) * 128)
                st_ = []
                for ki, (kt_i, mi) in enumerate(keys):
                    ps = pS[it % 4]
                    p_ = pt[it % R]
                    pm_ = ptm[it % R]
                    it += 1
                    kb.mm([lambda ps=ps, kt_i=kt_i: nc.tensor.matmul(
                        ps[:, :], lhsT=kT[:, kt_i * 128:(kt_i + 1) * 128], rhs=q4[:, :, qs], start=True, stop=True)],
                        reads=[kT, q4], writes=[ps])
                    kb.op("act", lambda ps=ps, p_=p_: nc.scalar.activation(
                        out=p_[:].rearrange("p h q -> p (h q)"), in_=ps[:, :], func=AF.Exp, scale=0.125),
                        reads=[ps], writes=[p_])
                    src = p_
                    if mi is not None:
                        eng = "dve" if it % 2 == 0 else "pool"
                        h_ = nc.vector if eng == "dve" else nc.gpsimd
                        kb.op(eng, lambda h_=h_, p_=p_, pm_=pm_, mi=mi: h_.tensor_tensor(
                            out=pm_[:], in0=p_[:], in1=wm[:, mi, :].unsqueeze(1).broadcast_to([128, 4, 128]), op=ALU.mult),
                            reads=[p_, wm], writes=[pm_])
                        src = pm_
                    st_.append((kt_i, src))
                return (qb, st_)

            def p2(state):
                qb, st_ = state
                qs = slice(qb * 128, (qb + 1) * 128)
                a = qb % 2
                po, pd = pO[a], pD[a]
                nk = len(st_)
                for ki, (kt_i, src) in enumerate(st_):
                    kb.mm([lambda po=po, src=src, kt_i=kt_i, ki=ki: nc.tensor.matmul(
                        po[:64, :], lhsT=vt[:, kt_i, :], rhs=src[:].rearrange("p h q -> p (h q)"),
                        start=(ki == 0), stop=(ki == nk - 1)),
                        lambda pd=pd, src=src, ki=ki: nc.tensor.matmul(
                        pd[:64, :], lhsT=ones64[:], rhs=src[:].rearrange("p h q -> p (h q)"),
                        start=(ki == 0), stop=(ki == nk - 1))],
                        reads=[vt, src, ones64], writes=[po, pd])
                o_, d_ = osb[a], dsb[a]
                kb.op("act", lambda po=po, o_=o_: nc.scalar.copy(out=o_[:].rearrange("p h q -> p (h q)"), in_=po[:64, :]),
                      reads=[po], writes=[o_])
                kb.op("act", lambda pd=pd, d_=d_: nc.scalar.copy(out=d_[:].rearrange("p h q -> p (h q)"), in_=pd[:64, :]),
                      reads=[pd], writes=[d_])
                kb.op("dve", lambda d_=d_: nc.vector.tensor_tensor(
                    out=d_[:], in0=d_[:], in1=sk[:, g * 4:(g + 1) * 4].unsqueeze(2).broadcast_to([64, 4, 128]), op=ALU.add),
                    reads=[d_, sk], writes=[d_])
                kb.op("dve", lambda d_=d_: nc.vector.reciprocal(out=d_[:], in_=d_[:]), reads=[d_], writes=[d_])
                kb.op("pool", lambda o_=o_, d_=d_: nc.gpsimd.tensor_tensor(
                    out=ost[:, :, qs], in0=o_[:], in1=d_[:], op=ALU.mult),
                    reads=[o_, d_], writes=[], acc_writes=[ost])

            prev = None
            for qb in range(NT):
                cur = p1(qb)
                if prev is not None:
                    p2(prev)
                prev = cur
            p2(prev)
            kb.dma("sp", cx.BR_C[g * 256:(g + 1) * 256, :].rearrange("(hh d) t -> d hh t", d=64), ost[:],
                   reads=[ost], writes=[], acc_writes=[cx.BR_C])


def wa_masks():
    kk = np.arange(128)[:, None]
    qq = np.arange(128)[None, :]
    m = np.zeros((128, 2, 128), np.float32)
    m[:, 0, :] = (kk >= qq)
    m[:, 1, :] = (kk <= qq)
    return {"m_wa": m}


def na_patterns():
    W = 64
    tok = np.arange(128)
    lr, lc = tok // W, tok % W
    pats = []
    keys = []
    for m in range(32):
        qr = 2 * m + lr[None, :]
        qc = lc[None, :]
        rs = np.clip(qr - 4, 0, 56)
        cs = np.clip(qc - 8, 0, 48)
        lst = []
        for kt_i in range(32):
            kr = 2 * kt_i + lr[:, None]
            kc = lc[:, None]
            valid = (kr >= rs) & (kr < rs + 8) & (kc >= cs) & (kc < cs + 16)
            if not valid.any():
                continue
            ri = np.clip(kr - qr + 7, 0, 14)
            ci = np.clip(kc - qc + 15, 0, 30)
            if 2 <= m <= 29:
                pid = kt_i - m + 2
                if m == 2:
                    pats.append((valid, ri, ci))
            else:
                pid = len(pats)
                pats.append((valid, ri, ci))
            lst.append((kt_i, pid))
        keys.append(lst)
    return keys, pats


def na_consts():
    keys, pats = na_patterns()
    n0 = len(keys[0]) + len(keys[1])
    order = list(range(n0, n0 + 5)) + list(range(0, n0)) + list(range(n0 + 5, len(pats)))
    remap = {old: new for new, old in enumerate(order)}
    pats2 = [pats[o] for o in order]
    keys2 = []
    for m, lst in enumerate(keys):
        if 2 <= m <= 29:
            keys2.append(lst)
        else:
            keys2.append([(k, remap[p]) for (k, p) in lst])
    valid = np.stack([p[0] for p in pats2], 1).astype(np.float32)
    ri = np.stack([p[1] for p in pats2], 1)
    ci = np.stack([p[2] for p in pats2], 1)
    return keys2, valid, ri, ci


def stage_na(cx, kb, nc, l, heads=None, bg_factory=None):
    keys_tab, valid_np, _, _ = na_consts()
    npat = valid_np.shape[1]
    with kb.scope():
        vmask = kb.sbuf("vmask", [128, npat, 128], F32)
        kb.dma("sp", vmask[:], cx.m_navalid[:], reads=[cx.m_navalid], writes=[vmask])
        ones64 = kb.sbuf("ones64n", [128, 64], BF16)
        kb.op("dve", lambda: nc.vector.memset(ones64[:], 1.0), writes=[ones64])
        qT = kb.sbuf("qTn", [64, T], BF16)
        kT = kb.sbuf("kTn", [64, T], BF16)
        vt = kb.sbuf("vtn", [128, NT, 64], BF16)
        ost = kb.sbuf("ostn", [64, T], BF16)
        bias = kb.sbuf("biasn", [128, npat, 128], F32)
        EB = kb.sbuf("EBn", [128, npat, 128], BF16)
        pS = [kb.psum("pSn%d" % i, [128, 512]) for i in range(4)]
        pO = [kb.psum("pOn%d" % i, [128, 512]) for i in range(2)]
        pD = [kb.psum("pDn%d" % i, [128, 512]) for i in range(2)]
        R = 6
        pt = [kb.sbuf("ptn%d" % i, [128, 4, 128], BF16) for i in range(R)]
        osb = [kb.sbuf("osbn%d" % i, [64, 256], F32) for i in range(2)]
        it = 0
        bg = None
        if bg_factory is not None:
            stw = [kb.sbuf("pcw%d" % i, [128, 8, 256], BF16) for i in range(4)]
            std = [kb.sbuf("pcd%d" % i, [128, 16, 1024], BF16) for i in range(2)]
            bg = bg_factory(stw, std)
        for h in (range(8) if heads is None else heads):
            kb.dma("sp", bias[:], cx.na_bias[l, h], reads=[cx.na_bias], writes=[bias])
            kb.op("act", lambda: nc.scalar.activation(out=bias[:], in_=bias[:], func=AF.Exp), reads=[bias], writes=[bias])
            kb.op("pool", lambda: nc.gpsimd.tensor_tensor(out=EB[:], in0=bias[:], in1=vmask[:], op=ALU.mult),
                  reads=[bias, vmask], writes=[EB])
            kb.dma("sp", qT[:], cx.QB[h * 64:(h + 1) * 64, :], reads=[cx.QB], writes=[qT])
            kb.dma("sp", kT[:], cx.KB_[h * 64:(h + 1) * 64, :], reads=[cx.KB_], writes=[kT])
            kb.dma("sp", vt[:], cx.VB_t[:, :, h * 64:(h + 1) * 64], reads=[cx.VB_t], writes=[vt])
            def p1(qb):
                nonlocal it
                if qb < 32:
                    keys = list(keys_tab[qb]) + [(32, None), (33, None)]
                else:
                    keys = [(32, None), (33, None)]
                qs = slice(qb * 128, (qb + 1) * 128)
                grp = [keys[i:i + 4] for i in range(0, len(keys), 4)]
                st_ = []
                for gk in grp:
                    ps = pS[it % 4]
                    p_ = pt[it % R]
                    it += 1
                    ng = len(gk)
                    kb.mm([(lambda ps=ps, j=j, kt_i=kt_i: nc.tensor.matmul(
                        ps[:, j * 128:(j + 1) * 128], lhsT=kT[:, kt_i * 128:(kt_i + 1) * 128], rhs=qT[:, qs],
                        start=True, stop=True)) for j, (kt_i, _) in enumerate(gk)],
                        reads=[kT, qT], writes=[ps])
                    kb.op("act", lambda ps=ps, p_=p_, ng=ng: nc.scalar.activation(
                        out=p_[:, :ng, :].rearrange("p h q -> p (h q)"), in_=ps[:, :ng * 128], func=AF.Exp, scale=0.125),
                        reads=[ps], writes=[p_])
                    nloc = sum(1 for (_, pid) in gk if pid is not None)
                    if nloc:
                        pid0 = gk[0][1]
                        eng = "dve" if it % 2 == 0 else "pool"
                        h_ = nc.vector if eng == "dve" else nc.gpsimd
                        kb.op(eng, lambda h_=h_, p_=p_, pid0=pid0, nloc=nloc: h_.tensor_tensor(
                            out=p_[:, :nloc, :], in0=p_[:, :nloc, :], in1=EB[:, pid0:pid0 + nloc, :], op=ALU.mult),
                            reads=[p_, EB], writes=[p_])
                    st_.append((gk, p_))
                return (qb, st_, len(keys))

            def p2(state):
                qb, st_, nk = state
                qs = slice(qb * 128, (qb + 1) * 128)
                a = qb % 2
                po = pO[a]
                pd = pD[a]
                done = 0
                for gk, p_ in st_:
                    fns = []
                    for j, (kt_i, _) in enumerate(gk):
                        first = (done == 0)
                        last = (done == nk - 1)
                        fns.append(lambda po=po, p_=p_, j=j, kt_i=kt_i, first=first, last=last: nc.tensor.matmul(
                            po[:64, 0:128], lhsT=vt[:, kt_i, :], rhs=p_[:, j, :], start=first, stop=last))
                        fns.append(lambda pd=pd, p_=p_, j=j, first=first, last=last: nc.tensor.matmul(
                            pd[:64, 0:128], lhsT=ones64[:], rhs=p_[:, j, :], start=first, stop=last))
                        done += 1
                    kb.mm(fns, reads=[vt, p_, ones64], writes=[po, pd])
                o_ = osb[a]
                kb.op("act", lambda po=po, o_=o_: nc.scalar.copy(out=o_[:, 0:128], in_=po[:64, 0:128]), reads=[po], writes=[o_])
                kb.op("act", lambda pd=pd, o_=o_: nc.scalar.copy(out=o_[:, 128:256], in_=pd[:64, 0:128]), reads=[pd, o_], writes=[o_])
                kb.op("dve", lambda o_=o_: nc.vector.reciprocal(out=o_[:, 128:256], in_=o_[:, 128:256]), reads=[o_], writes=[o_])
                kb.op("pool", lambda o_=o_: nc.gpsimd.tensor_tensor(
                    out=ost[:, qs], in0=o_[:, 0:128], in1=o_[:, 128:256], op=ALU.mult),
                    reads=[o_], writes=[], acc_writes=[ost])

            prev = None
            for qb in range(NT):
                cur = p1(qb)
                if prev is not None:
                    p2(prev)
                prev = cur
                if bg is not None and qb % 2 == 1:
                    next(bg, None)
            p2(prev)
            kb.dma("sp", cx.BR_B[h * 64:(h + 1) * 64, :], ost[:], reads=[ost], writes=[], acc_writes=[cx.BR_B])
        if bg is not None:
            for _ in bg:
                pass

LN_EPS = 1e-5
MB = 512
MBLKS = [(i * MB, min(MB, T - i * MB)) for i in range((T + MB - 1) // MB)]


def ln_setup(cx, kb, nc):
    ones = kb.sbuf("ln_ones", [128, 128], F32)
    kb.op("dve", lambda: nc.vector.memset(ones[:], 1.0 / D), writes=[ones])
    eps = kb.sbuf("ln_eps", [128, 1], F32)
    kb.op("dve", lambda: nc.vector.memset(eps[:], LN_EPS), writes=[eps])
    return ones, eps


def ln_block(kb, nc, r, nb, gam, bet, out, ones, eps, scr):
    rsq, ps1, ps2, mean, var = scr
    kb.op("act", lambda: nc.scalar.activation(out=rsq[:, :, :nb], in_=r[:, :, :nb], func=AF.Square), reads=[r], writes=[rsq])
    kb.mm([(lambda oc=oc: nc.tensor.matmul(ps1[:, :nb], lhsT=ones[:], rhs=r[:, oc, :nb], start=(oc == 0), stop=(oc == 7)))
           for oc in range(8)], reads=[ones, r], writes=[ps1])
    kb.mm([(lambda oc=oc: nc.tensor.matmul(ps2[:, :nb], lhsT=ones[:], rhs=rsq[:, oc, :nb], start=(oc == 0), stop=(oc == 7)))
           for oc in range(8)], reads=[ones, rsq], writes=[ps2])
    kb.op("act", lambda: nc.scalar.copy(out=mean[:, :nb], in_=ps1[:, :nb]), reads=[ps1], writes=[mean])
    kb.op("act", lambda: nc.scalar.copy(out=var[:, :nb], in_=ps2[:, :nb]), reads=[ps2], writes=[var])
    m2 = rsq
    kb.op("dve", lambda: nc.vector.tensor_tensor(out=m2[:, 0, :nb], in0=mean[:, :nb], in1=mean[:, :nb], op=ALU.mult),
          reads=[mean, rsq], writes=[rsq])
    kb.op("dve", lambda: nc.vector.tensor_tensor(out=var[:, :nb], in0=var[:, :nb], in1=m2[:, 0, :nb], op=ALU.subtract),
          reads=[var, rsq], writes=[var])
    kb.op("act", lambda: nc.scalar.activation(out=var[:, :nb], in_=var[:, :nb], func=AF.Sqrt, bias=eps[:]),
          reads=[var, eps], writes=[var])
    kb.op("dve", lambda: nc.vector.reciprocal(out=var[:, :nb], in_=var[:, :nb]), reads=[var], writes=[var])
    kb.op("dve", lambda: nc.vector.tensor_tensor(
        out=r[:, :, :nb], in0=r[:, :, :nb], in1=mean[:, :nb].unsqueeze(1).broadcast_to([128, 8, nb]), op=ALU.subtract),
        reads=[r, mean], writes=[r])
    kb.op("pool", lambda: nc.gpsimd.tensor_tensor(
        out=r[:, :, :nb], in0=r[:, :, :nb], in1=var[:, :nb].unsqueeze(1).broadcast_to([128, 8, nb]), op=ALU.mult),
        reads=[r, var], writes=[r])
    for oc in range(8):
        eng = "dve" if oc % 2 == 0 else "pool"
        h_ = nc.vector if eng == "dve" else nc.gpsimd
        kb.op(eng, lambda h_=h_, oc=oc: h_.tensor_scalar(
            out=out[:, oc, :nb], in0=r[:, oc, :nb], scalar1=gam[:, oc:oc + 1], scalar2=bet[:, oc:oc + 1],
            op0=ALU.mult, op1=ALU.add), reads=[r, gam, bet], writes=[out])


def load_ln_params(cx, kb, nc, l, which):
    gam = kb.sbuf("ln_g", [128, 8], F32)
    bet = kb.sbuf("ln_b", [128, 8], F32)
    kb.dma("sp", gam[:], cx.ln_gT[l, which], reads=[cx.ln_gT], writes=[gam])
    kb.dma("sp", bet[:], cx.ln_bT[l, which], reads=[cx.ln_bT], writes=[bet])
    return gam, bet


def stage_merge(cx, kb, nc, l, xsrc, modv):
    with kb.scope():
        ones, eps = ln_setup(cx, kb, nc)
        gam, bet = load_ln_params(cx, kb, nc, l, 0)
        wbr = kb.sbuf("wbr", [128, 4, 4, 1024], BF16)
        wout = kb.sbuf("wout", [128, 8, 1024], BF16)
        for i in range(4):
            kb.dma("pool", wbr[:, i], cx.w_branch[l, i].rearrange("(kc p) c -> p kc c", p=128), reads=[cx.w_branch], writes=[wbr])
        kb.dma("pool", wout[:], cx.w_out[l].rearrange("(kc p) c -> p kc c", p=128), reads=[cx.w_out], writes=[wout])
        wr = kb.sbuf("wr", [128, 8, 36], F32)
        kb.dma("sp", wr[:, :, 0:4], cx.moe_w_group[l].rearrange("(kc p) c -> p kc c", p=128), reads=[cx.moe_w_group], writes=[wr])
        kb.dma("sp", wr[:, :, 4:36], cx.moe_w_expert[l].rearrange("(kc p) c -> p kc c", p=128), reads=[cx.moe_w_expert], writes=[wr])
        rb = kb.sbuf("rb", [128, 36], F32)
        kb.dma("sp", rb[:, 0:4], cx.moe_b_group[l].partition_broadcast(128), reads=[cx.moe_b_group], writes=[rb])
        kb.dma("sp", rb[:, 4:36], cx.moe_b_expert[l].partition_broadcast(128), reads=[cx.moe_b_expert], writes=[rb])
        onep4 = kb.sbuf("onep4", [128, 8, 2], F32)
        kb.op("dve", lambda: nc.vector.tensor_scalar(out=onep4[:], in0=modv[:, 32:40, :], scalar1=1.0, scalar2=None, op0=ALU.add),
              reads=[modv], writes=[onep4])
        brt = [kb.sbuf("brt%d" % i, [128, 4, 4, MB], BF16) for i in range(1)]
        gtt = kb.sbuf("gtt", [128, 32, MB], BF16)
        xb = kb.sbuf("xbm", [128, 8, MB], F32)
        mrg = kb.sbuf("mrg", [128, 8, MB], BF16)
        r = kb.sbuf("rm", [128, 8, MB], F32)
        hx2b = kb.sbuf("hx2b", [128, 8, MB], BF16)
        rsq = kb.sbuf("rsqm", [128, 8, MB], F32)
        x1 = r
        hx2f = rsq
        mean = kb.sbuf("meanm", [128, MB], F32)
        var = kb.sbuf("varm", [128, MB], F32)
        csb = [kb.sbuf("csb%d" % i, [128, MB], F32) for i in range(4)]
        acc = kb.sbuf("accm", [128, MB], F32)
        mixs = kb.sbuf("mixs", [128, MB], F32)
        pbr = [kb.psum("pbr%d" % i, [128, MB]) for i in range(4)]
        pmx = kb.psum("pmx", [128, MB])
        ps1 = kb.psum("ps1m", [128, MB])
        ps2 = kb.psum("ps2m", [128, MB])
        prt = kb.psum("prt", [128, MB])
        brs = [cx.BR_A, cx.BR_B, cx.BR_C, cx.BR_D]
        lgt = kb.sbuf("lgt", [128, 36], F32)
        gmx = kb.sbuf("gmx", [128, 1], F32)
        ngm = kb.sbuf("ngm", [128, 1], F32)
        ge = kb.sbuf("ge", [128, 4], F32)
        gs = kb.sbuf("gsr", [128, 1], F32)
        oh = kb.sbuf("oh", [128, 4], F32)
        lem = kb.sbuf("lem", [128, 4, 8], F32)
        mx8 = kb.sbuf("mx8", [128, 8], F32)
        dd = kb.sbuf("dd", [128, 1], F32)
        w1 = kb.sbuf("w1", [128, 1], F32)
        w2 = kb.sbuf("w2", [128, 1], F32)
        tt1 = kb.sbuf("tt1", [128, 32], F32)
        tt2 = kb.sbuf("tt2", [128, 32], F32)
        wgt = kb.sbuf("wgt", [128, 32], F32)
        for bi, (t0, nb) in enumerate(MBLKS):
            col = blkcol(t0)
            b_ = brt[0]
            for i in range(4):
                kb.dma("sp", b_[:, i, :, :nb], brs[i][:, t0:t0 + nb].rearrange("(kc p) t -> p kc t", p=128),
                       reads=[brs[i]], writes=[b_])
            kb.dma("sp", gtt[:, :, :nb], cx.GATE[:, t0:t0 + nb].rearrange("(j p) t -> p j t", p=128), reads=[cx.GATE], writes=[gtt])
            kb.dma("sp", xb[:, :, :nb], xsrc[:, t0:t0 + nb].rearrange("(kc p) t -> p kc t", p=128), reads=[xsrc], writes=[xb])
            for oc in range(8):
                for i in range(4):
                    kb.mm([(lambda i=i, kc=kc, oc=oc: nc.tensor.matmul(
                        pbr[i][:, :nb], lhsT=wbr[:, i, kc, oc * 128:(oc + 1) * 128], rhs=b_[:, i, kc, :nb],
                        start=(kc == 0), stop=(kc == 3))) for kc in range(4)], reads=[wbr, b_], writes=[pbr[i]])
                    kb.op("act", lambda i=i: nc.scalar.copy(out=csb[i][:, :nb], in_=pbr[i][:, :nb]), reads=[pbr[i]], writes=[csb[i]])
                    eng = "dve" if i % 2 == 0 else "pool"
                    h_ = nc.vector if eng == "dve" else nc.gpsimd
                    kb.op(eng, lambda h_=h_, i=i, oc=oc: h_.tensor_tensor(
                        out=csb[i][:, :nb], in0=csb[i][:, :nb], in1=gtt[:, i * 8 + oc, :nb], op=ALU.mult),
                        reads=[csb[i], gtt], writes=[csb[i]])
                kb.op("dve", lambda: nc.vector.tensor_tensor(out=acc[:, :nb], in0=csb[0][:, :nb], in1=csb[1][:, :nb], op=ALU.add),
                      reads=[csb[0], csb[1]], writes=[acc])
                kb.op("pool", lambda: nc.gpsimd.tensor_tensor(out=csb[2][:, :nb], in0=csb[2][:, :nb], in1=csb[3][:, :nb], op=ALU.add),
                      reads=[csb[2], csb[3]], writes=[csb[2]])
                kb.op("dve", lambda oc=oc: nc.vector.tensor_tensor(out=mrg[:, oc, :nb], in0=acc[:, :nb], in1=csb[2][:, :nb], op=ALU.add),
                      reads=[acc, csb[2]], writes=[mrg])
            for oc in range(8):
                kb.mm([(lambda kc=kc, oc=oc: nc.tensor.matmul(
                    pmx[:, :nb], lhsT=wout[:, kc, oc * 128:(oc + 1) * 128], rhs=mrg[:, kc, :nb],
                    start=(kc == 0), stop=(kc == 7))) for kc in range(8)], reads=[wout, mrg], writes=[pmx])
                kb.op("act", lambda: nc.scalar.copy(out=mixs[:, :nb], in_=pmx[:, :nb]), reads=[pmx], writes=[mixs])
                kb.op("pool", lambda oc=oc: nc.gpsimd.tensor_scalar(
                    out=mixs[:, :nb], in0=mixs[:, :nb], scalar1=modv[:, 16 + oc, col:col + 1], scalar2=None, op0=ALU.mult),
                    reads=[mixs, modv], writes=[mixs])
                kb.op("dve", lambda oc=oc: nc.vector.scalar_tensor_tensor(
                    out=r[:, oc, :nb], in0=xb[:, oc, :nb], scalar=ALPHA, in1=mixs[:, :nb], op0=ALU.mult, op1=ALU.add),
                    reads=[xb, mixs], writes=[r])
            ln_block(kb, nc, r, nb, gam, bet, x1, ones, eps, (rsq, ps1, ps2, mean, var))
            kb.dma("sp", cx.XT[:, t0:t0 + nb].rearrange("(kc p) t -> p kc t", p=128), x1[:, :, :nb],
                   reads=[x1], writes=[], acc_writes=[cx.XT])
            for oc in range(8):
                eng = "dve" if oc % 2 == 0 else "pool"
                h_ = nc.vector if eng == "dve" else nc.gpsimd
                kb.op(eng, lambda h_=h_, oc=oc: h_.tensor_scalar(
                    out=hx2f[:, oc, :nb], in0=x1[:, oc, :nb], scalar1=onep4[:, oc, col:col + 1],
                    scalar2=modv[:, 24 + oc, col:col + 1], op0=ALU.mult, op1=ALU.add),
                    reads=[x1, onep4, modv], writes=[hx2f])
            kb.op("act", lambda: nc.scalar.copy(out=hx2b[:, :, :nb], in_=hx2f[:, :, :nb]), reads=[hx2f], writes=[hx2b])
            kb.dma("sp", cx.HX2[:, t0:t0 + nb].rearrange("(kc p) t -> p kc t", p=128), hx2b[:, :, :nb],
                   reads=[hx2b], writes=[], acc_writes=[cx.HX2])
            for tj in range(nb // 128):
                ti = t0 // 128 + tj
                tsl = slice(tj * 128, (tj + 1) * 128)
                kb.mm([(lambda kc=kc: nc.tensor.matmul(prt[:, 0:36], lhsT=hx2f[:, kc, tsl], rhs=wr[:, kc, :],
                                                       start=(kc == 0), stop=(kc == 7))) for kc in range(8)],
                      reads=[hx2f, wr], writes=[prt])
                kb.op("act", lambda: nc.scalar.copy(out=lgt[:], in_=prt[:, 0:36]), reads=[prt], writes=[lgt])
                kb.op("dve", lambda: nc.vector.tensor_tensor(out=lgt[:], in0=lgt[:], in1=rb[:], op=ALU.add), reads=[lgt, rb], writes=[lgt])
                kb.op("dve", lambda: nc.vector.tensor_reduce(out=gmx[:], in_=lgt[:, 0:4], axis=AX.X, op=ALU.max), reads=[lgt], writes=[gmx])
                kb.op("dve", lambda: nc.vector.tensor_scalar(out=ngm[:], in0=gmx[:], scalar1=-1.0, scalar2=None, op0=ALU.mult), reads=[gmx], writes=[ngm])
                kb.op("act", lambda: nc.scalar.activation(out=ge[:], in_=lgt[:, 0:4], func=AF.Exp, bias=ngm[:]), reads=[lgt, ngm], writes=[ge])
                kb.op("dve", lambda: nc.vector.tensor_reduce(out=gs[:], in_=ge[:], axis=AX.X, op=ALU.add), reads=[ge], writes=[gs])
                kb.op("dve", lambda: nc.vector.reciprocal(out=gs[:], in_=gs[:]), reads=[gs], writes=[gs])
                kb.op("dve", lambda: nc.vector.tensor_scalar(out=oh[:], in0=lgt[:, 0:4], scalar1=gmx[:], scalar2=None, op0=ALU.is_equal), reads=[lgt, gmx], writes=[oh])
                kb.op("dve", lambda: nc.vector.tensor_scalar(out=oh[:], in0=oh[:], scalar1=-1.0, scalar2=1e30, op0=ALU.add, op1=ALU.mult), reads=[oh], writes=[oh])
                kb.op("dve", lambda: nc.vector.tensor_tensor(
                    out=lem[:], in0=lgt[:, 4:36].rearrange("p (g e) -> p g e", e=8), in1=oh[:].unsqueeze(2).broadcast_to([128, 4, 8]), op=ALU.add),
                    reads=[lgt, oh], writes=[lem])
                lemf = lem[:].rearrange("p g e -> p (g e)")
                kb.op("dve", lambda: nc.vector.max(out=mx8[:], in_=lem[:].rearrange("p g e -> p (g e)")), reads=[lem], writes=[mx8])
                kb.op("dve", lambda: nc.vector.tensor_tensor(out=dd[:], in0=mx8[:, 1:2], in1=mx8[:, 0:1], op=ALU.subtract), reads=[mx8], writes=[dd])
                kb.op("act", lambda: nc.scalar.activation(out=dd[:], in_=dd[:], func=AF.Exp), reads=[dd], writes=[dd])
                kb.op("dve", lambda: nc.vector.tensor_scalar(out=w1[:], in0=dd[:], scalar1=1.0, scalar2=None, op0=ALU.add), reads=[dd], writes=[w1])
                kb.op("dve", lambda: nc.vector.reciprocal(out=w1[:], in_=w1[:]), reads=[w1], writes=[w1])
                kb.op("dve", lambda: nc.vector.tensor_tensor(out=w2[:], in0=dd[:], in1=w1[:], op=ALU.mult), reads=[dd, w1], writes=[w2])
                kb.op("dve", lambda: nc.vector.tensor_scalar(
                    out=tt1[:], in0=lem[:].rearrange("p g e -> p (g e)"), scalar1=mx8[:, 0:1], scalar2=w1[:], op0=ALU.is_equal, op1=ALU.mult),
                    reads=[lem, mx8, w1], writes=[tt1])
                kb.op("dve", lambda: nc.vector.tensor_scalar(
                    out=tt2[:], in0=lem[:].rearrange("p g e -> p (g e)"), scalar1=mx8[:, 1:2], scalar2=w2[:], op0=ALU.is_equal, op1=ALU.mult),
                    reads=[lem, mx8, w2], writes=[tt2])
                kb.op("dve", lambda: nc.vector.tensor_tensor(out=tt1[:], in0=tt1[:], in1=tt2[:], op=ALU.add), reads=[tt1, tt2], writes=[tt1])
                kb.op("dve", lambda: nc.vector.tensor_scalar(out=wgt[:], in0=tt1[:], scalar1=gs[:], scalar2=None, op0=ALU.mult), reads=[tt1, gs], writes=[wgt])
                kb.dma("sp", cx.WG_t[:, ti, :], wgt[:], reads=[wgt], writes=[], acc_writes=[cx.WG_t])


def stage_moe(cx, kb, nc, l, modv, yout=None, precast=True):
    with kb.scope():
        ones, eps = ln_setup(cx, kb, nc)
        gam, bet = load_ln_params(cx, kb, nc, l, 1)
        wgT = kb.sbuf("wgT", [32, T], F32)
        sel = kb.sbuf("sel", [32, 32, 128], F32)
        kb.dma("sp", sel[:], cx.m_sel[:], reads=[cx.m_sel], writes=[sel])
        ptr = [kb.psum("ptr%d" % i, [128, 512]) for i in range(2)]
        with kb.scope():
            wgl = kb.sbuf("wgl", [128, NT, 32], F32)
            id32 = kb.sbuf("id32", [128, 128], F32)
            kb.dma("sp", wgl[:], cx.WG_t[:], reads=[cx.WG_t], writes=[wgl])
            kb.dma("sp", id32[:], cx.m_ident32[:], reads=[cx.m_ident32], writes=[id32])
            for ti in range(NT):
                p_ = ptr[ti % 2]
                kb.mm([lambda p_=p_, ti=ti: nc.tensor.matmul(p_[:32, 0:128], lhsT=wgl[:, ti, :], rhs=id32[:], start=True, stop=True)],
                      reads=[wgl, id32], writes=[p_])
                kb.op("act", lambda p_=p_, ti=ti: nc.scalar.copy(out=wgT[:, ti * 128:(ti + 1) * 128], in_=p_[:32, 0:128]),
                      reads=[p_], writes=[], acc_writes=[wgT])
        if precast:
            precast_expert_weights(cx, kb, nc, l)
        hx = kb.sbuf("hxm", [128, 8, 512], BF16)
        yacc = kb.sbuf("yacc", [128, 8, 512], F32)
        hid = kb.sbuf("hid", [128, 16, 512], BF16)
        wdg = kb.sbuf("wdg", [128, 8, 2, 1024], BF16)
        NWB = 4
        wgs = [kb.sbuf("wgs%d" % i, [128, 8, 256], BF16) for i in range(NWB)]
        wus = [kb.sbuf("wus%d" % i, [128, 8, 256], BF16) for i in range(NWB)]
        xb = kb.sbuf("xbe", [128, 8, 512], F32)
        rsq = kb.sbuf("rsqe", [128, 8, 512], F32)
        mean = kb.sbuf("meane", [128, 512], F32)
        var = kb.sbuf("vare", [128, 512], F32)
        reps = [kb.sbuf("reps%d" % i, [128, 512], BF16) for i in range(2)]
        ssb = [kb.sbuf("ssb%d" % i, [128, 512], BF16) for i in range(3)]
        ytmp = [kb.sbuf("ytmp%d" % i, [128, 512], F32) for i in range(2)]
        NPS = 3
        pg = [kb.psum("pg%d" % i, [128, 512]) for i in range(NPS)]
        pu = [kb.psum("pu%d" % i, [128, 512]) for i in range(NPS)]
        py = ptr
        ps1 = pg[0]
        ps2 = pu[0]
        prep = ptr[1]
        nw = 0
        it = 0
        for bi, (t0, nb) in enumerate(BLKS):
            col = blkcol(t0)
            kb.dma("sp", hx[:, :, :nb], cx.HX2[:, t0:t0 + nb].rearrange("(kc p) t -> p kc t", p=128), reads=[cx.HX2], writes=[hx])
            kb.dma("sp", xb[:, :, :nb], cx.XT[:, t0:t0 + nb].rearrange("(kc p) t -> p kc t", p=128), reads=[cx.XT], writes=[xb])
            for g in range(4):
                kb.dma("sp", wdg[:].rearrange("p e hc d -> p (e hc d)"), cx.WDB[g], reads=[cx.WDB], writes=[wdg])
                for e_ in range(8):
                    e = g * 8 + e_
                    wg_, wu_ = wgs[nw % NWB], wus[nw % NWB]
                    nw += 1
                    kb.dma("sp", wg_[:].rearrange("p kc f -> p (kc f)"), cx.WGB[e], reads=[cx.WGB], writes=[wg_])
                    kb.dma("act", wu_[:].rearrange("p kc f -> p (kc f)"), cx.WUB[e], reads=[cx.WUB], writes=[wu_])
                    rp = reps[e % 2]
                    kb.mm([lambda e=e: nc.tensor.matmul(prep[:, :nb], lhsT=sel[:, e, :], rhs=wgT[:, t0:t0 + nb], start=True, stop=True)],
                          reads=[sel, wgT], writes=[prep])
                    kb.op("act", lambda rp=rp: nc.scalar.copy(out=rp[:, :nb], in_=prep[:, :nb]), reads=[prep], writes=[rp])
                    for hc in range(2):
                        a = it % NPS
                        it += 1
                        kb.mm([(lambda kc=kc, a=a, hc=hc, wg_=wg_: nc.tensor.matmul(
                            pg[a][:, :nb], lhsT=wg_[:, kc, hc * 128:(hc + 1) * 128], rhs=hx[:, kc, :nb],
                            start=(kc == 0), stop=(kc == 7))) for kc in range(8)], reads=[wg_, hx], writes=[pg[a]])
                        kb.mm([(lambda kc=kc, a=a, hc=hc, wu_=wu_: nc.tensor.matmul(
                            pu[a][:, :nb], lhsT=wu_[:, kc, hc * 128:(hc + 1) * 128], rhs=hx[:, kc, :nb],
                            start=(kc == 0), stop=(kc == 7))) for kc in range(8)], reads=[wu_, hx], writes=[pu[a]])
                        kb.op("act", lambda a=a: nc.scalar.activation(out=ssb[a][:, :nb], in_=pg[a][:, :nb], func=AF.Silu),
                              reads=[pg[a]], writes=[ssb[a]])
                        kb.op("pool", lambda a=a, rp=rp: nc.gpsimd.tensor_tensor(
                            out=ssb[a][:, :nb], in0=ssb[a][:, :nb], in1=rp[:, :nb], op=ALU.mult),
                            reads=[ssb[a], rp], writes=[ssb[a]])
                        kb.op("dve", lambda a=a, e_=e_, hc=hc: nc.vector.tensor_tensor(
                            out=hid[:, e_ * 2 + hc, :nb], in0=pu[a][:, :nb], in1=ssb[a][:, :nb], op=ALU.mult),
                            reads=[pu[a], ssb[a]], writes=[], acc_writes=[hid])
                for oc in range(8):
                    p_ = py[oc % 2]
                    fns = []
                    for j in range(16):
                        fns.append(lambda j=j, oc=oc, p_=p_: nc.tensor.matmul(
                            p_[:, :nb], lhsT=wdg[:, j // 2, j % 2, oc * 128:(oc + 1) * 128], rhs=hid[:, j, :nb],
                            start=(j == 0), stop=(j == 15)))
                    kb.mm(fns, reads=[wdg, hid], writes=[p_])
                    if g == 0:
                        kb.op("act", lambda oc=oc, p_=p_: nc.scalar.copy(out=yacc[:, oc, :nb], in_=p_[:, :nb]), reads=[p_], writes=[], acc_writes=[yacc])
                    else:
                        yt = ytmp[oc % 2]
                        kb.op("act", lambda yt=yt, p_=p_: nc.scalar.copy(out=yt[:, :nb], in_=p_[:, :nb]), reads=[p_], writes=[yt])
                        eng = "dve" if oc % 2 == 0 else "pool"
                        h_ = nc.vector if eng == "dve" else nc.gpsimd
                        kb.op(eng, lambda h_=h_, oc=oc, yt=yt: h_.tensor_tensor(
                            out=yacc[:, oc, :nb], in0=yacc[:, oc, :nb], in1=yt[:, :nb], op=ALU.add),
                            reads=[yt, yacc], writes=[], acc_writes=[yacc])
            for oc in range(8):
                kb.op("pool", lambda oc=oc: nc.gpsimd.tensor_scalar(
                    out=yacc[:, oc, :nb], in0=yacc[:, oc, :nb], scalar1=modv[:, 40 + oc, col:col + 1], scalar2=None, op0=ALU.mult),
                    reads=[yacc, modv], writes=[yacc])
                kb.op("dve", lambda oc=oc: nc.vector.scalar_tensor_tensor(
                    out=yacc[:, oc, :nb], in0=xb[:, oc, :nb], scalar=ALPHA, in1=yacc[:, oc, :nb], op0=ALU.mult, op1=ALU.add),
                    reads=[xb, yacc], writes=[yacc])
            ln_block(kb, nc, yacc, nb, gam, bet, yacc, ones, eps, (rsq, ps1, ps2, mean, var))
            kb.dma("sp", cx.XT[:, t0:t0 + nb].rearrange("(kc p) t -> p kc t", p=128), yacc[:, :, :nb],
                   reads=[yacc], writes=[], acc_writes=[cx.XT])
            if yout is not None and t0 < TL:
                kb.dma("sp", yout[:, t0:t0 + nb].rearrange("(kc p) t -> p kc t", p=128), yacc[:, :, :nb],
                       reads=[yacc], writes=[], acc_writes=[yout])


def precast_expert_weights(cx, kb, nc, l):
    with kb.scope():
        stw = [kb.sbuf("pcw%d" % i, [128, 8, 256], BF16) for i in range(4)]
        std = [kb.sbuf("pcd%d" % i, [128, 16, 1024], BF16) for i in range(2)]
        n = 0
        for g in range(4):
            d_ = std[g % 2]
            kb.dma("pool", d_[:], cx.moe_w_down[l, g].rearrange("e (hc p) d -> p (e hc) d", p=128), reads=[cx.moe_w_down], writes=[d_])
            kb.dma("sp", cx.WDB[g], d_[:].rearrange("p j d -> p (j d)"), reads=[d_], writes=[], acc_writes=[cx.WDB])
            for e_ in range(8):
                e = g * 8 + e_
                for src, dst in ((cx.moe_w_gate, cx.WGB), (cx.moe_w_up, cx.WUB)):
                    w_ = stw[n % 4]
                    n += 1
                    kb.dma("pool", w_[:], src[l, g, e_].rearrange("(kc p) f -> p kc f", p=128), reads=[src], writes=[w_])
                    kb.dma("sp", dst[e], w_[:].rearrange("p kc f -> p (kc f)"), reads=[w_], writes=[], acc_writes=[dst])


def precast_gen(cx, kb, nc, l, stw, std):
    pend = []
    n = 0
    for g in range(4):
        d_ = std[g % 2]
        kb.dma("pool", d_[:], cx.moe_w_down[l, g].rearrange("e (hc p) d -> p (e hc) d", p=128), reads=[cx.moe_w_down], writes=[d_])
        pend.append((cx.WDB, g, d_, "p j d -> p (j d)"))
        yield
        for e_ in range(8):
            e = g * 8 + e_
            for src, dst in ((cx.moe_w_gate, cx.WGB), (cx.moe_w_up, cx.WUB)):
                w_ = stw[n % len(stw)]
                n += 1
                kb.dma("pool", w_[:], src[l, g, e_].rearrange("(kc p) f -> p kc f", p=128), reads=[src], writes=[w_])
                pend.append((dst, e, w_, "p kc f -> p (kc f)"))
                yield
                while len(pend) > 2:
                    dst_, idx_, buf_, pat_ = pend.pop(0)
                    kb.dma("sp", dst_[idx_], buf_[:].rearrange(pat_), reads=[buf_], writes=[], acc_writes=[dst_])
    while pend:
        dst_, idx_, buf_, pat_ = pend.pop(0)
        kb.dma("sp", dst_[idx_], buf_[:].rearrange(pat_), reads=[buf_], writes=[], acc_writes=[dst_])
        yield


def rope_tables():
    t = np.arange(TL)
    row = (t // 64).astype(np.float32)
    colp = (t % 64).astype(np.float32)
    nf = 16
    inv = (np.float32(10000.0) ** (-np.arange(nf, dtype=np.float32) / np.float32(nf))).astype(np.float32)
    cos = np.ones((128, T), np.float32)
    sin = np.zeros((128, T), np.float32)
    for q in range(128):
        d = q % 64
        pos = row if d < 32 else colp
        ang = (pos * inv[d % 16]).astype(np.float32)
        cos[q, :TL] = np.cos(ang).astype(np.float32)
        sin[q, :TL] = np.sin(ang).astype(np.float32)
    pt = np.zeros((128, 128), np.float32)
    for m in range(128):
        if (m % 32) < 16:
            pt[m + 16, m] = -1.0
        else:
            pt[m - 16, m] = 1.0
    return cos, sin, pt


_CONST = {}
_NAIDX = {}


def consts():
    if not _CONST:
        cos, sin, pt = rope_tables()
        _CONST.update(rope_cos=cos, rope_sin=sin, rope_pt=pt)
        _CONST.update(scan_masks())
        _CONST.update(wa_masks())
        sel = np.zeros((32, 32, 128), np.float32)
        for e in range(32):
            sel[e, e, :] = 1.0
        _CONST["m_sel"] = sel
        _CONST["m_ident32"] = np.eye(128, dtype=np.float32)
        _k, _valid, _ri, _ci = na_consts()
        _CONST["m_navalid"] = _valid
        _NAIDX["ri"] = _ri
        _NAIDX["ci"] = _ci
    return _CONST


def core_inputs(inp, b):
    f = lambda a: np.ascontiguousarray(a, dtype=np.float32)
    m = {}
    m["xin"] = f(np.concatenate([inp["x"][b].T, inp["ctx"][b].T], axis=1))
    cv = np.stack([inp["c"][b].reshape(8, 128).T, inp["c_ctx"].reshape(8, 128).T], axis=2)
    m["cvec"] = f(cv)
    m["w_mod"] = f(inp["w_mod"])
    m["b_modT"] = f(inp["b_mod"].reshape(2, 48, 128).transpose(0, 2, 1))
    m["w_in"] = f(inp["w_in"])
    m["hg_gamma"] = f(inp["hg_gamma"])
    m["gla_lr_w"] = f(inp["gla_lr_w"])
    m["gla_lr_b"] = f(inp["gla_lr_b"])
    m["wa_sink"] = f(inp["wa_sink"])
    for k in ("w_branch", "w_out", "moe_w_group", "moe_b_group", "moe_w_expert", "moe_b_expert",
              "moe_w_gate", "moe_w_up", "moe_w_down"):
        m[k] = f(inp[k])
    m["ln_gT"] = f(inp["ln_g"].reshape(2, 2, 8, 128).transpose(0, 1, 3, 2))
    m["ln_bT"] = f(inp["ln_b"].reshape(2, 2, 8, 128).transpose(0, 1, 3, 2))
    consts()
    m["na_bias"] = f(inp["na_rpb"][:, :, _NAIDX["ri"], _NAIDX["ci"]])
    m.update(consts())
    return m


FUSED = 1


def build_program(layers, final):
    nc = bass.Bass("TRN2", target_bir_lowering=False)
    cx = Ctx()
    declare_inputs(cx, nc)
    if final:
        yout = Buf(nc.dram_tensor("yout", [D, TL], F32, kind="ExternalOutput"), "yout")
    else:
        yout = None
    with ExitStack() as st:
        kb = KB(nc, st)
        declare_scratch(cx, kb, debug=False, as_output=(() if final else ("XT",)))
        modT = [kb.sbuf("modT%d" % l, [128, 48, 2], F32) for l in range(2)]
        stage_mod(cx, kb, nc, modT)
        first = True
        for l in layers:
            xsrc = cx.xin if first else cx.XT
            first = False
            with kb.scope():
                lbt = [kb.sbuf("lbt%d" % i, [128, 1024], F32) for i in range(2)]
                omlt = [kb.sbuf("omlt%d" % i, [128, 1024], F32) for i in range(2)]
                stage_lb(cx, kb, nc, lbt, omlt)
                hxT = kb.sbuf("hxT", [128, 8, T], BF16)
                stage_modulate(cx, kb, nc, xsrc, modT[l], 0, 1, hxT)
                stage_inproj_fm(cx, kb, nc, l, hxT)
                stage_inproj_tm(cx, kb, nc, l, hxT, lbt[l], omlt[l])
            stage_gla_gate(cx, kb, nc, l)
            stage_scan(cx, kb, nc, "gla")
            stage_scan(cx, kb, nc, "hgrn")
            stage_wa(cx, kb, nc, l)
            stage_na(cx, kb, nc, l, bg_factory=(lambda stw, std, l=l: precast_gen(cx, kb, nc, l, stw, std)))
            stage_merge(cx, kb, nc, l, xsrc, modT[l])
            stage_moe(cx, kb, nc, l, modT[l], yout if (final and l == layers[-1]) else None, precast=False)
        if final:
            kb.finish([yout])
        else:
            kb.finish([cx.XT])
    return nc


def kernel(**inputs):
    inp = {k: np.asarray(v) for k, v in inputs.items()}
    in_maps = [core_inputs(inp, b) for b in range(4)]
    if FUSED:
        nc = build_program([0, 1], True)
        res = run_bass_kernel_spmd(nc, in_maps, core_ids=list(range(4)))
    else:
        nc0 = build_program([0], False)
        res0 = run_bass_kernel_spmd(nc0, in_maps, core_ids=list(range(4)))
        for b in range(4):
            in_maps[b]["xin"] = np.ascontiguousarray(res0.results[b]["XT"])
        nc1 = build_program([1], True)
        res = run_bass_kernel_spmd(nc1, in_maps, core_ids=list(range(4)))
    out = np.stack([np.ascontiguousarray(res.results[b]["yout"].T) for b in range(4)], axis=0)
    return out.astype(np.float32)
```

```python
import numpy as np
from contextlib import ExitStack
import concourse.bass as bass
import concourse.mybir as mybir
from concourse.bass_utils import run_bass_kernel_spmd
import os

F32 = mybir.dt.float32
BF16 = mybir.dt.bfloat16
U32 = mybir.dt.uint32
AF = mybir.ActivationFunctionType
ALU = mybir.AluOpType
AX = mybir.AxisListType


class Buf:
    __slots__ = ("t", "w", "r", "name")

    def __init__(self, t, name=""):
        self.t = t
        self.w = []
        self.r = []
        self.name = name

    def __getitem__(self, idx):
        return self.t[idx]


class Dep:
    __slots__ = ("w", "r")

    def __init__(self):
        self.w = []
        self.r = []


class Eng:
    def __init__(self, name, h, sem):
        self.name = name
        self.h = h
        self.sem = sem
        self.count = 0
        self.seen = {}


class KB:
    def __init__(self, nc, stack, n_dma_sems=8):
        self.nc = nc
        self.stack = stack
        self.sems = {}
        self.engs = {}
        for name, h in (("pe", nc.tensor), ("act", nc.scalar), ("dve", nc.vector),
                        ("pool", nc.gpsimd), ("sp", nc.sync)):
            s = stack.enter_context(nc.semaphore("s_" + name))
            self.sems[id(s)] = s
            self.engs[name] = Eng(name, h, s)
        self.dma_pool = {}
        for q in ("sp", "pool", "act"):
            lst = []
            for i in range(n_dma_sems):
                s = stack.enter_context(nc.semaphore("d_%s%d" % (q, i)))
                self.sems[id(s)] = s
                lst.append([s, 0])
            self.dma_pool[q] = [lst, 0]
        self.ninstr = 0

    def scope(self):
        kb = self

        class _Scope:
            def __enter__(self_s):
                self_s.prev = kb.stack
                self_s.st = ExitStack()
                self_s.st.__enter__()
                kb.stack = self_s.st
                kb.nscope = getattr(kb, "nscope", 0) + 1
                return self_s

            def __exit__(self_s, *a):
                if a[0] is None:
                    kb.barrier()
                kb.stack = self_s.prev
                return self_s.st.__exit__(*a)
        return _Scope()

    def sbuf(self, name, shape, dtype):
        name = "%s_s%d" % (name, getattr(self, "nscope", 0))
        t = self.stack.enter_context(self.nc.sbuf_tensor(name, list(shape), dtype))
        return Buf(t, name)

    def psum(self, name, shape, dtype=F32):
        name = "%s_s%d" % (name, getattr(self, "nscope", 0))
        t = self.stack.enter_context(self.nc.psum_tensor(name, list(shape), dtype))
        return Buf(t, name)

    def dram(self, name, shape, dtype, kind="Internal"):
        t = self.nc.dram_tensor(name, list(shape), dtype, kind=kind)
        return Buf(t, name)

    def _wait(self, e, deps):
        need = {}
        for (sid, val) in deps:
            if need.get(sid, 0) < val:
                need[sid] = val
        for sid, val in need.items():
            if e.seen.get(sid, 0) < val:
                e.h.wait_ge(self.sems[sid], val)
                e.seen[sid] = val
                self.ninstr += 1

    def _collect(self, reads, writes, acc_writes=()):
        deps = []
        for b in reads:
            deps += b.w
        for b in writes:
            deps += b.w
            deps += b.r
        for b in acc_writes:
            deps += b.r
        return deps

    @staticmethod
    def _compact(lst):
        mx = {}
        for (sid, v) in lst:
            if mx.get(sid, 0) < v:
                mx[sid] = v
        return list(mx.items())

    def _commit(self, token, reads, writes, acc_writes=()):
        for b in acc_writes:
            b.w.append(token)
            if len(b.w) > 64:
                b.w = self._compact(b.w)
        for b in reads:
            b.r.append(token)
            if len(b.r) > 64:
                mx = {}
                for (sid, v) in b.r:
                    if mx.get(sid, 0) < v:
                        mx[sid] = v
                b.r = list(mx.items())
        for b in writes:
            b.w = [token]
            b.r = []

    def _cutcheck(self):
        import os
        cut = os.environ.get("KB_CUT")
        self.nops = getattr(self, "nops", 0) + 1
        skip = os.environ.get("KB_SKIP")
        if skip and self.nops in [int(x) for x in skip.split(",")]:
            return True
        return cut is not None and self.nops > int(cut)

    def op(self, eng, fn, reads=(), writes=(), same_engine_sync=True, acc_writes=()):
        if self._cutcheck():
            return None
        e = self.engs[eng]
        deps = self._collect(reads, writes, acc_writes)
        if not same_engine_sync:
            deps = [d for d in deps if d[0] != id(e.sem)]
        self._wait(e, deps)
        ins = fn()
        e.count += 1
        ins.then_inc(e.sem, 1)
        self.ninstr += 1
        token = (id(e.sem), e.count)
        self._commit(token, reads, writes, acc_writes)
        return token

    def mm(self, fns, reads=(), writes=()):
        if self._cutcheck():
            return None
        e = self.engs["pe"]
        deps = self._collect(reads, writes)
        deps = [d for d in deps if d[0] != id(e.sem)]
        self._wait(e, deps)
        ins = None
        for fn in fns:
            ins = fn()
            self.ninstr += 1
        e.count += 1
        ins.then_inc(e.sem, 1)
        token = (id(e.sem), e.count)
        self._commit(token, reads, writes)
        return token

    def dma(self, q, out, in_, reads=(), writes=(), acc_writes=(), **kw):
        e = self.engs[q]
        pool = self.dma_pool[q]
        lst, idx = pool
        ent = lst[idx % len(lst)]
        pool[1] = idx + 1
        s, total = ent
        deps = self._collect(reads, writes, acc_writes)
        deps.append((id(s), total))
        import os
        if os.environ.get("DMA_SERIAL") and getattr(self, "_last_dma", {}).get(q):
            deps.append(self._last_dma[q])
        self._wait(e, deps)
        ins = e.h.dma_start(out=out, in_=in_, **kw)
        ins.then_inc(s, 16)
        ent[1] = total + 16
        self.ninstr += 1
        token = (id(s), total + 16)
        if not hasattr(self, "_last_dma"):
            self._last_dma = {}
        self._last_dma[q] = token
        self._commit(token, reads, writes, acc_writes)
        return token

    def init_memory(self, nbytes=196608):
        nc = self.nc
        with self.scope():
            n = nbytes // 4
            big = self.sbuf("initbig", [128, n], F32)
            q = n // 4
            self.op("dve", lambda: nc.vector.memset(big[:, 0:q], 0.0), writes=[])
            self.op("pool", lambda: nc.gpsimd.memset(big[:, q:2 * q], 0.0), writes=[])
            self.op("dve", lambda: nc.vector.memset(big[:, 2 * q:3 * q], 0.0), writes=[])
            self.op("pool", lambda: nc.gpsimd.memset(big[:, 3 * q:n], 0.0), writes=[])
            pss = [self.psum("initps%d" % i, [128, 512]) for i in range(8)]
            for p in pss:
                self.op("dve", lambda p=p: nc.vector.memset(p[:], 0.0), writes=[])

    def barrier(self):
        allt = []
        for e in self.engs.values():
            if e.count:
                allt.append((id(e.sem), e.count))
        for q, (lst, _) in self.dma_pool.items():
            for s, total in lst:
                if total:
                    allt.append((id(s), total))
        for e in self.engs.values():
            self._wait(e, allt)

    def finish(self, tokens_bufs):
        e = self.engs["sp"]
        deps = []
        for b in tokens_bufs:
            deps += b.w
        self._wait(e, deps)

T = 4352
TL = 4096

D = 1024
TL = 4096
TC = 256
T = TL + TC
NT = T // 128
BLKS = [(i * 512, 512) for i in range(8)] + [(4096, 256)]
DIN = 10528
ALPHA = (2.0 * 2) ** 0.25

COLS = dict(a_q=(0, 256), a_k=(256, 256), a_v=(512, 512), a_g=(1024, 512), a_lr=(1536, 32),
            b_q=(1568, 512), b_k=(2080, 512), b_v=(2592, 512),
            c_q=(3104, 512), c_k=(3616, 128), c_v=(3744, 128),
            d_q=(3872, 512), d_f=(4384, 1024), d_i=(5408, 512), d_g=(5920, 512), gate=(6432, 4096))


def blkcol(t0):
    return 1 if t0 >= TL else 0


class Ctx:
    pass


def declare_inputs(cx, nc):
    def din(name, shape, dt=F32):
        return Buf(nc.dram_tensor(name, list(shape), dt, kind="ExternalInput"), name)
    cx.xin = din("xin", [D, T])
    cx.cvec = din("cvec", [128, 8, 2])
    cx.w_mod = din("w_mod", [2, D, 6 * D])
    cx.b_modT = din("b_modT", [2, 128, 48])
    cx.w_in = din("w_in", [2, D, DIN])
    cx.rope_cos = din("rope_cos", [128, T])
    cx.rope_sin = din("rope_sin", [128, T])
    cx.rope_pt = din("rope_pt", [128, 128])
    cx.hg_gamma = din("hg_gamma", [2, 2, 512])
    cx.gla_lr_w = din("gla_lr_w", [2, 2, 16, 256])
    cx.gla_lr_b = din("gla_lr_b", [2, 2, 256])
    for nm in ("mg", "mh"):
        setattr(cx, nm + "_mq", din(nm + "_mq", [128, 2, 130]))
        setattr(cx, nm + "_mk", din(nm + "_mk", [128, 2, 128]))
    cx.m_ma = din("m_ma", [128, 2, 128])
    cx.m_ident = din("m_ident", [128, 128])
    cx.m_mau = din("m_mau", [128, 2, 128], U32)
    cx.m_wa = din("m_wa", [128, 2, 128])
    cx.wa_sink = din("wa_sink", [2, 8])
    cx.m_navalid = din("m_navalid", [128, 21, 128])
    cx.w_branch = din("w_branch", [2, 4, 512, 1024])
    cx.w_out = din("w_out", [2, 1024, 1024])
    cx.ln_gT = din("ln_gT", [2, 2, 128, 8])
    cx.ln_bT = din("ln_bT", [2, 2, 128, 8])
    cx.moe_w_group = din("moe_w_group", [2, 1024, 4])
    cx.moe_b_group = din("moe_b_group", [2, 4])
    cx.moe_w_expert = din("moe_w_expert", [2, 1024, 32])
    cx.moe_b_expert = din("moe_b_expert", [2, 32])
    cx.moe_w_gate = din("moe_w_gate", [2, 4, 8, 1024, 256])
    cx.moe_w_up = din("moe_w_up", [2, 4, 8, 1024, 256])
    cx.moe_w_down = din("moe_w_down", [2, 4, 8, 256, 1024])
    cx.m_sel = din("m_sel", [32, 32, 128])
    cx.m_ident32 = din("m_ident32", [128, 128])
    cx.na_bias = din("na_bias", [2, 8, 128, 21, 128])


def declare_scratch(cx, kb, debug, as_input=(), as_output=()):
    kind = "ExternalOutput" if debug else "Internal"

    def ds(name, shape, dt):
        b = kb.dram(name, shape, dt, kind=("ExternalInput" if name in as_input else ("ExternalOutput" if name in as_output else kind)))
        setattr(cx, name, b)
        return b
    ds("XT", [D, T], F32)
    ds("QA", [256, T], BF16)
    ds("GA", [512, T], BF16)
    ds("LR1", [33, T], F32)
    ds("QB", [512, T], BF16)
    ds("KB_", [512, T], BF16)
    ds("QC", [512, T], BF16)
    ds("KC", [128, T], BF16)
    ds("QD", [512, T], BF16)
    ds("GD", [512, T], BF16)
    ds("GATE", [4096, T], BF16)
    ds("KVA_t", [128, NT, 768], BF16)
    ds("VB_t", [128, NT, 512], BF16)
    ds("VC_t", [128, NT, 128], BF16)
    ds("VD_t", [128, NT, 512], BF16)
    ds("LGD_t", [128, NT, 1024], F32)
    ds("KD_t", [128, NT, 1024], BF16)
    ds("LGA_t", [128, NT, 512], F32)
    ds("MODT", [2, 128, 96], F32)
    ds("HX2", [D, T], BF16)
    ds("WGB", [32, 128, 2048], BF16)
    ds("WUB", [32, 128, 2048], BF16)
    ds("WDB", [4, 128, 16384], BF16)
    ds("WG_t", [128, NT, 32], F32)
    ds("WGT", [32, T], F32)
    ds("BR_A", [512, T], BF16)
    ds("BR_B", [512, T], BF16)
    ds("BR_C", [512, T], BF16)
    ds("BR_D", [512, T], BF16)


def stage_mod(cx, kb, nc, modT):
    with kb.scope():
        cv = kb.sbuf("cv", [128, 8, 2], F32)
        sc = kb.sbuf("sc", [128, 8, 2], F32)
        kb.dma("sp", cv[:], cx.cvec[:], reads=[cx.cvec], writes=[cv])
        kb.op("act", lambda: nc.scalar.activation(out=sc[:], in_=cv[:], func=AF.Silu), reads=[cv], writes=[sc])
        wbufs = [kb.sbuf("wm%d" % i, [128, 8, 1024], F32) for i in range(2)]
        bm = kb.sbuf("bm", [128, 48], F32)
        ps = kb.psum("ps_mod", [128, 96])
        wi = 0
        for l in range(2):
            kb.dma("sp", bm[:], cx.b_modT[l], reads=[cx.b_modT], writes=[bm])
            for g in range(6):
                wb = wbufs[wi % 2]
                wi += 1
                src = cx.w_mod[l, :, g * 1024:(g + 1) * 1024].rearrange("(kc p) c -> p kc c", p=128)
                kb.dma("sp", wb[:], src, reads=[cx.w_mod], writes=[wb])
                for j in range(8):
                    jj = g * 8 + j
                    fns = []
                    for kc in range(8):
                        fns.append(lambda kc=kc, j=j, jj=jj, wb=wb: nc.tensor.matmul(
                            ps[:, 2 * jj:2 * jj + 2], lhsT=wb[:, kc, j * 128:(j + 1) * 128], rhs=sc[:, kc, :],
                            start=(kc == 0), stop=(kc == 7)))
                    kb.mm(fns, reads=[wb, sc], writes=[ps])
            m = modT[l]
            kb.op("dve", lambda m=m: nc.vector.tensor_tensor(
                out=m[:], in0=ps[:].rearrange("p (j c) -> p j c", c=2),
                in1=bm[:].unsqueeze(2).broadcast_to([128, 48, 2]), op=ALU.add),
                reads=[ps, bm], writes=[m])
            kb.dma("sp", cx.MODT[l], m[:].rearrange("p j c -> p (j c)"), reads=[m], writes=[cx.MODT])


def stage_modulate(cx, kb, nc, src_dram, modv, shift_idx, scale_idx, hxT, hx32=None):
    with kb.scope():
        onep = kb.sbuf("onep", [128, 8, 2], F32)
        kb.op("dve", lambda: nc.vector.tensor_scalar(
            out=onep[:], in0=modv[:, scale_idx * 8:(scale_idx + 1) * 8, :], scalar1=1.0, scalar2=None, op0=ALU.add),
            reads=[modv], writes=[onep])
        xb = [kb.sbuf("xb%d" % i, [128, 8, 512], F32) for i in range(2)]
        for bi, (t0, nb) in enumerate(BLKS):
            col = blkcol(t0)
            x = xb[bi % 2]
            kb.dma("sp", x[:, :, :nb], src_dram[:, t0:t0 + nb].rearrange("(kc p) t -> p kc t", p=128),
                   reads=[src_dram], writes=[x])
            for kc in range(8):
                eng = "dve" if kc % 2 == 0 else "pool"
                h = nc.vector if eng == "dve" else nc.gpsimd
                kb.op(eng, lambda h=h, kc=kc, x=x: h.tensor_scalar(
                    out=hxT[:, kc, t0:t0 + nb], in0=x[:, kc, :nb],
                    scalar1=onep[:, kc, col:col + 1], scalar2=modv[:, shift_idx * 8 + kc, col:col + 1],
                    op0=ALU.mult, op1=ALU.add), reads=[x, onep, modv], writes=[hxT])


FM_GROUPS = [
    ("a_q", "QA", "copy"), ("a_g", "GA", "silu"), ("a_lr", "LR1", "copy32"),
    ("b_q", "QB", "copy"), ("b_k", "KB_", "copy"),
    ("c_q", "QC", "rope"), ("c_k", "KC", "rope"),
    ("d_q", "QD", "silu"), ("d_g", "GD", "silu"), ("gate", "GATE", "sigmoid"),
]


def stage_inproj_fm(cx, kb, nc, l, hxT, only=None):
    with kb.scope():
        wsl = [kb.sbuf("wsl%d" % i, [128, 8, 512], BF16) for i in range(2)]
        stg = [kb.sbuf("stg%d" % i, [128, T], BF16) for i in range(2)]
        stg32 = kb.sbuf("stg32", [32, T], F32)
        pss = [kb.psum("psf%d" % i, [128, 512]) for i in range(4)]
        psr = [kb.psum("psr%d" % i, [128, 512]) for i in range(2)]
        cosT = kb.sbuf("cosT", [128, T], F32)
        sinT = kb.sbuf("sinT", [128, T], F32)
        ptm = kb.sbuf("ptm", [128, 128], BF16)
        tmpb = [kb.sbuf("tmpb%d" % i, [128, 512], BF16) for i in range(2)]
        t1 = [kb.sbuf("t1_%d" % i, [128, 512], F32) for i in range(2)]
        t2 = [kb.sbuf("t2_%d" % i, [128, 512], F32) for i in range(2)]
        ones = kb.sbuf("ones1", [1, T], F32)
        kb.dma("sp", cosT[:], cx.rope_cos[:], reads=[cx.rope_cos], writes=[cosT])
        kb.dma("sp", sinT[:], cx.rope_sin[:], reads=[cx.rope_sin], writes=[sinT])
        kb.dma("pool", ptm[:], cx.rope_pt[:], reads=[cx.rope_pt], writes=[ptm])
        kb.op("dve", lambda: nc.vector.memset(ones[:], 1.0), writes=[ones])
        kb.dma("sp", cx.LR1[32:33, :], ones[:], reads=[ones], writes=[], acc_writes=[cx.LR1])
        nsl = 0
        nps = 0
        nst = 0
        nrp = 0
        for (name, dest, kind) in FM_GROUPS:
            if only is not None and name not in only:
                continue
            c0, nc_ = COLS[name]
            dbuf = getattr(cx, dest)
            for s0 in range(0, nc_, 512):
                sw = min(512, nc_ - s0)
                w = wsl[nsl % 2]
                nsl += 1
                kb.dma("pool", w[:, :, :sw],
                       cx.w_in[l, :, c0 + s0:c0 + s0 + sw].rearrange("(kc p) c -> p kc c", p=128),
                       reads=[cx.w_in], writes=[w])
                for m0 in range(0, sw, 128):
                    mw = min(128, sw - m0)
                    if kind == "copy32":
                        st = stg32
                    else:
                        st = stg[nst % 2]
                        nst += 1
                    for bi, (t0, nb) in enumerate(BLKS):
                        ps = pss[nps % 4]
                        nps += 1
                        fns = [(lambda kc=kc, ps=ps, w=w: nc.tensor.matmul(
                            ps[:mw, :nb], lhsT=w[:, kc, m0:m0 + mw], rhs=hxT[:, kc, t0:t0 + nb],
                            start=(kc == 0), stop=(kc == 7))) for kc in range(8)]
                        kb.mm(fns, reads=[w, hxT], writes=[ps])
                        o = st[:mw, t0:t0 + nb]
                        if kind in ("copy", "copy32"):
                            if nps % 2 == 0:
                                kb.op("dve", lambda ps=ps, o=o: nc.vector.tensor_copy(out=o, in_=ps[:mw, :nb]),
                                      reads=[ps], writes=[st])
                            else:
                                kb.op("act", lambda ps=ps, o=o: nc.scalar.copy(out=o, in_=ps[:mw, :nb]),
                                      reads=[ps], writes=[st])
                        elif kind == "silu":
                            kb.op("act", lambda ps=ps, o=o: nc.scalar.activation(out=o, in_=ps[:mw, :nb], func=AF.Silu),
                                  reads=[ps], writes=[st])
                        elif kind == "sigmoid":
                            kb.op("act", lambda ps=ps, o=o: nc.scalar.activation(out=o, in_=ps[:mw, :nb], func=AF.Sigmoid),
                                  reads=[ps], writes=[st])
                        elif kind == "rope":
                            tb = tmpb[nrp % 2]
                            a1 = t1[nrp % 2]
                            a2 = t2[nrp % 2]
                            pr = psr[nrp % 2]
                            nrp += 1
                            kb.op("act", lambda ps=ps, tb=tb: nc.scalar.copy(out=tb[:mw, :nb], in_=ps[:mw, :nb]),
                                  reads=[ps], writes=[tb])
                            kb.mm([lambda pr=pr, tb=tb: nc.tensor.matmul(pr[:mw, :nb], lhsT=ptm[:mw, :mw], rhs=tb[:mw, :nb],
                                                                         start=True, stop=True)],
                                  reads=[ptm, tb], writes=[pr])
                            kb.op("dve", lambda tb=tb, a1=a1: nc.vector.tensor_tensor(
                                out=a1[:mw, :nb], in0=tb[:mw, :nb], in1=cosT[:mw, t0:t0 + nb], op=ALU.mult),
                                reads=[tb, cosT], writes=[a1])
                            kb.op("dve", lambda pr=pr, a2=a2: nc.vector.tensor_tensor(
                                out=a2[:mw, :nb], in0=pr[:mw, :nb], in1=sinT[:mw, t0:t0 + nb], op=ALU.mult),
                                reads=[pr, sinT], writes=[a2])
                            kb.op("pool", lambda a1=a1, a2=a2, o=o: nc.gpsimd.tensor_tensor(
                                out=o, in0=a1[:mw, :nb], in1=a2[:mw, :nb], op=ALU.add),
                                reads=[a1, a2], writes=[st])
                    kb.dma("sp", dbuf[s0 + m0:s0 + m0 + mw, :], st[:mw, :], reads=[st], writes=[], acc_writes=[dbuf])


TM_GROUPS = [
    (256, 512, "KVA_t", 0, "copy"), (768, 256, "KVA_t", 512, "copy"),
    (2592, 512, "VB_t", 0, "copy"), (3744, 128, "VC_t", 0, "copy"),
    (5408, 512, "VD_t", 0, "copy"),
    (4384, 512, None, 0, "hgf"), (4896, 512, None, 512, "hgf"),
]


def stage_inproj_tm(cx, kb, nc, l, hxT, lbt, omlt, only=None):
    with kb.scope():
        wsl = [kb.sbuf("wtl%d" % i, [128, 8, 512], BF16) for i in range(2)]
        pss = [kb.psum("pst%d" % i, [128, 512]) for i in range(4)]
        stb = [kb.sbuf("stb%d" % i, [128, 512], BF16) for i in range(4)]
        e1 = [kb.sbuf("e1_%d" % i, [128, 512], F32) for i in range(2)]
        r1 = [kb.sbuf("r1_%d" % i, [128, 512], F32) for i in range(2)]
        lg = [kb.sbuf("lg_%d" % i, [128, 512], F32) for i in range(2)]
        kk = [kb.sbuf("kk_%d" % i, [128, 512], BF16) for i in range(2)]
        nsl = nps = nst = nh = 0
        for (c0, ncol, dest, doff, kind) in TM_GROUPS:
            if only is not None and kind not in only:
                continue
            w = wsl[nsl % 2]
            nsl += 1
            kb.dma("pool", w[:, :, :ncol], cx.w_in[l, :, c0:c0 + ncol].rearrange("(kc p) c -> p kc c", p=128),
                   reads=[cx.w_in], writes=[w])
            for ti in range(NT):
                ts = slice(ti * 128, (ti + 1) * 128)
                ps = pss[nps % 4]
                nps += 1
                fns = [(lambda kc=kc, ps=ps, w=w: nc.tensor.matmul(
                    ps[:, :ncol], lhsT=hxT[:, kc, ts], rhs=w[:, kc, :ncol],
                    start=(kc == 0), stop=(kc == 7))) for kc in range(8)]
                kb.mm(fns, reads=[w, hxT], writes=[ps])
                if kind == "copy":
                    st = stb[nst % 4]
                    nst += 1
                    if nst % 2 == 0:
                        kb.op("dve", lambda ps=ps, st=st: nc.vector.tensor_copy(out=st[:, :ncol], in_=ps[:, :ncol]),
                              reads=[ps], writes=[st])
                    else:
                        kb.op("act", lambda ps=ps, st=st: nc.scalar.copy(out=st[:, :ncol], in_=ps[:, :ncol]),
                              reads=[ps], writes=[st])
                    db = getattr(cx, dest)
                    kb.dma("sp", db[:, ti, doff:doff + ncol], st[:, :ncol], reads=[st], writes=[], acc_writes=[db])
                else:
                    e = e1[nh % 2]
                    r = r1[nh % 2]
                    g = lg[nh % 2]
                    k = kk[nh % 2]
                    nh += 1
                    cs = slice(doff, doff + 512)
                    kb.op("act", lambda ps=ps, r=r: nc.scalar.activation(out=r[:], in_=ps[:], func=AF.Sigmoid),
                          reads=[ps], writes=[r])
                    kb.op("dve", lambda r=r: nc.vector.tensor_tensor(out=r[:], in0=r[:], in1=omlt[:, cs], op=ALU.mult),
                          reads=[r, omlt], writes=[r])
                    kb.op("pool", lambda r=r: nc.gpsimd.tensor_tensor(out=r[:], in0=r[:], in1=lbt[:, cs], op=ALU.add),
                          reads=[r, lbt], writes=[r])
                    kb.op("act", lambda r=r, g=g: nc.scalar.activation(out=g[:], in_=r[:], func=AF.Ln),
                          reads=[r], writes=[g])
                    kb.op("pool", lambda r=r, k=k: nc.gpsimd.tensor_scalar(
                        out=k[:], in0=r[:], scalar1=-1.0, scalar2=1.0, op0=ALU.mult, op1=ALU.add),
                        reads=[r], writes=[k])
                    kb.dma("sp", cx.LGD_t[:, ti, cs], g[:], reads=[g], writes=[], acc_writes=[cx.LGD_t])
                    kb.dma("sp", cx.KD_t[:, ti, cs], k[:], reads=[k], writes=[], acc_writes=[cx.KD_t])


def stage_lb(cx, kb, nc, lbt, omlt):
    with kb.scope():
        gam = kb.sbuf("gam", [128, 2, 2, 512], F32)
        kb.dma("sp", gam[:].rearrange("p a b c -> p (a b c)"),
               cx.hg_gamma[:].rearrange("a b c -> (a b c)").partition_broadcast(128),
               reads=[cx.hg_gamma], writes=[gam])
        d = kb.sbuf("gamd", [128, 2, 512], F32)
        kb.op("dve", lambda: nc.vector.tensor_tensor(out=d[:], in0=gam[:, :, 0, :], in1=gam[:, :, 1, :], op=ALU.subtract),
              reads=[gam], writes=[d])
        kb.op("act", lambda: nc.scalar.activation(out=d[:], in_=d[:], func=AF.Exp), reads=[d], writes=[d])
        l1 = lbt[1]
        kb.op("dve", lambda: nc.vector.tensor_scalar(
            out=d[:], in0=d[:], scalar1=1.0, scalar2=None, op0=ALU.add), reads=[d], writes=[d])
        kb.op("dve", lambda: nc.vector.reciprocal(out=l1[:], in_=d[:].rearrange("p a c -> p (a c)")),
              reads=[d], writes=[l1])
        l0 = lbt[0]
        kb.op("dve", lambda: nc.vector.memset(l0[:], 0.0), writes=[l0])
        for l in range(2):
            o = omlt[l]
            lb = lbt[l]
            kb.op("dve", lambda o=o, lb=lb: nc.vector.tensor_scalar(
                out=o[:], in0=lb[:], scalar1=-1.0, scalar2=1.0, op0=ALU.mult, op1=ALU.add),
                reads=[lb], writes=[o])


def stage_gla_gate(cx, kb, nc, l):
    with kb.scope():
        wlr = kb.sbuf("wlr", [33, 512], F32)
        lr1 = kb.sbuf("lr1", [33, T], F32)
        kb.op("dve", lambda: nc.vector.memset(wlr[:], 0.0), writes=[wlr])
        kb.dma("sp", wlr[0:16, 0:256], cx.gla_lr_w[l, 0], reads=[cx.gla_lr_w], writes=[wlr])
        kb.dma("sp", wlr[16:32, 256:512], cx.gla_lr_w[l, 1], reads=[cx.gla_lr_w], writes=[wlr])
        kb.dma("sp", wlr[32:33, :], cx.gla_lr_b[l].rearrange("a c -> (a c)").unsqueeze(0),
               reads=[cx.gla_lr_b], writes=[wlr])
        kb.dma("sp", lr1[:], cx.LR1[:], reads=[cx.LR1], writes=[lr1])
        pss = [kb.psum("psg%d" % i, [128, 512]) for i in range(2)]
        ee = [kb.sbuf("ge%d" % i, [128, 512], F32) for i in range(2)]
        gg = [kb.sbuf("gg%d" % i, [128, 512], F32) for i in range(2)]
        for ti in range(NT):
            ts = slice(ti * 128, (ti + 1) * 128)
            ps = pss[ti % 2]
            e = ee[ti % 2]
            g = gg[ti % 2]
            kb.mm([lambda ps=ps: nc.tensor.matmul(ps[:], lhsT=lr1[:, ts], rhs=wlr[:], start=True, stop=True)],
                  reads=[lr1, wlr], writes=[ps])
            kb.op("act", lambda ps=ps, e=e: nc.scalar.activation(out=e[:], in_=ps[:], func=AF.Exp, scale=-1.0),
                  reads=[ps], writes=[e])
            kb.op("act", lambda e=e, g=g: nc.scalar.activation(out=g[:], in_=e[:], func=AF.Ln, bias=1.0),
                  reads=[e], writes=[g])
            kb.dma("sp", cx.LGA_t[:, ti, :], g[:], reads=[g], writes=[], acc_writes=[cx.LGA_t])

SCAN_CFG = {
    "gla": dict(H=4, dk=64, qsrc="QA", qscale=0.125, ksrc="KVA_t", kz=0, kcol0=0, vsrc="KVA_t", vcol0=256,
                gsrc="LGA_t", gz=256, gate="GA", dest="BR_A", mask="mg"),
    "hgrn": dict(H=4, dk=128, qsrc="QD", qscale=1.0, ksrc="KD_t", kz=512, kcol0=0, vsrc="VD_t", vcol0=0,
                 gsrc="LGD_t", gz=512, gate="GD", dest="BR_D", mask="mh"),
}


def view(b):
    return Buf(b.t, b.name + "_v")


def stage_scan(cx, kb, nc, which, heads=None):
    cfg = SCAN_CFG[which]
    H, dk = cfg["H"], cfg["dk"]
    dv = 128
    qsrc, ksrc, vsrc, gsrc = (getattr(cx, cfg[k]) for k in ("qsrc", "ksrc", "vsrc", "gsrc"))
    gate, dest = getattr(cx, cfg["gate"]), getattr(cx, cfg["dest"])
    mq_d, mk_d = getattr(cx, cfg["mask"] + "_mq"), getattr(cx, cfg["mask"] + "_mk")
    with kb.scope():
        mq = kb.sbuf("mq", [128, 2, 130], F32)
        mk = kb.sbuf("mk", [128, 2, 128], F32)
        ma = kb.sbuf("ma", [128, 2, 128], F32)
        ident = kb.sbuf("ident", [128, 128], BF16)
        onesdv = kb.sbuf("onesdv", [128, 128], F32)
        kb.dma("sp", mq[:], mq_d[:], reads=[mq_d], writes=[mq])
        kb.dma("sp", mk[:], mk_d[:], reads=[mk_d], writes=[mk])
        kb.dma("sp", ma[:], cx.m_ma[:], reads=[cx.m_ma], writes=[ma])
        mau = kb.sbuf("mau", [128, 2, 128], U32)
        kb.dma("sp", mau[:], cx.m_mau[:], reads=[cx.m_mau], writes=[mau])
        kb.dma("pool", ident[:], cx.m_ident[:], reads=[cx.m_ident], writes=[ident])
        kb.op("dve", lambda: nc.vector.memset(onesdv[:], 1.0 / dv), writes=[onesdv])
        vt = kb.sbuf("vt", [128, NT, dv], BF16)
        qT = kb.sbuf("qT", [128, T], BF16)
        gsil = kb.sbuf("gsil", [128, T], BF16)
        obr = kb.sbuf("obr", [128, T], BF16)
        gts = [kb.sbuf("gt%d" % i, [128, NT, dk], F32) for i in range(2)]
        kts = [kb.sbuf("kt%d" % i, [128, NT, dk], BF16) for i in range(2)]
        qt = [kb.sbuf("qt%d" % z, [128, T], BF16) for z in range(2)]
        atm = [kb.sbuf("atm%d" % z, [128, NT, 128], BF16) for z in range(2)]
        Sp = [kb.sbuf("Sp%d" % z, [128, 2 * NT, dv], BF16) for z in range(2)]
        for z in range(2):
            kb.op("pool", lambda z=z: nc.gpsimd.memset(atm[z][:], 0.0), writes=[atm[z]])
        Sst = [kb.sbuf("Sst%d" % i, [128, dv], F32) for i in range(2)]
        R = 3
        eq = [kb.sbuf("eq%d" % i, [128, 128], F32) for i in range(R)]
        ek = [kb.sbuf("ek%d" % i, [128, 128], F32) for i in range(R)]
        ekh = [kb.sbuf("ekh%d" % i, [128, 128], F32) for i in range(R)]
        ktl = [kb.sbuf("ktl%d" % i, [128, 128], BF16) for i in range(R)]
        khat = [kb.sbuf("khat%d" % i, [128, 256], BF16) for i in range(R)]
        ktr = [kb.sbuf("ktr%d" % i, [128, 128], F32) for i in range(R)]
        atr = [kb.sbuf("atr%d" % i, [128, 128], F32) for i in range(R)]
        usb = [kb.sbuf("usb%d" % i, [128, 256], F32) for i in range(R)]
        osb = [kb.sbuf("osb%d" % i, [128, 512], F32) for i in range(2)]
        cm = kb.sbuf("cm", [128, 2], F32)
        kb.op("dve", lambda: nc.vector.memset(cm[:], 0.0), writes=[cm])
        kb.op("dve", lambda: nc.vector.memset(cm[0:64, 0:1], 1.0), writes=[cm])
        kb.op("dve", lambda: nc.vector.memset(cm[64:128, 1:2], 1.0), writes=[cm])
        er2 = [kb.sbuf("er%d" % i, [128, 2], F32) for i in range(R)]
        ed2 = [kb.sbuf("ed%d" % i, [128, 2], F32) for i in range(R)]
        psA = [kb.psum("psA%d" % i, [128, 512]) for i in range(2)]
        psB = [kb.psum("psB%d" % i, [128, 512]) for i in range(2)]
        psA_ec = [view(p) for p in psA]
        import os
        if True:
            psK = [kb.psum("psK%d" % i, [128, 512]) for i in range(2)]
        else:
            psK = psA
        psA_kt = [view(p) for p in psK]
        psA_e2 = [view(p) for p in psK]
        psB_u = [view(p) for p in psB]
        psB_at = [view(p) for p in psB]
        psO = [kb.psum("psO%d" % i, [128, 512]) for i in range(1)] * 2
        psM = [kb.psum("psM%d" % i, [128, 512]) for i in range(1)] * 2
        sq = [kb.sbuf("sq%d" % i, [128, 512], F32) for i in range(2)]
        sd = [kb.sbuf("sd%d" % i, [128, 512], F32) for i in range(2)]
        on = [kb.sbuf("on%d" % i, [128, 512], F32) for i in range(2)]
        eps_t = kb.sbuf("eps_t", [128, 1], F32)
        kb.op("dve", lambda: nc.vector.memset(eps_t[:], 1e-6), writes=[eps_t])

        it = 0
        ghz = 0
        import os
        for h in (range(H) if heads is None else heads):
            vc0 = cfg["vcol0"] + h * dv
            kb.dma("sp", vt[:], vsrc[:, :, vc0:vc0 + dv], reads=[vsrc], writes=[vt])
            kb.dma("sp", qT[:dk, :], qsrc[h * dk:(h + 1) * dk, :], reads=[qsrc], writes=[qT])
            kb.dma("sp", gsil[:], gate[h * dv:(h + 1) * dv, :], reads=[gate], writes=[gsil])
            for z in range(2):
                gt = gts[ghz % 2]
                kt = kts[ghz % 2]
                ghz += 1
                gc0 = z * cfg["gz"] + h * dk
                kc0 = cfg["kcol0"] + z * cfg["kz"] + h * dk
                kb.dma("sp", gt[:], gsrc[:, :, gc0:gc0 + dk], reads=[gsrc], writes=[gt])
                if os.environ.get("KTMEMSET"):
                    kb.op("dve", lambda kt=kt: nc.vector.memset(kt[:], 0.0), writes=[kt])
                kb.dma("sp", kt[:], ksrc[:, :, kc0:kc0 + dk], reads=[ksrc], writes=[kt])
                order = [32, 33] + list(range(32)) if z == 0 else [33, 32] + list(range(31, -1, -1))
                import os
                DBG = os.environ.get("SCAN_DBG", "")
                if DBG == "loads":
                    continue
                if DBG.startswith("nt"):
                    order = order[:int(DBG[2:])]
                S = Sst[0]
                kb.op("dve", lambda S=S: nc.vector.memset(S[:], 0.0), writes=[S])
                si = 0
                def p1(ti, a, r):
                    ts = slice(ti * 128, (ti + 1) * 128)
                    pa, pb = psA[a], psB[a]
                    pk = psK[a]
                    kb.mm([lambda pa=pa, gt=gt: nc.tensor.matmul(pa[:dk, 0:130], lhsT=gt[:, ti, :], rhs=mq[:, z, :],
                                                                 start=True, stop=True)],
                          reads=[gt, mq], writes=[psA_ec[a]])
                    kb.mm([lambda pk=pk, kt=kt: nc.tensor.matmul(pk[:dk, 256:384], lhsT=(ident[:, :dk] if os.environ.get("A2ID") else kt[:, ti, :]), rhs=ident[:],
                                                                 start=True, stop=True)],
                          reads=[kt, ident], writes=[psA_kt[a]])
                    kb.mm([lambda pk=pk, gt=gt: nc.tensor.matmul(pk[:, 384:384 + dk], lhsT=mk[:, z, :], rhs=gt[:, ti, :],
                                                                 start=True, stop=True)],
                          reads=[gt, mk], writes=[psA_e2[a]])
                    kb.op("act", lambda pa=pa, o=eq[r]: nc.scalar.activation(out=o[:dk, :], in_=pa[:dk, 0:128], func=AF.Exp),
                          reads=[psA_ec[a]], writes=[eq[r]])
                    kb.op("act", lambda pa=pa, o=ek[r]: nc.scalar.activation(out=o[:dk, :], in_=pa[:dk, 0:128], func=AF.Exp, scale=-1.0),
                          reads=[psA_ec[a]], writes=[ek[r]])
                    kb.op("act", lambda pa=pa, o=er2[r]: nc.scalar.activation(out=o[:dk, :], in_=pa[:dk, 128:130], func=AF.Exp, scale=0.5),
                          reads=[psA_ec[a]], writes=[er2[r]])
                    kb.op("act", lambda pa=pa, o=ed2[r]: nc.scalar.activation(out=o[:dk, :], in_=pa[:dk, 128:130], func=AF.Exp),
                          reads=[psA_ec[a]], writes=[ed2[r]])
                    kb.op("act", lambda pk=pk, o=ekh[r]: nc.scalar.activation(out=o[:, :dk], in_=pk[:, 384:384 + dk], func=AF.Exp),
                          reads=[psA_e2[a]], writes=[ekh[r]])
                    qz = qt[z]
                    kb.op("dve", lambda qz=qz, e=eq[r]: nc.vector.scalar_tensor_tensor(
                        out=qz[:dk, ts], in0=qT[:dk, ts], scalar=cfg["qscale"], in1=e[:dk, :], op0=ALU.mult, op1=ALU.mult),
                        reads=[qT, eq[r]], writes=[], acc_writes=[qz])
                    kb.op("act", lambda pk=pk, o=ktr[r]: nc.scalar.copy(out=o[:dk, :], in_=pk[:dk, 256:384]),
                          reads=[psA_kt[a]], writes=[ktr[r]])
                    kb.op("dve", lambda e=ek[r], o=ktl[r], kr=ktr[r]: nc.vector.tensor_tensor(
                        out=o[:dk, :], in0=kr[:dk, :], in1=e[:dk, :], op=ALU.mult),
                        reads=[ktr[r], ek[r]], writes=[ktl[r]])
                    for c in range(2):
                        kb.op("dve", lambda kt=kt, e=ekh[r], o=khat[r], c=c: nc.vector.scalar_tensor_tensor(
                            out=o[:, c * 128:c * 128 + dk], in0=kt[:, ti, :], scalar=cm[:, c:c + 1], in1=e[:, :dk],
                            op0=ALU.mult, op1=ALU.mult),
                            reads=[kt, ekh[r], cm], writes=[], acc_writes=[khat[r]])
                    return None

                def p2(ti, a, r, S, si):
                    ts = slice(ti * 128, (ti + 1) * 128)
                    pa, pb = psA[a], psB[a]
                    qz = qt[z]
                    kb.mm([(lambda c=c, pb=pb, kh=khat[r]: nc.tensor.matmul(
                        pb[:dk, c * 128:(c + 1) * 128], lhsT=kh[:, c * 128:c * 128 + dk],
                        rhs=vt[:, ti, :], start=True, stop=True)) for c in range(2)],
                        reads=[khat[r], vt], writes=[psB_u[a]])
                    kb.mm([lambda pb=pb, k_=ktl[r], qz=qz: nc.tensor.matmul(
                        pb[:, 256:384], lhsT=k_[:dk, :], rhs=qz[:dk, ts], start=True, stop=True)],
                        reads=[ktl[r], qz], writes=[psB_at[a]])
                    az = atm[z]
                    kb.op("act", lambda pb=pb, o=atr[r]: nc.scalar.copy(out=o[:], in_=pb[:, 256:384]),
                          reads=[psB_at[a]], writes=[atr[r]])
                    kb.op("dve", lambda az=az, o=atr[r]: nc.vector.copy_predicated(
                        out=az[:, ti, :], mask=mau[:, z, :], data=o[:]),
                        reads=[atr[r], mau], writes=[], acc_writes=[az])
                    kb.op("act", lambda pb=pb, o=usb[r]: nc.scalar.copy(out=o[:dk, :], in_=pb[:dk, 0:256]),
                          reads=[psB_u[a]], writes=[usb[r]])
                    spz = Sp[z]
                    for c in ((0, 1) if z == 0 else (1, 0)):
                        ci = 2 * ti + c
                        kb.op("act", lambda S=S, spz=spz, ci=ci, e=er2[r], c=c: nc.scalar.activation(
                            out=spz[:dk, ci, :], in_=S[:dk, :], func=AF.Copy, scale=e[:dk, c:c + 1]),
                            reads=[S, er2[r]], writes=[], acc_writes=[spz])
                        S2 = Sst[(si + 1) % 2]
                        si += 1
                        kb.op("dve", lambda S=S, S2=S2, e=ed2[r], u=usb[r], c=c: nc.vector.scalar_tensor_tensor(
                            out=S2[:dk, :], in0=S[:dk, :], scalar=e[:dk, c:c + 1], in1=u[:dk, c * 128:(c + 1) * 128],
                            op0=ALU.mult, op1=ALU.add),
                            reads=[S, ed2[r], usb[r]], writes=[S2])
                        S = S2
                    return S, si

                sched = []
                for ti in order:
                    sched.append((ti, it % 2, it % R))
                    it += 1
                for idx in range(len(sched) + 1):
                    if idx < len(sched):
                        p1(*sched[idx])
                    if idx >= 1:
                        S, si = p2(*sched[idx - 1], S, si)
            groups = [list(range(g * 4, g * 4 + 4)) for g in range(8)] + [[32, 33]]
            if DBG:
                groups = []
            for gi, tiles in enumerate(groups):
                po = psO[gi % 2]
                pm = psM[gi % 2]
                nb = len(tiles) * 128
                t0 = tiles[0] * 128
                fns = []
                for k_, ti in enumerate(tiles):
                    cs = k_ * 128
                    for z in range(2):
                        fns.append(lambda po=po, ti=ti, cs=cs, z=z: nc.tensor.matmul(
                            po[:, cs:cs + 128], lhsT=vt[:, ti, :], rhs=atm[z][:, ti, :], start=(z == 0), stop=False))
                        for c in range(2):
                            fns.append(lambda po=po, ti=ti, cs=cs, z=z, c=c: nc.tensor.matmul(
                                po[:, cs + c * 64:cs + (c + 1) * 64], lhsT=Sp[z][:dk, 2 * ti + c, :],
                                rhs=qt[z][:dk, ti * 128 + c * 64:ti * 128 + (c + 1) * 64],
                                start=False, stop=(z == 1)))
                kb.mm(fns, reads=[vt, atm[0], atm[1], Sp[0], Sp[1], qt[0], qt[1]], writes=[po])
                s_, d_, o_ = sq[gi % 2], sd[gi % 2], on[gi % 2]
                kb.op("act", lambda po=po, s_=s_: nc.scalar.activation(out=s_[:, :nb], in_=po[:, :nb], func=AF.Square),
                      reads=[po], writes=[s_])
                kb.mm([lambda pm=pm, s_=s_: nc.tensor.matmul(pm[:, :nb], lhsT=onesdv[:], rhs=s_[:, :nb], start=True, stop=True)],
                      reads=[onesdv, s_], writes=[pm])
                kb.op("act", lambda pm=pm, d_=d_: nc.scalar.activation(out=d_[:, :nb], in_=pm[:, :nb], func=AF.Sqrt, bias=eps_t[:]),
                      reads=[pm, eps_t], writes=[d_])
                kb.op("dve", lambda d_=d_: nc.vector.reciprocal(out=d_[:, :nb], in_=d_[:, :nb]), reads=[d_], writes=[d_])
                ob_ = osb[gi % 2]
                kb.op("act", lambda po=po, ob_=ob_: nc.scalar.copy(out=ob_[:, :nb], in_=po[:, :nb]), reads=[po], writes=[ob_])
                kb.op("dve", lambda ob_=ob_, d_=d_, o_=o_: nc.vector.tensor_tensor(
                    out=o_[:, :nb], in0=ob_[:, :nb], in1=d_[:, :nb], op=ALU.mult), reads=[ob_, d_], writes=[o_])
                kb.op("pool", lambda o_=o_: nc.gpsimd.tensor_tensor(
                    out=obr[:, t0:t0 + nb], in0=o_[:, :nb], in1=gsil[:, t0:t0 + nb], op=ALU.mult),
                    reads=[o_, gsil], writes=[], acc_writes=[obr])
            kb.dma("sp", dest[h * dv:(h + 1) * dv, :], obr[:], reads=[obr], writes=[], acc_writes=[dest])


def scan_masks():
    j = np.arange(128)[:, None]
    i = np.arange(128)[None, :]
    same = (j // 64 == i // 64).astype(np.float32)
    out = {}
    for name, gs in (("mg", -1.0 / 16.0), ("mh", 1.0)):
        mq = np.zeros((128, 2, 130), np.float32)
        mk = np.zeros((128, 2, 128), np.float32)
        mq[:, 0, :128] = gs * same * ((j <= i).astype(np.float32) - 0.5)
        mq[:, 1, :128] = gs * same * ((j >= i).astype(np.float32) - 0.5)
        for z in range(2):
            for c in range(2):
                mq[:, z, 128 + c] = gs * (np.arange(128) // 64 == c)
        mk[:, 0, :] = gs * same * (j > i)
        mk[:, 1, :] = gs * same * (j < i)
        out[name + "_mq"] = mq
        out[name + "_mk"] = mk
    ma = np.zeros((128, 2, 128), np.float32)
    ma[:, 0, :] = same * (j <= i)
    ma[:, 1, :] = same * (j >= i)
    out["m_ma"] = ma
    out["m_mau"] = ma.astype(np.uint32)
    out["m_ident"] = np.eye(128, dtype=np.float32)
    return out


def stage_wa(cx, kb, nc, l, groups=None):
    with kb.scope():
        wm = kb.sbuf("wm", [128, 2, 128], BF16)
        kb.dma("pool", wm[:], cx.m_wa[:], reads=[cx.m_wa], writes=[wm])
        ones64 = kb.sbuf("ones64", [128, 64], BF16)
        kb.op("dve", lambda: nc.vector.memset(ones64[:], 1.0), writes=[ones64])
        sk = kb.sbuf("sk", [64, 8], F32)
        kb.dma("sp", sk[:], cx.wa_sink[l].partition_broadcast(64), reads=[cx.wa_sink], writes=[sk])
        kb.op("act", lambda: nc.scalar.activation(out=sk[:], in_=sk[:], func=AF.Exp), reads=[sk], writes=[sk])
        q4 = kb.sbuf("q4", [64, 4, T], BF16)
        kT = kb.sbuf("kTc", [64, T], BF16)
        vt = kb.sbuf("vtc", [128, NT, 64], BF16)
        ost = kb.sbuf("ost", [64, 4, T], BF16)
        pS = [kb.psum("pS%d" % i, [128, 512]) for i in range(4)]
        pO = [kb.psum("pO%d" % i, [128, 512]) for i in range(2)]
        pD = [kb.psum("pD%d" % i, [128, 512]) for i in range(2)]
        R = 10
        pt = [kb.sbuf("pt%d" % i, [128, 4, 128], BF16) for i in range(R)]
        ptm = [kb.sbuf("ptm%d" % i, [128, 4, 128], BF16) for i in range(R)]
        osb = [kb.sbuf("osbw%d" % i, [64, 4, 128], F32) for i in range(2)]
        dsb = [kb.sbuf("dsbw%d" % i, [64, 4, 128], F32) for i in range(2)]
        it = 0
        for g in (range(2) if groups is None else groups):
            kb.dma("sp", q4[:], cx.QC[g * 256:(g + 1) * 256, :].rearrange("(hh d) t -> d hh t", d=64),
                   reads=[cx.QC], writes=[q4])
            kb.dma("sp", kT[:], cx.KC[g * 64:(g + 1) * 64, :], reads=[cx.KC], writes=[kT])
            kb.dma("sp", vt[:], cx.VC_t[:, :, g * 64:(g + 1) * 64], reads=[cx.VC_t], writes=[vt])
            def p1(qb):
                nonlocal it
                if qb < 32:
                    keys = []
                    if qb > 0:
                        keys.append((qb - 1, 0))
                    keys.append((qb, None))
                    if qb < 31:
                        keys.append((qb + 1, 1))
                    keys += [(32, None), (33, None)]
                else:
                    keys = [(32, None), (33, None)]
                qs = slice(qb * 128, (qb + 1) * 128)
                st_ = []
                for ki, (kt_i, mi) in enumerate(keys):
                    ps = pS[it % 4]
                    p_ = pt[it % R]
                    pm_ = ptm[it % R]
                    it += 1
                    kb.mm([lambda ps=ps, kt_i=kt_i: nc.tensor.matmul(
                        ps[:, :], lhsT=kT[:, kt_i * 128:(kt_i + 1) * 128], rhs=q4[:, :, qs], start=True, stop=True)],
                        reads=[kT, q4], writes=[ps])
                    kb.op("act", lambda ps=ps, p_=p_: nc.scalar.activation(
                        out=p_[:].rearrange("p h q -> p (h q)"), in_=ps[:, :], func=AF.Exp, scale=0.125),
                        reads=[ps], writes=[p_])
                    src = p_
                    if mi is not None:
                        eng = "dve" if it % 2 == 0 else "pool"
                        h_ = nc.vector if eng == "dve" else nc.gpsimd
                        kb.op(eng, lambda h_=h_, p_=p_, pm_=pm_, mi=mi: h_.tensor_tensor(
                            out=pm_[:], in0=p_[:], in1=wm[:, mi, :].unsqueeze(1).broadcast_to([128, 4, 128]), op=ALU.mult),
                            reads=[p_, wm], writes=[pm_])
                        src = pm_
                    st_.append((kt_i, src))
                return (qb, st_)

            def p2(state):
                qb, st_ = state
                qs = slice(qb * 128, (qb + 1) * 128)
                a = qb % 2
                po, pd = pO[a], pD[a]
                nk = len(st_)
                for ki, (kt_i, src) in enumerate(st_):
                    kb.mm([lambda po=po, src=src, kt_i=kt_i, ki=ki: nc.tensor.matmul(
                        po[:64, :], lhsT=vt[:, kt_i, :], rhs=src[:].rearrange("p h q -> p (h q)"),
                        start=(ki == 0), stop=(ki == nk - 1)),
                        lambda pd=pd, src=src, ki=ki: nc.tensor.matmul(
                        pd[:64, :], lhsT=ones64[:], rhs=src[:].rearrange("p h q -> p (h q)"),
                        start=(ki == 0), stop=(ki == nk - 1))],
                        reads=[vt, src, ones64], writes=[po, pd])
                o_, d_ = osb[a], dsb[a]
                kb.op("act", lambda po=po, o_=o_: nc.scalar.copy(out=o_[:].rearrange("p h q -> p (h q)"), in_=po[:64, :]),
                      reads=[po], writes=[o_])
                kb.op("act", lambda pd=pd, d_=d_: nc.scalar.copy(out=d_[:].rearrange("p h q -> p (h q)"), in_=pd[:64, :]),
                      reads=[pd], writes=[d_])
                kb.op("dve", lambda d_=d_: nc.vector.tensor_tensor(
                    out=d_[:], in0=d_[:], in1=sk[:, g * 4:(g + 1) * 4].unsqueeze(2).broadcast_to([64, 4, 128]), op=ALU.add),
                    reads=[d_, sk], writes=[d_])
                kb.op("dve", lambda d_=d_: nc.vector.reciprocal(out=d_[:], in_=d_[:]), reads=[d_], writes=[d_])
                kb.op("pool", lambda o_=o_, d_=d_: nc.gpsimd.tensor_tensor(
                    out=ost[:, :, qs], in0=o_[:], in1=d_[:], op=ALU.mult),
                    reads=[o_, d_], writes=[], acc_writes=[ost])

            prev = None
            for qb in range(NT):
                cur = p1(qb)
                if prev is not None:
                    p2(prev)
                prev = cur
            p2(prev)
            kb.dma("sp", cx.BR_C[g * 256:(g + 1) * 256, :].rearrange("(hh d) t -> d hh t", d=64), ost[:],
                   reads=[ost], writes=[], acc_writes=[cx.BR_C])


def wa_masks():
    kk = np.arange(128)[:, None]
    qq = np.arange(128)[None, :]
    m = np.zeros((128, 2, 128), np.float32)
    m[:, 0, :] = (kk >= qq)
    m[:, 1, :] = (kk <= qq)
    return {"m_wa": m}


def na_patterns():
    W = 64
    tok = np.arange(128)
    lr, lc = tok // W, tok % W
    pats = []
    keys = []
    for m in range(32):
        qr = 2 * m + lr[None, :]
        qc = lc[None, :]
        rs = np.clip(qr - 4, 0, 56)
        cs = np.clip(qc - 8, 0, 48)
        lst = []
        for kt_i in range(32):
            kr = 2 * kt_i + lr[:, None]
            kc = lc[:, None]
            valid = (kr >= rs) & (kr < rs + 8) & (kc >= cs) & (kc < cs + 16)
            if not valid.any():
                continue
            ri = np.clip(kr - qr + 7, 0, 14)
            ci = np.clip(kc - qc + 15, 0, 30)
            if 2 <= m <= 29:
                pid = kt_i - m + 2
                if m == 2:
                    pats.append((valid, ri, ci))
            else:
                pid = len(pats)
                pats.append((valid, ri, ci))
            lst.append((kt_i, pid))
        keys.append(lst)
    return keys, pats


def na_consts():
    keys, pats = na_patterns()
    n0 = len(keys[0]) + len(keys[1])
    order = list(range(n0, n0 + 5)) + list(range(0, n0)) + list(range(n0 + 5, len(pats)))
    remap = {old: new for new, old in enumerate(order)}
    pats2 = [pats[o] for o in order]
    keys2 = []
    for m, lst in enumerate(keys):
        if 2 <= m <= 29:
            keys2.append(lst)
        else:
            keys2.append([(k, remap[p]) for (k, p) in lst])
    valid = np.stack([p[0] for p in pats2], 1).astype(np.float32)
    ri = np.stack([p[1] for p in pats2], 1)
    ci = np.stack([p[2] for p in pats2], 1)
    return keys2, valid, ri, ci


def stage_na(cx, kb, nc, l, heads=None, bg_factory=None):
    keys_tab, valid_np, _, _ = na_consts()
    npat = valid_np.shape[1]
    with kb.scope():
        vmask = kb.sbuf("vmask", [128, npat, 128], F32)
        kb.dma("sp", vmask[:], cx.m_navalid[:], reads=[cx.m_navalid], writes=[vmask])
        ones64 = kb.sbuf("ones64n", [128, 64], BF16)
        kb.op("dve", lambda: nc.vector.memset(ones64[:], 1.0), writes=[ones64])
        qT = kb.sbuf("qTn", [64, T], BF16)
        kT = kb.sbuf("kTn", [64, T], BF16)
        vt = kb.sbuf("vtn", [128, NT, 64], BF16)
        ost = kb.sbuf("ostn", [64, T], BF16)
        bias = kb.sbuf("biasn", [128, npat, 128], F32)
        EB = kb.sbuf("EBn", [128, npat, 128], BF16)
        pS = [kb.psum("pSn%d" % i, [128, 512]) for i in range(4)]
        pO = [kb.psum("pOn%d" % i, [128, 512]) for i in range(2)]
        pD = [kb.psum("pDn%d" % i, [128, 512]) for i in range(2)]
        R = 6
        pt = [kb.sbuf("ptn%d" % i, [128, 4, 128], BF16) for i in range(R)]
        osb = [kb.sbuf("osbn%d" % i, [64, 256], F32) for i in range(2)]
        it = 0
        bg = None
        if bg_factory is not None:
            stw = [kb.sbuf("pcw%d" % i, [128, 8, 256], BF16) for i in range(4)]
            std = [kb.sbuf("pcd%d" % i, [128, 16, 1024], BF16) for i in range(2)]
            bg = bg_factory(stw, std)
        for h in (range(8) if heads is None else heads):
            kb.dma("sp", bias[:], cx.na_bias[l, h], reads=[cx.na_bias], writes=[bias])
            kb.op("act", lambda: nc.scalar.activation(out=bias[:], in_=bias[:], func=AF.Exp), reads=[bias], writes=[bias])
            kb.op("pool", lambda: nc.gpsimd.tensor_tensor(out=EB[:], in0=bias[:], in1=vmask[:], op=ALU.mult),
                  reads=[bias, vmask], writes=[EB])
            kb.dma("sp", qT[:], cx.QB[h * 64:(h + 1) * 64, :], reads=[cx.QB], writes=[qT])
            kb.dma("sp", kT[:], cx.KB_[h * 64:(h + 1) * 64, :], reads=[cx.KB_], writes=[kT])
            kb.dma("sp", vt[:], cx.VB_t[:, :, h * 64:(h + 1) * 64], reads=[cx.VB_t], writes=[vt])
            def p1(qb):
                nonlocal it
                if qb < 32:
                    keys = list(keys_tab[qb]) + [(32, None), (33, None)]
                else:
                    keys = [(32, None), (33, None)]
                qs = slice(qb * 128, (qb + 1) * 128)
                grp = [keys[i:i + 4] for i in range(0, len(keys), 4)]
                st_ = []
                for gk in grp:
                    ps = pS[it % 4]
                    p_ = pt[it % R]
                    it += 1
                    ng = len(gk)
                    kb.mm([(lambda ps=ps, j=j, kt_i=kt_i: nc.tensor.matmul(
                        ps[:, j * 128:(j + 1) * 128], lhsT=kT[:, kt_i * 128:(kt_i + 1) * 128], rhs=qT[:, qs],
                        start=True, stop=True)) for j, (kt_i, _) in enumerate(gk)],
                        reads=[kT, qT], writes=[ps])
                    kb.op("act", lambda ps=ps, p_=p_, ng=ng: nc.scalar.activation(
                        out=p_[:, :ng, :].rearrange("p h q -> p (h q)"), in_=ps[:, :ng * 128], func=AF.Exp, scale=0.125),
                        reads=[ps], writes=[p_])
                    nloc = sum(1 for (_, pid) in gk if pid is not None)
                    if nloc:
                        pid0 = gk[0][1]
                        eng = "dve" if it % 2 == 0 else "pool"
                        h_ = nc.vector if eng == "dve" else nc.gpsimd
                        kb.op(eng, lambda h_=h_, p_=p_, pid0=pid0, nloc=nloc: h_.tensor_tensor(
                            out=p_[:, :nloc, :], in0=p_[:, :nloc, :], in1=EB[:, pid0:pid0 + nloc, :], op=ALU.mult),
                            reads=[p_, EB], writes=[p_])
                    st_.append((gk, p_))
                return (qb, st_, len(keys))

            def p2(state):
                qb, st_, nk = state
                qs = slice(qb * 128, (qb + 1) * 128)
                a = qb % 2
                po = pO[a]
                pd = pD[a]
                done = 0
                for gk, p_ in st_:
                    fns = []
                    for j, (kt_i, _) in enumerate(gk):
                        first = (done == 0)
                        last = (done == nk - 1)
                        fns.append(lambda po=po, p_=p_, j=j, kt_i=kt_i, first=first, last=last: nc.tensor.matmul(
                            po[:64, 0:128], lhsT=vt[:, kt_i, :], rhs=p_[:, j, :], start=first, stop=last))
                        fns.append(lambda pd=pd, p_=p_, j=j, first=first, last=last: nc.tensor.matmul(
                            pd[:64, 0:128], lhsT=ones64[:], rhs=p_[:, j, :], start=first, stop=last))
                        done += 1
                    kb.mm(fns, reads=[vt, p_, ones64], writes=[po, pd])
                o_ = osb[a]
                kb.op("act", lambda po=po, o_=o_: nc.scalar.copy(out=o_[:, 0:128], in_=po[:64, 0:128]), reads=[po], writes=[o_])
                kb.op("act", lambda pd=pd, o_=o_: nc.scalar.copy(out=o_[:, 128:256], in_=pd[:64, 0:128]), reads=[pd, o_], writes=[o_])
                kb.op("dve", lambda o_=o_: nc.vector.reciprocal(out=o_[:, 128:256], in_=o_[:, 128:256]), reads=[o_], writes=[o_])
                kb.op("pool", lambda o_=o_: nc.gpsimd.tensor_tensor(
                    out=ost[:, qs], in0=o_[:, 0:128], in1=o_[:, 128:256], op=ALU.mult),
                    reads=[o_], writes=[], acc_writes=[ost])

            prev = None
            for qb in range(NT):
                cur = p1(qb)
                if prev is not None:
                    p2(prev)
                prev = cur
                if bg is not None and qb % 2 == 1:
                    next(bg, None)
            p2(prev)
            kb.dma("sp", cx.BR_B[h * 64:(h + 1) * 64, :], ost[:], reads=[ost], writes=[], acc_writes=[cx.BR_B])
        if bg is not None:
            for _ in bg:
                pass

LN_EPS = 1e-5
MB = 512
MBLKS = [(i * MB, min(MB, T - i * MB)) for i in range((T + MB - 1) // MB)]


def ln_setup(cx, kb, nc):
    ones = kb.sbuf("ln_ones", [128, 128], F32)
    kb.op("dve", lambda: nc.vector.memset(ones[:], 1.0 / D), writes=[ones])
    eps = kb.sbuf("ln_eps", [128, 1], F32)
    kb.op("dve", lambda: nc.vector.memset(eps[:], LN_EPS), writes=[eps])
    return ones, eps


def ln_block(kb, nc, r, nb, gam, bet, out, ones, eps, scr):
    rsq, ps1, ps2, mean, var = scr
    kb.op("act", lambda: nc.scalar.activation(out=rsq[:, :, :nb], in_=r[:, :, :nb], func=AF.Square), reads=[r], writes=[rsq])
    kb.mm([(lambda oc=oc: nc.tensor.matmul(ps1[:, :nb], lhsT=ones[:], rhs=r[:, oc, :nb], start=(oc == 0), stop=(oc == 7)))
           for oc in range(8)], reads=[ones, r], writes=[ps1])
    kb.mm([(lambda oc=oc: nc.tensor.matmul(ps2[:, :nb], lhsT=ones[:], rhs=rsq[:, oc, :nb], start=(oc == 0), stop=(oc == 7)))
           for oc in range(8)], reads=[ones, rsq], writes=[ps2])
    kb.op("act", lambda: nc.scalar.copy(out=mean[:, :nb], in_=ps1[:, :nb]), reads=[ps1], writes=[mean])
    kb.op("act", lambda: nc.scalar.copy(out=var[:, :nb], in_=ps2[:, :nb]), reads=[ps2], writes=[var])
    m2 = rsq
    kb.op("dve", lambda: nc.vector.tensor_tensor(out=m2[:, 0, :nb], in0=mean[:, :nb], in1=mean[:, :nb], op=ALU.mult),
          reads=[mean, rsq], writes=[rsq])
    kb.op("dve", lambda: nc.vector.tensor_tensor(out=var[:, :nb], in0=var[:, :nb], in1=m2[:, 0, :nb], op=ALU.subtract),
          reads=[var, rsq], writes=[var])
    kb.op("act", lambda: nc.scalar.activation(out=var[:, :nb], in_=var[:, :nb], func=AF.Sqrt, bias=eps[:]),
          reads=[var, eps], writes=[var])
    kb.op("dve", lambda: nc.vector.reciprocal(out=var[:, :nb], in_=var[:, :nb]), reads=[var], writes=[var])
    for c0, eng in ((0, "dve"), (4, "pool")):
        h_ = nc.vector if eng == "dve" else nc.gpsimd
        kb.op(eng, lambda h_=h_, c0=c0: h_.tensor_tensor(
            out=r[:, c0:c0 + 4, :nb], in0=r[:, c0:c0 + 4, :nb],
            in1=mean[:, :nb].unsqueeze(1).broadcast_to([128, 4, nb]), op=ALU.subtract),
            reads=[r, mean], writes=[], acc_writes=[r])
    for c0, eng in ((0, "dve"), (4, "pool")):
        h_ = nc.vector if eng == "dve" else nc.gpsimd
        kb.op(eng, lambda h_=h_, c0=c0: h_.tensor_tensor(
            out=r[:, c0:c0 + 4, :nb], in0=r[:, c0:c0 + 4, :nb],
            in1=var[:, :nb].unsqueeze(1).broadcast_to([128, 4, nb]), op=ALU.mult),
            reads=[r, var], writes=[], acc_writes=[r])
    for oc in range(8):
        eng = "dve" if oc % 2 == 0 else "pool"
        h_ = nc.vector if eng == "dve" else nc.gpsimd
        kb.op(eng, lambda h_=h_, oc=oc: h_.tensor_scalar(
            out=out[:, oc, :nb], in0=r[:, oc, :nb], scalar1=gam[:, oc:oc + 1], scalar2=bet[:, oc:oc + 1],
            op0=ALU.mult, op1=ALU.add), reads=[r, gam, bet], writes=[out])


def load_ln_params(cx, kb, nc, l, which):
    gam = kb.sbuf("ln_g", [128, 8], F32)
    bet = kb.sbuf("ln_b", [128, 8], F32)
    kb.dma("sp", gam[:], cx.ln_gT[l, which], reads=[cx.ln_gT], writes=[gam])
    kb.dma("sp", bet[:], cx.ln_bT[l, which], reads=[cx.ln_bT], writes=[bet])
    return gam, bet


def stage_merge(cx, kb, nc, l, xsrc, modv):
    with kb.scope():
        ones, eps = ln_setup(cx, kb, nc)
        gam, bet = load_ln_params(cx, kb, nc, l, 0)
        wbr = kb.sbuf("wbr", [128, 4, 4, 1024], BF16)
        wout = kb.sbuf("wout", [128, 8, 1024], BF16)
        for i in range(4):
            kb.dma("pool", wbr[:, i], cx.w_branch[l, i].rearrange("(kc p) c -> p kc c", p=128), reads=[cx.w_branch], writes=[wbr])
        kb.dma("pool", wout[:], cx.w_out[l].rearrange("(kc p) c -> p kc c", p=128), reads=[cx.w_out], writes=[wout])
        wr = kb.sbuf("wr", [128, 8, 36], F32)
        kb.dma("sp", wr[:, :, 0:4], cx.moe_w_group[l].rearrange("(kc p) c -> p kc c", p=128), reads=[cx.moe_w_group], writes=[wr])
        kb.dma("sp", wr[:, :, 4:36], cx.moe_w_expert[l].rearrange("(kc p) c -> p kc c", p=128), reads=[cx.moe_w_expert], writes=[wr])
        rb = kb.sbuf("rb", [128, 36], F32)
        kb.dma("sp", rb[:, 0:4], cx.moe_b_group[l].partition_broadcast(128), reads=[cx.moe_b_group], writes=[rb])
        kb.dma("sp", rb[:, 4:36], cx.moe_b_expert[l].partition_broadcast(128), reads=[cx.moe_b_expert], writes=[rb])
        onep4 = kb.sbuf("onep4", [128, 8, 2], F32)
        kb.op("dve", lambda: nc.vector.tensor_scalar(out=onep4[:], in0=modv[:, 32:40, :], scalar1=1.0, scalar2=None, op0=ALU.add),
              reads=[modv], writes=[onep4])
        brt = [kb.sbuf("brt%d" % i, [128, 4, 4, MB], BF16) for i in range(1)]
        gtt = kb.sbuf("gtt", [128, 32, MB], BF16)
        xb = kb.sbuf("xbm", [128, 8, MB], F32)
        mrg = kb.sbuf("mrg", [128, 8, MB], BF16)
        r = kb.sbuf("rm", [128, 8, MB], F32)
        hx2b = kb.sbuf("hx2b", [128, 8, MB], BF16)
        rsq = kb.sbuf("rsqm", [128, 8, MB], F32)
        x1 = r
        hx2f = rsq
        mean = kb.sbuf("meanm", [128, MB], F32)
        var = kb.sbuf("varm", [128, MB], F32)
        csb = [kb.sbuf("csb%d" % i, [128, MB], F32) for i in range(4)]
        acc = kb.sbuf("accm", [128, MB], F32)
        mixs = kb.sbuf("mixs", [128, MB], F32)
        pbr = [kb.psum("pbr%d" % i, [128, MB]) for i in range(4)]
        pmx = kb.psum("pmx", [128, MB])
        ps1 = kb.psum("ps1m", [128, MB])
        ps2 = kb.psum("ps2m", [128, MB])
        prt = kb.psum("prt", [128, MB])
        brs = [cx.BR_A, cx.BR_B, cx.BR_C, cx.BR_D]
        lgt = kb.sbuf("lgt", [128, 36], F32)
        gmx = kb.sbuf("gmx", [128, 1], F32)
        ngm = kb.sbuf("ngm", [128, 1], F32)
        ge = kb.sbuf("ge", [128, 4], F32)
        gs = kb.sbuf("gsr", [128, 1], F32)
        oh = kb.sbuf("oh", [128, 4], F32)
        lem = kb.sbuf("lem", [128, 4, 8], F32)
        mx8 = kb.sbuf("mx8", [128, 8], F32)
        dd = kb.sbuf("dd", [128, 1], F32)
        w1 = kb.sbuf("w1", [128, 1], F32)
        w2 = kb.sbuf("w2", [128, 1], F32)
        tt1 = kb.sbuf("tt1", [128, 32], F32)
        tt2 = kb.sbuf("tt2", [128, 32], F32)
        wgt = kb.sbuf("wgt", [128, 32], F32)
        for bi, (t0, nb) in enumerate(MBLKS):
            col = blkcol(t0)
            b_ = brt[0]
            for i in range(4):
                kb.dma("sp", b_[:, i, :, :nb], brs[i][:, t0:t0 + nb].rearrange("(kc p) t -> p kc t", p=128),
                       reads=[brs[i]], writes=[b_])
            kb.dma("sp", gtt[:, :, :nb], cx.GATE[:, t0:t0 + nb].rearrange("(j p) t -> p j t", p=128), reads=[cx.GATE], writes=[gtt])
            kb.dma("sp", xb[:, :, :nb], xsrc[:, t0:t0 + nb].rearrange("(kc p) t -> p kc t", p=128), reads=[xsrc], writes=[xb])
            for oc in range(8):
                for i in range(4):
                    kb.mm([(lambda i=i, kc=kc, oc=oc: nc.tensor.matmul(
                        pbr[i][:, :nb], lhsT=wbr[:, i, kc, oc * 128:(oc + 1) * 128], rhs=b_[:, i, kc, :nb],
                        start=(kc == 0), stop=(kc == 3))) for kc in range(4)], reads=[wbr, b_], writes=[pbr[i]])
                    kb.op("act", lambda i=i: nc.scalar.copy(out=csb[i][:, :nb], in_=pbr[i][:, :nb]), reads=[pbr[i]], writes=[csb[i]])
                    eng = "dve" if i % 2 == 0 else "pool"
                    h_ = nc.vector if eng == "dve" else nc.gpsimd
                    kb.op(eng, lambda h_=h_, i=i, oc=oc: h_.tensor_tensor(
                        out=csb[i][:, :nb], in0=csb[i][:, :nb], in1=gtt[:, i * 8 + oc, :nb], op=ALU.mult),
                        reads=[csb[i], gtt], writes=[csb[i]])
                kb.op("dve", lambda: nc.vector.tensor_tensor(out=acc[:, :nb], in0=csb[0][:, :nb], in1=csb[1][:, :nb], op=ALU.add),
                      reads=[csb[0], csb[1]], writes=[acc])
                kb.op("pool", lambda: nc.gpsimd.tensor_tensor(out=csb[2][:, :nb], in0=csb[2][:, :nb], in1=csb[3][:, :nb], op=ALU.add),
                      reads=[csb[2], csb[3]], writes=[csb[2]])
                kb.op("dve", lambda oc=oc: nc.vector.tensor_tensor(out=mrg[:, oc, :nb], in0=acc[:, :nb], in1=csb[2][:, :nb], op=ALU.add),
                      reads=[acc, csb[2]], writes=[mrg])
            for oc in range(8):
                kb.mm([(lambda kc=kc, oc=oc: nc.tensor.matmul(
                    pmx[:, :nb], lhsT=wout[:, kc, oc * 128:(oc + 1) * 128], rhs=mrg[:, kc, :nb],
                    start=(kc == 0), stop=(kc == 7))) for kc in range(8)], reads=[wout, mrg], writes=[pmx])
                kb.op("act", lambda: nc.scalar.copy(out=mixs[:, :nb], in_=pmx[:, :nb]), reads=[pmx], writes=[mixs])
                kb.op("pool", lambda oc=oc: nc.gpsimd.tensor_scalar(
                    out=mixs[:, :nb], in0=mixs[:, :nb], scalar1=modv[:, 16 + oc, col:col + 1], scalar2=None, op0=ALU.mult),
                    reads=[mixs, modv], writes=[mixs])
                kb.op("dve", lambda oc=oc: nc.vector.scalar_tensor_tensor(
                    out=r[:, oc, :nb], in0=xb[:, oc, :nb], scalar=ALPHA, in1=mixs[:, :nb], op0=ALU.mult, op1=ALU.add),
                    reads=[xb, mixs], writes=[r])
            ln_block(kb, nc, r, nb, gam, bet, x1, ones, eps, (rsq, ps1, ps2, mean, var))
            kb.dma("sp", cx.XT[:, t0:t0 + nb].rearrange("(kc p) t -> p kc t", p=128), x1[:, :, :nb],
                   reads=[x1], writes=[], acc_writes=[cx.XT])
            for oc in range(8):
                eng = "dve" if oc % 2 == 0 else "pool"
                h_ = nc.vector if eng == "dve" else nc.gpsimd
                kb.op(eng, lambda h_=h_, oc=oc: h_.tensor_scalar(
                    out=hx2f[:, oc, :nb], in0=x1[:, oc, :nb], scalar1=onep4[:, oc, col:col + 1],
                    scalar2=modv[:, 24 + oc, col:col + 1], op0=ALU.mult, op1=ALU.add),
                    reads=[x1, onep4, modv], writes=[hx2f])
            kb.op("act", lambda: nc.scalar.copy(out=hx2b[:, :, :nb], in_=hx2f[:, :, :nb]), reads=[hx2f], writes=[hx2b])
            kb.dma("sp", cx.HX2[:, t0:t0 + nb].rearrange("(kc p) t -> p kc t", p=128), hx2b[:, :, :nb],
                   reads=[hx2b], writes=[], acc_writes=[cx.HX2])
            for tj in range(nb // 128):
                ti = t0 // 128 + tj
                tsl = slice(tj * 128, (tj + 1) * 128)
                kb.mm([(lambda kc=kc: nc.tensor.matmul(prt[:, 0:36], lhsT=hx2f[:, kc, tsl], rhs=wr[:, kc, :],
                                                       start=(kc == 0), stop=(kc == 7))) for kc in range(8)],
                      reads=[hx2f, wr], writes=[prt])
                kb.op("act", lambda: nc.scalar.copy(out=lgt[:], in_=prt[:, 0:36]), reads=[prt], writes=[lgt])
                kb.op("dve", lambda: nc.vector.tensor_tensor(out=lgt[:], in0=lgt[:], in1=rb[:], op=ALU.add), reads=[lgt, rb], writes=[lgt])
                kb.op("dve", lambda: nc.vector.tensor_reduce(out=gmx[:], in_=lgt[:, 0:4], axis=AX.X, op=ALU.max), reads=[lgt], writes=[gmx])
                kb.op("dve", lambda: nc.vector.tensor_scalar(out=ngm[:], in0=gmx[:], scalar1=-1.0, scalar2=None, op0=ALU.mult), reads=[gmx], writes=[ngm])
                kb.op("act", lambda: nc.scalar.activation(out=ge[:], in_=lgt[:, 0:4], func=AF.Exp, bias=ngm[:]), reads=[lgt, ngm], writes=[ge])
                kb.op("dve", lambda: nc.vector.tensor_reduce(out=gs[:], in_=ge[:], axis=AX.X, op=ALU.add), reads=[ge], writes=[gs])
                kb.op("dve", lambda: nc.vector.reciprocal(out=gs[:], in_=gs[:]), reads=[gs], writes=[gs])
                kb.op("dve", lambda: nc.vector.tensor_scalar(out=oh[:], in0=lgt[:, 0:4], scalar1=gmx[:], scalar2=None, op0=ALU.is_equal), reads=[lgt, gmx], writes=[oh])
                kb.op("dve", lambda: nc.vector.tensor_scalar(out=oh[:], in0=oh[:], scalar1=-1.0, scalar2=1e30, op0=ALU.add, op1=ALU.mult), reads=[oh], writes=[oh])
                kb.op("dve", lambda: nc.vector.tensor_tensor(
                    out=lem[:], in0=lgt[:, 4:36].rearrange("p (g e) -> p g e", e=8), in1=oh[:].unsqueeze(2).broadcast_to([128, 4, 8]), op=ALU.add),
                    reads=[lgt, oh], writes=[lem])
                lemf = lem[:].rearrange("p g e -> p (g e)")
                kb.op("dve", lambda: nc.vector.max(out=mx8[:], in_=lem[:].rearrange("p g e -> p (g e)")), reads=[lem], writes=[mx8])
                kb.op("dve", lambda: nc.vector.tensor_tensor(out=dd[:], in0=mx8[:, 1:2], in1=mx8[:, 0:1], op=ALU.subtract), reads=[mx8], writes=[dd])
                kb.op("act", lambda: nc.scalar.activation(out=dd[:], in_=dd[:], func=AF.Exp), reads=[dd], writes=[dd])
                kb.op("dve", lambda: nc.vector.tensor_scalar(out=w1[:], in0=dd[:], scalar1=1.0, scalar2=None, op0=ALU.add), reads=[dd], writes=[w1])
                kb.op("dve", lambda: nc.vector.reciprocal(out=w1[:], in_=w1[:]), reads=[w1], writes=[w1])
                kb.op("dve", lambda: nc.vector.tensor_tensor(out=w2[:], in0=dd[:], in1=w1[:], op=ALU.mult), reads=[dd, w1], writes=[w2])
                kb.op("dve", lambda: nc.vector.tensor_scalar(
                    out=tt1[:], in0=lem[:].rearrange("p g e -> p (g e)"), scalar1=mx8[:, 0:1], scalar2=w1[:], op0=ALU.is_equal, op1=ALU.mult),
                    reads=[lem, mx8, w1], writes=[tt1])
                kb.op("dve", lambda: nc.vector.tensor_scalar(
                    out=tt2[:], in0=lem[:].rearrange("p g e -> p (g e)"), scalar1=mx8[:, 1:2], scalar2=w2[:], op0=ALU.is_equal, op1=ALU.mult),
                    reads=[lem, mx8, w2], writes=[tt2])
                kb.op("dve", lambda: nc.vector.tensor_tensor(out=tt1[:], in0=tt1[:], in1=tt2[:], op=ALU.add), reads=[tt1, tt2], writes=[tt1])
                kb.op("dve", lambda: nc.vector.tensor_scalar(out=wgt[:], in0=tt1[:], scalar1=gs[:], scalar2=None, op0=ALU.mult), reads=[tt1, gs], writes=[wgt])
                kb.dma("sp", cx.WG_t[:, ti, :], wgt[:], reads=[wgt], writes=[], acc_writes=[cx.WG_t])


def stage_moe(cx, kb, nc, l, modv, yout=None, precast=True):
    with kb.scope():
        ones, eps = ln_setup(cx, kb, nc)
        gam, bet = load_ln_params(cx, kb, nc, l, 1)
        wgT = kb.sbuf("wgT", [32, T], F32)
        sel = kb.sbuf("sel", [32, 32, 128], F32)
        kb.dma("sp", sel[:], cx.m_sel[:], reads=[cx.m_sel], writes=[sel])
        ptr = [kb.psum("ptr%d" % i, [128, 512]) for i in range(2)]
        with kb.scope():
            wgl = kb.sbuf("wgl", [128, NT, 32], F32)
            id32 = kb.sbuf("id32", [128, 128], F32)
            kb.dma("sp", wgl[:], cx.WG_t[:], reads=[cx.WG_t], writes=[wgl])
            kb.dma("sp", id32[:], cx.m_ident32[:], reads=[cx.m_ident32], writes=[id32])
            for ti in range(NT):
                p_ = ptr[ti % 2]
                kb.mm([lambda p_=p_, ti=ti: nc.tensor.matmul(p_[:32, 0:128], lhsT=wgl[:, ti, :], rhs=id32[:], start=True, stop=True)],
                      reads=[wgl, id32], writes=[p_])
                kb.op("act", lambda p_=p_, ti=ti: nc.scalar.copy(out=wgT[:, ti * 128:(ti + 1) * 128], in_=p_[:32, 0:128]),
                      reads=[p_], writes=[], acc_writes=[wgT])
        if precast:
            precast_expert_weights(cx, kb, nc, l)
        hx = kb.sbuf("hxm", [128, 8, 512], BF16)
        yacc = kb.sbuf("yacc", [128, 8, 512], F32)
        hid = kb.sbuf("hid", [128, 16, 512], BF16)
        wdg = kb.sbuf("wdg", [128, 8, 2, 1024], BF16)
        NWB = 4
        wgs = [kb.sbuf("wgs%d" % i, [128, 8, 256], BF16) for i in range(NWB)]
        wus = [kb.sbuf("wus%d" % i, [128, 8, 256], BF16) for i in range(NWB)]
        xb = kb.sbuf("xbe", [128, 8, 512], F32)
        rsq = kb.sbuf("rsqe", [128, 8, 512], F32)
        mean = kb.sbuf("meane", [128, 512], F32)
        var = kb.sbuf("vare", [128, 512], F32)
        reps = [kb.sbuf("reps%d" % i, [128, 512], BF16) for i in range(2)]
        ssb = [kb.sbuf("ssb%d" % i, [128, 512], BF16) for i in range(3)]
        ytmp = [kb.sbuf("ytmp%d" % i, [128, 512], F32) for i in range(2)]
        NPS = 3
        pg = [kb.psum("pg%d" % i, [128, 512]) for i in range(NPS)]
        pu = [kb.psum("pu%d" % i, [128, 512]) for i in range(NPS)]
        py = ptr
        ps1 = pg[0]
        ps2 = pu[0]
        prep = ptr[1]
        nw = 0
        it = 0
        for bi, (t0, nb) in enumerate(BLKS):
            col = blkcol(t0)
            kb.dma("sp", hx[:, :, :nb], cx.HX2[:, t0:t0 + nb].rearrange("(kc p) t -> p kc t", p=128), reads=[cx.HX2], writes=[hx])
            kb.dma("sp", xb[:, :, :nb], cx.XT[:, t0:t0 + nb].rearrange("(kc p) t -> p kc t", p=128), reads=[cx.XT], writes=[xb])
            for g in range(4):
                kb.dma("sp", wdg[:].rearrange("p e hc d -> p (e hc d)"), cx.WDB[g], reads=[cx.WDB], writes=[wdg])
                for e_ in range(8):
                    e = g * 8 + e_
                    wg_, wu_ = wgs[nw % NWB], wus[nw % NWB]
                    nw += 1
                    kb.dma("sp", wg_[:].rearrange("p kc f -> p (kc f)"), cx.WGB[e], reads=[cx.WGB], writes=[wg_])
                    kb.dma("act", wu_[:].rearrange("p kc f -> p (kc f)"), cx.WUB[e], reads=[cx.WUB], writes=[wu_])
                    rp = reps[e % 2]
                    kb.mm([lambda e=e: nc.tensor.matmul(prep[:, :nb], lhsT=sel[:, e, :], rhs=wgT[:, t0:t0 + nb], start=True, stop=True)],
                          reads=[sel, wgT], writes=[prep])
                    kb.op("act", lambda rp=rp: nc.scalar.copy(out=rp[:, :nb], in_=prep[:, :nb]), reads=[prep], writes=[rp])
                    for hc in range(2):
                        a = it % NPS
                        it += 1
                        kb.mm([(lambda kc=kc, a=a, hc=hc, wg_=wg_: nc.tensor.matmul(
                            pg[a][:, :nb], lhsT=wg_[:, kc, hc * 128:(hc + 1) * 128], rhs=hx[:, kc, :nb],
                            start=(kc == 0), stop=(kc == 7))) for kc in range(8)], reads=[wg_, hx], writes=[pg[a]])
                        kb.mm([(lambda kc=kc, a=a, hc=hc, wu_=wu_: nc.tensor.matmul(
                            pu[a][:, :nb], lhsT=wu_[:, kc, hc * 128:(hc + 1) * 128], rhs=hx[:, kc, :nb],
                            start=(kc == 0), stop=(kc == 7))) for kc in range(8)], reads=[wu_, hx], writes=[pu[a]])
                        kb.op("act", lambda a=a: nc.scalar.activation(out=ssb[a][:, :nb], in_=pg[a][:, :nb], func=AF.Silu),
                              reads=[pg[a]], writes=[ssb[a]])
                        kb.op("pool", lambda a=a, rp=rp: nc.gpsimd.tensor_tensor(
                            out=ssb[a][:, :nb], in0=ssb[a][:, :nb], in1=rp[:, :nb], op=ALU.mult),
                            reads=[ssb[a], rp], writes=[ssb[a]])
                        kb.op("dve", lambda a=a, e_=e_, hc=hc: nc.vector.tensor_tensor(
                            out=hid[:, e_ * 2 + hc, :nb], in0=pu[a][:, :nb], in1=ssb[a][:, :nb], op=ALU.mult),
                            reads=[pu[a], ssb[a]], writes=[], acc_writes=[hid])
                for oc in range(8):
                    p_ = py[oc % 2]
                    fns = []
                    for j in range(16):
                        fns.append(lambda j=j, oc=oc, p_=p_: nc.tensor.matmul(
                            p_[:, :nb], lhsT=wdg[:, j // 2, j % 2, oc * 128:(oc + 1) * 128], rhs=hid[:, j, :nb],
                            start=(j == 0), stop=(j == 15)))
                    kb.mm(fns, reads=[wdg, hid], writes=[p_])
                    if g == 0:
                        kb.op("act", lambda oc=oc, p_=p_: nc.scalar.copy(out=yacc[:, oc, :nb], in_=p_[:, :nb]), reads=[p_], writes=[], acc_writes=[yacc])
                    else:
                        yt = ytmp[oc % 2]
                        kb.op("act", lambda yt=yt, p_=p_: nc.scalar.copy(out=yt[:, :nb], in_=p_[:, :nb]), reads=[p_], writes=[yt])
                        eng = "dve" if oc % 2 == 0 else "pool"
                        h_ = nc.vector if eng == "dve" else nc.gpsimd
                        kb.op(eng, lambda h_=h_, oc=oc, yt=yt: h_.tensor_tensor(
                            out=yacc[:, oc, :nb], in0=yacc[:, oc, :nb], in1=yt[:, :nb], op=ALU.add),
                            reads=[yt, yacc], writes=[], acc_writes=[yacc])
            for oc in range(8):
                kb.op("pool", lambda oc=oc: nc.gpsimd.tensor_scalar(
                    out=yacc[:, oc, :nb], in0=yacc[:, oc, :nb], scalar1=modv[:, 40 + oc, col:col + 1], scalar2=None, op0=ALU.mult),
                    reads=[yacc, modv], writes=[yacc])
                kb.op("dve", lambda oc=oc: nc.vector.scalar_tensor_tensor(
                    out=yacc[:, oc, :nb], in0=xb[:, oc, :nb], scalar=ALPHA, in1=yacc[:, oc, :nb], op0=ALU.mult, op1=ALU.add),
                    reads=[xb, yacc], writes=[yacc])
            ln_block(kb, nc, yacc, nb, gam, bet, yacc, ones, eps, (rsq, ps1, ps2, mean, var))
            kb.dma("sp", cx.XT[:, t0:t0 + nb].rearrange("(kc p) t -> p kc t", p=128), yacc[:, :, :nb],
                   reads=[yacc], writes=[], acc_writes=[cx.XT])
            if yout is not None and t0 < TL:
                kb.dma("sp", yout[:, t0:t0 + nb].rearrange("(kc p) t -> p kc t", p=128), yacc[:, :, :nb],
                       reads=[yacc], writes=[], acc_writes=[yout])


def precast_expert_weights(cx, kb, nc, l):
    with kb.scope():
        stw = [kb.sbuf("pcw%d" % i, [128, 8, 256], BF16) for i in range(4)]
        std = [kb.sbuf("pcd%d" % i, [128, 16, 1024], BF16) for i in range(2)]
        n = 0
        for g in range(4):
            d_ = std[g % 2]
            kb.dma("pool", d_[:], cx.moe_w_down[l, g].rearrange("e (hc p) d -> p (e hc) d", p=128), reads=[cx.moe_w_down], writes=[d_])
            kb.dma("sp", cx.WDB[g], d_[:].rearrange("p j d -> p (j d)"), reads=[d_], writes=[], acc_writes=[cx.WDB])
            for e_ in range(8):
                e = g * 8 + e_
                for src, dst in ((cx.moe_w_gate, cx.WGB), (cx.moe_w_up, cx.WUB)):
                    w_ = stw[n % 4]
                    n += 1
                    kb.dma("pool", w_[:], src[l, g, e_].rearrange("(kc p) f -> p kc f", p=128), reads=[src], writes=[w_])
                    kb.dma("sp", dst[e], w_[:].rearrange("p kc f -> p (kc f)"), reads=[w_], writes=[], acc_writes=[dst])


def precast_gen(cx, kb, nc, l, stw, std):
    pend = []
    n = 0
    for g in range(4):
        d_ = std[g % 2]
        kb.dma("pool", d_[:], cx.moe_w_down[l, g].rearrange("e (hc p) d -> p (e hc) d", p=128), reads=[cx.moe_w_down], writes=[d_])
        pend.append((cx.WDB, g, d_, "p j d -> p (j d)"))
        yield
        for e_ in range(8):
            e = g * 8 + e_
            for src, dst in ((cx.moe_w_gate, cx.WGB), (cx.moe_w_up, cx.WUB)):
                w_ = stw[n % len(stw)]
                n += 1
                kb.dma("pool", w_[:], src[l, g, e_].rearrange("(kc p) f -> p kc f", p=128), reads=[src], writes=[w_])
                pend.append((dst, e, w_, "p kc f -> p (kc f)"))
                yield
                while len(pend) > 2:
                    dst_, idx_, buf_, pat_ = pend.pop(0)
                    kb.dma("sp", dst_[idx_], buf_[:].rearrange(pat_), reads=[buf_], writes=[], acc_writes=[dst_])
    while pend:
        dst_, idx_, buf_, pat_ = pend.pop(0)
        kb.dma("sp", dst_[idx_], buf_[:].rearrange(pat_), reads=[buf_], writes=[], acc_writes=[dst_])
        yield


def rope_tables():
    t = np.arange(TL)
    row = (t // 64).astype(np.float32)
    colp = (t % 64).astype(np.float32)
    nf = 16
    inv = (np.float32(10000.0) ** (-np.arange(nf, dtype=np.float32) / np.float32(nf))).astype(np.float32)
    cos = np.ones((128, T), np.float32)
    sin = np.zeros((128, T), np.float32)
    for q in range(128):
        d = q % 64
        pos = row if d < 32 else colp
        ang = (pos * inv[d % 16]).astype(np.float32)
        cos[q, :TL] = np.cos(ang).astype(np.float32)
        sin[q, :TL] = np.sin(ang).astype(np.float32)
    pt = np.zeros((128, 128), np.float32)
    for m in range(128):
        if (m % 32) < 16:
            pt[m + 16, m] = -1.0
        else:
            pt[m - 16, m] = 1.0
    return cos, sin, pt


_CONST = {}
_NAIDX = {}


def consts():
    if not _CONST:
        cos, sin, pt = rope_tables()
        _CONST.update(rope_cos=cos, rope_sin=sin, rope_pt=pt)
        _CONST.update(scan_masks())
        _CONST.update(wa_masks())
        sel = np.zeros((32, 32, 128), np.float32)
        for e in range(32):
            sel[e, e, :] = 1.0
        _CONST["m_sel"] = sel
        _CONST["m_ident32"] = np.eye(128, dtype=np.float32)
        _k, _valid, _ri, _ci = na_consts()
        _CONST["m_navalid"] = _valid
        _NAIDX["ri"] = _ri
        _NAIDX["ci"] = _ci
    return _CONST


def core_inputs(inp, b):
    f = lambda a: np.ascontiguousarray(a, dtype=np.float32)
    m = {}
    m["xin"] = f(np.concatenate([inp["x"][b].T, inp["ctx"][b].T], axis=1))
    cv = np.stack([inp["c"][b].reshape(8, 128).T, inp["c_ctx"].reshape(8, 128).T], axis=2)
    m["cvec"] = f(cv)
    m["w_mod"] = f(inp["w_mod"])
    m["b_modT"] = f(inp["b_mod"].reshape(2, 48, 128).transpose(0, 2, 1))
    m["w_in"] = f(inp["w_in"])
    m["hg_gamma"] = f(inp["hg_gamma"])
    m["gla_lr_w"] = f(inp["gla_lr_w"])
    m["gla_lr_b"] = f(inp["gla_lr_b"])
    m["wa_sink"] = f(inp["wa_sink"])
    for k in ("w_branch", "w_out", "moe_w_group", "moe_b_group", "moe_w_expert", "moe_b_expert",
              "moe_w_gate", "moe_w_up", "moe_w_down"):
        m[k] = f(inp[k])
    m["ln_gT"] = f(inp["ln_g"].reshape(2, 2, 8, 128).transpose(0, 1, 3, 2))
    m["ln_bT"] = f(inp["ln_b"].reshape(2, 2, 8, 128).transpose(0, 1, 3, 2))
    consts()
    m["na_bias"] = f(inp["na_rpb"][:, :, _NAIDX["ri"], _NAIDX["ci"]])
    m.update(consts())
    return m


FUSED = 1


def build_program(layers, final):
    nc = bass.Bass("TRN2", target_bir_lowering=False)
    cx = Ctx()
    declare_inputs(cx, nc)
    if final:
        yout = Buf(nc.dram_tensor("yout", [D, TL], F32, kind="ExternalOutput"), "yout")
    else:
        yout = None
    with ExitStack() as st:
        kb = KB(nc, st)
        declare_scratch(cx, kb, debug=False, as_output=(() if final else ("XT",)))
        modT = [kb.sbuf("modT%d" % l, [128, 48, 2], F32) for l in range(2)]
        stage_mod(cx, kb, nc, modT)
        first = True
        for l in layers:
            xsrc = cx.xin if first else cx.XT
            first = False
            with kb.scope():
                lbt = [kb.sbuf("lbt%d" % i, [128, 1024], F32) for i in range(2)]
                omlt = [kb.sbuf("omlt%d" % i, [128, 1024], F32) for i in range(2)]
                stage_lb(cx, kb, nc, lbt, omlt)
                hxT = kb.sbuf("hxT", [128, 8, T], BF16)
                stage_modulate(cx, kb, nc, xsrc, modT[l], 0, 1, hxT)
                stage_inproj_fm(cx, kb, nc, l, hxT)
                stage_inproj_tm(cx, kb, nc, l, hxT, lbt[l], omlt[l])
            stage_gla_gate(cx, kb, nc, l)
            stage_scan(cx, kb, nc, "gla")
            stage_scan(cx, kb, nc, "hgrn")
            stage_wa(cx, kb, nc, l)
            stage_na(cx, kb, nc, l, bg_factory=(lambda stw, std, l=l: precast_gen(cx, kb, nc, l, stw, std)))
            stage_merge(cx, kb, nc, l, xsrc, modT[l])
            stage_moe(cx, kb, nc, l, modT[l], yout if (final and l == layers[-1]) else None, precast=False)
        if final:
            kb.finish([yout])
        else:
            kb.finish([cx.XT])
    return nc


def kernel(**inputs):
    inp = {k: np.asarray(v) for k, v in inputs.items()}
    in_maps = [core_inputs(inp, b) for b in range(4)]
    if FUSED:
        nc = build_program([0, 1], True)
        res = run_bass_kernel_spmd(nc, in_maps, core_ids=list(range(4)))
    else:
        nc0 = build_program([0], False)
        res0 = run_bass_kernel_spmd(nc0, in_maps, core_ids=list(range(4)))
        for b in range(4):
            in_maps[b]["xin"] = np.ascontiguousarray(res0.results[b]["XT"])
        nc1 = build_program([1], True)
        res = run_bass_kernel_spmd(nc1, in_maps, core_ids=list(range(4)))
    out = np.stack([np.ascontiguousarray(res.results[b]["yout"].T) for b in range(4)], axis=0)
    return out.astype(np.float32)
```
